# Optimizing a Trainium2 kernel written in Bass

```python
import jax, jax.numpy as jnp
from jax import lax
import numpy as np

D_MODEL = 1024
BATCH = 4
SEQ = 4096
DEPTH = 1

PLE_DIM = 256
MLA_HEADS = 8
MLA_NOPE_DIM = 64
MLA_ROPE_DIM = 32
MLA_V_DIM = 64
Q_LORA_RANK = 384
KV_LORA_RANK = 256
SB_HEADS = 8
SB_HEAD_DIM = 64
MLA_WIDTH = MLA_HEADS * MLA_V_DIM
SB_WIDTH = SB_HEADS * SB_HEAD_DIM
MIX_WIDTH = MLA_WIDTH + SB_WIDTH
IN_SPLITS = (Q_LORA_RANK,
             Q_LORA_RANK + KV_LORA_RANK,
             Q_LORA_RANK + KV_LORA_RANK + MLA_ROPE_DIM,
             Q_LORA_RANK + KV_LORA_RANK + MLA_ROPE_DIM + SB_WIDTH,
             Q_LORA_RANK + KV_LORA_RANK + MLA_ROPE_DIM + 2 * SB_WIDTH)
IN_COLS = Q_LORA_RANK + KV_LORA_RANK + MLA_ROPE_DIM + 3 * SB_WIDTH
ROPE_THETA = 10000.0
Q_BLOCK = 128
N_EXPERTS = 32
TOP_K = 4
D_FF = 1024
SWIGLU_LIMIT = 7.0
SWIGLU_ALPHA = 1.702
EXPERT_BLOCK = 128
RMS_EPS = 1e-6
MAX_POS_OFFSET = 4096

kernel_name = "hybrid_mla_stickbreak_moe_ple"


def rmsnorm(x, g):
    xf = x.astype(jnp.float32)
    y = xf * lax.rsqrt(jnp.mean(xf * xf, axis=-1, keepdims=True) + RMS_EPS)
    return (y * g.astype(jnp.float32)).astype(x.dtype)


def rope_tables(positions):
    inv_freq = ROPE_THETA ** (-jnp.arange(0, MLA_ROPE_DIM, 2, dtype=jnp.float32) / MLA_ROPE_DIM)
    ang = positions.astype(jnp.float32)[..., None] * inv_freq
    return jnp.cos(ang), jnp.sin(ang)


def apply_rope(x, cos, sin):
    xf = x.astype(jnp.float32)
    x1, x2 = jnp.split(xf, 2, axis=-1)
    return jnp.concatenate([x1 * cos - x2 * sin, x2 * cos + x1 * sin], axis=-1).astype(x.dtype)


def to_blocks(a):
    b, h, s, d = a.shape
    return jnp.moveaxis(a.reshape(b, h, s // Q_BLOCK, Q_BLOCK, d), 2, 0)


def from_blocks(o):
    n, b, h, qb, d = o.shape
    return jnp.moveaxis(o, 0, 2).reshape(b, h, n * qb, d).transpose(0, 2, 1, 3).reshape(b, n * qb, h * d)


def mixer_heads(q_nope, q_rope, k_nope, k_rope, v_mla, q_sb, k_sb, v_sb):
    seq = k_nope.shape[2]
    n_blk = seq // Q_BLOCK
    key_pos = jnp.arange(seq)
    mla_scale = (MLA_NOPE_DIM + MLA_ROPE_DIM) ** -0.5
    sb_scale = SB_HEAD_DIM ** -0.5

    def block(args):
        bi, qn, qr, qs = args
        q_pos = bi * Q_BLOCK + jnp.arange(Q_BLOCK)
        s = (jnp.einsum('bhqd,bhkd->bhqk', qn, k_nope, preferred_element_type=jnp.float32)
             + jnp.einsum('bhqr,bkr->bhqk', qr, k_rope, preferred_element_type=jnp.float32)) * mla_scale
        causal = key_pos[None, :] <= q_pos[:, None]
        w = jax.nn.softmax(jnp.where(causal, s, -jnp.inf), axis=-1)
        o_m = jnp.einsum('bhqk,bhkd->bhqd', w.astype(v_mla.dtype), v_mla)
        z = jnp.einsum('bhqd,bhkd->bhqk', qs, k_sb, preferred_element_type=jnp.float32) * sb_scale
        strict = key_pos[None, :] < q_pos[:, None]
        log_not = jnp.where(strict, jax.nn.log_sigmoid(-z), 0.0)
        after = lax.cumsum(log_not, axis=3, reverse=True) - log_not
        log_a = jnp.where(strict, jax.nn.log_sigmoid(z) + after, -jnp.inf)
        o_s = jnp.einsum('bhqk,bhkd->bhqd', jnp.exp(log_a).astype(v_sb.dtype), v_sb)
        return o_m, o_s

    o_m, o_s = lax.map(block, (jnp.arange(n_blk), to_blocks(q_nope), to_blocks(q_rope), to_blocks(q_sb)))
    return from_blocks(o_m), from_blocks(o_s)


def moe(u, w_router, b_router, w_gu, b_gu, w_dn, b_dn):
    b, s, d = u.shape
    t = b * s
    xt = u.reshape(t, d)
    logits = jnp.dot(xt, w_router, preferred_element_type=jnp.float32) + b_router.astype(jnp.float32)
    top_logit, top_e = lax.top_k(logits, TOP_K)
    gate = jax.nn.softmax(top_logit, axis=-1)
    n_pairs = t * TOP_K
    flat_e = top_e.reshape(-1)
    flat_tok = jnp.arange(n_pairs, dtype=jnp.int32) // TOP_K
    flat_g = gate.reshape(-1)
    order = jnp.argsort(flat_e)
    sorted_e = flat_e[order]
    counts = jnp.bincount(flat_e, length=N_EXPERTS)
    padded = (counts + EXPERT_BLOCK - 1) // EXPERT_BLOCK * EXPERT_BLOCK
    pad_end = jnp.cumsum(padded)
    pad_start = pad_end - padded
    start = jnp.cumsum(counts) - counts
    dest = pad_start[sorted_e] + jnp.arange(n_pairs) - start[sorted_e]
    n_blocks = -(-n_pairs // EXPERT_BLOCK) + N_EXPERTS
    n_rows = n_blocks * EXPERT_BLOCK
    row_tok = jnp.full((n_rows,), t, jnp.int32).at[dest].set(flat_tok[order])
    row_gate = jnp.zeros((n_rows,), jnp.float32).at[dest].set(flat_g[order])
    block_e = jnp.minimum(jnp.searchsorted(pad_end, jnp.arange(n_blocks) * EXPERT_BLOCK, side='right'),
                          N_EXPERTS - 1)
    x_pad = jnp.concatenate([xt, jnp.zeros((1, d), xt.dtype)], axis=0)
    xb = x_pad[row_tok].reshape(n_blocks, EXPERT_BLOCK, d)

    def expert_block(args):
        e, xs = args
        hh = jnp.dot(xs, w_gu[e]) + b_gu[e]
        glu, lin = jnp.split(hh, 2, axis=-1)
        glu = jnp.minimum(glu, SWIGLU_LIMIT)
        lin = jnp.clip(lin, -SWIGLU_LIMIT, SWIGLU_LIMIT)
        act = glu * jax.nn.sigmoid(SWIGLU_ALPHA * glu) * (lin + 1.0)
        return jnp.dot(act, w_dn[e]) + b_dn[e]

    yb = lax.map(expert_block, (block_e, xb))
    yr = yb.reshape(n_rows, d) * row_gate[:, None].astype(yb.dtype)
    y = jax.ops.segment_sum(yr, row_tok, num_segments=t + 1)[:t]
    return y.reshape(b, s, d)


def setup_inputs(seed: int = 0) -> dict:
    key = jax.random.key(seed)
    ks = jax.random.split(key, 24)

    def nrm(k, shape, scale):
        return jax.random.normal(k, shape, jnp.float32) * scale

    def gain(k, shape):
        return 1.0 + 0.05 * jax.random.normal(k, shape, jnp.float32)

    offsets = jax.random.randint(ks[2], (BATCH, 1), 0, MAX_POS_OFFSET, dtype=jnp.int32)
    positions = offsets + jnp.arange(SEQ, dtype=jnp.int32)[None, :]
    return {
        'x': nrm(ks[0], (BATCH, SEQ, D_MODEL), 1.0),
        'p': nrm(ks[1], (DEPTH, BATCH, SEQ, PLE_DIM), 1.0),
        'positions': positions,
        'w_in': nrm(ks[3], (DEPTH, D_MODEL, IN_COLS), D_MODEL ** -0.5),
        'g_attn': gain(ks[4], (DEPTH, D_MODEL)),
        'g_cq': gain(ks[5], (DEPTH, Q_LORA_RANK)),
        'w_uq': nrm(ks[6], (DEPTH, Q_LORA_RANK, MLA_HEADS * (MLA_NOPE_DIM + MLA_ROPE_DIM)), Q_LORA_RANK ** -0.5),
        'g_ckv': gain(ks[7], (DEPTH, KV_LORA_RANK)),
        'w_ukv': nrm(ks[8], (DEPTH, KV_LORA_RANK, MLA_HEADS * (MLA_NOPE_DIM + MLA_V_DIM)), KV_LORA_RANK ** -0.5),
        'g_out_mla': gain(ks[9], (DEPTH, MLA_WIDTH)),
        'g_out_sb': gain(ks[10], (DEPTH, SB_WIDTH)),
        'w_o': nrm(ks[11], (DEPTH, MIX_WIDTH, D_MODEL), MIX_WIDTH ** -0.5),
        'g_moe': gain(ks[12], (DEPTH, D_MODEL)),
        'w_router': nrm(ks[13], (DEPTH, D_MODEL, N_EXPERTS), D_MODEL ** -0.5),
        'b_router': nrm(ks[14], (DEPTH, N_EXPERTS), 0.01),
        'w_gu': nrm(ks[15], (DEPTH, N_EXPERTS, D_MODEL, 2 * D_FF), D_MODEL ** -0.5),
        'b_gu': nrm(ks[16], (DEPTH, N_EXPERTS, 2 * D_FF), 0.01),
        'w_dn': nrm(ks[17], (DEPTH, N_EXPERTS, D_FF, D_MODEL), D_FF ** -0.5),
        'b_dn': nrm(ks[18], (DEPTH, N_EXPERTS, D_MODEL), 0.01),
        'g_ple': gain(ks[19], (DEPTH, D_MODEL)),
        'w_ple_gate': nrm(ks[20], (DEPTH, D_MODEL, D_MODEL), D_MODEL ** -0.5),
        'w_ple_proj': nrm(ks[21], (DEPTH, PLE_DIM, D_MODEL), PLE_DIM ** -0.5),
        'g_final': gain(ks[22], (D_MODEL,)),
    }


def reference(x, p, positions, w_in, g_attn, g_cq, w_uq, g_ckv, w_ukv, g_out_mla, g_out_sb, w_o,
              g_moe, w_router, b_router, w_gu, b_gu, w_dn, b_dn, g_ple, w_ple_gate, w_ple_proj, g_final):
    b, s, _ = x.shape
    cos, sin = rope_tables(positions)

    def heads_first(a):
        return jnp.swapaxes(a, 1, 2)

    h = x
    for i in range(DEPTH):
        u = rmsnorm(h, g_attn[i])
        c_q, c_kv, k_r, q_s, k_s, v_s = jnp.split(u @ w_in[i], IN_SPLITS, axis=-1)
        q = (rmsnorm(c_q, g_cq[i]) @ w_uq[i]).reshape(b, s, MLA_HEADS, MLA_NOPE_DIM + MLA_ROPE_DIM)
        kv = (rmsnorm(c_kv, g_ckv[i]) @ w_ukv[i]).reshape(b, s, MLA_HEADS, MLA_NOPE_DIM + MLA_V_DIM)
        q_nope = q[..., :MLA_NOPE_DIM]
        q_rope = apply_rope(q[..., MLA_NOPE_DIM:], cos[:, :, None, :], sin[:, :, None, :])
        k_nope = kv[..., :MLA_NOPE_DIM]
        v_m = kv[..., MLA_NOPE_DIM:]
        k_rope = apply_rope(k_r, cos, sin)
        sb_shape = (b, s, SB_HEADS, SB_HEAD_DIM)
        o_m, o_s = mixer_heads(heads_first(q_nope), heads_first(q_rope), heads_first(k_nope), k_rope,
                               heads_first(v_m), heads_first(q_s.reshape(sb_shape)),
                               heads_first(k_s.reshape(sb_shape)), heads_first(v_s.reshape(sb_shape)))
        mixed = jnp.concatenate([rmsnorm(o_m, g_out_mla[i]), rmsnorm(o_s, g_out_sb[i])], axis=-1)
        h = h + mixed @ w_o[i]
        h = h + moe(rmsnorm(h, g_moe[i]), w_router[i], b_router[i], w_gu[i], b_gu[i], w_dn[i], b_dn[i])
        u = rmsnorm(h, g_ple[i])
        h = h + jax.nn.sigmoid(u @ w_ple_gate[i]) * (p[i] @ w_ple_proj[i])
    return rmsnorm(h, g_final)
```

```python
from contextlib import ExitStack
import numpy as np
import concourse.bass as bass
import concourse.mybir as mybir
from concourse.bass_utils import run_bass_kernel_spmd

F32 = mybir.dt.float32
BF16 = mybir.dt.bfloat16
I32 = mybir.dt.int32
AF = mybir.ActivationFunctionType
ALU = mybir.AluOpType

ENGS = ("pe", "act", "dve", "pool", "sp")
S = 4096
D = 1024
NE = 32
ORDER = ([6, 5, 3, 0], [7, 4, 2, 1])
NDIAG = [512, 512, 384, 384, 256, 256, 128, 128]
NEG = -30000.0
EPS = 1e-6


class Prog:
    def __init__(self, nc):
        self.nc = nc
        self.ops = {e: [] for e in ENGS}
        self.vcs = {}
        self.cur = {e: {} for e in ENGS}
        self.last_w = {}
        self.readers = {}
        self.excl = set()

    def op(self, eng, fn, reads=(), writes=(), dma=None):
        rec_ = _Rec()
        fn(rec_)
        assert len(rec_.calls) == 1
        fn = rec_.calls[0]
        clk = ("dma:" + dma) if dma else eng
        deps = []
        reads = list(reads)
        writes = list(writes)
        for k in reads:
            if k in self.excl and k not in writes:
                writes.append(k)
        for k in reads:
            lw = self.last_w.get(k)
            if lw:
                deps.append(lw)
        for k in writes:
            lw = self.last_w.get(k)
            if lw:
                deps.append(lw)
            for c, i in self.readers.get(k, {}).items():
                deps.append((c, i))
        cur = self.cur[eng]
        wmax = {}
        for (c, i) in deps:
            if c == "pe" and eng == "pe" and not dma:
                continue
            if cur.get(c, 0) >= i:
                continue
            wmax[c] = max(wmax.get(c, 0), i)
            for c2, i2 in self.vcs[c][i - 1].items():
                if cur.get(c2, 0) < i2:
                    cur[c2] = i2
            if cur.get(c, 0) < i:
                cur[c] = i
        vc = dict(cur)
        lst = self.vcs.setdefault(clk, [])
        lst.append(vc)
        idx = len(lst)
        vc[clk] = idx
        rec = {"fn": fn, "waits": wmax, "clk": clk, "idx": idx}
        self.ops[eng].append(rec)
        for k in reads:
            self.readers.setdefault(k, {})[clk] = idx
        for k in writes:
            self.last_w[k] = (clk, idx)
            self.readers[k] = {}
        return rec

    def finish_wait(self, eng):
        waits = {}
        for c, l in self.vcs.items():
            if len(l) and self.cur[eng].get(c, 0) < len(l):
                waits[c] = len(l)
                self.cur[eng][c] = len(l)
        self.ops[eng].append({"fn": None, "waits": waits, "clk": None, "idx": None})

    def barrier(self):
        for e in ENGS:
            self.finish_wait(e)
        full = {c: len(l) for c, l in self.vcs.items()}
        for e in ENGS:
            self.cur[e] = dict(full)

    def emit(self, stack):
        nc = self.nc
        waited = {}
        for e in ENGS:
            for r in self.ops[e]:
                for c, i in r["waits"].items():
                    waited.setdefault(c, set()).add(i)
        sems, semval = {}, {}
        for c, l in self.vcs.items():
            if c not in waited:
                continue
            sems[c] = stack.enter_context(nc.semaphore("s_" + c.replace(":", "_")))
            isd = c.startswith("dma:")
            v, m = 0, {}
            for i in range(1, len(l) + 1):
                if isd or i in waited[c]:
                    v += 16 if isd else 1
                    m[i] = v
            semval[c] = m
        block = stack.enter_context(nc.Block())
        engobj = {"pe": "tensor", "act": "scalar", "dve": "vector", "pool": "gpsimd", "sp": "sync"}

        def make(e):
            def body(eng):
                for r in self.ops[e]:
                    for c, i in r["waits"].items():
                        eng.wait_ge(sems[c], semval[c][i])
                    if r["fn"] is None:
                        continue
                    name, a, k = r["fn"]
                    ins = getattr(eng, name)(*a, **k)
                    c, i = r["clk"], r["idx"]
                    if c in sems and i in semval[c]:
                        ins.then_inc(sems[c], 16 if c.startswith("dma:") else 1)
            return body

        for e in ENGS:
            if self.ops[e]:
                getattr(block, engobj[e])(make(e))


class _Rec:
    def __init__(self):
        self.calls = []

    def __getattr__(self, name):
        def f(*a, **k):
            self.calls.append((name, a, k))
        return f


class Ring:
    def __init__(self, alloc, name, n, shape, dtype):
        self.tiles = [alloc("%s%d" % (name, i), shape, dtype) for i in range(n)]
        self.keys = ["%s%d" % (name, i) for i in range(n)]
        self.i = 0

    def next(self):
        t, k = self.tiles[self.i % len(self.tiles)], self.keys[self.i % len(self.tiles)]
        self.i += 1
        return t, k


class _Stop(Exception):
    pass


def build_program(stop_after=None, debug=False):
    nc = bass.Bass("TRN2", target_bir_lowering=False)

    def din(name, shape, dt=F32):
        return nc.dram_tensor(name, list(shape), dt, kind="ExternalInput").ap()

    def dscr(name, shape, dt=BF16):
        return nc.dram_tensor(name, list(shape), dt, kind="ExternalOutput" if debug else "Internal").ap()

    xa = din("xa", [S, D])
    xo = din("xo", [2048, D])
    po = din("po", [2048, 256])
    posa = din("posa", [1, S], I32)
    poso = din("poso", [1, 2048], I32)
    qrel = din("qrel", [1, 512])
    invf = din("invf", [128, 1])
    w_in = din("w_in", [D, 2208])
    g_attn = din("g_attn", [1, D])
    g_cq = din("g_cq", [1, 384])
    w_uq = din("w_uq", [384, 768])
    g_ckv = din("g_ckv", [1, 256])
    w_ukv = din("w_ukv", [256, 1024])
    g_out = din("g_out", [1, 1024])
    w_o = din("w_o", [D, D])
    g_moe = din("g_moe", [1, D])
    w_router = din("w_router", [D, NE])
    b_router = din("b_router", [1, NE])
    NEd = NE if stop_after in (None, "C2") else 1
    w_gu = din("w_gu", [NEd, D, 2048])
    b_gu = din("b_gu", [NE * 16, 128])
    w_dn = din("w_dn", [NEd, D, D])
    b_dn = din("b_dn", [NE, D])
    g_ple = din("g_ple", [1, D])
    w_pg = din("w_pg", [D, D])
    w_pp = din("w_pp", [256, D])
    g_final = din("g_final", [1, D])
    yo = nc.dram_tensor("yo", [2048, D], F32, kind="ExternalOutput").ap()

    KnT = dscr("KnT", [512, S])
    KrT = dscr("KrT", [32, S])
    VmD = dscr("VmD", [S, 512])
    KsT = dscr("KsT", [512, S])
    VsD = dscr("VsD", [S, 512])
    QmT = dscr("QmT", [8 * 96, 2048])
    QsT = dscr("QsT", [512, 2048])
    OTd = dscr("OTd", [1024, 2048])
    CAP = 512
    NS = CAP // 128
    Ud = nc.dram_tensor("Ud", [2048, D], BF16, kind="Internal").ap()
    Yd = nc.dram_tensor("Yd", [NE * CAP, D], F32, kind="Internal").ap()
    if debug:
        dbgH = nc.dram_tensor("dbgH", [2048, D], F32, kind="ExternalOutput").ap()
        dbgG = nc.dram_tensor("dbgG", [2048, NE], F32, kind="ExternalOutput").ap()
        dbgS = nc.dram_tensor("dbgS", [2048, 4], I32, kind="ExternalOutput").ap()
        dbgJ = nc.dram_tensor("dbgJ", [2048, 4], F32, kind="ExternalOutput").ap()

    top = ExitStack()
    with top:
        p = Prog(nc)
        if True:

            def mk_alloc(stack):
                def sb(n, s, d):
                    return stack.enter_context(nc.sbuf_tensor(n, list(s), d))

                def ps(n, s, d=F32):
                    return stack.enter_context(nc.psum_tensor(n, list(s), d))
                return sb, ps

            sb0, ps0 = mk_alloc(top)

            identf = sb0("identf", [128, 128], F32)
            ident = sb0("ident", [128, 128], BF16)
            ones = sb0("ones", [128, 128], BF16)
            ntri = sb0("ntri", [128, 128], BF16)
            nones = sb0("nones", [128, 128], BF16)
            zeros = sb0("zeros", [128, 128], BF16)
            p.op("pool", lambda e: e.memset(identf[:], 0.0), writes=["identf"])
            p.op("pool", lambda e: e.affine_select(out=identf[:], in_=identf[:], pattern=[[-1, 128]],
                                                    compare_op=ALU.not_equal, fill=1.0, base=0, channel_multiplier=1),
                 reads=["identf"], writes=["identf"])
            p.op("dve", lambda e: e.tensor_copy(out=ident[:], in_=identf[:]), reads=["identf"], writes=["ident"])
            p.op("pool", lambda e: e.memset(ones[:], 1.0), writes=["ones"])
            p.op("pool", lambda e: e.memset(nones[:], -1.0), writes=["nones"])
            p.op("pool", lambda e: e.memset(zeros[:], 0.0), writes=["zeros"])
            gcol = sb0("gcol", [128, 16], F32)
            p.op("sp", lambda e: e.dma_start(out=gcol[:, 0:3], in_=g_cq.rearrange("o (c p) -> p (o c)", p=128),
                                             allow_slow_non_contiguous=True), writes=["gcol"], dma="gcol")
            p.op("sp", lambda e: e.dma_start(out=gcol[:, 3:5], in_=g_ckv.rearrange("o (c p) -> p (o c)", p=128),
                                             allow_slow_non_contiguous=True), writes=["gcol"], dma="gcol")
            p.op("sp", lambda e: e.dma_start(out=gcol[:, 5:13], in_=g_out.rearrange("o (c p) -> p (o c)", p=128),
                                             allow_slow_non_contiguous=True), writes=["gcol"], dma="gcol")
            invc = sb0("invc", [128, 1], F32)
            p.op("sp", lambda e: e.dma_start(out=invc[:], in_=invf), writes=["invc"], dma="invc")

            def rstd_from(eng_out, src, n, rd, wr):
                p.op("act", lambda e: e.activation(out=eng_out, in_=src, func=AF.Ln, scale=1.0 / n, bias=EPS),
                     reads=rd, writes=wr)
                p.op("act", lambda e: e.activation(out=eng_out, in_=eng_out, func=AF.Exp, scale=-0.5),
                     reads=wr, writes=wr)

            sa = ExitStack()
            with sa:
                sb, ps = mk_alloc(sa)
                tmpf = sb("tmpf", [128, 128], F32)
                p.op("pool", lambda e: e.memset(tmpf[:], -1.0), writes=["tmpf"])
                p.op("pool", lambda e: e.affine_select(out=tmpf[:], in_=tmpf[:], pattern=[[-1, 128]],
                                                        compare_op=ALU.is_ge, fill=0.0, base=0, channel_multiplier=1),
                     reads=["tmpf"], writes=["tmpf"])
                p.op("dve", lambda e: e.tensor_copy(out=ntri[:], in_=tmpf[:]), reads=["tmpf"], writes=["ntri"])

                Win = sb("Win", [128, 8, 2208], BF16)
                for c in range(8):
                    p.op("pool", lambda e, c=c: e.dma_start(out=Win[:, c, :], in_=w_in[c * 128:(c + 1) * 128, :]),
                         writes=["Win"], dma="Win")
                Wuq = sb("Wuq", [128, 3, 768], BF16)
                Wuqr = sb("Wuqr", [128, 3, 768], BF16)
                p.op("pool", lambda e: e.dma_start(out=Wuq[:], in_=w_uq.rearrange("(c p) n -> p c n", p=128)),
                     writes=["Wuq"], dma="Wuq")
                Wkn = sb("Wkn", [128, 2, 512], BF16)
                Wv = sb("Wv", [128, 2, 512], BF16)
                ukv = w_ukv.rearrange("(c p) (h t d) -> p c h t d", p=128, h=8, t=2)
                for c in range(2):
                    p.op("pool", lambda e, c=c: e.dma_start(out=Wkn[:, c, :].rearrange("p (h d) -> p h d", h=8),
                                                          in_=ukv[:, c, :, 0, :]), writes=["Wkn"], dma="Wkn")
                    p.op("pool", lambda e, c=c: e.dma_start(out=Wv[:, c, :].rearrange("p (h d) -> p h d", h=8),
                                                          in_=ukv[:, c, :, 1, :]), writes=["Wv"], dma="Wv")
                p.op("pool", lambda e: e.memset(Wuqr[:], 0.0), writes=["Wuqr"])
                Wq4 = Wuq[:].rearrange("p c (h d) -> p c h d", h=8)
                Wr4 = Wuqr[:].rearrange("p c (h d) -> p c h d", h=8)
                for c in range(3):
                    p.op("dve", lambda e, c=c: e.tensor_scalar(out=Wr4[:, c, :, 64:80], in0=Wq4[:, c, :, 80:96], scalar1=-1.0,
                                                          scalar2=None, op0=ALU.mult), reads=["Wuq", "Wuqr"], writes=["Wuqr"])
                    p.op("dve", lambda e, c=c: e.tensor_copy(out=Wr4[:, c, :, 80:96], in_=Wq4[:, c, :, 64:80]),
                         reads=["Wuq", "Wuqr"], writes=["Wuqr"])
                Wkr = sb("Wkr", [128, 8, 64], BF16)
                p.op("dve", lambda e: e.tensor_copy(out=Wkr[:, :, 0:32], in_=Win[:, :, 640:672]), reads=["Win"], writes=["Wkr"])
                p.op("dve", lambda e: e.tensor_scalar(out=Wkr[:, :, 32:48], in0=Win[:, :, 656:672], scalar1=-1.0, scalar2=None,
                                                      op0=ALU.mult), reads=["Win", "Wkr"], writes=["Wkr"])
                p.op("dve", lambda e: e.tensor_copy(out=Wkr[:, :, 48:64], in_=Win[:, :, 640:656]), reads=["Win", "Wkr"], writes=["Wkr"])
                gattn = sb("gattn", [128, D], F32)
                p.op("sp", lambda e: e.dma_start(out=gattn[:], in_=g_attn.partition_broadcast(128)), writes=["gattn"], dma="gattn")

                def rope_table(Ct, St, pos_ap, n, rows, name, inv, kinv, sb):
                    posi = sb(name + "_pi", [128, n], I32)
                    ang = sb(name + "_ang", [128, n], F32)
                    kk = sb(name + "_k", [128, n], F32)
                    ki = sb(name + "_ki", [128, n], I32)
                    p.op("sp", lambda e: e.dma_start(out=posi[:], in_=pos_ap.partition_broadcast(128)), writes=[name + "pi"], dma=name + "pi")
                    p.op("dve", lambda e: e.tensor_copy(out=ang[:], in_=posi[:]), reads=[name + "pi"], writes=[name + "ang"])
                    p.op("dve", lambda e: e.tensor_scalar(out=ang[:], in0=ang[:], scalar1=inv[:, 0:1], scalar2=None, op0=ALU.mult),
                         reads=[name + "ang", kinv], writes=[name + "ang"])
                    for which, T in (("s", St), ("c", Ct)):
                        off = 0.0 if which == "s" else float(np.pi / 2)
                        p.op("dve", lambda e, off=off: e.tensor_scalar(out=kk[:], in0=ang[:], scalar1=off, scalar2=float(1.0 / (2 * np.pi)),
                                                                   op0=ALU.add, op1=ALU.mult), reads=[name + "ang"], writes=[name + "kk"])
                        p.op("dve", lambda e: e.tensor_copy(out=ki[:], in_=kk[:]), reads=[name + "kk"], writes=[name + "ki"])
                        p.op("dve", lambda e: e.tensor_copy(out=kk[:], in_=ki[:]), reads=[name + "ki"], writes=[name + "kk"])
                        p.op("dve", lambda e, T=T: e.scalar_tensor_tensor(out=T, in0=kk[:rows], scalar=-6.28125, in1=ang[:rows],
                                                                       op0=ALU.mult, op1=ALU.add),
                             reads=[name + "kk", name + "ang"], writes=[name + which])
                        p.op("dve", lambda e, T=T, off=off: e.scalar_tensor_tensor(out=T, in0=kk[:rows], scalar=float(-(2 * np.pi - 6.28125)), in1=T,
                                                                                op0=ALU.mult, op1=ALU.add),
                             reads=[name + "kk", name + which], writes=[name + which])
                        if off != 0.0:
                            p.op("dve", lambda e, T=T, off=off: e.tensor_scalar(out=T, in0=T, scalar1=off, scalar2=None, op0=ALU.add),
                                 reads=[name + which], writes=[name + which])
                        p.op("dve", lambda e: e.tensor_scalar(out=kk[:rows], in0=T, scalar1=float(np.pi), scalar2=float(-2 * np.pi),
                                                              op0=ALU.is_gt, op1=ALU.mult), reads=[name + which], writes=[name + "kk"])
                        p.op("dve", lambda e, T=T: e.tensor_tensor(out=T, in0=T, in1=kk[:rows], op=ALU.add),
                             reads=[name + which, name + "kk"], writes=[name + which])
                        p.op("dve", lambda e: e.tensor_scalar(out=kk[:rows], in0=T, scalar1=float(-np.pi), scalar2=float(2 * np.pi),
                                                              op0=ALU.is_lt, op1=ALU.mult), reads=[name + which], writes=[name + "kk"])
                        p.op("dve", lambda e, T=T: e.tensor_tensor(out=T, in0=T, in1=kk[:rows], op=ALU.add),
                             reads=[name + which, name + "kk"], writes=[name + which])
                        p.op("dve", lambda e, T=T: e.tensor_scalar(out=T, in0=T, scalar1=3.14159, scalar2=-3.14159, op0=ALU.min, op1=ALU.max),
                             reads=[name + which], writes=[name + which])
                        p.op("act", lambda e, T=T: e.activation(out=T, in_=T, func=AF.Sin), reads=[name + which], writes=[name + which])

                Ck = sb("Ck", [32, S], F32)
                Sk = sb("Sk", [32, S], F32)
                sa2 = ExitStack()
                with sa2:
                    rope_table(Ck[:], Sk[:], posa, S, 32, "rk", invc, "invc", mk_alloc(sa2)[0])
                    p.barrier()
                Cq = sb("Cq", [96, 2048], F32)
                Sq = sb("Sq", [96, 2048], F32)
                sa3 = ExitStack()
                with sa3:
                    sb3_ = mk_alloc(sa3)[0]
                    invq = sb3_("invq", [128, 1], F32)
                    p.op("pool", lambda e: e.memset(invq[:], 0.0), writes=["invq"])
                    p.op("sp", lambda e: e.dma_start(out=invq[64:96, :], in_=invf[0:32, :]), reads=["invq"], writes=["invq"], dma="invq")
                    rope_table(Cq[:], Sq[:], poso, 2048, 96, "rq", invq, "invq", sb3_)
                    p.barrier()
                    if stop_after == "R":
                        p.finish_wait("sp"); p.emit(top); return nc
                msc = float((64 + 32) ** -0.5)
                p.op("dve", lambda e: e.tensor_scalar(out=Cq[:], in0=Cq[:], scalar1=msc, scalar2=None, op0=ALU.mult),
                     reads=["rqc"], writes=["rqc"])
                p.op("dve", lambda e: e.tensor_scalar(out=Sq[:], in0=Sq[:], scalar1=msc, scalar2=None, op0=ALU.mult),
                     reads=["rqs"], writes=["rqs"])

                xt_r = Ring(sb, "xt", 2, [128, D], F32)
                junk = sb("junkA", [128, D], F32)
                ss_r = Ring(sb, "ssA", 2, [128, 1], F32)
                xn_r = Ring(sb, "xn", 2, [128, D], BF16)
                xnT_r = Ring(sb, "xnT", 2, [128, 8, 512], BF16)
                pT_r = Ring(ps, "pTA", 2, [128, 8, 128], BF16)
                pm_r = Ring(ps, "pmA", 4, [128, 512], F32)
                pss = ps("pssA", [128, 512], F32)
                sq_r = Ring(sb, "sqA", 2, [128, 512], BF16)
                rbc = sb("rbcA", [128, 512], F32)
                cn_r = Ring(sb, "cnA", 2, [128, 3, 512], BF16)
                ev_r = Ring(sb, "evA", 2, [128, 512], BF16)
                stg_r = Ring(sb, "stgA", 3, [128, 4, 512], BF16)
                stg8_r = Ring(sb, "stg8A", 2, [128, 8, 512], BF16)
                evf_r = Ring(sb, "evfA", 2, [128, 512], F32)
                evf2_r = Ring(sb, "evf2A", 2, [128, 512], F32)

                def make_xnT(src, tok0):
                    xnT, kT = xnT_r.next()
                    for sub in range(4):
                        xt, kx = xt_r.next()
                        ss, ks = ss_r.next()
                        xn, kn = xn_r.next()
                        pT, kp = pT_r.next()
                        r0 = tok0 + sub * 128
                        p.op("sp", lambda e, xt=xt, r0=r0: e.dma_start(out=xt[:], in_=src[r0:r0 + 128, :]), writes=[kx], dma=kx)
                        p.op("act", lambda e, xt=xt, ss=ss: e.activation(out=junk[:], in_=xt[:], func=AF.Square, accum_out=ss[:]),
                             reads=[kx], writes=["junkA", ks])
                        rstd_from(ss[:], ss[:], D, [ks], [ks])
                        p.op("dve", lambda e, xt=xt, ss=ss, xn=xn: e.scalar_tensor_tensor(out=xn[:], in0=xt[:], scalar=ss[:, 0:1], in1=gattn[:],
                                                                                      op0=ALU.mult, op1=ALU.mult),
                             reads=[kx, ks, "gattn"], writes=[kn])
                        for c in range(8):
                            p.op("pe", lambda e, c=c, xn=xn, pT=pT: e.transpose(out=pT[:, c, :], in_=xn[:, c * 128:(c + 1) * 128], identity=ident[:]),
                                 reads=[kn, "ident"], writes=[kp])
                        p.op("dve", lambda e, xnT=xnT, pT=pT, sub=sub: e.tensor_copy(out=xnT[:, :, sub * 128:(sub + 1) * 128], in_=pT[:]),
                             reads=[kp], writes=[kT])
                    return xnT, kT

                def proj_fm(xnT, kT, col0, m, wkey="Win", W=None):
                    W = Win if W is None else W
                    pm, kpm = pm_r.next()
                    for k in range(8):
                        p.op("pe", lambda e, k=k, pm=pm, W=W: e.matmul(out=pm[0:m, :], lhsT=W[:, k, col0:col0 + m], rhs=xnT[:, k, :],
                                                                  start=(k == 0), stop=(k == 7)),
                             reads=[kT, wkey], writes=[kpm])
                    return pm, kpm

                def lowrank_norm(pms, nch, width, gc0):
                    for i, (pm, kpm) in enumerate(pms):
                        sq, ksq = sq_r.next()
                        p.op("act", lambda e, pm=pm, sq=sq: e.activation(out=sq[:], in_=pm[:], func=AF.Square), reads=[kpm], writes=[ksq])
                        p.op("pe", lambda e, sq=sq, i=i: e.matmul(out=pss[:], lhsT=ones[:], rhs=sq[:], start=(i == 0), stop=(i == nch - 1)),
                             reads=[ksq, "ones"], writes=["pssA"])
                    rstd_from(rbc[:], pss[:], width, ["pssA"], ["rbcA"])
                    cn, kcn = cn_r.next()
                    for i, (pm, kpm) in enumerate(pms):
                        p.op("dve", lambda e, pm=pm, i=i, cn=cn: e.scalar_tensor_tensor(out=cn[:, i, :], in0=pm[:], scalar=gcol[:, gc0 + i:gc0 + i + 1],
                                                                                    in1=rbc[:], op0=ALU.mult, op1=ALU.mult),
                             reads=[kpm, "gcol", "rbcA"], writes=[kcn])
                    return cn, kcn

                def store_fm(pm, kpm, rows, dst, eng="dve", scale=None):
                    ev, kev = ev_r.next()
                    if scale is None:
                        if eng == "act":
                            p.op("act", lambda e: e.copy(out=ev[0:rows, :], in_=pm[0:rows, :]), reads=[kpm], writes=[kev])
                        else:
                            p.op("dve", lambda e: e.tensor_copy(out=ev[0:rows, :], in_=pm[0:rows, :]), reads=[kpm], writes=[kev])
                    else:
                        p.op("act", lambda e: e.mul(out=ev[0:rows, :], in_=pm[0:rows, :], mul=scale), reads=[kpm], writes=[kev])
                    p.op("pool", lambda e: e.dma_start(out=dst, in_=ev[0:rows, :]), reads=[kev], writes=[], dma=kev + "s")

                def evac_to(pm, kpm, dst, kdst, eng="dve", scale=None):
                    if scale is not None:
                        p.op("act", lambda e: e.mul(out=dst, in_=pm[:], mul=scale), reads=[kpm], writes=[kdst])
                    elif eng == "act":
                        p.op("act", lambda e: e.copy(out=dst, in_=pm[:]), reads=[kpm], writes=[kdst])
                    else:
                        p.op("dve", lambda e: e.tensor_copy(out=dst, in_=pm[:]), reads=[kpm], writes=[kdst])

                KnT_v = KnT.rearrange("(c p) n -> p c n", p=128)
                KsT_v = KsT.rearrange("(c p) n -> p c n", p=128)
                QsT_v = QsT.rearrange("(c p) n -> p c n", p=128)
                VmD_v = VmD.rearrange("(s p) n -> p s n", p=128)
                VsD_v = VsD.rearrange("(s p) n -> p s n", p=128)
                QmT_v = QmT.rearrange("(h r) n -> r h n", r=96)

                srcs = [(xa, g_ * 512) for g_ in range(8)] + [(xo, g_ * 512) for g_ in range(4)]
                nxt = make_xnT(*srcs[0])
                for G in range(8):
                    c0 = G * 512
                    xnT, kT = nxt
                    nxt = make_xnT(*srcs[G + 1])
                    ckv = [proj_fm(xnT, kT, 384 + m * 128, 128) for m in range(2)]
                    cn, kcn = lowrank_norm(ckv, 2, 256, 3)
                    pa, kpa = proj_fm(xnT, kT, 0, 32, "Wkr", Wkr)
                    pb, kpb = proj_fm(xnT, kT, 32, 32, "Wkr", Wkr)
                    t1, kt1 = evf_r.next()
                    t2, kt2 = evf2_r.next()
                    p.op("dve", lambda e, pa=pa, t1=t1, c0=c0: e.tensor_tensor(out=t1[0:32, :], in0=pa[0:32, :], in1=Ck[:, c0:c0 + 512], op=ALU.mult),
                         reads=[kpa, "rkc"], writes=[kt1])
                    p.op("dve", lambda e, pb=pb, t2=t2, c0=c0: e.tensor_tensor(out=t2[0:32, :], in0=pb[0:32, :], in1=Sk[:, c0:c0 + 512], op=ALU.mult),
                         reads=[kpb, "rks"], writes=[kt2])
                    ev, kev = ev_r.next()
                    p.op("dve", lambda e, t1=t1, t2=t2, ev=ev: e.tensor_tensor(out=ev[0:32, :], in0=t1[0:32, :], in1=t2[0:32, :], op=ALU.add),
                         reads=[kt1, kt2], writes=[kev])
                    p.op("sp", lambda e, ev=ev, c0=c0: e.dma_start(out=KrT[:, c0:c0 + 512], in_=ev[0:32, :]), reads=[kev], writes=[], dma=kev + "s")
                    st_, kst_ = stg_r.next()
                    for m in range(4):
                        pm, kpm = proj_fm(xnT, kT, 1184 + m * 128, 128)
                        evac_to(pm, kpm, st_[:, m, :], kst_, eng="act")
                    p.op("sp", lambda e: e.dma_start(out=KsT_v[:, :, c0:c0 + 512], in_=st_[:]), reads=[kst_], dma=kst_ + "s")
                    st_, kst_ = stg_r.next()
                    for sub in range(4):
                        pm, kpm = pm_r.next()
                        for k in range(8):
                            p.op("pe", lambda e, k=k, pm=pm, sub=sub, xnT=xnT: e.matmul(out=pm[:], lhsT=xnT[:, k, sub * 128:(sub + 1) * 128],
                                                                                   rhs=Win[:, k, 1696:2208], start=(k == 0), stop=(k == 7)),
                                 reads=[kT, "Win"], writes=[kpm])
                        evac_to(pm, kpm, st_[:, sub, :], kst_, eng="dve")
                    p.op("sp", lambda e: e.dma_start(out=VsD_v[:, G * 4:(G + 1) * 4, :], in_=st_[:]), reads=[kst_], dma=kst_ + "s")

                    st_, kst_ = stg_r.next()
                    for hp in range(4):
                        pm, kpm = pm_r.next()
                        for m in range(2):
                            p.op("pe", lambda e, m=m, pm=pm, hp=hp, cn=cn: e.matmul(out=pm[:], lhsT=Wkn[:, m, hp * 128:(hp + 1) * 128], rhs=cn[:, m, :],
                                                                               start=(m == 0), stop=(m == 1)),
                                 reads=[kcn, "Wkn"], writes=[kpm])
                        evac_to(pm, kpm, st_[:, hp, :], kst_, eng="act")
                    p.op("sp", lambda e: e.dma_start(out=KnT_v[:, :, c0:c0 + 512], in_=st_[:]), reads=[kst_], dma=kst_ + "s")
                    st_, kst_ = stg_r.next()
                    for sub in range(4):
                        pm, kpm = pm_r.next()
                        for m in range(2):
                            p.op("pe", lambda e, m=m, pm=pm, sub=sub, cn=cn: e.matmul(out=pm[:], lhsT=cn[:, m, sub * 128:(sub + 1) * 128], rhs=Wv[:, m, :],
                                                                                 start=(m == 0), stop=(m == 1)),
                                 reads=[kcn, "Wv"], writes=[kpm])
                        evac_to(pm, kpm, st_[:, sub, :], kst_, eng="dve")
                    p.op("sp", lambda e: e.dma_start(out=VmD_v[:, G * 4:(G + 1) * 4, :], in_=st_[:]), reads=[kst_], dma=kst_ + "s")
                for G in range(4):
                    c0 = G * 512
                    xnT, kT = nxt
                    if G + 1 < 4:
                        nxt = make_xnT(*srcs[8 + G + 1])
                    cq = [proj_fm(xnT, kT, m * 128, 128) for m in range(3)]
                    cn, kcn = lowrank_norm(cq, 3, 384, 0)
                    st8, kst8 = stg8_r.next()
                    for h in range(8):
                        pa, kpa = pm_r.next()
                        pb, kpb = pm_r.next()
                        for m in range(3):
                            p.op("pe", lambda e, m=m, pa=pa, h=h, cn=cn: e.matmul(out=pa[0:96, :], lhsT=Wuq[:, m, h * 96:(h + 1) * 96], rhs=cn[:, m, :],
                                                                             start=(m == 0), stop=(m == 2)),
                                 reads=[kcn, "Wuq"], writes=[kpa])
                        for m in range(3):
                            p.op("pe", lambda e, m=m, pb=pb, h=h, cn=cn: e.matmul(out=pb[0:96, :], lhsT=Wuqr[:, m, h * 96:(h + 1) * 96], rhs=cn[:, m, :],
                                                                             start=(m == 0), stop=(m == 2)),
                                 reads=[kcn, "Wuqr"], writes=[kpb])
                        t1, kt1 = evf_r.next()
                        t2, kt2 = evf2_r.next()
                        p.op("dve", lambda e, pa=pa, t1=t1, c0=c0: e.tensor_tensor(out=t1[0:96, :], in0=pa[0:96, :], in1=Cq[:, c0:c0 + 512], op=ALU.mult),
                             reads=[kpa, "rqc"], writes=[kt1])
                        p.op("dve", lambda e, pb=pb, t2=t2, c0=c0: e.tensor_tensor(out=t2[0:96, :], in0=pb[0:96, :], in1=Sq[:, c0:c0 + 512], op=ALU.mult),
                             reads=[kpb, "rqs"], writes=[kt2])
                        p.op("pool", lambda e, t1=t1, t2=t2: e.tensor_tensor(out=st8[0:96, h, :], in0=t1[0:96, :], in1=t2[0:96, :], op=ALU.add),
                             reads=[kt1, kt2], writes=[kst8])
                    p.op("sp", lambda e: e.dma_start(out=QmT_v[:, :, c0:c0 + 512], in_=st8[0:96, :, :]), reads=[kst8], dma=kst8 + "s")
                    st_, kst_ = stg_r.next()
                    for m in range(4):
                        pm, kpm = proj_fm(xnT, kT, 672 + m * 128, 128)
                        evac_to(pm, kpm, st_[:, m, :], kst_, scale=0.125)
                    p.op("sp", lambda e: e.dma_start(out=QsT_v[:, :, c0:c0 + 512], in_=st_[:]), reads=[kst_], dma=kst_ + "s")
                p.barrier()
                if stop_after == "A":
                    p.finish_wait("sp"); p.emit(top); return nc

            sbx = ExitStack()
            with sbx:
                sb, ps = mk_alloc(sbx)
                qrb = sb("qrb", [128, 512], F32)
                p.op("sp", lambda e: e.dma_start(out=qrb[:], in_=qrel.partition_broadcast(128)), writes=["qrb"], dma="qrb")
                kidx_i = sb("kidx_i", [128, 1], I32)
                krel = sb("krel", [128, 8], F32)
                p.op("pool", lambda e: e.iota(kidx_i[:], pattern=[[0, 1]], base=0, channel_multiplier=1), writes=["kidx_i"])
                p.op("dve", lambda e: e.tensor_copy(out=krel[:, 0:1], in_=kidx_i[:]), reads=["kidx_i"], writes=["krel"])
                for d in range(1, 8):
                    p.op("dve", lambda e, d=d: e.tensor_scalar(out=krel[:, d:d + 1], in0=krel[:, 0:1], scalar1=float(128 * d), scalar2=None, op0=ALU.add),
                         reads=["krel"], writes=["krel"])
                nmM = sb("nmM", [128, 8, 512], BF16)
                nmS = sb("nmS", [128, 8, 512], BF16)
                m01 = sb("m01", [128, 8, 512], BF16)
                for d in range(8):
                    p.op("dve", lambda e, d=d: e.tensor_scalar(out=nmM[:, d, :], in0=qrb[:], scalar1=krel[:, d:d + 1], scalar2=NEG, op0=ALU.is_lt, op1=ALU.mult),
                         reads=["qrb", "krel"], writes=["nmM"])
                    p.op("dve", lambda e, d=d: e.tensor_scalar(out=nmS[:, d, :], in0=qrb[:], scalar1=krel[:, d:d + 1], scalar2=NEG, op0=ALU.is_le, op1=ALU.mult),
                         reads=["qrb", "krel"], writes=["nmS"])
                    p.op("dve", lambda e, d=d: e.tensor_scalar(out=m01[:, d, :], in0=qrb[:], scalar1=krel[:, d:d + 1], scalar2=None, op0=ALU.is_gt),
                         reads=["qrb", "krel"], writes=["m01"])

                def blocks(t):
                    out = [(kb, 512, None) for kb in range(8 * t)]
                    out += [(8 * t + d, NDIAG[d], d) for d in range(8)]
                    return out

                sm = ExitStack()
                with sm:
                    def mla_gen():
                        sb, ps = mk_alloc(sm)
                        Vall = sb("Vall", [128, 32, 8, 65], BF16)
                        p.op("pool", lambda e: e.memset(Vall[:], 1.0), writes=["Vall"])
                        vsrc = VmD.rearrange("(kb p) (h d) -> p kb h d", p=128, h=8)
                        for hh in range(8):
                            p.op("sp", lambda e: e.dma_start(out=Vall[:, :, hh, 0:64], in_=vsrc[:, :, hh, :]),
                                 reads=["Vall"], writes=["Vall"], dma="Vall")
                        KT_r = Ring(sb, "KTm", 2, [96, S], BF16)
                        QT_r = Ring(sb, "QTm", 2, [96, 2048], BF16)
                        pS_r = Ring(ps, "pSm", 2, [128, 512], F32)
                        pO_r = Ring(ps, "pOm", 1, [128, 512], F32)
                        pB_r = Ring(ps, "pBm", 1, [128, 512], F32)
                        P_r = Ring(sb, "Pm", 3, [128, 512], BF16)
                        Of_r = Ring(sb, "Ofm", 2, [65, 512], F32)
                        On_r = Ring(sb, "Onm", 2, [64, 512], BF16)
                        sel = sb("sel65", [65, 64], F32)
                        p.op("pool", lambda e: e.memset(sel[:], 0.0), writes=["sel65"])
                        p.op("pool", lambda e: e.memset(sel[64:65, :], 1.0), reads=["sel65"], writes=["sel65"])
                        for h in range(8):
                            KT, kKT = KT_r.next()
                            QT, kQT = QT_r.next()
                            p.op("sp", lambda e, KT=KT, h=h: e.dma_start(out=KT[0:64, :], in_=KnT[h * 64:(h + 1) * 64, :]), writes=[kKT], dma=kKT)
                            p.op("sp", lambda e, KT=KT: e.dma_start(out=KT[64:96, :], in_=KrT[:, :]), writes=[kKT], dma=kKT)
                            p.op("sp", lambda e, QT=QT, h=h: e.dma_start(out=QT[:], in_=QmT[h * 96:(h + 1) * 96, :]), writes=[kQT], dma=kQT)
                            for t in range(4):
                                bl = blocks(t)
                                pO, kpO = pO_r.next()
                                nb = len(bl)
                                stage = {}

                                def stA(i):
                                    kb, N, d = bl[i]
                                    pS, kpS = pS_r.next()
                                    p.op("pe", lambda e: e.matmul(out=pS[:, 0:N], lhsT=KT[:, kb * 128:(kb + 1) * 128], rhs=QT[:, t * 512:t * 512 + N],
                                                                  start=True, stop=(d is None)), reads=[kKT, kQT], writes=[kpS])
                                    if d is not None:
                                        p.op("pe", lambda e: e.matmul(out=pS[:, 0:N], lhsT=ident[:], rhs=nmM[:, d, 0:N], start=False, stop=True),
                                             reads=["ident", "nmM"], writes=[kpS])
                                    P, kP = P_r.next()
                                    p.op("act", lambda e: e.activation(out=P[:, 0:N], in_=pS[:, 0:N], func=AF.Exp), reads=[kpS], writes=[kP])
                                    stage[i] = (P, kP)

                                def stB(i):
                                    kb, N, d = bl[i]
                                    P, kP = stage.pop(i)
                                    p.op("pe", lambda e: e.matmul(out=pO[0:65, 0:N], lhsT=Vall[:, kb, h, :], rhs=P[:, 0:N], start=(i == 0), stop=(i == nb - 1)),
                                         reads=["Vall", kP], writes=[kpO])

                                for it in range(nb + 2):
                                    if it < nb:
                                        stA(it)
                                    if it - 2 >= 0:
                                        stB(it - 2)
                                    yield
                                Of, kOf = Of_r.next()
                                p.op("dve", lambda e, Of=Of, pO=pO: e.tensor_copy(out=Of[:], in_=pO[0:65, :]), reads=[kpO], writes=[kOf])
                                p.op("dve", lambda e, Of=Of: e.reciprocal(out=Of[64:65, :], in_=Of[64:65, :]), reads=[kOf], writes=[kOf])
                                pB, kpB = pB_r.next()
                                p.op("pe", lambda e, Of=Of, pB=pB: e.matmul(out=pB[0:64, :], lhsT=sel[:], rhs=Of[:], start=True, stop=True),
                                     reads=[kOf, "sel65"], writes=[kpB])
                                On, kOn = On_r.next()
                                p.op("dve", lambda e, Of=Of, pB=pB, On=On: e.tensor_tensor(out=On[:], in0=Of[0:64, :], in1=pB[0:64, :], op=ALU.mult),
                                     reads=[kOf, kpB], writes=[kOn])
                                p.op("pool", lambda e, On=On, h=h, t=t: e.dma_start(out=OTd[h * 64:(h + 1) * 64, t * 512:(t + 1) * 512], in_=On[:]),
                                     reads=[kOn], writes=[], dma=kOn + "s")

                    def sb_gen():
                        sb, ps = mk_alloc(sm)
                        Vs = sb("Vsall", [128, 32, 512], BF16)
                        vsrc = VsD.rearrange("(kb p) n -> p kb n", p=128)
                        for q4 in range(4):
                            p.op("sp", lambda e, q4=q4: e.dma_start(out=Vs[:, q4 * 8:(q4 + 1) * 8, :], in_=vsrc[:, q4 * 8:(q4 + 1) * 8, :]),
                                 writes=["Vsall"], dma="Vsall")
                        KT_r = Ring(sb, "KTs", 2, [64, S], BF16)
                        QT_r = Ring(sb, "QTs", 2, [64, 2048], BF16)
                        pZ_r = Ring(ps, "pZs", 2, [128, 512], F32)
                        pL_r = Ring(ps, "pLs", 1, [128, 512], F32)
                        pO_r = Ring(ps, "pOs", 1, [128, 512], F32)
                        E_r = Ring(sb, "Es", 3, [128, 512], F32)
                        SP_r = Ring(sb, "SPs", 4, [128, 512], BF16)
                        SM_r = Ring(sb, "SMs", 4, [128, 512], BF16)
                        A_r = Ring(sb, "As", 3, [128, 512], BF16)
                        CR_r = Ring(sb, "CRs", 3, [128, 512], BF16)
                        On_r = Ring(sb, "Ons", 2, [64, 512], BF16)
                        for h in range(8):
                            KT, kKT = KT_r.next()
                            QT, kQT = QT_r.next()
                            p.op("sp", lambda e, KT=KT, h=h: e.dma_start(out=KT[:], in_=KsT[h * 64:(h + 1) * 64, :]), writes=[kKT], dma=kKT)
                            p.op("sp", lambda e, QT=QT, h=h: e.dma_start(out=QT[:], in_=QsT[h * 64:(h + 1) * 64, :]), writes=[kQT], dma=kQT)
                            for t in range(4):
                                bl = blocks(t)[::-1]
                                nb = len(bl)
                                pO, kpO = pO_r.next()
                                p.op("pe", lambda e, pO=pO: e.matmul(out=pO[0:64, :], lhsT=zeros[:, 0:64], rhs=m01[:, 0, :],
                                                                      start=True, stop=False), reads=["zeros", "m01"], writes=[kpO])
                                stage = {}
                                carry = {"t": None, "k": None}

                                def stA(i):
                                    kb, N, d = bl[i]
                                    pZ, kpZ = pZ_r.next()
                                    p.op("pe", lambda e: e.matmul(out=pZ[:, 0:N], lhsT=KT[:, kb * 128:(kb + 1) * 128], rhs=QT[:, t * 512:t * 512 + N],
                                                                  start=True, stop=True), reads=[kKT, kQT], writes=[kpZ])
                                    E, kE = E_r.next()
                                    p.op("act", lambda e: e.activation(out=E[:, 0:N], in_=pZ[:, 0:N], func=AF.Exp), reads=[kpZ], writes=[kE])
                                    SPt, kSP = SP_r.next()
                                    p.op("act", lambda e: e.activation(out=SPt[:, 0:N], in_=E[:, 0:N], func=AF.Ln, bias=1.0), reads=[kE], writes=[kSP])
                                    if d is not None:
                                        SM, kSM = SM_r.next()
                                        p.op("dve", lambda e: e.tensor_tensor(out=SM[:, 0:N], in0=SPt[:, 0:N], in1=m01[:, d, 0:N], op=ALU.mult),
                                             reads=[kSP, "m01"], writes=[kSM])
                                    else:
                                        SM, kSM = SPt, kSP
                                    cprev, kcprev = carry["t"], carry["k"]
                                    stage[i] = (SM, kSM, cprev, kcprev, E, kE)
                                    if i < nb - 1:
                                        cn_, kcn_ = CR_r.next()
                                        if cprev is None:
                                            if N < 512:
                                                p.op("pool", lambda e: e.memset(cn_[:, N:512], 0.0), writes=[kcn_])
                                            p.op("pool", lambda e: e.tensor_copy(out=cn_[:, 0:N], in_=SM[:, 0:N]), reads=[kSM], writes=[kcn_])
                                        else:
                                            if N < 512:
                                                p.op("pool", lambda e: e.tensor_copy(out=cn_[:, N:512], in_=cprev[:, N:512]), reads=[kcprev], writes=[kcn_])
                                            p.op("dve", lambda e: e.tensor_tensor(out=cn_[:, 0:N], in0=cprev[:, 0:N], in1=SM[:, 0:N], op=ALU.add),
                                                 reads=[kcprev, kSM], writes=[kcn_])
                                        carry["t"], carry["k"] = cn_, kcn_

                                def stB(i):
                                    kb, N, d = bl[i]
                                    SM, kSM, cprev, kcprev, E, kE = stage[i]
                                    pL, kpL = pL_r.next()
                                    last = "tri"
                                    if cprev is not None:
                                        last = "carry"
                                    if d is not None:
                                        last = "mask"
                                    p.op("pe", lambda e: e.matmul(out=pL[:, 0:N], lhsT=ntri[:], rhs=SM[:, 0:N], start=True, stop=(last == "tri")),
                                         reads=["ntri", kSM], writes=[kpL])
                                    if cprev is not None:
                                        p.op("pe", lambda e: e.matmul(out=pL[:, 0:N], lhsT=nones[:], rhs=cprev[:, 0:N], start=False, stop=(last == "carry")),
                                             reads=["nones", kcprev], writes=[kpL])
                                    if d is not None:
                                        p.op("pe", lambda e: e.matmul(out=pL[:, 0:N], lhsT=ident[:], rhs=nmS[:, d, 0:N], start=False, stop=True),
                                             reads=["ident", "nmS"], writes=[kpL])
                                    A, kA = A_r.next()
                                    p.op("act", lambda e: e.activation(out=A[:, 0:N], in_=pL[:, 0:N], func=AF.Exp), reads=[kpL], writes=[kA])
                                    p.op("dve", lambda e: e.tensor_tensor(out=A[:, 0:N], in0=A[:, 0:N], in1=E[:, 0:N], op=ALU.mult), reads=[kA, kE], writes=[kA])
                                    stage[i] = (A, kA)

                                def stC(i):
                                    kb, N, d = bl[i]
                                    A, kA = stage.pop(i)
                                    p.op("pe", lambda e: e.matmul(out=pO[0:64, 0:N], lhsT=Vs[:, kb, h * 64:(h + 1) * 64], rhs=A[:, 0:N], start=False, stop=(i == nb - 1)),
                                         reads=["Vsall", kA], writes=[kpO])

                                for it in range(nb + 2):
                                    if it < nb:
                                        stA(it)
                                    if 0 <= it - 1 < nb:
                                        stB(it - 1)
                                    if it - 2 >= 0:
                                        stC(it - 2)
                                    yield
                                On, kOn = On_r.next()
                                p.op("dve", lambda e, On=On, pO=pO: e.tensor_copy(out=On[:], in_=pO[0:64, :]), reads=[kpO], writes=[kOn])
                                p.op("pool", lambda e, On=On, h=h, t=t: e.dma_start(out=OTd[512 + h * 64:512 + (h + 1) * 64, t * 512:(t + 1) * 512], in_=On[:]),
                                     reads=[kOn], writes=[], dma=kOn + "s")

                    gens = [mla_gen(), sb_gen()]
                    while gens:
                        for g_ in list(gens):
                            try:
                                next(g_)
                            except StopIteration:
                                gens.remove(g_)
                    p.barrier()
                    if stop_after == "B":
                        p.finish_wait("sp"); p.emit(top); return nc
            sc = ExitStack()
            with sc:
                sb, ps = mk_alloc(sc)
                H = sb("H", [128, 16, D], F32)
                posm = sb("posm", [128, 16, NE], F32)
                maskb = sb("maskb", [128, 16, NE], BF16)
                gj = sb("gj", [128, 16, 4], F32)
                slI = sb("slI", [128, 16, 4], I32)
                Gt = sb("Gt", [128, 16, NE], F32)
                bgT = sb("bgT", [128, 512], F32)
                s1 = ExitStack()
                with s1:
                    sb1, ps1 = mk_alloc(s1)
                    gbc = sb1("gbc", [128, D], F32)
                    junk = sb1("junkC", [128, D], BF16)
                    Wo = sb1("Wo", [128, 8, D], BF16)
                    p.op("pool", lambda e: e.dma_start(out=Wo[:], in_=w_o.rearrange("(c p) n -> p c n", p=128)), writes=["Wo"], dma="Wo")
                    Wr = sb1("Wr", [128, 8, NE], F32)
                    p.op("sp", lambda e: e.dma_start(out=Wr[:], in_=w_router.rearrange("(c p) n -> p c n", p=128)), writes=["Wr"], dma="Wr")
                    brb = sb1("brb", [128, NE], F32)
                    p.op("sp", lambda e: e.dma_start(out=brb[:], in_=b_router.partition_broadcast(128)), writes=["brb"], dma="brb")
                    p.op("sp", lambda e: e.dma_start(out=gbc[:], in_=g_moe.partition_broadcast(128)), writes=["gbc"], dma="gbc")
                    bgl = sb1("bgl", [128, 4, 128], F32)
                    p.op("sp", lambda e: e.dma_start(out=bgl[:], in_=b_gu.rearrange("(a r) q -> r a q", r=128)), writes=["bgl"], dma="bgl")
                    pTf_r = Ring(ps1, "pTf", 1, [128, 4, 128], F32)
                    pbt, kpbt = pTf_r.next()
                    for a in range(4):
                        p.op("pe", lambda e, a=a: e.transpose(out=pbt[:, a, :], in_=bgl[:, a, :], identity=identf[:]), reads=["bgl", "identf"], writes=[kpbt])
                    p.op("dve", lambda e: e.tensor_copy(out=bgT[:].rearrange("p (a q) -> p a q", a=4), in_=pbt[:]), reads=[kpbt], writes=["bgT"])

                    OT_r = Ring(sb1, "OTl", 1, [128, 8, 512], BF16)
                    sq_r = Ring(sb1, "sqC", 2, [128, 512], BF16)
                    pss_r = Ring(ps1, "pssC", 1, [128, 512], F32)
                    rb_r = Ring(sb1, "rbC", 2, [128, 512], F32)
                    MX = sb1("MX", [128, 8, 512], BF16)
                    pA_r = Ring(ps1, "pAC", 2, [128, 512], F32)
                    xr_r = Ring(sb1, "xrC", 2, [128, D], F32)
                    ssc_r = Ring(sb1, "sscC", 2, [128, 1], F32)
                    u32_r = Ring(sb1, "u32C", 2, [128, D], F32)
                    uhT_r = Ring(sb1, "uhT", 2, [128, 8, 128], BF16)
                    tris = sb1("tris", [128, 128], BF16)
                    tmpf1 = sb1("tmpf1", [128, 128], F32)
                    p.op("pool", lambda e: e.memset(tmpf1[:], 1.0), writes=["tmpf1"])
                    p.op("pool", lambda e: e.affine_select(out=tmpf1[:], in_=tmpf1[:], pattern=[[1, 128]], compare_op=ALU.is_gt, fill=0.0,
                                                            base=0, channel_multiplier=-1), reads=["tmpf1"], writes=["tmpf1"])
                    p.op("dve", lambda e: e.tensor_copy(out=tris[:], in_=tmpf1[:]), reads=["tmpf1"], writes=["tris"])
                    gtmp_r = Ring(sb1, "gtmpC", 2, [128, NE], F32)
                    xnb_r = Ring(sb1, "xnbC", 2, [128, D], BF16)
                    ecap_i = sb1("ecap_i", [128, NE], I32)
                    ecap = sb1("ecap", [128, NE], F32)
                    p.op("pool", lambda e: e.iota(ecap_i[:], pattern=[[CAP, NE]], base=0, channel_multiplier=0), writes=["ecap_i"])
                    p.op("dve", lambda e: e.tensor_copy(out=ecap[:], in_=ecap_i[:]), reads=["ecap_i"], writes=["ecap"])
                    pq_r = Ring(sb1, "pq", 2, [128, NE], F32)
                    oh = sb1("oh", [128, NE], F32)
                    pr = sb1("pr", [128, NE], F32)
                    slf_r = Ring(sb1, "slf", 2, [128, 4], F32)
                    ulo_r = Ring(sb1, "uloC", 2, [128, D], BF16)
                    uloT_r = Ring(sb1, "uloT", 2, [128, 8, 128], BF16)
                    pTb_r = Ring(ps1, "pTb", 2, [128, 8, 128], BF16)
                    Wrh = sb1("Wrh", [128, 8, NE], BF16)
                    Wrl = sb1("Wrl", [128, 8, NE], BF16)
                    p.op("dve", lambda e: e.tensor_copy(out=Wrh[:], in_=Wr[:]), reads=["Wr"], writes=["Wrh"])
                    p.op("dve", lambda e: e.tensor_tensor(out=Wrl[:], in0=Wr[:], in1=Wrh[:], op=ALU.subtract), reads=["Wr", "Wrh"], writes=["Wrl"])
                    pLg_r = Ring(ps1, "pLg", 2, [128, 2, NE], F32)
                    lg_r = Ring(sb1, "lgC", 2, [128, NE], F32)
                    mx8_r = Ring(sb1, "mx8C", 2, [128, 8], F32)
                    msk_r = Ring(sb1, "mskC", 2, [128, NE], F32)
                    ex_r = Ring(sb1, "exC", 2, [128, NE], F32)
                    sm_r = Ring(sb1, "smC", 2, [128, 1], F32)
                    otv = OTd.rearrange("(c p) n -> p c n", p=128)
                    if stop_after == "C1a":
                        p.barrier()
                        p.op("sp", lambda e: e.dma_start(out=dbgH.rearrange("(t p) n -> p t n", p=128), in_=H[:]), reads=["H%d" % i_ for i_ in range(16)], dma="dbgH")
                        p.op("sp", lambda e: e.dma_start(out=dbgG.rearrange("(t p) n -> p t n", p=128), in_=Gt[:]), reads=["Gt%d" % i_ for i_ in range(16)], dma="dbgG")
                        p.finish_wait("sp"); p.emit(top); return nc
                    for G in range(4):
                        OT, kOT = OT_r.next()
                        p.op("sp", lambda e, OT=OT, G=G: e.dma_start(out=OT[:], in_=otv[:, :, G * 512:(G + 1) * 512]), writes=[kOT], dma=kOT)
                        rbs = []
                        for grp in range(2):
                            pss, kpss = pss_r.next()
                            for i in range(4):
                                c = grp * 4 + i
                                sq, ksq = sq_r.next()
                                p.op("act", lambda e, sq=sq, OT=OT, c=c: e.activation(out=sq[:], in_=OT[:, c, :], func=AF.Square), reads=[kOT], writes=[ksq])
                                p.op("pe", lambda e, sq=sq, pss=pss, i=i: e.matmul(out=pss[:], lhsT=ones[:], rhs=sq[:], start=(i == 0), stop=(i == 3)),
                                     reads=[ksq, "ones"], writes=[kpss])
                            rb, krb = rb_r.next()
                            rstd_from(rb[:], pss[:], 512, [kpss], [krb])
                            rbs.append((rb, krb))
                        for c in range(8):
                            rb, krb = rbs[c // 4]
                            p.op("dve", lambda e, c=c, rb=rb, OT=OT: e.scalar_tensor_tensor(out=MX[:, c, :], in0=OT[:, c, :], scalar=gcol[:, 5 + c:6 + c], in1=rb[:],
                                                                                        op0=ALU.mult, op1=ALU.mult),
                                 reads=[kOT, "gcol", krb], writes=["MX"])
                        def c1_tile(G, sub):
                            tile = G * 4 + sub
                            b_ = sub % 2
                            uhT, kuhT = uhT_r.tiles[b_], uhT_r.keys[b_]
                            uloT, kuloT = uloT_r.tiles[b_], uloT_r.keys[b_]
                            pq, kpq = pq_r.tiles[b_], pq_r.keys[b_]
                            slf, kslf = slf_r.tiles[b_], slf_r.keys[b_]
                            pLg2, kpLg = pLg_r.tiles[b_], pLg_r.keys[b_]
                            pLg = pLg2[:, 0, :]
                            pPos = pLg2[:, 1, :]
                            yield
                            xr, kxr = xr_r.next()
                            yield
                            p.op("sp", lambda e, xr=xr, tile=tile: e.dma_start(out=xr[:], in_=xo[tile * 128:(tile + 1) * 128, :]), writes=[kxr], dma=kxr)
                            yield
                            for half in range(2):
                                pA, kpA = pA_r.next()
                                for c in range(8):
                                    p.op("pe", lambda e, c=c, pA=pA, sub=sub, half=half: e.matmul(out=pA[:], lhsT=MX[:, c, sub * 128:(sub + 1) * 128],
                                                                                              rhs=Wo[:, c, half * 512:(half + 1) * 512], start=(c == 0), stop=(c == 7)),
                                         reads=["MX", "Wo"], writes=[kpA])
                                p.op("dve", lambda e, pA=pA, xr=xr, tile=tile, half=half: e.tensor_tensor(out=H[:, tile, half * 512:(half + 1) * 512], in0=pA[:],
                                                                                                      in1=xr[:, half * 512:(half + 1) * 512], op=ALU.add),
                                     reads=[kpA, kxr], writes=["H%d" % tile])
                            yield
                            yield
                            ssc, kss = ssc_r.next()
                            yield
                            p.op("act", lambda e, ssc=ssc, tile=tile: e.activation(out=junk[:], in_=H[:, tile, :], func=AF.Square, accum_out=ssc[:]),
                                 reads=["H%d" % tile], writes=["junkC", kss])
                            yield
                            rstd_from(ssc[:], ssc[:], D, [kss], [kss])
                            yield
                            u32, ku = u32_r.next()
                            yield
                            p.op("dve", lambda e, ssc=ssc, u32=u32, tile=tile: e.scalar_tensor_tensor(out=u32[:], in0=H[:, tile, :], scalar=ssc[:, 0:1], in1=gbc[:],
                                                                                                  op0=ALU.mult, op1=ALU.mult),
                                 reads=["H%d" % tile, kss, "gbc"], writes=[ku])
                            yield
                            xnb, kuhi = xnb_r.next()
                            yield
                            ulo, kulo = ulo_r.next()
                            yield
                            p.op("act", lambda e: e.copy(out=xnb[:], in_=u32[:]), reads=[ku], writes=[kuhi])
                            yield
                            p.op("pool", lambda e: e.dma_start(out=Ud[tile * 128:(tile + 1) * 128, :], in_=xnb[:]), reads=[kuhi], dma=kuhi + "s")
                            yield
                            p.op("dve", lambda e: e.tensor_tensor(out=ulo[:], in0=u32[:], in1=xnb[:], op=ALU.subtract), reads=[ku, kuhi], writes=[kulo])
                            yield
                            pT, kpT = pTb_r.next()
                            yield
                            for c in range(8):
                                p.op("pe", lambda e: e.transpose(out=pT[:, c, :], in_=xnb[:, c * 128:(c + 1) * 128], identity=ident[:]), reads=[kuhi, "ident"], writes=[kpT])
                            yield
                            p.op("dve", lambda e: e.tensor_copy(out=uhT[:], in_=pT[:]), reads=[kpT], writes=[kuhT])
                            yield
                            pT2, kpT2 = pTb_r.next()
                            yield
                            for c in range(8):
                                p.op("pe", lambda e: e.transpose(out=pT2[:, c, :], in_=ulo[:, c * 128:(c + 1) * 128], identity=ident[:]), reads=[kulo, "ident"], writes=[kpT2])
                            yield
                            p.op("act", lambda e: e.copy(out=uloT[:], in_=pT2[:]), reads=[kpT2], writes=[kuloT])
                            yield
                            n_ = 0
                            yield
                            for (A_, kA_, W_, kW_) in (("hi", kuhT, Wrh, "Wrh"), ("lo", kuloT, Wrh, "Wrh"), ("hi", kuhT, Wrl, "Wrl")):
                                for k in range(8):
                                    lh = uhT[:, k, :] if A_ == "hi" else uloT[:, k, :]
                                    p.op("pe", lambda e: e.matmul(out=pLg, lhsT=lh, rhs=W_[:, k, :], start=(n_ == 0), stop=(n_ == 23)),
                                         reads=[kA_, kW_], writes=[kpLg])
                                    n_ += 1
                            yield
                            lg, klg = lg_r.next()
                            yield
                            p.op("dve", lambda e, lg=lg: e.tensor_tensor(out=lg[:], in0=pLg, in1=brb[:], op=ALU.add), reads=[kpLg, "brb"], writes=[klg])
                            yield
                            mx8, kmx = mx8_r.next()
                            yield
                            p.op("dve", lambda e, lg=lg, mx8=mx8: e.max(out=mx8[:], in_=lg[:]), reads=[klg], writes=[kmx])
                            yield
                            msk, kmsk = msk_r.next()
                            yield
                            p.op("dve", lambda e, lg=lg, mx8=mx8, msk=msk: e.tensor_scalar(out=msk[:], in0=lg[:], scalar1=mx8[:, 3:4], scalar2=None, op0=ALU.is_ge),
                                 reads=[klg, kmx], writes=[kmsk])
                            yield
                            p.op("dve", lambda e, mx8=mx8: e.tensor_scalar(out=mx8[:, 7:8], in0=mx8[:, 0:1], scalar1=-1.0, scalar2=None, op0=ALU.mult),
                                 reads=[kmx, kmsk], writes=[kmx])
                            yield
                            ex, kex = ex_r.next()
                            yield
                            p.op("act", lambda e, lg=lg, mx8=mx8, ex=ex: e.activation(out=ex[:], in_=lg[:], func=AF.Exp, bias=mx8[:, 7:8]), reads=[klg, kmx], writes=[kex])
                            yield
                            sm_, ksm = sm_r.next()
                            yield
                            p.op("dve", lambda e, ex=ex, msk=msk: e.tensor_tensor(out=ex[:], in0=ex[:], in1=msk[:], op=ALU.mult), reads=[kex, kmsk], writes=[kex])
                            yield
                            p.op("dve", lambda e, ex=ex, sm_=sm_: e.reduce_sum(out=sm_[:], in_=ex[:], axis=mybir.AxisListType.X), reads=[kex], writes=[ksm])
                            yield
                            p.op("dve", lambda e, sm_=sm_: e.reciprocal(out=sm_[:], in_=sm_[:]), reads=[ksm], writes=[ksm])
                            yield
                            p.op("dve", lambda e, ex=ex, sm_=sm_, tile=tile: e.tensor_scalar(out=Gt[:, tile, :], in0=ex[:], scalar1=sm_[:, 0:1], scalar2=None, op0=ALU.mult),
                                 reads=[kex, ksm], writes=["Gt%d" % tile])
                            yield
                            p.op("dve", lambda e: e.tensor_copy(out=maskb[:, tile, :], in_=msk[:]), reads=[kmsk], writes=["maskb%d" % tile])
                            yield
                            gtmp, kgtmp = gtmp_r.next()
                            yield
                            p.op("pe", lambda e: e.matmul(out=pPos, lhsT=tris[:], rhs=maskb[:, tile, :], start=True, stop=(tile == 0)),
                                 reads=["tris", "maskb%d" % tile], writes=[kpLg])
                            yield
                            for j_ in range(tile):
                                p.op("pe", lambda e: e.matmul(out=pPos, lhsT=ones[:], rhs=maskb[:, j_, :], start=False, stop=(j_ == tile - 1)),
                                     reads=["ones", "maskb%d" % j_], writes=[kpLg])
                            yield
                            p.op("dve", lambda e: e.scalar_tensor_tensor(out=gtmp[:], in0=pPos, scalar=1.0, in1=msk[:], op0=ALU.add, op1=ALU.mult),
                                 reads=[kpLg, kmsk, kgtmp], writes=[kgtmp])
                            yield
                            p.op("dve", lambda e: e.tensor_scalar(out=posm[:, tile, :], in0=gtmp[:], scalar1=-1.0, scalar2=None, op0=ALU.add),
                                 reads=[kgtmp], writes=["posm%d" % tile])
                            yield
                            p.op("dve", lambda e: e.scalar_tensor_tensor(out=pq[:], in0=pPos, scalar=float(CAP - 1), in1=ecap[:], op0=ALU.min, op1=ALU.add),
                                 reads=[kpLg, "ecap"], writes=[kpq])
                            yield
                            yield
                            p.op("dve", lambda e: e.scalar_tensor_tensor(out=gtmp[:], in0=pPos, scalar=float(CAP) - 0.5, in1=Gt[:, tile, :], op0=ALU.is_lt, op1=ALU.mult),
                                 reads=[kpLg, "Gt%d" % tile, kgtmp], writes=[kgtmp])
                            yield
                            for j_ in range(4):
                                p.op("dve", lambda e: e.tensor_scalar(out=oh[:], in0=lg[:], scalar1=mx8[:, j_:j_ + 1], scalar2=None, op0=ALU.is_equal),
                                     reads=[klg, kmx], writes=["oh"])
                                p.op("dve", lambda e: e.tensor_tensor(out=pr[:], in0=oh[:], in1=pq[:], op=ALU.mult), reads=["oh", kpq], writes=["pr"])
                                p.op("dve", lambda e: e.reduce_sum(out=slf[:, j_:j_ + 1], in_=pr[:], axis=mybir.AxisListType.X), reads=["pr"], writes=[kslf])
                                p.op("dve", lambda e: e.tensor_tensor(out=pr[:], in0=oh[:], in1=gtmp[:], op=ALU.mult), reads=["oh", kgtmp, "pr"], writes=["pr"])
                                p.op("dve", lambda e: e.reduce_sum(out=gj[:, tile, j_:j_ + 1], in_=pr[:], axis=mybir.AxisListType.X), reads=["pr"], writes=["gj%d" % tile])
                            yield
                            p.op("dve", lambda e: e.tensor_copy(out=slI[:, tile, :], in_=slf[:]), reads=[kslf], writes=["slI%d" % tile])

                        for pair in range(2):
                            gens = [c1_tile(G, pair * 2), c1_tile(G, pair * 2 + 1)]
                            while gens:
                                for g_ in list(gens):
                                    try:
                                        next(g_)
                                    except StopIteration:
                                        gens.remove(g_)
                    p.barrier()
                    if stop_after == "C1":
                        if debug:
                            p.op("sp", lambda e: e.dma_start(out=dbgS.rearrange("(t p) n -> p t n", p=128), in_=slI[:]), reads=["slI%d" % i_ for i_ in range(16)], dma="dbgS")
                            p.op("sp", lambda e: e.dma_start(out=dbgJ.rearrange("(t p) n -> p t n", p=128), in_=gj[:]), reads=["gj%d" % i_ for i_ in range(16)], dma="dbgJ")
                            p.op("sp", lambda e: e.dma_start(out=dbgH.rearrange("(t p) n -> p t n", p=128), in_=H[:]), reads=["H%d" % i_ for i_ in range(16)], dma="dbgH")
                            p.op("sp", lambda e: e.dma_start(out=dbgG.rearrange("(t p) n -> p t n", p=128), in_=Gt[:]), reads=["Gt%d" % i_ for i_ in range(16)], dma="dbgG")
                        p.finish_wait("sp"); p.emit(top); return nc
                s2 = ExitStack()
                with s2:
                    sb2, ps2 = mk_alloc(s2)
                    W_r = Ring(sb2, "Wx", 6, [128, 8, 512], BF16)
                    bd_r = Ring(sb2, "bdn", 2, [1, D], BF16)
                    pGL_r = Ring(ps2, "pGL", 4, [128, 512], F32)
                    pA_r = Ring(ps2, "pA", 2, [128, 512], F32)
                    pTs = ps2("pTs", [128, 8, 128], BF16)
                    ptk = ps2("ptk", [128, 8], F32)
                    gl_r = Ring(sb2, "gl", 1, [128, CAP], F32)
                    sg_r = Ring(sb2, "sg", 1, [128, CAP], F32)
                    Sel = sb2("Sel", [128, 16, CAP], BF16)
                    Xe_r = Ring(sb2, "Xe", 2, [128, NS, D], BF16)
                    XeT = sb2("XeT", [128, 8, CAP], BF16)
                    aT = sb2("aTs", [128, 8, CAP], BF16)
                    Yst_r = Ring(sb2, "Yst", 2, [128, D], F32)
                    tks = sb2("tks", [128, 8], F32)
                    tkf = sb2("tkf", [128, 4], F32)
                    tkI_r = Ring(sb2, "tkI", 2, [128, 4], I32)
                    iota_i = sb2("iota_i", [128, CAP], I32)
                    iota_f = sb2("iota_f", [128, CAP], F32)
                    p.op("pool", lambda e: e.iota(iota_i[:], pattern=[[1, CAP]], base=0, channel_multiplier=0), writes=["iota_i"])
                    p.op("dve", lambda e: e.tensor_copy(out=iota_f[:], in_=iota_i[:]), reads=["iota_i"], writes=["iota_f"])
                    tid = sb2("tid", [128, 16], I32)
                    tidx = sb2("tidx", [128, 16], I32)
                    tidhl = sb2("tidhl", [128, 16, 2], BF16)
                    p.op("pool", lambda e: e.iota(tid[:], pattern=[[128, 16]], base=0, channel_multiplier=1), writes=["tid"])
                    p.op("dve", lambda e: e.tensor_scalar(out=tidx[:], in0=tid[:], scalar1=6, scalar2=None, op0=ALU.arith_shift_right), reads=["tid"], writes=["tidx"])
                    p.op("dve", lambda e: e.tensor_copy(out=tidhl[:, :, 0], in_=tidx[:]), reads=["tidx"], writes=["tidhl"])
                    p.op("dve", lambda e: e.tensor_scalar(out=tidx[:], in0=tid[:], scalar1=63, scalar2=None, op0=ALU.bitwise_and), reads=["tid", "tidx", "tidhl"], writes=["tidx"])
                    p.op("dve", lambda e: e.tensor_copy(out=tidhl[:, :, 1], in_=tidx[:]), reads=["tidx", "tidhl"], writes=["tidhl"])
                    bg3 = bgT[:].rearrange("p (e c) -> p e c", c=16)
                    p.op("dve", lambda e: e.tensor_scalar(out=bg3[:, :, 8:16], in0=bg3[:, :, 8:16], scalar1=1.0, scalar2=None, op0=ALU.add),
                         reads=["bgT"], writes=["bgT"])

                    def load_w(src, slot):
                        Wt, kW = W_r.tiles[slot], W_r.keys[slot]
                        p.op("pool", lambda e: e.dma_start(out=Wt[:], in_=src.rearrange("(c p) n -> p c n", p=128)), writes=[kW], dma=kW)
                        return Wt, kW

                    def load_bd(ex_):
                        bd, kbd = bd_r.next()
                        p.op("pool", lambda e: e.dma_start(out=bd[:], in_=b_dn[ex_:ex_ + 1, :]), writes=[kbd], dma=kbd)
                        return bd, kbd

                    xe_of = {}

                    def sel_build(ex_, tiles):
                        for tile in tiles:
                            p.op("dve", lambda e: e.tensor_scalar(out=Sel[:, tile, :], in0=iota_f[:], scalar1=posm[:, tile, ex_:ex_ + 1], scalar2=None, op0=ALU.is_equal),
                                 reads=["iota_f", "posm%d" % tile], writes=["Sel%d" % tile])

                    def dispatch(ex_, build=True):
                        if build:
                            sel_build(ex_, range(16))
                        for s_ in range(NS):
                            for tile in range(16):
                                p.op("pe", lambda e: e.matmul(out=ptk[:, 2 * s_:2 * s_ + 2], lhsT=Sel[:, tile, s_ * 128:(s_ + 1) * 128], rhs=tidhl[:, tile, :],
                                                              start=(tile == 0), stop=(tile == 15)), reads=["Sel%d" % tile, "tidhl"], writes=["ptk"])
                        p.op("dve", lambda e: e.tensor_copy(out=tks[:, 0:2 * NS], in_=ptk[:, 0:2 * NS]), reads=["ptk"], writes=["tks"])
                        tk3 = tks[:, 0:2 * NS].rearrange("p (s t) -> p s t", t=2)
                        p.op("dve", lambda e: e.scalar_tensor_tensor(out=tkf[:, 0:NS], in0=tk3[:, :, 0], scalar=64.0, in1=tk3[:, :, 1], op0=ALU.mult, op1=ALU.add),
                             reads=["tks"], writes=["tkf"])
                        tkI, ktkI = tkI_r.next()
                        p.op("dve", lambda e: e.tensor_copy(out=tkI[:, 0:NS], in_=tkf[:, 0:NS]), reads=["tkf"], writes=[ktkI])
                        Xe, kXe = Xe_r.next()
                        for s_ in range(NS):
                            p.op("pool", lambda e: e.indirect_dma_start(out=Xe[:, s_, :], out_offset=None, in_=Ud[:, :],
                                                                         in_offset=bass.IndirectOffsetOnAxis(ap=tkI[:, s_:s_ + 1], axis=0)),
                                 reads=[ktkI], writes=[kXe], dma=kXe)
                        xe_of[ex_] = (Xe, kXe)

                    def transposes(ex_):
                        Xe, kXe = xe_of.pop(ex_)
                        for s_ in range(NS):
                            for k in range(8):
                                p.op("pe", lambda e: e.transpose(out=pTs[:, k, :], in_=Xe[:, s_, k * 128:(k + 1) * 128], identity=ident[:]),
                                     reads=[kXe, "ident"], writes=["pTs"])
                            if s_ % 2 == 0:
                                p.op("act", lambda e: e.copy(out=XeT[:, :, s_ * 128:(s_ + 1) * 128], in_=pTs[:]), reads=["pTs"], writes=["XeT"])
                            else:
                                p.op("dve", lambda e: e.tensor_copy(out=XeT[:, :, s_ * 128:(s_ + 1) * 128], in_=pTs[:]), reads=["pTs"], writes=["XeT"])

                    def gu_stage(ex_, st, Wg, kWg, Wl, kWl, sel_for=None):
                        for mc in range(4):
                            if sel_for is not None:
                                sel_build(sel_for, range(mc * 4, mc * 4 + 4))
                            c = st * 4 + mc
                            pG, kpG = pGL_r.next()
                            pLn, kpLn = pGL_r.next()
                            for k in range(8):
                                p.op("pe", lambda e: e.matmul(out=pG[:, 0:CAP], lhsT=Wg[:, k, mc * 128:(mc + 1) * 128], rhs=XeT[:, k, :],
                                                              start=(k == 0), stop=(k == 7)), reads=[kWg, "XeT"], writes=[kpG])
                            for k in range(8):
                                p.op("pe", lambda e: e.matmul(out=pLn[:, 0:CAP], lhsT=Wl[:, k, mc * 128:(mc + 1) * 128], rhs=XeT[:, k, :],
                                                              start=(k == 0), stop=(k == 7)), reads=[kWl, "XeT"], writes=[kpLn])
                            gl, kgl = gl_r.next()
                            sg, ksg = sg_r.next()
                            bgc = ex_ * 16 + c
                            blc = ex_ * 16 + 8 + c
                            kaT = "aT_%d" % st
                            p.op("dve", lambda e: e.tensor_scalar(out=gl[:], in0=pG[:, 0:CAP], scalar1=bgT[:, bgc:bgc + 1], scalar2=7.0, op0=ALU.add, op1=ALU.min),
                                 reads=[kpG, "bgT"], writes=[kgl])
                            p.op("act", lambda e: e.activation(out=sg[:], in_=gl[:], func=AF.Sigmoid, scale=1.702), reads=[kgl], writes=[ksg])
                            p.op("dve", lambda e: e.tensor_tensor(out=sg[:], in0=sg[:], in1=gl[:], op=ALU.mult), reads=[kgl, ksg], writes=[ksg])
                            p.op("dve", lambda e: e.tensor_scalar(out=gl[:], in0=pLn[:, 0:CAP], scalar1=bgT[:, blc:blc + 1], scalar2=-6.0, op0=ALU.add, op1=ALU.max),
                                 reads=[kpLn, "bgT", kgl], writes=[kgl])
                            p.op("dve", lambda e: e.scalar_tensor_tensor(out=aT[:, c, :], in0=gl[:], scalar=8.0, in1=sg[:], op0=ALU.min, op1=ALU.mult),
                                 reads=[ksg, kgl], writes=[kaT])

                    def dn_stage(ex_, d0, d1, bd, kbd):
                        for s_ in range(NS):
                            Yst, kY = Yst_r.next()
                            for half in range(2):
                                Wd, kWd = (d0, d1)[half]
                                pA, kpA = pA_r.next()
                                for c in range(8):
                                    p.op("pe", lambda e: e.matmul(out=pA[:], lhsT=aT[:, c, s_ * 128:(s_ + 1) * 128], rhs=Wd[:, c, :], start=(c == 0), stop=False),
                                         reads=["aT_%d" % (c // 4), kWd], writes=[kpA])
                                p.op("pe", lambda e: e.matmul(out=pA[:], lhsT=ones[0:1, :], rhs=bd[0:1, half * 512:(half + 1) * 512], start=False, stop=True),
                                     reads=["ones", kbd], writes=[kpA])
                                p.op("act", lambda e: e.copy(out=Yst[:, half * 512:(half + 1) * 512], in_=pA[:]), reads=[kpA], writes=[kY])
                            r0 = ex_ * CAP + s_ * 128
                            p.op("sp", lambda e: e.dma_start(out=Yd[r0:r0 + 128, :], in_=Yst[:]), reads=[kY], dma=kY + "s")

                    def loads_gl0(ex_):
                        return load_w(w_gu[ex_, :, 0:512], 0), load_w(w_gu[ex_, :, 1024:1536], 1)

                    def loads_gl1(ex_):
                        return load_w(w_gu[ex_, :, 512:1024], 2), load_w(w_gu[ex_, :, 1536:2048], 3)

                    def loads_d(ex_):
                        return load_w(w_dn[ex_, :, 0:512], 4), load_w(w_dn[ex_, :, 512:1024], 5), load_bd(ex_)

                    g0, l0 = loads_gl0(0)
                    g1, l1 = loads_gl1(0)
                    d0, d1, (bd, kbd) = loads_d(0)
                    dispatch(0)
                    dispatch(1)
                    for ex_ in range(NE):
                        transposes(ex_)
                        gu_stage(ex_, 0, g0[0], g0[1], l0[0], l0[1], sel_for=(ex_ + 2 if ex_ + 2 < NE else None))
                        if ex_ + 1 < NE:
                            g0n, l0n = loads_gl0(ex_ + 1)
                        if ex_ + 2 < NE:
                            dispatch(ex_ + 2, build=False)
                        gu_stage(ex_, 1, g1[0], g1[1], l1[0], l1[1])
                        if ex_ + 1 < NE:
                            g1n, l1n = loads_gl1(ex_ + 1)
                        dn_stage(ex_, d0, d1, bd, kbd)
                        if ex_ + 1 < NE:
                            d0, d1, (bd, kbd) = loads_d(ex_ + 1)
                            g0, l0, g1, l1 = g0n, l0n, g1n, l1n
                    p.barrier()
                    Yg_r = Ring(sb2, "Yg", 4, [128, D], F32)
                    for tile in range(16):
                        hk = "H%d" % tile
                        for j_ in range(4):
                            Yg, kYg = Yg_r.next()
                            p.op("pool", lambda e: e.indirect_dma_start(out=Yg[:, :], out_offset=None, in_=Yd[:, :],
                                                                         in_offset=bass.IndirectOffsetOnAxis(ap=slI[:, tile, j_:j_ + 1], axis=0)),
                                 reads=["slI%d" % tile], writes=[kYg], dma=kYg)
                            p.op("dve", lambda e: e.scalar_tensor_tensor(out=H[:, tile, :], in0=Yg[:], scalar=gj[:, tile, j_:j_ + 1], in1=H[:, tile, :], op0=ALU.mult, op1=ALU.add),
                                 reads=[kYg, "gj%d" % tile, hk], writes=[hk])
                    p.barrier()
                    if stop_after == "C2":
                        if debug:
                            p.op("sp", lambda e: e.dma_start(out=dbgH.rearrange("(t p) n -> p t n", p=128), in_=H[:]), reads=["H%d" % i_ for i_ in range(16)], dma="dbgH")
                            p.op("sp", lambda e: e.dma_start(out=dbgG.rearrange("(t p) n -> p t n", p=128), in_=Gt[:]), reads=["Gt%d" % i_ for i_ in range(16)], dma="dbgG")
                        p.finish_wait("sp"); p.emit(top); return nc
                s3 = ExitStack()
                with s3:
                    sb3, ps3 = mk_alloc(s3)
                    gbc = sb3("gbc3", [128, D], F32)
                    junk = sb3("junkC3", [128, D], BF16)
                    Wpg = sb3("Wpg", [128, 8, D], BF16)
                    Wpp = sb3("Wpp", [128, 2, D], BF16)
                    p.op("pool", lambda e: e.dma_start(out=Wpg[:], in_=w_pg.rearrange("(c p) n -> p c n", p=128)), writes=["Wpg"], dma="Wpg")
                    p.op("pool", lambda e: e.dma_start(out=Wpp[:], in_=w_pp.rearrange("(c p) n -> p c n", p=128)), writes=["Wpp"], dma="Wpp")
                    gfin = sb3("gfin", [128, D], F32)
                    p.op("sp", lambda e: e.dma_start(out=gbc[:], in_=g_ple.partition_broadcast(128)), writes=["gbc"], dma="gbc3")
                    p.op("sp", lambda e: e.dma_start(out=gfin[:], in_=g_final.partition_broadcast(128)), writes=["gfin"], dma="gfin")
                    ss3_r = Ring(sb3, "ss3", 2, [128, 1], F32)
                    u3_r = Ring(sb3, "u3", 2, [128, D], BF16)
                    pT3_r = Ring(ps3, "pT3", 2, [128, 8, 128], BF16)
                    u3T_r = Ring(sb3, "u3T", 2, [128, 8, 128], BF16)
                    pp_r = Ring(sb3, "ppl", 2, [128, 256], F32)
                    ppb_r = Ring(sb3, "ppb", 2, [128, 256], BF16)
                    ppT_r = Ring(sb3, "ppT", 2, [128, 2, 128], BF16)
                    pg_r = Ring(ps3, "pg3", 2, [128, 512], F32)
                    pj_r = Ring(ps3, "pj3", 2, [128, 512], F32)
                    sg3_r = Ring(sb3, "sg3", 2, [128, 512], F32)
                    o_r = Ring(sb3, "o3", 2, [128, D], F32)
                    def c3_tile(tile):
                        hk = "H%d" % tile
                        yield
                        ss3, kss = ss3_r.next()
                        yield
                        p.op("act", lambda e, ss3=ss3, tile=tile: e.activation(out=junk[:], in_=H[:, tile, :], func=AF.Square, accum_out=ss3[:]), reads=[hk], writes=["junkC", kss])
                        yield
                        rstd_from(ss3[:], ss3[:], D, [kss], [kss])
                        yield
                        u3, ku3 = u3_r.next()
                        yield
                        p.op("dve", lambda e, ss3=ss3, u3=u3, tile=tile: e.scalar_tensor_tensor(out=u3[:], in0=H[:, tile, :], scalar=ss3[:, 0:1], in1=gbc[:], op0=ALU.mult, op1=ALU.mult),
                             reads=[hk, kss, "gbc"], writes=[ku3])
                        yield
                        pT, kpT = pT3_r.next()
                        yield
                        for c in range(8):
                            p.op("pe", lambda e, c=c, pT=pT, u3=u3: e.transpose(out=pT[:, c, :], in_=u3[:, c * 128:(c + 1) * 128], identity=ident[:]), reads=[ku3, "ident"], writes=[kpT])
                        yield
                        u3T, ku3T = u3T_r.next()
                        yield
                        p.op("act", lambda e, pT=pT, u3T=u3T: e.copy(out=u3T[:], in_=pT[:]), reads=[kpT], writes=[ku3T])
                        yield
                        pp, kpp = pp_r.next()
                        yield
                        p.op("sp", lambda e, pp=pp, tile=tile: e.dma_start(out=pp[:], in_=po[tile * 128:(tile + 1) * 128, :]), writes=[kpp], dma=kpp)
                        yield
                        ppb, kppb = ppb_r.next()
                        yield
                        p.op("pool", lambda e, pp=pp, ppb=ppb: e.tensor_copy(out=ppb[:], in_=pp[:]), reads=[kpp], writes=[kppb])
                        yield
                        pT2, kpT2 = pT3_r.next()
                        yield
                        for c in range(2):
                            p.op("pe", lambda e, c=c, pT2=pT2, ppb=ppb: e.transpose(out=pT2[:, c, :], in_=ppb[:, c * 128:(c + 1) * 128], identity=ident[:]), reads=[kppb, "ident"], writes=[kpT2])
                        yield
                        ppT, kppT = ppT_r.next()
                        yield
                        p.op("act", lambda e, pT2=pT2, ppT=ppT: e.copy(out=ppT[:], in_=pT2[:, 0:2, :]), reads=[kpT2], writes=[kppT])
                        yield
                        for half in range(2):
                            pg, kpg = pg_r.next()
                            pj, kpj = pj_r.next()
                            for c in range(8):
                                p.op("pe", lambda e, c=c, pg=pg, u3T=u3T, half=half: e.matmul(out=pg[:], lhsT=u3T[:, c, :], rhs=Wpg[:, c, half * 512:(half + 1) * 512], start=(c == 0), stop=(c == 7)),
                                     reads=[ku3T, "Wpg"], writes=[kpg])
                            for c in range(2):
                                p.op("pe", lambda e, c=c, pj=pj, ppT=ppT, half=half: e.matmul(out=pj[:], lhsT=ppT[:, c, :], rhs=Wpp[:, c, half * 512:(half + 1) * 512], start=(c == 0), stop=(c == 1)),
                                     reads=[kppT, "Wpp"], writes=[kpj])
                            sg, ksg = sg3_r.next()
                            p.op("act", lambda e, sg=sg, pg=pg: e.activation(out=sg[:], in_=pg[:], func=AF.Sigmoid), reads=[kpg], writes=[ksg])
                            p.op("dve", lambda e, sg=sg, pj=pj: e.tensor_tensor(out=sg[:], in0=sg[:], in1=pj[:], op=ALU.mult), reads=[ksg, kpj], writes=[ksg])
                            p.op("dve", lambda e, sg=sg, tile=tile, half=half: e.tensor_tensor(out=H[:, tile, half * 512:(half + 1) * 512], in0=H[:, tile, half * 512:(half + 1) * 512], in1=sg[:], op=ALU.add),
                                 reads=[ksg, hk], writes=[hk])
                        yield
                        ss4, kss4 = ss3_r.next()
                        yield
                        p.op("act", lambda e, ss4=ss4, tile=tile: e.activation(out=junk[:], in_=H[:, tile, :], func=AF.Square, accum_out=ss4[:]), reads=[hk], writes=["junkC", kss4])
                        yield
                        rstd_from(ss4[:], ss4[:], D, [kss4], [kss4])
                        yield
                        ot, kot = o_r.next()
                        yield
                        p.op("dve", lambda e, ss4=ss4, ot=ot, tile=tile: e.scalar_tensor_tensor(out=ot[:], in0=H[:, tile, :], scalar=ss4[:, 0:1], in1=gfin[:], op0=ALU.mult, op1=ALU.mult),
                             reads=[hk, kss4, "gfin"], writes=[kot])
                        yield
                        p.op("sp", lambda e, ot=ot, tile=tile: e.dma_start(out=yo[tile * 128:(tile + 1) * 128, :], in_=ot[:]), reads=[kot], writes=["yo"], dma=kot + "s")

                    for pair in range(8):
                        gens = [c3_tile(pair * 2), c3_tile(pair * 2 + 1)]
                        while gens:
                            for g_ in list(gens):
                                try:
                                    next(g_)
                                except StopIteration:
                                    gens.remove(g_)
        p.finish_wait("sp")
        p.emit(top)
    return nc


_CACHE = {}


def _perm(j):
    idx = []
    for t in range(4):
        for blk in ORDER[j]:
            b0 = (8 * t + blk) * 128
            idx.append(np.arange(b0, b0 + 128))
    return np.concatenate(idx)


def kernel(x, p, positions, w_in, g_attn, g_cq, w_uq, g_ckv, w_ukv, g_out_mla, g_out_sb, w_o,
           g_moe, w_router, b_router, w_gu, b_gu, w_dn, b_dn, g_ple, w_ple_gate, w_ple_proj, g_final):
    if "nc" not in _CACHE:
        _CACHE["nc"] = build_program()
    nc = _CACHE["nc"]
    in_maps, perms = make_in_maps(x, p, positions, w_in, g_attn, g_cq, w_uq, g_ckv, w_ukv, g_out_mla, g_out_sb, w_o,
                                  g_moe, w_router, b_router, w_gu, b_gu, w_dn, b_dn, g_ple, w_ple_gate, w_ple_proj, g_final)
    res = run_bass_kernel_spmd(nc, in_maps, core_ids=list(range(8)))
    out = np.empty((4, S, D), np.float32)
    for c in range(8):
        b, j = c // 2, c % 2
        out[b, perms[j]] = np.asarray(res.results[c]["yo"])
    return out


def make_in_maps(x, p, positions, w_in, g_attn, g_cq, w_uq, g_ckv, w_ukv, g_out_mla, g_out_sb, w_o,
                 g_moe, w_router, b_router, w_gu, b_gu, w_dn, b_dn, g_ple, w_ple_gate, w_ple_proj, g_final):
    f = lambda a: np.ascontiguousarray(np.asarray(a))
    x = f(x); p = f(p); positions = f(positions)
    invf = np.zeros((128, 1), np.float32)
    fr = (10000.0 ** (-np.arange(0, 32, 2, dtype=np.float32) / 32.0)).astype(np.float32)
    invf[0:16, 0] = fr
    invf[16:32, 0] = fr
    shared = {
        "invf": invf,
        "w_in": f(w_in[0]), "g_attn": f(g_attn[0:1]), "g_cq": f(g_cq[0:1]), "w_uq": f(w_uq[0]),
        "g_ckv": f(g_ckv[0:1]), "w_ukv": f(w_ukv[0]),
        "g_out": f(np.concatenate([np.asarray(g_out_mla[0]), np.asarray(g_out_sb[0])])[None, :]),
        "w_o": f(w_o[0]), "g_moe": f(g_moe[0:1]), "w_router": f(w_router[0]), "b_router": f(b_router[0:1]),
        "w_gu": f(w_gu[0]), "b_gu": f(np.asarray(b_gu[0]).reshape(NE * 16, 128)), "w_dn": f(w_dn[0]), "b_dn": f(b_dn[0]),
        "g_ple": f(g_ple[0:1]), "w_pg": f(w_ple_gate[0]), "w_pp": f(w_ple_proj[0]), "g_final": f(np.asarray(g_final)[None, :]),
    }
    in_maps = []
    perms = [_perm(0), _perm(1)]
    for c in range(8):
        b, j = c // 2, c % 2
        pm = perms[j]
        qr = np.concatenate([np.arange(blk * 128, blk * 128 + 128) for blk in ORDER[j]]).astype(np.float32)[None, :]
        m = dict(shared)
        m["xa"] = x[b]
        m["xo"] = f(x[b][pm])
        m["po"] = f(p[0, b][pm])
        m["posa"] = f(positions[b:b + 1].astype(np.int32))
        m["poso"] = f(positions[b:b + 1, pm].astype(np.int32))
        m["qrel"] = f(qr)
        in_maps.append(m)
    return in_maps, perms
```

```python
from contextlib import ExitStack
import numpy as np
import concourse.bass as bass
import concourse.mybir as mybir
from concourse.bass_utils import run_bass_kernel_spmd

F32 = mybir.dt.float32
BF16 = mybir.dt.bfloat16
I32 = mybir.dt.int32
AF = mybir.ActivationFunctionType
ALU = mybir.AluOpType

ENGS = ("pe", "act", "dve", "pool", "sp")
S = 4096
D = 1024
NE = 32
ORDER = ([6, 5, 3, 0], [7, 4, 2, 1])
NDIAG = [512, 512, 384, 384, 256, 256, 128, 128]
NEG = -30000.0
EPS = 1e-6


class Prog:
    def __init__(self, nc):
        self.nc = nc
        self.ops = {e: [] for e in ENGS}
        self.vcs = {}
        self.cur = {e: {} for e in ENGS}
        self.last_w = {}
        self.readers = {}
        self.excl = set()

    def op(self, eng, fn, reads=(), writes=(), dma=None):
        rec_ = _Rec()
        fn(rec_)
        assert len(rec_.calls) == 1
        fn = rec_.calls[0]
        clk = ("dma:" + dma) if dma else eng
        deps = []
        reads = list(reads)
        writes = list(writes)
        for k in reads:
            if k in self.excl and k not in writes:
                writes.append(k)
        for k in reads:
            lw = self.last_w.get(k)
            if lw:
                deps.append(lw)
        for k in writes:
            lw = self.last_w.get(k)
            if lw:
                deps.append(lw)
            for c, i in self.readers.get(k, {}).items():
                deps.append((c, i))
        cur = self.cur[eng]
        wmax = {}
        for (c, i) in deps:
            if c == "pe" and eng == "pe" and not dma:
                continue
            if cur.get(c, 0) >= i:
                continue
            wmax[c] = max(wmax.get(c, 0), i)
            for c2, i2 in self.vcs[c][i - 1].items():
                if cur.get(c2, 0) < i2:
                    cur[c2] = i2
            if cur.get(c, 0) < i:
                cur[c] = i
        vc = dict(cur)
        lst = self.vcs.setdefault(clk, [])
        lst.append(vc)
        idx = len(lst)
        vc[clk] = idx
        rec = {"fn": fn, "waits": wmax, "clk": clk, "idx": idx}
        self.ops[eng].append(rec)
        for k in reads:
            self.readers.setdefault(k, {})[clk] = idx
        for k in writes:
            self.last_w[k] = (clk, idx)
            self.readers[k] = {}
        return rec

    def finish_wait(self, eng):
        waits = {}
        for c, l in self.vcs.items():
            if len(l) and self.cur[eng].get(c, 0) < len(l):
                waits[c] = len(l)
                self.cur[eng][c] = len(l)
        self.ops[eng].append({"fn": None, "waits": waits, "clk": None, "idx": None})

    def barrier(self):
        for e in ENGS:
            self.finish_wait(e)
        full = {c: len(l) for c, l in self.vcs.items()}
        for e in ENGS:
            self.cur[e] = dict(full)

    def emit(self, stack):
        nc = self.nc
        waited = {}
        for e in ENGS:
            for r in self.ops[e]:
                for c, i in r["waits"].items():
                    waited.setdefault(c, set()).add(i)
        sems, semval = {}, {}
        for c, l in self.vcs.items():
            if c not in waited:
                continue
            sems[c] = stack.enter_context(nc.semaphore("s_" + c.replace(":", "_")))
            isd = c.startswith("dma:")
            v, m = 0, {}
            for i in range(1, len(l) + 1):
                if isd or i in waited[c]:
                    v += 16 if isd else 1
                    m[i] = v
            semval[c] = m
        block = stack.enter_context(nc.Block())
        engobj = {"pe": "tensor", "act": "scalar", "dve": "vector", "pool": "gpsimd", "sp": "sync"}

        def make(e):
            def body(eng):
                for r in self.ops[e]:
                    for c, i in r["waits"].items():
                        eng.wait_ge(sems[c], semval[c][i])
                    if r["fn"] is None:
                        continue
                    name, a, k = r["fn"]
                    ins = getattr(eng, name)(*a, **k)
                    c, i = r["clk"], r["idx"]
                    if c in sems and i in semval[c]:
                        ins.then_inc(sems[c], 16 if c.startswith("dma:") else 1)
            return body

        for e in ENGS:
            if self.ops[e]:
                getattr(block, engobj[e])(make(e))


class _Rec:
    def __init__(self):
        self.calls = []

    def __getattr__(self, name):
        def f(*a, **k):
            self.calls.append((name, a, k))
        return f


class Ring:
    def __init__(self, alloc, name, n, shape, dtype):
        self.tiles = [alloc("%s%d" % (name, i), shape, dtype) for i in range(n)]
        self.keys = ["%s%d" % (name, i) for i in range(n)]
        self.i = 0

    def next(self):
        t, k = self.tiles[self.i % len(self.tiles)], self.keys[self.i % len(self.tiles)]
        self.i += 1
        return t, k


class _Stop(Exception):
    pass


def build_program(stop_after=None, debug=False):
    nc = bass.Bass("TRN2", target_bir_lowering=False)

    def din(name, shape, dt=F32):
        return nc.dram_tensor(name, list(shape), dt, kind="ExternalInput").ap()

    def dscr(name, shape, dt=BF16):
        return nc.dram_tensor(name, list(shape), dt, kind="ExternalOutput" if debug else "Internal").ap()

    xa = din("xa", [S, D])
    xo = din("xo", [2048, D])
    po = din("po", [2048, 256])
    posa = din("posa", [1, S], I32)
    poso = din("poso", [1, 2048], I32)
    qrel = din("qrel", [1, 512])
    invf = din("invf", [128, 1])
    w_in = din("w_in", [D, 2208])
    g_attn = din("g_attn", [1, D])
    g_cq = din("g_cq", [1, 384])
    w_uq = din("w_uq", [384, 768])
    g_ckv = din("g_ckv", [1, 256])
    w_ukv = din("w_ukv", [256, 1024])
    g_out = din("g_out", [1, 1024])
    w_o = din("w_o", [D, D])
    g_moe = din("g_moe", [1, D])
    w_router = din("w_router", [D, NE])
    b_router = din("b_router", [1, NE])
    NEd = NE if stop_after in (None, "C2") else 1
    w_gu = din("w_gu", [NEd, D, 2048])
    b_gu = din("b_gu", [NE * 16, 128])
    w_dn = din("w_dn", [NEd, D, D])
    b_dn = din("b_dn", [NE, D])
    g_ple = din("g_ple", [1, D])
    w_pg = din("w_pg", [D, D])
    w_pp = din("w_pp", [256, D])
    g_final = din("g_final", [1, D])
    yo = nc.dram_tensor("yo", [2048, D], F32, kind="ExternalOutput").ap()

    KnT = dscr("KnT", [512, S])
    KrT = dscr("KrT", [32, S])
    VmD = dscr("VmD", [S, 512])
    KsT = dscr("KsT", [512, S])
    VsD = dscr("VsD", [S, 512])
    QmT = dscr("QmT", [8 * 96, 2048])
    QsT = dscr("QsT", [512, 2048])
    OTd = dscr("OTd", [1024, 2048])
    CAP = 512
    NS = CAP // 128
    Ud = nc.dram_tensor("Ud", [2048, D], BF16, kind="Internal").ap()
    Yd = nc.dram_tensor("Yd", [NE * CAP, D], F32, kind="Internal").ap()
    if debug:
        dbgH = nc.dram_tensor("dbgH", [2048, D], F32, kind="ExternalOutput").ap()
        dbgG = nc.dram_tensor("dbgG", [2048, NE], F32, kind="ExternalOutput").ap()
        dbgS = nc.dram_tensor("dbgS", [2048, 4], I32, kind="ExternalOutput").ap()
        dbgJ = nc.dram_tensor("dbgJ", [2048, 4], F32, kind="ExternalOutput").ap()

    top = ExitStack()
    with top:
        p = Prog(nc)
        if True:

            def mk_alloc(stack):
                def sb(n, s, d):
                    return stack.enter_context(nc.sbuf_tensor(n, list(s), d))

                def ps(n, s, d=F32):
                    return stack.enter_context(nc.psum_tensor(n, list(s), d))
                return sb, ps

            sb0, ps0 = mk_alloc(top)

            identf = sb0("identf", [128, 128], F32)
            ident = sb0("ident", [128, 128], BF16)
            ones = sb0("ones", [128, 128], BF16)
            ntri = sb0("ntri", [128, 128], BF16)
            nones = sb0("nones", [128, 128], BF16)
            zeros = sb0("zeros", [128, 128], BF16)
            p.op("pool", lambda e: e.memset(identf[:], 0.0), writes=["identf"])
            p.op("pool", lambda e: e.affine_select(out=identf[:], in_=identf[:], pattern=[[-1, 128]],
                                                    compare_op=ALU.not_equal, fill=1.0, base=0, channel_multiplier=1),
                 reads=["identf"], writes=["identf"])
            p.op("dve", lambda e: e.tensor_copy(out=ident[:], in_=identf[:]), reads=["identf"], writes=["ident"])
            p.op("pool", lambda e: e.memset(ones[:], 1.0), writes=["ones"])
            p.op("pool", lambda e: e.memset(nones[:], -1.0), writes=["nones"])
            p.op("pool", lambda e: e.memset(zeros[:], 0.0), writes=["zeros"])
            gcol = sb0("gcol", [128, 16], F32)
            p.op("sp", lambda e: e.dma_start(out=gcol[:, 0:3], in_=g_cq.rearrange("o (c p) -> p (o c)", p=128),
                                             allow_slow_non_contiguous=True), writes=["gcol"], dma="gcol")
            p.op("sp", lambda e: e.dma_start(out=gcol[:, 3:5], in_=g_ckv.rearrange("o (c p) -> p (o c)", p=128),
                                             allow_slow_non_contiguous=True), writes=["gcol"], dma="gcol")
            p.op("sp", lambda e: e.dma_start(out=gcol[:, 5:13], in_=g_out.rearrange("o (c p) -> p (o c)", p=128),
                                             allow_slow_non_contiguous=True), writes=["gcol"], dma="gcol")
            invc = sb0("invc", [128, 1], F32)
            p.op("sp", lambda e: e.dma_start(out=invc[:], in_=invf), writes=["invc"], dma="invc")

            def rstd_from(eng_out, src, n, rd, wr):
                p.op("act", lambda e: e.activation(out=eng_out, in_=src, func=AF.Ln, scale=1.0 / n, bias=EPS),
                     reads=rd, writes=wr)
                p.op("act", lambda e: e.activation(out=eng_out, in_=eng_out, func=AF.Exp, scale=-0.5),
                     reads=wr, writes=wr)

            sa = ExitStack()
            with sa:
                sb, ps = mk_alloc(sa)
                tmpf = sb("tmpf", [128, 128], F32)
                p.op("pool", lambda e: e.memset(tmpf[:], -1.0), writes=["tmpf"])
                p.op("pool", lambda e: e.affine_select(out=tmpf[:], in_=tmpf[:], pattern=[[-1, 128]],
                                                        compare_op=ALU.is_ge, fill=0.0, base=0, channel_multiplier=1),
                     reads=["tmpf"], writes=["tmpf"])
                p.op("dve", lambda e: e.tensor_copy(out=ntri[:], in_=tmpf[:]), reads=["tmpf"], writes=["ntri"])

                Win = sb("Win", [128, 8, 2208], BF16)
                for c in range(8):
                    p.op("pool", lambda e, c=c: e.dma_start(out=Win[:, c, :], in_=w_in[c * 128:(c + 1) * 128, :]),
                         writes=["Win"], dma="Win")
                Wuq = sb("Wuq", [128, 3, 768], BF16)
                Wuqr = sb("Wuqr", [128, 3, 768], BF16)
                p.op("pool", lambda e: e.dma_start(out=Wuq[:], in_=w_uq.rearrange("(c p) n -> p c n", p=128)),
                     writes=["Wuq"], dma="Wuq")
                Wkn = sb("Wkn", [128, 2, 512], BF16)
                Wv = sb("Wv", [128, 2, 512], BF16)
                ukv = w_ukv.rearrange("(c p) (h t d) -> p c h t d", p=128, h=8, t=2)
                for c in range(2):
                    p.op("pool", lambda e, c=c: e.dma_start(out=Wkn[:, c, :].rearrange("p (h d) -> p h d", h=8),
                                                          in_=ukv[:, c, :, 0, :]), writes=["Wkn"], dma="Wkn")
                    p.op("pool", lambda e, c=c: e.dma_start(out=Wv[:, c, :].rearrange("p (h d) -> p h d", h=8),
                                                          in_=ukv[:, c, :, 1, :]), writes=["Wv"], dma="Wv")
                p.op("pool", lambda e: e.memset(Wuqr[:], 0.0), writes=["Wuqr"])
                Wq4 = Wuq[:].rearrange("p c (h d) -> p c h d", h=8)
                Wr4 = Wuqr[:].rearrange("p c (h d) -> p c h d", h=8)
                for c in range(3):
                    p.op("dve", lambda e, c=c: e.tensor_scalar(out=Wr4[:, c, :, 64:80], in0=Wq4[:, c, :, 80:96], scalar1=-1.0,
                                                          scalar2=None, op0=ALU.mult), reads=["Wuq", "Wuqr"], writes=["Wuqr"])
                    p.op("dve", lambda e, c=c: e.tensor_copy(out=Wr4[:, c, :, 80:96], in_=Wq4[:, c, :, 64:80]),
                         reads=["Wuq", "Wuqr"], writes=["Wuqr"])
                Wkr = sb("Wkr", [128, 8, 64], BF16)
                p.op("dve", lambda e: e.tensor_copy(out=Wkr[:, :, 0:32], in_=Win[:, :, 640:672]), reads=["Win"], writes=["Wkr"])
                p.op("dve", lambda e: e.tensor_scalar(out=Wkr[:, :, 32:48], in0=Win[:, :, 656:672], scalar1=-1.0, scalar2=None,
                                                      op0=ALU.mult), reads=["Win", "Wkr"], writes=["Wkr"])
                p.op("dve", lambda e: e.tensor_copy(out=Wkr[:, :, 48:64], in_=Win[:, :, 640:656]), reads=["Win", "Wkr"], writes=["Wkr"])
                gattn = sb("gattn", [128, D], F32)
                p.op("sp", lambda e: e.dma_start(out=gattn[:], in_=g_attn.partition_broadcast(128)), writes=["gattn"], dma="gattn")

                def rope_table(Ct, St, pos_ap, n, rows, name, inv, kinv, sb):
                    posi = sb(name + "_pi", [128, n], I32)
                    ang = sb(name + "_ang", [128, n], F32)
                    kk = sb(name + "_k", [128, n], F32)
                    ki = sb(name + "_ki", [128, n], I32)
                    p.op("sp", lambda e: e.dma_start(out=posi[:], in_=pos_ap.partition_broadcast(128)), writes=[name + "pi"], dma=name + "pi")
                    p.op("dve", lambda e: e.tensor_copy(out=ang[:], in_=posi[:]), reads=[name + "pi"], writes=[name + "ang"])
                    p.op("dve", lambda e: e.tensor_scalar(out=ang[:], in0=ang[:], scalar1=inv[:, 0:1], scalar2=None, op0=ALU.mult),
                         reads=[name + "ang", kinv], writes=[name + "ang"])
                    for which, T in (("s", St), ("c", Ct)):
                        off = 0.0 if which == "s" else float(np.pi / 2)
                        p.op("dve", lambda e, off=off: e.tensor_scalar(out=kk[:], in0=ang[:], scalar1=off, scalar2=float(1.0 / (2 * np.pi)),
                                                                   op0=ALU.add, op1=ALU.mult), reads=[name + "ang"], writes=[name + "kk"])
                        p.op("dve", lambda e: e.tensor_copy(out=ki[:], in_=kk[:]), reads=[name + "kk"], writes=[name + "ki"])
                        p.op("dve", lambda e: e.tensor_copy(out=kk[:], in_=ki[:]), reads=[name + "ki"], writes=[name + "kk"])
                        p.op("dve", lambda e, T=T: e.scalar_tensor_tensor(out=T, in0=kk[:rows], scalar=-6.28125, in1=ang[:rows],
                                                                       op0=ALU.mult, op1=ALU.add),
                             reads=[name + "kk", name + "ang"], writes=[name + which])
                        p.op("dve", lambda e, T=T, off=off: e.scalar_tensor_tensor(out=T, in0=kk[:rows], scalar=float(-(2 * np.pi - 6.28125)), in1=T,
                                                                                op0=ALU.mult, op1=ALU.add),
                             reads=[name + "kk", name + which], writes=[name + which])
                        if off != 0.0:
                            p.op("dve", lambda e, T=T, off=off: e.tensor_scalar(out=T, in0=T, scalar1=off, scalar2=None, op0=ALU.add),
                                 reads=[name + which], writes=[name + which])
                        p.op("dve", lambda e: e.tensor_scalar(out=kk[:rows], in0=T, scalar1=float(np.pi), scalar2=float(-2 * np.pi),
                                                              op0=ALU.is_gt, op1=ALU.mult), reads=[name + which], writes=[name + "kk"])
                        p.op("dve", lambda e, T=T: e.tensor_tensor(out=T, in0=T, in1=kk[:rows], op=ALU.add),
                             reads=[name + which, name + "kk"], writes=[name + which])
                        p.op("dve", lambda e: e.tensor_scalar(out=kk[:rows], in0=T, scalar1=float(-np.pi), scalar2=float(2 * np.pi),
                                                              op0=ALU.is_lt, op1=ALU.mult), reads=[name + which], writes=[name + "kk"])
                        p.op("dve", lambda e, T=T: e.tensor_tensor(out=T, in0=T, in1=kk[:rows], op=ALU.add),
                             reads=[name + which, name + "kk"], writes=[name + which])
                        p.op("dve", lambda e, T=T: e.tensor_scalar(out=T, in0=T, scalar1=3.14159, scalar2=-3.14159, op0=ALU.min, op1=ALU.max),
                             reads=[name + which], writes=[name + which])
                        p.op("act", lambda e, T=T: e.activation(out=T, in_=T, func=AF.Sin), reads=[name + which], writes=[name + which])

                Ck = sb("Ck", [32, S], F32)
                Sk = sb("Sk", [32, S], F32)
                sa2 = ExitStack()
                with sa2:
                    rope_table(Ck[:], Sk[:], posa, S, 32, "rk", invc, "invc", mk_alloc(sa2)[0])
                    p.barrier()
                Cq = sb("Cq", [96, 2048], F32)
                Sq = sb("Sq", [96, 2048], F32)
                sa3 = ExitStack()
                with sa3:
                    sb3_ = mk_alloc(sa3)[0]
                    invq = sb3_("invq", [128, 1], F32)
                    p.op("pool", lambda e: e.memset(invq[:], 0.0), writes=["invq"])
                    p.op("sp", lambda e: e.dma_start(out=invq[64:96, :], in_=invf[0:32, :]), reads=["invq"], writes=["invq"], dma="invq")
                    rope_table(Cq[:], Sq[:], poso, 2048, 96, "rq", invq, "invq", sb3_)
                    p.barrier()
                    if stop_after == "R":
                        p.finish_wait("sp"); p.emit(top); return nc
                msc = float((64 + 32) ** -0.5)
                p.op("dve", lambda e: e.tensor_scalar(out=Cq[:], in0=Cq[:], scalar1=msc, scalar2=None, op0=ALU.mult),
                     reads=["rqc"], writes=["rqc"])
                p.op("dve", lambda e: e.tensor_scalar(out=Sq[:], in0=Sq[:], scalar1=msc, scalar2=None, op0=ALU.mult),
                     reads=["rqs"], writes=["rqs"])

                xt_r = Ring(sb, "xt", 4, [128, D], F32)
                junk = sb("junkA", [128, D], F32)
                ss_r = Ring(sb, "ssA", 4, [128, 1], F32)
                xn_r = Ring(sb, "xn", 4, [128, D], BF16)
                xnT_r = Ring(sb, "xnT", 2, [128, 8, 512], BF16)
                pT_r = Ring(ps, "pTA", 2, [128, 8, 128], BF16)
                pm_r = Ring(ps, "pmA", 4, [128, 512], F32)
                pss = ps("pssA", [128, 512], F32)
                sq_r = Ring(sb, "sqA", 2, [128, 512], BF16)
                rbc = sb("rbcA", [128, 512], F32)
                cn_r = Ring(sb, "cnA", 2, [128, 3, 512], BF16)
                ev_r = Ring(sb, "evA", 2, [128, 512], BF16)
                stg_r = Ring(sb, "stgA", 3, [128, 4, 512], BF16)
                stg8_r = Ring(sb, "stg8A", 2, [128, 8, 512], BF16)
                evf_r = Ring(sb, "evfA", 2, [128, 512], F32)
                evf2_r = Ring(sb, "evf2A", 2, [128, 512], F32)

                def make_xnT(src, tok0):
                    xnT, kT = xnT_r.next()
                    xs = []
                    for sub in range(4):
                        xt, kx = xt_r.next()
                        ss, ks = ss_r.next()
                        xn, kn = xn_r.next()
                        r0 = tok0 + sub * 128
                        p.op("sp", lambda e: e.dma_start(out=xt[:], in_=src[r0:r0 + 128, :]), writes=[kx], dma=kx)
                        p.op("act", lambda e: e.activation(out=junk[:], in_=xt[:], func=AF.Square, accum_out=ss[:]),
                             reads=[kx], writes=["junkA", ks])
                        rstd_from(ss[:], ss[:], D, [ks], [ks])
                        p.op("dve", lambda e: e.scalar_tensor_tensor(out=xn[:], in0=xt[:], scalar=ss[:, 0:1], in1=gattn[:],
                                                                     op0=ALU.mult, op1=ALU.mult),
                             reads=[kx, ks, "gattn"], writes=[kn])
                        xs.append((xn, kn))
                    for sub in range(4):
                        xn, kn = xs[sub]
                        pT, kp = pT_r.next()
                        for c in range(8):
                            p.op("pe", lambda e: e.transpose(out=pT[:, c, :], in_=xn[:, c * 128:(c + 1) * 128], identity=ident[:]),
                                 reads=[kn, "ident"], writes=[kp])
                        p.op("dve", lambda e: e.tensor_copy(out=xnT[:, :, sub * 128:(sub + 1) * 128], in_=pT[:]),
                             reads=[kp], writes=[kT])
                    return xnT, kT

                def proj_fm(xnT, kT, col0, m, wkey="Win", W=None):
                    W = Win if W is None else W
                    pm, kpm = pm_r.next()
                    for k in range(8):
                        p.op("pe", lambda e, k=k, pm=pm, W=W: e.matmul(out=pm[0:m, :], lhsT=W[:, k, col0:col0 + m], rhs=xnT[:, k, :],
                                                                  start=(k == 0), stop=(k == 7)),
                             reads=[kT, wkey], writes=[kpm])
                    return pm, kpm

                def lowrank_norm(pms, nch, width, gc0):
                    for i, (pm, kpm) in enumerate(pms):
                        sq, ksq = sq_r.next()
                        p.op("act", lambda e, pm=pm, sq=sq: e.activation(out=sq[:], in_=pm[:], func=AF.Square), reads=[kpm], writes=[ksq])
                        p.op("pe", lambda e, sq=sq, i=i: e.matmul(out=pss[:], lhsT=ones[:], rhs=sq[:], start=(i == 0), stop=(i == nch - 1)),
                             reads=[ksq, "ones"], writes=["pssA"])
                    rstd_from(rbc[:], pss[:], width, ["pssA"], ["rbcA"])
                    cn, kcn = cn_r.next()
                    for i, (pm, kpm) in enumerate(pms):
                        p.op("dve", lambda e, pm=pm, i=i, cn=cn: e.scalar_tensor_tensor(out=cn[:, i, :], in0=pm[:], scalar=gcol[:, gc0 + i:gc0 + i + 1],
                                                                                    in1=rbc[:], op0=ALU.mult, op1=ALU.mult),
                             reads=[kpm, "gcol", "rbcA"], writes=[kcn])
                    return cn, kcn

                def store_fm(pm, kpm, rows, dst, eng="dve", scale=None):
                    ev, kev = ev_r.next()
                    if scale is None:
                        if eng == "act":
                            p.op("act", lambda e: e.copy(out=ev[0:rows, :], in_=pm[0:rows, :]), reads=[kpm], writes=[kev])
                        else:
                            p.op("dve", lambda e: e.tensor_copy(out=ev[0:rows, :], in_=pm[0:rows, :]), reads=[kpm], writes=[kev])
                    else:
                        p.op("act", lambda e: e.mul(out=ev[0:rows, :], in_=pm[0:rows, :], mul=scale), reads=[kpm], writes=[kev])
                    p.op("pool", lambda e: e.dma_start(out=dst, in_=ev[0:rows, :]), reads=[kev], writes=[], dma=kev + "s")

                def evac_to(pm, kpm, dst, kdst, eng="dve", scale=None):
                    if scale is not None:
                        p.op("act", lambda e: e.mul(out=dst, in_=pm[:], mul=scale), reads=[kpm], writes=[kdst])
                    elif eng == "act":
                        p.op("act", lambda e: e.copy(out=dst, in_=pm[:]), reads=[kpm], writes=[kdst])
                    else:
                        p.op("dve", lambda e: e.tensor_copy(out=dst, in_=pm[:]), reads=[kpm], writes=[kdst])

                KnT_v = KnT.rearrange("(c p) n -> p c n", p=128)
                KsT_v = KsT.rearrange("(c p) n -> p c n", p=128)
                QsT_v = QsT.rearrange("(c p) n -> p c n", p=128)
                VmD_v = VmD.rearrange("(s p) n -> p s n", p=128)
                VsD_v = VsD.rearrange("(s p) n -> p s n", p=128)
                QmT_v = QmT.rearrange("(h r) n -> r h n", r=96)

                srcs = [(xa, g_ * 512) for g_ in range(8)] + [(xo, g_ * 512) for g_ in range(4)]
                nxt = make_xnT(*srcs[0])
                for G in range(8):
                    c0 = G * 512
                    xnT, kT = nxt
                    nxt = make_xnT(*srcs[G + 1])
                    ckv = [proj_fm(xnT, kT, 384 + m * 128, 128) for m in range(2)]
                    cn, kcn = lowrank_norm(ckv, 2, 256, 3)
                    pa, kpa = proj_fm(xnT, kT, 0, 32, "Wkr", Wkr)
                    pb, kpb = proj_fm(xnT, kT, 32, 32, "Wkr", Wkr)
                    t1, kt1 = evf_r.next()
                    t2, kt2 = evf2_r.next()
                    p.op("dve", lambda e, pa=pa, t1=t1, c0=c0: e.tensor_tensor(out=t1[0:32, :], in0=pa[0:32, :], in1=Ck[:, c0:c0 + 512], op=ALU.mult),
                         reads=[kpa, "rkc"], writes=[kt1])
                    p.op("dve", lambda e, pb=pb, t2=t2, c0=c0: e.tensor_tensor(out=t2[0:32, :], in0=pb[0:32, :], in1=Sk[:, c0:c0 + 512], op=ALU.mult),
                         reads=[kpb, "rks"], writes=[kt2])
                    ev, kev = ev_r.next()
                    p.op("dve", lambda e, t1=t1, t2=t2, ev=ev: e.tensor_tensor(out=ev[0:32, :], in0=t1[0:32, :], in1=t2[0:32, :], op=ALU.add),
                         reads=[kt1, kt2], writes=[kev])
                    p.op("sp", lambda e, ev=ev, c0=c0: e.dma_start(out=KrT[:, c0:c0 + 512], in_=ev[0:32, :]), reads=[kev], writes=[], dma=kev + "s")
                    st_, kst_ = stg_r.next()
                    for m in range(4):
                        pm, kpm = proj_fm(xnT, kT, 1184 + m * 128, 128)
                        evac_to(pm, kpm, st_[:, m, :], kst_, eng="act")
                    p.op("sp", lambda e: e.dma_start(out=KsT_v[:, :, c0:c0 + 512], in_=st_[:]), reads=[kst_], dma=kst_ + "s")
                    st_, kst_ = stg_r.next()
                    for sub in range(4):
                        pm, kpm = pm_r.next()
                        for k in range(8):
                            p.op("pe", lambda e, k=k, pm=pm, sub=sub, xnT=xnT: e.matmul(out=pm[:], lhsT=xnT[:, k, sub * 128:(sub + 1) * 128],
                                                                                   rhs=Win[:, k, 1696:2208], start=(k == 0), stop=(k == 7)),
                                 reads=[kT, "Win"], writes=[kpm])
                        evac_to(pm, kpm, st_[:, sub, :], kst_, eng="dve")
                    p.op("sp", lambda e: e.dma_start(out=VsD_v[:, G * 4:(G + 1) * 4, :], in_=st_[:]), reads=[kst_], dma=kst_ + "s")

                    st_, kst_ = stg_r.next()
                    for hp in range(4):
                        pm, kpm = pm_r.next()
                        for m in range(2):
                            p.op("pe", lambda e, m=m, pm=pm, hp=hp, cn=cn: e.matmul(out=pm[:], lhsT=Wkn[:, m, hp * 128:(hp + 1) * 128], rhs=cn[:, m, :],
                                                                               start=(m == 0), stop=(m == 1)),
                                 reads=[kcn, "Wkn"], writes=[kpm])
                        evac_to(pm, kpm, st_[:, hp, :], kst_, eng="act")
                    p.op("sp", lambda e: e.dma_start(out=KnT_v[:, :, c0:c0 + 512], in_=st_[:]), reads=[kst_], dma=kst_ + "s")
                    st_, kst_ = stg_r.next()
                    for sub in range(4):
                        pm, kpm = pm_r.next()
                        for m in range(2):
                            p.op("pe", lambda e, m=m, pm=pm, sub=sub, cn=cn: e.matmul(out=pm[:], lhsT=cn[:, m, sub * 128:(sub + 1) * 128], rhs=Wv[:, m, :],
                                                                                 start=(m == 0), stop=(m == 1)),
                                 reads=[kcn, "Wv"], writes=[kpm])
                        evac_to(pm, kpm, st_[:, sub, :], kst_, eng="dve")
                    p.op("sp", lambda e: e.dma_start(out=VmD_v[:, G * 4:(G + 1) * 4, :], in_=st_[:]), reads=[kst_], dma=kst_ + "s")
                for G in range(4):
                    c0 = G * 512
                    xnT, kT = nxt
                    if G + 1 < 4:
                        nxt = make_xnT(*srcs[8 + G + 1])
                    cq = [proj_fm(xnT, kT, m * 128, 128) for m in range(3)]
                    cn, kcn = lowrank_norm(cq, 3, 384, 0)
                    st8, kst8 = stg8_r.next()
                    for h in range(8):
                        pa, kpa = pm_r.next()
                        pb, kpb = pm_r.next()
                        for m in range(3):
                            p.op("pe", lambda e, m=m, pa=pa, h=h, cn=cn: e.matmul(out=pa[0:96, :], lhsT=Wuq[:, m, h * 96:(h + 1) * 96], rhs=cn[:, m, :],
                                                                             start=(m == 0), stop=(m == 2)),
                                 reads=[kcn, "Wuq"], writes=[kpa])
                        for m in range(3):
                            p.op("pe", lambda e, m=m, pb=pb, h=h, cn=cn: e.matmul(out=pb[0:96, :], lhsT=Wuqr[:, m, h * 96:(h + 1) * 96], rhs=cn[:, m, :],
                                                                             start=(m == 0), stop=(m == 2)),
                                 reads=[kcn, "Wuqr"], writes=[kpb])
                        t1, kt1 = evf_r.next()
                        t2, kt2 = evf2_r.next()
                        p.op("dve", lambda e, pa=pa, t1=t1, c0=c0: e.tensor_tensor(out=t1[0:96, :], in0=pa[0:96, :], in1=Cq[:, c0:c0 + 512], op=ALU.mult),
                             reads=[kpa, "rqc"], writes=[kt1])
                        p.op("dve", lambda e, pb=pb, t2=t2, c0=c0: e.tensor_tensor(out=t2[0:96, :], in0=pb[0:96, :], in1=Sq[:, c0:c0 + 512], op=ALU.mult),
                             reads=[kpb, "rqs"], writes=[kt2])
                        p.op("pool", lambda e, t1=t1, t2=t2: e.tensor_tensor(out=st8[0:96, h, :], in0=t1[0:96, :], in1=t2[0:96, :], op=ALU.add),
                             reads=[kt1, kt2], writes=[kst8])
                    p.op("sp", lambda e: e.dma_start(out=QmT_v[:, :, c0:c0 + 512], in_=st8[0:96, :, :]), reads=[kst8], dma=kst8 + "s")
                    st_, kst_ = stg_r.next()
                    for m in range(4):
                        pm, kpm = proj_fm(xnT, kT, 672 + m * 128, 128)
                        evac_to(pm, kpm, st_[:, m, :], kst_, scale=0.125)
                    p.op("sp", lambda e: e.dma_start(out=QsT_v[:, :, c0:c0 + 512], in_=st_[:]), reads=[kst_], dma=kst_ + "s")
                p.barrier()
                if stop_after == "A":
                    p.finish_wait("sp"); p.emit(top); return nc

            sbx = ExitStack()
            with sbx:
                sb, ps = mk_alloc(sbx)
                qrb = sb("qrb", [128, 512], F32)
                p.op("sp", lambda e: e.dma_start(out=qrb[:], in_=qrel.partition_broadcast(128)), writes=["qrb"], dma="qrb")
                kidx_i = sb("kidx_i", [128, 1], I32)
                krel = sb("krel", [128, 8], F32)
                p.op("pool", lambda e: e.iota(kidx_i[:], pattern=[[0, 1]], base=0, channel_multiplier=1), writes=["kidx_i"])
                p.op("dve", lambda e: e.tensor_copy(out=krel[:, 0:1], in_=kidx_i[:]), reads=["kidx_i"], writes=["krel"])
                for d in range(1, 8):
                    p.op("dve", lambda e, d=d: e.tensor_scalar(out=krel[:, d:d + 1], in0=krel[:, 0:1], scalar1=float(128 * d), scalar2=None, op0=ALU.add),
                         reads=["krel"], writes=["krel"])
                nmM = sb("nmM", [128, 8, 512], BF16)
                nmS = sb("nmS", [128, 8, 512], BF16)
                m01 = sb("m01", [128, 8, 512], BF16)
                for d in range(8):
                    p.op("dve", lambda e, d=d: e.tensor_scalar(out=nmM[:, d, :], in0=qrb[:], scalar1=krel[:, d:d + 1], scalar2=NEG, op0=ALU.is_lt, op1=ALU.mult),
                         reads=["qrb", "krel"], writes=["nmM"])
                    p.op("dve", lambda e, d=d: e.tensor_scalar(out=nmS[:, d, :], in0=qrb[:], scalar1=krel[:, d:d + 1], scalar2=NEG, op0=ALU.is_le, op1=ALU.mult),
                         reads=["qrb", "krel"], writes=["nmS"])
                    p.op("dve", lambda e, d=d: e.tensor_scalar(out=m01[:, d, :], in0=qrb[:], scalar1=krel[:, d:d + 1], scalar2=None, op0=ALU.is_gt),
                         reads=["qrb", "krel"], writes=["m01"])

                def blocks(t):
                    out = [(kb, 512, None) for kb in range(8 * t)]
                    out += [(8 * t + d, NDIAG[d], d) for d in range(8)]
                    return out

                sm = ExitStack()
                with sm:
                    def mla_gen():
                        sb, ps = mk_alloc(sm)
                        Vall = sb("Vall", [128, 32, 8, 65], BF16)
                        p.op("pool", lambda e: e.memset(Vall[:], 1.0), writes=["Vall"])
                        vsrc = VmD.rearrange("(kb p) (h d) -> p kb h d", p=128, h=8)
                        for hh in range(8):
                            p.op("sp", lambda e: e.dma_start(out=Vall[:, :, hh, 0:64], in_=vsrc[:, :, hh, :]),
                                 reads=["Vall"], writes=["Vall"], dma="Vall")
                        KT_r = Ring(sb, "KTm", 2, [96, S], BF16)
                        QT_r = Ring(sb, "QTm", 2, [96, 2048], BF16)
                        pS_r = Ring(ps, "pSm", 2, [128, 512], F32)
                        pO_r = Ring(ps, "pOm", 1, [128, 512], F32)
                        pB_r = Ring(ps, "pBm", 1, [128, 512], F32)
                        P_r = Ring(sb, "Pm", 3, [128, 512], BF16)
                        Of_r = Ring(sb, "Ofm", 2, [65, 512], F32)
                        On_r = Ring(sb, "Onm", 2, [64, 512], BF16)
                        sel = sb("sel65", [65, 64], F32)
                        p.op("pool", lambda e: e.memset(sel[:], 0.0), writes=["sel65"])
                        p.op("pool", lambda e: e.memset(sel[64:65, :], 1.0), reads=["sel65"], writes=["sel65"])
                        for h in range(8):
                            KT, kKT = KT_r.next()
                            QT, kQT = QT_r.next()
                            p.op("sp", lambda e, KT=KT, h=h: e.dma_start(out=KT[0:64, :], in_=KnT[h * 64:(h + 1) * 64, :]), writes=[kKT], dma=kKT)
                            p.op("sp", lambda e, KT=KT: e.dma_start(out=KT[64:96, :], in_=KrT[:, :]), writes=[kKT], dma=kKT)
                            p.op("sp", lambda e, QT=QT, h=h: e.dma_start(out=QT[:], in_=QmT[h * 96:(h + 1) * 96, :]), writes=[kQT], dma=kQT)
                            for t in range(4):
                                bl = blocks(t)
                                pO, kpO = pO_r.next()
                                nb = len(bl)
                                stage = {}

                                def stA(i):
                                    kb, N, d = bl[i]
                                    pS, kpS = pS_r.next()
                                    p.op("pe", lambda e: e.matmul(out=pS[:, 0:N], lhsT=KT[:, kb * 128:(kb + 1) * 128], rhs=QT[:, t * 512:t * 512 + N],
                                                                  start=True, stop=(d is None)), reads=[kKT, kQT], writes=[kpS])
                                    if d is not None:
                                        p.op("pe", lambda e: e.matmul(out=pS[:, 0:N], lhsT=ident[:], rhs=nmM[:, d, 0:N], start=False, stop=True),
                                             reads=["ident", "nmM"], writes=[kpS])
                                    P, kP = P_r.next()
                                    p.op("act", lambda e: e.activation(out=P[:, 0:N], in_=pS[:, 0:N], func=AF.Exp), reads=[kpS], writes=[kP])
                                    stage[i] = (P, kP)

                                def stB(i):
                                    kb, N, d = bl[i]
                                    P, kP = stage.pop(i)
                                    p.op("pe", lambda e: e.matmul(out=pO[0:65, 0:N], lhsT=Vall[:, kb, h, :], rhs=P[:, 0:N], start=(i == 0), stop=(i == nb - 1)),
                                         reads=["Vall", kP], writes=[kpO])

                                for it in range(nb + 2):
                                    if it < nb:
                                        stA(it)
                                    if it - 2 >= 0:
                                        stB(it - 2)
                                    yield
                                Of, kOf = Of_r.next()
                                p.op("dve", lambda e, Of=Of, pO=pO: e.tensor_copy(out=Of[:], in_=pO[0:65, :]), reads=[kpO], writes=[kOf])
                                p.op("dve", lambda e, Of=Of: e.reciprocal(out=Of[64:65, :], in_=Of[64:65, :]), reads=[kOf], writes=[kOf])
                                pB, kpB = pB_r.next()
                                p.op("pe", lambda e, Of=Of, pB=pB: e.matmul(out=pB[0:64, :], lhsT=sel[:], rhs=Of[:], start=True, stop=True),
                                     reads=[kOf, "sel65"], writes=[kpB])
                                On, kOn = On_r.next()
                                p.op("dve", lambda e, Of=Of, pB=pB, On=On: e.tensor_tensor(out=On[:], in0=Of[0:64, :], in1=pB[0:64, :], op=ALU.mult),
                                     reads=[kOf, kpB], writes=[kOn])
                                p.op("pool", lambda e, On=On, h=h, t=t: e.dma_start(out=OTd[h * 64:(h + 1) * 64, t * 512:(t + 1) * 512], in_=On[:]),
                                     reads=[kOn], writes=[], dma=kOn + "s")

                    def sb_gen():
                        sb, ps = mk_alloc(sm)
                        Vs = sb("Vsall", [128, 32, 512], BF16)
                        vsrc = VsD.rearrange("(kb p) n -> p kb n", p=128)
                        for q4 in range(4):
                            p.op("sp", lambda e, q4=q4: e.dma_start(out=Vs[:, q4 * 8:(q4 + 1) * 8, :], in_=vsrc[:, q4 * 8:(q4 + 1) * 8, :]),
                                 writes=["Vsall"], dma="Vsall")
                        KT_r = Ring(sb, "KTs", 2, [64, S], BF16)
                        QT_r = Ring(sb, "QTs", 2, [64, 2048], BF16)
                        pZ_r = Ring(ps, "pZs", 2, [128, 512], F32)
                        pL_r = Ring(ps, "pLs", 1, [128, 512], F32)
                        pO_r = Ring(ps, "pOs", 1, [128, 512], F32)
                        E_r = Ring(sb, "Es", 3, [128, 512], F32)
                        SP_r = Ring(sb, "SPs", 4, [128, 512], BF16)
                        SM_r = Ring(sb, "SMs", 4, [128, 512], BF16)
                        A_r = Ring(sb, "As", 3, [128, 512], BF16)
                        CR_r = Ring(sb, "CRs", 3, [128, 512], BF16)
                        On_r = Ring(sb, "Ons", 2, [64, 512], BF16)
                        for h in range(8):
                            KT, kKT = KT_r.next()
                            QT, kQT = QT_r.next()
                            p.op("sp", lambda e, KT=KT, h=h: e.dma_start(out=KT[:], in_=KsT[h * 64:(h + 1) * 64, :]), writes=[kKT], dma=kKT)
                            p.op("sp", lambda e, QT=QT, h=h: e.dma_start(out=QT[:], in_=QsT[h * 64:(h + 1) * 64, :]), writes=[kQT], dma=kQT)
                            for t in range(4):
                                bl = blocks(t)[::-1]
                                nb = len(bl)
                                pO, kpO = pO_r.next()
                                p.op("pe", lambda e, pO=pO: e.matmul(out=pO[0:64, :], lhsT=zeros[:, 0:64], rhs=m01[:, 0, :],
                                                                      start=True, stop=False), reads=["zeros", "m01"], writes=[kpO])
                                stage = {}
                                carry = {"t": None, "k": None}

                                def stA(i):
                                    kb, N, d = bl[i]
                                    pZ, kpZ = pZ_r.next()
                                    p.op("pe", lambda e: e.matmul(out=pZ[:, 0:N], lhsT=KT[:, kb * 128:(kb + 1) * 128], rhs=QT[:, t * 512:t * 512 + N],
                                                                  start=True, stop=True), reads=[kKT, kQT], writes=[kpZ])
                                    E, kE = E_r.next()
                                    p.op("act", lambda e: e.activation(out=E[:, 0:N], in_=pZ[:, 0:N], func=AF.Exp), reads=[kpZ], writes=[kE])
                                    SPt, kSP = SP_r.next()
                                    p.op("act", lambda e: e.activation(out=SPt[:, 0:N], in_=E[:, 0:N], func=AF.Ln, bias=1.0), reads=[kE], writes=[kSP])
                                    if d is not None:
                                        SM, kSM = SM_r.next()
                                        p.op("dve", lambda e: e.tensor_tensor(out=SM[:, 0:N], in0=SPt[:, 0:N], in1=m01[:, d, 0:N], op=ALU.mult),
                                             reads=[kSP, "m01"], writes=[kSM])
                                    else:
                                        SM, kSM = SPt, kSP
                                    cprev, kcprev = carry["t"], carry["k"]
                                    stage[i] = (SM, kSM, cprev, kcprev, E, kE)
                                    if i < nb - 1:
                                        cn_, kcn_ = CR_r.next()
                                        if cprev is None:
                                            if N < 512:
                                                p.op("pool", lambda e: e.memset(cn_[:, N:512], 0.0), writes=[kcn_])
                                            p.op("pool", lambda e: e.tensor_copy(out=cn_[:, 0:N], in_=SM[:, 0:N]), reads=[kSM], writes=[kcn_])
                                        else:
                                            if N < 512:
                                                p.op("pool", lambda e: e.tensor_copy(out=cn_[:, N:512], in_=cprev[:, N:512]), reads=[kcprev], writes=[kcn_])
                                            p.op("dve", lambda e: e.tensor_tensor(out=cn_[:, 0:N], in0=cprev[:, 0:N], in1=SM[:, 0:N], op=ALU.add),
                                                 reads=[kcprev, kSM], writes=[kcn_])
                                        carry["t"], carry["k"] = cn_, kcn_

                                def stB(i):
                                    kb, N, d = bl[i]
                                    SM, kSM, cprev, kcprev, E, kE = stage[i]
                                    pL, kpL = pL_r.next()
                                    last = "tri"
                                    if cprev is not None:
                                        last = "carry"
                                    if d is not None:
                                        last = "mask"
                                    p.op("pe", lambda e: e.matmul(out=pL[:, 0:N], lhsT=ntri[:], rhs=SM[:, 0:N], start=True, stop=(last == "tri")),
                                         reads=["ntri", kSM], writes=[kpL])
                                    if cprev is not None:
                                        p.op("pe", lambda e: e.matmul(out=pL[:, 0:N], lhsT=nones[:], rhs=cprev[:, 0:N], start=False, stop=(last == "carry")),
                                             reads=["nones", kcprev], writes=[kpL])
                                    if d is not None:
                                        p.op("pe", lambda e: e.matmul(out=pL[:, 0:N], lhsT=ident[:], rhs=nmS[:, d, 0:N], start=False, stop=True),
                                             reads=["ident", "nmS"], writes=[kpL])
                                    A, kA = A_r.next()
                                    p.op("act", lambda e: e.activation(out=A[:, 0:N], in_=pL[:, 0:N], func=AF.Exp), reads=[kpL], writes=[kA])
                                    p.op("dve", lambda e: e.tensor_tensor(out=A[:, 0:N], in0=A[:, 0:N], in1=E[:, 0:N], op=ALU.mult), reads=[kA, kE], writes=[kA])
                                    stage[i] = (A, kA)

                                def stC(i):
                                    kb, N, d = bl[i]
                                    A, kA = stage.pop(i)
                                    p.op("pe", lambda e: e.matmul(out=pO[0:64, 0:N], lhsT=Vs[:, kb, h * 64:(h + 1) * 64], rhs=A[:, 0:N], start=False, stop=(i == nb - 1)),
                                         reads=["Vsall", kA], writes=[kpO])

                                for it in range(nb + 2):
                                    if it < nb:
                                        stA(it)
                                    if 0 <= it - 1 < nb:
                                        stB(it - 1)
                                    if it - 2 >= 0:
                                        stC(it - 2)
                                    yield
                                On, kOn = On_r.next()
                                p.op("dve", lambda e, On=On, pO=pO: e.tensor_copy(out=On[:], in_=pO[0:64, :]), reads=[kpO], writes=[kOn])
                                p.op("pool", lambda e, On=On, h=h, t=t: e.dma_start(out=OTd[512 + h * 64:512 + (h + 1) * 64, t * 512:(t + 1) * 512], in_=On[:]),
                                     reads=[kOn], writes=[], dma=kOn + "s")

                    gens = [mla_gen(), sb_gen()]
                    while gens:
                        for g_ in list(gens):
                            try:
                                next(g_)
                            except StopIteration:
                                gens.remove(g_)
                    p.barrier()
                    if stop_after == "B":
                        p.finish_wait("sp"); p.emit(top); return nc
            sc = ExitStack()
            with sc:
                sb, ps = mk_alloc(sc)
                H = sb("H", [128, 16, D], F32)
                posm = sb("posm", [128, 16, NE], F32)
                maskb = sb("maskb", [128, 16, NE], BF16)
                gj = sb("gj", [128, 16, 4], F32)
                slI = sb("slI", [128, 16, 4], I32)
                Gt = sb("Gt", [128, 16, NE], F32)
                bgT = sb("bgT", [128, 512], F32)
                s1 = ExitStack()
                with s1:
                    sb1, ps1 = mk_alloc(s1)
                    gbc = sb1("gbc", [128, D], F32)
                    junk = sb1("junkC", [128, D], BF16)
                    Wo = sb1("Wo", [128, 8, D], BF16)
                    p.op("pool", lambda e: e.dma_start(out=Wo[:], in_=w_o.rearrange("(c p) n -> p c n", p=128)), writes=["Wo"], dma="Wo")
                    Wr = sb1("Wr", [128, 8, NE], F32)
                    p.op("sp", lambda e: e.dma_start(out=Wr[:], in_=w_router.rearrange("(c p) n -> p c n", p=128)), writes=["Wr"], dma="Wr")
                    brb = sb1("brb", [128, NE], F32)
                    p.op("sp", lambda e: e.dma_start(out=brb[:], in_=b_router.partition_broadcast(128)), writes=["brb"], dma="brb")
                    p.op("sp", lambda e: e.dma_start(out=gbc[:], in_=g_moe.partition_broadcast(128)), writes=["gbc"], dma="gbc")
                    bgl = sb1("bgl", [128, 4, 128], F32)
                    p.op("sp", lambda e: e.dma_start(out=bgl[:], in_=b_gu.rearrange("(a r) q -> r a q", r=128)), writes=["bgl"], dma="bgl")
                    pTf_r = Ring(ps1, "pTf", 1, [128, 4, 128], F32)
                    pbt, kpbt = pTf_r.next()
                    for a in range(4):
                        p.op("pe", lambda e, a=a: e.transpose(out=pbt[:, a, :], in_=bgl[:, a, :], identity=identf[:]), reads=["bgl", "identf"], writes=[kpbt])
                    p.op("dve", lambda e: e.tensor_copy(out=bgT[:].rearrange("p (a q) -> p a q", a=4), in_=pbt[:]), reads=[kpbt], writes=["bgT"])

                    OT_r = Ring(sb1, "OTl", 1, [128, 8, 512], BF16)
                    sq_r = Ring(sb1, "sqC", 2, [128, 512], BF16)
                    pss_r = Ring(ps1, "pssC", 1, [128, 512], F32)
                    rb_r = Ring(sb1, "rbC", 2, [128, 512], F32)
                    MX = sb1("MX", [128, 8, 512], BF16)
                    pA_r = Ring(ps1, "pAC", 2, [128, 512], F32)
                    xr_r = Ring(sb1, "xrC", 2, [128, D], F32)
                    ssc_r = Ring(sb1, "sscC", 2, [128, 1], F32)
                    u32_r = Ring(sb1, "u32C", 2, [128, D], F32)
                    uhT_r = Ring(sb1, "uhT", 2, [128, 8, 128], BF16)
                    tris = sb1("tris", [128, 128], BF16)
                    tmpf1 = sb1("tmpf1", [128, 128], F32)
                    p.op("pool", lambda e: e.memset(tmpf1[:], 1.0), writes=["tmpf1"])
                    p.op("pool", lambda e: e.affine_select(out=tmpf1[:], in_=tmpf1[:], pattern=[[1, 128]], compare_op=ALU.is_gt, fill=0.0,
                                                            base=0, channel_multiplier=-1), reads=["tmpf1"], writes=["tmpf1"])
                    p.op("dve", lambda e: e.tensor_copy(out=tris[:], in_=tmpf1[:]), reads=["tmpf1"], writes=["tris"])
                    gtmp_r = Ring(sb1, "gtmpC", 2, [128, NE], F32)
                    xnb_r = Ring(sb1, "xnbC", 2, [128, D], BF16)
                    ecap_i = sb1("ecap_i", [128, NE], I32)
                    ecap = sb1("ecap", [128, NE], F32)
                    p.op("pool", lambda e: e.iota(ecap_i[:], pattern=[[CAP, NE]], base=0, channel_multiplier=0), writes=["ecap_i"])
                    p.op("dve", lambda e: e.tensor_copy(out=ecap[:], in_=ecap_i[:]), reads=["ecap_i"], writes=["ecap"])
                    pq_r = Ring(sb1, "pq", 2, [128, NE], F32)
                    oh = sb1("oh", [128, NE], F32)
                    pr = sb1("pr", [128, NE], F32)
                    slf_r = Ring(sb1, "slf", 2, [128, 4], F32)
                    ulo_r = Ring(sb1, "uloC", 2, [128, D], BF16)
                    uloT_r = Ring(sb1, "uloT", 2, [128, 8, 128], BF16)
                    pTb_r = Ring(ps1, "pTb", 2, [128, 8, 128], BF16)
                    Wrh = sb1("Wrh", [128, 8, NE], BF16)
                    Wrl = sb1("Wrl", [128, 8, NE], BF16)
                    p.op("dve", lambda e: e.tensor_copy(out=Wrh[:], in_=Wr[:]), reads=["Wr"], writes=["Wrh"])
                    p.op("dve", lambda e: e.tensor_tensor(out=Wrl[:], in0=Wr[:], in1=Wrh[:], op=ALU.subtract), reads=["Wr", "Wrh"], writes=["Wrl"])
                    pLg_r = Ring(ps1, "pLg", 2, [128, 2, NE], F32)
                    lg_r = Ring(sb1, "lgC", 2, [128, NE], F32)
                    mx8_r = Ring(sb1, "mx8C", 2, [128, 8], F32)
                    msk_r = Ring(sb1, "mskC", 2, [128, NE], F32)
                    ex_r = Ring(sb1, "exC", 2, [128, NE], F32)
                    sm_r = Ring(sb1, "smC", 2, [128, 1], F32)
                    otv = OTd.rearrange("(c p) n -> p c n", p=128)
                    if stop_after == "C1a":
                        p.barrier()
                        p.op("sp", lambda e: e.dma_start(out=dbgH.rearrange("(t p) n -> p t n", p=128), in_=H[:]), reads=["H%d" % i_ for i_ in range(16)], dma="dbgH")
                        p.op("sp", lambda e: e.dma_start(out=dbgG.rearrange("(t p) n -> p t n", p=128), in_=Gt[:]), reads=["Gt%d" % i_ for i_ in range(16)], dma="dbgG")
                        p.finish_wait("sp"); p.emit(top); return nc
                    for G in range(4):
                        OT, kOT = OT_r.next()
                        p.op("sp", lambda e, OT=OT, G=G: e.dma_start(out=OT[:], in_=otv[:, :, G * 512:(G + 1) * 512]), writes=[kOT], dma=kOT)
                        rbs = []
                        for grp in range(2):
                            pss, kpss = pss_r.next()
                            for i in range(4):
                                c = grp * 4 + i
                                sq, ksq = sq_r.next()
                                p.op("act", lambda e, sq=sq, OT=OT, c=c: e.activation(out=sq[:], in_=OT[:, c, :], func=AF.Square), reads=[kOT], writes=[ksq])
                                p.op("pe", lambda e, sq=sq, pss=pss, i=i: e.matmul(out=pss[:], lhsT=ones[:], rhs=sq[:], start=(i == 0), stop=(i == 3)),
                                     reads=[ksq, "ones"], writes=[kpss])
                            rb, krb = rb_r.next()
                            rstd_from(rb[:], pss[:], 512, [kpss], [krb])
                            rbs.append((rb, krb))
                        for c in range(8):
                            rb, krb = rbs[c // 4]
                            p.op("dve", lambda e, c=c, rb=rb, OT=OT: e.scalar_tensor_tensor(out=MX[:, c, :], in0=OT[:, c, :], scalar=gcol[:, 5 + c:6 + c], in1=rb[:],
                                                                                        op0=ALU.mult, op1=ALU.mult),
                                 reads=[kOT, "gcol", krb], writes=["MX"])
                        def c1_tile(G, sub):
                            tile = G * 4 + sub
                            b_ = sub % 2
                            uhT, kuhT = uhT_r.tiles[b_], uhT_r.keys[b_]
                            uloT, kuloT = uloT_r.tiles[b_], uloT_r.keys[b_]
                            pq, kpq = pq_r.tiles[b_], pq_r.keys[b_]
                            slf, kslf = slf_r.tiles[b_], slf_r.keys[b_]
                            pLg2, kpLg = pLg_r.tiles[b_], pLg_r.keys[b_]
                            pLg = pLg2[:, 0, :]
                            pPos = pLg2[:, 1, :]
                            yield
                            xr, kxr = xr_r.next()
                            yield
                            p.op("sp", lambda e, xr=xr, tile=tile: e.dma_start(out=xr[:], in_=xo[tile * 128:(tile + 1) * 128, :]), writes=[kxr], dma=kxr)
                            yield
                            for half in range(2):
                                pA, kpA = pA_r.next()
                                for c in range(8):
                                    p.op("pe", lambda e, c=c, pA=pA, sub=sub, half=half: e.matmul(out=pA[:], lhsT=MX[:, c, sub * 128:(sub + 1) * 128],
                                                                                              rhs=Wo[:, c, half * 512:(half + 1) * 512], start=(c == 0), stop=(c == 7)),
                                         reads=["MX", "Wo"], writes=[kpA])
                                p.op("dve", lambda e, pA=pA, xr=xr, tile=tile, half=half: e.tensor_tensor(out=H[:, tile, half * 512:(half + 1) * 512], in0=pA[:],
                                                                                                      in1=xr[:, half * 512:(half + 1) * 512], op=ALU.add),
                                     reads=[kpA, kxr], writes=["H%d" % tile])
                            yield
                            yield
                            ssc, kss = ssc_r.next()
                            yield
                            p.op("act", lambda e, ssc=ssc, tile=tile: e.activation(out=junk[:], in_=H[:, tile, :], func=AF.Square, accum_out=ssc[:]),
                                 reads=["H%d" % tile], writes=["junkC", kss])
                            yield
                            rstd_from(ssc[:], ssc[:], D, [kss], [kss])
                            yield
                            u32, ku = u32_r.next()
                            yield
                            p.op("dve", lambda e, ssc=ssc, u32=u32, tile=tile: e.scalar_tensor_tensor(out=u32[:], in0=H[:, tile, :], scalar=ssc[:, 0:1], in1=gbc[:],
                                                                                                  op0=ALU.mult, op1=ALU.mult),
                                 reads=["H%d" % tile, kss, "gbc"], writes=[ku])
                            yield
                            xnb, kuhi = xnb_r.next()
                            yield
                            ulo, kulo = ulo_r.next()
                            yield
                            p.op("act", lambda e: e.copy(out=xnb[:], in_=u32[:]), reads=[ku], writes=[kuhi])
                            yield
                            p.op("pool", lambda e: e.dma_start(out=Ud[tile * 128:(tile + 1) * 128, :], in_=xnb[:]), reads=[kuhi], dma=kuhi + "s")
                            yield
                            p.op("dve", lambda e: e.tensor_tensor(out=ulo[:], in0=u32[:], in1=xnb[:], op=ALU.subtract), reads=[ku, kuhi], writes=[kulo])
                            yield
                            pT, kpT = pTb_r.next()
                            yield
                            for c in range(8):
                                p.op("pe", lambda e: e.transpose(out=pT[:, c, :], in_=xnb[:, c * 128:(c + 1) * 128], identity=ident[:]), reads=[kuhi, "ident"], writes=[kpT])
                            yield
                            p.op("dve", lambda e: e.tensor_copy(out=uhT[:], in_=pT[:]), reads=[kpT], writes=[kuhT])
                            yield
                            pT2, kpT2 = pTb_r.next()
                            yield
                            for c in range(8):
                                p.op("pe", lambda e: e.transpose(out=pT2[:, c, :], in_=ulo[:, c * 128:(c + 1) * 128], identity=ident[:]), reads=[kulo, "ident"], writes=[kpT2])
                            yield
                            p.op("act", lambda e: e.copy(out=uloT[:], in_=pT2[:]), reads=[kpT2], writes=[kuloT])
                            yield
                            n_ = 0
                            yield
                            for (A_, kA_, W_, kW_) in (("hi", kuhT, Wrh, "Wrh"), ("lo", kuloT, Wrh, "Wrh"), ("hi", kuhT, Wrl, "Wrl")):
                                for k in range(8):
                                    lh = uhT[:, k, :] if A_ == "hi" else uloT[:, k, :]
                                    p.op("pe", lambda e: e.matmul(out=pLg, lhsT=lh, rhs=W_[:, k, :], start=(n_ == 0), stop=(n_ == 23)),
                                         reads=[kA_, kW_], writes=[kpLg])
                                    n_ += 1
                            yield
                            lg, klg = lg_r.next()
                            yield
                            p.op("dve", lambda e, lg=lg: e.tensor_tensor(out=lg[:], in0=pLg, in1=brb[:], op=ALU.add), reads=[kpLg, "brb"], writes=[klg])
                            yield
                            mx8, kmx = mx8_r.next()
                            yield
                            p.op("dve", lambda e, lg=lg, mx8=mx8: e.max(out=mx8[:], in_=lg[:]), reads=[klg], writes=[kmx])
                            yield
                            msk, kmsk = msk_r.next()
                            yield
                            p.op("dve", lambda e, lg=lg, mx8=mx8, msk=msk: e.tensor_scalar(out=msk[:], in0=lg[:], scalar1=mx8[:, 3:4], scalar2=None, op0=ALU.is_ge),
                                 reads=[klg, kmx], writes=[kmsk])
                            yield
                            p.op("dve", lambda e, mx8=mx8: e.tensor_scalar(out=mx8[:, 7:8], in0=mx8[:, 0:1], scalar1=-1.0, scalar2=None, op0=ALU.mult),
                                 reads=[kmx, kmsk], writes=[kmx])
                            yield
                            ex, kex = ex_r.next()
                            yield
                            p.op("act", lambda e, lg=lg, mx8=mx8, ex=ex: e.activation(out=ex[:], in_=lg[:], func=AF.Exp, bias=mx8[:, 7:8]), reads=[klg, kmx], writes=[kex])
                            yield
                            sm_, ksm = sm_r.next()
                            yield
                            p.op("dve", lambda e, ex=ex, msk=msk: e.tensor_tensor(out=ex[:], in0=ex[:], in1=msk[:], op=ALU.mult), reads=[kex, kmsk], writes=[kex])
                            yield
                            p.op("dve", lambda e, ex=ex, sm_=sm_: e.reduce_sum(out=sm_[:], in_=ex[:], axis=mybir.AxisListType.X), reads=[kex], writes=[ksm])
                            yield
                            p.op("dve", lambda e, sm_=sm_: e.reciprocal(out=sm_[:], in_=sm_[:]), reads=[ksm], writes=[ksm])
                            yield
                            p.op("dve", lambda e, ex=ex, sm_=sm_, tile=tile: e.tensor_scalar(out=Gt[:, tile, :], in0=ex[:], scalar1=sm_[:, 0:1], scalar2=None, op0=ALU.mult),
                                 reads=[kex, ksm], writes=["Gt%d" % tile])
                            yield
                            p.op("dve", lambda e: e.tensor_copy(out=maskb[:, tile, :], in_=msk[:]), reads=[kmsk], writes=["maskb%d" % tile])
                            yield
                            gtmp, kgtmp = gtmp_r.next()
                            yield
                            p.op("pe", lambda e: e.matmul(out=pPos, lhsT=tris[:], rhs=maskb[:, tile, :], start=True, stop=(tile == 0)),
                                 reads=["tris", "maskb%d" % tile], writes=[kpLg])
                            yield
                            for j_ in range(tile):
                                p.op("pe", lambda e: e.matmul(out=pPos, lhsT=ones[:], rhs=maskb[:, j_, :], start=False, stop=(j_ == tile - 1)),
                                     reads=["ones", "maskb%d" % j_], writes=[kpLg])
                            yield
                            p.op("dve", lambda e: e.scalar_tensor_tensor(out=gtmp[:], in0=pPos, scalar=1.0, in1=msk[:], op0=ALU.add, op1=ALU.mult),
                                 reads=[kpLg, kmsk, kgtmp], writes=[kgtmp])
                            yield
                            p.op("dve", lambda e: e.tensor_scalar(out=posm[:, tile, :], in0=gtmp[:], scalar1=-1.0, scalar2=None, op0=ALU.add),
                                 reads=[kgtmp], writes=["posm%d" % tile])
                            yield
                            p.op("dve", lambda e: e.scalar_tensor_tensor(out=pq[:], in0=pPos, scalar=float(CAP - 1), in1=ecap[:], op0=ALU.min, op1=ALU.add),
                                 reads=[kpLg, "ecap"], writes=[kpq])
                            yield
                            yield
                            p.op("dve", lambda e: e.scalar_tensor_tensor(out=gtmp[:], in0=pPos, scalar=float(CAP) - 0.5, in1=Gt[:, tile, :], op0=ALU.is_lt, op1=ALU.mult),
                                 reads=[kpLg, "Gt%d" % tile, kgtmp], writes=[kgtmp])
                            yield
                            for j_ in range(4):
                                p.op("dve", lambda e: e.tensor_scalar(out=oh[:], in0=lg[:], scalar1=mx8[:, j_:j_ + 1], scalar2=None, op0=ALU.is_equal),
                                     reads=[klg, kmx], writes=["oh"])
                                p.op("dve", lambda e: e.tensor_tensor(out=pr[:], in0=oh[:], in1=pq[:], op=ALU.mult), reads=["oh", kpq], writes=["pr"])
                                p.op("dve", lambda e: e.reduce_sum(out=slf[:, j_:j_ + 1], in_=pr[:], axis=mybir.AxisListType.X), reads=["pr"], writes=[kslf])
                                p.op("dve", lambda e: e.tensor_tensor(out=pr[:], in0=oh[:], in1=gtmp[:], op=ALU.mult), reads=["oh", kgtmp, "pr"], writes=["pr"])
                                p.op("dve", lambda e: e.reduce_sum(out=gj[:, tile, j_:j_ + 1], in_=pr[:], axis=mybir.AxisListType.X), reads=["pr"], writes=["gj%d" % tile])
                            yield
                            p.op("dve", lambda e: e.tensor_copy(out=slI[:, tile, :], in_=slf[:]), reads=[kslf], writes=["slI%d" % tile])

                        for pair in range(2):
                            gens = [c1_tile(G, pair * 2), c1_tile(G, pair * 2 + 1)]
                            while gens:
                                for g_ in list(gens):
                                    try:
                                        next(g_)
                                    except StopIteration:
                                        gens.remove(g_)
                    p.barrier()
                    if stop_after == "C1":
                        if debug:
                            p.op("sp", lambda e: e.dma_start(out=dbgS.rearrange("(t p) n -> p t n", p=128), in_=slI[:]), reads=["slI%d" % i_ for i_ in range(16)], dma="dbgS")
                            p.op("sp", lambda e: e.dma_start(out=dbgJ.rearrange("(t p) n -> p t n", p=128), in_=gj[:]), reads=["gj%d" % i_ for i_ in range(16)], dma="dbgJ")
                            p.op("sp", lambda e: e.dma_start(out=dbgH.rearrange("(t p) n -> p t n", p=128), in_=H[:]), reads=["H%d" % i_ for i_ in range(16)], dma="dbgH")
                            p.op("sp", lambda e: e.dma_start(out=dbgG.rearrange("(t p) n -> p t n", p=128), in_=Gt[:]), reads=["Gt%d" % i_ for i_ in range(16)], dma="dbgG")
                        p.finish_wait("sp"); p.emit(top); return nc
                s2 = ExitStack()
                with s2:
                    sb2, ps2 = mk_alloc(s2)
                    W_r = Ring(sb2, "Wx", 6, [128, 8, 512], BF16)
                    bd_r = Ring(sb2, "bdn", 2, [1, D], BF16)
                    pGL_r = Ring(ps2, "pGL", 4, [128, 512], F32)
                    pA_r = Ring(ps2, "pA", 2, [128, 512], F32)
                    pTs = ps2("pTs", [128, 8, 128], BF16)
                    ptk = ps2("ptk", [128, 8], F32)
                    gl_r = Ring(sb2, "gl", 1, [128, CAP], F32)
                    sg_r = Ring(sb2, "sg", 1, [128, CAP], F32)
                    Sel = sb2("Sel", [128, 16, CAP], BF16)
                    Xe_r = Ring(sb2, "Xe", 2, [128, NS, D], BF16)
                    XeT = sb2("XeT", [128, 8, CAP], BF16)
                    aT = sb2("aTs", [128, 8, CAP], BF16)
                    Yst_r = Ring(sb2, "Yst", 2, [128, D], F32)
                    tks = sb2("tks", [128, 8], F32)
                    tkf = sb2("tkf", [128, 4], F32)
                    tkI_r = Ring(sb2, "tkI", 2, [128, 4], I32)
                    iota_i = sb2("iota_i", [128, CAP], I32)
                    iota_f = sb2("iota_f", [128, CAP], F32)
                    p.op("pool", lambda e: e.iota(iota_i[:], pattern=[[1, CAP]], base=0, channel_multiplier=0), writes=["iota_i"])
                    p.op("dve", lambda e: e.tensor_copy(out=iota_f[:], in_=iota_i[:]), reads=["iota_i"], writes=["iota_f"])
                    tid = sb2("tid", [128, 16], I32)
                    tidx = sb2("tidx", [128, 16], I32)
                    tidhl = sb2("tidhl", [128, 16, 2], BF16)
                    p.op("pool", lambda e: e.iota(tid[:], pattern=[[128, 16]], base=0, channel_multiplier=1), writes=["tid"])
                    p.op("dve", lambda e: e.tensor_scalar(out=tidx[:], in0=tid[:], scalar1=6, scalar2=None, op0=ALU.arith_shift_right), reads=["tid"], writes=["tidx"])
                    p.op("dve", lambda e: e.tensor_copy(out=tidhl[:, :, 0], in_=tidx[:]), reads=["tidx"], writes=["tidhl"])
                    p.op("dve", lambda e: e.tensor_scalar(out=tidx[:], in0=tid[:], scalar1=63, scalar2=None, op0=ALU.bitwise_and), reads=["tid", "tidx", "tidhl"], writes=["tidx"])
                    p.op("dve", lambda e: e.tensor_copy(out=tidhl[:, :, 1], in_=tidx[:]), reads=["tidx", "tidhl"], writes=["tidhl"])
                    bg3 = bgT[:].rearrange("p (e c) -> p e c", c=16)
                    p.op("dve", lambda e: e.tensor_scalar(out=bg3[:, :, 8:16], in0=bg3[:, :, 8:16], scalar1=1.0, scalar2=None, op0=ALU.add),
                         reads=["bgT"], writes=["bgT"])

                    def load_w(src, slot):
                        Wt, kW = W_r.tiles[slot], W_r.keys[slot]
                        p.op("pool", lambda e: e.dma_start(out=Wt[:], in_=src.rearrange("(c p) n -> p c n", p=128)), writes=[kW], dma=kW)
                        return Wt, kW

                    def load_bd(ex_):
                        bd, kbd = bd_r.next()
                        p.op("pool", lambda e: e.dma_start(out=bd[:], in_=b_dn[ex_:ex_ + 1, :]), writes=[kbd], dma=kbd)
                        return bd, kbd

                    xe_of = {}

                    def sel_build(ex_, tiles):
                        for tile in tiles:
                            p.op("dve", lambda e: e.tensor_scalar(out=Sel[:, tile, :], in0=iota_f[:], scalar1=posm[:, tile, ex_:ex_ + 1], scalar2=None, op0=ALU.is_equal),
                                 reads=["iota_f", "posm%d" % tile], writes=["Sel%d" % tile])

                    def dispatch(ex_, build=True):
                        if build:
                            sel_build(ex_, range(16))
                        for s_ in range(NS):
                            for tile in range(16):
                                p.op("pe", lambda e: e.matmul(out=ptk[:, 2 * s_:2 * s_ + 2], lhsT=Sel[:, tile, s_ * 128:(s_ + 1) * 128], rhs=tidhl[:, tile, :],
                                                              start=(tile == 0), stop=(tile == 15)), reads=["Sel%d" % tile, "tidhl"], writes=["ptk"])
                        p.op("dve", lambda e: e.tensor_copy(out=tks[:, 0:2 * NS], in_=ptk[:, 0:2 * NS]), reads=["ptk"], writes=["tks"])
                        tk3 = tks[:, 0:2 * NS].rearrange("p (s t) -> p s t", t=2)
                        p.op("dve", lambda e: e.scalar_tensor_tensor(out=tkf[:, 0:NS], in0=tk3[:, :, 0], scalar=64.0, in1=tk3[:, :, 1], op0=ALU.mult, op1=ALU.add),
                             reads=["tks"], writes=["tkf"])
                        tkI, ktkI = tkI_r.next()
                        p.op("dve", lambda e: e.tensor_copy(out=tkI[:, 0:NS], in_=tkf[:, 0:NS]), reads=["tkf"], writes=[ktkI])
                        Xe, kXe = Xe_r.next()
                        for s_ in range(NS):
                            p.op("pool", lambda e: e.indirect_dma_start(out=Xe[:, s_, :], out_offset=None, in_=Ud[:, :],
                                                                         in_offset=bass.IndirectOffsetOnAxis(ap=tkI[:, s_:s_ + 1], axis=0)),
                                 reads=[ktkI], writes=[kXe], dma=kXe)
                        xe_of[ex_] = (Xe, kXe)

                    def transposes(ex_):
                        Xe, kXe = xe_of.pop(ex_)
                        for s_ in range(NS):
                            for k in range(8):
                                p.op("pe", lambda e: e.transpose(out=pTs[:, k, :], in_=Xe[:, s_, k * 128:(k + 1) * 128], identity=ident[:]),
                                     reads=[kXe, "ident"], writes=["pTs"])
                            if s_ % 2 == 0:
                                p.op("act", lambda e: e.copy(out=XeT[:, :, s_ * 128:(s_ + 1) * 128], in_=pTs[:]), reads=["pTs"], writes=["XeT"])
                            else:
                                p.op("dve", lambda e: e.tensor_copy(out=XeT[:, :, s_ * 128:(s_ + 1) * 128], in_=pTs[:]), reads=["pTs"], writes=["XeT"])

                    def gu_stage(ex_, st, Wg, kWg, Wl, kWl, sel_for=None):
                        for mc in range(4):
                            if sel_for is not None:
                                sel_build(sel_for, range(mc * 4, mc * 4 + 4))
                            c = st * 4 + mc
                            pG, kpG = pGL_r.next()
                            pLn, kpLn = pGL_r.next()
                            for k in range(8):
                                p.op("pe", lambda e: e.matmul(out=pG[:, 0:CAP], lhsT=Wg[:, k, mc * 128:(mc + 1) * 128], rhs=XeT[:, k, :],
                                                              start=(k == 0), stop=(k == 7)), reads=[kWg, "XeT"], writes=[kpG])
                            for k in range(8):
                                p.op("pe", lambda e: e.matmul(out=pLn[:, 0:CAP], lhsT=Wl[:, k, mc * 128:(mc + 1) * 128], rhs=XeT[:, k, :],
                                                              start=(k == 0), stop=(k == 7)), reads=[kWl, "XeT"], writes=[kpLn])
                            gl, kgl = gl_r.next()
                            sg, ksg = sg_r.next()
                            bgc = ex_ * 16 + c
                            blc = ex_ * 16 + 8 + c
                            kaT = "aT_%d" % st
                            p.op("dve", lambda e: e.tensor_scalar(out=gl[:], in0=pG[:, 0:CAP], scalar1=bgT[:, bgc:bgc + 1], scalar2=7.0, op0=ALU.add, op1=ALU.min),
                                 reads=[kpG, "bgT"], writes=[kgl])
                            p.op("act", lambda e: e.activation(out=sg[:], in_=gl[:], func=AF.Sigmoid, scale=1.702), reads=[kgl], writes=[ksg])
                            p.op("dve", lambda e: e.tensor_tensor(out=sg[:], in0=sg[:], in1=gl[:], op=ALU.mult), reads=[kgl, ksg], writes=[ksg])
                            p.op("dve", lambda e: e.tensor_scalar(out=gl[:], in0=pLn[:, 0:CAP], scalar1=bgT[:, blc:blc + 1], scalar2=-6.0, op0=ALU.add, op1=ALU.max),
                                 reads=[kpLn, "bgT", kgl], writes=[kgl])
                            p.op("dve", lambda e: e.scalar_tensor_tensor(out=aT[:, c, :], in0=gl[:], scalar=8.0, in1=sg[:], op0=ALU.min, op1=ALU.mult),
                                 reads=[ksg, kgl], writes=[kaT])

                    def dn_stage(ex_, d0, d1, bd, kbd):
                        for s_ in range(NS):
                            Yst, kY = Yst_r.next()
                            for half in range(2):
                                Wd, kWd = (d0, d1)[half]
                                pA, kpA = pA_r.next()
                                for c in range(8):
                                    p.op("pe", lambda e: e.matmul(out=pA[:], lhsT=aT[:, c, s_ * 128:(s_ + 1) * 128], rhs=Wd[:, c, :], start=(c == 0), stop=False),
                                         reads=["aT_%d" % (c // 4), kWd], writes=[kpA])
                                p.op("pe", lambda e: e.matmul(out=pA[:], lhsT=ones[0:1, :], rhs=bd[0:1, half * 512:(half + 1) * 512], start=False, stop=True),
                                     reads=["ones", kbd], writes=[kpA])
                                p.op("act", lambda e: e.copy(out=Yst[:, half * 512:(half + 1) * 512], in_=pA[:]), reads=[kpA], writes=[kY])
                            r0 = ex_ * CAP + s_ * 128
                            p.op("sp", lambda e: e.dma_start(out=Yd[r0:r0 + 128, :], in_=Yst[:]), reads=[kY], dma=kY + "s")

                    def loads_gl0(ex_):
                        return load_w(w_gu[ex_, :, 0:512], 0), load_w(w_gu[ex_, :, 1024:1536], 1)

                    def loads_gl1(ex_):
                        return load_w(w_gu[ex_, :, 512:1024], 2), load_w(w_gu[ex_, :, 1536:2048], 3)

                    def loads_d(ex_):
                        return load_w(w_dn[ex_, :, 0:512], 4), load_w(w_dn[ex_, :, 512:1024], 5), load_bd(ex_)

                    g0, l0 = loads_gl0(0)
                    g1, l1 = loads_gl1(0)
                    d0, d1, (bd, kbd) = loads_d(0)
                    dispatch(0)
                    dispatch(1)
                    for ex_ in range(NE):
                        transposes(ex_)
                        gu_stage(ex_, 0, g0[0], g0[1], l0[0], l0[1], sel_for=(ex_ + 2 if ex_ + 2 < NE else None))
                        if ex_ + 1 < NE:
                            g0n, l0n = loads_gl0(ex_ + 1)
                        if ex_ + 2 < NE:
                            dispatch(ex_ + 2, build=False)
                        gu_stage(ex_, 1, g1[0], g1[1], l1[0], l1[1])
                        if ex_ + 1 < NE:
                            g1n, l1n = loads_gl1(ex_ + 1)
                        dn_stage(ex_, d0, d1, bd, kbd)
                        if ex_ + 1 < NE:
                            d0, d1, (bd, kbd) = loads_d(ex_ + 1)
                            g0, l0, g1, l1 = g0n, l0n, g1n, l1n
                    p.barrier()
                    Yg_r = Ring(sb2, "Yg", 4, [128, D], F32)
                    for tile in range(16):
                        hk = "H%d" % tile
                        for j_ in range(4):
                            Yg, kYg = Yg_r.next()
                            p.op("pool", lambda e: e.indirect_dma_start(out=Yg[:, :], out_offset=None, in_=Yd[:, :],
                                                                         in_offset=bass.IndirectOffsetOnAxis(ap=slI[:, tile, j_:j_ + 1], axis=0)),
                                 reads=["slI%d" % tile], writes=[kYg], dma=kYg)
                            p.op("dve", lambda e: e.scalar_tensor_tensor(out=H[:, tile, :], in0=Yg[:], scalar=gj[:, tile, j_:j_ + 1], in1=H[:, tile, :], op0=ALU.mult, op1=ALU.add),
                                 reads=[kYg, "gj%d" % tile, hk], writes=[hk])
                    p.barrier()
                    if stop_after == "C2":
                        if debug:
                            p.op("sp", lambda e: e.dma_start(out=dbgH.rearrange("(t p) n -> p t n", p=128), in_=H[:]), reads=["H%d" % i_ for i_ in range(16)], dma="dbgH")
                            p.op("sp", lambda e: e.dma_start(out=dbgG.rearrange("(t p) n -> p t n", p=128), in_=Gt[:]), reads=["Gt%d" % i_ for i_ in range(16)], dma="dbgG")
                        p.finish_wait("sp"); p.emit(top); return nc
                s3 = ExitStack()
                with s3:
                    sb3, ps3 = mk_alloc(s3)
                    gbc = sb3("gbc3", [128, D], F32)
                    junk = sb3("junkC3", [128, D], BF16)
                    Wpg = sb3("Wpg", [128, 8, D], BF16)
                    Wpp = sb3("Wpp", [128, 2, D], BF16)
                    p.op("pool", lambda e: e.dma_start(out=Wpg[:], in_=w_pg.rearrange("(c p) n -> p c n", p=128)), writes=["Wpg"], dma="Wpg")
                    p.op("pool", lambda e: e.dma_start(out=Wpp[:], in_=w_pp.rearrange("(c p) n -> p c n", p=128)), writes=["Wpp"], dma="Wpp")
                    gfin = sb3("gfin", [128, D], F32)
                    p.op("sp", lambda e: e.dma_start(out=gbc[:], in_=g_ple.partition_broadcast(128)), writes=["gbc"], dma="gbc3")
                    p.op("sp", lambda e: e.dma_start(out=gfin[:], in_=g_final.partition_broadcast(128)), writes=["gfin"], dma="gfin")
                    ss3_r = Ring(sb3, "ss3", 2, [128, 1], F32)
                    u3_r = Ring(sb3, "u3", 2, [128, D], BF16)
                    pT3_r = Ring(ps3, "pT3", 2, [128, 8, 128], BF16)
                    u3T_r = Ring(sb3, "u3T", 2, [128, 8, 128], BF16)
                    pp_r = Ring(sb3, "ppl", 2, [128, 256], F32)
                    ppb_r = Ring(sb3, "ppb", 2, [128, 256], BF16)
                    ppT_r = Ring(sb3, "ppT", 2, [128, 2, 128], BF16)
                    pg_r = Ring(ps3, "pg3", 2, [128, 512], F32)
                    pj_r = Ring(ps3, "pj3", 2, [128, 512], F32)
                    sg3_r = Ring(sb3, "sg3", 2, [128, 512], F32)
                    o_r = Ring(sb3, "o3", 2, [128, D], F32)
                    def c3_tile(tile):
                        hk = "H%d" % tile
                        yield
                        ss3, kss = ss3_r.next()
                        yield
                        p.op("act", lambda e, ss3=ss3, tile=tile: e.activation(out=junk[:], in_=H[:, tile, :], func=AF.Square, accum_out=ss3[:]), reads=[hk], writes=["junkC", kss])
                        yield
                        rstd_from(ss3[:], ss3[:], D, [kss], [kss])
                        yield
                        u3, ku3 = u3_r.next()
                        yield
                        p.op("dve", lambda e, ss3=ss3, u3=u3, tile=tile: e.scalar_tensor_tensor(out=u3[:], in0=H[:, tile, :], scalar=ss3[:, 0:1], in1=gbc[:], op0=ALU.mult, op1=ALU.mult),
                             reads=[hk, kss, "gbc"], writes=[ku3])
                        yield
                        pT, kpT = pT3_r.next()
                        yield
                        for c in range(8):
                            p.op("pe", lambda e, c=c, pT=pT, u3=u3: e.transpose(out=pT[:, c, :], in_=u3[:, c * 128:(c + 1) * 128], identity=ident[:]), reads=[ku3, "ident"], writes=[kpT])
                        yield
                        u3T, ku3T = u3T_r.next()
                        yield
                        p.op("act", lambda e, pT=pT, u3T=u3T: e.copy(out=u3T[:], in_=pT[:]), reads=[kpT], writes=[ku3T])
                        yield
                        pp, kpp = pp_r.next()
                        yield
                        p.op("sp", lambda e, pp=pp, tile=tile: e.dma_start(out=pp[:], in_=po[tile * 128:(tile + 1) * 128, :]), writes=[kpp], dma=kpp)
                        yield
                        ppb, kppb = ppb_r.next()
                        yield
                        p.op("pool", lambda e, pp=pp, ppb=ppb: e.tensor_copy(out=ppb[:], in_=pp[:]), reads=[kpp], writes=[kppb])
                        yield
                        pT2, kpT2 = pT3_r.next()
                        yield
                        for c in range(2):
                            p.op("pe", lambda e, c=c, pT2=pT2, ppb=ppb: e.transpose(out=pT2[:, c, :], in_=ppb[:, c * 128:(c + 1) * 128], identity=ident[:]), reads=[kppb, "ident"], writes=[kpT2])
                        yield
                        ppT, kppT = ppT_r.next()
                        yield
                        p.op("act", lambda e, pT2=pT2, ppT=ppT: e.copy(out=ppT[:], in_=pT2[:, 0:2, :]), reads=[kpT2], writes=[kppT])
                        yield
                        for half in range(2):
                            pg, kpg = pg_r.next()
                            pj, kpj = pj_r.next()
                            for c in range(8):
                                p.op("pe", lambda e, c=c, pg=pg, u3T=u3T, half=half: e.matmul(out=pg[:], lhsT=u3T[:, c, :], rhs=Wpg[:, c, half * 512:(half + 1) * 512], start=(c == 0), stop=(c == 7)),
                                     reads=[ku3T, "Wpg"], writes=[kpg])
                            for c in range(2):
                                p.op("pe", lambda e, c=c, pj=pj, ppT=ppT, half=half: e.matmul(out=pj[:], lhsT=ppT[:, c, :], rhs=Wpp[:, c, half * 512:(half + 1) * 512], start=(c == 0), stop=(c == 1)),
                                     reads=[kppT, "Wpp"], writes=[kpj])
                            sg, ksg = sg3_r.next()
                            p.op("act", lambda e, sg=sg, pg=pg: e.activation(out=sg[:], in_=pg[:], func=AF.Sigmoid), reads=[kpg], writes=[ksg])
                            p.op("dve", lambda e, sg=sg, pj=pj: e.tensor_tensor(out=sg[:], in0=sg[:], in1=pj[:], op=ALU.mult), reads=[ksg, kpj], writes=[ksg])
                            p.op("dve", lambda e, sg=sg, tile=tile, half=half: e.tensor_tensor(out=H[:, tile, half * 512:(half + 1) * 512], in0=H[:, tile, half * 512:(half + 1) * 512], in1=sg[:], op=ALU.add),
                                 reads=[ksg, hk], writes=[hk])
                        yield
                        ss4, kss4 = ss3_r.next()
                        yield
                        p.op("act", lambda e, ss4=ss4, tile=tile: e.activation(out=junk[:], in_=H[:, tile, :], func=AF.Square, accum_out=ss4[:]), reads=[hk], writes=["junkC", kss4])
                        yield
                        rstd_from(ss4[:], ss4[:], D, [kss4], [kss4])
                        yield
                        ot, kot = o_r.next()
                        yield
                        p.op("dve", lambda e, ss4=ss4, ot=ot, tile=tile: e.scalar_tensor_tensor(out=ot[:], in0=H[:, tile, :], scalar=ss4[:, 0:1], in1=gfin[:], op0=ALU.mult, op1=ALU.mult),
                             reads=[hk, kss4, "gfin"], writes=[kot])
                        yield
                        p.op("sp", lambda e, ot=ot, tile=tile: e.dma_start(out=yo[tile * 128:(tile + 1) * 128, :], in_=ot[:]), reads=[kot], writes=["yo"], dma=kot + "s")

                    for pair in range(8):
                        gens = [c3_tile(pair * 2), c3_tile(pair * 2 + 1)]
                        while gens:
                            for g_ in list(gens):
                                try:
                                    next(g_)
                                except StopIteration:
                                    gens.remove(g_)
        p.finish_wait("sp")
        p.emit(top)
    return nc


_CACHE = {}


def _perm(j):
    idx = []
    for t in range(4):
        for blk in ORDER[j]:
            b0 = (8 * t + blk) * 128
            idx.append(np.arange(b0, b0 + 128))
    return np.concatenate(idx)


def kernel(x, p, positions, w_in, g_attn, g_cq, w_uq, g_ckv, w_ukv, g_out_mla, g_out_sb, w_o,
           g_moe, w_router, b_router, w_gu, b_gu, w_dn, b_dn, g_ple, w_ple_gate, w_ple_proj, g_final):
    if "nc" not in _CACHE:
        _CACHE["nc"] = build_program()
    nc = _CACHE["nc"]
    in_maps, perms = make_in_maps(x, p, positions, w_in, g_attn, g_cq, w_uq, g_ckv, w_ukv, g_out_mla, g_out_sb, w_o,
                                  g_moe, w_router, b_router, w_gu, b_gu, w_dn, b_dn, g_ple, w_ple_gate, w_ple_proj, g_final)
    res = run_bass_kernel_spmd(nc, in_maps, core_ids=list(range(8)))
    out = np.empty((4, S, D), np.float32)
    for c in range(8):
        b, j = c // 2, c % 2
        out[b, perms[j]] = np.asarray(res.results[c]["yo"])
    return out


def make_in_maps(x, p, positions, w_in, g_attn, g_cq, w_uq, g_ckv, w_ukv, g_out_mla, g_out_sb, w_o,
                 g_moe, w_router, b_router, w_gu, b_gu, w_dn, b_dn, g_ple, w_ple_gate, w_ple_proj, g_final):
    f = lambda a: np.ascontiguousarray(np.asarray(a))
    x = f(x); p = f(p); positions = f(positions)
    invf = np.zeros((128, 1), np.float32)
    fr = (10000.0 ** (-np.arange(0, 32, 2, dtype=np.float32) / 32.0)).astype(np.float32)
    invf[0:16, 0] = fr
    invf[16:32, 0] = fr
    shared = {
        "invf": invf,
        "w_in": f(w_in[0]), "g_attn": f(g_attn[0:1]), "g_cq": f(g_cq[0:1]), "w_uq": f(w_uq[0]),
        "g_ckv": f(g_ckv[0:1]), "w_ukv": f(w_ukv[0]),
        "g_out": f(np.concatenate([np.asarray(g_out_mla[0]), np.asarray(g_out_sb[0])])[None, :]),
        "w_o": f(w_o[0]), "g_moe": f(g_moe[0:1]), "w_router": f(w_router[0]), "b_router": f(b_router[0:1]),
        "w_gu": f(w_gu[0]), "b_gu": f(np.asarray(b_gu[0]).reshape(NE * 16, 128)), "w_dn": f(w_dn[0]), "b_dn": f(b_dn[0]),
        "g_ple": f(g_ple[0:1]), "w_pg": f(w_ple_gate[0]), "w_pp": f(w_ple_proj[0]), "g_final": f(np.asarray(g_final)[None, :]),
    }
    in_maps = []
    perms = [_perm(0), _perm(1)]
    for c in range(8):
        b, j = c // 2, c % 2
        pm = perms[j]
        qr = np.concatenate([np.arange(blk * 128, blk * 128 + 128) for blk in ORDER[j]]).astype(np.float32)[None, :]
        m = dict(shared)
        m["xa"] = x[b]
        m["xo"] = f(x[b][pm])
        m["po"] = f(p[0, b][pm])
        m["posa"] = f(positions[b:b + 1].astype(np.int32))
        m["poso"] = f(positions[b:b + 1, pm].astype(np.int32))
        m["qrel"] = f(qr)
        in_maps.append(m)
    return in_maps, perms
```

```python
from contextlib import ExitStack
import numpy as np
import concourse.bass as bass
import concourse.mybir as mybir
from concourse.bass_utils import run_bass_kernel_spmd

F32 = mybir.dt.float32
BF16 = mybir.dt.bfloat16
I32 = mybir.dt.int32
AF = mybir.ActivationFunctionType
ALU = mybir.AluOpType

ENGS = ("pe", "act", "dve", "pool", "sp")
S = 4096
D = 1024
NE = 32
ORDER = ([6, 5, 3, 0], [7, 4, 2, 1])
NDIAG = [512, 512, 384, 384, 256, 256, 128, 128]
NEG = -30000.0
EPS = 1e-6


class Prog:
    def __init__(self, nc):
        self.nc = nc
        self.ops = {e: [] for e in ENGS}
        self.vcs = {}
        self.cur = {e: {} for e in ENGS}
        self.last_w = {}
        self.readers = {}
        self.excl = set()

    def op(self, eng, fn, reads=(), writes=(), dma=None):
        rec_ = _Rec()
        fn(rec_)
        assert len(rec_.calls) == 1
        fn = rec_.calls[0]
        clk = ("dma:" + dma) if dma else eng
        deps = []
        reads = list(reads)
        writes = list(writes)
        for k in reads:
            if k in self.excl and k not in writes:
                writes.append(k)
        for k in reads:
            lw = self.last_w.get(k)
            if lw:
                deps.append(lw)
        for k in writes:
            lw = self.last_w.get(k)
            if lw:
                deps.append(lw)
            for c, i in self.readers.get(k, {}).items():
                deps.append((c, i))
        cur = self.cur[eng]
        wmax = {}
        for (c, i) in deps:
            if c == "pe" and eng == "pe" and not dma:
                continue
            if cur.get(c, 0) >= i:
                continue
            wmax[c] = max(wmax.get(c, 0), i)
            for c2, i2 in self.vcs[c][i - 1].items():
                if cur.get(c2, 0) < i2:
                    cur[c2] = i2
            if cur.get(c, 0) < i:
                cur[c] = i
        vc = dict(cur)
        lst = self.vcs.setdefault(clk, [])
        lst.append(vc)
        idx = len(lst)
        vc[clk] = idx
        rec = {"fn": fn, "waits": wmax, "clk": clk, "idx": idx}
        self.ops[eng].append(rec)
        for k in reads:
            self.readers.setdefault(k, {})[clk] = idx
        for k in writes:
            self.last_w[k] = (clk, idx)
            self.readers[k] = {}
        return rec

    def finish_wait(self, eng):
        waits = {}
        for c, l in self.vcs.items():
            if len(l) and self.cur[eng].get(c, 0) < len(l):
                waits[c] = len(l)
                self.cur[eng][c] = len(l)
        self.ops[eng].append({"fn": None, "waits": waits, "clk": None, "idx": None})

    def barrier(self):
        for e in ENGS:
            self.finish_wait(e)
        full = {c: len(l) for c, l in self.vcs.items()}
        for e in ENGS:
            self.cur[e] = dict(full)

    def emit(self, stack):
        nc = self.nc
        waited = {}
        for e in ENGS:
            for r in self.ops[e]:
                for c, i in r["waits"].items():
                    waited.setdefault(c, set()).add(i)
        sems, semval = {}, {}
        for c, l in self.vcs.items():
            if c not in waited:
                continue
            sems[c] = stack.enter_context(nc.semaphore("s_" + c.replace(":", "_")))
            isd = c.startswith("dma:")
            v, m = 0, {}
            for i in range(1, len(l) + 1):
                if isd or i in waited[c]:
                    v += 16 if isd else 1
                    m[i] = v
            semval[c] = m
        block = stack.enter_context(nc.Block())
        engobj = {"pe": "tensor", "act": "scalar", "dve": "vector", "pool": "gpsimd", "sp": "sync"}

        def make(e):
            def body(eng):
                for r in self.ops[e]:
                    for c, i in r["waits"].items():
                        eng.wait_ge(sems[c], semval[c][i])
                    if r["fn"] is None:
                        continue
                    name, a, k = r["fn"]
                    ins = getattr(eng, name)(*a, **k)
                    c, i = r["clk"], r["idx"]
                    if c in sems and i in semval[c]:
                        ins.then_inc(sems[c], 16 if c.startswith("dma:") else 1)
            return body

        for e in ENGS:
            if self.ops[e]:
                getattr(block, engobj[e])(make(e))


class _Rec:
    def __init__(self):
        self.calls = []

    def __getattr__(self, name):
        def f(*a, **k):
            self.calls.append((name, a, k))
        return f


class Ring:
    def __init__(self, alloc, name, n, shape, dtype):
        self.tiles = [alloc("%s%d" % (name, i), shape, dtype) for i in range(n)]
        self.keys = ["%s%d" % (name, i) for i in range(n)]
        self.i = 0

    def next(self):
        t, k = self.tiles[self.i % len(self.tiles)], self.keys[self.i % len(self.tiles)]
        self.i += 1
        return t, k


class _Stop(Exception):
    pass


def build_program(stop_after=None, debug=False):
    nc = bass.Bass("TRN2", target_bir_lowering=False)

    def din(name, shape, dt=F32):
        return nc.dram_tensor(name, list(shape), dt, kind="ExternalInput").ap()

    def dscr(name, shape, dt=BF16):
        return nc.dram_tensor(name, list(shape), dt, kind="ExternalOutput" if debug else "Internal").ap()

    xa = din("xa", [S, D])
    xo = din("xo", [2048, D])
    po = din("po", [2048, 256])
    posa = din("posa", [1, S], I32)
    poso = din("poso", [1, 2048], I32)
    qrel = din("qrel", [1, 512])
    invf = din("invf", [128, 1])
    w_in = din("w_in", [D, 2208])
    g_attn = din("g_attn", [1, D])
    g_cq = din("g_cq", [1, 384])
    w_uq = din("w_uq", [384, 768])
    g_ckv = din("g_ckv", [1, 256])
    w_ukv = din("w_ukv", [256, 1024])
    g_out = din("g_out", [1, 1024])
    w_o = din("w_o", [D, D])
    g_moe = din("g_moe", [1, D])
    w_router = din("w_router", [D, NE])
    b_router = din("b_router", [1, NE])
    NEd = NE if stop_after in (None, "C2") else 1
    w_gu = din("w_gu", [NEd, D, 2048])
    b_gu = din("b_gu", [NE * 16, 128])
    w_dn = din("w_dn", [NEd, D, D])
    b_dn = din("b_dn", [NE, D])
    g_ple = din("g_ple", [1, D])
    w_pg = din("w_pg", [D, D])
    w_pp = din("w_pp", [256, D])
    g_final = din("g_final", [1, D])
    yo = nc.dram_tensor("yo", [2048, D], F32, kind="ExternalOutput").ap()

    KnT = dscr("KnT", [512, S])
    KrT = dscr("KrT", [32, S])
    VmD = dscr("VmD", [S, 512])
    KsT = dscr("KsT", [512, S])
    VsD = dscr("VsD", [S, 512])
    QmT = dscr("QmT", [8 * 96, 2048])
    QsT = dscr("QsT", [512, 2048])
    OTd = dscr("OTd", [1024, 2048])
    CAP = 512
    NS = CAP // 128
    Ud = nc.dram_tensor("Ud", [2048, D], BF16, kind="Internal").ap()
    Yd = nc.dram_tensor("Yd", [NE * CAP, D], F32, kind="Internal").ap()
    if debug:
        dbgH = nc.dram_tensor("dbgH", [2048, D], F32, kind="ExternalOutput").ap()
        dbgG = nc.dram_tensor("dbgG", [2048, NE], F32, kind="ExternalOutput").ap()
        dbgS = nc.dram_tensor("dbgS", [2048, 4], I32, kind="ExternalOutput").ap()
        dbgJ = nc.dram_tensor("dbgJ", [2048, 4], F32, kind="ExternalOutput").ap()

    top = ExitStack()
    with top:
        p = Prog(nc)
        if True:

            def mk_alloc(stack):
                def sb(n, s, d):
                    return stack.enter_context(nc.sbuf_tensor(n, list(s), d))

                def ps(n, s, d=F32):
                    return stack.enter_context(nc.psum_tensor(n, list(s), d))
                return sb, ps

            sb0, ps0 = mk_alloc(top)

            identf = sb0("identf", [128, 128], F32)
            ident = sb0("ident", [128, 128], BF16)
            ones = sb0("ones", [128, 128], BF16)
            ntri = sb0("ntri", [128, 128], BF16)
            nones = sb0("nones", [128, 128], BF16)
            zeros = sb0("zeros", [128, 128], BF16)
            p.op("pool", lambda e: e.memset(identf[:], 0.0), writes=["identf"])
            p.op("pool", lambda e: e.affine_select(out=identf[:], in_=identf[:], pattern=[[-1, 128]],
                                                    compare_op=ALU.not_equal, fill=1.0, base=0, channel_multiplier=1),
                 reads=["identf"], writes=["identf"])
            p.op("dve", lambda e: e.tensor_copy(out=ident[:], in_=identf[:]), reads=["identf"], writes=["ident"])
            p.op("pool", lambda e: e.memset(ones[:], 1.0), writes=["ones"])
            p.op("pool", lambda e: e.memset(nones[:], -1.0), writes=["nones"])
            p.op("pool", lambda e: e.memset(zeros[:], 0.0), writes=["zeros"])
            gcol = sb0("gcol", [128, 16], F32)
            p.op("sp", lambda e: e.dma_start(out=gcol[:, 0:3], in_=g_cq.rearrange("o (c p) -> p (o c)", p=128),
                                             allow_slow_non_contiguous=True), writes=["gcol"], dma="gcol")
            p.op("sp", lambda e: e.dma_start(out=gcol[:, 3:5], in_=g_ckv.rearrange("o (c p) -> p (o c)", p=128),
                                             allow_slow_non_contiguous=True), writes=["gcol"], dma="gcol")
            p.op("sp", lambda e: e.dma_start(out=gcol[:, 5:13], in_=g_out.rearrange("o (c p) -> p (o c)", p=128),
                                             allow_slow_non_contiguous=True), writes=["gcol"], dma="gcol")
            invc = sb0("invc", [128, 1], F32)
            p.op("sp", lambda e: e.dma_start(out=invc[:], in_=invf), writes=["invc"], dma="invc")

            def rstd_from(eng_out, src, n, rd, wr):
                p.op("act", lambda e: e.activation(out=eng_out, in_=src, func=AF.Ln, scale=1.0 / n, bias=EPS),
                     reads=rd, writes=wr)
                p.op("act", lambda e: e.activation(out=eng_out, in_=eng_out, func=AF.Exp, scale=-0.5),
                     reads=wr, writes=wr)

            sa = ExitStack()
            with sa:
                sb, ps = mk_alloc(sa)
                tmpf = sb("tmpf", [128, 128], F32)
                p.op("pool", lambda e: e.memset(tmpf[:], -1.0), writes=["tmpf"])
                p.op("pool", lambda e: e.affine_select(out=tmpf[:], in_=tmpf[:], pattern=[[-1, 128]],
                                                        compare_op=ALU.is_ge, fill=0.0, base=0, channel_multiplier=1),
                     reads=["tmpf"], writes=["tmpf"])
                p.op("dve", lambda e: e.tensor_copy(out=ntri[:], in_=tmpf[:]), reads=["tmpf"], writes=["ntri"])

                Win = sb("Win", [128, 8, 2208], BF16)
                for c in range(8):
                    p.op("pool", lambda e, c=c: e.dma_start(out=Win[:, c, :], in_=w_in[c * 128:(c + 1) * 128, :]),
                         writes=["Win"], dma="Win")
                Wuq = sb("Wuq", [128, 3, 768], BF16)
                Wuqr = sb("Wuqr", [128, 3, 768], BF16)
                p.op("pool", lambda e: e.dma_start(out=Wuq[:], in_=w_uq.rearrange("(c p) n -> p c n", p=128)),
                     writes=["Wuq"], dma="Wuq")
                Wkn = sb("Wkn", [128, 2, 512], BF16)
                Wv = sb("Wv", [128, 2, 512], BF16)
                ukv = w_ukv.rearrange("(c p) (h t d) -> p c h t d", p=128, h=8, t=2)
                for c in range(2):
                    p.op("pool", lambda e, c=c: e.dma_start(out=Wkn[:, c, :].rearrange("p (h d) -> p h d", h=8),
                                                          in_=ukv[:, c, :, 0, :]), writes=["Wkn"], dma="Wkn")
                    p.op("pool", lambda e, c=c: e.dma_start(out=Wv[:, c, :].rearrange("p (h d) -> p h d", h=8),
                                                          in_=ukv[:, c, :, 1, :]), writes=["Wv"], dma="Wv")
                p.op("pool", lambda e: e.memset(Wuqr[:], 0.0), writes=["Wuqr"])
                Wq4 = Wuq[:].rearrange("p c (h d) -> p c h d", h=8)
                Wr4 = Wuqr[:].rearrange("p c (h d) -> p c h d", h=8)
                for c in range(3):
                    p.op("dve", lambda e, c=c: e.tensor_scalar(out=Wr4[:, c, :, 64:80], in0=Wq4[:, c, :, 80:96], scalar1=-1.0,
                                                          scalar2=None, op0=ALU.mult), reads=["Wuq", "Wuqr"], writes=["Wuqr"])
                    p.op("dve", lambda e, c=c: e.tensor_copy(out=Wr4[:, c, :, 80:96], in_=Wq4[:, c, :, 64:80]),
                         reads=["Wuq", "Wuqr"], writes=["Wuqr"])
                Wkr = sb("Wkr", [128, 8, 64], BF16)
                p.op("dve", lambda e: e.tensor_copy(out=Wkr[:, :, 0:32], in_=Win[:, :, 640:672]), reads=["Win"], writes=["Wkr"])
                p.op("dve", lambda e: e.tensor_scalar(out=Wkr[:, :, 32:48], in0=Win[:, :, 656:672], scalar1=-1.0, scalar2=None,
                                                      op0=ALU.mult), reads=["Win", "Wkr"], writes=["Wkr"])
                p.op("dve", lambda e: e.tensor_copy(out=Wkr[:, :, 48:64], in_=Win[:, :, 640:656]), reads=["Win", "Wkr"], writes=["Wkr"])
                gattn = sb("gattn", [128, D], F32)
                p.op("sp", lambda e: e.dma_start(out=gattn[:], in_=g_attn.partition_broadcast(128)), writes=["gattn"], dma="gattn")

                def rope_table(Ct, St, pos_ap, n, rows, name, inv, kinv, sb):
                    posi = sb(name + "_pi", [128, n], I32)
                    ang = sb(name + "_ang", [128, n], F32)
                    kk = sb(name + "_k", [128, n], F32)
                    ki = sb(name + "_ki", [128, n], I32)
                    p.op("sp", lambda e: e.dma_start(out=posi[:], in_=pos_ap.partition_broadcast(128)), writes=[name + "pi"], dma=name + "pi")
                    p.op("dve", lambda e: e.tensor_copy(out=ang[:], in_=posi[:]), reads=[name + "pi"], writes=[name + "ang"])
                    p.op("dve", lambda e: e.tensor_scalar(out=ang[:], in0=ang[:], scalar1=inv[:, 0:1], scalar2=None, op0=ALU.mult),
                         reads=[name + "ang", kinv], writes=[name + "ang"])
                    for which, T in (("s", St), ("c", Ct)):
                        off = 0.0 if which == "s" else float(np.pi / 2)
                        p.op("dve", lambda e, off=off: e.tensor_scalar(out=kk[:], in0=ang[:], scalar1=off, scalar2=float(1.0 / (2 * np.pi)),
                                                                   op0=ALU.add, op1=ALU.mult), reads=[name + "ang"], writes=[name + "kk"])
                        p.op("dve", lambda e: e.tensor_copy(out=ki[:], in_=kk[:]), reads=[name + "kk"], writes=[name + "ki"])
                        p.op("dve", lambda e: e.tensor_copy(out=kk[:], in_=ki[:]), reads=[name + "ki"], writes=[name + "kk"])
                        p.op("dve", lambda e, T=T: e.scalar_tensor_tensor(out=T, in0=kk[:rows], scalar=-6.28125, in1=ang[:rows],
                                                                       op0=ALU.mult, op1=ALU.add),
                             reads=[name + "kk", name + "ang"], writes=[name + which])
                        p.op("dve", lambda e, T=T, off=off: e.scalar_tensor_tensor(out=T, in0=kk[:rows], scalar=float(-(2 * np.pi - 6.28125)), in1=T,
                                                                                op0=ALU.mult, op1=ALU.add),
                             reads=[name + "kk", name + which], writes=[name + which])
                        if off != 0.0:
                            p.op("dve", lambda e, T=T, off=off: e.tensor_scalar(out=T, in0=T, scalar1=off, scalar2=None, op0=ALU.add),
                                 reads=[name + which], writes=[name + which])
                        p.op("dve", lambda e: e.tensor_scalar(out=kk[:rows], in0=T, scalar1=float(np.pi), scalar2=float(-2 * np.pi),
                                                              op0=ALU.is_gt, op1=ALU.mult), reads=[name + which], writes=[name + "kk"])
                        p.op("dve", lambda e, T=T: e.tensor_tensor(out=T, in0=T, in1=kk[:rows], op=ALU.add),
                             reads=[name + which, name + "kk"], writes=[name + which])
                        p.op("dve", lambda e: e.tensor_scalar(out=kk[:rows], in0=T, scalar1=float(-np.pi), scalar2=float(2 * np.pi),
                                                              op0=ALU.is_lt, op1=ALU.mult), reads=[name + which], writes=[name + "kk"])
                        p.op("dve", lambda e, T=T: e.tensor_tensor(out=T, in0=T, in1=kk[:rows], op=ALU.add),
                             reads=[name + which, name + "kk"], writes=[name + which])
                        p.op("dve", lambda e, T=T: e.tensor_scalar(out=T, in0=T, scalar1=3.14159, scalar2=-3.14159, op0=ALU.min, op1=ALU.max),
                             reads=[name + which], writes=[name + which])
                        p.op("act", lambda e, T=T: e.activation(out=T, in_=T, func=AF.Sin), reads=[name + which], writes=[name + which])

                Ck = sb("Ck", [32, S], F32)
                Sk = sb("Sk", [32, S], F32)
                sa2 = ExitStack()
                with sa2:
                    rope_table(Ck[:], Sk[:], posa, S, 32, "rk", invc, "invc", mk_alloc(sa2)[0])
                    p.barrier()
                Cq = sb("Cq", [96, 2048], F32)
                Sq = sb("Sq", [96, 2048], F32)
                sa3 = ExitStack()
                with sa3:
                    sb3_ = mk_alloc(sa3)[0]
                    invq = sb3_("invq", [128, 1], F32)
                    p.op("pool", lambda e: e.memset(invq[:], 0.0), writes=["invq"])
                    p.op("sp", lambda e: e.dma_start(out=invq[64:96, :], in_=invf[0:32, :]), reads=["invq"], writes=["invq"], dma="invq")
                    rope_table(Cq[:], Sq[:], poso, 2048, 96, "rq", invq, "invq", sb3_)
                    p.barrier()
                    if stop_after == "R":
                        p.finish_wait("sp"); p.emit(top); return nc
                msc = float((64 + 32) ** -0.5)
                p.op("dve", lambda e: e.tensor_scalar(out=Cq[:], in0=Cq[:], scalar1=msc, scalar2=None, op0=ALU.mult),
                     reads=["rqc"], writes=["rqc"])
                p.op("dve", lambda e: e.tensor_scalar(out=Sq[:], in0=Sq[:], scalar1=msc, scalar2=None, op0=ALU.mult),
                     reads=["rqs"], writes=["rqs"])

                xt_r = Ring(sb, "xt", 4, [128, D], F32)
                junk = sb("junkA", [128, D], F32)
                ss_r = Ring(sb, "ssA", 4, [128, 1], F32)
                xn_r = Ring(sb, "xn", 4, [128, D], BF16)
                xnT_r = Ring(sb, "xnT", 2, [128, 8, 512], BF16)
                pT_r = Ring(ps, "pTA", 2, [128, 8, 128], BF16)
                pm_r = Ring(ps, "pmA", 4, [128, 512], F32)
                pss = ps("pssA", [128, 512], F32)
                sq_r = Ring(sb, "sqA", 2, [128, 512], BF16)
                rbc = sb("rbcA", [128, 512], F32)
                cn_r = Ring(sb, "cnA", 2, [128, 3, 512], BF16)
                ev_r = Ring(sb, "evA", 2, [128, 512], BF16)
                stg_r = Ring(sb, "stgA", 3, [128, 4, 512], BF16)
                stg8_r = Ring(sb, "stg8A", 2, [128, 8, 512], BF16)
                evf_r = Ring(sb, "evfA", 2, [128, 512], F32)
                evf2_r = Ring(sb, "evf2A", 2, [128, 512], F32)

                def make_xnT(src, tok0):
                    xnT, kT = xnT_r.next()
                    xs = []
                    for sub in range(4):
                        xt, kx = xt_r.next()
                        ss, ks = ss_r.next()
                        xn, kn = xn_r.next()
                        r0 = tok0 + sub * 128
                        p.op("sp", lambda e: e.dma_start(out=xt[:], in_=src[r0:r0 + 128, :]), writes=[kx], dma=kx)
                        p.op("act", lambda e: e.activation(out=junk[:], in_=xt[:], func=AF.Square, accum_out=ss[:]),
                             reads=[kx], writes=["junkA", ks])
                        rstd_from(ss[:], ss[:], D, [ks], [ks])
                        p.op("dve", lambda e: e.scalar_tensor_tensor(out=xn[:], in0=xt[:], scalar=ss[:, 0:1], in1=gattn[:],
                                                                     op0=ALU.mult, op1=ALU.mult),
                             reads=[kx, ks, "gattn"], writes=[kn])
                        xs.append((xn, kn))
                    for sub in range(4):
                        xn, kn = xs[sub]
                        pT, kp = pT_r.next()
                        for c in range(8):
                            p.op("pe", lambda e: e.transpose(out=pT[:, c, :], in_=xn[:, c * 128:(c + 1) * 128], identity=ident[:]),
                                 reads=[kn, "ident"], writes=[kp])
                        p.op("dve", lambda e: e.tensor_copy(out=xnT[:, :, sub * 128:(sub + 1) * 128], in_=pT[:]),
                             reads=[kp], writes=[kT])
                    return xnT, kT

                def proj_fm(xnT, kT, col0, m, wkey="Win", W=None):
                    W = Win if W is None else W
                    pm, kpm = pm_r.next()
                    for k in range(8):
                        p.op("pe", lambda e, k=k, pm=pm, W=W: e.matmul(out=pm[0:m, :], lhsT=W[:, k, col0:col0 + m], rhs=xnT[:, k, :],
                                                                  start=(k == 0), stop=(k == 7)),
                             reads=[kT, wkey], writes=[kpm])
                    return pm, kpm

                def lowrank_norm(pms, nch, width, gc0):
                    for i, (pm, kpm) in enumerate(pms):
                        sq, ksq = sq_r.next()
                        p.op("act", lambda e, pm=pm, sq=sq: e.activation(out=sq[:], in_=pm[:], func=AF.Square), reads=[kpm], writes=[ksq])
                        p.op("pe", lambda e, sq=sq, i=i: e.matmul(out=pss[:], lhsT=ones[:], rhs=sq[:], start=(i == 0), stop=(i == nch - 1)),
                             reads=[ksq, "ones"], writes=["pssA"])
                    rstd_from(rbc[:], pss[:], width, ["pssA"], ["rbcA"])
                    cn, kcn = cn_r.next()
                    for i, (pm, kpm) in enumerate(pms):
                        p.op("dve", lambda e, pm=pm, i=i, cn=cn: e.scalar_tensor_tensor(out=cn[:, i, :], in0=pm[:], scalar=gcol[:, gc0 + i:gc0 + i + 1],
                                                                                    in1=rbc[:], op0=ALU.mult, op1=ALU.mult),
                             reads=[kpm, "gcol", "rbcA"], writes=[kcn])
                    return cn, kcn

                def store_fm(pm, kpm, rows, dst, eng="dve", scale=None):
                    ev, kev = ev_r.next()
                    if scale is None:
                        if eng == "act":
                            p.op("act", lambda e: e.copy(out=ev[0:rows, :], in_=pm[0:rows, :]), reads=[kpm], writes=[kev])
                        else:
                            p.op("dve", lambda e: e.tensor_copy(out=ev[0:rows, :], in_=pm[0:rows, :]), reads=[kpm], writes=[kev])
                    else:
                        p.op("act", lambda e: e.mul(out=ev[0:rows, :], in_=pm[0:rows, :], mul=scale), reads=[kpm], writes=[kev])
                    p.op("pool", lambda e: e.dma_start(out=dst, in_=ev[0:rows, :]), reads=[kev], writes=[], dma=kev + "s")

                def evac_to(pm, kpm, dst, kdst, eng="dve", scale=None):
                    if scale is not None:
                        p.op("act", lambda e: e.mul(out=dst, in_=pm[:], mul=scale), reads=[kpm], writes=[kdst])
                    elif eng == "act":
                        p.op("act", lambda e: e.copy(out=dst, in_=pm[:]), reads=[kpm], writes=[kdst])
                    else:
                        p.op("dve", lambda e: e.tensor_copy(out=dst, in_=pm[:]), reads=[kpm], writes=[kdst])

                KnT_v = KnT.rearrange("(c p) n -> p c n", p=128)
                KsT_v = KsT.rearrange("(c p) n -> p c n", p=128)
                QsT_v = QsT.rearrange("(c p) n -> p c n", p=128)
                VmD_v = VmD.rearrange("(s p) n -> p s n", p=128)
                VsD_v = VsD.rearrange("(s p) n -> p s n", p=128)
                QmT_v = QmT.rearrange("(h r) n -> r h n", r=96)

                srcs = [(xa, g_ * 512) for g_ in range(8)] + [(xo, g_ * 512) for g_ in range(4)]
                nxt = make_xnT(*srcs[0])
                for G in range(8):
                    c0 = G * 512
                    xnT, kT = nxt
                    nxt = make_xnT(*srcs[G + 1])
                    ckv = [proj_fm(xnT, kT, 384 + m * 128, 128) for m in range(2)]
                    cn, kcn = lowrank_norm(ckv, 2, 256, 3)
                    pa, kpa = proj_fm(xnT, kT, 0, 32, "Wkr", Wkr)
                    pb, kpb = proj_fm(xnT, kT, 32, 32, "Wkr", Wkr)
                    t1, kt1 = evf_r.next()
                    t2, kt2 = evf2_r.next()
                    p.op("dve", lambda e, pa=pa, t1=t1, c0=c0: e.tensor_tensor(out=t1[0:32, :], in0=pa[0:32, :], in1=Ck[:, c0:c0 + 512], op=ALU.mult),
                         reads=[kpa, "rkc"], writes=[kt1])
                    p.op("dve", lambda e, pb=pb, t2=t2, c0=c0: e.tensor_tensor(out=t2[0:32, :], in0=pb[0:32, :], in1=Sk[:, c0:c0 + 512], op=ALU.mult),
                         reads=[kpb, "rks"], writes=[kt2])
                    ev, kev = ev_r.next()
                    p.op("dve", lambda e, t1=t1, t2=t2, ev=ev: e.tensor_tensor(out=ev[0:32, :], in0=t1[0:32, :], in1=t2[0:32, :], op=ALU.add),
                         reads=[kt1, kt2], writes=[kev])
                    p.op("sp", lambda e, ev=ev, c0=c0: e.dma_start(out=KrT[:, c0:c0 + 512], in_=ev[0:32, :]), reads=[kev], writes=[], dma=kev + "s")
                    st_, kst_ = stg_r.next()
                    for m in range(4):
                        pm, kpm = proj_fm(xnT, kT, 1184 + m * 128, 128)
                        evac_to(pm, kpm, st_[:, m, :], kst_, eng="act")
                    p.op("sp", lambda e: e.dma_start(out=KsT_v[:, :, c0:c0 + 512], in_=st_[:]), reads=[kst_], dma=kst_ + "s")
                    st_, kst_ = stg_r.next()
                    for sub in range(4):
                        pm, kpm = pm_r.next()
                        for k in range(8):
                            p.op("pe", lambda e, k=k, pm=pm, sub=sub, xnT=xnT: e.matmul(out=pm[:], lhsT=xnT[:, k, sub * 128:(sub + 1) * 128],
                                                                                   rhs=Win[:, k, 1696:2208], start=(k == 0), stop=(k == 7)),
                                 reads=[kT, "Win"], writes=[kpm])
                        evac_to(pm, kpm, st_[:, sub, :], kst_, eng="dve")
                    p.op("sp", lambda e: e.dma_start(out=VsD_v[:, G * 4:(G + 1) * 4, :], in_=st_[:]), reads=[kst_], dma=kst_ + "s")

                    st_, kst_ = stg_r.next()
                    for hp in range(4):
                        pm, kpm = pm_r.next()
                        for m in range(2):
                            p.op("pe", lambda e, m=m, pm=pm, hp=hp, cn=cn: e.matmul(out=pm[:], lhsT=Wkn[:, m, hp * 128:(hp + 1) * 128], rhs=cn[:, m, :],
                                                                               start=(m == 0), stop=(m == 1)),
                                 reads=[kcn, "Wkn"], writes=[kpm])
                        evac_to(pm, kpm, st_[:, hp, :], kst_, eng="act")
                    p.op("sp", lambda e: e.dma_start(out=KnT_v[:, :, c0:c0 + 512], in_=st_[:]), reads=[kst_], dma=kst_ + "s")
                    st_, kst_ = stg_r.next()
                    for sub in range(4):
                        pm, kpm = pm_r.next()
                        for m in range(2):
                            p.op("pe", lambda e, m=m, pm=pm, sub=sub, cn=cn: e.matmul(out=pm[:], lhsT=cn[:, m, sub * 128:(sub + 1) * 128], rhs=Wv[:, m, :],
                                                                                 start=(m == 0), stop=(m == 1)),
                                 reads=[kcn, "Wv"], writes=[kpm])
                        evac_to(pm, kpm, st_[:, sub, :], kst_, eng="dve")
                    p.op("sp", lambda e: e.dma_start(out=VmD_v[:, G * 4:(G + 1) * 4, :], in_=st_[:]), reads=[kst_], dma=kst_ + "s")
                for G in range(4):
                    c0 = G * 512
                    xnT, kT = nxt
                    if G + 1 < 4:
                        nxt = make_xnT(*srcs[8 + G + 1])
                    cq = [proj_fm(xnT, kT, m * 128, 128) for m in range(3)]
                    cn, kcn = lowrank_norm(cq, 3, 384, 0)
                    st8, kst8 = stg8_r.next()
                    for h in range(8):
                        pa, kpa = pm_r.next()
                        pb, kpb = pm_r.next()
                        for m in range(3):
                            p.op("pe", lambda e, m=m, pa=pa, h=h, cn=cn: e.matmul(out=pa[0:96, :], lhsT=Wuq[:, m, h * 96:(h + 1) * 96], rhs=cn[:, m, :],
                                                                             start=(m == 0), stop=(m == 2)),
                                 reads=[kcn, "Wuq"], writes=[kpa])
                        for m in range(3):
                            p.op("pe", lambda e, m=m, pb=pb, h=h, cn=cn: e.matmul(out=pb[0:96, :], lhsT=Wuqr[:, m, h * 96:(h + 1) * 96], rhs=cn[:, m, :],
                                                                             start=(m == 0), stop=(m == 2)),
                                 reads=[kcn, "Wuqr"], writes=[kpb])
                        t1, kt1 = evf_r.next()
                        t2, kt2 = evf2_r.next()
                        p.op("dve", lambda e, pa=pa, t1=t1, c0=c0: e.tensor_tensor(out=t1[0:96, :], in0=pa[0:96, :], in1=Cq[:, c0:c0 + 512], op=ALU.mult),
                             reads=[kpa, "rqc"], writes=[kt1])
                        p.op("dve", lambda e, pb=pb, t2=t2, c0=c0: e.tensor_tensor(out=t2[0:96, :], in0=pb[0:96, :], in1=Sq[:, c0:c0 + 512], op=ALU.mult),
                             reads=[kpb, "rqs"], writes=[kt2])
                        p.op("pool", lambda e, t1=t1, t2=t2: e.tensor_tensor(out=st8[0:96, h, :], in0=t1[0:96, :], in1=t2[0:96, :], op=ALU.add),
                             reads=[kt1, kt2], writes=[kst8])
                    p.op("sp", lambda e: e.dma_start(out=QmT_v[:, :, c0:c0 + 512], in_=st8[0:96, :, :]), reads=[kst8], dma=kst8 + "s")
                    st_, kst_ = stg_r.next()
                    for m in range(4):
                        pm, kpm = proj_fm(xnT, kT, 672 + m * 128, 128)
                        evac_to(pm, kpm, st_[:, m, :], kst_, scale=0.125)
                    p.op("sp", lambda e: e.dma_start(out=QsT_v[:, :, c0:c0 + 512], in_=st_[:]), reads=[kst_], dma=kst_ + "s")
                p.barrier()
                if stop_after == "A":
                    p.finish_wait("sp"); p.emit(top); return nc

            sbx = ExitStack()
            with sbx:
                sb, ps = mk_alloc(sbx)
                qrb = sb("qrb", [128, 512], F32)
                p.op("sp", lambda e: e.dma_start(out=qrb[:], in_=qrel.partition_broadcast(128)), writes=["qrb"], dma="qrb")
                kidx_i = sb("kidx_i", [128, 1], I32)
                krel = sb("krel", [128, 8], F32)
                p.op("pool", lambda e: e.iota(kidx_i[:], pattern=[[0, 1]], base=0, channel_multiplier=1), writes=["kidx_i"])
                p.op("dve", lambda e: e.tensor_copy(out=krel[:, 0:1], in_=kidx_i[:]), reads=["kidx_i"], writes=["krel"])
                for d in range(1, 8):
                    p.op("dve", lambda e, d=d: e.tensor_scalar(out=krel[:, d:d + 1], in0=krel[:, 0:1], scalar1=float(128 * d), scalar2=None, op0=ALU.add),
                         reads=["krel"], writes=["krel"])
                nmM = sb("nmM", [128, 8, 512], BF16)
                nmS = sb("nmS", [128, 8, 512], BF16)
                m01 = sb("m01", [128, 8, 512], BF16)
                for d in range(8):
                    p.op("dve", lambda e, d=d: e.tensor_scalar(out=nmM[:, d, :], in0=qrb[:], scalar1=krel[:, d:d + 1], scalar2=NEG, op0=ALU.is_lt, op1=ALU.mult),
                         reads=["qrb", "krel"], writes=["nmM"])
                    p.op("dve", lambda e, d=d: e.tensor_scalar(out=nmS[:, d, :], in0=qrb[:], scalar1=krel[:, d:d + 1], scalar2=NEG, op0=ALU.is_le, op1=ALU.mult),
                         reads=["qrb", "krel"], writes=["nmS"])
                    p.op("dve", lambda e, d=d: e.tensor_scalar(out=m01[:, d, :], in0=qrb[:], scalar1=krel[:, d:d + 1], scalar2=None, op0=ALU.is_gt),
                         reads=["qrb", "krel"], writes=["m01"])

                def blocks(t):
                    out = [(kb, 512, None) for kb in range(8 * t)]
                    out += [(8 * t + d, NDIAG[d], d) for d in range(8)]
                    return out

                sm = ExitStack()
                with sm:
                    def mla_gen():
                        sb, ps = mk_alloc(sm)
                        Vall = sb("Vall", [128, 32, 8, 65], BF16)
                        p.op("pool", lambda e: e.memset(Vall[:], 1.0), writes=["Vall"])
                        vsrc = VmD.rearrange("(kb p) (h d) -> p kb h d", p=128, h=8)
                        for hh in range(8):
                            p.op("sp", lambda e: e.dma_start(out=Vall[:, :, hh, 0:64], in_=vsrc[:, :, hh, :]),
                                 reads=["Vall"], writes=["Vall"], dma="Vall")
                        KT_r = Ring(sb, "KTm", 2, [96, S], BF16)
                        QT_r = Ring(sb, "QTm", 2, [96, 2048], BF16)
                        pS_r = Ring(ps, "pSm", 2, [128, 512], F32)
                        pO_r = Ring(ps, "pOm", 1, [128, 512], F32)
                        pB_r = Ring(ps, "pBm", 1, [128, 512], F32)
                        P_r = Ring(sb, "Pm", 3, [128, 512], BF16)
                        Of_r = Ring(sb, "Ofm", 2, [65, 512], F32)
                        On_r = Ring(sb, "Onm", 2, [64, 512], BF16)
                        sel = sb("sel65", [65, 64], F32)
                        p.op("pool", lambda e: e.memset(sel[:], 0.0), writes=["sel65"])
                        p.op("pool", lambda e: e.memset(sel[64:65, :], 1.0), reads=["sel65"], writes=["sel65"])
                        for h in range(8):
                            KT, kKT = KT_r.next()
                            QT, kQT = QT_r.next()
                            p.op("sp", lambda e, KT=KT, h=h: e.dma_start(out=KT[0:64, :], in_=KnT[h * 64:(h + 1) * 64, :]), writes=[kKT], dma=kKT)
                            p.op("sp", lambda e, KT=KT: e.dma_start(out=KT[64:96, :], in_=KrT[:, :]), writes=[kKT], dma=kKT)
                            p.op("sp", lambda e, QT=QT, h=h: e.dma_start(out=QT[:], in_=QmT[h * 96:(h + 1) * 96, :]), writes=[kQT], dma=kQT)
                            for t in range(4):
                                bl = blocks(t)
                                pO, kpO = pO_r.next()
                                nb = len(bl)
                                stage = {}

                                def stA(i):
                                    kb, N, d = bl[i]
                                    pS, kpS = pS_r.next()
                                    p.op("pe", lambda e: e.matmul(out=pS[:, 0:N], lhsT=KT[:, kb * 128:(kb + 1) * 128], rhs=QT[:, t * 512:t * 512 + N],
                                                                  start=True, stop=(d is None)), reads=[kKT, kQT], writes=[kpS])
                                    if d is not None:
                                        p.op("pe", lambda e: e.matmul(out=pS[:, 0:N], lhsT=ident[:], rhs=nmM[:, d, 0:N], start=False, stop=True),
                                             reads=["ident", "nmM"], writes=[kpS])
                                    P, kP = P_r.next()
                                    p.op("act", lambda e: e.activation(out=P[:, 0:N], in_=pS[:, 0:N], func=AF.Exp), reads=[kpS], writes=[kP])
                                    stage[i] = (P, kP)

                                def stB(i):
                                    kb, N, d = bl[i]
                                    P, kP = stage.pop(i)
                                    p.op("pe", lambda e: e.matmul(out=pO[0:65, 0:N], lhsT=Vall[:, kb, h, :], rhs=P[:, 0:N], start=(i == 0), stop=(i == nb - 1)),
                                         reads=["Vall", kP], writes=[kpO])

                                for it in range(nb + 2):
                                    if it < nb:
                                        stA(it)
                                    if it - 2 >= 0:
                                        stB(it - 2)
                                    yield
                                Of, kOf = Of_r.next()
                                p.op("dve", lambda e, Of=Of, pO=pO: e.tensor_copy(out=Of[:], in_=pO[0:65, :]), reads=[kpO], writes=[kOf])
                                p.op("dve", lambda e, Of=Of: e.reciprocal(out=Of[64:65, :], in_=Of[64:65, :]), reads=[kOf], writes=[kOf])
                                pB, kpB = pB_r.next()
                                p.op("pe", lambda e, Of=Of, pB=pB: e.matmul(out=pB[0:64, :], lhsT=sel[:], rhs=Of[:], start=True, stop=True),
                                     reads=[kOf, "sel65"], writes=[kpB])
                                On, kOn = On_r.next()
                                p.op("dve", lambda e, Of=Of, pB=pB, On=On: e.tensor_tensor(out=On[:], in0=Of[0:64, :], in1=pB[0:64, :], op=ALU.mult),
                                     reads=[kOf, kpB], writes=[kOn])
                                p.op("pool", lambda e, On=On, h=h, t=t: e.dma_start(out=OTd[h * 64:(h + 1) * 64, t * 512:(t + 1) * 512], in_=On[:]),
                                     reads=[kOn], writes=[], dma=kOn + "s")

                    def sb_gen():
                        sb, ps = mk_alloc(sm)
                        Vs = sb("Vsall", [128, 32, 512], BF16)
                        vsrc = VsD.rearrange("(kb p) n -> p kb n", p=128)
                        for q4 in range(4):
                            p.op("sp", lambda e, q4=q4: e.dma_start(out=Vs[:, q4 * 8:(q4 + 1) * 8, :], in_=vsrc[:, q4 * 8:(q4 + 1) * 8, :]),
                                 writes=["Vsall"], dma="Vsall")
                        KT_r = Ring(sb, "KTs", 2, [64, S], BF16)
                        QT_r = Ring(sb, "QTs", 2, [64, 2048], BF16)
                        pZ_r = Ring(ps, "pZs", 2, [128, 512], F32)
                        pL_r = Ring(ps, "pLs", 1, [128, 512], F32)
                        pO_r = Ring(ps, "pOs", 1, [128, 512], F32)
                        E_r = Ring(sb, "Es", 3, [128, 512], F32)
                        SP_r = Ring(sb, "SPs", 4, [128, 512], BF16)
                        SM_r = Ring(sb, "SMs", 4, [128, 512], BF16)
                        A_r = Ring(sb, "As", 3, [128, 512], BF16)
                        CR_r = Ring(sb, "CRs", 3, [128, 512], BF16)
                        On_r = Ring(sb, "Ons", 2, [64, 512], BF16)
                        for h in range(8):
                            KT, kKT = KT_r.next()
                            QT, kQT = QT_r.next()
                            p.op("sp", lambda e, KT=KT, h=h: e.dma_start(out=KT[:], in_=KsT[h * 64:(h + 1) * 64, :]), writes=[kKT], dma=kKT)
                            p.op("sp", lambda e, QT=QT, h=h: e.dma_start(out=QT[:], in_=QsT[h * 64:(h + 1) * 64, :]), writes=[kQT], dma=kQT)
                            for t in range(4):
                                bl = blocks(t)[::-1]
                                nb = len(bl)
                                pO, kpO = pO_r.next()
                                p.op("pe", lambda e, pO=pO: e.matmul(out=pO[0:64, :], lhsT=zeros[:, 0:64], rhs=m01[:, 0, :],
                                                                      start=True, stop=False), reads=["zeros", "m01"], writes=[kpO])
                                stage = {}
                                carry = {"t": None, "k": None}

                                def stA(i):
                                    kb, N, d = bl[i]
                                    pZ, kpZ = pZ_r.next()
                                    p.op("pe", lambda e: e.matmul(out=pZ[:, 0:N], lhsT=KT[:, kb * 128:(kb + 1) * 128], rhs=QT[:, t * 512:t * 512 + N],
                                                                  start=True, stop=True), reads=[kKT, kQT], writes=[kpZ])
                                    E, kE = E_r.next()
                                    p.op("act", lambda e: e.activation(out=E[:, 0:N], in_=pZ[:, 0:N], func=AF.Exp), reads=[kpZ], writes=[kE])
                                    SPt, kSP = SP_r.next()
                                    p.op("act", lambda e: e.activation(out=SPt[:, 0:N], in_=E[:, 0:N], func=AF.Ln, bias=1.0), reads=[kE], writes=[kSP])
                                    if d is not None:
                                        SM, kSM = SM_r.next()
                                        p.op("dve", lambda e: e.tensor_tensor(out=SM[:, 0:N], in0=SPt[:, 0:N], in1=m01[:, d, 0:N], op=ALU.mult),
                                             reads=[kSP, "m01"], writes=[kSM])
                                    else:
                                        SM, kSM = SPt, kSP
                                    cprev, kcprev = carry["t"], carry["k"]
                                    stage[i] = (SM, kSM, cprev, kcprev, E, kE)
                                    if i < nb - 1:
                                        cn_, kcn_ = CR_r.next()
                                        if cprev is None:
                                            if N < 512:
                                                p.op("pool", lambda e: e.memset(cn_[:, N:512], 0.0), writes=[kcn_])
                                            p.op("pool", lambda e: e.tensor_copy(out=cn_[:, 0:N], in_=SM[:, 0:N]), reads=[kSM], writes=[kcn_])
                                        else:
                                            if N < 512:
                                                p.op("pool", lambda e: e.tensor_copy(out=cn_[:, N:512], in_=cprev[:, N:512]), reads=[kcprev], writes=[kcn_])
                                            p.op("dve", lambda e: e.tensor_tensor(out=cn_[:, 0:N], in0=cprev[:, 0:N], in1=SM[:, 0:N], op=ALU.add),
                                                 reads=[kcprev, kSM], writes=[kcn_])
                                        carry["t"], carry["k"] = cn_, kcn_

                                def stB(i):
                                    kb, N, d = bl[i]
                                    SM, kSM, cprev, kcprev, E, kE = stage[i]
                                    pL, kpL = pL_r.next()
                                    last = "tri"
                                    if cprev is not None:
                                        last = "carry"
                                    if d is not None:
                                        last = "mask"
                                    p.op("pe", lambda e: e.matmul(out=pL[:, 0:N], lhsT=ntri[:], rhs=SM[:, 0:N], start=True, stop=(last == "tri")),
                                         reads=["ntri", kSM], writes=[kpL])
                                    if cprev is not None:
                                        p.op("pe", lambda e: e.matmul(out=pL[:, 0:N], lhsT=nones[:], rhs=cprev[:, 0:N], start=False, stop=(last == "carry")),
                                             reads=["nones", kcprev], writes=[kpL])
                                    if d is not None:
                                        p.op("pe", lambda e: e.matmul(out=pL[:, 0:N], lhsT=ident[:], rhs=nmS[:, d, 0:N], start=False, stop=True),
                                             reads=["ident", "nmS"], writes=[kpL])
                                    A, kA = A_r.next()
                                    p.op("act", lambda e: e.activation(out=A[:, 0:N], in_=pL[:, 0:N], func=AF.Exp), reads=[kpL], writes=[kA])
                                    p.op("dve", lambda e: e.tensor_tensor(out=A[:, 0:N], in0=A[:, 0:N], in1=E[:, 0:N], op=ALU.mult), reads=[kA, kE], writes=[kA])
                                    stage[i] = (A, kA)

                                def stC(i):
                                    kb, N, d = bl[i]
                                    A, kA = stage.pop(i)
                                    p.op("pe", lambda e: e.matmul(out=pO[0:64, 0:N], lhsT=Vs[:, kb, h * 64:(h + 1) * 64], rhs=A[:, 0:N], start=False, stop=(i == nb - 1)),
                                         reads=["Vsall", kA], writes=[kpO])

                                for it in range(nb + 2):
                                    if it < nb:
                                        stA(it)
                                    if 0 <= it - 1 < nb:
                                        stB(it - 1)
                                    if it - 2 >= 0:
                                        stC(it - 2)
                                    yield
                                On, kOn = On_r.next()
                                p.op("dve", lambda e, On=On, pO=pO: e.tensor_copy(out=On[:], in_=pO[0:64, :]), reads=[kpO], writes=[kOn])
                                p.op("pool", lambda e, On=On, h=h, t=t: e.dma_start(out=OTd[512 + h * 64:512 + (h + 1) * 64, t * 512:(t + 1) * 512], in_=On[:]),
                                     reads=[kOn], writes=[], dma=kOn + "s")

                    gens = [mla_gen(), sb_gen()]
                    while gens:
                        for g_ in list(gens):
                            try:
                                next(g_)
                            except StopIteration:
                                gens.remove(g_)
                    p.barrier()
                    if stop_after == "B":
                        p.finish_wait("sp"); p.emit(top); return nc
            sc = ExitStack()
            with sc:
                sb, ps = mk_alloc(sc)
                H = sb("H", [128, 16, D], F32)
                posm = sb("posm", [128, 16, NE], F32)
                maskb = sb("maskb", [128, 16, NE], BF16)
                gj = sb("gj", [128, 16, 4], F32)
                slI = sb("slI", [128, 16, 4], I32)
                Gt = sb("Gt", [128, 16, NE], F32)
                bgT = sb("bgT", [128, 512], F32)
                s1 = ExitStack()
                with s1:
                    sb1, ps1 = mk_alloc(s1)
                    gbc = sb1("gbc", [128, D], F32)
                    junk = sb1("junkC", [128, D], BF16)
                    Wo = sb1("Wo", [128, 8, D], BF16)
                    p.op("pool", lambda e: e.dma_start(out=Wo[:], in_=w_o.rearrange("(c p) n -> p c n", p=128)), writes=["Wo"], dma="Wo")
                    Wr = sb1("Wr", [128, 8, NE], F32)
                    p.op("sp", lambda e: e.dma_start(out=Wr[:], in_=w_router.rearrange("(c p) n -> p c n", p=128)), writes=["Wr"], dma="Wr")
                    brb = sb1("brb", [128, NE], F32)
                    p.op("sp", lambda e: e.dma_start(out=brb[:], in_=b_router.partition_broadcast(128)), writes=["brb"], dma="brb")
                    p.op("sp", lambda e: e.dma_start(out=gbc[:], in_=g_moe.partition_broadcast(128)), writes=["gbc"], dma="gbc")
                    bgl = sb1("bgl", [128, 4, 128], F32)
                    p.op("sp", lambda e: e.dma_start(out=bgl[:], in_=b_gu.rearrange("(a r) q -> r a q", r=128)), writes=["bgl"], dma="bgl")
                    pTf_r = Ring(ps1, "pTf", 1, [128, 4, 128], F32)
                    pbt, kpbt = pTf_r.next()
                    for a in range(4):
                        p.op("pe", lambda e, a=a: e.transpose(out=pbt[:, a, :], in_=bgl[:, a, :], identity=identf[:]), reads=["bgl", "identf"], writes=[kpbt])
                    p.op("dve", lambda e: e.tensor_copy(out=bgT[:].rearrange("p (a q) -> p a q", a=4), in_=pbt[:]), reads=[kpbt], writes=["bgT"])

                    OT_r = Ring(sb1, "OTl", 1, [128, 8, 512], BF16)
                    sq_r = Ring(sb1, "sqC", 2, [128, 512], BF16)
                    pss_r = Ring(ps1, "pssC", 1, [128, 512], F32)
                    rb_r = Ring(sb1, "rbC", 2, [128, 512], F32)
                    MX = sb1("MX", [128, 8, 512], BF16)
                    pA_r = Ring(ps1, "pAC", 2, [128, 512], F32)
                    xr_r = Ring(sb1, "xrC", 2, [128, D], F32)
                    ssc_r = Ring(sb1, "sscC", 2, [128, 1], F32)
                    u32_r = Ring(sb1, "u32C", 2, [128, D], F32)
                    uhT_r = Ring(sb1, "uhT", 2, [128, 8, 128], BF16)
                    tris = sb1("tris", [128, 128], BF16)
                    tmpf1 = sb1("tmpf1", [128, 128], F32)
                    p.op("pool", lambda e: e.memset(tmpf1[:], 1.0), writes=["tmpf1"])
                    p.op("pool", lambda e: e.affine_select(out=tmpf1[:], in_=tmpf1[:], pattern=[[1, 128]], compare_op=ALU.is_gt, fill=0.0,
                                                            base=0, channel_multiplier=-1), reads=["tmpf1"], writes=["tmpf1"])
                    p.op("dve", lambda e: e.tensor_copy(out=tris[:], in_=tmpf1[:]), reads=["tmpf1"], writes=["tris"])
                    gtmp_r = Ring(sb1, "gtmpC", 2, [128, NE], F32)
                    xnb_r = Ring(sb1, "xnbC", 2, [128, D], BF16)
                    ecap_i = sb1("ecap_i", [128, NE], I32)
                    ecap = sb1("ecap", [128, NE], F32)
                    p.op("pool", lambda e: e.iota(ecap_i[:], pattern=[[CAP, NE]], base=0, channel_multiplier=0), writes=["ecap_i"])
                    p.op("dve", lambda e: e.tensor_copy(out=ecap[:], in_=ecap_i[:]), reads=["ecap_i"], writes=["ecap"])
                    pq_r = Ring(sb1, "pq", 2, [128, NE], F32)
                    oh = sb1("oh", [128, NE], F32)
                    pr = sb1("pr", [128, NE], F32)
                    slf_r = Ring(sb1, "slf", 2, [128, 4], F32)
                    ulo_r = Ring(sb1, "uloC", 2, [128, D], BF16)
                    uloT_r = Ring(sb1, "uloT", 2, [128, 8, 128], BF16)
                    pTb_r = Ring(ps1, "pTb", 2, [128, 8, 128], BF16)
                    Wrh = sb1("Wrh", [128, 8, NE], BF16)
                    Wrl = sb1("Wrl", [128, 8, NE], BF16)
                    p.op("dve", lambda e: e.tensor_copy(out=Wrh[:], in_=Wr[:]), reads=["Wr"], writes=["Wrh"])
                    p.op("dve", lambda e: e.tensor_tensor(out=Wrl[:], in0=Wr[:], in1=Wrh[:], op=ALU.subtract), reads=["Wr", "Wrh"], writes=["Wrl"])
                    pLg_r = Ring(ps1, "pLg", 2, [128, 2, NE], F32)
                    lg_r = Ring(sb1, "lgC", 2, [128, NE], F32)
                    mx8_r = Ring(sb1, "mx8C", 2, [128, 8], F32)
                    msk_r = Ring(sb1, "mskC", 2, [128, NE], F32)
                    ex_r = Ring(sb1, "exC", 2, [128, NE], F32)
                    sm_r = Ring(sb1, "smC", 2, [128, 1], F32)
                    otv = OTd.rearrange("(c p) n -> p c n", p=128)
                    if stop_after == "C1a":
                        p.barrier()
                        p.op("sp", lambda e: e.dma_start(out=dbgH.rearrange("(t p) n -> p t n", p=128), in_=H[:]), reads=["H%d" % i_ for i_ in range(16)], dma="dbgH")
                        p.op("sp", lambda e: e.dma_start(out=dbgG.rearrange("(t p) n -> p t n", p=128), in_=Gt[:]), reads=["Gt%d" % i_ for i_ in range(16)], dma="dbgG")
                        p.finish_wait("sp"); p.emit(top); return nc
                    for G in range(4):
                        OT, kOT = OT_r.next()
                        p.op("sp", lambda e, OT=OT, G=G: e.dma_start(out=OT[:], in_=otv[:, :, G * 512:(G + 1) * 512]), writes=[kOT], dma=kOT)
                        rbs = []
                        for grp in range(2):
                            pss, kpss = pss_r.next()
                            for i in range(4):
                                c = grp * 4 + i
                                sq, ksq = sq_r.next()
                                p.op("act", lambda e, sq=sq, OT=OT, c=c: e.activation(out=sq[:], in_=OT[:, c, :], func=AF.Square), reads=[kOT], writes=[ksq])
                                p.op("pe", lambda e, sq=sq, pss=pss, i=i: e.matmul(out=pss[:], lhsT=ones[:], rhs=sq[:], start=(i == 0), stop=(i == 3)),
                                     reads=[ksq, "ones"], writes=[kpss])
                            rb, krb = rb_r.next()
                            rstd_from(rb[:], pss[:], 512, [kpss], [krb])
                            rbs.append((rb, krb))
                        for c in range(8):
                            rb, krb = rbs[c // 4]
                            p.op("dve", lambda e, c=c, rb=rb, OT=OT: e.scalar_tensor_tensor(out=MX[:, c, :], in0=OT[:, c, :], scalar=gcol[:, 5 + c:6 + c], in1=rb[:],
                                                                                        op0=ALU.mult, op1=ALU.mult),
                                 reads=[kOT, "gcol", krb], writes=["MX"])
                        def c1_tile(G, sub):
                            tile = G * 4 + sub
                            b_ = sub % 2
                            uhT, kuhT = uhT_r.tiles[b_], uhT_r.keys[b_]
                            uloT, kuloT = uloT_r.tiles[b_], uloT_r.keys[b_]
                            pq, kpq = pq_r.tiles[b_], pq_r.keys[b_]
                            slf, kslf = slf_r.tiles[b_], slf_r.keys[b_]
                            pLg2, kpLg = pLg_r.tiles[b_], pLg_r.keys[b_]
                            pLg = pLg2[:, 0, :]
                            pPos = pLg2[:, 1, :]
                            yield
                            xr, kxr = xr_r.next()
                            yield
                            p.op("sp", lambda e, xr=xr, tile=tile: e.dma_start(out=xr[:], in_=xo[tile * 128:(tile + 1) * 128, :]), writes=[kxr], dma=kxr)
                            yield
                            for half in range(2):
                                pA, kpA = pA_r.next()
                                for c in range(8):
                                    p.op("pe", lambda e, c=c, pA=pA, sub=sub, half=half: e.matmul(out=pA[:], lhsT=MX[:, c, sub * 128:(sub + 1) * 128],
                                                                                              rhs=Wo[:, c, half * 512:(half + 1) * 512], start=(c == 0), stop=(c == 7)),
                                         reads=["MX", "Wo"], writes=[kpA])
                                p.op("dve", lambda e, pA=pA, xr=xr, tile=tile, half=half: e.tensor_tensor(out=H[:, tile, half * 512:(half + 1) * 512], in0=pA[:],
                                                                                                      in1=xr[:, half * 512:(half + 1) * 512], op=ALU.add),
                                     reads=[kpA, kxr], writes=["H%d" % tile])
                            yield
                            yield
                            ssc, kss = ssc_r.next()
                            yield
                            p.op("act", lambda e, ssc=ssc, tile=tile: e.activation(out=junk[:], in_=H[:, tile, :], func=AF.Square, accum_out=ssc[:]),
                                 reads=["H%d" % tile], writes=["junkC", kss])
                            yield
                            rstd_from(ssc[:], ssc[:], D, [kss], [kss])
                            yield
                            u32, ku = u32_r.next()
                            yield
                            p.op("dve", lambda e, ssc=ssc, u32=u32, tile=tile: e.scalar_tensor_tensor(out=u32[:], in0=H[:, tile, :], scalar=ssc[:, 0:1], in1=gbc[:],
                                                                                                  op0=ALU.mult, op1=ALU.mult),
                                 reads=["H%d" % tile, kss, "gbc"], writes=[ku])
                            yield
                            xnb, kuhi = xnb_r.next()
                            yield
                            ulo, kulo = ulo_r.next()
                            yield
                            p.op("act", lambda e: e.copy(out=xnb[:], in_=u32[:]), reads=[ku], writes=[kuhi])
                            yield
                            p.op("pool", lambda e: e.dma_start(out=Ud[tile * 128:(tile + 1) * 128, :], in_=xnb[:]), reads=[kuhi], dma=kuhi + "s")
                            yield
                            p.op("dve", lambda e: e.tensor_tensor(out=ulo[:], in0=u32[:], in1=xnb[:], op=ALU.subtract), reads=[ku, kuhi], writes=[kulo])
                            yield
                            pT, kpT = pTb_r.next()
                            yield
                            for c in range(8):
                                p.op("pe", lambda e: e.transpose(out=pT[:, c, :], in_=xnb[:, c * 128:(c + 1) * 128], identity=ident[:]), reads=[kuhi, "ident"], writes=[kpT])
                            yield
                            p.op("dve", lambda e: e.tensor_copy(out=uhT[:], in_=pT[:]), reads=[kpT], writes=[kuhT])
                            yield
                            pT2, kpT2 = pTb_r.next()
                            yield
                            for c in range(8):
                                p.op("pe", lambda e: e.transpose(out=pT2[:, c, :], in_=ulo[:, c * 128:(c + 1) * 128], identity=ident[:]), reads=[kulo, "ident"], writes=[kpT2])
                            yield
                            p.op("act", lambda e: e.copy(out=uloT[:], in_=pT2[:]), reads=[kpT2], writes=[kuloT])
                            yield
                            n_ = 0
                            yield
                            for (A_, kA_, W_, kW_) in (("hi", kuhT, Wrh, "Wrh"), ("lo", kuloT, Wrh, "Wrh"), ("hi", kuhT, Wrl, "Wrl")):
                                for k in range(8):
                                    lh = uhT[:, k, :] if A_ == "hi" else uloT[:, k, :]
                                    p.op("pe", lambda e: e.matmul(out=pLg, lhsT=lh, rhs=W_[:, k, :], start=(n_ == 0), stop=(n_ == 23)),
                                         reads=[kA_, kW_], writes=[kpLg])
                                    n_ += 1
                            yield
                            lg, klg = lg_r.next()
                            yield
                            p.op("dve", lambda e, lg=lg: e.tensor_tensor(out=lg[:], in0=pLg, in1=brb[:], op=ALU.add), reads=[kpLg, "brb"], writes=[klg])
                            yield
                            mx8, kmx = mx8_r.next()
                            yield
                            p.op("dve", lambda e, lg=lg, mx8=mx8: e.max(out=mx8[:], in_=lg[:]), reads=[klg], writes=[kmx])
                            yield
                            msk, kmsk = msk_r.next()
                            yield
                            p.op("dve", lambda e, lg=lg, mx8=mx8, msk=msk: e.tensor_scalar(out=msk[:], in0=lg[:], scalar1=mx8[:, 3:4], scalar2=None, op0=ALU.is_ge),
                                 reads=[klg, kmx], writes=[kmsk])
                            yield
                            p.op("dve", lambda e, mx8=mx8: e.tensor_scalar(out=mx8[:, 7:8], in0=mx8[:, 0:1], scalar1=-1.0, scalar2=None, op0=ALU.mult),
                                 reads=[kmx, kmsk], writes=[kmx])
                            yield
                            ex, kex = ex_r.next()
                            yield
                            p.op("act", lambda e, lg=lg, mx8=mx8, ex=ex: e.activation(out=ex[:], in_=lg[:], func=AF.Exp, bias=mx8[:, 7:8]), reads=[klg, kmx], writes=[kex])
                            yield
                            sm_, ksm = sm_r.next()
                            yield
                            p.op("dve", lambda e, ex=ex, msk=msk: e.tensor_tensor(out=ex[:], in0=ex[:], in1=msk[:], op=ALU.mult), reads=[kex, kmsk], writes=[kex])
                            yield
                            p.op("dve", lambda e, ex=ex, sm_=sm_: e.reduce_sum(out=sm_[:], in_=ex[:], axis=mybir.AxisListType.X), reads=[kex], writes=[ksm])
                            yield
                            p.op("dve", lambda e, sm_=sm_: e.reciprocal(out=sm_[:], in_=sm_[:]), reads=[ksm], writes=[ksm])
                            yield
                            p.op("dve", lambda e, ex=ex, sm_=sm_, tile=tile: e.tensor_scalar(out=Gt[:, tile, :], in0=ex[:], scalar1=sm_[:, 0:1], scalar2=None, op0=ALU.mult),
                                 reads=[kex, ksm], writes=["Gt%d" % tile])
                            yield
                            p.op("dve", lambda e: e.tensor_copy(out=maskb[:, tile, :], in_=msk[:]), reads=[kmsk], writes=["maskb%d" % tile])
                            yield
                            gtmp, kgtmp = gtmp_r.next()
                            yield
                            p.op("pe", lambda e: e.matmul(out=pPos, lhsT=tris[:], rhs=maskb[:, tile, :], start=True, stop=(tile == 0)),
                                 reads=["tris", "maskb%d" % tile], writes=[kpLg])
                            yield
                            for j_ in range(tile):
                                p.op("pe", lambda e: e.matmul(out=pPos, lhsT=ones[:], rhs=maskb[:, j_, :], start=False, stop=(j_ == tile - 1)),
                                     reads=["ones", "maskb%d" % j_], writes=[kpLg])
                            yield
                            p.op("dve", lambda e: e.scalar_tensor_tensor(out=gtmp[:], in0=pPos, scalar=1.0, in1=msk[:], op0=ALU.add, op1=ALU.mult),
                                 reads=[kpLg, kmsk, kgtmp], writes=[kgtmp])
                            yield
                            p.op("dve", lambda e: e.tensor_scalar(out=posm[:, tile, :], in0=gtmp[:], scalar1=-1.0, scalar2=None, op0=ALU.add),
                                 reads=[kgtmp], writes=["posm%d" % tile])
                            yield
                            p.op("dve", lambda e: e.scalar_tensor_tensor(out=pq[:], in0=pPos, scalar=float(CAP - 1), in1=ecap[:], op0=ALU.min, op1=ALU.add),
                                 reads=[kpLg, "ecap"], writes=[kpq])
                            yield
                            yield
                            p.op("dve", lambda e: e.scalar_tensor_tensor(out=gtmp[:], in0=pPos, scalar=float(CAP) - 0.5, in1=Gt[:, tile, :], op0=ALU.is_lt, op1=ALU.mult),
                                 reads=[kpLg, "Gt%d" % tile, kgtmp], writes=[kgtmp])
                            yield
                            for j_ in range(4):
                                p.op("dve", lambda e: e.tensor_scalar(out=oh[:], in0=lg[:], scalar1=mx8[:, j_:j_ + 1], scalar2=None, op0=ALU.is_equal),
                                     reads=[klg, kmx], writes=["oh"])
                                p.op("dve", lambda e: e.tensor_tensor(out=pr[:], in0=oh[:], in1=pq[:], op=ALU.mult), reads=["oh", kpq], writes=["pr"])
                                p.op("dve", lambda e: e.reduce_sum(out=slf[:, j_:j_ + 1], in_=pr[:], axis=mybir.AxisListType.X), reads=["pr"], writes=[kslf])
                                p.op("dve", lambda e: e.tensor_tensor(out=pr[:], in0=oh[:], in1=gtmp[:], op=ALU.mult), reads=["oh", kgtmp, "pr"], writes=["pr"])
                                p.op("dve", lambda e: e.reduce_sum(out=gj[:, tile, j_:j_ + 1], in_=pr[:], axis=mybir.AxisListType.X), reads=["pr"], writes=["gj%d" % tile])
                            yield
                            p.op("dve", lambda e: e.tensor_copy(out=slI[:, tile, :], in_=slf[:]), reads=[kslf], writes=["slI%d" % tile])

                        for pair in range(2):
                            gens = [c1_tile(G, pair * 2), c1_tile(G, pair * 2 + 1)]
                            while gens:
                                for g_ in list(gens):
                                    try:
                                        next(g_)
                                    except StopIteration:
                                        gens.remove(g_)
                    p.barrier()
                    if stop_after == "C1":
                        if debug:
                            p.op("sp", lambda e: e.dma_start(out=dbgS.rearrange("(t p) n -> p t n", p=128), in_=slI[:]), reads=["slI%d" % i_ for i_ in range(16)], dma="dbgS")
                            p.op("sp", lambda e: e.dma_start(out=dbgJ.rearrange("(t p) n -> p t n", p=128), in_=gj[:]), reads=["gj%d" % i_ for i_ in range(16)], dma="dbgJ")
                            p.op("sp", lambda e: e.dma_start(out=dbgH.rearrange("(t p) n -> p t n", p=128), in_=H[:]), reads=["H%d" % i_ for i_ in range(16)], dma="dbgH")
                            p.op("sp", lambda e: e.dma_start(out=dbgG.rearrange("(t p) n -> p t n", p=128), in_=Gt[:]), reads=["Gt%d" % i_ for i_ in range(16)], dma="dbgG")
                        p.finish_wait("sp"); p.emit(top); return nc
                s2 = ExitStack()
                with s2:
                    sb2, ps2 = mk_alloc(s2)
                    W_r = Ring(sb2, "Wx", 6, [128, 8, 512], BF16)
                    bd_r = Ring(sb2, "bdn", 2, [1, D], BF16)
                    pGL_r = Ring(ps2, "pGL", 4, [128, 512], F32)
                    pA_r = Ring(ps2, "pA", 2, [128, 512], F32)
                    pTs = ps2("pTs", [128, 8, 128], BF16)
                    ptk = ps2("ptk", [128, 8], F32)
                    gl_r = Ring(sb2, "gl", 1, [128, CAP], F32)
                    sg_r = Ring(sb2, "sg", 1, [128, CAP], F32)
                    Sel = sb2("Sel", [128, 16, CAP], BF16)
                    Xe_r = Ring(sb2, "Xe", 2, [128, NS, D], BF16)
                    XeT = sb2("XeT", [128, 8, CAP], BF16)
                    aT = sb2("aTs", [128, 8, CAP], BF16)
                    Yst_r = Ring(sb2, "Yst", 2, [128, D], F32)
                    tks = sb2("tks", [128, 8], F32)
                    tkf = sb2("tkf", [128, 4], F32)
                    tkI_r = Ring(sb2, "tkI", 2, [128, 4], I32)
                    iota_i = sb2("iota_i", [128, CAP], I32)
                    iota_f = sb2("iota_f", [128, CAP], F32)
                    p.op("pool", lambda e: e.iota(iota_i[:], pattern=[[1, CAP]], base=0, channel_multiplier=0), writes=["iota_i"])
                    p.op("dve", lambda e: e.tensor_copy(out=iota_f[:], in_=iota_i[:]), reads=["iota_i"], writes=["iota_f"])
                    tid = sb2("tid", [128, 16], I32)
                    tidx = sb2("tidx", [128, 16], I32)
                    tidhl = sb2("tidhl", [128, 16, 2], BF16)
                    p.op("pool", lambda e: e.iota(tid[:], pattern=[[128, 16]], base=0, channel_multiplier=1), writes=["tid"])
                    p.op("dve", lambda e: e.tensor_scalar(out=tidx[:], in0=tid[:], scalar1=6, scalar2=None, op0=ALU.arith_shift_right), reads=["tid"], writes=["tidx"])
                    p.op("dve", lambda e: e.tensor_copy(out=tidhl[:, :, 0], in_=tidx[:]), reads=["tidx"], writes=["tidhl"])
                    p.op("dve", lambda e: e.tensor_scalar(out=tidx[:], in0=tid[:], scalar1=63, scalar2=None, op0=ALU.bitwise_and), reads=["tid", "tidx", "tidhl"], writes=["tidx"])
                    p.op("dve", lambda e: e.tensor_copy(out=tidhl[:, :, 1], in_=tidx[:]), reads=["tidx", "tidhl"], writes=["tidhl"])
                    bg3 = bgT[:].rearrange("p (e c) -> p e c", c=16)
                    p.op("dve", lambda e: e.tensor_scalar(out=bg3[:, :, 8:16], in0=bg3[:, :, 8:16], scalar1=1.0, scalar2=None, op0=ALU.add),
                         reads=["bgT"], writes=["bgT"])

                    def load_w(src, slot):
                        Wt, kW = W_r.tiles[slot], W_r.keys[slot]
                        p.op("pool", lambda e: e.dma_start(out=Wt[:], in_=src.rearrange("(c p) n -> p c n", p=128)), writes=[kW], dma=kW)
                        return Wt, kW

                    def load_bd(ex_):
                        bd, kbd = bd_r.next()
                        p.op("pool", lambda e: e.dma_start(out=bd[:], in_=b_dn[ex_:ex_ + 1, :]), writes=[kbd], dma=kbd)
                        return bd, kbd

                    xe_of = {}

                    def sel_build(ex_, tiles):
                        for tile in tiles:
                            p.op("dve", lambda e: e.tensor_scalar(out=Sel[:, tile, :], in0=iota_f[:], scalar1=posm[:, tile, ex_:ex_ + 1], scalar2=None, op0=ALU.is_equal),
                                 reads=["iota_f", "posm%d" % tile], writes=["Sel%d" % tile])

                    def dispatch(ex_, build=True):
                        if build:
                            sel_build(ex_, range(16))
                        for s_ in range(NS):
                            for tile in range(16):
                                p.op("pe", lambda e: e.matmul(out=ptk[:, 2 * s_:2 * s_ + 2], lhsT=Sel[:, tile, s_ * 128:(s_ + 1) * 128], rhs=tidhl[:, tile, :],
                                                              start=(tile == 0), stop=(tile == 15)), reads=["Sel%d" % tile, "tidhl"], writes=["ptk"])
                        p.op("dve", lambda e: e.tensor_copy(out=tks[:, 0:2 * NS], in_=ptk[:, 0:2 * NS]), reads=["ptk"], writes=["tks"])
                        tk3 = tks[:, 0:2 * NS].rearrange("p (s t) -> p s t", t=2)
                        p.op("dve", lambda e: e.scalar_tensor_tensor(out=tkf[:, 0:NS], in0=tk3[:, :, 0], scalar=64.0, in1=tk3[:, :, 1], op0=ALU.mult, op1=ALU.add),
                             reads=["tks"], writes=["tkf"])
                        tkI, ktkI = tkI_r.next()
                        p.op("dve", lambda e: e.tensor_copy(out=tkI[:, 0:NS], in_=tkf[:, 0:NS]), reads=["tkf"], writes=[ktkI])
                        Xe, kXe = Xe_r.next()
                        for s_ in range(NS):
                            p.op("pool", lambda e: e.indirect_dma_start(out=Xe[:, s_, :], out_offset=None, in_=Ud[:, :],
                                                                         in_offset=bass.IndirectOffsetOnAxis(ap=tkI[:, s_:s_ + 1], axis=0)),
                                 reads=[ktkI], writes=[kXe], dma=kXe)
                        xe_of[ex_] = (Xe, kXe)

                    def transposes_s(ex_, s_):
                        Xe, kXe = xe_of[ex_]
                        for k in range(8):
                            p.op("pe", lambda e: e.transpose(out=pTs[:, k, :], in_=Xe[:, s_, k * 128:(k + 1) * 128], identity=ident[:]),
                                 reads=[kXe, "ident"], writes=["pTs"])
                        if s_ % 2 == 0:
                            p.op("act", lambda e: e.copy(out=XeT[:, :, s_ * 128:(s_ + 1) * 128], in_=pTs[:]), reads=["pTs"], writes=["XeT"])
                        else:
                            p.op("dve", lambda e: e.tensor_copy(out=XeT[:, :, s_ * 128:(s_ + 1) * 128], in_=pTs[:]), reads=["pTs"], writes=["XeT"])
                        if s_ == NS - 1:
                            xe_of.pop(ex_)

                    def transposes(ex_):
                        for s_ in range(NS):
                            transposes_s(ex_, s_)

                    def gu_stage(ex_, st, Wg, kWg, Wl, kWl, sel_for=None):
                        for mc in range(4):
                            if sel_for is not None:
                                sel_build(sel_for, range(mc * 4, mc * 4 + 4))
                            c = st * 4 + mc
                            pG, kpG = pGL_r.next()
                            pLn, kpLn = pGL_r.next()
                            for k in range(8):
                                p.op("pe", lambda e: e.matmul(out=pG[:, 0:CAP], lhsT=Wg[:, k, mc * 128:(mc + 1) * 128], rhs=XeT[:, k, :],
                                                              start=(k == 0), stop=(k == 7)), reads=[kWg, "XeT"], writes=[kpG])
                            for k in range(8):
                                p.op("pe", lambda e: e.matmul(out=pLn[:, 0:CAP], lhsT=Wl[:, k, mc * 128:(mc + 1) * 128], rhs=XeT[:, k, :],
                                                              start=(k == 0), stop=(k == 7)), reads=[kWl, "XeT"], writes=[kpLn])
                            gl, kgl = gl_r.next()
                            sg, ksg = sg_r.next()
                            bgc = ex_ * 16 + c
                            blc = ex_ * 16 + 8 + c
                            kaT = "aT_%d" % st
                            p.op("dve", lambda e: e.tensor_scalar(out=gl[:], in0=pG[:, 0:CAP], scalar1=bgT[:, bgc:bgc + 1], scalar2=7.0, op0=ALU.add, op1=ALU.min),
                                 reads=[kpG, "bgT"], writes=[kgl])
                            p.op("act", lambda e: e.activation(out=sg[:], in_=gl[:], func=AF.Sigmoid, scale=1.702), reads=[kgl], writes=[ksg])
                            p.op("dve", lambda e: e.tensor_tensor(out=sg[:], in0=sg[:], in1=gl[:], op=ALU.mult), reads=[kgl, ksg], writes=[ksg])
                            p.op("dve", lambda e: e.tensor_scalar(out=gl[:], in0=pLn[:, 0:CAP], scalar1=bgT[:, blc:blc + 1], scalar2=-6.0, op0=ALU.add, op1=ALU.max),
                                 reads=[kpLn, "bgT", kgl], writes=[kgl])
                            p.op("dve", lambda e: e.scalar_tensor_tensor(out=aT[:, c, :], in0=gl[:], scalar=8.0, in1=sg[:], op0=ALU.min, op1=ALU.mult),
                                 reads=[ksg, kgl], writes=[kaT])

                    def dn_stage(ex_, d0, d1, bd, kbd, nxt=None):
                        for s_ in range(NS):
                            if nxt is not None:
                                transposes_s(nxt, s_)
                            Yst, kY = Yst_r.next()
                            for half in range(2):
                                Wd, kWd = (d0, d1)[half]
                                pA, kpA = pA_r.next()
                                for c in range(8):
                                    p.op("pe", lambda e: e.matmul(out=pA[:], lhsT=aT[:, c, s_ * 128:(s_ + 1) * 128], rhs=Wd[:, c, :], start=(c == 0), stop=False),
                                         reads=["aT_%d" % (c // 4), kWd], writes=[kpA])
                                p.op("pe", lambda e: e.matmul(out=pA[:], lhsT=ones[0:1, :], rhs=bd[0:1, half * 512:(half + 1) * 512], start=False, stop=True),
                                     reads=["ones", kbd], writes=[kpA])
                                p.op("act", lambda e: e.copy(out=Yst[:, half * 512:(half + 1) * 512], in_=pA[:]), reads=[kpA], writes=[kY])
                            r0 = ex_ * CAP + s_ * 128
                            p.op("sp", lambda e: e.dma_start(out=Yd[r0:r0 + 128, :], in_=Yst[:]), reads=[kY], dma=kY + "s")

                    def loads_gl0(ex_):
                        return load_w(w_gu[ex_, :, 0:512], 0), load_w(w_gu[ex_, :, 1024:1536], 1)

                    def loads_gl1(ex_):
                        return load_w(w_gu[ex_, :, 512:1024], 2), load_w(w_gu[ex_, :, 1536:2048], 3)

                    def loads_d(ex_):
                        return load_w(w_dn[ex_, :, 0:512], 4), load_w(w_dn[ex_, :, 512:1024], 5), load_bd(ex_)

                    g0, l0 = loads_gl0(0)
                    g1, l1 = loads_gl1(0)
                    d0, d1, (bd, kbd) = loads_d(0)
                    dispatch(0)
                    dispatch(1)
                    transposes(0)
                    for ex_ in range(NE):
                        gu_stage(ex_, 0, g0[0], g0[1], l0[0], l0[1], sel_for=(ex_ + 2 if ex_ + 2 < NE else None))
                        if ex_ + 1 < NE:
                            g0n, l0n = loads_gl0(ex_ + 1)
                        if ex_ + 2 < NE:
                            dispatch(ex_ + 2, build=False)
                        gu_stage(ex_, 1, g1[0], g1[1], l1[0], l1[1])
                        if ex_ + 1 < NE:
                            g1n, l1n = loads_gl1(ex_ + 1)
                        dn_stage(ex_, d0, d1, bd, kbd, nxt=(ex_ + 1 if ex_ + 1 < NE else None))
                        if ex_ + 1 < NE:
                            d0, d1, (bd, kbd) = loads_d(ex_ + 1)
                            g0, l0, g1, l1 = g0n, l0n, g1n, l1n
                    p.barrier()
                    Yg_r = Ring(sb2, "Yg", 4, [128, D], F32)
                    for tile in range(16):
                        hk = "H%d" % tile
                        for j_ in range(4):
                            Yg, kYg = Yg_r.next()
                            p.op("pool", lambda e: e.indirect_dma_start(out=Yg[:, :], out_offset=None, in_=Yd[:, :],
                                                                         in_offset=bass.IndirectOffsetOnAxis(ap=slI[:, tile, j_:j_ + 1], axis=0)),
                                 reads=["slI%d" % tile], writes=[kYg], dma=kYg)
                            p.op("dve", lambda e: e.scalar_tensor_tensor(out=H[:, tile, :], in0=Yg[:], scalar=gj[:, tile, j_:j_ + 1], in1=H[:, tile, :], op0=ALU.mult, op1=ALU.add),
                                 reads=[kYg, "gj%d" % tile, hk], writes=[hk])
                    p.barrier()
                    if stop_after == "C2":
                        if debug:
                            p.op("sp", lambda e: e.dma_start(out=dbgH.rearrange("(t p) n -> p t n", p=128), in_=H[:]), reads=["H%d" % i_ for i_ in range(16)], dma="dbgH")
                            p.op("sp", lambda e: e.dma_start(out=dbgG.rearrange("(t p) n -> p t n", p=128), in_=Gt[:]), reads=["Gt%d" % i_ for i_ in range(16)], dma="dbgG")
                        p.finish_wait("sp"); p.emit(top); return nc
                s3 = ExitStack()
                with s3:
                    sb3, ps3 = mk_alloc(s3)
                    gbc = sb3("gbc3", [128, D], F32)
                    junk = sb3("junkC3", [128, D], BF16)
                    Wpg = sb3("Wpg", [128, 8, D], BF16)
                    Wpp = sb3("Wpp", [128, 2, D], BF16)
                    p.op("pool", lambda e: e.dma_start(out=Wpg[:], in_=w_pg.rearrange("(c p) n -> p c n", p=128)), writes=["Wpg"], dma="Wpg")
                    p.op("pool", lambda e: e.dma_start(out=Wpp[:], in_=w_pp.rearrange("(c p) n -> p c n", p=128)), writes=["Wpp"], dma="Wpp")
                    gfin = sb3("gfin", [128, D], F32)
                    p.op("sp", lambda e: e.dma_start(out=gbc[:], in_=g_ple.partition_broadcast(128)), writes=["gbc"], dma="gbc3")
                    p.op("sp", lambda e: e.dma_start(out=gfin[:], in_=g_final.partition_broadcast(128)), writes=["gfin"], dma="gfin")
                    ss3_r = Ring(sb3, "ss3", 2, [128, 1], F32)
                    u3_r = Ring(sb3, "u3", 2, [128, D], BF16)
                    pT3_r = Ring(ps3, "pT3", 2, [128, 8, 128], BF16)
                    u3T_r = Ring(sb3, "u3T", 2, [128, 8, 128], BF16)
                    pp_r = Ring(sb3, "ppl", 2, [128, 256], F32)
                    ppb_r = Ring(sb3, "ppb", 2, [128, 256], BF16)
                    ppT_r = Ring(sb3, "ppT", 2, [128, 2, 128], BF16)
                    pg_r = Ring(ps3, "pg3", 2, [128, 512], F32)
                    pj_r = Ring(ps3, "pj3", 2, [128, 512], F32)
                    sg3_r = Ring(sb3, "sg3", 2, [128, 512], F32)
                    o_r = Ring(sb3, "o3", 2, [128, D], F32)
                    def c3_tile(tile):
                        hk = "H%d" % tile
                        yield
                        ss3, kss = ss3_r.next()
                        yield
                        p.op("act", lambda e, ss3=ss3, tile=tile: e.activation(out=junk[:], in_=H[:, tile, :], func=AF.Square, accum_out=ss3[:]), reads=[hk], writes=["junkC", kss])
                        yield
                        rstd_from(ss3[:], ss3[:], D, [kss], [kss])
                        yield
                        u3, ku3 = u3_r.next()
                        yield
                        p.op("dve", lambda e, ss3=ss3, u3=u3, tile=tile: e.scalar_tensor_tensor(out=u3[:], in0=H[:, tile, :], scalar=ss3[:, 0:1], in1=gbc[:], op0=ALU.mult, op1=ALU.mult),
                             reads=[hk, kss, "gbc"], writes=[ku3])
                        yield
                        pT, kpT = pT3_r.next()
                        yield
                        for c in range(8):
                            p.op("pe", lambda e, c=c, pT=pT, u3=u3: e.transpose(out=pT[:, c, :], in_=u3[:, c * 128:(c + 1) * 128], identity=ident[:]), reads=[ku3, "ident"], writes=[kpT])
                        yield
                        u3T, ku3T = u3T_r.next()
                        yield
                        p.op("act", lambda e, pT=pT, u3T=u3T: e.copy(out=u3T[:], in_=pT[:]), reads=[kpT], writes=[ku3T])
                        yield
                        pp, kpp = pp_r.next()
                        yield
                        p.op("sp", lambda e, pp=pp, tile=tile: e.dma_start(out=pp[:], in_=po[tile * 128:(tile + 1) * 128, :]), writes=[kpp], dma=kpp)
                        yield
                        ppb, kppb = ppb_r.next()
                        yield
                        p.op("pool", lambda e, pp=pp, ppb=ppb: e.tensor_copy(out=ppb[:], in_=pp[:]), reads=[kpp], writes=[kppb])
                        yield
                        pT2, kpT2 = pT3_r.next()
                        yield
                        for c in range(2):
                            p.op("pe", lambda e, c=c, pT2=pT2, ppb=ppb: e.transpose(out=pT2[:, c, :], in_=ppb[:, c * 128:(c + 1) * 128], identity=ident[:]), reads=[kppb, "ident"], writes=[kpT2])
                        yield
                        ppT, kppT = ppT_r.next()
                        yield
                        p.op("act", lambda e, pT2=pT2, ppT=ppT: e.copy(out=ppT[:], in_=pT2[:, 0:2, :]), reads=[kpT2], writes=[kppT])
                        yield
                        for half in range(2):
                            pg, kpg = pg_r.next()
                            pj, kpj = pj_r.next()
                            for c in range(8):
                                p.op("pe", lambda e, c=c, pg=pg, u3T=u3T, half=half: e.matmul(out=pg[:], lhsT=u3T[:, c, :], rhs=Wpg[:, c, half * 512:(half + 1) * 512], start=(c == 0), stop=(c == 7)),
                                     reads=[ku3T, "Wpg"], writes=[kpg])
                            for c in range(2):
                                p.op("pe", lambda e, c=c, pj=pj, ppT=ppT, half=half: e.matmul(out=pj[:], lhsT=ppT[:, c, :], rhs=Wpp[:, c, half * 512:(half + 1) * 512], start=(c == 0), stop=(c == 1)),
                                     reads=[kppT, "Wpp"], writes=[kpj])
                            sg, ksg = sg3_r.next()
                            p.op("act", lambda e, sg=sg, pg=pg: e.activation(out=sg[:], in_=pg[:], func=AF.Sigmoid), reads=[kpg], writes=[ksg])
                            p.op("dve", lambda e, sg=sg, pj=pj: e.tensor_tensor(out=sg[:], in0=sg[:], in1=pj[:], op=ALU.mult), reads=[ksg, kpj], writes=[ksg])
                            p.op("dve", lambda e, sg=sg, tile=tile, half=half: e.tensor_tensor(out=H[:, tile, half * 512:(half + 1) * 512], in0=H[:, tile, half * 512:(half + 1) * 512], in1=sg[:], op=ALU.add),
                                 reads=[ksg, hk], writes=[hk])
                        yield
                        ss4, kss4 = ss3_r.next()
                        yield
                        p.op("act", lambda e, ss4=ss4, tile=tile: e.activation(out=junk[:], in_=H[:, tile, :], func=AF.Square, accum_out=ss4[:]), reads=[hk], writes=["junkC", kss4])
                        yield
                        rstd_from(ss4[:], ss4[:], D, [kss4], [kss4])
                        yield
                        ot, kot = o_r.next()
                        yield
                        p.op("dve", lambda e, ss4=ss4, ot=ot, tile=tile: e.scalar_tensor_tensor(out=ot[:], in0=H[:, tile, :], scalar=ss4[:, 0:1], in1=gfin[:], op0=ALU.mult, op1=ALU.mult),
                             reads=[hk, kss4, "gfin"], writes=[kot])
                        yield
                        p.op("sp", lambda e, ot=ot, tile=tile: e.dma_start(out=yo[tile * 128:(tile + 1) * 128, :], in_=ot[:]), reads=[kot], writes=["yo"], dma=kot + "s")

                    for pair in range(8):
                        gens = [c3_tile(pair * 2), c3_tile(pair * 2 + 1)]
                        while gens:
                            for g_ in list(gens):
                                try:
                                    next(g_)
                                except StopIteration:
                                    gens.remove(g_)
        p.finish_wait("sp")
        p.emit(top)
    return nc


_CACHE = {}


def _perm(j):
    idx = []
    for t in range(4):
        for blk in ORDER[j]:
            b0 = (8 * t + blk) * 128
            idx.append(np.arange(b0, b0 + 128))
    return np.concatenate(idx)


def kernel(x, p, positions, w_in, g_attn, g_cq, w_uq, g_ckv, w_ukv, g_out_mla, g_out_sb, w_o,
           g_moe, w_router, b_router, w_gu, b_gu, w_dn, b_dn, g_ple, w_ple_gate, w_ple_proj, g_final):
    if "nc" not in _CACHE:
        _CACHE["nc"] = build_program()
    nc = _CACHE["nc"]
    in_maps, perms = make_in_maps(x, p, positions, w_in, g_attn, g_cq, w_uq, g_ckv, w_ukv, g_out_mla, g_out_sb, w_o,
                                  g_moe, w_router, b_router, w_gu, b_gu, w_dn, b_dn, g_ple, w_ple_gate, w_ple_proj, g_final)
    res = run_bass_kernel_spmd(nc, in_maps, core_ids=list(range(8)))
    out = np.empty((4, S, D), np.float32)
    for c in range(8):
        b, j = c // 2, c % 2
        out[b, perms[j]] = np.asarray(res.results[c]["yo"])
    return out


def make_in_maps(x, p, positions, w_in, g_attn, g_cq, w_uq, g_ckv, w_ukv, g_out_mla, g_out_sb, w_o,
                 g_moe, w_router, b_router, w_gu, b_gu, w_dn, b_dn, g_ple, w_ple_gate, w_ple_proj, g_final):
    f = lambda a: np.ascontiguousarray(np.asarray(a))
    x = f(x); p = f(p); positions = f(positions)
    invf = np.zeros((128, 1), np.float32)
    fr = (10000.0 ** (-np.arange(0, 32, 2, dtype=np.float32) / 32.0)).astype(np.float32)
    invf[0:16, 0] = fr
    invf[16:32, 0] = fr
    shared = {
        "invf": invf,
        "w_in": f(w_in[0]), "g_attn": f(g_attn[0:1]), "g_cq": f(g_cq[0:1]), "w_uq": f(w_uq[0]),
        "g_ckv": f(g_ckv[0:1]), "w_ukv": f(w_ukv[0]),
        "g_out": f(np.concatenate([np.asarray(g_out_mla[0]), np.asarray(g_out_sb[0])])[None, :]),
        "w_o": f(w_o[0]), "g_moe": f(g_moe[0:1]), "w_router": f(w_router[0]), "b_router": f(b_router[0:1]),
        "w_gu": f(w_gu[0]), "b_gu": f(np.asarray(b_gu[0]).reshape(NE * 16, 128)), "w_dn": f(w_dn[0]), "b_dn": f(b_dn[0]),
        "g_ple": f(g_ple[0:1]), "w_pg": f(w_ple_gate[0]), "w_pp": f(w_ple_proj[0]), "g_final": f(np.asarray(g_final)[None, :]),
    }
    in_maps = []
    perms = [_perm(0), _perm(1)]
    for c in range(8):
        b, j = c // 2, c % 2
        pm = perms[j]
        qr = np.concatenate([np.arange(blk * 128, blk * 128 + 128) for blk in ORDER[j]]).astype(np.float32)[None, :]
        m = dict(shared)
        m["xa"] = x[b]
        m["xo"] = f(x[b][pm])
        m["po"] = f(p[0, b][pm])
        m["posa"] = f(positions[b:b + 1].astype(np.int32))
        m["poso"] = f(positions[b:b + 1, pm].astype(np.int32))
        m["qrel"] = f(qr)
        in_maps.append(m)
    return in_maps, perms
```

```python
from contextlib import ExitStack
import numpy as np
import concourse.bass as bass
import concourse.mybir as mybir
from concourse.bass_utils import run_bass_kernel_spmd

F32 = mybir.dt.float32
BF16 = mybir.dt.bfloat16
I32 = mybir.dt.int32
AF = mybir.ActivationFunctionType
ALU = mybir.AluOpType

ENGS = ("pe", "act", "dve", "pool", "sp")
S = 4096
D = 1024
NE = 32
ORDER = ([6, 5, 3, 0], [7, 4, 2, 1])
NDIAG = [512, 512, 384, 384, 256, 256, 128, 128]
NEG = -30000.0
EPS = 1e-6


class Prog:
    def __init__(self, nc):
        self.nc = nc
        self.ops = {e: [] for e in ENGS}
        self.vcs = {}
        self.cur = {e: {} for e in ENGS}
        self.last_w = {}
        self.readers = {}
        self.excl = set()

    def op(self, eng, fn, reads=(), writes=(), dma=None):
        rec_ = _Rec()
        fn(rec_)
        assert len(rec_.calls) == 1
        fn = rec_.calls[0]
        clk = ("dma:" + dma) if dma else eng
        deps = []
        reads = list(reads)
        writes = list(writes)
        for k in reads:
            if k in self.excl and k not in writes:
                writes.append(k)
        for k in reads:
            lw = self.last_w.get(k)
            if lw:
                deps.append(lw)
        for k in writes:
            lw = self.last_w.get(k)
            if lw:
                deps.append(lw)
            for c, i in self.readers.get(k, {}).items():
                deps.append((c, i))
        cur = self.cur[eng]
        wmax = {}
        for (c, i) in deps:
            if c == "pe" and eng == "pe" and not dma:
                continue
            if cur.get(c, 0) >= i:
                continue
            wmax[c] = max(wmax.get(c, 0), i)
            for c2, i2 in self.vcs[c][i - 1].items():
                if cur.get(c2, 0) < i2:
                    cur[c2] = i2
            if cur.get(c, 0) < i:
                cur[c] = i
        vc = dict(cur)
        lst = self.vcs.setdefault(clk, [])
        lst.append(vc)
        idx = len(lst)
        vc[clk] = idx
        rec = {"fn": fn, "waits": wmax, "clk": clk, "idx": idx}
        self.ops[eng].append(rec)
        for k in reads:
            self.readers.setdefault(k, {})[clk] = idx
        for k in writes:
            self.last_w[k] = (clk, idx)
            self.readers[k] = {}
        return rec

    def finish_wait(self, eng):
        waits = {}
        for c, l in self.vcs.items():
            if len(l) and self.cur[eng].get(c, 0) < len(l):
                waits[c] = len(l)
                self.cur[eng][c] = len(l)
        self.ops[eng].append({"fn": None, "waits": waits, "clk": None, "idx": None})

    def barrier(self):
        for e in ENGS:
            self.finish_wait(e)
        full = {c: len(l) for c, l in self.vcs.items()}
        for e in ENGS:
            self.cur[e] = dict(full)

    def emit(self, stack):
        nc = self.nc
        waited = {}
        for e in ENGS:
            for r in self.ops[e]:
                for c, i in r["waits"].items():
                    waited.setdefault(c, set()).add(i)
        sems, semval = {}, {}
        for c, l in self.vcs.items():
            if c not in waited:
                continue
            sems[c] = stack.enter_context(nc.semaphore("s_" + c.replace(":", "_")))
            isd = c.startswith("dma:")
            v, m = 0, {}
            for i in range(1, len(l) + 1):
                if isd or i in waited[c]:
                    v += 16 if isd else 1
                    m[i] = v
            semval[c] = m
        block = stack.enter_context(nc.Block())
        engobj = {"pe": "tensor", "act": "scalar", "dve": "vector", "pool": "gpsimd", "sp": "sync"}

        def make(e):
            def body(eng):
                for r in self.ops[e]:
                    for c, i in r["waits"].items():
                        eng.wait_ge(sems[c], semval[c][i])
                    if r["fn"] is None:
                        continue
                    name, a, k = r["fn"]
                    ins = getattr(eng, name)(*a, **k)
                    c, i = r["clk"], r["idx"]
                    if c in sems and i in semval[c]:
                        ins.then_inc(sems[c], 16 if c.startswith("dma:") else 1)
            return body

        for e in ENGS:
            if self.ops[e]:
                getattr(block, engobj[e])(make(e))


class _Rec:
    def __init__(self):
        self.calls = []

    def __getattr__(self, name):
        def f(*a, **k):
            self.calls.append((name, a, k))
        return f


class Ring:
    def __init__(self, alloc, name, n, shape, dtype):
        self.tiles = [alloc("%s%d" % (name, i), shape, dtype) for i in range(n)]
        self.keys = ["%s%d" % (name, i) for i in range(n)]
        self.i = 0

    def next(self):
        t, k = self.tiles[self.i % len(self.tiles)], self.keys[self.i % len(self.tiles)]
        self.i += 1
        return t, k


class _Stop(Exception):
    pass


def build_program(stop_after=None, debug=False):
    nc = bass.Bass("TRN2", target_bir_lowering=False)

    def din(name, shape, dt=F32):
        return nc.dram_tensor(name, list(shape), dt, kind="ExternalInput").ap()

    def dscr(name, shape, dt=BF16):
        return nc.dram_tensor(name, list(shape), dt, kind="ExternalOutput" if debug else "Internal").ap()

    xa = din("xa", [S, D])
    xo = din("xo", [2048, D])
    po = din("po", [2048, 256])
    posa = din("posa", [1, S], I32)
    poso = din("poso", [1, 2048], I32)
    qrel = din("qrel", [1, 512])
    invf = din("invf", [128, 1])
    w_in = din("w_in", [D, 2208])
    g_attn = din("g_attn", [1, D])
    g_cq = din("g_cq", [1, 384])
    w_uq = din("w_uq", [384, 768])
    g_ckv = din("g_ckv", [1, 256])
    w_ukv = din("w_ukv", [256, 1024])
    g_out = din("g_out", [1, 1024])
    w_o = din("w_o", [D, D])
    g_moe = din("g_moe", [1, D])
    w_router = din("w_router", [D, NE])
    b_router = din("b_router", [1, NE])
    NEd = NE if stop_after in (None, "C2") else 1
    w_gu = din("w_gu", [NEd, D, 2048])
    b_gu = din("b_gu", [NE * 16, 128])
    w_dn = din("w_dn", [NEd, D, D])
    b_dn = din("b_dn", [NE, D])
    g_ple = din("g_ple", [1, D])
    w_pg = din("w_pg", [D, D])
    w_pp = din("w_pp", [256, D])
    g_final = din("g_final", [1, D])
    yo = nc.dram_tensor("yo", [2048, D], F32, kind="ExternalOutput").ap()

    KnT = dscr("KnT", [512, S])
    KrT = dscr("KrT", [32, S])
    VmD = dscr("VmD", [S, 512])
    KsT = dscr("KsT", [512, S])
    VsD = dscr("VsD", [S, 512])
    QmT = dscr("QmT", [8 * 96, 2048])
    QsT = dscr("QsT", [512, 2048])
    OTd = dscr("OTd", [1024, 2048])
    CAP = 512
    NS = CAP // 128
    Ud = nc.dram_tensor("Ud", [2048, D], BF16, kind="Internal").ap()
    Yd = nc.dram_tensor("Yd", [NE * CAP, D], F32, kind="Internal").ap()
    if debug:
        dbgH = nc.dram_tensor("dbgH", [2048, D], F32, kind="ExternalOutput").ap()
        dbgG = nc.dram_tensor("dbgG", [2048, NE], F32, kind="ExternalOutput").ap()
        dbgS = nc.dram_tensor("dbgS", [2048, 4], I32, kind="ExternalOutput").ap()
        dbgJ = nc.dram_tensor("dbgJ", [2048, 4], F32, kind="ExternalOutput").ap()

    top = ExitStack()
    with top:
        p = Prog(nc)
        if True:

            def mk_alloc(stack):
                def sb(n, s, d):
                    return stack.enter_context(nc.sbuf_tensor(n, list(s), d))

                def ps(n, s, d=F32):
                    return stack.enter_context(nc.psum_tensor(n, list(s), d))
                return sb, ps

            sb0, ps0 = mk_alloc(top)

            identf = sb0("identf", [128, 128], F32)
            ident = sb0("ident", [128, 128], BF16)
            ones = sb0("ones", [128, 128], BF16)
            ntri = sb0("ntri", [128, 128], BF16)
            nones = sb0("nones", [128, 128], BF16)
            zeros = sb0("zeros", [128, 128], BF16)
            p.op("pool", lambda e: e.memset(identf[:], 0.0), writes=["identf"])
            p.op("pool", lambda e: e.affine_select(out=identf[:], in_=identf[:], pattern=[[-1, 128]],
                                                    compare_op=ALU.not_equal, fill=1.0, base=0, channel_multiplier=1),
                 reads=["identf"], writes=["identf"])
            p.op("dve", lambda e: e.tensor_copy(out=ident[:], in_=identf[:]), reads=["identf"], writes=["ident"])
            p.op("pool", lambda e: e.memset(ones[:], 1.0), writes=["ones"])
            p.op("pool", lambda e: e.memset(nones[:], -1.0), writes=["nones"])
            p.op("pool", lambda e: e.memset(zeros[:], 0.0), writes=["zeros"])
            gcol = sb0("gcol", [128, 16], F32)
            p.op("sp", lambda e: e.dma_start(out=gcol[:, 0:3], in_=g_cq.rearrange("o (c p) -> p (o c)", p=128),
                                             allow_slow_non_contiguous=True), writes=["gcol"], dma="gcol")
            p.op("sp", lambda e: e.dma_start(out=gcol[:, 3:5], in_=g_ckv.rearrange("o (c p) -> p (o c)", p=128),
                                             allow_slow_non_contiguous=True), writes=["gcol"], dma="gcol")
            p.op("sp", lambda e: e.dma_start(out=gcol[:, 5:13], in_=g_out.rearrange("o (c p) -> p (o c)", p=128),
                                             allow_slow_non_contiguous=True), writes=["gcol"], dma="gcol")
            invc = sb0("invc", [128, 1], F32)
            p.op("sp", lambda e: e.dma_start(out=invc[:], in_=invf), writes=["invc"], dma="invc")

            def rstd_from(eng_out, src, n, rd, wr):
                p.op("act", lambda e: e.activation(out=eng_out, in_=src, func=AF.Ln, scale=1.0 / n, bias=EPS),
                     reads=rd, writes=wr)
                p.op("act", lambda e: e.activation(out=eng_out, in_=eng_out, func=AF.Exp, scale=-0.5),
                     reads=wr, writes=wr)

            sa = ExitStack()
            with sa:
                sb, ps = mk_alloc(sa)
                tmpf = sb("tmpf", [128, 128], F32)
                p.op("pool", lambda e: e.memset(tmpf[:], -1.0), writes=["tmpf"])
                p.op("pool", lambda e: e.affine_select(out=tmpf[:], in_=tmpf[:], pattern=[[-1, 128]],
                                                        compare_op=ALU.is_ge, fill=0.0, base=0, channel_multiplier=1),
                     reads=["tmpf"], writes=["tmpf"])
                p.op("dve", lambda e: e.tensor_copy(out=ntri[:], in_=tmpf[:]), reads=["tmpf"], writes=["ntri"])

                Win = sb("Win", [128, 8, 2208], BF16)
                for c in range(8):
                    p.op("pool", lambda e, c=c: e.dma_start(out=Win[:, c, :], in_=w_in[c * 128:(c + 1) * 128, :]),
                         writes=["Win"], dma="Win")
                Wuq = sb("Wuq", [128, 3, 768], BF16)
                Wuqr = sb("Wuqr", [128, 3, 768], BF16)
                p.op("pool", lambda e: e.dma_start(out=Wuq[:], in_=w_uq.rearrange("(c p) n -> p c n", p=128)),
                     writes=["Wuq"], dma="Wuq")
                Wkn = sb("Wkn", [128, 2, 512], BF16)
                Wv = sb("Wv", [128, 2, 512], BF16)
                ukv = w_ukv.rearrange("(c p) (h t d) -> p c h t d", p=128, h=8, t=2)
                for c in range(2):
                    p.op("pool", lambda e, c=c: e.dma_start(out=Wkn[:, c, :].rearrange("p (h d) -> p h d", h=8),
                                                          in_=ukv[:, c, :, 0, :]), writes=["Wkn"], dma="Wkn")
                    p.op("pool", lambda e, c=c: e.dma_start(out=Wv[:, c, :].rearrange("p (h d) -> p h d", h=8),
                                                          in_=ukv[:, c, :, 1, :]), writes=["Wv"], dma="Wv")
                p.op("pool", lambda e: e.memset(Wuqr[:], 0.0), writes=["Wuqr"])
                Wq4 = Wuq[:].rearrange("p c (h d) -> p c h d", h=8)
                Wr4 = Wuqr[:].rearrange("p c (h d) -> p c h d", h=8)
                for c in range(3):
                    p.op("dve", lambda e, c=c: e.tensor_scalar(out=Wr4[:, c, :, 64:80], in0=Wq4[:, c, :, 80:96], scalar1=-1.0,
                                                          scalar2=None, op0=ALU.mult), reads=["Wuq", "Wuqr"], writes=["Wuqr"])
                    p.op("dve", lambda e, c=c: e.tensor_copy(out=Wr4[:, c, :, 80:96], in_=Wq4[:, c, :, 64:80]),
                         reads=["Wuq", "Wuqr"], writes=["Wuqr"])
                Wkr = sb("Wkr", [128, 8, 64], BF16)
                p.op("dve", lambda e: e.tensor_copy(out=Wkr[:, :, 0:32], in_=Win[:, :, 640:672]), reads=["Win"], writes=["Wkr"])
                p.op("dve", lambda e: e.tensor_scalar(out=Wkr[:, :, 32:48], in0=Win[:, :, 656:672], scalar1=-1.0, scalar2=None,
                                                      op0=ALU.mult), reads=["Win", "Wkr"], writes=["Wkr"])
                p.op("dve", lambda e: e.tensor_copy(out=Wkr[:, :, 48:64], in_=Win[:, :, 640:656]), reads=["Win", "Wkr"], writes=["Wkr"])
                gattn = sb("gattn", [128, D], F32)
                p.op("sp", lambda e: e.dma_start(out=gattn[:], in_=g_attn.partition_broadcast(128)), writes=["gattn"], dma="gattn")

                def rope_table(Ct, St, pos_ap, n, rows, name, inv, kinv, sb):
                    posi = sb(name + "_pi", [128, n], I32)
                    ang = sb(name + "_ang", [128, n], F32)
                    kk = sb(name + "_k", [128, n], F32)
                    ki = sb(name + "_ki", [128, n], I32)
                    p.op("sp", lambda e: e.dma_start(out=posi[:], in_=pos_ap.partition_broadcast(128)), writes=[name + "pi"], dma=name + "pi")
                    p.op("dve", lambda e: e.tensor_copy(out=ang[:], in_=posi[:]), reads=[name + "pi"], writes=[name + "ang"])
                    p.op("dve", lambda e: e.tensor_scalar(out=ang[:], in0=ang[:], scalar1=inv[:, 0:1], scalar2=None, op0=ALU.mult),
                         reads=[name + "ang", kinv], writes=[name + "ang"])
                    for which, T in (("s", St), ("c", Ct)):
                        off = 0.0 if which == "s" else float(np.pi / 2)
                        p.op("dve", lambda e, off=off: e.tensor_scalar(out=kk[:], in0=ang[:], scalar1=off, scalar2=float(1.0 / (2 * np.pi)),
                                                                   op0=ALU.add, op1=ALU.mult), reads=[name + "ang"], writes=[name + "kk"])
                        p.op("dve", lambda e: e.tensor_copy(out=ki[:], in_=kk[:]), reads=[name + "kk"], writes=[name + "ki"])
                        p.op("dve", lambda e: e.tensor_copy(out=kk[:], in_=ki[:]), reads=[name + "ki"], writes=[name + "kk"])
                        p.op("dve", lambda e, T=T: e.scalar_tensor_tensor(out=T, in0=kk[:rows], scalar=-6.28125, in1=ang[:rows],
                                                                       op0=ALU.mult, op1=ALU.add),
                             reads=[name + "kk", name + "ang"], writes=[name + which])
                        p.op("dve", lambda e, T=T, off=off: e.scalar_tensor_tensor(out=T, in0=kk[:rows], scalar=float(-(2 * np.pi - 6.28125)), in1=T,
                                                                                op0=ALU.mult, op1=ALU.add),
                             reads=[name + "kk", name + which], writes=[name + which])
                        if off != 0.0:
                            p.op("dve", lambda e, T=T, off=off: e.tensor_scalar(out=T, in0=T, scalar1=off, scalar2=None, op0=ALU.add),
                                 reads=[name + which], writes=[name + which])
                        p.op("dve", lambda e: e.tensor_scalar(out=kk[:rows], in0=T, scalar1=float(np.pi), scalar2=float(-2 * np.pi),
                                                              op0=ALU.is_gt, op1=ALU.mult), reads=[name + which], writes=[name + "kk"])
                        p.op("dve", lambda e, T=T: e.tensor_tensor(out=T, in0=T, in1=kk[:rows], op=ALU.add),
                             reads=[name + which, name + "kk"], writes=[name + which])
                        p.op("dve", lambda e: e.tensor_scalar(out=kk[:rows], in0=T, scalar1=float(-np.pi), scalar2=float(2 * np.pi),
                                                              op0=ALU.is_lt, op1=ALU.mult), reads=[name + which], writes=[name + "kk"])
                        p.op("dve", lambda e, T=T: e.tensor_tensor(out=T, in0=T, in1=kk[:rows], op=ALU.add),
                             reads=[name + which, name + "kk"], writes=[name + which])
                        p.op("dve", lambda e, T=T: e.tensor_scalar(out=T, in0=T, scalar1=3.14159, scalar2=-3.14159, op0=ALU.min, op1=ALU.max),
                             reads=[name + which], writes=[name + which])
                        p.op("act", lambda e, T=T: e.activation(out=T, in_=T, func=AF.Sin), reads=[name + which], writes=[name + which])

                Ck = sb("Ck", [32, S], F32)
                Sk = sb("Sk", [32, S], F32)
                sa2 = ExitStack()
                with sa2:
                    rope_table(Ck[:], Sk[:], posa, S, 32, "rk", invc, "invc", mk_alloc(sa2)[0])
                    p.barrier()
                Cq = sb("Cq", [96, 2048], F32)
                Sq = sb("Sq", [96, 2048], F32)
                sa3 = ExitStack()
                with sa3:
                    sb3_ = mk_alloc(sa3)[0]
                    invq = sb3_("invq", [128, 1], F32)
                    p.op("pool", lambda e: e.memset(invq[:], 0.0), writes=["invq"])
                    p.op("sp", lambda e: e.dma_start(out=invq[64:96, :], in_=invf[0:32, :]), reads=["invq"], writes=["invq"], dma="invq")
                    rope_table(Cq[:], Sq[:], poso, 2048, 96, "rq", invq, "invq", sb3_)
                    p.barrier()
                    if stop_after == "R":
                        p.finish_wait("sp"); p.emit(top); return nc
                msc = float((64 + 32) ** -0.5)
                p.op("dve", lambda e: e.tensor_scalar(out=Cq[:], in0=Cq[:], scalar1=msc, scalar2=None, op0=ALU.mult),
                     reads=["rqc"], writes=["rqc"])
                p.op("dve", lambda e: e.tensor_scalar(out=Sq[:], in0=Sq[:], scalar1=msc, scalar2=None, op0=ALU.mult),
                     reads=["rqs"], writes=["rqs"])

                xt_r = Ring(sb, "xt", 4, [128, D], F32)
                junk = sb("junkA", [128, D], F32)
                ss_r = Ring(sb, "ssA", 4, [128, 1], F32)
                xn_r = Ring(sb, "xn", 4, [128, D], BF16)
                xnT_r = Ring(sb, "xnT", 2, [128, 8, 512], BF16)
                pT_r = Ring(ps, "pTA", 2, [128, 8, 128], BF16)
                pm_r = Ring(ps, "pmA", 4, [128, 512], F32)
                pss = ps("pssA", [128, 512], F32)
                sq_r = Ring(sb, "sqA", 2, [128, 512], BF16)
                rbc = sb("rbcA", [128, 512], F32)
                cn_r = Ring(sb, "cnA", 2, [128, 3, 512], BF16)
                ev_r = Ring(sb, "evA", 2, [128, 512], BF16)
                stg_r = Ring(sb, "stgA", 3, [128, 4, 512], BF16)
                stg8_r = Ring(sb, "stg8A", 2, [128, 8, 512], BF16)
                evf_r = Ring(sb, "evfA", 2, [128, 512], F32)
                evf2_r = Ring(sb, "evf2A", 2, [128, 512], F32)

                def make_xnT(src, tok0):
                    xnT, kT = xnT_r.next()
                    xs = []
                    for sub in range(4):
                        xt, kx = xt_r.next()
                        ss, ks = ss_r.next()
                        xn, kn = xn_r.next()
                        r0 = tok0 + sub * 128
                        p.op("sp", lambda e: e.dma_start(out=xt[:], in_=src[r0:r0 + 128, :]), writes=[kx], dma=kx)
                        p.op("act", lambda e: e.activation(out=junk[:], in_=xt[:], func=AF.Square, accum_out=ss[:]),
                             reads=[kx], writes=["junkA", ks])
                        rstd_from(ss[:], ss[:], D, [ks], [ks])
                        p.op("dve", lambda e: e.scalar_tensor_tensor(out=xn[:], in0=xt[:], scalar=ss[:, 0:1], in1=gattn[:],
                                                                     op0=ALU.mult, op1=ALU.mult),
                             reads=[kx, ks, "gattn"], writes=[kn])
                        xs.append((xn, kn))
                    for sub in range(4):
                        xn, kn = xs[sub]
                        pT, kp = pT_r.next()
                        for c in range(8):
                            p.op("pe", lambda e: e.transpose(out=pT[:, c, :], in_=xn[:, c * 128:(c + 1) * 128], identity=ident[:]),
                                 reads=[kn, "ident"], writes=[kp])
                        p.op("dve", lambda e: e.tensor_copy(out=xnT[:, :, sub * 128:(sub + 1) * 128], in_=pT[:]),
                             reads=[kp], writes=[kT])
                    return xnT, kT

                def proj_fm(xnT, kT, col0, m, wkey="Win", W=None):
                    W = Win if W is None else W
                    pm, kpm = pm_r.next()
                    for k in range(8):
                        p.op("pe", lambda e, k=k, pm=pm, W=W: e.matmul(out=pm[0:m, :], lhsT=W[:, k, col0:col0 + m], rhs=xnT[:, k, :],
                                                                  start=(k == 0), stop=(k == 7)),
                             reads=[kT, wkey], writes=[kpm])
                    return pm, kpm

                def lowrank_norm(pms, nch, width, gc0):
                    for i, (pm, kpm) in enumerate(pms):
                        sq, ksq = sq_r.next()
                        p.op("act", lambda e, pm=pm, sq=sq: e.activation(out=sq[:], in_=pm[:], func=AF.Square), reads=[kpm], writes=[ksq])
                        p.op("pe", lambda e, sq=sq, i=i: e.matmul(out=pss[:], lhsT=ones[:], rhs=sq[:], start=(i == 0), stop=(i == nch - 1)),
                             reads=[ksq, "ones"], writes=["pssA"])
                    rstd_from(rbc[:], pss[:], width, ["pssA"], ["rbcA"])
                    cn, kcn = cn_r.next()
                    for i, (pm, kpm) in enumerate(pms):
                        p.op("dve", lambda e, pm=pm, i=i, cn=cn: e.scalar_tensor_tensor(out=cn[:, i, :], in0=pm[:], scalar=gcol[:, gc0 + i:gc0 + i + 1],
                                                                                    in1=rbc[:], op0=ALU.mult, op1=ALU.mult),
                             reads=[kpm, "gcol", "rbcA"], writes=[kcn])
                    return cn, kcn

                def store_fm(pm, kpm, rows, dst, eng="dve", scale=None):
                    ev, kev = ev_r.next()
                    if scale is None:
                        if eng == "act":
                            p.op("act", lambda e: e.copy(out=ev[0:rows, :], in_=pm[0:rows, :]), reads=[kpm], writes=[kev])
                        else:
                            p.op("dve", lambda e: e.tensor_copy(out=ev[0:rows, :], in_=pm[0:rows, :]), reads=[kpm], writes=[kev])
                    else:
                        p.op("act", lambda e: e.mul(out=ev[0:rows, :], in_=pm[0:rows, :], mul=scale), reads=[kpm], writes=[kev])
                    p.op("pool", lambda e: e.dma_start(out=dst, in_=ev[0:rows, :]), reads=[kev], writes=[], dma=kev + "s")

                def evac_to(pm, kpm, dst, kdst, eng="dve", scale=None):
                    if scale is not None:
                        p.op("act", lambda e: e.mul(out=dst, in_=pm[:], mul=scale), reads=[kpm], writes=[kdst])
                    elif eng == "act":
                        p.op("act", lambda e: e.copy(out=dst, in_=pm[:]), reads=[kpm], writes=[kdst])
                    else:
                        p.op("dve", lambda e: e.tensor_copy(out=dst, in_=pm[:]), reads=[kpm], writes=[kdst])

                KnT_v = KnT.rearrange("(c p) n -> p c n", p=128)
                KsT_v = KsT.rearrange("(c p) n -> p c n", p=128)
                QsT_v = QsT.rearrange("(c p) n -> p c n", p=128)
                VmD_v = VmD.rearrange("(s p) n -> p s n", p=128)
                VsD_v = VsD.rearrange("(s p) n -> p s n", p=128)
                QmT_v = QmT.rearrange("(h r) n -> r h n", r=96)

                srcs = [(xa, g_ * 512) for g_ in range(8)] + [(xo, g_ * 512) for g_ in range(4)]
                nxt = make_xnT(*srcs[0])
                for G in range(8):
                    c0 = G * 512
                    xnT, kT = nxt
                    nxt = make_xnT(*srcs[G + 1])
                    ckv = [proj_fm(xnT, kT, 384 + m * 128, 128) for m in range(2)]
                    cn, kcn = lowrank_norm(ckv, 2, 256, 3)
                    pa, kpa = proj_fm(xnT, kT, 0, 32, "Wkr", Wkr)
                    pb, kpb = proj_fm(xnT, kT, 32, 32, "Wkr", Wkr)
                    t1, kt1 = evf_r.next()
                    t2, kt2 = evf2_r.next()
                    p.op("dve", lambda e, pa=pa, t1=t1, c0=c0: e.tensor_tensor(out=t1[0:32, :], in0=pa[0:32, :], in1=Ck[:, c0:c0 + 512], op=ALU.mult),
                         reads=[kpa, "rkc"], writes=[kt1])
                    p.op("dve", lambda e, pb=pb, t2=t2, c0=c0: e.tensor_tensor(out=t2[0:32, :], in0=pb[0:32, :], in1=Sk[:, c0:c0 + 512], op=ALU.mult),
                         reads=[kpb, "rks"], writes=[kt2])
                    ev, kev = ev_r.next()
                    p.op("dve", lambda e, t1=t1, t2=t2, ev=ev: e.tensor_tensor(out=ev[0:32, :], in0=t1[0:32, :], in1=t2[0:32, :], op=ALU.add),
                         reads=[kt1, kt2], writes=[kev])
                    p.op("sp", lambda e, ev=ev, c0=c0: e.dma_start(out=KrT[:, c0:c0 + 512], in_=ev[0:32, :]), reads=[kev], writes=[], dma=kev + "s")
                    st_, kst_ = stg_r.next()
                    for m in range(4):
                        pm, kpm = proj_fm(xnT, kT, 1184 + m * 128, 128)
                        evac_to(pm, kpm, st_[:, m, :], kst_, eng="act")
                    p.op("sp", lambda e: e.dma_start(out=KsT_v[:, :, c0:c0 + 512], in_=st_[:]), reads=[kst_], dma=kst_ + "s")
                    st_, kst_ = stg_r.next()
                    for sub in range(4):
                        pm, kpm = pm_r.next()
                        for k in range(8):
                            p.op("pe", lambda e, k=k, pm=pm, sub=sub, xnT=xnT: e.matmul(out=pm[:], lhsT=xnT[:, k, sub * 128:(sub + 1) * 128],
                                                                                   rhs=Win[:, k, 1696:2208], start=(k == 0), stop=(k == 7)),
                                 reads=[kT, "Win"], writes=[kpm])
                        evac_to(pm, kpm, st_[:, sub, :], kst_, eng="dve")
                    p.op("sp", lambda e: e.dma_start(out=VsD_v[:, G * 4:(G + 1) * 4, :], in_=st_[:]), reads=[kst_], dma=kst_ + "s")

                    st_, kst_ = stg_r.next()
                    for hp in range(4):
                        pm, kpm = pm_r.next()
                        for m in range(2):
                            p.op("pe", lambda e, m=m, pm=pm, hp=hp, cn=cn: e.matmul(out=pm[:], lhsT=Wkn[:, m, hp * 128:(hp + 1) * 128], rhs=cn[:, m, :],
                                                                               start=(m == 0), stop=(m == 1)),
                                 reads=[kcn, "Wkn"], writes=[kpm])
                        evac_to(pm, kpm, st_[:, hp, :], kst_, eng="act")
                    p.op("sp", lambda e: e.dma_start(out=KnT_v[:, :, c0:c0 + 512], in_=st_[:]), reads=[kst_], dma=kst_ + "s")
                    st_, kst_ = stg_r.next()
                    for sub in range(4):
                        pm, kpm = pm_r.next()
                        for m in range(2):
                            p.op("pe", lambda e, m=m, pm=pm, sub=sub, cn=cn: e.matmul(out=pm[:], lhsT=cn[:, m, sub * 128:(sub + 1) * 128], rhs=Wv[:, m, :],
                                                                                 start=(m == 0), stop=(m == 1)),
                                 reads=[kcn, "Wv"], writes=[kpm])
                        evac_to(pm, kpm, st_[:, sub, :], kst_, eng="dve")
                    p.op("sp", lambda e: e.dma_start(out=VmD_v[:, G * 4:(G + 1) * 4, :], in_=st_[:]), reads=[kst_], dma=kst_ + "s")
                for G in range(4):
                    c0 = G * 512
                    xnT, kT = nxt
                    if G + 1 < 4:
                        nxt = make_xnT(*srcs[8 + G + 1])
                    cq = [proj_fm(xnT, kT, m * 128, 128) for m in range(3)]
                    cn, kcn = lowrank_norm(cq, 3, 384, 0)
                    st8, kst8 = stg8_r.next()
                    for h in range(8):
                        pa, kpa = pm_r.next()
                        pb, kpb = pm_r.next()
                        for m in range(3):
                            p.op("pe", lambda e, m=m, pa=pa, h=h, cn=cn: e.matmul(out=pa[0:96, :], lhsT=Wuq[:, m, h * 96:(h + 1) * 96], rhs=cn[:, m, :],
                                                                             start=(m == 0), stop=(m == 2)),
                                 reads=[kcn, "Wuq"], writes=[kpa])
                        for m in range(3):
                            p.op("pe", lambda e, m=m, pb=pb, h=h, cn=cn: e.matmul(out=pb[0:96, :], lhsT=Wuqr[:, m, h * 96:(h + 1) * 96], rhs=cn[:, m, :],
                                                                             start=(m == 0), stop=(m == 2)),
                                 reads=[kcn, "Wuqr"], writes=[kpb])
                        t1, kt1 = evf_r.next()
                        t2, kt2 = evf2_r.next()
                        p.op("dve", lambda e, pa=pa, t1=t1, c0=c0: e.tensor_tensor(out=t1[0:96, :], in0=pa[0:96, :], in1=Cq[:, c0:c0 + 512], op=ALU.mult),
                             reads=[kpa, "rqc"], writes=[kt1])
                        p.op("dve", lambda e, pb=pb, t2=t2, c0=c0: e.tensor_tensor(out=t2[0:96, :], in0=pb[0:96, :], in1=Sq[:, c0:c0 + 512], op=ALU.mult),
                             reads=[kpb, "rqs"], writes=[kt2])
                        p.op("pool", lambda e, t1=t1, t2=t2: e.tensor_tensor(out=st8[0:96, h, :], in0=t1[0:96, :], in1=t2[0:96, :], op=ALU.add),
                             reads=[kt1, kt2], writes=[kst8])
                    p.op("sp", lambda e: e.dma_start(out=QmT_v[:, :, c0:c0 + 512], in_=st8[0:96, :, :]), reads=[kst8], dma=kst8 + "s")
                    st_, kst_ = stg_r.next()
                    for m in range(4):
                        pm, kpm = proj_fm(xnT, kT, 672 + m * 128, 128)
                        evac_to(pm, kpm, st_[:, m, :], kst_, scale=0.125)
                    p.op("sp", lambda e: e.dma_start(out=QsT_v[:, :, c0:c0 + 512], in_=st_[:]), reads=[kst_], dma=kst_ + "s")
                p.barrier()
                if stop_after == "A":
                    p.finish_wait("sp"); p.emit(top); return nc

            sbx = ExitStack()
            with sbx:
                sb, ps = mk_alloc(sbx)
                qrb = sb("qrb", [128, 512], F32)
                p.op("sp", lambda e: e.dma_start(out=qrb[:], in_=qrel.partition_broadcast(128)), writes=["qrb"], dma="qrb")
                kidx_i = sb("kidx_i", [128, 1], I32)
                krel = sb("krel", [128, 8], F32)
                p.op("pool", lambda e: e.iota(kidx_i[:], pattern=[[0, 1]], base=0, channel_multiplier=1), writes=["kidx_i"])
                p.op("dve", lambda e: e.tensor_copy(out=krel[:, 0:1], in_=kidx_i[:]), reads=["kidx_i"], writes=["krel"])
                for d in range(1, 8):
                    p.op("dve", lambda e, d=d: e.tensor_scalar(out=krel[:, d:d + 1], in0=krel[:, 0:1], scalar1=float(128 * d), scalar2=None, op0=ALU.add),
                         reads=["krel"], writes=["krel"])
                nmM = sb("nmM", [128, 8, 512], BF16)
                nmS = sb("nmS", [128, 8, 512], BF16)
                m01 = sb("m01", [128, 8, 512], BF16)
                for d in range(8):
                    p.op("dve", lambda e, d=d: e.tensor_scalar(out=nmM[:, d, :], in0=qrb[:], scalar1=krel[:, d:d + 1], scalar2=NEG, op0=ALU.is_lt, op1=ALU.mult),
                         reads=["qrb", "krel"], writes=["nmM"])
                    p.op("dve", lambda e, d=d: e.tensor_scalar(out=nmS[:, d, :], in0=qrb[:], scalar1=krel[:, d:d + 1], scalar2=NEG, op0=ALU.is_le, op1=ALU.mult),
                         reads=["qrb", "krel"], writes=["nmS"])
                    p.op("dve", lambda e, d=d: e.tensor_scalar(out=m01[:, d, :], in0=qrb[:], scalar1=krel[:, d:d + 1], scalar2=None, op0=ALU.is_gt),
                         reads=["qrb", "krel"], writes=["m01"])

                def blocks(t):
                    out = [(kb, 512, None) for kb in range(8 * t)]
                    out += [(8 * t + d, NDIAG[d], d) for d in range(8)]
                    return out

                sm = ExitStack()
                with sm:
                    def mla_gen():
                        sb, ps = mk_alloc(sm)
                        Vall = sb("Vall", [128, 32, 8, 65], BF16)
                        p.op("pool", lambda e: e.memset(Vall[:], 1.0), writes=["Vall"])
                        vsrc = VmD.rearrange("(kb p) (h d) -> p kb h d", p=128, h=8)
                        for hh in range(8):
                            p.op("sp", lambda e: e.dma_start(out=Vall[:, :, hh, 0:64], in_=vsrc[:, :, hh, :]),
                                 reads=["Vall"], writes=["Vall"], dma="Vall")
                        KT_r = Ring(sb, "KTm", 2, [96, S], BF16)
                        QT_r = Ring(sb, "QTm", 2, [96, 2048], BF16)
                        pS_r = Ring(ps, "pSm", 2, [128, 512], F32)
                        pO_r = Ring(ps, "pOm", 1, [128, 512], F32)
                        pB_r = Ring(ps, "pBm", 1, [128, 512], F32)
                        P_r = Ring(sb, "Pm", 3, [128, 512], BF16)
                        Of_r = Ring(sb, "Ofm", 2, [65, 512], F32)
                        On_r = Ring(sb, "Onm", 2, [64, 512], BF16)
                        sel = sb("sel65", [65, 64], F32)
                        p.op("pool", lambda e: e.memset(sel[:], 0.0), writes=["sel65"])
                        p.op("pool", lambda e: e.memset(sel[64:65, :], 1.0), reads=["sel65"], writes=["sel65"])
                        for h in range(8):
                            KT, kKT = KT_r.next()
                            QT, kQT = QT_r.next()
                            p.op("sp", lambda e, KT=KT, h=h: e.dma_start(out=KT[0:64, :], in_=KnT[h * 64:(h + 1) * 64, :]), writes=[kKT], dma=kKT)
                            p.op("sp", lambda e, KT=KT: e.dma_start(out=KT[64:96, :], in_=KrT[:, :]), writes=[kKT], dma=kKT)
                            p.op("sp", lambda e, QT=QT, h=h: e.dma_start(out=QT[:], in_=QmT[h * 96:(h + 1) * 96, :]), writes=[kQT], dma=kQT)
                            for t in range(4):
                                bl = blocks(t)
                                pO, kpO = pO_r.next()
                                nb = len(bl)
                                stage = {}

                                def stA(i):
                                    kb, N, d = bl[i]
                                    pS, kpS = pS_r.next()
                                    p.op("pe", lambda e: e.matmul(out=pS[:, 0:N], lhsT=KT[:, kb * 128:(kb + 1) * 128], rhs=QT[:, t * 512:t * 512 + N],
                                                                  start=True, stop=(d is None)), reads=[kKT, kQT], writes=[kpS])
                                    if d is not None:
                                        p.op("pe", lambda e: e.matmul(out=pS[:, 0:N], lhsT=ident[:], rhs=nmM[:, d, 0:N], start=False, stop=True),
                                             reads=["ident", "nmM"], writes=[kpS])
                                    P, kP = P_r.next()
                                    p.op("act", lambda e: e.activation(out=P[:, 0:N], in_=pS[:, 0:N], func=AF.Exp), reads=[kpS], writes=[kP])
                                    stage[i] = (P, kP)

                                def stB(i):
                                    kb, N, d = bl[i]
                                    P, kP = stage.pop(i)
                                    p.op("pe", lambda e: e.matmul(out=pO[0:65, 0:N], lhsT=Vall[:, kb, h, :], rhs=P[:, 0:N], start=(i == 0), stop=(i == nb - 1)),
                                         reads=["Vall", kP], writes=[kpO])

                                for it in range(nb + 2):
                                    if it < nb:
                                        stA(it)
                                    if it - 2 >= 0:
                                        stB(it - 2)
                                    yield
                                Of, kOf = Of_r.next()
                                p.op("dve", lambda e, Of=Of, pO=pO: e.tensor_copy(out=Of[:], in_=pO[0:65, :]), reads=[kpO], writes=[kOf])
                                p.op("dve", lambda e, Of=Of: e.reciprocal(out=Of[64:65, :], in_=Of[64:65, :]), reads=[kOf], writes=[kOf])
                                pB, kpB = pB_r.next()
                                p.op("pe", lambda e, Of=Of, pB=pB: e.matmul(out=pB[0:64, :], lhsT=sel[:], rhs=Of[:], start=True, stop=True),
                                     reads=[kOf, "sel65"], writes=[kpB])
                                On, kOn = On_r.next()
                                p.op("dve", lambda e, Of=Of, pB=pB, On=On: e.tensor_tensor(out=On[:], in0=Of[0:64, :], in1=pB[0:64, :], op=ALU.mult),
                                     reads=[kOf, kpB], writes=[kOn])
                                p.op("pool", lambda e, On=On, h=h, t=t: e.dma_start(out=OTd[h * 64:(h + 1) * 64, t * 512:(t + 1) * 512], in_=On[:]),
                                     reads=[kOn], writes=[], dma=kOn + "s")

                    def sb_gen():
                        sb, ps = mk_alloc(sm)
                        Vs = sb("Vsall", [128, 32, 512], BF16)
                        vsrc = VsD.rearrange("(kb p) n -> p kb n", p=128)
                        for q4 in range(4):
                            p.op("sp", lambda e, q4=q4: e.dma_start(out=Vs[:, q4 * 8:(q4 + 1) * 8, :], in_=vsrc[:, q4 * 8:(q4 + 1) * 8, :]),
                                 writes=["Vsall"], dma="Vsall")
                        KT_r = Ring(sb, "KTs", 2, [64, S], BF16)
                        QT_r = Ring(sb, "QTs", 2, [64, 2048], BF16)
                        pZ_r = Ring(ps, "pZs", 2, [128, 512], F32)
                        pL_r = Ring(ps, "pLs", 1, [128, 512], F32)
                        pO_r = Ring(ps, "pOs", 1, [128, 512], F32)
                        E_r = Ring(sb, "Es", 3, [128, 512], F32)
                        SP_r = Ring(sb, "SPs", 4, [128, 512], BF16)
                        SM_r = Ring(sb, "SMs", 4, [128, 512], BF16)
                        A_r = Ring(sb, "As", 3, [128, 512], BF16)
                        CR_r = Ring(sb, "CRs", 3, [128, 512], BF16)
                        On_r = Ring(sb, "Ons", 2, [64, 512], BF16)
                        for h in range(8):
                            KT, kKT = KT_r.next()
                            QT, kQT = QT_r.next()
                            p.op("sp", lambda e, KT=KT, h=h: e.dma_start(out=KT[:], in_=KsT[h * 64:(h + 1) * 64, :]), writes=[kKT], dma=kKT)
                            p.op("sp", lambda e, QT=QT, h=h: e.dma_start(out=QT[:], in_=QsT[h * 64:(h + 1) * 64, :]), writes=[kQT], dma=kQT)
                            for t in range(4):
                                bl = blocks(t)[::-1]
                                nb = len(bl)
                                pO, kpO = pO_r.next()
                                p.op("pe", lambda e, pO=pO: e.matmul(out=pO[0:64, :], lhsT=zeros[:, 0:64], rhs=m01[:, 0, :],
                                                                      start=True, stop=False), reads=["zeros", "m01"], writes=[kpO])
                                stage = {}
                                carry = {"t": None, "k": None}

                                def stA(i):
                                    kb, N, d = bl[i]
                                    pZ, kpZ = pZ_r.next()
                                    p.op("pe", lambda e: e.matmul(out=pZ[:, 0:N], lhsT=KT[:, kb * 128:(kb + 1) * 128], rhs=QT[:, t * 512:t * 512 + N],
                                                                  start=True, stop=True), reads=[kKT, kQT], writes=[kpZ])
                                    E, kE = E_r.next()
                                    p.op("act", lambda e: e.activation(out=E[:, 0:N], in_=pZ[:, 0:N], func=AF.Exp), reads=[kpZ], writes=[kE])
                                    SPt, kSP = SP_r.next()
                                    p.op("act", lambda e: e.activation(out=SPt[:, 0:N], in_=E[:, 0:N], func=AF.Ln, bias=1.0), reads=[kE], writes=[kSP])
                                    if d is not None:
                                        SM, kSM = SM_r.next()
                                        p.op("dve", lambda e: e.tensor_tensor(out=SM[:, 0:N], in0=SPt[:, 0:N], in1=m01[:, d, 0:N], op=ALU.mult),
                                             reads=[kSP, "m01"], writes=[kSM])
                                    else:
                                        SM, kSM = SPt, kSP
                                    cprev, kcprev = carry["t"], carry["k"]
                                    stage[i] = (SM, kSM, cprev, kcprev, E, kE)
                                    if i < nb - 1:
                                        cn_, kcn_ = CR_r.next()
                                        if cprev is None:
                                            if N < 512:
                                                p.op("pool", lambda e: e.memset(cn_[:, N:512], 0.0), writes=[kcn_])
                                            p.op("pool", lambda e: e.tensor_copy(out=cn_[:, 0:N], in_=SM[:, 0:N]), reads=[kSM], writes=[kcn_])
                                        else:
                                            if N < 512:
                                                p.op("pool", lambda e: e.tensor_copy(out=cn_[:, N:512], in_=cprev[:, N:512]), reads=[kcprev], writes=[kcn_])
                                            p.op("dve", lambda e: e.tensor_tensor(out=cn_[:, 0:N], in0=cprev[:, 0:N], in1=SM[:, 0:N], op=ALU.add),
                                                 reads=[kcprev, kSM], writes=[kcn_])
                                        carry["t"], carry["k"] = cn_, kcn_

                                def stB(i):
                                    kb, N, d = bl[i]
                                    SM, kSM, cprev, kcprev, E, kE = stage[i]
                                    pL, kpL = pL_r.next()
                                    last = "tri"
                                    if cprev is not None:
                                        last = "carry"
                                    if d is not None:
                                        last = "mask"
                                    p.op("pe", lambda e: e.matmul(out=pL[:, 0:N], lhsT=ntri[:], rhs=SM[:, 0:N], start=True, stop=(last == "tri")),
                                         reads=["ntri", kSM], writes=[kpL])
                                    if cprev is not None:
                                        p.op("pe", lambda e: e.matmul(out=pL[:, 0:N], lhsT=nones[:], rhs=cprev[:, 0:N], start=False, stop=(last == "carry")),
                                             reads=["nones", kcprev], writes=[kpL])
                                    if d is not None:
                                        p.op("pe", lambda e: e.matmul(out=pL[:, 0:N], lhsT=ident[:], rhs=nmS[:, d, 0:N], start=False, stop=True),
                                             reads=["ident", "nmS"], writes=[kpL])
                                    A, kA = A_r.next()
                                    p.op("act", lambda e: e.activation(out=A[:, 0:N], in_=pL[:, 0:N], func=AF.Exp), reads=[kpL], writes=[kA])
                                    p.op("dve", lambda e: e.tensor_tensor(out=A[:, 0:N], in0=A[:, 0:N], in1=E[:, 0:N], op=ALU.mult), reads=[kA, kE], writes=[kA])
                                    stage[i] = (A, kA)

                                def stC(i):
                                    kb, N, d = bl[i]
                                    A, kA = stage.pop(i)
                                    p.op("pe", lambda e: e.matmul(out=pO[0:64, 0:N], lhsT=Vs[:, kb, h * 64:(h + 1) * 64], rhs=A[:, 0:N], start=False, stop=(i == nb - 1)),
                                         reads=["Vsall", kA], writes=[kpO])

                                for it in range(nb + 2):
                                    if it < nb:
                                        stA(it)
                                    if 0 <= it - 1 < nb:
                                        stB(it - 1)
                                    if it - 2 >= 0:
                                        stC(it - 2)
                                    yield
                                On, kOn = On_r.next()
                                p.op("dve", lambda e, On=On, pO=pO: e.tensor_copy(out=On[:], in_=pO[0:64, :]), reads=[kpO], writes=[kOn])
                                p.op("pool", lambda e, On=On, h=h, t=t: e.dma_start(out=OTd[512 + h * 64:512 + (h + 1) * 64, t * 512:(t + 1) * 512], in_=On[:]),
                                     reads=[kOn], writes=[], dma=kOn + "s")

                    gens = [mla_gen(), sb_gen()]
                    while gens:
                        for g_ in list(gens):
                            try:
                                next(g_)
                            except StopIteration:
                                gens.remove(g_)
                    p.barrier()
                    if stop_after == "B":
                        p.finish_wait("sp"); p.emit(top); return nc
            sc = ExitStack()
            with sc:
                sb, ps = mk_alloc(sc)
                H = sb("H", [128, 16, D], F32)
                posm = sb("posm", [128, 16, NE], F32)
                maskb = sb("maskb", [128, 16, NE], BF16)
                gj = sb("gj", [128, 16, 4], F32)
                slI = sb("slI", [128, 16, 4], I32)
                Gt = sb("Gt", [128, 16, NE], F32)
                bgT = sb("bgT", [128, 512], F32)
                s1 = ExitStack()
                with s1:
                    sb1, ps1 = mk_alloc(s1)
                    gbc = sb1("gbc", [128, D], F32)
                    junk = sb1("junkC", [128, D], BF16)
                    Wo = sb1("Wo", [128, 8, D], BF16)
                    p.op("pool", lambda e: e.dma_start(out=Wo[:], in_=w_o.rearrange("(c p) n -> p c n", p=128)), writes=["Wo"], dma="Wo")
                    Wr = sb1("Wr", [128, 8, NE], F32)
                    p.op("sp", lambda e: e.dma_start(out=Wr[:], in_=w_router.rearrange("(c p) n -> p c n", p=128)), writes=["Wr"], dma="Wr")
                    brb = sb1("brb", [128, NE], F32)
                    p.op("sp", lambda e: e.dma_start(out=brb[:], in_=b_router.partition_broadcast(128)), writes=["brb"], dma="brb")
                    p.op("sp", lambda e: e.dma_start(out=gbc[:], in_=g_moe.partition_broadcast(128)), writes=["gbc"], dma="gbc")
                    bgl = sb1("bgl", [128, 4, 128], F32)
                    p.op("sp", lambda e: e.dma_start(out=bgl[:], in_=b_gu.rearrange("(a r) q -> r a q", r=128)), writes=["bgl"], dma="bgl")
                    pTf_r = Ring(ps1, "pTf", 1, [128, 4, 128], F32)
                    pbt, kpbt = pTf_r.next()
                    for a in range(4):
                        p.op("pe", lambda e, a=a: e.transpose(out=pbt[:, a, :], in_=bgl[:, a, :], identity=identf[:]), reads=["bgl", "identf"], writes=[kpbt])
                    p.op("dve", lambda e: e.tensor_copy(out=bgT[:].rearrange("p (a q) -> p a q", a=4), in_=pbt[:]), reads=[kpbt], writes=["bgT"])

                    OT_r = Ring(sb1, "OTl", 1, [128, 8, 512], BF16)
                    sq_r = Ring(sb1, "sqC", 2, [128, 512], BF16)
                    pss_r = Ring(ps1, "pssC", 1, [128, 512], F32)
                    rb_r = Ring(sb1, "rbC", 2, [128, 512], F32)
                    MX = sb1("MX", [128, 8, 512], BF16)
                    pA_r = Ring(ps1, "pAC", 2, [128, 512], F32)
                    xr_r = Ring(sb1, "xrC", 2, [128, D], F32)
                    ssc_r = Ring(sb1, "sscC", 2, [128, 1], F32)
                    u32_r = Ring(sb1, "u32C", 2, [128, D], F32)
                    uhT_r = Ring(sb1, "uhT", 2, [128, 8, 128], BF16)
                    tris = sb1("tris", [128, 128], BF16)
                    tmpf1 = sb1("tmpf1", [128, 128], F32)
                    p.op("pool", lambda e: e.memset(tmpf1[:], 1.0), writes=["tmpf1"])
                    p.op("pool", lambda e: e.affine_select(out=tmpf1[:], in_=tmpf1[:], pattern=[[1, 128]], compare_op=ALU.is_gt, fill=0.0,
                                                            base=0, channel_multiplier=-1), reads=["tmpf1"], writes=["tmpf1"])
                    p.op("dve", lambda e: e.tensor_copy(out=tris[:], in_=tmpf1[:]), reads=["tmpf1"], writes=["tris"])
                    gtmp_r = Ring(sb1, "gtmpC", 2, [128, NE], F32)
                    xnb_r = Ring(sb1, "xnbC", 2, [128, D], BF16)
                    ecap_i = sb1("ecap_i", [128, NE], I32)
                    ecap = sb1("ecap", [128, NE], F32)
                    p.op("pool", lambda e: e.iota(ecap_i[:], pattern=[[CAP, NE]], base=0, channel_multiplier=0), writes=["ecap_i"])
                    p.op("dve", lambda e: e.tensor_copy(out=ecap[:], in_=ecap_i[:]), reads=["ecap_i"], writes=["ecap"])
                    pq_r = Ring(sb1, "pq", 2, [128, NE], F32)
                    oh = sb1("oh", [128, NE], F32)
                    pr = sb1("pr", [128, NE], F32)
                    slf_r = Ring(sb1, "slf", 2, [128, 4], F32)
                    ulo_r = Ring(sb1, "uloC", 2, [128, D], BF16)
                    uloT_r = Ring(sb1, "uloT", 2, [128, 8, 128], BF16)
                    pTb_r = Ring(ps1, "pTb", 2, [128, 8, 128], BF16)
                    Wrh = sb1("Wrh", [128, 8, NE], BF16)
                    Wrl = sb1("Wrl", [128, 8, NE], BF16)
                    p.op("dve", lambda e: e.tensor_copy(out=Wrh[:], in_=Wr[:]), reads=["Wr"], writes=["Wrh"])
                    p.op("dve", lambda e: e.tensor_tensor(out=Wrl[:], in0=Wr[:], in1=Wrh[:], op=ALU.subtract), reads=["Wr", "Wrh"], writes=["Wrl"])
                    pLg_r = Ring(ps1, "pLg", 2, [128, 2, NE], F32)
                    lg_r = Ring(sb1, "lgC", 2, [128, NE], F32)
                    mx8_r = Ring(sb1, "mx8C", 2, [128, 8], F32)
                    msk_r = Ring(sb1, "mskC", 2, [128, NE], F32)
                    ex_r = Ring(sb1, "exC", 2, [128, NE], F32)
                    sm_r = Ring(sb1, "smC", 2, [128, 1], F32)
                    otv = OTd.rearrange("(c p) n -> p c n", p=128)
                    if stop_after == "C1a":
                        p.barrier()
                        p.op("sp", lambda e: e.dma_start(out=dbgH.rearrange("(t p) n -> p t n", p=128), in_=H[:]), reads=["H%d" % i_ for i_ in range(16)], dma="dbgH")
                        p.op("sp", lambda e: e.dma_start(out=dbgG.rearrange("(t p) n -> p t n", p=128), in_=Gt[:]), reads=["Gt%d" % i_ for i_ in range(16)], dma="dbgG")
                        p.finish_wait("sp"); p.emit(top); return nc
                    for G in range(4):
                        OT, kOT = OT_r.next()
                        p.op("sp", lambda e, OT=OT, G=G: e.dma_start(out=OT[:], in_=otv[:, :, G * 512:(G + 1) * 512]), writes=[kOT], dma=kOT)
                        rbs = []
                        for grp in range(2):
                            pss, kpss = pss_r.next()
                            for i in range(4):
                                c = grp * 4 + i
                                sq, ksq = sq_r.next()
                                p.op("act", lambda e, sq=sq, OT=OT, c=c: e.activation(out=sq[:], in_=OT[:, c, :], func=AF.Square), reads=[kOT], writes=[ksq])
                                p.op("pe", lambda e, sq=sq, pss=pss, i=i: e.matmul(out=pss[:], lhsT=ones[:], rhs=sq[:], start=(i == 0), stop=(i == 3)),
                                     reads=[ksq, "ones"], writes=[kpss])
                            rb, krb = rb_r.next()
                            rstd_from(rb[:], pss[:], 512, [kpss], [krb])
                            rbs.append((rb, krb))
                        for c in range(8):
                            rb, krb = rbs[c // 4]
                            p.op("dve", lambda e, c=c, rb=rb, OT=OT: e.scalar_tensor_tensor(out=MX[:, c, :], in0=OT[:, c, :], scalar=gcol[:, 5 + c:6 + c], in1=rb[:],
                                                                                        op0=ALU.mult, op1=ALU.mult),
                                 reads=[kOT, "gcol", krb], writes=["MX"])
                        def c1_tile(G, sub):
                            tile = G * 4 + sub
                            b_ = sub % 2
                            uhT, kuhT = uhT_r.tiles[b_], uhT_r.keys[b_]
                            uloT, kuloT = uloT_r.tiles[b_], uloT_r.keys[b_]
                            pq, kpq = pq_r.tiles[b_], pq_r.keys[b_]
                            slf, kslf = slf_r.tiles[b_], slf_r.keys[b_]
                            pLg2, kpLg = pLg_r.tiles[b_], pLg_r.keys[b_]
                            pLg = pLg2[:, 0, :]
                            pPos = pLg2[:, 1, :]
                            yield
                            xr, kxr = xr_r.next()
                            yield
                            p.op("sp", lambda e, xr=xr, tile=tile: e.dma_start(out=xr[:], in_=xo[tile * 128:(tile + 1) * 128, :]), writes=[kxr], dma=kxr)
                            yield
                            for half in range(2):
                                pA, kpA = pA_r.next()
                                for c in range(8):
                                    p.op("pe", lambda e, c=c, pA=pA, sub=sub, half=half: e.matmul(out=pA[:], lhsT=MX[:, c, sub * 128:(sub + 1) * 128],
                                                                                              rhs=Wo[:, c, half * 512:(half + 1) * 512], start=(c == 0), stop=(c == 7)),
                                         reads=["MX", "Wo"], writes=[kpA])
                                p.op("dve", lambda e, pA=pA, xr=xr, tile=tile, half=half: e.tensor_tensor(out=H[:, tile, half * 512:(half + 1) * 512], in0=pA[:],
                                                                                                      in1=xr[:, half * 512:(half + 1) * 512], op=ALU.add),
                                     reads=[kpA, kxr], writes=["H%d" % tile])
                            yield
                            yield
                            ssc, kss = ssc_r.next()
                            yield
                            p.op("act", lambda e, ssc=ssc, tile=tile: e.activation(out=junk[:], in_=H[:, tile, :], func=AF.Square, accum_out=ssc[:]),
                                 reads=["H%d" % tile], writes=["junkC", kss])
                            yield
                            rstd_from(ssc[:], ssc[:], D, [kss], [kss])
                            yield
                            u32, ku = u32_r.next()
                            yield
                            p.op("dve", lambda e, ssc=ssc, u32=u32, tile=tile: e.scalar_tensor_tensor(out=u32[:], in0=H[:, tile, :], scalar=ssc[:, 0:1], in1=gbc[:],
                                                                                                  op0=ALU.mult, op1=ALU.mult),
                                 reads=["H%d" % tile, kss, "gbc"], writes=[ku])
                            yield
                            xnb, kuhi = xnb_r.next()
                            yield
                            ulo, kulo = ulo_r.next()
                            yield
                            p.op("act", lambda e: e.copy(out=xnb[:], in_=u32[:]), reads=[ku], writes=[kuhi])
                            yield
                            p.op("pool", lambda e: e.dma_start(out=Ud[tile * 128:(tile + 1) * 128, :], in_=xnb[:]), reads=[kuhi], dma=kuhi + "s")
                            yield
                            p.op("dve", lambda e: e.tensor_tensor(out=ulo[:], in0=u32[:], in1=xnb[:], op=ALU.subtract), reads=[ku, kuhi], writes=[kulo])
                            yield
                            pT, kpT = pTb_r.next()
                            yield
                            for c in range(8):
                                p.op("pe", lambda e: e.transpose(out=pT[:, c, :], in_=xnb[:, c * 128:(c + 1) * 128], identity=ident[:]), reads=[kuhi, "ident"], writes=[kpT])
                            yield
                            p.op("dve", lambda e: e.tensor_copy(out=uhT[:], in_=pT[:]), reads=[kpT], writes=[kuhT])
                            yield
                            pT2, kpT2 = pTb_r.next()
                            yield
                            for c in range(8):
                                p.op("pe", lambda e: e.transpose(out=pT2[:, c, :], in_=ulo[:, c * 128:(c + 1) * 128], identity=ident[:]), reads=[kulo, "ident"], writes=[kpT2])
                            yield
                            p.op("act", lambda e: e.copy(out=uloT[:], in_=pT2[:]), reads=[kpT2], writes=[kuloT])
                            yield
                            n_ = 0
                            yield
                            for (A_, kA_, W_, kW_) in (("hi", kuhT, Wrh, "Wrh"), ("lo", kuloT, Wrh, "Wrh"), ("hi", kuhT, Wrl, "Wrl")):
                                for k in range(8):
                                    lh = uhT[:, k, :] if A_ == "hi" else uloT[:, k, :]
                                    p.op("pe", lambda e: e.matmul(out=pLg, lhsT=lh, rhs=W_[:, k, :], start=(n_ == 0), stop=(n_ == 23)),
                                         reads=[kA_, kW_], writes=[kpLg])
                                    n_ += 1
                            yield
                            lg, klg = lg_r.next()
                            yield
                            p.op("dve", lambda e, lg=lg: e.tensor_tensor(out=lg[:], in0=pLg, in1=brb[:], op=ALU.add), reads=[kpLg, "brb"], writes=[klg])
                            yield
                            mx8, kmx = mx8_r.next()
                            yield
                            p.op("dve", lambda e, lg=lg, mx8=mx8: e.max(out=mx8[:], in_=lg[:]), reads=[klg], writes=[kmx])
                            yield
                            msk, kmsk = msk_r.next()
                            yield
                            p.op("dve", lambda e, lg=lg, mx8=mx8, msk=msk: e.tensor_scalar(out=msk[:], in0=lg[:], scalar1=mx8[:, 3:4], scalar2=None, op0=ALU.is_ge),
                                 reads=[klg, kmx], writes=[kmsk])
                            yield
                            p.op("dve", lambda e, mx8=mx8: e.tensor_scalar(out=mx8[:, 7:8], in0=mx8[:, 0:1], scalar1=-1.0, scalar2=None, op0=ALU.mult),
                                 reads=[kmx, kmsk], writes=[kmx])
                            yield
                            ex, kex = ex_r.next()
                            yield
                            p.op("act", lambda e, lg=lg, mx8=mx8, ex=ex: e.activation(out=ex[:], in_=lg[:], func=AF.Exp, bias=mx8[:, 7:8]), reads=[klg, kmx], writes=[kex])
                            yield
                            sm_, ksm = sm_r.next()
                            yield
                            p.op("dve", lambda e, ex=ex, msk=msk: e.tensor_tensor(out=ex[:], in0=ex[:], in1=msk[:], op=ALU.mult), reads=[kex, kmsk], writes=[kex])
                            yield
                            p.op("dve", lambda e, ex=ex, sm_=sm_: e.reduce_sum(out=sm_[:], in_=ex[:], axis=mybir.AxisListType.X), reads=[kex], writes=[ksm])
                            yield
                            p.op("dve", lambda e, sm_=sm_: e.reciprocal(out=sm_[:], in_=sm_[:]), reads=[ksm], writes=[ksm])
                            yield
                            p.op("dve", lambda e, ex=ex, sm_=sm_, tile=tile: e.tensor_scalar(out=Gt[:, tile, :], in0=ex[:], scalar1=sm_[:, 0:1], scalar2=None, op0=ALU.mult),
                                 reads=[kex, ksm], writes=["Gt%d" % tile])
                            yield
                            p.op("dve", lambda e: e.tensor_copy(out=maskb[:, tile, :], in_=msk[:]), reads=[kmsk], writes=["maskb%d" % tile])
                            yield
                            gtmp, kgtmp = gtmp_r.next()
                            yield
                            p.op("pe", lambda e: e.matmul(out=pPos, lhsT=tris[:], rhs=maskb[:, tile, :], start=True, stop=(tile == 0)),
                                 reads=["tris", "maskb%d" % tile], writes=[kpLg])
                            yield
                            for j_ in range(tile):
                                p.op("pe", lambda e: e.matmul(out=pPos, lhsT=ones[:], rhs=maskb[:, j_, :], start=False, stop=(j_ == tile - 1)),
                                     reads=["ones", "maskb%d" % j_], writes=[kpLg])
                            yield
                            p.op("dve", lambda e: e.scalar_tensor_tensor(out=gtmp[:], in0=pPos, scalar=1.0, in1=msk[:], op0=ALU.add, op1=ALU.mult),
                                 reads=[kpLg, kmsk, kgtmp], writes=[kgtmp])
                            yield
                            p.op("dve", lambda e: e.tensor_scalar(out=posm[:, tile, :], in0=gtmp[:], scalar1=-1.0, scalar2=None, op0=ALU.add),
                                 reads=[kgtmp], writes=["posm%d" % tile])
                            yield
                            p.op("dve", lambda e: e.scalar_tensor_tensor(out=pq[:], in0=pPos, scalar=float(CAP - 1), in1=ecap[:], op0=ALU.min, op1=ALU.add),
                                 reads=[kpLg, "ecap"], writes=[kpq])
                            yield
                            yield
                            p.op("dve", lambda e: e.scalar_tensor_tensor(out=gtmp[:], in0=pPos, scalar=float(CAP) - 0.5, in1=Gt[:, tile, :], op0=ALU.is_lt, op1=ALU.mult),
                                 reads=[kpLg, "Gt%d" % tile, kgtmp], writes=[kgtmp])
                            yield
                            for j_ in range(4):
                                p.op("dve", lambda e: e.tensor_scalar(out=oh[:], in0=lg[:], scalar1=mx8[:, j_:j_ + 1], scalar2=None, op0=ALU.is_equal),
                                     reads=[klg, kmx], writes=["oh"])
                                p.op("dve", lambda e: e.tensor_tensor(out=pr[:], in0=oh[:], in1=pq[:], op=ALU.mult), reads=["oh", kpq], writes=["pr"])
                                p.op("dve", lambda e: e.reduce_sum(out=slf[:, j_:j_ + 1], in_=pr[:], axis=mybir.AxisListType.X), reads=["pr"], writes=[kslf])
                                p.op("dve", lambda e: e.tensor_tensor(out=pr[:], in0=oh[:], in1=gtmp[:], op=ALU.mult), reads=["oh", kgtmp, "pr"], writes=["pr"])
                                p.op("dve", lambda e: e.reduce_sum(out=gj[:, tile, j_:j_ + 1], in_=pr[:], axis=mybir.AxisListType.X), reads=["pr"], writes=["gj%d" % tile])
                            yield
                            p.op("dve", lambda e: e.tensor_copy(out=slI[:, tile, :], in_=slf[:]), reads=[kslf], writes=["slI%d" % tile])

                        for pair in range(2):
                            gens = [c1_tile(G, pair * 2), c1_tile(G, pair * 2 + 1)]
                            while gens:
                                for g_ in list(gens):
                                    try:
                                        next(g_)
                                    except StopIteration:
                                        gens.remove(g_)
                    p.barrier()
                    if stop_after == "C1":
                        if debug:
                            p.op("sp", lambda e: e.dma_start(out=dbgS.rearrange("(t p) n -> p t n", p=128), in_=slI[:]), reads=["slI%d" % i_ for i_ in range(16)], dma="dbgS")
                            p.op("sp", lambda e: e.dma_start(out=dbgJ.rearrange("(t p) n -> p t n", p=128), in_=gj[:]), reads=["gj%d" % i_ for i_ in range(16)], dma="dbgJ")
                            p.op("sp", lambda e: e.dma_start(out=dbgH.rearrange("(t p) n -> p t n", p=128), in_=H[:]), reads=["H%d" % i_ for i_ in range(16)], dma="dbgH")
                            p.op("sp", lambda e: e.dma_start(out=dbgG.rearrange("(t p) n -> p t n", p=128), in_=Gt[:]), reads=["Gt%d" % i_ for i_ in range(16)], dma="dbgG")
                        p.finish_wait("sp"); p.emit(top); return nc
                s2 = ExitStack()
                with s2:
                    sb2, ps2 = mk_alloc(s2)
                    W_r = Ring(sb2, "Wx", 6, [128, 8, 512], BF16)
                    bd_r = Ring(sb2, "bdn", 2, [1, D], BF16)
                    pGL_r = Ring(ps2, "pGL", 4, [128, 512], F32)
                    pA_r = Ring(ps2, "pA", 2, [128, 512], F32)
                    pTs = ps2("pTs", [128, 8, 128], BF16)
                    ptk = ps2("ptk", [128, 8], F32)
                    gl_r = Ring(sb2, "gl", 1, [128, CAP], F32)
                    sg_r = Ring(sb2, "sg", 1, [128, CAP], F32)
                    Sel = sb2("Sel", [128, 16, CAP], BF16)
                    Xe_r = Ring(sb2, "Xe", 2, [128, NS, D], BF16)
                    XeT = sb2("XeT", [128, 8, CAP], BF16)
                    aT = sb2("aTs", [128, 8, CAP], BF16)
                    Yst_r = Ring(sb2, "Yst", 2, [128, D], F32)
                    tks = sb2("tks", [128, 8], F32)
                    tkf = sb2("tkf", [128, 4], F32)
                    tkI_r = Ring(sb2, "tkI", 2, [128, 4], I32)
                    iota_i = sb2("iota_i", [128, CAP], I32)
                    iota_f = sb2("iota_f", [128, CAP], F32)
                    p.op("pool", lambda e: e.iota(iota_i[:], pattern=[[1, CAP]], base=0, channel_multiplier=0), writes=["iota_i"])
                    p.op("dve", lambda e: e.tensor_copy(out=iota_f[:], in_=iota_i[:]), reads=["iota_i"], writes=["iota_f"])
                    tid = sb2("tid", [128, 16], I32)
                    tidx = sb2("tidx", [128, 16], I32)
                    tidhl = sb2("tidhl", [128, 16, 2], BF16)
                    p.op("pool", lambda e: e.iota(tid[:], pattern=[[128, 16]], base=0, channel_multiplier=1), writes=["tid"])
                    p.op("dve", lambda e: e.tensor_scalar(out=tidx[:], in0=tid[:], scalar1=6, scalar2=None, op0=ALU.arith_shift_right), reads=["tid"], writes=["tidx"])
                    p.op("dve", lambda e: e.tensor_copy(out=tidhl[:, :, 0], in_=tidx[:]), reads=["tidx"], writes=["tidhl"])
                    p.op("dve", lambda e: e.tensor_scalar(out=tidx[:], in0=tid[:], scalar1=63, scalar2=None, op0=ALU.bitwise_and), reads=["tid", "tidx", "tidhl"], writes=["tidx"])
                    p.op("dve", lambda e: e.tensor_copy(out=tidhl[:, :, 1], in_=tidx[:]), reads=["tidx", "tidhl"], writes=["tidhl"])
                    bg3 = bgT[:].rearrange("p (e c) -> p e c", c=16)
                    p.op("dve", lambda e: e.tensor_scalar(out=bg3[:, :, 8:16], in0=bg3[:, :, 8:16], scalar1=1.0, scalar2=None, op0=ALU.add),
                         reads=["bgT"], writes=["bgT"])

                    def load_w(src, slot):
                        Wt, kW = W_r.tiles[slot], W_r.keys[slot]
                        p.op("pool", lambda e: e.dma_start(out=Wt[:], in_=src.rearrange("(c p) n -> p c n", p=128)), writes=[kW], dma=kW)
                        return Wt, kW

                    def load_bd(ex_):
                        bd, kbd = bd_r.next()
                        p.op("pool", lambda e: e.dma_start(out=bd[:], in_=b_dn[ex_:ex_ + 1, :]), writes=[kbd], dma=kbd)
                        return bd, kbd

                    xe_of = {}

                    def sel_build(ex_, tiles):
                        for tile in tiles:
                            p.op("dve", lambda e: e.tensor_scalar(out=Sel[:, tile, :], in0=iota_f[:], scalar1=posm[:, tile, ex_:ex_ + 1], scalar2=None, op0=ALU.is_equal),
                                 reads=["iota_f", "posm%d" % tile], writes=["Sel%d" % tile])

                    def dispatch(ex_, build=True):
                        if build:
                            sel_build(ex_, range(16))
                        for s_ in range(NS):
                            for tile in range(16):
                                p.op("pe", lambda e: e.matmul(out=ptk[:, 2 * s_:2 * s_ + 2], lhsT=Sel[:, tile, s_ * 128:(s_ + 1) * 128], rhs=tidhl[:, tile, :],
                                                              start=(tile == 0), stop=(tile == 15)), reads=["Sel%d" % tile, "tidhl"], writes=["ptk"])
                        p.op("dve", lambda e: e.tensor_copy(out=tks[:, 0:2 * NS], in_=ptk[:, 0:2 * NS]), reads=["ptk"], writes=["tks"])
                        tk3 = tks[:, 0:2 * NS].rearrange("p (s t) -> p s t", t=2)
                        p.op("dve", lambda e: e.scalar_tensor_tensor(out=tkf[:, 0:NS], in0=tk3[:, :, 0], scalar=64.0, in1=tk3[:, :, 1], op0=ALU.mult, op1=ALU.add),
                             reads=["tks"], writes=["tkf"])
                        tkI, ktkI = tkI_r.next()
                        p.op("dve", lambda e: e.tensor_copy(out=tkI[:, 0:NS], in_=tkf[:, 0:NS]), reads=["tkf"], writes=[ktkI])
                        Xe, kXe = Xe_r.next()
                        for s_ in range(NS):
                            p.op("pool", lambda e: e.indirect_dma_start(out=Xe[:, s_, :], out_offset=None, in_=Ud[:, :],
                                                                         in_offset=bass.IndirectOffsetOnAxis(ap=tkI[:, s_:s_ + 1], axis=0)),
                                 reads=[ktkI], writes=[kXe], dma=kXe)
                        xe_of[ex_] = (Xe, kXe)

                    def transposes_s(ex_, s_):
                        Xe, kXe = xe_of[ex_]
                        for k in range(8):
                            p.op("pe", lambda e: e.transpose(out=pTs[:, k, :], in_=Xe[:, s_, k * 128:(k + 1) * 128], identity=ident[:]),
                                 reads=[kXe, "ident"], writes=["pTs"])
                        if s_ % 2 == 0:
                            p.op("act", lambda e: e.copy(out=XeT[:, :, s_ * 128:(s_ + 1) * 128], in_=pTs[:]), reads=["pTs"], writes=["XeT"])
                        else:
                            p.op("dve", lambda e: e.tensor_copy(out=XeT[:, :, s_ * 128:(s_ + 1) * 128], in_=pTs[:]), reads=["pTs"], writes=["XeT"])
                        if s_ == NS - 1:
                            xe_of.pop(ex_)

                    def transposes(ex_):
                        for s_ in range(NS):
                            transposes_s(ex_, s_)

                    def gu_stage(ex_, st, Wg, kWg, Wl, kWl, sel_for=None):
                        for mc in range(4):
                            if sel_for is not None:
                                sel_build(sel_for, range(mc * 4, mc * 4 + 4))
                            c = st * 4 + mc
                            pG, kpG = pGL_r.next()
                            pLn, kpLn = pGL_r.next()
                            for k in range(8):
                                p.op("pe", lambda e: e.matmul(out=pG[:, 0:CAP], lhsT=Wg[:, k, mc * 128:(mc + 1) * 128], rhs=XeT[:, k, :],
                                                              start=(k == 0), stop=(k == 7)), reads=[kWg, "XeT"], writes=[kpG])
                            for k in range(8):
                                p.op("pe", lambda e: e.matmul(out=pLn[:, 0:CAP], lhsT=Wl[:, k, mc * 128:(mc + 1) * 128], rhs=XeT[:, k, :],
                                                              start=(k == 0), stop=(k == 7)), reads=[kWl, "XeT"], writes=[kpLn])
                            gl, kgl = gl_r.next()
                            sg, ksg = sg_r.next()
                            bgc = ex_ * 16 + c
                            blc = ex_ * 16 + 8 + c
                            kaT = "aT_%d" % st
                            p.op("dve", lambda e: e.tensor_scalar(out=gl[:], in0=pG[:, 0:CAP], scalar1=bgT[:, bgc:bgc + 1], scalar2=7.0, op0=ALU.add, op1=ALU.min),
                                 reads=[kpG, "bgT"], writes=[kgl])
                            p.op("act", lambda e: e.activation(out=sg[:], in_=gl[:], func=AF.Sigmoid, scale=1.702), reads=[kgl], writes=[ksg])
                            p.op("dve", lambda e: e.tensor_tensor(out=sg[:], in0=sg[:], in1=gl[:], op=ALU.mult), reads=[kgl, ksg], writes=[ksg])
                            p.op("dve", lambda e: e.tensor_scalar(out=gl[:], in0=pLn[:, 0:CAP], scalar1=bgT[:, blc:blc + 1], scalar2=-6.0, op0=ALU.add, op1=ALU.max),
                                 reads=[kpLn, "bgT", kgl], writes=[kgl])
                            p.op("dve", lambda e: e.scalar_tensor_tensor(out=aT[:, c, :], in0=gl[:], scalar=8.0, in1=sg[:], op0=ALU.min, op1=ALU.mult),
                                 reads=[ksg, kgl], writes=[kaT])

                    def dn_stage(ex_, d0, d1, bd, kbd, nxt=None):
                        for s_ in range(NS):
                            if nxt is not None:
                                transposes_s(nxt, s_)
                            Yst, kY = Yst_r.next()
                            for half in range(2):
                                Wd, kWd = (d0, d1)[half]
                                pA, kpA = pA_r.next()
                                for c in range(8):
                                    p.op("pe", lambda e: e.matmul(out=pA[:], lhsT=aT[:, c, s_ * 128:(s_ + 1) * 128], rhs=Wd[:, c, :], start=(c == 0), stop=False),
                                         reads=["aT_%d" % (c // 4), kWd], writes=[kpA])
                                p.op("pe", lambda e: e.matmul(out=pA[:], lhsT=ones[0:1, :], rhs=bd[0:1, half * 512:(half + 1) * 512], start=False, stop=True),
                                     reads=["ones", kbd], writes=[kpA])
                                p.op("act", lambda e: e.copy(out=Yst[:, half * 512:(half + 1) * 512], in_=pA[:]), reads=[kpA], writes=[kY])
                            r0 = ex_ * CAP + s_ * 128
                            p.op("sp", lambda e: e.dma_start(out=Yd[r0:r0 + 128, :], in_=Yst[:]), reads=[kY], dma=kY + "s")

                    def loads_gl0(ex_):
                        return load_w(w_gu[ex_, :, 0:512], 0), load_w(w_gu[ex_, :, 1024:1536], 1)

                    def loads_gl1(ex_):
                        return load_w(w_gu[ex_, :, 512:1024], 2), load_w(w_gu[ex_, :, 1536:2048], 3)

                    def loads_d(ex_):
                        return load_w(w_dn[ex_, :, 0:512], 4), load_w(w_dn[ex_, :, 512:1024], 5), load_bd(ex_)

                    g0, l0 = loads_gl0(0)
                    g1, l1 = loads_gl1(0)
                    d0, d1, (bd, kbd) = loads_d(0)
                    dispatch(0)
                    dispatch(1)
                    transposes(0)
                    for ex_ in range(NE):
                        gu_stage(ex_, 0, g0[0], g0[1], l0[0], l0[1], sel_for=(ex_ + 2 if ex_ + 2 < NE else None))
                        if ex_ + 1 < NE:
                            g0n, l0n = loads_gl0(ex_ + 1)
                        gu_stage(ex_, 1, g1[0], g1[1], l1[0], l1[1])
                        if ex_ + 2 < NE:
                            dispatch(ex_ + 2, build=False)
                        if ex_ + 1 < NE:
                            g1n, l1n = loads_gl1(ex_ + 1)
                        dn_stage(ex_, d0, d1, bd, kbd, nxt=(ex_ + 1 if ex_ + 1 < NE else None))
                        if ex_ + 1 < NE:
                            d0, d1, (bd, kbd) = loads_d(ex_ + 1)
                            g0, l0, g1, l1 = g0n, l0n, g1n, l1n
                    p.barrier()
                    Yg_r = Ring(sb2, "Yg", 4, [128, D], F32)
                    for tile in range(16):
                        hk = "H%d" % tile
                        for j_ in range(4):
                            Yg, kYg = Yg_r.next()
                            p.op("pool", lambda e: e.indirect_dma_start(out=Yg[:, :], out_offset=None, in_=Yd[:, :],
                                                                         in_offset=bass.IndirectOffsetOnAxis(ap=slI[:, tile, j_:j_ + 1], axis=0)),
                                 reads=["slI%d" % tile], writes=[kYg], dma=kYg)
                            p.op("dve", lambda e: e.scalar_tensor_tensor(out=H[:, tile, :], in0=Yg[:], scalar=gj[:, tile, j_:j_ + 1], in1=H[:, tile, :], op0=ALU.mult, op1=ALU.add),
                                 reads=[kYg, "gj%d" % tile, hk], writes=[hk])
                    p.barrier()
                    if stop_after == "C2":
                        if debug:
                            p.op("sp", lambda e: e.dma_start(out=dbgH.rearrange("(t p) n -> p t n", p=128), in_=H[:]), reads=["H%d" % i_ for i_ in range(16)], dma="dbgH")
                            p.op("sp", lambda e: e.dma_start(out=dbgG.rearrange("(t p) n -> p t n", p=128), in_=Gt[:]), reads=["Gt%d" % i_ for i_ in range(16)], dma="dbgG")
                        p.finish_wait("sp"); p.emit(top); return nc
                s3 = ExitStack()
                with s3:
                    sb3, ps3 = mk_alloc(s3)
                    gbc = sb3("gbc3", [128, D], F32)
                    junk = sb3("junkC3", [128, D], BF16)
                    Wpg = sb3("Wpg", [128, 8, D], BF16)
                    Wpp = sb3("Wpp", [128, 2, D], BF16)
                    p.op("pool", lambda e: e.dma_start(out=Wpg[:], in_=w_pg.rearrange("(c p) n -> p c n", p=128)), writes=["Wpg"], dma="Wpg")
                    p.op("pool", lambda e: e.dma_start(out=Wpp[:], in_=w_pp.rearrange("(c p) n -> p c n", p=128)), writes=["Wpp"], dma="Wpp")
                    gfin = sb3("gfin", [128, D], F32)
                    p.op("sp", lambda e: e.dma_start(out=gbc[:], in_=g_ple.partition_broadcast(128)), writes=["gbc"], dma="gbc3")
                    p.op("sp", lambda e: e.dma_start(out=gfin[:], in_=g_final.partition_broadcast(128)), writes=["gfin"], dma="gfin")
                    ss3_r = Ring(sb3, "ss3", 2, [128, 1], F32)
                    u3_r = Ring(sb3, "u3", 2, [128, D], BF16)
                    pT3_r = Ring(ps3, "pT3", 2, [128, 8, 128], BF16)
                    u3T_r = Ring(sb3, "u3T", 2, [128, 8, 128], BF16)
                    pp_r = Ring(sb3, "ppl", 2, [128, 256], F32)
                    ppb_r = Ring(sb3, "ppb", 2, [128, 256], BF16)
                    ppT_r = Ring(sb3, "ppT", 2, [128, 2, 128], BF16)
                    pg_r = Ring(ps3, "pg3", 2, [128, 512], F32)
                    pj_r = Ring(ps3, "pj3", 2, [128, 512], F32)
                    sg3_r = Ring(sb3, "sg3", 2, [128, 512], F32)
                    o_r = Ring(sb3, "o3", 2, [128, D], F32)
                    def c3_tile(tile):
                        hk = "H%d" % tile
                        yield
                        ss3, kss = ss3_r.next()
                        yield
                        p.op("act", lambda e, ss3=ss3, tile=tile: e.activation(out=junk[:], in_=H[:, tile, :], func=AF.Square, accum_out=ss3[:]), reads=[hk], writes=["junkC", kss])
                        yield
                        rstd_from(ss3[:], ss3[:], D, [kss], [kss])
                        yield
                        u3, ku3 = u3_r.next()
                        yield
                        p.op("dve", lambda e, ss3=ss3, u3=u3, tile=tile: e.scalar_tensor_tensor(out=u3[:], in0=H[:, tile, :], scalar=ss3[:, 0:1], in1=gbc[:], op0=ALU.mult, op1=ALU.mult),
                             reads=[hk, kss, "gbc"], writes=[ku3])
                        yield
                        pT, kpT = pT3_r.next()
                        yield
                        for c in range(8):
                            p.op("pe", lambda e, c=c, pT=pT, u3=u3: e.transpose(out=pT[:, c, :], in_=u3[:, c * 128:(c + 1) * 128], identity=ident[:]), reads=[ku3, "ident"], writes=[kpT])
                        yield
                        u3T, ku3T = u3T_r.next()
                        yield
                        p.op("act", lambda e, pT=pT, u3T=u3T: e.copy(out=u3T[:], in_=pT[:]), reads=[kpT], writes=[ku3T])
                        yield
                        pp, kpp = pp_r.next()
                        yield
                        p.op("sp", lambda e, pp=pp, tile=tile: e.dma_start(out=pp[:], in_=po[tile * 128:(tile + 1) * 128, :]), writes=[kpp], dma=kpp)
                        yield
                        ppb, kppb = ppb_r.next()
                        yield
                        p.op("pool", lambda e, pp=pp, ppb=ppb: e.tensor_copy(out=ppb[:], in_=pp[:]), reads=[kpp], writes=[kppb])
                        yield
                        pT2, kpT2 = pT3_r.next()
                        yield
                        for c in range(2):
                            p.op("pe", lambda e, c=c, pT2=pT2, ppb=ppb: e.transpose(out=pT2[:, c, :], in_=ppb[:, c * 128:(c + 1) * 128], identity=ident[:]), reads=[kppb, "ident"], writes=[kpT2])
                        yield
                        ppT, kppT = ppT_r.next()
                        yield
                        p.op("act", lambda e, pT2=pT2, ppT=ppT: e.copy(out=ppT[:], in_=pT2[:, 0:2, :]), reads=[kpT2], writes=[kppT])
                        yield
                        for half in range(2):
                            pg, kpg = pg_r.next()
                            pj, kpj = pj_r.next()
                            for c in range(8):
                                p.op("pe", lambda e, c=c, pg=pg, u3T=u3T, half=half: e.matmul(out=pg[:], lhsT=u3T[:, c, :], rhs=Wpg[:, c, half * 512:(half + 1) * 512], start=(c == 0), stop=(c == 7)),
                                     reads=[ku3T, "Wpg"], writes=[kpg])
                            for c in range(2):
                                p.op("pe", lambda e, c=c, pj=pj, ppT=ppT, half=half: e.matmul(out=pj[:], lhsT=ppT[:, c, :], rhs=Wpp[:, c, half * 512:(half + 1) * 512], start=(c == 0), stop=(c == 1)),
                                     reads=[kppT, "Wpp"], writes=[kpj])
                            sg, ksg = sg3_r.next()
                            p.op("act", lambda e, sg=sg, pg=pg: e.activation(out=sg[:], in_=pg[:], func=AF.Sigmoid), reads=[kpg], writes=[ksg])
                            p.op("dve", lambda e, sg=sg, pj=pj: e.tensor_tensor(out=sg[:], in0=sg[:], in1=pj[:], op=ALU.mult), reads=[ksg, kpj], writes=[ksg])
                            p.op("dve", lambda e, sg=sg, tile=tile, half=half: e.tensor_tensor(out=H[:, tile, half * 512:(half + 1) * 512], in0=H[:, tile, half * 512:(half + 1) * 512], in1=sg[:], op=ALU.add),
                                 reads=[ksg, hk], writes=[hk])
                        yield
                        ss4, kss4 = ss3_r.next()
                        yield
                        p.op("act", lambda e, ss4=ss4, tile=tile: e.activation(out=junk[:], in_=H[:, tile, :], func=AF.Square, accum_out=ss4[:]), reads=[hk], writes=["junkC", kss4])
                        yield
                        rstd_from(ss4[:], ss4[:], D, [kss4], [kss4])
                        yield
                        ot, kot = o_r.next()
                        yield
                        p.op("dve", lambda e, ss4=ss4, ot=ot, tile=tile: e.scalar_tensor_tensor(out=ot[:], in0=H[:, tile, :], scalar=ss4[:, 0:1], in1=gfin[:], op0=ALU.mult, op1=ALU.mult),
                             reads=[hk, kss4, "gfin"], writes=[kot])
                        yield
                        p.op("sp", lambda e, ot=ot, tile=tile: e.dma_start(out=yo[tile * 128:(tile + 1) * 128, :], in_=ot[:]), reads=[kot], writes=["yo"], dma=kot + "s")

                    for pair in range(8):
                        gens = [c3_tile(pair * 2), c3_tile(pair * 2 + 1)]
                        while gens:
                            for g_ in list(gens):
                                try:
                                    next(g_)
                                except StopIteration:
                                    gens.remove(g_)
        p.finish_wait("sp")
        p.emit(top)
    return nc


_CACHE = {}


def _perm(j):
    idx = []
    for t in range(4):
        for blk in ORDER[j]:
            b0 = (8 * t + blk) * 128
            idx.append(np.arange(b0, b0 + 128))
    return np.concatenate(idx)


def kernel(x, p, positions, w_in, g_attn, g_cq, w_uq, g_ckv, w_ukv, g_out_mla, g_out_sb, w_o,
           g_moe, w_router, b_router, w_gu, b_gu, w_dn, b_dn, g_ple, w_ple_gate, w_ple_proj, g_final):
    if "nc" not in _CACHE:
        _CACHE["nc"] = build_program()
    nc = _CACHE["nc"]
    in_maps, perms = make_in_maps(x, p, positions, w_in, g_attn, g_cq, w_uq, g_ckv, w_ukv, g_out_mla, g_out_sb, w_o,
                                  g_moe, w_router, b_router, w_gu, b_gu, w_dn, b_dn, g_ple, w_ple_gate, w_ple_proj, g_final)
    res = run_bass_kernel_spmd(nc, in_maps, core_ids=list(range(8)))
    out = np.empty((4, S, D), np.float32)
    for c in range(8):
        b, j = c // 2, c % 2
        out[b, perms[j]] = np.asarray(res.results[c]["yo"])
    return out


def make_in_maps(x, p, positions, w_in, g_attn, g_cq, w_uq, g_ckv, w_ukv, g_out_mla, g_out_sb, w_o,
                 g_moe, w_router, b_router, w_gu, b_gu, w_dn, b_dn, g_ple, w_ple_gate, w_ple_proj, g_final):
    f = lambda a: np.ascontiguousarray(np.asarray(a))
    x = f(x); p = f(p); positions = f(positions)
    invf = np.zeros((128, 1), np.float32)
    fr = (10000.0 ** (-np.arange(0, 32, 2, dtype=np.float32) / 32.0)).astype(np.float32)
    invf[0:16, 0] = fr
    invf[16:32, 0] = fr
    shared = {
        "invf": invf,
        "w_in": f(w_in[0]), "g_attn": f(g_attn[0:1]), "g_cq": f(g_cq[0:1]), "w_uq": f(w_uq[0]),
        "g_ckv": f(g_ckv[0:1]), "w_ukv": f(w_ukv[0]),
        "g_out": f(np.concatenate([np.asarray(g_out_mla[0]), np.asarray(g_out_sb[0])])[None, :]),
        "w_o": f(w_o[0]), "g_moe": f(g_moe[0:1]), "w_router": f(w_router[0]), "b_router": f(b_router[0:1]),
        "w_gu": f(w_gu[0]), "b_gu": f(np.asarray(b_gu[0]).reshape(NE * 16, 128)), "w_dn": f(w_dn[0]), "b_dn": f(b_dn[0]),
        "g_ple": f(g_ple[0:1]), "w_pg": f(w_ple_gate[0]), "w_pp": f(w_ple_proj[0]), "g_final": f(np.asarray(g_final)[None, :]),
    }
    in_maps = []
    perms = [_perm(0), _perm(1)]
    for c in range(8):
        b, j = c // 2, c % 2
        pm = perms[j]
        qr = np.concatenate([np.arange(blk * 128, blk * 128 + 128) for blk in ORDER[j]]).astype(np.float32)[None, :]
        m = dict(shared)
        m["xa"] = x[b]
        m["xo"] = f(x[b][pm])
        m["po"] = f(p[0, b][pm])
        m["posa"] = f(positions[b:b + 1].astype(np.int32))
        m["poso"] = f(positions[b:b + 1, pm].astype(np.int32))
        m["qrel"] = f(qr)
        in_maps.append(m)
    return in_maps, perms
```

```python
from contextlib import ExitStack
import numpy as np
import concourse.bass as bass
import concourse.mybir as mybir
from concourse.bass_utils import run_bass_kernel_spmd

F32 = mybir.dt.float32
BF16 = mybir.dt.bfloat16
I32 = mybir.dt.int32
AF = mybir.ActivationFunctionType
ALU = mybir.AluOpType

ENGS = ("pe", "act", "dve", "pool", "sp")
S = 4096
D = 1024
NE = 32
ORDER = ([6, 5, 3, 0], [7, 4, 2, 1])
NDIAG = [512, 512, 384, 384, 256, 256, 128, 128]
NEG = -30000.0
EPS = 1e-6


class Prog:
    def __init__(self, nc):
        self.nc = nc
        self.ops = {e: [] for e in ENGS}
        self.vcs = {}
        self.cur = {e: {} for e in ENGS}
        self.last_w = {}
        self.readers = {}
        self.excl = set()

    def op(self, eng, fn, reads=(), writes=(), dma=None):
        rec_ = _Rec()
        fn(rec_)
        assert len(rec_.calls) == 1
        fn = rec_.calls[0]
        clk = ("dma:" + dma) if dma else eng
        deps = []
        reads = list(reads)
        writes = list(writes)
        for k in reads:
            if k in self.excl and k not in writes:
                writes.append(k)
        for k in reads:
            lw = self.last_w.get(k)
            if lw:
                deps.append(lw)
        for k in writes:
            lw = self.last_w.get(k)
            if lw:
                deps.append(lw)
            for c, i in self.readers.get(k, {}).items():
                deps.append((c, i))
        cur = self.cur[eng]
        wmax = {}
        for (c, i) in deps:
            if c == "pe" and eng == "pe" and not dma:
                continue
            if cur.get(c, 0) >= i:
                continue
            wmax[c] = max(wmax.get(c, 0), i)
            for c2, i2 in self.vcs[c][i - 1].items():
                if cur.get(c2, 0) < i2:
                    cur[c2] = i2
            if cur.get(c, 0) < i:
                cur[c] = i
        vc = dict(cur)
        lst = self.vcs.setdefault(clk, [])
        lst.append(vc)
        idx = len(lst)
        vc[clk] = idx
        rec = {"fn": fn, "waits": wmax, "clk": clk, "idx": idx}
        self.ops[eng].append(rec)
        for k in reads:
            self.readers.setdefault(k, {})[clk] = idx
        for k in writes:
            self.last_w[k] = (clk, idx)
            self.readers[k] = {}
        return rec

    def finish_wait(self, eng):
        waits = {}
        for c, l in self.vcs.items():
            if len(l) and self.cur[eng].get(c, 0) < len(l):
                waits[c] = len(l)
                self.cur[eng][c] = len(l)
        self.ops[eng].append({"fn": None, "waits": waits, "clk": None, "idx": None})

    def barrier(self):
        for e in ENGS:
            self.finish_wait(e)
        full = {c: len(l) for c, l in self.vcs.items()}
        for e in ENGS:
            self.cur[e] = dict(full)

    def emit(self, stack):
        nc = self.nc
        waited = {}
        for e in ENGS:
            for r in self.ops[e]:
                for c, i in r["waits"].items():
                    waited.setdefault(c, set()).add(i)
        sems, semval = {}, {}
        for c, l in self.vcs.items():
            if c not in waited:
                continue
            sems[c] = stack.enter_context(nc.semaphore("s_" + c.replace(":", "_")))
            isd = c.startswith("dma:")
            v, m = 0, {}
            for i in range(1, len(l) + 1):
                if isd or i in waited[c]:
                    v += 16 if isd else 1
                    m[i] = v
            semval[c] = m
        block = stack.enter_context(nc.Block())
        engobj = {"pe": "tensor", "act": "scalar", "dve": "vector", "pool": "gpsimd", "sp": "sync"}

        def make(e):
            def body(eng):
                for r in self.ops[e]:
                    for c, i in r["waits"].items():
                        eng.wait_ge(sems[c], semval[c][i])
                    if r["fn"] is None:
                        continue
                    name, a, k = r["fn"]
                    ins = getattr(eng, name)(*a, **k)
                    c, i = r["clk"], r["idx"]
                    if c in sems and i in semval[c]:
                        ins.then_inc(sems[c], 16 if c.startswith("dma:") else 1)
            return body

        for e in ENGS:
            if self.ops[e]:
                getattr(block, engobj[e])(make(e))


class _Rec:
    def __init__(self):
        self.calls = []

    def __getattr__(self, name):
        def f(*a, **k):
            self.calls.append((name, a, k))
        return f


class Ring:
    def __init__(self, alloc, name, n, shape, dtype):
        self.tiles = [alloc("%s%d" % (name, i), shape, dtype) for i in range(n)]
        self.keys = ["%s%d" % (name, i) for i in range(n)]
        self.i = 0

    def next(self):
        t, k = self.tiles[self.i % len(self.tiles)], self.keys[self.i % len(self.tiles)]
        self.i += 1
        return t, k


class _Stop(Exception):
    pass


def build_program(stop_after=None, debug=False):
    nc = bass.Bass("TRN2", target_bir_lowering=False)

    def din(name, shape, dt=F32):
        return nc.dram_tensor(name, list(shape), dt, kind="ExternalInput").ap()

    def dscr(name, shape, dt=BF16):
        return nc.dram_tensor(name, list(shape), dt, kind="ExternalOutput" if debug else "Internal").ap()

    xa = din("xa", [S, D])
    xo = din("xo", [2048, D])
    po = din("po", [2048, 256])
    posa = din("posa", [1, S], I32)
    poso = din("poso", [1, 2048], I32)
    qrel = din("qrel", [1, 512])
    invf = din("invf", [128, 1])
    w_in = din("w_in", [D, 2208])
    g_attn = din("g_attn", [1, D])
    g_cq = din("g_cq", [1, 384])
    w_uq = din("w_uq", [384, 768])
    g_ckv = din("g_ckv", [1, 256])
    w_ukv = din("w_ukv", [256, 1024])
    g_out = din("g_out", [1, 1024])
    w_o = din("w_o", [D, D])
    g_moe = din("g_moe", [1, D])
    w_router = din("w_router", [D, NE])
    b_router = din("b_router", [1, NE])
    NEd = NE if stop_after in (None, "C2") else 1
    w_gu = din("w_gu", [NEd, D, 2048])
    b_gu = din("b_gu", [NE * 16, 128])
    w_dn = din("w_dn", [NEd, D, D])
    b_dn = din("b_dn", [NE, D])
    g_ple = din("g_ple", [1, D])
    w_pg = din("w_pg", [D, D])
    w_pp = din("w_pp", [256, D])
    g_final = din("g_final", [1, D])
    yo = nc.dram_tensor("yo", [2048, D], F32, kind="ExternalOutput").ap()

    KnT = dscr("KnT", [512, S])
    KrT = dscr("KrT", [32, S])
    VmD = dscr("VmD", [S, 512])
    KsT = dscr("KsT", [512, S])
    VsD = dscr("VsD", [S, 512])
    QmT = dscr("QmT", [8 * 96, 2048])
    QsT = dscr("QsT", [512, 2048])
    OTd = dscr("OTd", [1024, 2048])
    CAP = 512
    NS = CAP // 128
    Ud = nc.dram_tensor("Ud", [2048, D], BF16, kind="Internal").ap()
    Yd = nc.dram_tensor("Yd", [NE * CAP, D], F32, kind="Internal").ap()
    if debug:
        dbgH = nc.dram_tensor("dbgH", [2048, D], F32, kind="ExternalOutput").ap()
        dbgG = nc.dram_tensor("dbgG", [2048, NE], F32, kind="ExternalOutput").ap()
        dbgS = nc.dram_tensor("dbgS", [2048, 4], I32, kind="ExternalOutput").ap()
        dbgJ = nc.dram_tensor("dbgJ", [2048, 4], F32, kind="ExternalOutput").ap()

    top = ExitStack()
    with top:
        p = Prog(nc)
        if True:

            def mk_alloc(stack):
                def sb(n, s, d):
                    return stack.enter_context(nc.sbuf_tensor(n, list(s), d))

                def ps(n, s, d=F32):
                    return stack.enter_context(nc.psum_tensor(n, list(s), d))
                return sb, ps

            sb0, ps0 = mk_alloc(top)

            identf = sb0("identf", [128, 128], F32)
            ident = sb0("ident", [128, 128], BF16)
            ones = sb0("ones", [128, 128], BF16)
            ntri = sb0("ntri", [128, 128], BF16)
            nones = sb0("nones", [128, 128], BF16)
            zeros = sb0("zeros", [128, 128], BF16)
            p.op("pool", lambda e: e.memset(identf[:], 0.0), writes=["identf"])
            p.op("pool", lambda e: e.affine_select(out=identf[:], in_=identf[:], pattern=[[-1, 128]],
                                                    compare_op=ALU.not_equal, fill=1.0, base=0, channel_multiplier=1),
                 reads=["identf"], writes=["identf"])
            p.op("dve", lambda e: e.tensor_copy(out=ident[:], in_=identf[:]), reads=["identf"], writes=["ident"])
            p.op("pool", lambda e: e.memset(ones[:], 1.0), writes=["ones"])
            p.op("pool", lambda e: e.memset(nones[:], -1.0), writes=["nones"])
            p.op("pool", lambda e: e.memset(zeros[:], 0.0), writes=["zeros"])
            gcol = sb0("gcol", [128, 16], F32)
            p.op("sp", lambda e: e.dma_start(out=gcol[:, 0:3], in_=g_cq.rearrange("o (c p) -> p (o c)", p=128),
                                             allow_slow_non_contiguous=True), writes=["gcol"], dma="gcol")
            p.op("sp", lambda e: e.dma_start(out=gcol[:, 3:5], in_=g_ckv.rearrange("o (c p) -> p (o c)", p=128),
                                             allow_slow_non_contiguous=True), writes=["gcol"], dma="gcol")
            p.op("sp", lambda e: e.dma_start(out=gcol[:, 5:13], in_=g_out.rearrange("o (c p) -> p (o c)", p=128),
                                             allow_slow_non_contiguous=True), writes=["gcol"], dma="gcol")
            invc = sb0("invc", [128, 1], F32)
            p.op("sp", lambda e: e.dma_start(out=invc[:], in_=invf), writes=["invc"], dma="invc")

            def rstd_from(eng_out, src, n, rd, wr):
                p.op("act", lambda e: e.activation(out=eng_out, in_=src, func=AF.Ln, scale=1.0 / n, bias=EPS),
                     reads=rd, writes=wr)
                p.op("act", lambda e: e.activation(out=eng_out, in_=eng_out, func=AF.Exp, scale=-0.5),
                     reads=wr, writes=wr)

            sa = ExitStack()
            with sa:
                sb, ps = mk_alloc(sa)
                tmpf = sb("tmpf", [128, 128], F32)
                p.op("pool", lambda e: e.memset(tmpf[:], -1.0), writes=["tmpf"])
                p.op("pool", lambda e: e.affine_select(out=tmpf[:], in_=tmpf[:], pattern=[[-1, 128]],
                                                        compare_op=ALU.is_ge, fill=0.0, base=0, channel_multiplier=1),
                     reads=["tmpf"], writes=["tmpf"])
                p.op("dve", lambda e: e.tensor_copy(out=ntri[:], in_=tmpf[:]), reads=["tmpf"], writes=["ntri"])

                Win = sb("Win", [128, 8, 2208], BF16)
                for c in range(8):
                    p.op("pool", lambda e, c=c: e.dma_start(out=Win[:, c, :], in_=w_in[c * 128:(c + 1) * 128, :]),
                         writes=["Win"], dma="Win")
                Wuq = sb("Wuq", [128, 3, 768], BF16)
                Wuqr = sb("Wuqr", [128, 3, 768], BF16)
                p.op("pool", lambda e: e.dma_start(out=Wuq[:], in_=w_uq.rearrange("(c p) n -> p c n", p=128)),
                     writes=["Wuq"], dma="Wuq")
                Wkn = sb("Wkn", [128, 2, 512], BF16)
                Wv = sb("Wv", [128, 2, 512], BF16)
                ukv = w_ukv.rearrange("(c p) (h t d) -> p c h t d", p=128, h=8, t=2)
                for c in range(2):
                    p.op("pool", lambda e, c=c: e.dma_start(out=Wkn[:, c, :].rearrange("p (h d) -> p h d", h=8),
                                                          in_=ukv[:, c, :, 0, :]), writes=["Wkn"], dma="Wkn")
                    p.op("pool", lambda e, c=c: e.dma_start(out=Wv[:, c, :].rearrange("p (h d) -> p h d", h=8),
                                                          in_=ukv[:, c, :, 1, :]), writes=["Wv"], dma="Wv")
                p.op("pool", lambda e: e.memset(Wuqr[:], 0.0), writes=["Wuqr"])
                Wq4 = Wuq[:].rearrange("p c (h d) -> p c h d", h=8)
                Wr4 = Wuqr[:].rearrange("p c (h d) -> p c h d", h=8)
                for c in range(3):
                    p.op("dve", lambda e, c=c: e.tensor_scalar(out=Wr4[:, c, :, 64:80], in0=Wq4[:, c, :, 80:96], scalar1=-1.0,
                                                          scalar2=None, op0=ALU.mult), reads=["Wuq", "Wuqr"], writes=["Wuqr"])
                    p.op("dve", lambda e, c=c: e.tensor_copy(out=Wr4[:, c, :, 80:96], in_=Wq4[:, c, :, 64:80]),
                         reads=["Wuq", "Wuqr"], writes=["Wuqr"])
                Wkr = sb("Wkr", [128, 8, 64], BF16)
                p.op("dve", lambda e: e.tensor_copy(out=Wkr[:, :, 0:32], in_=Win[:, :, 640:672]), reads=["Win"], writes=["Wkr"])
                p.op("dve", lambda e: e.tensor_scalar(out=Wkr[:, :, 32:48], in0=Win[:, :, 656:672], scalar1=-1.0, scalar2=None,
                                                      op0=ALU.mult), reads=["Win", "Wkr"], writes=["Wkr"])
                p.op("dve", lambda e: e.tensor_copy(out=Wkr[:, :, 48:64], in_=Win[:, :, 640:656]), reads=["Win", "Wkr"], writes=["Wkr"])
                gattn = sb("gattn", [128, D], F32)
                p.op("sp", lambda e: e.dma_start(out=gattn[:], in_=g_attn.partition_broadcast(128)), writes=["gattn"], dma="gattn")

                def rope_table(Ct, St, pos_ap, n, rows, name, inv, kinv, sb):
                    posi = sb(name + "_pi", [128, n], I32)
                    ang = sb(name + "_ang", [128, n], F32)
                    kk = sb(name + "_k", [128, n], F32)
                    ki = sb(name + "_ki", [128, n], I32)
                    p.op("sp", lambda e: e.dma_start(out=posi[:], in_=pos_ap.partition_broadcast(128)), writes=[name + "pi"], dma=name + "pi")
                    p.op("dve", lambda e: e.tensor_copy(out=ang[:], in_=posi[:]), reads=[name + "pi"], writes=[name + "ang"])
                    p.op("dve", lambda e: e.tensor_scalar(out=ang[:], in0=ang[:], scalar1=inv[:, 0:1], scalar2=None, op0=ALU.mult),
                         reads=[name + "ang", kinv], writes=[name + "ang"])
                    for which, T in (("s", St), ("c", Ct)):
                        off = 0.0 if which == "s" else float(np.pi / 2)
                        p.op("dve", lambda e, off=off: e.tensor_scalar(out=kk[:], in0=ang[:], scalar1=off, scalar2=float(1.0 / (2 * np.pi)),
                                                                   op0=ALU.add, op1=ALU.mult), reads=[name + "ang"], writes=[name + "kk"])
                        p.op("dve", lambda e: e.tensor_copy(out=ki[:], in_=kk[:]), reads=[name + "kk"], writes=[name + "ki"])
                        p.op("dve", lambda e: e.tensor_copy(out=kk[:], in_=ki[:]), reads=[name + "ki"], writes=[name + "kk"])
                        p.op("dve", lambda e, T=T: e.scalar_tensor_tensor(out=T, in0=kk[:rows], scalar=-6.28125, in1=ang[:rows],
                                                                       op0=ALU.mult, op1=ALU.add),
                             reads=[name + "kk", name + "ang"], writes=[name + which])
                        p.op("dve", lambda e, T=T, off=off: e.scalar_tensor_tensor(out=T, in0=kk[:rows], scalar=float(-(2 * np.pi - 6.28125)), in1=T,
                                                                                op0=ALU.mult, op1=ALU.add),
                             reads=[name + "kk", name + which], writes=[name + which])
                        if off != 0.0:
                            p.op("dve", lambda e, T=T, off=off: e.tensor_scalar(out=T, in0=T, scalar1=off, scalar2=None, op0=ALU.add),
                                 reads=[name + which], writes=[name + which])
                        p.op("dve", lambda e: e.tensor_scalar(out=kk[:rows], in0=T, scalar1=float(np.pi), scalar2=float(-2 * np.pi),
                                                              op0=ALU.is_gt, op1=ALU.mult), reads=[name + which], writes=[name + "kk"])
                        p.op("dve", lambda e, T=T: e.tensor_tensor(out=T, in0=T, in1=kk[:rows], op=ALU.add),
                             reads=[name + which, name + "kk"], writes=[name + which])
                        p.op("dve", lambda e: e.tensor_scalar(out=kk[:rows], in0=T, scalar1=float(-np.pi), scalar2=float(2 * np.pi),
                                                              op0=ALU.is_lt, op1=ALU.mult), reads=[name + which], writes=[name + "kk"])
                        p.op("dve", lambda e, T=T: e.tensor_tensor(out=T, in0=T, in1=kk[:rows], op=ALU.add),
                             reads=[name + which, name + "kk"], writes=[name + which])
                        p.op("dve", lambda e, T=T: e.tensor_scalar(out=T, in0=T, scalar1=3.14159, scalar2=-3.14159, op0=ALU.min, op1=ALU.max),
                             reads=[name + which], writes=[name + which])
                        p.op("act", lambda e, T=T: e.activation(out=T, in_=T, func=AF.Sin), reads=[name + which], writes=[name + which])

                Ck = sb("Ck", [32, S], F32)
                Sk = sb("Sk", [32, S], F32)
                sa2 = ExitStack()
                with sa2:
                    rope_table(Ck[:], Sk[:], posa, S, 32, "rk", invc, "invc", mk_alloc(sa2)[0])
                    p.barrier()
                Cq = sb("Cq", [96, 2048], F32)
                Sq = sb("Sq", [96, 2048], F32)
                sa3 = ExitStack()
                with sa3:
                    sb3_ = mk_alloc(sa3)[0]
                    invq = sb3_("invq", [128, 1], F32)
                    p.op("pool", lambda e: e.memset(invq[:], 0.0), writes=["invq"])
                    p.op("sp", lambda e: e.dma_start(out=invq[64:96, :], in_=invf[0:32, :]), reads=["invq"], writes=["invq"], dma="invq")
                    rope_table(Cq[:], Sq[:], poso, 2048, 96, "rq", invq, "invq", sb3_)
                    p.barrier()
                    if stop_after == "R":
                        p.finish_wait("sp"); p.emit(top); return nc
                msc = float((64 + 32) ** -0.5)
                p.op("dve", lambda e: e.tensor_scalar(out=Cq[:], in0=Cq[:], scalar1=msc, scalar2=None, op0=ALU.mult),
                     reads=["rqc"], writes=["rqc"])
                p.op("dve", lambda e: e.tensor_scalar(out=Sq[:], in0=Sq[:], scalar1=msc, scalar2=None, op0=ALU.mult),
                     reads=["rqs"], writes=["rqs"])

                xt_r = Ring(sb, "xt", 4, [128, D], F32)
                junk = sb("junkA", [128, D], F32)
                ss_r = Ring(sb, "ssA", 4, [128, 1], F32)
                xn_r = Ring(sb, "xn", 4, [128, D], BF16)
                xnT_r = Ring(sb, "xnT", 2, [128, 8, 512], BF16)
                pT_r = Ring(ps, "pTA", 2, [128, 8, 128], BF16)
                pm_r = Ring(ps, "pmA", 4, [128, 512], F32)
                pss = ps("pssA", [128, 512], F32)
                sq_r = Ring(sb, "sqA", 2, [128, 512], BF16)
                rbc = sb("rbcA", [128, 512], F32)
                cn_r = Ring(sb, "cnA", 2, [128, 3, 512], BF16)
                ev_r = Ring(sb, "evA", 2, [128, 512], BF16)
                stg_r = Ring(sb, "stgA", 3, [128, 4, 512], BF16)
                stg8_r = Ring(sb, "stg8A", 2, [128, 8, 512], BF16)
                evf_r = Ring(sb, "evfA", 2, [128, 512], F32)
                evf2_r = Ring(sb, "evf2A", 2, [128, 512], F32)

                def make_xnT(src, tok0, defer=False):
                    xnT, kT = xnT_r.next()
                    xs = []
                    for sub in range(4):
                        xt, kx = xt_r.next()
                        ss, ks = ss_r.next()
                        xn, kn = xn_r.next()
                        r0 = tok0 + sub * 128
                        p.op("sp", lambda e: e.dma_start(out=xt[:], in_=src[r0:r0 + 128, :]), writes=[kx], dma=kx)
                        p.op("act", lambda e: e.activation(out=junk[:], in_=xt[:], func=AF.Square, accum_out=ss[:]),
                             reads=[kx], writes=["junkA", ks])
                        rstd_from(ss[:], ss[:], D, [ks], [ks])
                        p.op("dve", lambda e: e.scalar_tensor_tensor(out=xn[:], in0=xt[:], scalar=ss[:, 0:1], in1=gattn[:],
                                                                     op0=ALU.mult, op1=ALU.mult),
                             reads=[kx, ks, "gattn"], writes=[kn])
                        xs.append((xn, kn))
                    def tr(sub):
                        xn, kn = xs[sub]
                        pT, kp = pT_r.next()
                        for c in range(8):
                            p.op("pe", lambda e: e.transpose(out=pT[:, c, :], in_=xn[:, c * 128:(c + 1) * 128], identity=ident[:]),
                                 reads=[kn, "ident"], writes=[kp])
                        p.op("dve", lambda e: e.tensor_copy(out=xnT[:, :, sub * 128:(sub + 1) * 128], in_=pT[:]),
                             reads=[kp], writes=[kT])
                    if defer:
                        return xnT, kT, tr
                    for sub in range(4):
                        tr(sub)
                    return xnT, kT

                def proj_fm(xnT, kT, col0, m, wkey="Win", W=None):
                    W = Win if W is None else W
                    pm, kpm = pm_r.next()
                    for k in range(8):
                        p.op("pe", lambda e, k=k, pm=pm, W=W: e.matmul(out=pm[0:m, :], lhsT=W[:, k, col0:col0 + m], rhs=xnT[:, k, :],
                                                                  start=(k == 0), stop=(k == 7)),
                             reads=[kT, wkey], writes=[kpm])
                    return pm, kpm

                def lowrank_norm(pms, nch, width, gc0):
                    for i, (pm, kpm) in enumerate(pms):
                        sq, ksq = sq_r.next()
                        p.op("act", lambda e, pm=pm, sq=sq: e.activation(out=sq[:], in_=pm[:], func=AF.Square), reads=[kpm], writes=[ksq])
                        p.op("pe", lambda e, sq=sq, i=i: e.matmul(out=pss[:], lhsT=ones[:], rhs=sq[:], start=(i == 0), stop=(i == nch - 1)),
                             reads=[ksq, "ones"], writes=["pssA"])
                    rstd_from(rbc[:], pss[:], width, ["pssA"], ["rbcA"])
                    cn, kcn = cn_r.next()
                    for i, (pm, kpm) in enumerate(pms):
                        p.op("dve", lambda e, pm=pm, i=i, cn=cn: e.scalar_tensor_tensor(out=cn[:, i, :], in0=pm[:], scalar=gcol[:, gc0 + i:gc0 + i + 1],
                                                                                    in1=rbc[:], op0=ALU.mult, op1=ALU.mult),
                             reads=[kpm, "gcol", "rbcA"], writes=[kcn])
                    return cn, kcn

                def store_fm(pm, kpm, rows, dst, eng="dve", scale=None):
                    ev, kev = ev_r.next()
                    if scale is None:
                        if eng == "act":
                            p.op("act", lambda e: e.copy(out=ev[0:rows, :], in_=pm[0:rows, :]), reads=[kpm], writes=[kev])
                        else:
                            p.op("dve", lambda e: e.tensor_copy(out=ev[0:rows, :], in_=pm[0:rows, :]), reads=[kpm], writes=[kev])
                    else:
                        p.op("act", lambda e: e.mul(out=ev[0:rows, :], in_=pm[0:rows, :], mul=scale), reads=[kpm], writes=[kev])
                    p.op("pool", lambda e: e.dma_start(out=dst, in_=ev[0:rows, :]), reads=[kev], writes=[], dma=kev + "s")

                def evac_to(pm, kpm, dst, kdst, eng="dve", scale=None):
                    if scale is not None:
                        p.op("act", lambda e: e.mul(out=dst, in_=pm[:], mul=scale), reads=[kpm], writes=[kdst])
                    elif eng == "act":
                        p.op("act", lambda e: e.copy(out=dst, in_=pm[:]), reads=[kpm], writes=[kdst])
                    else:
                        p.op("dve", lambda e: e.tensor_copy(out=dst, in_=pm[:]), reads=[kpm], writes=[kdst])

                KnT_v = KnT.rearrange("(c p) n -> p c n", p=128)
                KsT_v = KsT.rearrange("(c p) n -> p c n", p=128)
                QsT_v = QsT.rearrange("(c p) n -> p c n", p=128)
                VmD_v = VmD.rearrange("(s p) n -> p s n", p=128)
                VsD_v = VsD.rearrange("(s p) n -> p s n", p=128)
                QmT_v = QmT.rearrange("(h r) n -> r h n", r=96)

                srcs = [(xa, g_ * 512) for g_ in range(8)] + [(xo, g_ * 512) for g_ in range(4)]
                nxt = make_xnT(*srcs[0])
                for G in range(8):
                    c0 = G * 512
                    xnT, kT = nxt
                    nx_xnT, nx_kT, nx_tr = make_xnT(*srcs[G + 1], defer=True)
                    nxt = (nx_xnT, nx_kT)
                    ckv = [proj_fm(xnT, kT, 384 + m * 128, 128) for m in range(2)]
                    nx_tr(0)
                    cn, kcn = lowrank_norm(ckv, 2, 256, 3)
                    pa, kpa = proj_fm(xnT, kT, 0, 32, "Wkr", Wkr)
                    pb, kpb = proj_fm(xnT, kT, 32, 32, "Wkr", Wkr)
                    t1, kt1 = evf_r.next()
                    t2, kt2 = evf2_r.next()
                    p.op("dve", lambda e, pa=pa, t1=t1, c0=c0: e.tensor_tensor(out=t1[0:32, :], in0=pa[0:32, :], in1=Ck[:, c0:c0 + 512], op=ALU.mult),
                         reads=[kpa, "rkc"], writes=[kt1])
                    p.op("dve", lambda e, pb=pb, t2=t2, c0=c0: e.tensor_tensor(out=t2[0:32, :], in0=pb[0:32, :], in1=Sk[:, c0:c0 + 512], op=ALU.mult),
                         reads=[kpb, "rks"], writes=[kt2])
                    ev, kev = ev_r.next()
                    p.op("dve", lambda e, t1=t1, t2=t2, ev=ev: e.tensor_tensor(out=ev[0:32, :], in0=t1[0:32, :], in1=t2[0:32, :], op=ALU.add),
                         reads=[kt1, kt2], writes=[kev])
                    p.op("sp", lambda e, ev=ev, c0=c0: e.dma_start(out=KrT[:, c0:c0 + 512], in_=ev[0:32, :]), reads=[kev], writes=[], dma=kev + "s")
                    nx_tr(1)
                    st_, kst_ = stg_r.next()
                    for m in range(4):
                        pm, kpm = proj_fm(xnT, kT, 1184 + m * 128, 128)
                        evac_to(pm, kpm, st_[:, m, :], kst_, eng="act")
                    p.op("sp", lambda e: e.dma_start(out=KsT_v[:, :, c0:c0 + 512], in_=st_[:]), reads=[kst_], dma=kst_ + "s")
                    nx_tr(2)
                    st_, kst_ = stg_r.next()
                    for sub in range(4):
                        pm, kpm = pm_r.next()
                        for k in range(8):
                            p.op("pe", lambda e, k=k, pm=pm, sub=sub, xnT=xnT: e.matmul(out=pm[:], lhsT=xnT[:, k, sub * 128:(sub + 1) * 128],
                                                                                   rhs=Win[:, k, 1696:2208], start=(k == 0), stop=(k == 7)),
                                 reads=[kT, "Win"], writes=[kpm])
                        evac_to(pm, kpm, st_[:, sub, :], kst_, eng="dve")
                    p.op("sp", lambda e: e.dma_start(out=VsD_v[:, G * 4:(G + 1) * 4, :], in_=st_[:]), reads=[kst_], dma=kst_ + "s")

                    nx_tr(3)
                    st_, kst_ = stg_r.next()
                    for hp in range(4):
                        pm, kpm = pm_r.next()
                        for m in range(2):
                            p.op("pe", lambda e, m=m, pm=pm, hp=hp, cn=cn: e.matmul(out=pm[:], lhsT=Wkn[:, m, hp * 128:(hp + 1) * 128], rhs=cn[:, m, :],
                                                                               start=(m == 0), stop=(m == 1)),
                                 reads=[kcn, "Wkn"], writes=[kpm])
                        evac_to(pm, kpm, st_[:, hp, :], kst_, eng="act")
                    p.op("sp", lambda e: e.dma_start(out=KnT_v[:, :, c0:c0 + 512], in_=st_[:]), reads=[kst_], dma=kst_ + "s")
                    st_, kst_ = stg_r.next()
                    for sub in range(4):
                        pm, kpm = pm_r.next()
                        for m in range(2):
                            p.op("pe", lambda e, m=m, pm=pm, sub=sub, cn=cn: e.matmul(out=pm[:], lhsT=cn[:, m, sub * 128:(sub + 1) * 128], rhs=Wv[:, m, :],
                                                                                 start=(m == 0), stop=(m == 1)),
                                 reads=[kcn, "Wv"], writes=[kpm])
                        evac_to(pm, kpm, st_[:, sub, :], kst_, eng="dve")
                    p.op("sp", lambda e: e.dma_start(out=VmD_v[:, G * 4:(G + 1) * 4, :], in_=st_[:]), reads=[kst_], dma=kst_ + "s")
                for G in range(4):
                    c0 = G * 512
                    xnT, kT = nxt
                    if G + 1 < 4:
                        nxt = make_xnT(*srcs[8 + G + 1])
                    cq = [proj_fm(xnT, kT, m * 128, 128) for m in range(3)]
                    cn, kcn = lowrank_norm(cq, 3, 384, 0)
                    st8, kst8 = stg8_r.next()
                    for h in range(8):
                        pa, kpa = pm_r.next()
                        pb, kpb = pm_r.next()
                        for m in range(3):
                            p.op("pe", lambda e, m=m, pa=pa, h=h, cn=cn: e.matmul(out=pa[0:96, :], lhsT=Wuq[:, m, h * 96:(h + 1) * 96], rhs=cn[:, m, :],
                                                                             start=(m == 0), stop=(m == 2)),
                                 reads=[kcn, "Wuq"], writes=[kpa])
                        for m in range(3):
                            p.op("pe", lambda e, m=m, pb=pb, h=h, cn=cn: e.matmul(out=pb[0:96, :], lhsT=Wuqr[:, m, h * 96:(h + 1) * 96], rhs=cn[:, m, :],
                                                                             start=(m == 0), stop=(m == 2)),
                                 reads=[kcn, "Wuqr"], writes=[kpb])
                        t1, kt1 = evf_r.next()
                        t2, kt2 = evf2_r.next()
                        p.op("dve", lambda e, pa=pa, t1=t1, c0=c0: e.tensor_tensor(out=t1[0:96, :], in0=pa[0:96, :], in1=Cq[:, c0:c0 + 512], op=ALU.mult),
                             reads=[kpa, "rqc"], writes=[kt1])
                        p.op("dve", lambda e, pb=pb, t2=t2, c0=c0: e.tensor_tensor(out=t2[0:96, :], in0=pb[0:96, :], in1=Sq[:, c0:c0 + 512], op=ALU.mult),
                             reads=[kpb, "rqs"], writes=[kt2])
                        p.op("pool", lambda e, t1=t1, t2=t2: e.tensor_tensor(out=st8[0:96, h, :], in0=t1[0:96, :], in1=t2[0:96, :], op=ALU.add),
                             reads=[kt1, kt2], writes=[kst8])
                    p.op("sp", lambda e: e.dma_start(out=QmT_v[:, :, c0:c0 + 512], in_=st8[0:96, :, :]), reads=[kst8], dma=kst8 + "s")
                    st_, kst_ = stg_r.next()
                    for m in range(4):
                        pm, kpm = proj_fm(xnT, kT, 672 + m * 128, 128)
                        evac_to(pm, kpm, st_[:, m, :], kst_, scale=0.125)
                    p.op("sp", lambda e: e.dma_start(out=QsT_v[:, :, c0:c0 + 512], in_=st_[:]), reads=[kst_], dma=kst_ + "s")
                p.barrier()
                if stop_after == "A":
                    p.finish_wait("sp"); p.emit(top); return nc

            sbx = ExitStack()
            with sbx:
                sb, ps = mk_alloc(sbx)
                qrb = sb("qrb", [128, 512], F32)
                p.op("sp", lambda e: e.dma_start(out=qrb[:], in_=qrel.partition_broadcast(128)), writes=["qrb"], dma="qrb")
                kidx_i = sb("kidx_i", [128, 1], I32)
                krel = sb("krel", [128, 8], F32)
                p.op("pool", lambda e: e.iota(kidx_i[:], pattern=[[0, 1]], base=0, channel_multiplier=1), writes=["kidx_i"])
                p.op("dve", lambda e: e.tensor_copy(out=krel[:, 0:1], in_=kidx_i[:]), reads=["kidx_i"], writes=["krel"])
                for d in range(1, 8):
                    p.op("dve", lambda e, d=d: e.tensor_scalar(out=krel[:, d:d + 1], in0=krel[:, 0:1], scalar1=float(128 * d), scalar2=None, op0=ALU.add),
                         reads=["krel"], writes=["krel"])
                nmM = sb("nmM", [128, 8, 512], BF16)
                nmS = sb("nmS", [128, 8, 512], BF16)
                m01 = sb("m01", [128, 8, 512], BF16)
                for d in range(8):
                    p.op("dve", lambda e, d=d: e.tensor_scalar(out=nmM[:, d, :], in0=qrb[:], scalar1=krel[:, d:d + 1], scalar2=NEG, op0=ALU.is_lt, op1=ALU.mult),
                         reads=["qrb", "krel"], writes=["nmM"])
                    p.op("dve", lambda e, d=d: e.tensor_scalar(out=nmS[:, d, :], in0=qrb[:], scalar1=krel[:, d:d + 1], scalar2=NEG, op0=ALU.is_le, op1=ALU.mult),
                         reads=["qrb", "krel"], writes=["nmS"])
                    p.op("dve", lambda e, d=d: e.tensor_scalar(out=m01[:, d, :], in0=qrb[:], scalar1=krel[:, d:d + 1], scalar2=None, op0=ALU.is_gt),
                         reads=["qrb", "krel"], writes=["m01"])

                def blocks(t):
                    out = [(kb, 512, None) for kb in range(8 * t)]
                    out += [(8 * t + d, NDIAG[d], d) for d in range(8)]
                    return out

                sm = ExitStack()
                with sm:
                    def mla_gen():
                        sb, ps = mk_alloc(sm)
                        Vall = sb("Vall", [128, 32, 8, 65], BF16)
                        p.op("pool", lambda e: e.memset(Vall[:], 1.0), writes=["Vall"])
                        vsrc = VmD.rearrange("(kb p) (h d) -> p kb h d", p=128, h=8)
                        for hh in range(8):
                            p.op("sp", lambda e: e.dma_start(out=Vall[:, :, hh, 0:64], in_=vsrc[:, :, hh, :]),
                                 reads=["Vall"], writes=["Vall"], dma="Vall")
                        KT_r = Ring(sb, "KTm", 2, [96, S], BF16)
                        QT_r = Ring(sb, "QTm", 2, [96, 2048], BF16)
                        pS_r = Ring(ps, "pSm", 2, [128, 512], F32)
                        pO_r = Ring(ps, "pOm", 1, [128, 512], F32)
                        pB_r = Ring(ps, "pBm", 1, [128, 512], F32)
                        P_r = Ring(sb, "Pm", 3, [128, 512], BF16)
                        Of_r = Ring(sb, "Ofm", 2, [65, 512], F32)
                        On_r = Ring(sb, "Onm", 2, [64, 512], BF16)
                        sel = sb("sel65", [65, 64], F32)
                        p.op("pool", lambda e: e.memset(sel[:], 0.0), writes=["sel65"])
                        p.op("pool", lambda e: e.memset(sel[64:65, :], 1.0), reads=["sel65"], writes=["sel65"])
                        for h in range(8):
                            KT, kKT = KT_r.next()
                            QT, kQT = QT_r.next()
                            p.op("sp", lambda e, KT=KT, h=h: e.dma_start(out=KT[0:64, :], in_=KnT[h * 64:(h + 1) * 64, :]), writes=[kKT], dma=kKT)
                            p.op("sp", lambda e, KT=KT: e.dma_start(out=KT[64:96, :], in_=KrT[:, :]), writes=[kKT], dma=kKT)
                            p.op("sp", lambda e, QT=QT, h=h: e.dma_start(out=QT[:], in_=QmT[h * 96:(h + 1) * 96, :]), writes=[kQT], dma=kQT)
                            for t in range(4):
                                bl = blocks(t)
                                pO, kpO = pO_r.next()
                                nb = len(bl)
                                stage = {}

                                def stA(i):
                                    kb, N, d = bl[i]
                                    pS, kpS = pS_r.next()
                                    p.op("pe", lambda e: e.matmul(out=pS[:, 0:N], lhsT=KT[:, kb * 128:(kb + 1) * 128], rhs=QT[:, t * 512:t * 512 + N],
                                                                  start=True, stop=(d is None)), reads=[kKT, kQT], writes=[kpS])
                                    if d is not None:
                                        p.op("pe", lambda e: e.matmul(out=pS[:, 0:N], lhsT=ident[:], rhs=nmM[:, d, 0:N], start=False, stop=True),
                                             reads=["ident", "nmM"], writes=[kpS])
                                    P, kP = P_r.next()
                                    p.op("act", lambda e: e.activation(out=P[:, 0:N], in_=pS[:, 0:N], func=AF.Exp), reads=[kpS], writes=[kP])
                                    stage[i] = (P, kP)

                                def stB(i):
                                    kb, N, d = bl[i]
                                    P, kP = stage.pop(i)
                                    p.op("pe", lambda e: e.matmul(out=pO[0:65, 0:N], lhsT=Vall[:, kb, h, :], rhs=P[:, 0:N], start=(i == 0), stop=(i == nb - 1)),
                                         reads=["Vall", kP], writes=[kpO])

                                for it in range(nb + 2):
                                    if it < nb:
                                        stA(it)
                                    if it - 2 >= 0:
                                        stB(it - 2)
                                    yield
                                Of, kOf = Of_r.next()
                                p.op("dve", lambda e, Of=Of, pO=pO: e.tensor_copy(out=Of[:], in_=pO[0:65, :]), reads=[kpO], writes=[kOf])
                                p.op("dve", lambda e, Of=Of: e.reciprocal(out=Of[64:65, :], in_=Of[64:65, :]), reads=[kOf], writes=[kOf])
                                pB, kpB = pB_r.next()
                                p.op("pe", lambda e, Of=Of, pB=pB: e.matmul(out=pB[0:64, :], lhsT=sel[:], rhs=Of[:], start=True, stop=True),
                                     reads=[kOf, "sel65"], writes=[kpB])
                                On, kOn = On_r.next()
                                p.op("dve", lambda e, Of=Of, pB=pB, On=On: e.tensor_tensor(out=On[:], in0=Of[0:64, :], in1=pB[0:64, :], op=ALU.mult),
                                     reads=[kOf, kpB], writes=[kOn])
                                p.op("pool", lambda e, On=On, h=h, t=t: e.dma_start(out=OTd[h * 64:(h + 1) * 64, t * 512:(t + 1) * 512], in_=On[:]),
                                     reads=[kOn], writes=[], dma=kOn + "s")

                    def sb_gen():
                        sb, ps = mk_alloc(sm)
                        Vs = sb("Vsall", [128, 32, 512], BF16)
                        vsrc = VsD.rearrange("(kb p) n -> p kb n", p=128)
                        for q4 in range(4):
                            p.op("sp", lambda e, q4=q4: e.dma_start(out=Vs[:, q4 * 8:(q4 + 1) * 8, :], in_=vsrc[:, q4 * 8:(q4 + 1) * 8, :]),
                                 writes=["Vsall"], dma="Vsall")
                        KT_r = Ring(sb, "KTs", 2, [64, S], BF16)
                        QT_r = Ring(sb, "QTs", 2, [64, 2048], BF16)
                        pZ_r = Ring(ps, "pZs", 2, [128, 512], F32)
                        pL_r = Ring(ps, "pLs", 1, [128, 512], F32)
                        pO_r = Ring(ps, "pOs", 1, [128, 512], F32)
                        E_r = Ring(sb, "Es", 3, [128, 512], F32)
                        SP_r = Ring(sb, "SPs", 4, [128, 512], BF16)
                        SM_r = Ring(sb, "SMs", 4, [128, 512], BF16)
                        A_r = Ring(sb, "As", 3, [128, 512], BF16)
                        CR_r = Ring(sb, "CRs", 3, [128, 512], BF16)
                        On_r = Ring(sb, "Ons", 2, [64, 512], BF16)
                        for h in range(8):
                            KT, kKT = KT_r.next()
                            QT, kQT = QT_r.next()
                            p.op("sp", lambda e, KT=KT, h=h: e.dma_start(out=KT[:], in_=KsT[h * 64:(h + 1) * 64, :]), writes=[kKT], dma=kKT)
                            p.op("sp", lambda e, QT=QT, h=h: e.dma_start(out=QT[:], in_=QsT[h * 64:(h + 1) * 64, :]), writes=[kQT], dma=kQT)
                            for t in range(4):
                                bl = blocks(t)[::-1]
                                nb = len(bl)
                                pO, kpO = pO_r.next()
                                p.op("pe", lambda e, pO=pO: e.matmul(out=pO[0:64, :], lhsT=zeros[:, 0:64], rhs=m01[:, 0, :],
                                                                      start=True, stop=False), reads=["zeros", "m01"], writes=[kpO])
                                stage = {}
                                carry = {"t": None, "k": None}

                                def stA(i):
                                    kb, N, d = bl[i]
                                    pZ, kpZ = pZ_r.next()
                                    p.op("pe", lambda e: e.matmul(out=pZ[:, 0:N], lhsT=KT[:, kb * 128:(kb + 1) * 128], rhs=QT[:, t * 512:t * 512 + N],
                                                                  start=True, stop=True), reads=[kKT, kQT], writes=[kpZ])
                                    E, kE = E_r.next()
                                    p.op("act", lambda e: e.activation(out=E[:, 0:N], in_=pZ[:, 0:N], func=AF.Exp), reads=[kpZ], writes=[kE])
                                    SPt, kSP = SP_r.next()
                                    p.op("act", lambda e: e.activation(out=SPt[:, 0:N], in_=E[:, 0:N], func=AF.Ln, bias=1.0), reads=[kE], writes=[kSP])
                                    if d is not None:
                                        SM, kSM = SM_r.next()
                                        p.op("dve", lambda e: e.tensor_tensor(out=SM[:, 0:N], in0=SPt[:, 0:N], in1=m01[:, d, 0:N], op=ALU.mult),
                                             reads=[kSP, "m01"], writes=[kSM])
                                    else:
                                        SM, kSM = SPt, kSP
                                    cprev, kcprev = carry["t"], carry["k"]
                                    stage[i] = (SM, kSM, cprev, kcprev, E, kE)
                                    if i < nb - 1:
                                        cn_, kcn_ = CR_r.next()
                                        if cprev is None:
                                            if N < 512:
                                                p.op("pool", lambda e: e.memset(cn_[:, N:512], 0.0), writes=[kcn_])
                                            p.op("pool", lambda e: e.tensor_copy(out=cn_[:, 0:N], in_=SM[:, 0:N]), reads=[kSM], writes=[kcn_])
                                        else:
                                            if N < 512:
                                                p.op("pool", lambda e: e.tensor_copy(out=cn_[:, N:512], in_=cprev[:, N:512]), reads=[kcprev], writes=[kcn_])
                                            p.op("dve", lambda e: e.tensor_tensor(out=cn_[:, 0:N], in0=cprev[:, 0:N], in1=SM[:, 0:N], op=ALU.add),
                                                 reads=[kcprev, kSM], writes=[kcn_])
                                        carry["t"], carry["k"] = cn_, kcn_

                                def stB(i):
                                    kb, N, d = bl[i]
                                    SM, kSM, cprev, kcprev, E, kE = stage[i]
                                    pL, kpL = pL_r.next()
                                    last = "tri"
                                    if cprev is not None:
                                        last = "carry"
                                    if d is not None:
                                        last = "mask"
                                    p.op("pe", lambda e: e.matmul(out=pL[:, 0:N], lhsT=ntri[:], rhs=SM[:, 0:N], start=True, stop=(last == "tri")),
                                         reads=["ntri", kSM], writes=[kpL])
                                    if cprev is not None:
                                        p.op("pe", lambda e: e.matmul(out=pL[:, 0:N], lhsT=nones[:], rhs=cprev[:, 0:N], start=False, stop=(last == "carry")),
                                             reads=["nones", kcprev], writes=[kpL])
                                    if d is not None:
                                        p.op("pe", lambda e: e.matmul(out=pL[:, 0:N], lhsT=ident[:], rhs=nmS[:, d, 0:N], start=False, stop=True),
                                             reads=["ident", "nmS"], writes=[kpL])
                                    A, kA = A_r.next()
                                    p.op("act", lambda e: e.activation(out=A[:, 0:N], in_=pL[:, 0:N], func=AF.Exp), reads=[kpL], writes=[kA])
                                    p.op("dve", lambda e: e.tensor_tensor(out=A[:, 0:N], in0=A[:, 0:N], in1=E[:, 0:N], op=ALU.mult), reads=[kA, kE], writes=[kA])
                                    stage[i] = (A, kA)

                                def stC(i):
                                    kb, N, d = bl[i]
                                    A, kA = stage.pop(i)
                                    p.op("pe", lambda e: e.matmul(out=pO[0:64, 0:N], lhsT=Vs[:, kb, h * 64:(h + 1) * 64], rhs=A[:, 0:N], start=False, stop=(i == nb - 1)),
                                         reads=["Vsall", kA], writes=[kpO])

                                for it in range(nb + 2):
                                    if it < nb:
                                        stA(it)
                                    if 0 <= it - 1 < nb:
                                        stB(it - 1)
                                    if it - 2 >= 0:
                                        stC(it - 2)
                                    yield
                                On, kOn = On_r.next()
                                p.op("dve", lambda e, On=On, pO=pO: e.tensor_copy(out=On[:], in_=pO[0:64, :]), reads=[kpO], writes=[kOn])
                                p.op("pool", lambda e, On=On, h=h, t=t: e.dma_start(out=OTd[512 + h * 64:512 + (h + 1) * 64, t * 512:(t + 1) * 512], in_=On[:]),
                                     reads=[kOn], writes=[], dma=kOn + "s")

                    gens = [mla_gen(), sb_gen()]
                    while gens:
                        for g_ in list(gens):
                            try:
                                next(g_)
                            except StopIteration:
                                gens.remove(g_)
                    p.barrier()
                    if stop_after == "B":
                        p.finish_wait("sp"); p.emit(top); return nc
            sc = ExitStack()
            with sc:
                sb, ps = mk_alloc(sc)
                H = sb("H", [128, 16, D], F32)
                posm = sb("posm", [128, 16, NE], F32)
                maskb = sb("maskb", [128, 16, NE], BF16)
                gj = sb("gj", [128, 16, 4], F32)
                slI = sb("slI", [128, 16, 4], I32)
                Gt = sb("Gt", [128, 16, NE], F32)
                bgT = sb("bgT", [128, 512], F32)
                s1 = ExitStack()
                with s1:
                    sb1, ps1 = mk_alloc(s1)
                    gbc = sb1("gbc", [128, D], F32)
                    junk = sb1("junkC", [128, D], BF16)
                    Wo = sb1("Wo", [128, 8, D], BF16)
                    p.op("pool", lambda e: e.dma_start(out=Wo[:], in_=w_o.rearrange("(c p) n -> p c n", p=128)), writes=["Wo"], dma="Wo")
                    Wr = sb1("Wr", [128, 8, NE], F32)
                    p.op("sp", lambda e: e.dma_start(out=Wr[:], in_=w_router.rearrange("(c p) n -> p c n", p=128)), writes=["Wr"], dma="Wr")
                    brb = sb1("brb", [128, NE], F32)
                    p.op("sp", lambda e: e.dma_start(out=brb[:], in_=b_router.partition_broadcast(128)), writes=["brb"], dma="brb")
                    p.op("sp", lambda e: e.dma_start(out=gbc[:], in_=g_moe.partition_broadcast(128)), writes=["gbc"], dma="gbc")
                    bgl = sb1("bgl", [128, 4, 128], F32)
                    p.op("sp", lambda e: e.dma_start(out=bgl[:], in_=b_gu.rearrange("(a r) q -> r a q", r=128)), writes=["bgl"], dma="bgl")
                    pTf_r = Ring(ps1, "pTf", 1, [128, 4, 128], F32)
                    pbt, kpbt = pTf_r.next()
                    for a in range(4):
                        p.op("pe", lambda e, a=a: e.transpose(out=pbt[:, a, :], in_=bgl[:, a, :], identity=identf[:]), reads=["bgl", "identf"], writes=[kpbt])
                    p.op("dve", lambda e: e.tensor_copy(out=bgT[:].rearrange("p (a q) -> p a q", a=4), in_=pbt[:]), reads=[kpbt], writes=["bgT"])

                    OT_r = Ring(sb1, "OTl", 1, [128, 8, 512], BF16)
                    sq_r = Ring(sb1, "sqC", 2, [128, 512], BF16)
                    pss_r = Ring(ps1, "pssC", 1, [128, 512], F32)
                    rb_r = Ring(sb1, "rbC", 2, [128, 512], F32)
                    MX = sb1("MX", [128, 8, 512], BF16)
                    pA_r = Ring(ps1, "pAC", 2, [128, 512], F32)
                    xr_r = Ring(sb1, "xrC", 2, [128, D], F32)
                    ssc_r = Ring(sb1, "sscC", 2, [128, 1], F32)
                    u32_r = Ring(sb1, "u32C", 2, [128, D], F32)
                    uhT_r = Ring(sb1, "uhT", 2, [128, 8, 128], BF16)
                    tris = sb1("tris", [128, 128], BF16)
                    tmpf1 = sb1("tmpf1", [128, 128], F32)
                    p.op("pool", lambda e: e.memset(tmpf1[:], 1.0), writes=["tmpf1"])
                    p.op("pool", lambda e: e.affine_select(out=tmpf1[:], in_=tmpf1[:], pattern=[[1, 128]], compare_op=ALU.is_gt, fill=0.0,
                                                            base=0, channel_multiplier=-1), reads=["tmpf1"], writes=["tmpf1"])
                    p.op("dve", lambda e: e.tensor_copy(out=tris[:], in_=tmpf1[:]), reads=["tmpf1"], writes=["tris"])
                    gtmp_r = Ring(sb1, "gtmpC", 2, [128, NE], F32)
                    xnb_r = Ring(sb1, "xnbC", 2, [128, D], BF16)
                    ecap_i = sb1("ecap_i", [128, NE], I32)
                    ecap = sb1("ecap", [128, NE], F32)
                    p.op("pool", lambda e: e.iota(ecap_i[:], pattern=[[CAP, NE]], base=0, channel_multiplier=0), writes=["ecap_i"])
                    p.op("dve", lambda e: e.tensor_copy(out=ecap[:], in_=ecap_i[:]), reads=["ecap_i"], writes=["ecap"])
                    pq_r = Ring(sb1, "pq", 2, [128, NE], F32)
                    oh = sb1("oh", [128, NE], F32)
                    pr = sb1("pr", [128, NE], F32)
                    slf_r = Ring(sb1, "slf", 2, [128, 4], F32)
                    ulo_r = Ring(sb1, "uloC", 2, [128, D], BF16)
                    uloT_r = Ring(sb1, "uloT", 2, [128, 8, 128], BF16)
                    pTb_r = Ring(ps1, "pTb", 2, [128, 8, 128], BF16)
                    Wrh = sb1("Wrh", [128, 8, NE], BF16)
                    Wrl = sb1("Wrl", [128, 8, NE], BF16)
                    p.op("dve", lambda e: e.tensor_copy(out=Wrh[:], in_=Wr[:]), reads=["Wr"], writes=["Wrh"])
                    p.op("dve", lambda e: e.tensor_tensor(out=Wrl[:], in0=Wr[:], in1=Wrh[:], op=ALU.subtract), reads=["Wr", "Wrh"], writes=["Wrl"])
                    pLg_r = Ring(ps1, "pLg", 2, [128, 2, NE], F32)
                    lg_r = Ring(sb1, "lgC", 2, [128, NE], F32)
                    mx8_r = Ring(sb1, "mx8C", 2, [128, 8], F32)
                    msk_r = Ring(sb1, "mskC", 2, [128, NE], F32)
                    ex_r = Ring(sb1, "exC", 2, [128, NE], F32)
                    sm_r = Ring(sb1, "smC", 2, [128, 1], F32)
                    otv = OTd.rearrange("(c p) n -> p c n", p=128)
                    if stop_after == "C1a":
                        p.barrier()
                        p.op("sp", lambda e: e.dma_start(out=dbgH.rearrange("(t p) n -> p t n", p=128), in_=H[:]), reads=["H%d" % i_ for i_ in range(16)], dma="dbgH")
                        p.op("sp", lambda e: e.dma_start(out=dbgG.rearrange("(t p) n -> p t n", p=128), in_=Gt[:]), reads=["Gt%d" % i_ for i_ in range(16)], dma="dbgG")
                        p.finish_wait("sp"); p.emit(top); return nc
                    for G in range(4):
                        OT, kOT = OT_r.next()
                        p.op("sp", lambda e, OT=OT, G=G: e.dma_start(out=OT[:], in_=otv[:, :, G * 512:(G + 1) * 512]), writes=[kOT], dma=kOT)
                        rbs = []
                        for grp in range(2):
                            pss, kpss = pss_r.next()
                            for i in range(4):
                                c = grp * 4 + i
                                sq, ksq = sq_r.next()
                                p.op("act", lambda e, sq=sq, OT=OT, c=c: e.activation(out=sq[:], in_=OT[:, c, :], func=AF.Square), reads=[kOT], writes=[ksq])
                                p.op("pe", lambda e, sq=sq, pss=pss, i=i: e.matmul(out=pss[:], lhsT=ones[:], rhs=sq[:], start=(i == 0), stop=(i == 3)),
                                     reads=[ksq, "ones"], writes=[kpss])
                            rb, krb = rb_r.next()
                            rstd_from(rb[:], pss[:], 512, [kpss], [krb])
                            rbs.append((rb, krb))
                        for c in range(8):
                            rb, krb = rbs[c // 4]
                            p.op("dve", lambda e, c=c, rb=rb, OT=OT: e.scalar_tensor_tensor(out=MX[:, c, :], in0=OT[:, c, :], scalar=gcol[:, 5 + c:6 + c], in1=rb[:],
                                                                                        op0=ALU.mult, op1=ALU.mult),
                                 reads=[kOT, "gcol", krb], writes=["MX"])
                        def c1_tile(G, sub):
                            tile = G * 4 + sub
                            b_ = sub % 2
                            uhT, kuhT = uhT_r.tiles[b_], uhT_r.keys[b_]
                            uloT, kuloT = uloT_r.tiles[b_], uloT_r.keys[b_]
                            pq, kpq = pq_r.tiles[b_], pq_r.keys[b_]
                            slf, kslf = slf_r.tiles[b_], slf_r.keys[b_]
                            pLg2, kpLg = pLg_r.tiles[b_], pLg_r.keys[b_]
                            pLg = pLg2[:, 0, :]
                            pPos = pLg2[:, 1, :]
                            yield
                            xr, kxr = xr_r.next()
                            yield
                            p.op("sp", lambda e, xr=xr, tile=tile: e.dma_start(out=xr[:], in_=xo[tile * 128:(tile + 1) * 128, :]), writes=[kxr], dma=kxr)
                            yield
                            for half in range(2):
                                pA, kpA = pA_r.next()
                                for c in range(8):
                                    p.op("pe", lambda e, c=c, pA=pA, sub=sub, half=half: e.matmul(out=pA[:], lhsT=MX[:, c, sub * 128:(sub + 1) * 128],
                                                                                              rhs=Wo[:, c, half * 512:(half + 1) * 512], start=(c == 0), stop=(c == 7)),
                                         reads=["MX", "Wo"], writes=[kpA])
                                p.op("dve", lambda e, pA=pA, xr=xr, tile=tile, half=half: e.tensor_tensor(out=H[:, tile, half * 512:(half + 1) * 512], in0=pA[:],
                                                                                                      in1=xr[:, half * 512:(half + 1) * 512], op=ALU.add),
                                     reads=[kpA, kxr], writes=["H%d" % tile])
                            yield
                            yield
                            ssc, kss = ssc_r.next()
                            yield
                            p.op("act", lambda e, ssc=ssc, tile=tile: e.activation(out=junk[:], in_=H[:, tile, :], func=AF.Square, accum_out=ssc[:]),
                                 reads=["H%d" % tile], writes=["junkC", kss])
                            yield
                            rstd_from(ssc[:], ssc[:], D, [kss], [kss])
                            yield
                            u32, ku = u32_r.next()
                            yield
                            p.op("dve", lambda e, ssc=ssc, u32=u32, tile=tile: e.scalar_tensor_tensor(out=u32[:], in0=H[:, tile, :], scalar=ssc[:, 0:1], in1=gbc[:],
                                                                                                  op0=ALU.mult, op1=ALU.mult),
                                 reads=["H%d" % tile, kss, "gbc"], writes=[ku])
                            yield
                            xnb, kuhi = xnb_r.next()
                            yield
                            ulo, kulo = ulo_r.next()
                            yield
                            p.op("act", lambda e: e.copy(out=xnb[:], in_=u32[:]), reads=[ku], writes=[kuhi])
                            yield
                            p.op("pool", lambda e: e.dma_start(out=Ud[tile * 128:(tile + 1) * 128, :], in_=xnb[:]), reads=[kuhi], dma=kuhi + "s")
                            yield
                            p.op("dve", lambda e: e.tensor_tensor(out=ulo[:], in0=u32[:], in1=xnb[:], op=ALU.subtract), reads=[ku, kuhi], writes=[kulo])
                            yield
                            pT, kpT = pTb_r.next()
                            yield
                            for c in range(8):
                                p.op("pe", lambda e: e.transpose(out=pT[:, c, :], in_=xnb[:, c * 128:(c + 1) * 128], identity=ident[:]), reads=[kuhi, "ident"], writes=[kpT])
                            yield
                            p.op("dve", lambda e: e.tensor_copy(out=uhT[:], in_=pT[:]), reads=[kpT], writes=[kuhT])
                            yield
                            pT2, kpT2 = pTb_r.next()
                            yield
                            for c in range(8):
                                p.op("pe", lambda e: e.transpose(out=pT2[:, c, :], in_=ulo[:, c * 128:(c + 1) * 128], identity=ident[:]), reads=[kulo, "ident"], writes=[kpT2])
                            yield
                            p.op("act", lambda e: e.copy(out=uloT[:], in_=pT2[:]), reads=[kpT2], writes=[kuloT])
                            yield
                            n_ = 0
                            yield
                            for (A_, kA_, W_, kW_) in (("hi", kuhT, Wrh, "Wrh"), ("lo", kuloT, Wrh, "Wrh"), ("hi", kuhT, Wrl, "Wrl")):
                                for k in range(8):
                                    lh = uhT[:, k, :] if A_ == "hi" else uloT[:, k, :]
                                    p.op("pe", lambda e: e.matmul(out=pLg, lhsT=lh, rhs=W_[:, k, :], start=(n_ == 0), stop=(n_ == 23)),
                                         reads=[kA_, kW_], writes=[kpLg])
                                    n_ += 1
                            yield
                            lg, klg = lg_r.next()
                            yield
                            p.op("dve", lambda e, lg=lg: e.tensor_tensor(out=lg[:], in0=pLg, in1=brb[:], op=ALU.add), reads=[kpLg, "brb"], writes=[klg])
                            yield
                            mx8, kmx = mx8_r.next()
                            yield
                            p.op("dve", lambda e, lg=lg, mx8=mx8: e.max(out=mx8[:], in_=lg[:]), reads=[klg], writes=[kmx])
                            yield
                            msk, kmsk = msk_r.next()
                            yield
                            p.op("dve", lambda e, lg=lg, mx8=mx8, msk=msk: e.tensor_scalar(out=msk[:], in0=lg[:], scalar1=mx8[:, 3:4], scalar2=None, op0=ALU.is_ge),
                                 reads=[klg, kmx], writes=[kmsk])
                            yield
                            p.op("dve", lambda e, mx8=mx8: e.tensor_scalar(out=mx8[:, 7:8], in0=mx8[:, 0:1], scalar1=-1.0, scalar2=None, op0=ALU.mult),
                                 reads=[kmx, kmsk], writes=[kmx])
                            yield
                            ex, kex = ex_r.next()
                            yield
                            p.op("act", lambda e, lg=lg, mx8=mx8, ex=ex: e.activation(out=ex[:], in_=lg[:], func=AF.Exp, bias=mx8[:, 7:8]), reads=[klg, kmx], writes=[kex])
                            yield
                            sm_, ksm = sm_r.next()
                            yield
                            p.op("dve", lambda e, ex=ex, msk=msk: e.tensor_tensor(out=ex[:], in0=ex[:], in1=msk[:], op=ALU.mult), reads=[kex, kmsk], writes=[kex])
                            yield
                            p.op("dve", lambda e, ex=ex, sm_=sm_: e.reduce_sum(out=sm_[:], in_=ex[:], axis=mybir.AxisListType.X), reads=[kex], writes=[ksm])
                            yield
                            p.op("dve", lambda e, sm_=sm_: e.reciprocal(out=sm_[:], in_=sm_[:]), reads=[ksm], writes=[ksm])
                            yield
                            p.op("dve", lambda e, ex=ex, sm_=sm_, tile=tile: e.tensor_scalar(out=Gt[:, tile, :], in0=ex[:], scalar1=sm_[:, 0:1], scalar2=None, op0=ALU.mult),
                                 reads=[kex, ksm], writes=["Gt%d" % tile])
                            yield
                            p.op("dve", lambda e: e.tensor_copy(out=maskb[:, tile, :], in_=msk[:]), reads=[kmsk], writes=["maskb%d" % tile])
                            yield
                            gtmp, kgtmp = gtmp_r.next()
                            yield
                            p.op("pe", lambda e: e.matmul(out=pPos, lhsT=tris[:], rhs=maskb[:, tile, :], start=True, stop=(tile == 0)),
                                 reads=["tris", "maskb%d" % tile], writes=[kpLg])
                            yield
                            for j_ in range(tile):
                                p.op("pe", lambda e: e.matmul(out=pPos, lhsT=ones[:], rhs=maskb[:, j_, :], start=False, stop=(j_ == tile - 1)),
                                     reads=["ones", "maskb%d" % j_], writes=[kpLg])
                            yield
                            p.op("dve", lambda e: e.scalar_tensor_tensor(out=gtmp[:], in0=pPos, scalar=1.0, in1=msk[:], op0=ALU.add, op1=ALU.mult),
                                 reads=[kpLg, kmsk, kgtmp], writes=[kgtmp])
                            yield
                            p.op("dve", lambda e: e.tensor_scalar(out=posm[:, tile, :], in0=gtmp[:], scalar1=-1.0, scalar2=None, op0=ALU.add),
                                 reads=[kgtmp], writes=["posm%d" % tile])
                            yield
                            p.op("dve", lambda e: e.scalar_tensor_tensor(out=pq[:], in0=pPos, scalar=float(CAP - 1), in1=ecap[:], op0=ALU.min, op1=ALU.add),
                                 reads=[kpLg, "ecap"], writes=[kpq])
                            yield
                            yield
                            p.op("dve", lambda e: e.scalar_tensor_tensor(out=gtmp[:], in0=pPos, scalar=float(CAP) - 0.5, in1=Gt[:, tile, :], op0=ALU.is_lt, op1=ALU.mult),
                                 reads=[kpLg, "Gt%d" % tile, kgtmp], writes=[kgtmp])
                            yield
                            for j_ in range(4):
                                p.op("dve", lambda e: e.tensor_scalar(out=oh[:], in0=lg[:], scalar1=mx8[:, j_:j_ + 1], scalar2=None, op0=ALU.is_equal),
                                     reads=[klg, kmx], writes=["oh"])
                                p.op("dve", lambda e: e.tensor_tensor(out=pr[:], in0=oh[:], in1=pq[:], op=ALU.mult), reads=["oh", kpq], writes=["pr"])
                                p.op("dve", lambda e: e.reduce_sum(out=slf[:, j_:j_ + 1], in_=pr[:], axis=mybir.AxisListType.X), reads=["pr"], writes=[kslf])
                                p.op("dve", lambda e: e.tensor_tensor(out=pr[:], in0=oh[:], in1=gtmp[:], op=ALU.mult), reads=["oh", kgtmp, "pr"], writes=["pr"])
                                p.op("dve", lambda e: e.reduce_sum(out=gj[:, tile, j_:j_ + 1], in_=pr[:], axis=mybir.AxisListType.X), reads=["pr"], writes=["gj%d" % tile])
                            yield
                            p.op("dve", lambda e: e.tensor_copy(out=slI[:, tile, :], in_=slf[:]), reads=[kslf], writes=["slI%d" % tile])

                        for pair in range(2):
                            gens = [c1_tile(G, pair * 2), c1_tile(G, pair * 2 + 1)]
                            while gens:
                                for g_ in list(gens):
                                    try:
                                        next(g_)
                                    except StopIteration:
                                        gens.remove(g_)
                    p.barrier()
                    if stop_after == "C1":
                        if debug:
                            p.op("sp", lambda e: e.dma_start(out=dbgS.rearrange("(t p) n -> p t n", p=128), in_=slI[:]), reads=["slI%d" % i_ for i_ in range(16)], dma="dbgS")
                            p.op("sp", lambda e: e.dma_start(out=dbgJ.rearrange("(t p) n -> p t n", p=128), in_=gj[:]), reads=["gj%d" % i_ for i_ in range(16)], dma="dbgJ")
                            p.op("sp", lambda e: e.dma_start(out=dbgH.rearrange("(t p) n -> p t n", p=128), in_=H[:]), reads=["H%d" % i_ for i_ in range(16)], dma="dbgH")
                            p.op("sp", lambda e: e.dma_start(out=dbgG.rearrange("(t p) n -> p t n", p=128), in_=Gt[:]), reads=["Gt%d" % i_ for i_ in range(16)], dma="dbgG")
                        p.finish_wait("sp"); p.emit(top); return nc
                s2 = ExitStack()
                with s2:
                    sb2, ps2 = mk_alloc(s2)
                    W_r = Ring(sb2, "Wx", 6, [128, 8, 512], BF16)
                    bd_r = Ring(sb2, "bdn", 2, [1, D], BF16)
                    pGL_r = Ring(ps2, "pGL", 4, [128, 512], F32)
                    pA_r = Ring(ps2, "pA", 2, [128, 512], F32)
                    pTs = ps2("pTs", [128, 8, 128], BF16)
                    ptk = ps2("ptk", [128, 8], F32)
                    gl_r = Ring(sb2, "gl", 1, [128, CAP], F32)
                    sg_r = Ring(sb2, "sg", 1, [128, CAP], F32)
                    Sel = sb2("Sel", [128, 16, CAP], BF16)
                    Xe_r = Ring(sb2, "Xe", 2, [128, NS, D], BF16)
                    XeT = sb2("XeT", [128, 8, CAP], BF16)
                    aT = sb2("aTs", [128, 8, CAP], BF16)
                    Yst_r = Ring(sb2, "Yst", 2, [128, D], F32)
                    tks = sb2("tks", [128, 8], F32)
                    tkf = sb2("tkf", [128, 4], F32)
                    tkI_r = Ring(sb2, "tkI", 2, [128, 4], I32)
                    iota_i = sb2("iota_i", [128, CAP], I32)
                    iota_f = sb2("iota_f", [128, CAP], F32)
                    p.op("pool", lambda e: e.iota(iota_i[:], pattern=[[1, CAP]], base=0, channel_multiplier=0), writes=["iota_i"])
                    p.op("dve", lambda e: e.tensor_copy(out=iota_f[:], in_=iota_i[:]), reads=["iota_i"], writes=["iota_f"])
                    tid = sb2("tid", [128, 16], I32)
                    tidx = sb2("tidx", [128, 16], I32)
                    tidhl = sb2("tidhl", [128, 16, 2], BF16)
                    p.op("pool", lambda e: e.iota(tid[:], pattern=[[128, 16]], base=0, channel_multiplier=1), writes=["tid"])
                    p.op("dve", lambda e: e.tensor_scalar(out=tidx[:], in0=tid[:], scalar1=6, scalar2=None, op0=ALU.arith_shift_right), reads=["tid"], writes=["tidx"])
                    p.op("dve", lambda e: e.tensor_copy(out=tidhl[:, :, 0], in_=tidx[:]), reads=["tidx"], writes=["tidhl"])
                    p.op("dve", lambda e: e.tensor_scalar(out=tidx[:], in0=tid[:], scalar1=63, scalar2=None, op0=ALU.bitwise_and), reads=["tid", "tidx", "tidhl"], writes=["tidx"])
                    p.op("dve", lambda e: e.tensor_copy(out=tidhl[:, :, 1], in_=tidx[:]), reads=["tidx", "tidhl"], writes=["tidhl"])
                    bg3 = bgT[:].rearrange("p (e c) -> p e c", c=16)
                    p.op("dve", lambda e: e.tensor_scalar(out=bg3[:, :, 8:16], in0=bg3[:, :, 8:16], scalar1=1.0, scalar2=None, op0=ALU.add),
                         reads=["bgT"], writes=["bgT"])

                    def load_w(src, slot):
                        Wt, kW = W_r.tiles[slot], W_r.keys[slot]
                        p.op("pool", lambda e: e.dma_start(out=Wt[:], in_=src.rearrange("(c p) n -> p c n", p=128)), writes=[kW], dma=kW)
                        return Wt, kW

                    def load_bd(ex_):
                        bd, kbd = bd_r.next()
                        p.op("pool", lambda e: e.dma_start(out=bd[:], in_=b_dn[ex_:ex_ + 1, :]), writes=[kbd], dma=kbd)
                        return bd, kbd

                    xe_of = {}

                    def sel_build(ex_, tiles):
                        for tile in tiles:
                            p.op("dve", lambda e: e.tensor_scalar(out=Sel[:, tile, :], in0=iota_f[:], scalar1=posm[:, tile, ex_:ex_ + 1], scalar2=None, op0=ALU.is_equal),
                                 reads=["iota_f", "posm%d" % tile], writes=["Sel%d" % tile])

                    def dispatch(ex_, build=True):
                        if build:
                            sel_build(ex_, range(16))
                        for s_ in range(NS):
                            for tile in range(16):
                                p.op("pe", lambda e: e.matmul(out=ptk[:, 2 * s_:2 * s_ + 2], lhsT=Sel[:, tile, s_ * 128:(s_ + 1) * 128], rhs=tidhl[:, tile, :],
                                                              start=(tile == 0), stop=(tile == 15)), reads=["Sel%d" % tile, "tidhl"], writes=["ptk"])
                        p.op("dve", lambda e: e.tensor_copy(out=tks[:, 0:2 * NS], in_=ptk[:, 0:2 * NS]), reads=["ptk"], writes=["tks"])
                        tk3 = tks[:, 0:2 * NS].rearrange("p (s t) -> p s t", t=2)
                        p.op("dve", lambda e: e.scalar_tensor_tensor(out=tkf[:, 0:NS], in0=tk3[:, :, 0], scalar=64.0, in1=tk3[:, :, 1], op0=ALU.mult, op1=ALU.add),
                             reads=["tks"], writes=["tkf"])
                        tkI, ktkI = tkI_r.next()
                        p.op("dve", lambda e: e.tensor_copy(out=tkI[:, 0:NS], in_=tkf[:, 0:NS]), reads=["tkf"], writes=[ktkI])
                        Xe, kXe = Xe_r.next()
                        for s_ in range(NS):
                            p.op("pool", lambda e: e.indirect_dma_start(out=Xe[:, s_, :], out_offset=None, in_=Ud[:, :],
                                                                         in_offset=bass.IndirectOffsetOnAxis(ap=tkI[:, s_:s_ + 1], axis=0)),
                                 reads=[ktkI], writes=[kXe], dma=kXe)
                        xe_of[ex_] = (Xe, kXe)

                    def transposes_s(ex_, s_):
                        Xe, kXe = xe_of[ex_]
                        for k in range(8):
                            p.op("pe", lambda e: e.transpose(out=pTs[:, k, :], in_=Xe[:, s_, k * 128:(k + 1) * 128], identity=ident[:]),
                                 reads=[kXe, "ident"], writes=["pTs"])
                        if s_ % 2 == 0:
                            p.op("act", lambda e: e.copy(out=XeT[:, :, s_ * 128:(s_ + 1) * 128], in_=pTs[:]), reads=["pTs"], writes=["XeT"])
                        else:
                            p.op("dve", lambda e: e.tensor_copy(out=XeT[:, :, s_ * 128:(s_ + 1) * 128], in_=pTs[:]), reads=["pTs"], writes=["XeT"])
                        if s_ == NS - 1:
                            xe_of.pop(ex_)

                    def transposes(ex_):
                        for s_ in range(NS):
                            transposes_s(ex_, s_)

                    def gu_stage(ex_, st, Wg, kWg, Wl, kWl, sel_for=None):
                        for mc in range(4):
                            if sel_for is not None:
                                sel_build(sel_for, range(mc * 4, mc * 4 + 4))
                            c = st * 4 + mc
                            pG, kpG = pGL_r.next()
                            pLn, kpLn = pGL_r.next()
                            for k in range(8):
                                p.op("pe", lambda e: e.matmul(out=pG[:, 0:CAP], lhsT=Wg[:, k, mc * 128:(mc + 1) * 128], rhs=XeT[:, k, :],
                                                              start=(k == 0), stop=(k == 7)), reads=[kWg, "XeT"], writes=[kpG])
                            for k in range(8):
                                p.op("pe", lambda e: e.matmul(out=pLn[:, 0:CAP], lhsT=Wl[:, k, mc * 128:(mc + 1) * 128], rhs=XeT[:, k, :],
                                                              start=(k == 0), stop=(k == 7)), reads=[kWl, "XeT"], writes=[kpLn])
                            gl, kgl = gl_r.next()
                            sg, ksg = sg_r.next()
                            bgc = ex_ * 16 + c
                            blc = ex_ * 16 + 8 + c
                            kaT = "aT_%d" % st
                            p.op("dve", lambda e: e.tensor_scalar(out=gl[:], in0=pG[:, 0:CAP], scalar1=bgT[:, bgc:bgc + 1], scalar2=7.0, op0=ALU.add, op1=ALU.min),
                                 reads=[kpG, "bgT"], writes=[kgl])
                            p.op("act", lambda e: e.activation(out=sg[:], in_=gl[:], func=AF.Sigmoid, scale=1.702), reads=[kgl], writes=[ksg])
                            p.op("dve", lambda e: e.tensor_tensor(out=sg[:], in0=sg[:], in1=gl[:], op=ALU.mult), reads=[kgl, ksg], writes=[ksg])
                            p.op("dve", lambda e: e.tensor_scalar(out=gl[:], in0=pLn[:, 0:CAP], scalar1=bgT[:, blc:blc + 1], scalar2=-6.0, op0=ALU.add, op1=ALU.max),
                                 reads=[kpLn, "bgT", kgl], writes=[kgl])
                            p.op("dve", lambda e: e.scalar_tensor_tensor(out=aT[:, c, :], in0=gl[:], scalar=8.0, in1=sg[:], op0=ALU.min, op1=ALU.mult),
                                 reads=[ksg, kgl], writes=[kaT])

                    def dn_stage(ex_, d0, d1, bd, kbd, nxt=None):
                        for s_ in range(NS):
                            if nxt is not None:
                                transposes_s(nxt, s_)
                            Yst, kY = Yst_r.next()
                            for half in range(2):
                                Wd, kWd = (d0, d1)[half]
                                pA, kpA = pA_r.next()
                                for c in range(8):
                                    p.op("pe", lambda e: e.matmul(out=pA[:], lhsT=aT[:, c, s_ * 128:(s_ + 1) * 128], rhs=Wd[:, c, :], start=(c == 0), stop=False),
                                         reads=["aT_%d" % (c // 4), kWd], writes=[kpA])
                                p.op("pe", lambda e: e.matmul(out=pA[:], lhsT=ones[0:1, :], rhs=bd[0:1, half * 512:(half + 1) * 512], start=False, stop=True),
                                     reads=["ones", kbd], writes=[kpA])
                                p.op("act", lambda e: e.copy(out=Yst[:, half * 512:(half + 1) * 512], in_=pA[:]), reads=[kpA], writes=[kY])
                            r0 = ex_ * CAP + s_ * 128
                            p.op("sp", lambda e: e.dma_start(out=Yd[r0:r0 + 128, :], in_=Yst[:]), reads=[kY], dma=kY + "s")

                    def loads_gl0(ex_):
                        return load_w(w_gu[ex_, :, 0:512], 0), load_w(w_gu[ex_, :, 1024:1536], 1)

                    def loads_gl1(ex_):
                        return load_w(w_gu[ex_, :, 512:1024], 2), load_w(w_gu[ex_, :, 1536:2048], 3)

                    def loads_d(ex_):
                        return load_w(w_dn[ex_, :, 0:512], 4), load_w(w_dn[ex_, :, 512:1024], 5), load_bd(ex_)

                    g0, l0 = loads_gl0(0)
                    g1, l1 = loads_gl1(0)
                    d0, d1, (bd, kbd) = loads_d(0)
                    dispatch(0)
                    dispatch(1)
                    transposes(0)
                    for ex_ in range(NE):
                        gu_stage(ex_, 0, g0[0], g0[1], l0[0], l0[1], sel_for=(ex_ + 2 if ex_ + 2 < NE else None))
                        if ex_ + 1 < NE:
                            g0n, l0n = loads_gl0(ex_ + 1)
                        gu_stage(ex_, 1, g1[0], g1[1], l1[0], l1[1])
                        if ex_ + 2 < NE:
                            dispatch(ex_ + 2, build=False)
                        if ex_ + 1 < NE:
                            g1n, l1n = loads_gl1(ex_ + 1)
                        dn_stage(ex_, d0, d1, bd, kbd, nxt=(ex_ + 1 if ex_ + 1 < NE else None))
                        if ex_ + 1 < NE:
                            d0, d1, (bd, kbd) = loads_d(ex_ + 1)
                            g0, l0, g1, l1 = g0n, l0n, g1n, l1n
                    p.barrier()
                    Yg_r = Ring(sb2, "Yg", 4, [128, D], F32)
                    for tile in range(16):
                        hk = "H%d" % tile
                        for j_ in range(4):
                            Yg, kYg = Yg_r.next()
                            p.op("pool", lambda e: e.indirect_dma_start(out=Yg[:, :], out_offset=None, in_=Yd[:, :],
                                                                         in_offset=bass.IndirectOffsetOnAxis(ap=slI[:, tile, j_:j_ + 1], axis=0)),
                                 reads=["slI%d" % tile], writes=[kYg], dma=kYg)
                            p.op("dve", lambda e: e.scalar_tensor_tensor(out=H[:, tile, :], in0=Yg[:], scalar=gj[:, tile, j_:j_ + 1], in1=H[:, tile, :], op0=ALU.mult, op1=ALU.add),
                                 reads=[kYg, "gj%d" % tile, hk], writes=[hk])
                    p.barrier()
                    if stop_after == "C2":
                        if debug:
                            p.op("sp", lambda e: e.dma_start(out=dbgH.rearrange("(t p) n -> p t n", p=128), in_=H[:]), reads=["H%d" % i_ for i_ in range(16)], dma="dbgH")
                            p.op("sp", lambda e: e.dma_start(out=dbgG.rearrange("(t p) n -> p t n", p=128), in_=Gt[:]), reads=["Gt%d" % i_ for i_ in range(16)], dma="dbgG")
                        p.finish_wait("sp"); p.emit(top); return nc
                s3 = ExitStack()
                with s3:
                    sb3, ps3 = mk_alloc(s3)
                    gbc = sb3("gbc3", [128, D], F32)
                    junk = sb3("junkC3", [128, D], BF16)
                    Wpg = sb3("Wpg", [128, 8, D], BF16)
                    Wpp = sb3("Wpp", [128, 2, D], BF16)
                    p.op("pool", lambda e: e.dma_start(out=Wpg[:], in_=w_pg.rearrange("(c p) n -> p c n", p=128)), writes=["Wpg"], dma="Wpg")
                    p.op("pool", lambda e: e.dma_start(out=Wpp[:], in_=w_pp.rearrange("(c p) n -> p c n", p=128)), writes=["Wpp"], dma="Wpp")
                    gfin = sb3("gfin", [128, D], F32)
                    p.op("sp", lambda e: e.dma_start(out=gbc[:], in_=g_ple.partition_broadcast(128)), writes=["gbc"], dma="gbc3")
                    p.op("sp", lambda e: e.dma_start(out=gfin[:], in_=g_final.partition_broadcast(128)), writes=["gfin"], dma="gfin")
                    ss3_r = Ring(sb3, "ss3", 2, [128, 1], F32)
                    u3_r = Ring(sb3, "u3", 2, [128, D], BF16)
                    pT3_r = Ring(ps3, "pT3", 2, [128, 8, 128], BF16)
                    u3T_r = Ring(sb3, "u3T", 2, [128, 8, 128], BF16)
                    pp_r = Ring(sb3, "ppl", 2, [128, 256], F32)
                    ppb_r = Ring(sb3, "ppb", 2, [128, 256], BF16)
                    ppT_r = Ring(sb3, "ppT", 2, [128, 2, 128], BF16)
                    pg_r = Ring(ps3, "pg3", 2, [128, 512], F32)
                    pj_r = Ring(ps3, "pj3", 2, [128, 512], F32)
                    sg3_r = Ring(sb3, "sg3", 2, [128, 512], F32)
                    o_r = Ring(sb3, "o3", 2, [128, D], F32)
                    def c3_tile(tile):
                        hk = "H%d" % tile
                        yield
                        ss3, kss = ss3_r.next()
                        yield
                        p.op("act", lambda e, ss3=ss3, tile=tile: e.activation(out=junk[:], in_=H[:, tile, :], func=AF.Square, accum_out=ss3[:]), reads=[hk], writes=["junkC", kss])
                        yield
                        rstd_from(ss3[:], ss3[:], D, [kss], [kss])
                        yield
                        u3, ku3 = u3_r.next()
                        yield
                        p.op("dve", lambda e, ss3=ss3, u3=u3, tile=tile: e.scalar_tensor_tensor(out=u3[:], in0=H[:, tile, :], scalar=ss3[:, 0:1], in1=gbc[:], op0=ALU.mult, op1=ALU.mult),
                             reads=[hk, kss, "gbc"], writes=[ku3])
                        yield
                        pT, kpT = pT3_r.next()
                        yield
                        for c in range(8):
                            p.op("pe", lambda e, c=c, pT=pT, u3=u3: e.transpose(out=pT[:, c, :], in_=u3[:, c * 128:(c + 1) * 128], identity=ident[:]), reads=[ku3, "ident"], writes=[kpT])
                        yield
                        u3T, ku3T = u3T_r.next()
                        yield
                        p.op("act", lambda e, pT=pT, u3T=u3T: e.copy(out=u3T[:], in_=pT[:]), reads=[kpT], writes=[ku3T])
                        yield
                        pp, kpp = pp_r.next()
                        yield
                        p.op("sp", lambda e, pp=pp, tile=tile: e.dma_start(out=pp[:], in_=po[tile * 128:(tile + 1) * 128, :]), writes=[kpp], dma=kpp)
                        yield
                        ppb, kppb = ppb_r.next()
                        yield
                        p.op("pool", lambda e, pp=pp, ppb=ppb: e.tensor_copy(out=ppb[:], in_=pp[:]), reads=[kpp], writes=[kppb])
                        yield
                        pT2, kpT2 = pT3_r.next()
                        yield
                        for c in range(2):
                            p.op("pe", lambda e, c=c, pT2=pT2, ppb=ppb: e.transpose(out=pT2[:, c, :], in_=ppb[:, c * 128:(c + 1) * 128], identity=ident[:]), reads=[kppb, "ident"], writes=[kpT2])
                        yield
                        ppT, kppT = ppT_r.next()
                        yield
                        p.op("act", lambda e, pT2=pT2, ppT=ppT: e.copy(out=ppT[:], in_=pT2[:, 0:2, :]), reads=[kpT2], writes=[kppT])
                        yield
                        for half in range(2):
                            pg, kpg = pg_r.next()
                            pj, kpj = pj_r.next()
                            for c in range(8):
                                p.op("pe", lambda e, c=c, pg=pg, u3T=u3T, half=half: e.matmul(out=pg[:], lhsT=u3T[:, c, :], rhs=Wpg[:, c, half * 512:(half + 1) * 512], start=(c == 0), stop=(c == 7)),
                                     reads=[ku3T, "Wpg"], writes=[kpg])
                            for c in range(2):
                                p.op("pe", lambda e, c=c, pj=pj, ppT=ppT, half=half: e.matmul(out=pj[:], lhsT=ppT[:, c, :], rhs=Wpp[:, c, half * 512:(half + 1) * 512], start=(c == 0), stop=(c == 1)),
                                     reads=[kppT, "Wpp"], writes=[kpj])
                            sg, ksg = sg3_r.next()
                            p.op("act", lambda e, sg=sg, pg=pg: e.activation(out=sg[:], in_=pg[:], func=AF.Sigmoid), reads=[kpg], writes=[ksg])
                            p.op("dve", lambda e, sg=sg, pj=pj: e.tensor_tensor(out=sg[:], in0=sg[:], in1=pj[:], op=ALU.mult), reads=[ksg, kpj], writes=[ksg])
                            p.op("dve", lambda e, sg=sg, tile=tile, half=half: e.tensor_tensor(out=H[:, tile, half * 512:(half + 1) * 512], in0=H[:, tile, half * 512:(half + 1) * 512], in1=sg[:], op=ALU.add),
                                 reads=[ksg, hk], writes=[hk])
                        yield
                        ss4, kss4 = ss3_r.next()
                        yield
                        p.op("act", lambda e, ss4=ss4, tile=tile: e.activation(out=junk[:], in_=H[:, tile, :], func=AF.Square, accum_out=ss4[:]), reads=[hk], writes=["junkC", kss4])
                        yield
                        rstd_from(ss4[:], ss4[:], D, [kss4], [kss4])
                        yield
                        ot, kot = o_r.next()
                        yield
                        p.op("dve", lambda e, ss4=ss4, ot=ot, tile=tile: e.scalar_tensor_tensor(out=ot[:], in0=H[:, tile, :], scalar=ss4[:, 0:1], in1=gfin[:], op0=ALU.mult, op1=ALU.mult),
                             reads=[hk, kss4, "gfin"], writes=[kot])
                        yield
                        p.op("sp", lambda e, ot=ot, tile=tile: e.dma_start(out=yo[tile * 128:(tile + 1) * 128, :], in_=ot[:]), reads=[kot], writes=["yo"], dma=kot + "s")

                    for pair in range(8):
                        gens = [c3_tile(pair * 2), c3_tile(pair * 2 + 1)]
                        while gens:
                            for g_ in list(gens):
                                try:
                                    next(g_)
                                except StopIteration:
                                    gens.remove(g_)
        p.finish_wait("sp")
        p.emit(top)
    return nc


_CACHE = {}


def _perm(j):
    idx = []
    for t in range(4):
        for blk in ORDER[j]:
            b0 = (8 * t + blk) * 128
            idx.append(np.arange(b0, b0 + 128))
    return np.concatenate(idx)


def kernel(x, p, positions, w_in, g_attn, g_cq, w_uq, g_ckv, w_ukv, g_out_mla, g_out_sb, w_o,
           g_moe, w_router, b_router, w_gu, b_gu, w_dn, b_dn, g_ple, w_ple_gate, w_ple_proj, g_final):
    if "nc" not in _CACHE:
        _CACHE["nc"] = build_program()
    nc = _CACHE["nc"]
    in_maps, perms = make_in_maps(x, p, positions, w_in, g_attn, g_cq, w_uq, g_ckv, w_ukv, g_out_mla, g_out_sb, w_o,
                                  g_moe, w_router, b_router, w_gu, b_gu, w_dn, b_dn, g_ple, w_ple_gate, w_ple_proj, g_final)
    res = run_bass_kernel_spmd(nc, in_maps, core_ids=list(range(8)))
    out = np.empty((4, S, D), np.float32)
    for c in range(8):
        b, j = c // 2, c % 2
        out[b, perms[j]] = np.asarray(res.results[c]["yo"])
    return out


def make_in_maps(x, p, positions, w_in, g_attn, g_cq, w_uq, g_ckv, w_ukv, g_out_mla, g_out_sb, w_o,
                 g_moe, w_router, b_router, w_gu, b_gu, w_dn, b_dn, g_ple, w_ple_gate, w_ple_proj, g_final):
    f = lambda a: np.ascontiguousarray(np.asarray(a))
    x = f(x); p = f(p); positions = f(positions)
    invf = np.zeros((128, 1), np.float32)
    fr = (10000.0 ** (-np.arange(0, 32, 2, dtype=np.float32) / 32.0)).astype(np.float32)
    invf[0:16, 0] = fr
    invf[16:32, 0] = fr
    shared = {
        "invf": invf,
        "w_in": f(w_in[0]), "g_attn": f(g_attn[0:1]), "g_cq": f(g_cq[0:1]), "w_uq": f(w_uq[0]),
        "g_ckv": f(g_ckv[0:1]), "w_ukv": f(w_ukv[0]),
        "g_out": f(np.concatenate([np.asarray(g_out_mla[0]), np.asarray(g_out_sb[0])])[None, :]),
        "w_o": f(w_o[0]), "g_moe": f(g_moe[0:1]), "w_router": f(w_router[0]), "b_router": f(b_router[0:1]),
        "w_gu": f(w_gu[0]), "b_gu": f(np.asarray(b_gu[0]).reshape(NE * 16, 128)), "w_dn": f(w_dn[0]), "b_dn": f(b_dn[0]),
        "g_ple": f(g_ple[0:1]), "w_pg": f(w_ple_gate[0]), "w_pp": f(w_ple_proj[0]), "g_final": f(np.asarray(g_final)[None, :]),
    }
    in_maps = []
    perms = [_perm(0), _perm(1)]
    for c in range(8):
        b, j = c // 2, c % 2
        pm = perms[j]
        qr = np.concatenate([np.arange(blk * 128, blk * 128 + 128) for blk in ORDER[j]]).astype(np.float32)[None, :]
        m = dict(shared)
        m["xa"] = x[b]
        m["xo"] = f(x[b][pm])
        m["po"] = f(p[0, b][pm])
        m["posa"] = f(positions[b:b + 1].astype(np.int32))
        m["poso"] = f(positions[b:b + 1, pm].astype(np.int32))
        m["qrel"] = f(qr)
        in_maps.append(m)
    return in_maps, perms
```

```python
from contextlib import ExitStack
import numpy as np
import concourse.bass as bass
import concourse.mybir as mybir
from concourse.bass_utils import run_bass_kernel_spmd

F32 = mybir.dt.float32
BF16 = mybir.dt.bfloat16
I32 = mybir.dt.int32
AF = mybir.ActivationFunctionType
ALU = mybir.AluOpType

ENGS = ("pe", "act", "dve", "pool", "sp")
S = 4096
D = 1024
NE = 32
ORDER = ([6, 5, 3, 0], [7, 4, 2, 1])
NDIAG = [512, 512, 384, 384, 256, 256, 128, 128]
NEG = -30000.0
EPS = 1e-6


class Prog:
    def __init__(self, nc):
        self.nc = nc
        self.ops = {e: [] for e in ENGS}
        self.vcs = {}
        self.cur = {e: {} for e in ENGS}
        self.last_w = {}
        self.readers = {}
        self.excl = set()

    def op(self, eng, fn, reads=(), writes=(), dma=None):
        rec_ = _Rec()
        fn(rec_)
        assert len(rec_.calls) == 1
        fn = rec_.calls[0]
        clk = ("dma:" + dma) if dma else eng
        deps = []
        reads = list(reads)
        writes = list(writes)
        for k in reads:
            if k in self.excl and k not in writes:
                writes.append(k)
        for k in reads:
            lw = self.last_w.get(k)
            if lw:
                deps.append(lw)
        for k in writes:
            lw = self.last_w.get(k)
            if lw:
                deps.append(lw)
            for c, i in self.readers.get(k, {}).items():
                deps.append((c, i))
        cur = self.cur[eng]
        wmax = {}
        for (c, i) in deps:
            if c == "pe" and eng == "pe" and not dma:
                continue
            if cur.get(c, 0) >= i:
                continue
            wmax[c] = max(wmax.get(c, 0), i)
            for c2, i2 in self.vcs[c][i - 1].items():
                if cur.get(c2, 0) < i2:
                    cur[c2] = i2
            if cur.get(c, 0) < i:
                cur[c] = i
        vc = dict(cur)
        lst = self.vcs.setdefault(clk, [])
        lst.append(vc)
        idx = len(lst)
        vc[clk] = idx
        rec = {"fn": fn, "waits": wmax, "clk": clk, "idx": idx}
        self.ops[eng].append(rec)
        for k in reads:
            self.readers.setdefault(k, {})[clk] = idx
        for k in writes:
            self.last_w[k] = (clk, idx)
            self.readers[k] = {}
        return rec

    def finish_wait(self, eng):
        waits = {}
        for c, l in self.vcs.items():
            if len(l) and self.cur[eng].get(c, 0) < len(l):
                waits[c] = len(l)
                self.cur[eng][c] = len(l)
        self.ops[eng].append({"fn": None, "waits": waits, "clk": None, "idx": None})

    def barrier(self):
        for e in ENGS:
            self.finish_wait(e)
        full = {c: len(l) for c, l in self.vcs.items()}
        for e in ENGS:
            self.cur[e] = dict(full)

    def emit(self, stack):
        nc = self.nc
        waited = {}
        for e in ENGS:
            for r in self.ops[e]:
                for c, i in r["waits"].items():
                    waited.setdefault(c, set()).add(i)
        sems, semval = {}, {}
        for c, l in self.vcs.items():
            if c not in waited:
                continue
            sems[c] = stack.enter_context(nc.semaphore("s_" + c.replace(":", "_")))
            isd = c.startswith("dma:")
            v, m = 0, {}
            for i in range(1, len(l) + 1):
                if isd or i in waited[c]:
                    v += 16 if isd else 1
                    m[i] = v
            semval[c] = m
        block = stack.enter_context(nc.Block())
        engobj = {"pe": "tensor", "act": "scalar", "dve": "vector", "pool": "gpsimd", "sp": "sync"}

        def make(e):
            def body(eng):
                for r in self.ops[e]:
                    for c, i in r["waits"].items():
                        eng.wait_ge(sems[c], semval[c][i])
                    if r["fn"] is None:
                        continue
                    name, a, k = r["fn"]
                    ins = getattr(eng, name)(*a, **k)
                    c, i = r["clk"], r["idx"]
                    if c in sems and i in semval[c]:
                        ins.then_inc(sems[c], 16 if c.startswith("dma:") else 1)
            return body

        for e in ENGS:
            if self.ops[e]:
                getattr(block, engobj[e])(make(e))


class _Rec:
    def __init__(self):
        self.calls = []

    def __getattr__(self, name):
        def f(*a, **k):
            self.calls.append((name, a, k))
        return f


class Ring:
    def __init__(self, alloc, name, n, shape, dtype):
        self.tiles = [alloc("%s%d" % (name, i), shape, dtype) for i in range(n)]
        self.keys = ["%s%d" % (name, i) for i in range(n)]
        self.i = 0

    def next(self):
        t, k = self.tiles[self.i % len(self.tiles)], self.keys[self.i % len(self.tiles)]
        self.i += 1
        return t, k


class _Stop(Exception):
    pass


def build_program(stop_after=None, debug=False):
    nc = bass.Bass("TRN2", target_bir_lowering=False)

    def din(name, shape, dt=F32):
        return nc.dram_tensor(name, list(shape), dt, kind="ExternalInput").ap()

    def dscr(name, shape, dt=BF16):
        return nc.dram_tensor(name, list(shape), dt, kind="ExternalOutput" if debug else "Internal").ap()

    xa = din("xa", [S, D])
    xo = din("xo", [2048, D])
    po = din("po", [2048, 256])
    posa = din("posa", [1, S], I32)
    poso = din("poso", [1, 2048], I32)
    qrel = din("qrel", [1, 512])
    invf = din("invf", [128, 1])
    w_in = din("w_in", [D, 2208])
    g_attn = din("g_attn", [1, D])
    g_cq = din("g_cq", [1, 384])
    w_uq = din("w_uq", [384, 768])
    g_ckv = din("g_ckv", [1, 256])
    w_ukv = din("w_ukv", [256, 1024])
    g_out = din("g_out", [1, 1024])
    w_o = din("w_o", [D, D])
    g_moe = din("g_moe", [1, D])
    w_router = din("w_router", [D, NE])
    b_router = din("b_router", [1, NE])
    NEd = NE if stop_after in (None, "C2") else 1
    w_gu = din("w_gu", [NEd, D, 2048])
    b_gu = din("b_gu", [NE * 16, 128])
    w_dn = din("w_dn", [NEd, D, D])
    b_dn = din("b_dn", [NE, D])
    g_ple = din("g_ple", [1, D])
    w_pg = din("w_pg", [D, D])
    w_pp = din("w_pp", [256, D])
    g_final = din("g_final", [1, D])
    yo = nc.dram_tensor("yo", [2048, D], F32, kind="ExternalOutput").ap()

    KnT = dscr("KnT", [512, S])
    KrT = dscr("KrT", [32, S])
    VmD = dscr("VmD", [S, 512])
    KsT = dscr("KsT", [512, S])
    VsD = dscr("VsD", [S, 512])
    QmT = dscr("QmT", [8 * 96, 2048])
    QsT = dscr("QsT", [512, 2048])
    OTd = dscr("OTd", [1024, 2048])
    CAP = 512
    NS = CAP // 128
    Ud = nc.dram_tensor("Ud", [2048, D], BF16, kind="Internal").ap()
    Yd = nc.dram_tensor("Yd", [NE * CAP, D], F32, kind="Internal").ap()
    if debug:
        dbgH = nc.dram_tensor("dbgH", [2048, D], F32, kind="ExternalOutput").ap()
        dbgG = nc.dram_tensor("dbgG", [2048, NE], F32, kind="ExternalOutput").ap()
        dbgS = nc.dram_tensor("dbgS", [2048, 4], I32, kind="ExternalOutput").ap()
        dbgJ = nc.dram_tensor("dbgJ", [2048, 4], F32, kind="ExternalOutput").ap()

    top = ExitStack()
    with top:
        p = Prog(nc)
        if True:

            def mk_alloc(stack):
                def sb(n, s, d):
                    return stack.enter_context(nc.sbuf_tensor(n, list(s), d))

                def ps(n, s, d=F32):
                    return stack.enter_context(nc.psum_tensor(n, list(s), d))
                return sb, ps

            sb0, ps0 = mk_alloc(top)

            identf = sb0("identf", [128, 128], F32)
            ident = sb0("ident", [128, 128], BF16)
            ones = sb0("ones", [128, 128], BF16)
            ntri = sb0("ntri", [128, 128], BF16)
            nones = sb0("nones", [128, 128], BF16)
            zeros = sb0("zeros", [128, 128], BF16)
            p.op("pool", lambda e: e.memset(identf[:], 0.0), writes=["identf"])
            p.op("pool", lambda e: e.affine_select(out=identf[:], in_=identf[:], pattern=[[-1, 128]],
                                                    compare_op=ALU.not_equal, fill=1.0, base=0, channel_multiplier=1),
                 reads=["identf"], writes=["identf"])
            p.op("dve", lambda e: e.tensor_copy(out=ident[:], in_=identf[:]), reads=["identf"], writes=["ident"])
            p.op("pool", lambda e: e.memset(ones[:], 1.0), writes=["ones"])
            p.op("pool", lambda e: e.memset(nones[:], -1.0), writes=["nones"])
            p.op("pool", lambda e: e.memset(zeros[:], 0.0), writes=["zeros"])
            gcol = sb0("gcol", [128, 16], F32)
            p.op("sp", lambda e: e.dma_start(out=gcol[:, 0:3], in_=g_cq.rearrange("o (c p) -> p (o c)", p=128),
                                             allow_slow_non_contiguous=True), writes=["gcol"], dma="gcol")
            p.op("sp", lambda e: e.dma_start(out=gcol[:, 3:5], in_=g_ckv.rearrange("o (c p) -> p (o c)", p=128),
                                             allow_slow_non_contiguous=True), writes=["gcol"], dma="gcol")
            p.op("sp", lambda e: e.dma_start(out=gcol[:, 5:13], in_=g_out.rearrange("o (c p) -> p (o c)", p=128),
                                             allow_slow_non_contiguous=True), writes=["gcol"], dma="gcol")
            invc = sb0("invc", [128, 1], F32)
            p.op("sp", lambda e: e.dma_start(out=invc[:], in_=invf), writes=["invc"], dma="invc")

            def rstd_from(eng_out, src, n, rd, wr):
                p.op("act", lambda e: e.activation(out=eng_out, in_=src, func=AF.Ln, scale=1.0 / n, bias=EPS),
                     reads=rd, writes=wr)
                p.op("act", lambda e: e.activation(out=eng_out, in_=eng_out, func=AF.Exp, scale=-0.5),
                     reads=wr, writes=wr)

            sa = ExitStack()
            with sa:
                sb, ps = mk_alloc(sa)
                tmpf = sb("tmpf", [128, 128], F32)
                p.op("pool", lambda e: e.memset(tmpf[:], -1.0), writes=["tmpf"])
                p.op("pool", lambda e: e.affine_select(out=tmpf[:], in_=tmpf[:], pattern=[[-1, 128]],
                                                        compare_op=ALU.is_ge, fill=0.0, base=0, channel_multiplier=1),
                     reads=["tmpf"], writes=["tmpf"])
                p.op("dve", lambda e: e.tensor_copy(out=ntri[:], in_=tmpf[:]), reads=["tmpf"], writes=["ntri"])

                Win = sb("Win", [128, 8, 2208], BF16)
                for c in range(8):
                    p.op("pool", lambda e, c=c: e.dma_start(out=Win[:, c, :], in_=w_in[c * 128:(c + 1) * 128, :]),
                         writes=["Win"], dma="Win")
                Wuq = sb("Wuq", [128, 3, 768], BF16)
                Wuqr = sb("Wuqr", [128, 3, 768], BF16)
                p.op("pool", lambda e: e.dma_start(out=Wuq[:], in_=w_uq.rearrange("(c p) n -> p c n", p=128)),
                     writes=["Wuq"], dma="Wuq")
                Wkn = sb("Wkn", [128, 2, 512], BF16)
                Wv = sb("Wv", [128, 2, 512], BF16)
                ukv = w_ukv.rearrange("(c p) (h t d) -> p c h t d", p=128, h=8, t=2)
                for c in range(2):
                    p.op("pool", lambda e, c=c: e.dma_start(out=Wkn[:, c, :].rearrange("p (h d) -> p h d", h=8),
                                                          in_=ukv[:, c, :, 0, :]), writes=["Wkn"], dma="Wkn")
                    p.op("pool", lambda e, c=c: e.dma_start(out=Wv[:, c, :].rearrange("p (h d) -> p h d", h=8),
                                                          in_=ukv[:, c, :, 1, :]), writes=["Wv"], dma="Wv")
                p.op("pool", lambda e: e.memset(Wuqr[:], 0.0), writes=["Wuqr"])
                Wq4 = Wuq[:].rearrange("p c (h d) -> p c h d", h=8)
                Wr4 = Wuqr[:].rearrange("p c (h d) -> p c h d", h=8)
                for c in range(3):
                    p.op("dve", lambda e, c=c: e.tensor_scalar(out=Wr4[:, c, :, 64:80], in0=Wq4[:, c, :, 80:96], scalar1=-1.0,
                                                          scalar2=None, op0=ALU.mult), reads=["Wuq", "Wuqr"], writes=["Wuqr"])
                    p.op("dve", lambda e, c=c: e.tensor_copy(out=Wr4[:, c, :, 80:96], in_=Wq4[:, c, :, 64:80]),
                         reads=["Wuq", "Wuqr"], writes=["Wuqr"])
                Wkr = sb("Wkr", [128, 8, 64], BF16)
                p.op("dve", lambda e: e.tensor_copy(out=Wkr[:, :, 0:32], in_=Win[:, :, 640:672]), reads=["Win"], writes=["Wkr"])
                p.op("dve", lambda e: e.tensor_scalar(out=Wkr[:, :, 32:48], in0=Win[:, :, 656:672], scalar1=-1.0, scalar2=None,
                                                      op0=ALU.mult), reads=["Win", "Wkr"], writes=["Wkr"])
                p.op("dve", lambda e: e.tensor_copy(out=Wkr[:, :, 48:64], in_=Win[:, :, 640:656]), reads=["Win", "Wkr"], writes=["Wkr"])
                gattn = sb("gattn", [128, D], F32)
                p.op("sp", lambda e: e.dma_start(out=gattn[:], in_=g_attn.partition_broadcast(128)), writes=["gattn"], dma="gattn")

                def rope_table(Ct, St, pos_ap, n, rows, name, inv, kinv, sb):
                    posi = sb(name + "_pi", [128, n], I32)
                    ang = sb(name + "_ang", [128, n], F32)
                    kk = sb(name + "_k", [128, n], F32)
                    ki = sb(name + "_ki", [128, n], I32)
                    p.op("sp", lambda e: e.dma_start(out=posi[:], in_=pos_ap.partition_broadcast(128)), writes=[name + "pi"], dma=name + "pi")
                    p.op("dve", lambda e: e.tensor_copy(out=ang[:], in_=posi[:]), reads=[name + "pi"], writes=[name + "ang"])
                    p.op("dve", lambda e: e.tensor_scalar(out=ang[:], in0=ang[:], scalar1=inv[:, 0:1], scalar2=None, op0=ALU.mult),
                         reads=[name + "ang", kinv], writes=[name + "ang"])
                    for which, T in (("s", St), ("c", Ct)):
                        off = 0.0 if which == "s" else float(np.pi / 2)
                        p.op("dve", lambda e, off=off: e.tensor_scalar(out=kk[:], in0=ang[:], scalar1=off, scalar2=float(1.0 / (2 * np.pi)),
                                                                   op0=ALU.add, op1=ALU.mult), reads=[name + "ang"], writes=[name + "kk"])
                        p.op("dve", lambda e: e.tensor_copy(out=ki[:], in_=kk[:]), reads=[name + "kk"], writes=[name + "ki"])
                        p.op("dve", lambda e: e.tensor_copy(out=kk[:], in_=ki[:]), reads=[name + "ki"], writes=[name + "kk"])
                        p.op("dve", lambda e, T=T: e.scalar_tensor_tensor(out=T, in0=kk[:rows], scalar=-6.28125, in1=ang[:rows],
                                                                       op0=ALU.mult, op1=ALU.add),
                             reads=[name + "kk", name + "ang"], writes=[name + which])
                        p.op("dve", lambda e, T=T, off=off: e.scalar_tensor_tensor(out=T, in0=kk[:rows], scalar=float(-(2 * np.pi - 6.28125)), in1=T,
                                                                                op0=ALU.mult, op1=ALU.add),
                             reads=[name + "kk", name + which], writes=[name + which])
                        if off != 0.0:
                            p.op("dve", lambda e, T=T, off=off: e.tensor_scalar(out=T, in0=T, scalar1=off, scalar2=None, op0=ALU.add),
                                 reads=[name + which], writes=[name + which])
                        p.op("dve", lambda e: e.tensor_scalar(out=kk[:rows], in0=T, scalar1=float(np.pi), scalar2=float(-2 * np.pi),
                                                              op0=ALU.is_gt, op1=ALU.mult), reads=[name + which], writes=[name + "kk"])
                        p.op("dve", lambda e, T=T: e.tensor_tensor(out=T, in0=T, in1=kk[:rows], op=ALU.add),
                             reads=[name + which, name + "kk"], writes=[name + which])
                        p.op("dve", lambda e: e.tensor_scalar(out=kk[:rows], in0=T, scalar1=float(-np.pi), scalar2=float(2 * np.pi),
                                                              op0=ALU.is_lt, op1=ALU.mult), reads=[name + which], writes=[name + "kk"])
                        p.op("dve", lambda e, T=T: e.tensor_tensor(out=T, in0=T, in1=kk[:rows], op=ALU.add),
                             reads=[name + which, name + "kk"], writes=[name + which])
                        p.op("dve", lambda e, T=T: e.tensor_scalar(out=T, in0=T, scalar1=3.14159, scalar2=-3.14159, op0=ALU.min, op1=ALU.max),
                             reads=[name + which], writes=[name + which])
                        p.op("act", lambda e, T=T: e.activation(out=T, in_=T, func=AF.Sin), reads=[name + which], writes=[name + which])

                Ck = sb("Ck", [32, S], F32)
                Sk = sb("Sk", [32, S], F32)
                sa2 = ExitStack()
                with sa2:
                    rope_table(Ck[:], Sk[:], posa, S, 32, "rk", invc, "invc", mk_alloc(sa2)[0])
                    p.barrier()
                Cq = sb("Cq", [96, 2048], F32)
                Sq = sb("Sq", [96, 2048], F32)
                sa3 = ExitStack()
                with sa3:
                    sb3_ = mk_alloc(sa3)[0]
                    invq = sb3_("invq", [128, 1], F32)
                    p.op("pool", lambda e: e.memset(invq[:], 0.0), writes=["invq"])
                    p.op("sp", lambda e: e.dma_start(out=invq[64:96, :], in_=invf[0:32, :]), reads=["invq"], writes=["invq"], dma="invq")
                    rope_table(Cq[:], Sq[:], poso, 2048, 96, "rq", invq, "invq", sb3_)
                    p.barrier()
                    if stop_after == "R":
                        p.finish_wait("sp"); p.emit(top); return nc
                msc = float((64 + 32) ** -0.5)
                p.op("dve", lambda e: e.tensor_scalar(out=Cq[:], in0=Cq[:], scalar1=msc, scalar2=None, op0=ALU.mult),
                     reads=["rqc"], writes=["rqc"])
                p.op("dve", lambda e: e.tensor_scalar(out=Sq[:], in0=Sq[:], scalar1=msc, scalar2=None, op0=ALU.mult),
                     reads=["rqs"], writes=["rqs"])

                xt_r = Ring(sb, "xt", 4, [128, D], F32)
                junk = sb("junkA", [128, D], F32)
                ss_r = Ring(sb, "ssA", 4, [128, 1], F32)
                xn_r = Ring(sb, "xn", 4, [128, D], BF16)
                xnT_r = Ring(sb, "xnT", 2, [128, 8, 512], BF16)
                pT_r = Ring(ps, "pTA", 2, [128, 8, 128], BF16)
                pm_r = Ring(ps, "pmA", 4, [128, 512], F32)
                pss = ps("pssA", [128, 512], F32)
                sq_r = Ring(sb, "sqA", 2, [128, 512], BF16)
                rbc = sb("rbcA", [128, 512], F32)
                cn_r = Ring(sb, "cnA", 2, [128, 3, 512], BF16)
                ev_r = Ring(sb, "evA", 2, [128, 512], BF16)
                stg_r = Ring(sb, "stgA", 3, [128, 4, 512], BF16)
                stg8_r = Ring(sb, "stg8A", 2, [128, 8, 512], BF16)
                evf_r = Ring(sb, "evfA", 2, [128, 512], F32)
                evf2_r = Ring(sb, "evf2A", 2, [128, 512], F32)

                def make_xnT(src, tok0, defer=False):
                    xnT, kT = xnT_r.next()
                    xs = []
                    for sub in range(4):
                        xt, kx = xt_r.next()
                        ss, ks = ss_r.next()
                        xn, kn = xn_r.next()
                        r0 = tok0 + sub * 128
                        p.op("sp", lambda e: e.dma_start(out=xt[:], in_=src[r0:r0 + 128, :]), writes=[kx], dma=kx)
                        p.op("act", lambda e: e.activation(out=junk[:], in_=xt[:], func=AF.Square, accum_out=ss[:]),
                             reads=[kx], writes=["junkA", ks])
                        rstd_from(ss[:], ss[:], D, [ks], [ks])
                        p.op("dve", lambda e: e.scalar_tensor_tensor(out=xn[:], in0=xt[:], scalar=ss[:, 0:1], in1=gattn[:],
                                                                     op0=ALU.mult, op1=ALU.mult),
                             reads=[kx, ks, "gattn"], writes=[kn])
                        xs.append((xn, kn))
                    def tr(sub):
                        xn, kn = xs[sub]
                        pT, kp = pT_r.next()
                        for c in range(8):
                            p.op("pe", lambda e: e.transpose(out=pT[:, c, :], in_=xn[:, c * 128:(c + 1) * 128], identity=ident[:]),
                                 reads=[kn, "ident"], writes=[kp])
                        p.op("dve", lambda e: e.tensor_copy(out=xnT[:, :, sub * 128:(sub + 1) * 128], in_=pT[:]),
                             reads=[kp], writes=[kT])
                    if defer:
                        return xnT, kT, tr
                    for sub in range(4):
                        tr(sub)
                    return xnT, kT

                def proj_fm(xnT, kT, col0, m, wkey="Win", W=None):
                    W = Win if W is None else W
                    pm, kpm = pm_r.next()
                    for k in range(8):
                        p.op("pe", lambda e, k=k, pm=pm, W=W: e.matmul(out=pm[0:m, :], lhsT=W[:, k, col0:col0 + m], rhs=xnT[:, k, :],
                                                                  start=(k == 0), stop=(k == 7)),
                             reads=[kT, wkey], writes=[kpm])
                    return pm, kpm

                def lowrank_norm(pms, nch, width, gc0):
                    for i, (pm, kpm) in enumerate(pms):
                        sq, ksq = sq_r.next()
                        p.op("act", lambda e, pm=pm, sq=sq: e.activation(out=sq[:], in_=pm[:], func=AF.Square), reads=[kpm], writes=[ksq])
                        p.op("pe", lambda e, sq=sq, i=i: e.matmul(out=pss[:], lhsT=ones[:], rhs=sq[:], start=(i == 0), stop=(i == nch - 1)),
                             reads=[ksq, "ones"], writes=["pssA"])
                    rstd_from(rbc[:], pss[:], width, ["pssA"], ["rbcA"])
                    cn, kcn = cn_r.next()
                    for i, (pm, kpm) in enumerate(pms):
                        p.op("dve", lambda e, pm=pm, i=i, cn=cn: e.scalar_tensor_tensor(out=cn[:, i, :], in0=pm[:], scalar=gcol[:, gc0 + i:gc0 + i + 1],
                                                                                    in1=rbc[:], op0=ALU.mult, op1=ALU.mult),
                             reads=[kpm, "gcol", "rbcA"], writes=[kcn])
                    return cn, kcn

                def store_fm(pm, kpm, rows, dst, eng="dve", scale=None):
                    ev, kev = ev_r.next()
                    if scale is None:
                        if eng == "act":
                            p.op("act", lambda e: e.copy(out=ev[0:rows, :], in_=pm[0:rows, :]), reads=[kpm], writes=[kev])
                        else:
                            p.op("dve", lambda e: e.tensor_copy(out=ev[0:rows, :], in_=pm[0:rows, :]), reads=[kpm], writes=[kev])
                    else:
                        p.op("act", lambda e: e.mul(out=ev[0:rows, :], in_=pm[0:rows, :], mul=scale), reads=[kpm], writes=[kev])
                    p.op("pool", lambda e: e.dma_start(out=dst, in_=ev[0:rows, :]), reads=[kev], writes=[], dma=kev + "s")

                def evac_to(pm, kpm, dst, kdst, eng="dve", scale=None):
                    if scale is not None:
                        p.op("act", lambda e: e.mul(out=dst, in_=pm[:], mul=scale), reads=[kpm], writes=[kdst])
                    elif eng == "act":
                        p.op("act", lambda e: e.copy(out=dst, in_=pm[:]), reads=[kpm], writes=[kdst])
                    else:
                        p.op("dve", lambda e: e.tensor_copy(out=dst, in_=pm[:]), reads=[kpm], writes=[kdst])

                KnT_v = KnT.rearrange("(c p) n -> p c n", p=128)
                KsT_v = KsT.rearrange("(c p) n -> p c n", p=128)
                QsT_v = QsT.rearrange("(c p) n -> p c n", p=128)
                VmD_v = VmD.rearrange("(s p) n -> p s n", p=128)
                VsD_v = VsD.rearrange("(s p) n -> p s n", p=128)
                QmT_v = QmT.rearrange("(h r) n -> r h n", r=96)

                srcs = [(xa, g_ * 512) for g_ in range(8)] + [(xo, g_ * 512) for g_ in range(4)]
                nxt = make_xnT(*srcs[0])
                for G in range(8):
                    c0 = G * 512
                    xnT, kT = nxt
                    nx_xnT, nx_kT, nx_tr = make_xnT(*srcs[G + 1], defer=True)
                    nxt = (nx_xnT, nx_kT)
                    ckv = [proj_fm(xnT, kT, 384 + m * 128, 128) for m in range(2)]
                    nx_tr(0)
                    cn, kcn = lowrank_norm(ckv, 2, 256, 3)
                    pa, kpa = proj_fm(xnT, kT, 0, 32, "Wkr", Wkr)
                    pb, kpb = proj_fm(xnT, kT, 32, 32, "Wkr", Wkr)
                    t1, kt1 = evf_r.next()
                    t2, kt2 = evf2_r.next()
                    p.op("dve", lambda e, pa=pa, t1=t1, c0=c0: e.tensor_tensor(out=t1[0:32, :], in0=pa[0:32, :], in1=Ck[:, c0:c0 + 512], op=ALU.mult),
                         reads=[kpa, "rkc"], writes=[kt1])
                    p.op("dve", lambda e, pb=pb, t2=t2, c0=c0: e.tensor_tensor(out=t2[0:32, :], in0=pb[0:32, :], in1=Sk[:, c0:c0 + 512], op=ALU.mult),
                         reads=[kpb, "rks"], writes=[kt2])
                    ev, kev = ev_r.next()
                    p.op("dve", lambda e, t1=t1, t2=t2, ev=ev: e.tensor_tensor(out=ev[0:32, :], in0=t1[0:32, :], in1=t2[0:32, :], op=ALU.add),
                         reads=[kt1, kt2], writes=[kev])
                    p.op("sp", lambda e, ev=ev, c0=c0: e.dma_start(out=KrT[:, c0:c0 + 512], in_=ev[0:32, :]), reads=[kev], writes=[], dma=kev + "s")
                    nx_tr(1)
                    st_, kst_ = stg_r.next()
                    for m in range(4):
                        pm, kpm = proj_fm(xnT, kT, 1184 + m * 128, 128)
                        evac_to(pm, kpm, st_[:, m, :], kst_, eng="act")
                    p.op("sp", lambda e: e.dma_start(out=KsT_v[:, :, c0:c0 + 512], in_=st_[:]), reads=[kst_], dma=kst_ + "s")
                    nx_tr(2)
                    st_, kst_ = stg_r.next()
                    for sub in range(4):
                        pm, kpm = pm_r.next()
                        for k in range(8):
                            p.op("pe", lambda e, k=k, pm=pm, sub=sub, xnT=xnT: e.matmul(out=pm[:], lhsT=xnT[:, k, sub * 128:(sub + 1) * 128],
                                                                                   rhs=Win[:, k, 1696:2208], start=(k == 0), stop=(k == 7)),
                                 reads=[kT, "Win"], writes=[kpm])
                        evac_to(pm, kpm, st_[:, sub, :], kst_, eng="dve")
                    p.op("sp", lambda e: e.dma_start(out=VsD_v[:, G * 4:(G + 1) * 4, :], in_=st_[:]), reads=[kst_], dma=kst_ + "s")

                    nx_tr(3)
                    st_, kst_ = stg_r.next()
                    for hp in range(4):
                        pm, kpm = pm_r.next()
                        for m in range(2):
                            p.op("pe", lambda e, m=m, pm=pm, hp=hp, cn=cn: e.matmul(out=pm[:], lhsT=Wkn[:, m, hp * 128:(hp + 1) * 128], rhs=cn[:, m, :],
                                                                               start=(m == 0), stop=(m == 1)),
                                 reads=[kcn, "Wkn"], writes=[kpm])
                        evac_to(pm, kpm, st_[:, hp, :], kst_, eng="act")
                    p.op("sp", lambda e: e.dma_start(out=KnT_v[:, :, c0:c0 + 512], in_=st_[:]), reads=[kst_], dma=kst_ + "s")
                    st_, kst_ = stg_r.next()
                    for sub in range(4):
                        pm, kpm = pm_r.next()
                        for m in range(2):
                            p.op("pe", lambda e, m=m, pm=pm, sub=sub, cn=cn: e.matmul(out=pm[:], lhsT=cn[:, m, sub * 128:(sub + 1) * 128], rhs=Wv[:, m, :],
                                                                                 start=(m == 0), stop=(m == 1)),
                                 reads=[kcn, "Wv"], writes=[kpm])
                        evac_to(pm, kpm, st_[:, sub, :], kst_, eng="dve")
                    p.op("sp", lambda e: e.dma_start(out=VmD_v[:, G * 4:(G + 1) * 4, :], in_=st_[:]), reads=[kst_], dma=kst_ + "s")
                for G in range(4):
                    c0 = G * 512
                    xnT, kT = nxt
                    if G + 1 < 4:
                        nxt = make_xnT(*srcs[8 + G + 1])
                    cq = [proj_fm(xnT, kT, m * 128, 128) for m in range(3)]
                    cn, kcn = lowrank_norm(cq, 3, 384, 0)
                    st8, kst8 = stg8_r.next()
                    for h in range(8):
                        pa, kpa = pm_r.next()
                        pb, kpb = pm_r.next()
                        for m in range(3):
                            p.op("pe", lambda e, m=m, pa=pa, h=h, cn=cn: e.matmul(out=pa[0:96, :], lhsT=Wuq[:, m, h * 96:(h + 1) * 96], rhs=cn[:, m, :],
                                                                             start=(m == 0), stop=(m == 2)),
                                 reads=[kcn, "Wuq"], writes=[kpa])
                        for m in range(3):
                            p.op("pe", lambda e, m=m, pb=pb, h=h, cn=cn: e.matmul(out=pb[0:96, :], lhsT=Wuqr[:, m, h * 96:(h + 1) * 96], rhs=cn[:, m, :],
                                                                             start=(m == 0), stop=(m == 2)),
                                 reads=[kcn, "Wuqr"], writes=[kpb])
                        t1, kt1 = evf_r.next()
                        t2, kt2 = evf2_r.next()
                        p.op("dve", lambda e, pa=pa, t1=t1, c0=c0: e.tensor_tensor(out=t1[0:96, :], in0=pa[0:96, :], in1=Cq[:, c0:c0 + 512], op=ALU.mult),
                             reads=[kpa, "rqc"], writes=[kt1])
                        p.op("dve", lambda e, pb=pb, t2=t2, c0=c0: e.tensor_tensor(out=t2[0:96, :], in0=pb[0:96, :], in1=Sq[:, c0:c0 + 512], op=ALU.mult),
                             reads=[kpb, "rqs"], writes=[kt2])
                        p.op("pool", lambda e, t1=t1, t2=t2: e.tensor_tensor(out=st8[0:96, h, :], in0=t1[0:96, :], in1=t2[0:96, :], op=ALU.add),
                             reads=[kt1, kt2], writes=[kst8])
                    p.op("sp", lambda e: e.dma_start(out=QmT_v[:, :, c0:c0 + 512], in_=st8[0:96, :, :]), reads=[kst8], dma=kst8 + "s")
                    st_, kst_ = stg_r.next()
                    for m in range(4):
                        pm, kpm = proj_fm(xnT, kT, 672 + m * 128, 128)
                        evac_to(pm, kpm, st_[:, m, :], kst_, scale=0.125)
                    p.op("sp", lambda e: e.dma_start(out=QsT_v[:, :, c0:c0 + 512], in_=st_[:]), reads=[kst_], dma=kst_ + "s")
                p.barrier()
                if stop_after == "A":
                    p.finish_wait("sp"); p.emit(top); return nc

            sbx = ExitStack()
            with sbx:
                sb, ps = mk_alloc(sbx)
                qrb = sb("qrb", [128, 512], F32)
                p.op("sp", lambda e: e.dma_start(out=qrb[:], in_=qrel.partition_broadcast(128)), writes=["qrb"], dma="qrb")
                kidx_i = sb("kidx_i", [128, 1], I32)
                krel = sb("krel", [128, 8], F32)
                p.op("pool", lambda e: e.iota(kidx_i[:], pattern=[[0, 1]], base=0, channel_multiplier=1), writes=["kidx_i"])
                p.op("dve", lambda e: e.tensor_copy(out=krel[:, 0:1], in_=kidx_i[:]), reads=["kidx_i"], writes=["krel"])
                for d in range(1, 8):
                    p.op("dve", lambda e, d=d: e.tensor_scalar(out=krel[:, d:d + 1], in0=krel[:, 0:1], scalar1=float(128 * d), scalar2=None, op0=ALU.add),
                         reads=["krel"], writes=["krel"])
                nmM = sb("nmM", [128, 8, 512], BF16)
                nmS = sb("nmS", [128, 8, 512], BF16)
                m01 = sb("m01", [128, 8, 512], BF16)
                for d in range(8):
                    p.op("dve", lambda e, d=d: e.tensor_scalar(out=nmM[:, d, :], in0=qrb[:], scalar1=krel[:, d:d + 1], scalar2=NEG, op0=ALU.is_lt, op1=ALU.mult),
                         reads=["qrb", "krel"], writes=["nmM"])
                    p.op("dve", lambda e, d=d: e.tensor_scalar(out=nmS[:, d, :], in0=qrb[:], scalar1=krel[:, d:d + 1], scalar2=NEG, op0=ALU.is_le, op1=ALU.mult),
                         reads=["qrb", "krel"], writes=["nmS"])
                    p.op("dve", lambda e, d=d: e.tensor_scalar(out=m01[:, d, :], in0=qrb[:], scalar1=krel[:, d:d + 1], scalar2=None, op0=ALU.is_gt),
                         reads=["qrb", "krel"], writes=["m01"])

                def blocks(t):
                    out = [(kb, 512, None) for kb in range(8 * t)]
                    out += [(8 * t + d, NDIAG[d], d) for d in range(8)]
                    return out

                sm = ExitStack()
                with sm:
                    def mla_gen():
                        sb, ps = mk_alloc(sm)
                        Vall = sb("Vall", [128, 32, 8, 65], BF16)
                        p.op("pool", lambda e: e.memset(Vall[:], 1.0), writes=["Vall"])
                        vsrc = VmD.rearrange("(kb p) (h d) -> p kb h d", p=128, h=8)
                        for hh in range(8):
                            p.op("sp", lambda e: e.dma_start(out=Vall[:, :, hh, 0:64], in_=vsrc[:, :, hh, :]),
                                 reads=["Vall"], writes=["Vall"], dma="Vall")
                        KT_r = Ring(sb, "KTm", 2, [96, S], BF16)
                        QT_r = Ring(sb, "QTm", 2, [96, 2048], BF16)
                        pS_r = Ring(ps, "pSm", 2, [128, 512], F32)
                        pO_r = Ring(ps, "pOm", 1, [128, 512], F32)
                        pB_r = Ring(ps, "pBm", 1, [128, 512], F32)
                        P_r = Ring(sb, "Pm", 3, [128, 512], BF16)
                        Of_r = Ring(sb, "Ofm", 2, [65, 512], F32)
                        On_r = Ring(sb, "Onm", 2, [64, 512], BF16)
                        sel = sb("sel65", [65, 64], F32)
                        p.op("pool", lambda e: e.memset(sel[:], 0.0), writes=["sel65"])
                        p.op("pool", lambda e: e.memset(sel[64:65, :], 1.0), reads=["sel65"], writes=["sel65"])
                        for h in range(8):
                            KT, kKT = KT_r.next()
                            QT, kQT = QT_r.next()
                            p.op("sp", lambda e, KT=KT, h=h: e.dma_start(out=KT[0:64, :], in_=KnT[h * 64:(h + 1) * 64, :]), writes=[kKT], dma=kKT)
                            p.op("sp", lambda e, KT=KT: e.dma_start(out=KT[64:96, :], in_=KrT[:, :]), writes=[kKT], dma=kKT)
                            p.op("sp", lambda e, QT=QT, h=h: e.dma_start(out=QT[:], in_=QmT[h * 96:(h + 1) * 96, :]), writes=[kQT], dma=kQT)
                            for t in range(4):
                                bl = blocks(t)
                                pO, kpO = pO_r.next()
                                nb = len(bl)
                                stage = {}

                                def stA(i):
                                    kb, N, d = bl[i]
                                    pS, kpS = pS_r.next()
                                    p.op("pe", lambda e: e.matmul(out=pS[:, 0:N], lhsT=KT[:, kb * 128:(kb + 1) * 128], rhs=QT[:, t * 512:t * 512 + N],
                                                                  start=True, stop=(d is None)), reads=[kKT, kQT], writes=[kpS])
                                    if d is not None:
                                        p.op("pe", lambda e: e.matmul(out=pS[:, 0:N], lhsT=ident[:], rhs=nmM[:, d, 0:N], start=False, stop=True),
                                             reads=["ident", "nmM"], writes=[kpS])
                                    P, kP = P_r.next()
                                    p.op("act", lambda e: e.activation(out=P[:, 0:N], in_=pS[:, 0:N], func=AF.Exp), reads=[kpS], writes=[kP])
                                    stage[i] = (P, kP)

                                def stB(i):
                                    kb, N, d = bl[i]
                                    P, kP = stage.pop(i)
                                    p.op("pe", lambda e: e.matmul(out=pO[0:65, 0:N], lhsT=Vall[:, kb, h, :], rhs=P[:, 0:N], start=(i == 0), stop=(i == nb - 1)),
                                         reads=["Vall", kP], writes=[kpO])

                                for it in range(nb + 2):
                                    if it < nb:
                                        stA(it)
                                    if it - 2 >= 0:
                                        stB(it - 2)
                                    yield
                                Of, kOf = Of_r.next()
                                p.op("dve", lambda e, Of=Of, pO=pO: e.tensor_copy(out=Of[:], in_=pO[0:65, :]), reads=[kpO], writes=[kOf])
                                p.op("dve", lambda e, Of=Of: e.reciprocal(out=Of[64:65, :], in_=Of[64:65, :]), reads=[kOf], writes=[kOf])
                                pB, kpB = pB_r.next()
                                p.op("pe", lambda e, Of=Of, pB=pB: e.matmul(out=pB[0:64, :], lhsT=sel[:], rhs=Of[:], start=True, stop=True),
                                     reads=[kOf, "sel65"], writes=[kpB])
                                On, kOn = On_r.next()
                                p.op("dve", lambda e, Of=Of, pB=pB, On=On: e.tensor_tensor(out=On[:], in0=Of[0:64, :], in1=pB[0:64, :], op=ALU.mult),
                                     reads=[kOf, kpB], writes=[kOn])
                                p.op("pool", lambda e, On=On, h=h, t=t: e.dma_start(out=OTd[h * 64:(h + 1) * 64, t * 512:(t + 1) * 512], in_=On[:]),
                                     reads=[kOn], writes=[], dma=kOn + "s")

                    def sb_gen():
                        sb, ps = mk_alloc(sm)
                        Vs = sb("Vsall", [128, 32, 512], BF16)
                        vsrc = VsD.rearrange("(kb p) n -> p kb n", p=128)
                        for q4 in range(4):
                            p.op("sp", lambda e, q4=q4: e.dma_start(out=Vs[:, q4 * 8:(q4 + 1) * 8, :], in_=vsrc[:, q4 * 8:(q4 + 1) * 8, :]),
                                 writes=["Vsall"], dma="Vsall")
                        KT_r = Ring(sb, "KTs", 2, [64, S], BF16)
                        QT_r = Ring(sb, "QTs", 2, [64, 2048], BF16)
                        pZ_r = Ring(ps, "pZs", 2, [128, 512], F32)
                        pL_r = Ring(ps, "pLs", 1, [128, 512], F32)
                        pO_r = Ring(ps, "pOs", 1, [128, 512], F32)
                        E_r = Ring(sb, "Es", 3, [128, 512], F32)
                        SP_r = Ring(sb, "SPs", 4, [128, 512], BF16)
                        SM_r = Ring(sb, "SMs", 4, [128, 512], BF16)
                        A_r = Ring(sb, "As", 3, [128, 512], BF16)
                        CR_r = Ring(sb, "CRs", 3, [128, 512], BF16)
                        On_r = Ring(sb, "Ons", 2, [64, 512], BF16)
                        for h in range(8):
                            KT, kKT = KT_r.next()
                            QT, kQT = QT_r.next()
                            p.op("sp", lambda e, KT=KT, h=h: e.dma_start(out=KT[:], in_=KsT[h * 64:(h + 1) * 64, :]), writes=[kKT], dma=kKT)
                            p.op("sp", lambda e, QT=QT, h=h: e.dma_start(out=QT[:], in_=QsT[h * 64:(h + 1) * 64, :]), writes=[kQT], dma=kQT)
                            for t in range(4):
                                bl = blocks(t)[::-1]
                                nb = len(bl)
                                pO, kpO = pO_r.next()
                                p.op("pe", lambda e, pO=pO: e.matmul(out=pO[0:64, :], lhsT=zeros[:, 0:64], rhs=m01[:, 0, :],
                                                                      start=True, stop=False), reads=["zeros", "m01"], writes=[kpO])
                                stage = {}
                                carry = {"t": None, "k": None}

                                def stA(i):
                                    kb, N, d = bl[i]
                                    pZ, kpZ = pZ_r.next()
                                    p.op("pe", lambda e: e.matmul(out=pZ[:, 0:N], lhsT=KT[:, kb * 128:(kb + 1) * 128], rhs=QT[:, t * 512:t * 512 + N],
                                                                  start=True, stop=True), reads=[kKT, kQT], writes=[kpZ])
                                    E, kE = E_r.next()
                                    p.op("act", lambda e: e.activation(out=E[:, 0:N], in_=pZ[:, 0:N], func=AF.Exp), reads=[kpZ], writes=[kE])
                                    SPt, kSP = SP_r.next()
                                    p.op("act", lambda e: e.activation(out=SPt[:, 0:N], in_=E[:, 0:N], func=AF.Ln, bias=1.0), reads=[kE], writes=[kSP])
                                    if d is not None:
                                        SM, kSM = SM_r.next()
                                        p.op("dve", lambda e: e.tensor_tensor(out=SM[:, 0:N], in0=SPt[:, 0:N], in1=m01[:, d, 0:N], op=ALU.mult),
                                             reads=[kSP, "m01"], writes=[kSM])
                                    else:
                                        SM, kSM = SPt, kSP
                                    cprev, kcprev = carry["t"], carry["k"]
                                    stage[i] = (SM, kSM, cprev, kcprev, E, kE)
                                    if i < nb - 1:
                                        cn_, kcn_ = CR_r.next()
                                        if cprev is None:
                                            if N < 512:
                                                p.op("pool", lambda e: e.memset(cn_[:, N:512], 0.0), writes=[kcn_])
                                            p.op("pool", lambda e: e.tensor_copy(out=cn_[:, 0:N], in_=SM[:, 0:N]), reads=[kSM], writes=[kcn_])
                                        else:
                                            if N < 512:
                                                p.op("pool", lambda e: e.tensor_copy(out=cn_[:, N:512], in_=cprev[:, N:512]), reads=[kcprev], writes=[kcn_])
                                            p.op("dve", lambda e: e.tensor_tensor(out=cn_[:, 0:N], in0=cprev[:, 0:N], in1=SM[:, 0:N], op=ALU.add),
                                                 reads=[kcprev, kSM], writes=[kcn_])
                                        carry["t"], carry["k"] = cn_, kcn_

                                def stB(i):
                                    kb, N, d = bl[i]
                                    SM, kSM, cprev, kcprev, E, kE = stage[i]
                                    pL, kpL = pL_r.next()
                                    last = "tri"
                                    if cprev is not None:
                                        last = "carry"
                                    if d is not None:
                                        last = "mask"
                                    p.op("pe", lambda e: e.matmul(out=pL[:, 0:N], lhsT=ntri[:], rhs=SM[:, 0:N], start=True, stop=(last == "tri")),
                                         reads=["ntri", kSM], writes=[kpL])
                                    if cprev is not None:
                                        p.op("pe", lambda e: e.matmul(out=pL[:, 0:N], lhsT=nones[:], rhs=cprev[:, 0:N], start=False, stop=(last == "carry")),
                                             reads=["nones", kcprev], writes=[kpL])
                                    if d is not None:
                                        p.op("pe", lambda e: e.matmul(out=pL[:, 0:N], lhsT=ident[:], rhs=nmS[:, d, 0:N], start=False, stop=True),
                                             reads=["ident", "nmS"], writes=[kpL])
                                    A, kA = A_r.next()
                                    p.op("act", lambda e: e.activation(out=A[:, 0:N], in_=pL[:, 0:N], func=AF.Exp), reads=[kpL], writes=[kA])
                                    p.op("dve", lambda e: e.tensor_tensor(out=A[:, 0:N], in0=A[:, 0:N], in1=E[:, 0:N], op=ALU.mult), reads=[kA, kE], writes=[kA])
                                    stage[i] = (A, kA)

                                def stC(i):
                                    kb, N, d = bl[i]
                                    A, kA = stage.pop(i)
                                    p.op("pe", lambda e: e.matmul(out=pO[0:64, 0:N], lhsT=Vs[:, kb, h * 64:(h + 1) * 64], rhs=A[:, 0:N], start=False, stop=(i == nb - 1)),
                                         reads=["Vsall", kA], writes=[kpO])

                                for it in range(nb + 2):
                                    if it < nb:
                                        stA(it)
                                    if 0 <= it - 1 < nb:
                                        stB(it - 1)
                                    if it - 2 >= 0:
                                        stC(it - 2)
                                    yield
                                On, kOn = On_r.next()
                                p.op("dve", lambda e, On=On, pO=pO: e.tensor_copy(out=On[:], in_=pO[0:64, :]), reads=[kpO], writes=[kOn])
                                p.op("pool", lambda e, On=On, h=h, t=t: e.dma_start(out=OTd[512 + h * 64:512 + (h + 1) * 64, t * 512:(t + 1) * 512], in_=On[:]),
                                     reads=[kOn], writes=[], dma=kOn + "s")

                    gens = [mla_gen(), sb_gen()]
                    while gens:
                        for g_ in list(gens):
                            try:
                                next(g_)
                            except StopIteration:
                                gens.remove(g_)
                    p.barrier()
                    if stop_after == "B":
                        p.finish_wait("sp"); p.emit(top); return nc
            sc = ExitStack()
            with sc:
                sb, ps = mk_alloc(sc)
                H = sb("H", [128, 16, D], F32)
                posm = sb("posm", [128, 16, NE], F32)
                maskb = sb("maskb", [128, 16, NE], BF16)
                gj = sb("gj", [128, 16, 4], F32)
                slI = sb("slI", [128, 16, 4], I32)
                Gt = sb("Gt", [128, 16, NE], F32)
                bgT = sb("bgT", [128, 512], F32)
                s1 = ExitStack()
                with s1:
                    sb1, ps1 = mk_alloc(s1)
                    gbc = sb1("gbc", [128, D], F32)
                    junk = sb1("junkC", [128, D], BF16)
                    Wo = sb1("Wo", [128, 8, D], BF16)
                    p.op("pool", lambda e: e.dma_start(out=Wo[:], in_=w_o.rearrange("(c p) n -> p c n", p=128)), writes=["Wo"], dma="Wo")
                    Wr = sb1("Wr", [128, 8, NE], F32)
                    p.op("sp", lambda e: e.dma_start(out=Wr[:], in_=w_router.rearrange("(c p) n -> p c n", p=128)), writes=["Wr"], dma="Wr")
                    brb = sb1("brb", [128, NE], F32)
                    p.op("sp", lambda e: e.dma_start(out=brb[:], in_=b_router.partition_broadcast(128)), writes=["brb"], dma="brb")
                    p.op("sp", lambda e: e.dma_start(out=gbc[:], in_=g_moe.partition_broadcast(128)), writes=["gbc"], dma="gbc")
                    bgl = sb1("bgl", [128, 4, 128], F32)
                    p.op("sp", lambda e: e.dma_start(out=bgl[:], in_=b_gu.rearrange("(a r) q -> r a q", r=128)), writes=["bgl"], dma="bgl")
                    pTf_r = Ring(ps1, "pTf", 1, [128, 4, 128], F32)
                    pbt, kpbt = pTf_r.next()
                    for a in range(4):
                        p.op("pe", lambda e, a=a: e.transpose(out=pbt[:, a, :], in_=bgl[:, a, :], identity=identf[:]), reads=["bgl", "identf"], writes=[kpbt])
                    p.op("dve", lambda e: e.tensor_copy(out=bgT[:].rearrange("p (a q) -> p a q", a=4), in_=pbt[:]), reads=[kpbt], writes=["bgT"])

                    OT_r = Ring(sb1, "OTl", 1, [128, 8, 512], BF16)
                    sq_r = Ring(sb1, "sqC", 2, [128, 512], BF16)
                    pss_r = Ring(ps1, "pssC", 1, [128, 512], F32)
                    rb_r = Ring(sb1, "rbC", 2, [128, 512], F32)
                    MX = sb1("MX", [128, 8, 512], BF16)
                    pA_r = Ring(ps1, "pAC", 2, [128, 512], F32)
                    xr_r = Ring(sb1, "xrC", 2, [128, D], F32)
                    ssc_r = Ring(sb1, "sscC", 2, [128, 1], F32)
                    u32_r = Ring(sb1, "u32C", 2, [128, D], F32)
                    uhT_r = Ring(sb1, "uhT", 2, [128, 8, 128], BF16)
                    tris = sb1("tris", [128, 128], BF16)
                    tmpf1 = sb1("tmpf1", [128, 128], F32)
                    p.op("pool", lambda e: e.memset(tmpf1[:], 1.0), writes=["tmpf1"])
                    p.op("pool", lambda e: e.affine_select(out=tmpf1[:], in_=tmpf1[:], pattern=[[1, 128]], compare_op=ALU.is_gt, fill=0.0,
                                                            base=0, channel_multiplier=-1), reads=["tmpf1"], writes=["tmpf1"])
                    p.op("dve", lambda e: e.tensor_copy(out=tris[:], in_=tmpf1[:]), reads=["tmpf1"], writes=["tris"])
                    gtmp_r = Ring(sb1, "gtmpC", 2, [128, NE], F32)
                    xnb_r = Ring(sb1, "xnbC", 2, [128, D], BF16)
                    ecap_i = sb1("ecap_i", [128, NE], I32)
                    ecap = sb1("ecap", [128, NE], F32)
                    p.op("pool", lambda e: e.iota(ecap_i[:], pattern=[[CAP, NE]], base=0, channel_multiplier=0), writes=["ecap_i"])
                    p.op("dve", lambda e: e.tensor_copy(out=ecap[:], in_=ecap_i[:]), reads=["ecap_i"], writes=["ecap"])
                    pq_r = Ring(sb1, "pq", 2, [128, NE], F32)
                    oh4_r = Ring(sb1, "oh4", 2, [128, 4, NE], F32)
                    pr4_r = Ring(sb1, "pr4", 2, [128, 4, NE], F32)
                    oh = sb1("oh", [128, NE], F32)
                    pr = sb1("pr", [128, NE], F32)
                    slf_r = Ring(sb1, "slf", 2, [128, 4], F32)
                    ulo_r = Ring(sb1, "uloC", 2, [128, D], BF16)
                    uloT_r = Ring(sb1, "uloT", 2, [128, 8, 128], BF16)
                    pTb_r = Ring(ps1, "pTb", 2, [128, 8, 128], BF16)
                    Wrh = sb1("Wrh", [128, 8, NE], BF16)
                    Wrl = sb1("Wrl", [128, 8, NE], BF16)
                    p.op("dve", lambda e: e.tensor_copy(out=Wrh[:], in_=Wr[:]), reads=["Wr"], writes=["Wrh"])
                    p.op("dve", lambda e: e.tensor_tensor(out=Wrl[:], in0=Wr[:], in1=Wrh[:], op=ALU.subtract), reads=["Wr", "Wrh"], writes=["Wrl"])
                    pLg_r = Ring(ps1, "pLg", 2, [128, 2, NE], F32)
                    lg_r = Ring(sb1, "lgC", 2, [128, NE], F32)
                    mx8_r = Ring(sb1, "mx8C", 2, [128, 8], F32)
                    msk_r = Ring(sb1, "mskC", 2, [128, NE], F32)
                    ex_r = Ring(sb1, "exC", 2, [128, NE], F32)
                    sm_r = Ring(sb1, "smC", 2, [128, 1], F32)
                    otv = OTd.rearrange("(c p) n -> p c n", p=128)
                    if stop_after == "C1a":
                        p.barrier()
                        p.op("sp", lambda e: e.dma_start(out=dbgH.rearrange("(t p) n -> p t n", p=128), in_=H[:]), reads=["H%d" % i_ for i_ in range(16)], dma="dbgH")
                        p.op("sp", lambda e: e.dma_start(out=dbgG.rearrange("(t p) n -> p t n", p=128), in_=Gt[:]), reads=["Gt%d" % i_ for i_ in range(16)], dma="dbgG")
                        p.finish_wait("sp"); p.emit(top); return nc
                    for G in range(4):
                        OT, kOT = OT_r.next()
                        p.op("sp", lambda e, OT=OT, G=G: e.dma_start(out=OT[:], in_=otv[:, :, G * 512:(G + 1) * 512]), writes=[kOT], dma=kOT)
                        rbs = []
                        for grp in range(2):
                            pss, kpss = pss_r.next()
                            for i in range(4):
                                c = grp * 4 + i
                                sq, ksq = sq_r.next()
                                p.op("act", lambda e, sq=sq, OT=OT, c=c: e.activation(out=sq[:], in_=OT[:, c, :], func=AF.Square), reads=[kOT], writes=[ksq])
                                p.op("pe", lambda e, sq=sq, pss=pss, i=i: e.matmul(out=pss[:], lhsT=ones[:], rhs=sq[:], start=(i == 0), stop=(i == 3)),
                                     reads=[ksq, "ones"], writes=[kpss])
                            rb, krb = rb_r.next()
                            rstd_from(rb[:], pss[:], 512, [kpss], [krb])
                            rbs.append((rb, krb))
                        for c in range(8):
                            rb, krb = rbs[c // 4]
                            p.op("dve", lambda e, c=c, rb=rb, OT=OT: e.scalar_tensor_tensor(out=MX[:, c, :], in0=OT[:, c, :], scalar=gcol[:, 5 + c:6 + c], in1=rb[:],
                                                                                        op0=ALU.mult, op1=ALU.mult),
                                 reads=[kOT, "gcol", krb], writes=["MX"])
                        def c1_tile(G, sub):
                            tile = G * 4 + sub
                            b_ = sub % 2
                            uhT, kuhT = uhT_r.tiles[b_], uhT_r.keys[b_]
                            uloT, kuloT = uloT_r.tiles[b_], uloT_r.keys[b_]
                            pq, kpq = pq_r.tiles[b_], pq_r.keys[b_]
                            slf, kslf = slf_r.tiles[b_], slf_r.keys[b_]
                            pLg2, kpLg = pLg_r.tiles[b_], pLg_r.keys[b_]
                            pLg = pLg2[:, 0, :]
                            pPos = pLg2[:, 1, :]
                            yield
                            xr, kxr = xr_r.next()
                            yield
                            p.op("sp", lambda e, xr=xr, tile=tile: e.dma_start(out=xr[:], in_=xo[tile * 128:(tile + 1) * 128, :]), writes=[kxr], dma=kxr)
                            yield
                            for half in range(2):
                                pA, kpA = pA_r.next()
                                for c in range(8):
                                    p.op("pe", lambda e, c=c, pA=pA, sub=sub, half=half: e.matmul(out=pA[:], lhsT=MX[:, c, sub * 128:(sub + 1) * 128],
                                                                                              rhs=Wo[:, c, half * 512:(half + 1) * 512], start=(c == 0), stop=(c == 7)),
                                         reads=["MX", "Wo"], writes=[kpA])
                                p.op("dve", lambda e, pA=pA, xr=xr, tile=tile, half=half: e.tensor_tensor(out=H[:, tile, half * 512:(half + 1) * 512], in0=pA[:],
                                                                                                      in1=xr[:, half * 512:(half + 1) * 512], op=ALU.add),
                                     reads=[kpA, kxr], writes=["H%d" % tile])
                            yield
                            yield
                            ssc, kss = ssc_r.next()
                            yield
                            p.op("act", lambda e, ssc=ssc, tile=tile: e.activation(out=junk[:], in_=H[:, tile, :], func=AF.Square, accum_out=ssc[:]),
                                 reads=["H%d" % tile], writes=["junkC", kss])
                            yield
                            rstd_from(ssc[:], ssc[:], D, [kss], [kss])
                            yield
                            u32, ku = u32_r.next()
                            yield
                            p.op("dve", lambda e, ssc=ssc, u32=u32, tile=tile: e.scalar_tensor_tensor(out=u32[:], in0=H[:, tile, :], scalar=ssc[:, 0:1], in1=gbc[:],
                                                                                                  op0=ALU.mult, op1=ALU.mult),
                                 reads=["H%d" % tile, kss, "gbc"], writes=[ku])
                            yield
                            xnb, kuhi = xnb_r.next()
                            yield
                            ulo, kulo = ulo_r.next()
                            yield
                            p.op("act", lambda e: e.copy(out=xnb[:], in_=u32[:]), reads=[ku], writes=[kuhi])
                            yield
                            p.op("pool", lambda e: e.dma_start(out=Ud[tile * 128:(tile + 1) * 128, :], in_=xnb[:]), reads=[kuhi], dma=kuhi + "s")
                            yield
                            p.op("dve", lambda e: e.tensor_tensor(out=ulo[:], in0=u32[:], in1=xnb[:], op=ALU.subtract), reads=[ku, kuhi], writes=[kulo])
                            yield
                            pT, kpT = pTb_r.next()
                            yield
                            for c in range(8):
                                p.op("pe", lambda e: e.transpose(out=pT[:, c, :], in_=xnb[:, c * 128:(c + 1) * 128], identity=ident[:]), reads=[kuhi, "ident"], writes=[kpT])
                            yield
                            p.op("dve", lambda e: e.tensor_copy(out=uhT[:], in_=pT[:]), reads=[kpT], writes=[kuhT])
                            yield
                            pT2, kpT2 = pTb_r.next()
                            yield
                            for c in range(8):
                                p.op("pe", lambda e: e.transpose(out=pT2[:, c, :], in_=ulo[:, c * 128:(c + 1) * 128], identity=ident[:]), reads=[kulo, "ident"], writes=[kpT2])
                            yield
                            p.op("act", lambda e: e.copy(out=uloT[:], in_=pT2[:]), reads=[kpT2], writes=[kuloT])
                            yield
                            n_ = 0
                            yield
                            for (A_, kA_, W_, kW_) in (("hi", kuhT, Wrh, "Wrh"), ("lo", kuloT, Wrh, "Wrh"), ("hi", kuhT, Wrl, "Wrl")):
                                for k in range(8):
                                    lh = uhT[:, k, :] if A_ == "hi" else uloT[:, k, :]
                                    p.op("pe", lambda e: e.matmul(out=pLg, lhsT=lh, rhs=W_[:, k, :], start=(n_ == 0), stop=(n_ == 23)),
                                         reads=[kA_, kW_], writes=[kpLg])
                                    n_ += 1
                            yield
                            lg, klg = lg_r.next()
                            yield
                            p.op("dve", lambda e, lg=lg: e.tensor_tensor(out=lg[:], in0=pLg, in1=brb[:], op=ALU.add), reads=[kpLg, "brb"], writes=[klg])
                            yield
                            mx8, kmx = mx8_r.next()
                            yield
                            p.op("dve", lambda e, lg=lg, mx8=mx8: e.max(out=mx8[:], in_=lg[:]), reads=[klg], writes=[kmx])
                            yield
                            msk, kmsk = msk_r.next()
                            yield
                            p.op("dve", lambda e, lg=lg, mx8=mx8, msk=msk: e.tensor_scalar(out=msk[:], in0=lg[:], scalar1=mx8[:, 3:4], scalar2=None, op0=ALU.is_ge),
                                 reads=[klg, kmx], writes=[kmsk])
                            yield
                            p.op("dve", lambda e, mx8=mx8: e.tensor_scalar(out=mx8[:, 7:8], in0=mx8[:, 0:1], scalar1=-1.0, scalar2=None, op0=ALU.mult),
                                 reads=[kmx, kmsk], writes=[kmx])
                            yield
                            ex, kex = ex_r.next()
                            yield
                            p.op("act", lambda e, lg=lg, mx8=mx8, ex=ex: e.activation(out=ex[:], in_=lg[:], func=AF.Exp, bias=mx8[:, 7:8]), reads=[klg, kmx], writes=[kex])
                            yield
                            sm_, ksm = sm_r.next()
                            yield
                            p.op("dve", lambda e, ex=ex, msk=msk: e.tensor_tensor(out=ex[:], in0=ex[:], in1=msk[:], op=ALU.mult), reads=[kex, kmsk], writes=[kex])
                            yield
                            p.op("dve", lambda e, ex=ex, sm_=sm_: e.reduce_sum(out=sm_[:], in_=ex[:], axis=mybir.AxisListType.X), reads=[kex], writes=[ksm])
                            yield
                            p.op("dve", lambda e, sm_=sm_: e.reciprocal(out=sm_[:], in_=sm_[:]), reads=[ksm], writes=[ksm])
                            yield
                            p.op("dve", lambda e, ex=ex, sm_=sm_, tile=tile: e.tensor_scalar(out=Gt[:, tile, :], in0=ex[:], scalar1=sm_[:, 0:1], scalar2=None, op0=ALU.mult),
                                 reads=[kex, ksm], writes=["Gt%d" % tile])
                            yield
                            p.op("dve", lambda e: e.tensor_copy(out=maskb[:, tile, :], in_=msk[:]), reads=[kmsk], writes=["maskb%d" % tile])
                            yield
                            gtmp, kgtmp = gtmp_r.next()
                            yield
                            p.op("pe", lambda e: e.matmul(out=pPos, lhsT=tris[:], rhs=maskb[:, tile, :], start=True, stop=(tile == 0)),
                                 reads=["tris", "maskb%d" % tile], writes=[kpLg])
                            yield
                            for j_ in range(tile):
                                p.op("pe", lambda e: e.matmul(out=pPos, lhsT=ones[:], rhs=maskb[:, j_, :], start=False, stop=(j_ == tile - 1)),
                                     reads=["ones", "maskb%d" % j_], writes=[kpLg])
                            yield
                            p.op("dve", lambda e: e.scalar_tensor_tensor(out=gtmp[:], in0=pPos, scalar=1.0, in1=msk[:], op0=ALU.add, op1=ALU.mult),
                                 reads=[kpLg, kmsk, kgtmp], writes=[kgtmp])
                            yield
                            p.op("dve", lambda e: e.tensor_scalar(out=posm[:, tile, :], in0=gtmp[:], scalar1=-1.0, scalar2=None, op0=ALU.add),
                                 reads=[kgtmp], writes=["posm%d" % tile])
                            yield
                            p.op("dve", lambda e: e.scalar_tensor_tensor(out=pq[:], in0=pPos, scalar=float(CAP - 1), in1=ecap[:], op0=ALU.min, op1=ALU.add),
                                 reads=[kpLg, "ecap"], writes=[kpq])
                            yield
                            yield
                            p.op("dve", lambda e: e.scalar_tensor_tensor(out=gtmp[:], in0=pPos, scalar=float(CAP) - 0.5, in1=Gt[:, tile, :], op0=ALU.is_lt, op1=ALU.mult),
                                 reads=[kpLg, "Gt%d" % tile, kgtmp], writes=[kgtmp])
                            yield
                            oh4, koh4 = oh4_r.tiles[b_], oh4_r.keys[b_]
                            pr4, kpr4 = pr4_r.tiles[b_], pr4_r.keys[b_]
                            lg_b = lg[:].unsqueeze(1).to_broadcast([128, 4, NE])
                            mx_b = mx8[:, 0:4].unsqueeze(2).to_broadcast([128, 4, NE])
                            p.op("dve", lambda e: e.tensor_tensor(out=oh4[:], in0=lg_b, in1=mx_b, op=ALU.is_equal), reads=[klg, kmx], writes=[koh4])
                            yield
                            p.op("dve", lambda e: e.tensor_tensor(out=pr4[:], in0=oh4[:], in1=pq[:].unsqueeze(1).to_broadcast([128, 4, NE]), op=ALU.mult),
                                 reads=[koh4, kpq], writes=[kpr4])
                            yield
                            p.op("dve", lambda e: e.reduce_sum(out=slf[:, 0:4], in_=pr4[:], axis=mybir.AxisListType.X), reads=[kpr4], writes=[kslf])
                            yield
                            p.op("dve", lambda e: e.tensor_tensor(out=pr4[:], in0=oh4[:], in1=gtmp[:].unsqueeze(1).to_broadcast([128, 4, NE]), op=ALU.mult),
                                 reads=[koh4, kgtmp, kpr4], writes=[kpr4])
                            yield
                            p.op("dve", lambda e: e.reduce_sum(out=gj[:, tile, :], in_=pr4[:], axis=mybir.AxisListType.X), reads=[kpr4], writes=["gj%d" % tile])
                            yield
                            p.op("dve", lambda e: e.tensor_copy(out=slI[:, tile, :], in_=slf[:]), reads=[kslf], writes=["slI%d" % tile])

                        for pair in range(2):
                            gens = [c1_tile(G, pair * 2), c1_tile(G, pair * 2 + 1)]
                            while gens:
                                for g_ in list(gens):
                                    try:
                                        next(g_)
                                    except StopIteration:
                                        gens.remove(g_)
                    p.barrier()
                    if stop_after == "C1":
                        if debug:
                            p.op("sp", lambda e: e.dma_start(out=dbgS.rearrange("(t p) n -> p t n", p=128), in_=slI[:]), reads=["slI%d" % i_ for i_ in range(16)], dma="dbgS")
                            p.op("sp", lambda e: e.dma_start(out=dbgJ.rearrange("(t p) n -> p t n", p=128), in_=gj[:]), reads=["gj%d" % i_ for i_ in range(16)], dma="dbgJ")
                            p.op("sp", lambda e: e.dma_start(out=dbgH.rearrange("(t p) n -> p t n", p=128), in_=H[:]), reads=["H%d" % i_ for i_ in range(16)], dma="dbgH")
                            p.op("sp", lambda e: e.dma_start(out=dbgG.rearrange("(t p) n -> p t n", p=128), in_=Gt[:]), reads=["Gt%d" % i_ for i_ in range(16)], dma="dbgG")
                        p.finish_wait("sp"); p.emit(top); return nc
                s2 = ExitStack()
                with s2:
                    sb2, ps2 = mk_alloc(s2)
                    W_r = Ring(sb2, "Wx", 6, [128, 8, 512], BF16)
                    bd_r = Ring(sb2, "bdn", 2, [1, D], BF16)
                    pGL_r = Ring(ps2, "pGL", 4, [128, 512], F32)
                    pA_r = Ring(ps2, "pA", 2, [128, 512], F32)
                    pTs = ps2("pTs", [128, 8, 128], BF16)
                    ptk = ps2("ptk", [128, 8], F32)
                    gl_r = Ring(sb2, "gl", 1, [128, CAP], F32)
                    sg_r = Ring(sb2, "sg", 1, [128, CAP], F32)
                    Sel = sb2("Sel", [128, 16, CAP], BF16)
                    Xe_r = Ring(sb2, "Xe", 2, [128, NS, D], BF16)
                    XeT = sb2("XeT", [128, 8, CAP], BF16)
                    aT = sb2("aTs", [128, 8, CAP], BF16)
                    Yst_r = Ring(sb2, "Yst", 2, [128, D], F32)
                    tks = sb2("tks", [128, 8], F32)
                    tkf = sb2("tkf", [128, 4], F32)
                    tkI_r = Ring(sb2, "tkI", 2, [128, 4], I32)
                    iota_i = sb2("iota_i", [128, CAP], I32)
                    iota_f = sb2("iota_f", [128, CAP], F32)
                    p.op("pool", lambda e: e.iota(iota_i[:], pattern=[[1, CAP]], base=0, channel_multiplier=0), writes=["iota_i"])
                    p.op("dve", lambda e: e.tensor_copy(out=iota_f[:], in_=iota_i[:]), reads=["iota_i"], writes=["iota_f"])
                    tid = sb2("tid", [128, 16], I32)
                    tidx = sb2("tidx", [128, 16], I32)
                    tidhl = sb2("tidhl", [128, 16, 2], BF16)
                    p.op("pool", lambda e: e.iota(tid[:], pattern=[[128, 16]], base=0, channel_multiplier=1), writes=["tid"])
                    p.op("dve", lambda e: e.tensor_scalar(out=tidx[:], in0=tid[:], scalar1=6, scalar2=None, op0=ALU.arith_shift_right), reads=["tid"], writes=["tidx"])
                    p.op("dve", lambda e: e.tensor_copy(out=tidhl[:, :, 0], in_=tidx[:]), reads=["tidx"], writes=["tidhl"])
                    p.op("dve", lambda e: e.tensor_scalar(out=tidx[:], in0=tid[:], scalar1=63, scalar2=None, op0=ALU.bitwise_and), reads=["tid", "tidx", "tidhl"], writes=["tidx"])
                    p.op("dve", lambda e: e.tensor_copy(out=tidhl[:, :, 1], in_=tidx[:]), reads=["tidx", "tidhl"], writes=["tidhl"])
                    bg3 = bgT[:].rearrange("p (e c) -> p e c", c=16)
                    p.op("dve", lambda e: e.tensor_scalar(out=bg3[:, :, 8:16], in0=bg3[:, :, 8:16], scalar1=1.0, scalar2=None, op0=ALU.add),
                         reads=["bgT"], writes=["bgT"])

                    def load_w(src, slot):
                        Wt, kW = W_r.tiles[slot], W_r.keys[slot]
                        p.op("pool", lambda e: e.dma_start(out=Wt[:], in_=src.rearrange("(c p) n -> p c n", p=128)), writes=[kW], dma=kW)
                        return Wt, kW

                    def load_bd(ex_):
                        bd, kbd = bd_r.next()
                        p.op("pool", lambda e: e.dma_start(out=bd[:], in_=b_dn[ex_:ex_ + 1, :]), writes=[kbd], dma=kbd)
                        return bd, kbd

                    xe_of = {}

                    def sel_build(ex_, tiles):
                        for tile in tiles:
                            p.op("dve", lambda e: e.tensor_scalar(out=Sel[:, tile, :], in0=iota_f[:], scalar1=posm[:, tile, ex_:ex_ + 1], scalar2=None, op0=ALU.is_equal),
                                 reads=["iota_f", "posm%d" % tile], writes=["Sel%d" % tile])

                    def dispatch(ex_, build=True):
                        if build:
                            sel_build(ex_, range(16))
                        for s_ in range(NS):
                            for tile in range(16):
                                p.op("pe", lambda e: e.matmul(out=ptk[:, 2 * s_:2 * s_ + 2], lhsT=Sel[:, tile, s_ * 128:(s_ + 1) * 128], rhs=tidhl[:, tile, :],
                                                              start=(tile == 0), stop=(tile == 15)), reads=["Sel%d" % tile, "tidhl"], writes=["ptk"])
                        p.op("dve", lambda e: e.tensor_copy(out=tks[:, 0:2 * NS], in_=ptk[:, 0:2 * NS]), reads=["ptk"], writes=["tks"])
                        tk3 = tks[:, 0:2 * NS].rearrange("p (s t) -> p s t", t=2)
                        p.op("dve", lambda e: e.scalar_tensor_tensor(out=tkf[:, 0:NS], in0=tk3[:, :, 0], scalar=64.0, in1=tk3[:, :, 1], op0=ALU.mult, op1=ALU.add),
                             reads=["tks"], writes=["tkf"])
                        tkI, ktkI = tkI_r.next()
                        p.op("dve", lambda e: e.tensor_copy(out=tkI[:, 0:NS], in_=tkf[:, 0:NS]), reads=["tkf"], writes=[ktkI])
                        Xe, kXe = Xe_r.next()
                        for s_ in range(NS):
                            p.op("pool", lambda e: e.indirect_dma_start(out=Xe[:, s_, :], out_offset=None, in_=Ud[:, :],
                                                                         in_offset=bass.IndirectOffsetOnAxis(ap=tkI[:, s_:s_ + 1], axis=0)),
                                 reads=[ktkI], writes=[kXe], dma=kXe)
                        xe_of[ex_] = (Xe, kXe)

                    def transposes_s(ex_, s_):
                        Xe, kXe = xe_of[ex_]
                        for k in range(8):
                            p.op("pe", lambda e: e.transpose(out=pTs[:, k, :], in_=Xe[:, s_, k * 128:(k + 1) * 128], identity=ident[:]),
                                 reads=[kXe, "ident"], writes=["pTs"])
                        if s_ % 2 == 0:
                            p.op("act", lambda e: e.copy(out=XeT[:, :, s_ * 128:(s_ + 1) * 128], in_=pTs[:]), reads=["pTs"], writes=["XeT"])
                        else:
                            p.op("dve", lambda e: e.tensor_copy(out=XeT[:, :, s_ * 128:(s_ + 1) * 128], in_=pTs[:]), reads=["pTs"], writes=["XeT"])
                        if s_ == NS - 1:
                            xe_of.pop(ex_)

                    def transposes(ex_):
                        for s_ in range(NS):
                            transposes_s(ex_, s_)

                    def gu_stage(ex_, st, Wg, kWg, Wl, kWl, sel_for=None):
                        for mc in range(4):
                            if sel_for is not None:
                                sel_build(sel_for, range(mc * 4, mc * 4 + 4))
                            c = st * 4 + mc
                            pG, kpG = pGL_r.next()
                            pLn, kpLn = pGL_r.next()
                            for k in range(8):
                                p.op("pe", lambda e: e.matmul(out=pG[:, 0:CAP], lhsT=Wg[:, k, mc * 128:(mc + 1) * 128], rhs=XeT[:, k, :],
                                                              start=(k == 0), stop=(k == 7)), reads=[kWg, "XeT"], writes=[kpG])
                            for k in range(8):
                                p.op("pe", lambda e: e.matmul(out=pLn[:, 0:CAP], lhsT=Wl[:, k, mc * 128:(mc + 1) * 128], rhs=XeT[:, k, :],
                                                              start=(k == 0), stop=(k == 7)), reads=[kWl, "XeT"], writes=[kpLn])
                            gl, kgl = gl_r.next()
                            sg, ksg = sg_r.next()
                            bgc = ex_ * 16 + c
                            blc = ex_ * 16 + 8 + c
                            kaT = "aT_%d" % st
                            p.op("dve", lambda e: e.tensor_scalar(out=gl[:], in0=pG[:, 0:CAP], scalar1=bgT[:, bgc:bgc + 1], scalar2=7.0, op0=ALU.add, op1=ALU.min),
                                 reads=[kpG, "bgT"], writes=[kgl])
                            p.op("act", lambda e: e.activation(out=sg[:], in_=gl[:], func=AF.Sigmoid, scale=1.702), reads=[kgl], writes=[ksg])
                            p.op("dve", lambda e: e.tensor_tensor(out=sg[:], in0=sg[:], in1=gl[:], op=ALU.mult), reads=[kgl, ksg], writes=[ksg])
                            p.op("dve", lambda e: e.tensor_scalar(out=gl[:], in0=pLn[:, 0:CAP], scalar1=bgT[:, blc:blc + 1], scalar2=-6.0, op0=ALU.add, op1=ALU.max),
                                 reads=[kpLn, "bgT", kgl], writes=[kgl])
                            p.op("dve", lambda e: e.scalar_tensor_tensor(out=aT[:, c, :], in0=gl[:], scalar=8.0, in1=sg[:], op0=ALU.min, op1=ALU.mult),
                                 reads=[ksg, kgl], writes=[kaT])

                    def dn_stage(ex_, d0, d1, bd, kbd, nxt=None):
                        for s_ in range(NS):
                            if nxt is not None:
                                transposes_s(nxt, s_)
                            Yst, kY = Yst_r.next()
                            for half in range(2):
                                Wd, kWd = (d0, d1)[half]
                                pA, kpA = pA_r.next()
                                for c in range(8):
                                    p.op("pe", lambda e: e.matmul(out=pA[:], lhsT=aT[:, c, s_ * 128:(s_ + 1) * 128], rhs=Wd[:, c, :], start=(c == 0), stop=False),
                                         reads=["aT_%d" % (c // 4), kWd], writes=[kpA])
                                p.op("pe", lambda e: e.matmul(out=pA[:], lhsT=ones[0:1, :], rhs=bd[0:1, half * 512:(half + 1) * 512], start=False, stop=True),
                                     reads=["ones", kbd], writes=[kpA])
                                p.op("act", lambda e: e.copy(out=Yst[:, half * 512:(half + 1) * 512], in_=pA[:]), reads=[kpA], writes=[kY])
                            r0 = ex_ * CAP + s_ * 128
                            p.op("sp", lambda e: e.dma_start(out=Yd[r0:r0 + 128, :], in_=Yst[:]), reads=[kY], dma=kY + "s")

                    def loads_gl0(ex_):
                        return load_w(w_gu[ex_, :, 0:512], 0), load_w(w_gu[ex_, :, 1024:1536], 1)

                    def loads_gl1(ex_):
                        return load_w(w_gu[ex_, :, 512:1024], 2), load_w(w_gu[ex_, :, 1536:2048], 3)

                    def loads_d(ex_):
                        return load_w(w_dn[ex_, :, 0:512], 4), load_w(w_dn[ex_, :, 512:1024], 5), load_bd(ex_)

                    g0, l0 = loads_gl0(0)
                    g1, l1 = loads_gl1(0)
                    d0, d1, (bd, kbd) = loads_d(0)
                    dispatch(0)
                    dispatch(1)
                    transposes(0)
                    for ex_ in range(NE):
                        gu_stage(ex_, 0, g0[0], g0[1], l0[0], l0[1], sel_for=(ex_ + 2 if ex_ + 2 < NE else None))
                        if ex_ + 1 < NE:
                            g0n, l0n = loads_gl0(ex_ + 1)
                        gu_stage(ex_, 1, g1[0], g1[1], l1[0], l1[1])
                        if ex_ + 2 < NE:
                            dispatch(ex_ + 2, build=False)
                        if ex_ + 1 < NE:
                            g1n, l1n = loads_gl1(ex_ + 1)
                        dn_stage(ex_, d0, d1, bd, kbd, nxt=(ex_ + 1 if ex_ + 1 < NE else None))
                        if ex_ + 1 < NE:
                            d0, d1, (bd, kbd) = loads_d(ex_ + 1)
                            g0, l0, g1, l1 = g0n, l0n, g1n, l1n
                    p.barrier()
                    Yg_r = Ring(sb2, "Yg", 4, [128, D], F32)
                    for tile in range(16):
                        hk = "H%d" % tile
                        for j_ in range(4):
                            Yg, kYg = Yg_r.next()
                            p.op("pool", lambda e: e.indirect_dma_start(out=Yg[:, :], out_offset=None, in_=Yd[:, :],
                                                                         in_offset=bass.IndirectOffsetOnAxis(ap=slI[:, tile, j_:j_ + 1], axis=0)),
                                 reads=["slI%d" % tile], writes=[kYg], dma=kYg)
                            p.op("dve", lambda e: e.scalar_tensor_tensor(out=H[:, tile, :], in0=Yg[:], scalar=gj[:, tile, j_:j_ + 1], in1=H[:, tile, :], op0=ALU.mult, op1=ALU.add),
                                 reads=[kYg, "gj%d" % tile, hk], writes=[hk])
                    p.barrier()
                    if stop_after == "C2":
                        if debug:
                            p.op("sp", lambda e: e.dma_start(out=dbgH.rearrange("(t p) n -> p t n", p=128), in_=H[:]), reads=["H%d" % i_ for i_ in range(16)], dma="dbgH")
                            p.op("sp", lambda e: e.dma_start(out=dbgG.rearrange("(t p) n -> p t n", p=128), in_=Gt[:]), reads=["Gt%d" % i_ for i_ in range(16)], dma="dbgG")
                        p.finish_wait("sp"); p.emit(top); return nc
                s3 = ExitStack()
                with s3:
                    sb3, ps3 = mk_alloc(s3)
                    gbc = sb3("gbc3", [128, D], F32)
                    junk = sb3("junkC3", [128, D], BF16)
                    Wpg = sb3("Wpg", [128, 8, D], BF16)
                    Wpp = sb3("Wpp", [128, 2, D], BF16)
                    p.op("pool", lambda e: e.dma_start(out=Wpg[:], in_=w_pg.rearrange("(c p) n -> p c n", p=128)), writes=["Wpg"], dma="Wpg")
                    p.op("pool", lambda e: e.dma_start(out=Wpp[:], in_=w_pp.rearrange("(c p) n -> p c n", p=128)), writes=["Wpp"], dma="Wpp")
                    gfin = sb3("gfin", [128, D], F32)
                    p.op("sp", lambda e: e.dma_start(out=gbc[:], in_=g_ple.partition_broadcast(128)), writes=["gbc"], dma="gbc3")
                    p.op("sp", lambda e: e.dma_start(out=gfin[:], in_=g_final.partition_broadcast(128)), writes=["gfin"], dma="gfin")
                    ss3_r = Ring(sb3, "ss3", 2, [128, 1], F32)
                    u3_r = Ring(sb3, "u3", 2, [128, D], BF16)
                    pT3_r = Ring(ps3, "pT3", 2, [128, 8, 128], BF16)
                    u3T_r = Ring(sb3, "u3T", 2, [128, 8, 128], BF16)
                    pp_r = Ring(sb3, "ppl", 2, [128, 256], F32)
                    ppb_r = Ring(sb3, "ppb", 2, [128, 256], BF16)
                    ppT_r = Ring(sb3, "ppT", 2, [128, 2, 128], BF16)
                    pg_r = Ring(ps3, "pg3", 2, [128, 512], F32)
                    pj_r = Ring(ps3, "pj3", 2, [128, 512], F32)
                    sg3_r = Ring(sb3, "sg3", 2, [128, 512], F32)
                    o_r = Ring(sb3, "o3", 2, [128, D], F32)
                    def c3_tile(tile):
                        hk = "H%d" % tile
                        yield
                        ss3, kss = ss3_r.next()
                        yield
                        p.op("act", lambda e, ss3=ss3, tile=tile: e.activation(out=junk[:], in_=H[:, tile, :], func=AF.Square, accum_out=ss3[:]), reads=[hk], writes=["junkC", kss])
                        yield
                        rstd_from(ss3[:], ss3[:], D, [kss], [kss])
                        yield
                        u3, ku3 = u3_r.next()
                        yield
                        p.op("dve", lambda e, ss3=ss3, u3=u3, tile=tile: e.scalar_tensor_tensor(out=u3[:], in0=H[:, tile, :], scalar=ss3[:, 0:1], in1=gbc[:], op0=ALU.mult, op1=ALU.mult),
                             reads=[hk, kss, "gbc"], writes=[ku3])
                        yield
                        pT, kpT = pT3_r.next()
                        yield
                        for c in range(8):
                            p.op("pe", lambda e, c=c, pT=pT, u3=u3: e.transpose(out=pT[:, c, :], in_=u3[:, c * 128:(c + 1) * 128], identity=ident[:]), reads=[ku3, "ident"], writes=[kpT])
                        yield
                        u3T, ku3T = u3T_r.next()
                        yield
                        p.op("act", lambda e, pT=pT, u3T=u3T: e.copy(out=u3T[:], in_=pT[:]), reads=[kpT], writes=[ku3T])
                        yield
                        pp, kpp = pp_r.next()
                        yield
                        p.op("sp", lambda e, pp=pp, tile=tile: e.dma_start(out=pp[:], in_=po[tile * 128:(tile + 1) * 128, :]), writes=[kpp], dma=kpp)
                        yield
                        ppb, kppb = ppb_r.next()
                        yield
                        p.op("pool", lambda e, pp=pp, ppb=ppb: e.tensor_copy(out=ppb[:], in_=pp[:]), reads=[kpp], writes=[kppb])
                        yield
                        pT2, kpT2 = pT3_r.next()
                        yield
                        for c in range(2):
                            p.op("pe", lambda e, c=c, pT2=pT2, ppb=ppb: e.transpose(out=pT2[:, c, :], in_=ppb[:, c * 128:(c + 1) * 128], identity=ident[:]), reads=[kppb, "ident"], writes=[kpT2])
                        yield
                        ppT, kppT = ppT_r.next()
                        yield
                        p.op("act", lambda e, pT2=pT2, ppT=ppT: e.copy(out=ppT[:], in_=pT2[:, 0:2, :]), reads=[kpT2], writes=[kppT])
                        yield
                        for half in range(2):
                            pg, kpg = pg_r.next()
                            pj, kpj = pj_r.next()
                            for c in range(8):
                                p.op("pe", lambda e, c=c, pg=pg, u3T=u3T, half=half: e.matmul(out=pg[:], lhsT=u3T[:, c, :], rhs=Wpg[:, c, half * 512:(half + 1) * 512], start=(c == 0), stop=(c == 7)),
                                     reads=[ku3T, "Wpg"], writes=[kpg])
                            for c in range(2):
                                p.op("pe", lambda e, c=c, pj=pj, ppT=ppT, half=half: e.matmul(out=pj[:], lhsT=ppT[:, c, :], rhs=Wpp[:, c, half * 512:(half + 1) * 512], start=(c == 0), stop=(c == 1)),
                                     reads=[kppT, "Wpp"], writes=[kpj])
                            sg, ksg = sg3_r.next()
                            p.op("act", lambda e, sg=sg, pg=pg: e.activation(out=sg[:], in_=pg[:], func=AF.Sigmoid), reads=[kpg], writes=[ksg])
                            p.op("dve", lambda e, sg=sg, pj=pj: e.tensor_tensor(out=sg[:], in0=sg[:], in1=pj[:], op=ALU.mult), reads=[ksg, kpj], writes=[ksg])
                            p.op("dve", lambda e, sg=sg, tile=tile, half=half: e.tensor_tensor(out=H[:, tile, half * 512:(half + 1) * 512], in0=H[:, tile, half * 512:(half + 1) * 512], in1=sg[:], op=ALU.add),
                                 reads=[ksg, hk], writes=[hk])
                        yield
                        ss4, kss4 = ss3_r.next()
                        yield
                        p.op("act", lambda e, ss4=ss4, tile=tile: e.activation(out=junk[:], in_=H[:, tile, :], func=AF.Square, accum_out=ss4[:]), reads=[hk], writes=["junkC", kss4])
                        yield
                        rstd_from(ss4[:], ss4[:], D, [kss4], [kss4])
                        yield
                        ot, kot = o_r.next()
                        yield
                        p.op("dve", lambda e, ss4=ss4, ot=ot, tile=tile: e.scalar_tensor_tensor(out=ot[:], in0=H[:, tile, :], scalar=ss4[:, 0:1], in1=gfin[:], op0=ALU.mult, op1=ALU.mult),
                             reads=[hk, kss4, "gfin"], writes=[kot])
                        yield
                        p.op("sp", lambda e, ot=ot, tile=tile: e.dma_start(out=yo[tile * 128:(tile + 1) * 128, :], in_=ot[:]), reads=[kot], writes=["yo"], dma=kot + "s")

                    for pair in range(8):
                        gens = [c3_tile(pair * 2), c3_tile(pair * 2 + 1)]
                        while gens:
                            for g_ in list(gens):
                                try:
                                    next(g_)
                                except StopIteration:
                                    gens.remove(g_)
        p.finish_wait("sp")
        p.emit(top)
    return nc


_CACHE = {}


def _perm(j):
    idx = []
    for t in range(4):
        for blk in ORDER[j]:
            b0 = (8 * t + blk) * 128
            idx.append(np.arange(b0, b0 + 128))
    return np.concatenate(idx)


def kernel(x, p, positions, w_in, g_attn, g_cq, w_uq, g_ckv, w_ukv, g_out_mla, g_out_sb, w_o,
           g_moe, w_router, b_router, w_gu, b_gu, w_dn, b_dn, g_ple, w_ple_gate, w_ple_proj, g_final):
    if "nc" not in _CACHE:
        _CACHE["nc"] = build_program()
    nc = _CACHE["nc"]
    in_maps, perms = make_in_maps(x, p, positions, w_in, g_attn, g_cq, w_uq, g_ckv, w_ukv, g_out_mla, g_out_sb, w_o,
                                  g_moe, w_router, b_router, w_gu, b_gu, w_dn, b_dn, g_ple, w_ple_gate, w_ple_proj, g_final)
    res = run_bass_kernel_spmd(nc, in_maps, core_ids=list(range(8)))
    out = np.empty((4, S, D), np.float32)
    for c in range(8):
        b, j = c // 2, c % 2
        out[b, perms[j]] = np.asarray(res.results[c]["yo"])
    return out


def make_in_maps(x, p, positions, w_in, g_attn, g_cq, w_uq, g_ckv, w_ukv, g_out_mla, g_out_sb, w_o,
                 g_moe, w_router, b_router, w_gu, b_gu, w_dn, b_dn, g_ple, w_ple_gate, w_ple_proj, g_final):
    f = lambda a: np.ascontiguousarray(np.asarray(a))
    x = f(x); p = f(p); positions = f(positions)
    invf = np.zeros((128, 1), np.float32)
    fr = (10000.0 ** (-np.arange(0, 32, 2, dtype=np.float32) / 32.0)).astype(np.float32)
    invf[0:16, 0] = fr
    invf[16:32, 0] = fr
    shared = {
        "invf": invf,
        "w_in": f(w_in[0]), "g_attn": f(g_attn[0:1]), "g_cq": f(g_cq[0:1]), "w_uq": f(w_uq[0]),
        "g_ckv": f(g_ckv[0:1]), "w_ukv": f(w_ukv[0]),
        "g_out": f(np.concatenate([np.asarray(g_out_mla[0]), np.asarray(g_out_sb[0])])[None, :]),
        "w_o": f(w_o[0]), "g_moe": f(g_moe[0:1]), "w_router": f(w_router[0]), "b_router": f(b_router[0:1]),
        "w_gu": f(w_gu[0]), "b_gu": f(np.asarray(b_gu[0]).reshape(NE * 16, 128)), "w_dn": f(w_dn[0]), "b_dn": f(b_dn[0]),
        "g_ple": f(g_ple[0:1]), "w_pg": f(w_ple_gate[0]), "w_pp": f(w_ple_proj[0]), "g_final": f(np.asarray(g_final)[None, :]),
    }
    in_maps = []
    perms = [_perm(0), _perm(1)]
    for c in range(8):
        b, j = c // 2, c % 2
        pm = perms[j]
        qr = np.concatenate([np.arange(blk * 128, blk * 128 + 128) for blk in ORDER[j]]).astype(np.float32)[None, :]
        m = dict(shared)
        m["xa"] = x[b]
        m["xo"] = f(x[b][pm])
        m["po"] = f(p[0, b][pm])
        m["posa"] = f(positions[b:b + 1].astype(np.int32))
        m["poso"] = f(positions[b:b + 1, pm].astype(np.int32))
        m["qrel"] = f(qr)
        in_maps.append(m)
    return in_maps, perms
```

```python
from contextlib import ExitStack
import numpy as np
import concourse.bass as bass
import concourse.mybir as mybir
from concourse.bass_utils import run_bass_kernel_spmd

F32 = mybir.dt.float32
BF16 = mybir.dt.bfloat16
I32 = mybir.dt.int32
AF = mybir.ActivationFunctionType
ALU = mybir.AluOpType

ENGS = ("pe", "act", "dve", "pool", "sp")
S = 4096
D = 1024
NE = 32
ORDER = ([6, 5, 3, 0], [7, 4, 2, 1])
NDIAG = [512, 512, 384, 384, 256, 256, 128, 128]
NEG = -30000.0
EPS = 1e-6


class Prog:
    def __init__(self, nc):
        self.nc = nc
        self.ops = {e: [] for e in ENGS}
        self.vcs = {}
        self.cur = {e: {} for e in ENGS}
        self.last_w = {}
        self.readers = {}
        self.excl = set()

    def op(self, eng, fn, reads=(), writes=(), dma=None):
        rec_ = _Rec()
        fn(rec_)
        assert len(rec_.calls) == 1
        fn = rec_.calls[0]
        clk = ("dma:" + dma) if dma else eng
        deps = []
        reads = list(reads)
        writes = list(writes)
        for k in reads:
            if k in self.excl and k not in writes:
                writes.append(k)
        for k in reads:
            lw = self.last_w.get(k)
            if lw:
                deps.append(lw)
        for k in writes:
            lw = self.last_w.get(k)
            if lw:
                deps.append(lw)
            for c, i in self.readers.get(k, {}).items():
                deps.append((c, i))
        cur = self.cur[eng]
        wmax = {}
        for (c, i) in deps:
            if c == "pe" and eng == "pe" and not dma:
                continue
            if cur.get(c, 0) >= i:
                continue
            wmax[c] = max(wmax.get(c, 0), i)
            for c2, i2 in self.vcs[c][i - 1].items():
                if cur.get(c2, 0) < i2:
                    cur[c2] = i2
            if cur.get(c, 0) < i:
                cur[c] = i
        vc = dict(cur)
        lst = self.vcs.setdefault(clk, [])
        lst.append(vc)
        idx = len(lst)
        vc[clk] = idx
        rec = {"fn": fn, "waits": wmax, "clk": clk, "idx": idx}
        self.ops[eng].append(rec)
        for k in reads:
            self.readers.setdefault(k, {})[clk] = idx
        for k in writes:
            self.last_w[k] = (clk, idx)
            self.readers[k] = {}
        return rec

    def finish_wait(self, eng):
        waits = {}
        for c, l in self.vcs.items():
            if len(l) and self.cur[eng].get(c, 0) < len(l):
                waits[c] = len(l)
                self.cur[eng][c] = len(l)
        self.ops[eng].append({"fn": None, "waits": waits, "clk": None, "idx": None})

    def barrier(self):
        for e in ENGS:
            self.finish_wait(e)
        full = {c: len(l) for c, l in self.vcs.items()}
        for e in ENGS:
            self.cur[e] = dict(full)

    def emit(self, stack):
        nc = self.nc
        waited = {}
        for e in ENGS:
            for r in self.ops[e]:
                for c, i in r["waits"].items():
                    waited.setdefault(c, set()).add(i)
        sems, semval = {}, {}
        for c, l in self.vcs.items():
            if c not in waited:
                continue
            sems[c] = stack.enter_context(nc.semaphore("s_" + c.replace(":", "_")))
            isd = c.startswith("dma:")
            v, m = 0, {}
            for i in range(1, len(l) + 1):
                if isd or i in waited[c]:
                    v += 16 if isd else 1
                    m[i] = v
            semval[c] = m
        block = stack.enter_context(nc.Block())
        engobj = {"pe": "tensor", "act": "scalar", "dve": "vector", "pool": "gpsimd", "sp": "sync"}

        def make(e):
            def body(eng):
                for r in self.ops[e]:
                    for c, i in r["waits"].items():
                        eng.wait_ge(sems[c], semval[c][i])
                    if r["fn"] is None:
                        continue
                    name, a, k = r["fn"]
                    ins = getattr(eng, name)(*a, **k)
                    c, i = r["clk"], r["idx"]
                    if c in sems and i in semval[c]:
                        ins.then_inc(sems[c], 16 if c.startswith("dma:") else 1)
            return body

        for e in ENGS:
            if self.ops[e]:
                getattr(block, engobj[e])(make(e))


class _Rec:
    def __init__(self):
        self.calls = []

    def __getattr__(self, name):
        def f(*a, **k):
            self.calls.append((name, a, k))
        return f


class Ring:
    def __init__(self, alloc, name, n, shape, dtype):
        self.tiles = [alloc("%s%d" % (name, i), shape, dtype) for i in range(n)]
        self.keys = ["%s%d" % (name, i) for i in range(n)]
        self.i = 0

    def next(self):
        t, k = self.tiles[self.i % len(self.tiles)], self.keys[self.i % len(self.tiles)]
        self.i += 1
        return t, k


class _Stop(Exception):
    pass


def build_program(stop_after=None, debug=False):
    nc = bass.Bass("TRN2", target_bir_lowering=False)

    def din(name, shape, dt=F32):
        return nc.dram_tensor(name, list(shape), dt, kind="ExternalInput").ap()

    def dscr(name, shape, dt=BF16):
        return nc.dram_tensor(name, list(shape), dt, kind="ExternalOutput" if debug else "Internal").ap()

    xa = din("xa", [S, D])
    xo = din("xo", [2048, D])
    po = din("po", [2048, 256])
    posa = din("posa", [1, S], I32)
    poso = din("poso", [1, 2048], I32)
    qrel = din("qrel", [1, 512])
    invf = din("invf", [128, 1])
    w_in = din("w_in", [D, 2208])
    g_attn = din("g_attn", [1, D])
    g_cq = din("g_cq", [1, 384])
    w_uq = din("w_uq", [384, 768])
    g_ckv = din("g_ckv", [1, 256])
    w_ukv = din("w_ukv", [256, 1024])
    g_out = din("g_out", [1, 1024])
    w_o = din("w_o", [D, D])
    g_moe = din("g_moe", [1, D])
    w_router = din("w_router", [D, NE])
    b_router = din("b_router", [1, NE])
    NEd = NE if stop_after in (None, "C2") else 1
    w_gu = din("w_gu", [NEd, D, 2048])
    b_gu = din("b_gu", [NE * 16, 128])
    w_dn = din("w_dn", [NEd, D, D])
    b_dn = din("b_dn", [NE, D])
    g_ple = din("g_ple", [1, D])
    w_pg = din("w_pg", [D, D])
    w_pp = din("w_pp", [256, D])
    g_final = din("g_final", [1, D])
    yo = nc.dram_tensor("yo", [2048, D], F32, kind="ExternalOutput").ap()

    KnT = dscr("KnT", [512, S])
    KrT = dscr("KrT", [32, S])
    VmD = dscr("VmD", [S, 512])
    KsT = dscr("KsT", [512, S])
    VsD = dscr("VsD", [S, 512])
    QmT = dscr("QmT", [8 * 96, 2048])
    QsT = dscr("QsT", [512, 2048])
    OTd = dscr("OTd", [1024, 2048])
    CAP = 512
    NS = CAP // 128
    Ud = nc.dram_tensor("Ud", [2048, D], BF16, kind="Internal").ap()
    Yd = nc.dram_tensor("Yd", [NE * CAP, D], F32, kind="Internal").ap()
    if debug:
        dbgH = nc.dram_tensor("dbgH", [2048, D], F32, kind="ExternalOutput").ap()
        dbgG = nc.dram_tensor("dbgG", [2048, NE], F32, kind="ExternalOutput").ap()
        dbgS = nc.dram_tensor("dbgS", [2048, 4], I32, kind="ExternalOutput").ap()
        dbgJ = nc.dram_tensor("dbgJ", [2048, 4], F32, kind="ExternalOutput").ap()

    top = ExitStack()
    with top:
        p = Prog(nc)
        if True:

            def mk_alloc(stack):
                def sb(n, s, d):
                    return stack.enter_context(nc.sbuf_tensor(n, list(s), d))

                def ps(n, s, d=F32):
                    return stack.enter_context(nc.psum_tensor(n, list(s), d))
                return sb, ps

            sb0, ps0 = mk_alloc(top)

            identf = sb0("identf", [128, 128], F32)
            ident = sb0("ident", [128, 128], BF16)
            ones = sb0("ones", [128, 128], BF16)
            ntri = sb0("ntri", [128, 128], BF16)
            nones = sb0("nones", [128, 128], BF16)
            zeros = sb0("zeros", [128, 128], BF16)
            p.op("pool", lambda e: e.memset(identf[:], 0.0), writes=["identf"])
            p.op("pool", lambda e: e.affine_select(out=identf[:], in_=identf[:], pattern=[[-1, 128]],
                                                    compare_op=ALU.not_equal, fill=1.0, base=0, channel_multiplier=1),
                 reads=["identf"], writes=["identf"])
            p.op("dve", lambda e: e.tensor_copy(out=ident[:], in_=identf[:]), reads=["identf"], writes=["ident"])
            p.op("pool", lambda e: e.memset(ones[:], 1.0), writes=["ones"])
            p.op("pool", lambda e: e.memset(nones[:], -1.0), writes=["nones"])
            p.op("pool", lambda e: e.memset(zeros[:], 0.0), writes=["zeros"])
            gcol = sb0("gcol", [128, 16], F32)
            p.op("sp", lambda e: e.dma_start(out=gcol[:, 0:3], in_=g_cq.rearrange("o (c p) -> p (o c)", p=128),
                                             allow_slow_non_contiguous=True), writes=["gcol"], dma="gcol")
            p.op("sp", lambda e: e.dma_start(out=gcol[:, 3:5], in_=g_ckv.rearrange("o (c p) -> p (o c)", p=128),
                                             allow_slow_non_contiguous=True), writes=["gcol"], dma="gcol")
            p.op("sp", lambda e: e.dma_start(out=gcol[:, 5:13], in_=g_out.rearrange("o (c p) -> p (o c)", p=128),
                                             allow_slow_non_contiguous=True), writes=["gcol"], dma="gcol")
            invc = sb0("invc", [128, 1], F32)
            p.op("sp", lambda e: e.dma_start(out=invc[:], in_=invf), writes=["invc"], dma="invc")

            def rstd_from(eng_out, src, n, rd, wr):
                p.op("act", lambda e: e.activation(out=eng_out, in_=src, func=AF.Ln, scale=1.0 / n, bias=EPS),
                     reads=rd, writes=wr)
                p.op("act", lambda e: e.activation(out=eng_out, in_=eng_out, func=AF.Exp, scale=-0.5),
                     reads=wr, writes=wr)

            sa = ExitStack()
            with sa:
                sb, ps = mk_alloc(sa)
                tmpf = sb("tmpf", [128, 128], F32)
                p.op("pool", lambda e: e.memset(tmpf[:], -1.0), writes=["tmpf"])
                p.op("pool", lambda e: e.affine_select(out=tmpf[:], in_=tmpf[:], pattern=[[-1, 128]],
                                                        compare_op=ALU.is_ge, fill=0.0, base=0, channel_multiplier=1),
                     reads=["tmpf"], writes=["tmpf"])
                p.op("dve", lambda e: e.tensor_copy(out=ntri[:], in_=tmpf[:]), reads=["tmpf"], writes=["ntri"])

                Win = sb("Win", [128, 8, 2208], BF16)
                for c in range(8):
                    p.op("pool", lambda e, c=c: e.dma_start(out=Win[:, c, :], in_=w_in[c * 128:(c + 1) * 128, :]),
                         writes=["Win"], dma="Win")
                Wuq = sb("Wuq", [128, 3, 768], BF16)
                Wuqr = sb("Wuqr", [128, 3, 768], BF16)
                p.op("pool", lambda e: e.dma_start(out=Wuq[:], in_=w_uq.rearrange("(c p) n -> p c n", p=128)),
                     writes=["Wuq"], dma="Wuq")
                Wkn = sb("Wkn", [128, 2, 512], BF16)
                Wv = sb("Wv", [128, 2, 512], BF16)
                ukv = w_ukv.rearrange("(c p) (h t d) -> p c h t d", p=128, h=8, t=2)
                for c in range(2):
                    p.op("pool", lambda e, c=c: e.dma_start(out=Wkn[:, c, :].rearrange("p (h d) -> p h d", h=8),
                                                          in_=ukv[:, c, :, 0, :]), writes=["Wkn"], dma="Wkn")
                    p.op("pool", lambda e, c=c: e.dma_start(out=Wv[:, c, :].rearrange("p (h d) -> p h d", h=8),
                                                          in_=ukv[:, c, :, 1, :]), writes=["Wv"], dma="Wv")
                p.op("pool", lambda e: e.memset(Wuqr[:], 0.0), writes=["Wuqr"])
                Wq4 = Wuq[:].rearrange("p c (h d) -> p c h d", h=8)
                Wr4 = Wuqr[:].rearrange("p c (h d) -> p c h d", h=8)
                for c in range(3):
                    p.op("dve", lambda e, c=c: e.tensor_scalar(out=Wr4[:, c, :, 64:80], in0=Wq4[:, c, :, 80:96], scalar1=-1.0,
                                                          scalar2=None, op0=ALU.mult), reads=["Wuq", "Wuqr"], writes=["Wuqr"])
                    p.op("dve", lambda e, c=c: e.tensor_copy(out=Wr4[:, c, :, 80:96], in_=Wq4[:, c, :, 64:80]),
                         reads=["Wuq", "Wuqr"], writes=["Wuqr"])
                Wkr = sb("Wkr", [128, 8, 64], BF16)
                p.op("dve", lambda e: e.tensor_copy(out=Wkr[:, :, 0:32], in_=Win[:, :, 640:672]), reads=["Win"], writes=["Wkr"])
                p.op("dve", lambda e: e.tensor_scalar(out=Wkr[:, :, 32:48], in0=Win[:, :, 656:672], scalar1=-1.0, scalar2=None,
                                                      op0=ALU.mult), reads=["Win", "Wkr"], writes=["Wkr"])
                p.op("dve", lambda e: e.tensor_copy(out=Wkr[:, :, 48:64], in_=Win[:, :, 640:656]), reads=["Win", "Wkr"], writes=["Wkr"])
                gattn = sb("gattn", [128, D], F32)
                p.op("sp", lambda e: e.dma_start(out=gattn[:], in_=g_attn.partition_broadcast(128)), writes=["gattn"], dma="gattn")

                def rope_table(Ct, St, pos_ap, n, rows, name, inv, kinv, sb):
                    posi = sb(name + "_pi", [128, n], I32)
                    ang = sb(name + "_ang", [128, n], F32)
                    kk = sb(name + "_k", [128, n], F32)
                    ki = sb(name + "_ki", [128, n], I32)
                    p.op("sp", lambda e: e.dma_start(out=posi[:], in_=pos_ap.partition_broadcast(128)), writes=[name + "pi"], dma=name + "pi")
                    p.op("dve", lambda e: e.tensor_copy(out=ang[:], in_=posi[:]), reads=[name + "pi"], writes=[name + "ang"])
                    p.op("dve", lambda e: e.tensor_scalar(out=ang[:], in0=ang[:], scalar1=inv[:, 0:1], scalar2=None, op0=ALU.mult),
                         reads=[name + "ang", kinv], writes=[name + "ang"])
                    for which, T in (("s", St), ("c", Ct)):
                        off = 0.0 if which == "s" else float(np.pi / 2)
                        p.op("dve", lambda e, off=off: e.tensor_scalar(out=kk[:], in0=ang[:], scalar1=off, scalar2=float(1.0 / (2 * np.pi)),
                                                                   op0=ALU.add, op1=ALU.mult), reads=[name + "ang"], writes=[name + "kk"])
                        p.op("dve", lambda e: e.tensor_copy(out=ki[:], in_=kk[:]), reads=[name + "kk"], writes=[name + "ki"])
                        p.op("dve", lambda e: e.tensor_copy(out=kk[:], in_=ki[:]), reads=[name + "ki"], writes=[name + "kk"])
                        p.op("dve", lambda e, T=T: e.scalar_tensor_tensor(out=T, in0=kk[:rows], scalar=-6.28125, in1=ang[:rows],
                                                                       op0=ALU.mult, op1=ALU.add),
                             reads=[name + "kk", name + "ang"], writes=[name + which])
                        p.op("dve", lambda e, T=T, off=off: e.scalar_tensor_tensor(out=T, in0=kk[:rows], scalar=float(-(2 * np.pi - 6.28125)), in1=T,
                                                                                op0=ALU.mult, op1=ALU.add),
                             reads=[name + "kk", name + which], writes=[name + which])
                        if off != 0.0:
                            p.op("dve", lambda e, T=T, off=off: e.tensor_scalar(out=T, in0=T, scalar1=off, scalar2=None, op0=ALU.add),
                                 reads=[name + which], writes=[name + which])
                        p.op("dve", lambda e: e.tensor_scalar(out=kk[:rows], in0=T, scalar1=float(np.pi), scalar2=float(-2 * np.pi),
                                                              op0=ALU.is_gt, op1=ALU.mult), reads=[name + which], writes=[name + "kk"])
                        p.op("dve", lambda e, T=T: e.tensor_tensor(out=T, in0=T, in1=kk[:rows], op=ALU.add),
                             reads=[name + which, name + "kk"], writes=[name + which])
                        p.op("dve", lambda e: e.tensor_scalar(out=kk[:rows], in0=T, scalar1=float(-np.pi), scalar2=float(2 * np.pi),
                                                              op0=ALU.is_lt, op1=ALU.mult), reads=[name + which], writes=[name + "kk"])
                        p.op("dve", lambda e, T=T: e.tensor_tensor(out=T, in0=T, in1=kk[:rows], op=ALU.add),
                             reads=[name + which, name + "kk"], writes=[name + which])
                        p.op("dve", lambda e, T=T: e.tensor_scalar(out=T, in0=T, scalar1=3.14159, scalar2=-3.14159, op0=ALU.min, op1=ALU.max),
                             reads=[name + which], writes=[name + which])
                        p.op("act", lambda e, T=T: e.activation(out=T, in_=T, func=AF.Sin), reads=[name + which], writes=[name + which])

                Ck = sb("Ck", [32, S], F32)
                Sk = sb("Sk", [32, S], F32)
                sa2 = ExitStack()
                with sa2:
                    rope_table(Ck[:], Sk[:], posa, S, 32, "rk", invc, "invc", mk_alloc(sa2)[0])
                    p.barrier()
                Cq = sb("Cq", [96, 2048], F32)
                Sq = sb("Sq", [96, 2048], F32)
                sa3 = ExitStack()
                with sa3:
                    sb3_ = mk_alloc(sa3)[0]
                    invq = sb3_("invq", [128, 1], F32)
                    p.op("pool", lambda e: e.memset(invq[:], 0.0), writes=["invq"])
                    p.op("sp", lambda e: e.dma_start(out=invq[64:96, :], in_=invf[0:32, :]), reads=["invq"], writes=["invq"], dma="invq")
                    rope_table(Cq[:], Sq[:], poso, 2048, 96, "rq", invq, "invq", sb3_)
                    p.barrier()
                    if stop_after == "R":
                        p.finish_wait("sp"); p.emit(top); return nc
                msc = float((64 + 32) ** -0.5)
                p.op("dve", lambda e: e.tensor_scalar(out=Cq[:], in0=Cq[:], scalar1=msc, scalar2=None, op0=ALU.mult),
                     reads=["rqc"], writes=["rqc"])
                p.op("dve", lambda e: e.tensor_scalar(out=Sq[:], in0=Sq[:], scalar1=msc, scalar2=None, op0=ALU.mult),
                     reads=["rqs"], writes=["rqs"])

                xt_r = Ring(sb, "xt", 4, [128, D], F32)
                junk = sb("junkA", [128, D], F32)
                ss_r = Ring(sb, "ssA", 4, [128, 1], F32)
                xn_r = Ring(sb, "xn", 4, [128, D], BF16)
                xnT_r = Ring(sb, "xnT", 2, [128, 8, 512], BF16)
                pT_r = Ring(ps, "pTA", 2, [128, 8, 128], BF16)
                pm_r = Ring(ps, "pmA", 4, [128, 512], F32)
                pss = ps("pssA", [128, 512], F32)
                sq_r = Ring(sb, "sqA", 2, [128, 512], BF16)
                rbc = sb("rbcA", [128, 512], F32)
                cn_r = Ring(sb, "cnA", 2, [128, 3, 512], BF16)
                ev_r = Ring(sb, "evA", 2, [128, 512], BF16)
                stg_r = Ring(sb, "stgA", 3, [128, 4, 512], BF16)
                stg8_r = Ring(sb, "stg8A", 2, [128, 8, 512], BF16)
                evf_r = Ring(sb, "evfA", 2, [128, 512], F32)
                evf2_r = Ring(sb, "evf2A", 2, [128, 512], F32)

                def make_xnT(src, tok0, defer=False):
                    xnT, kT = xnT_r.next()
                    xs = []
                    for sub in range(4):
                        xt, kx = xt_r.next()
                        ss, ks = ss_r.next()
                        xn, kn = xn_r.next()
                        r0 = tok0 + sub * 128
                        p.op("sp", lambda e: e.dma_start(out=xt[:], in_=src[r0:r0 + 128, :]), writes=[kx], dma=kx)
                        p.op("act", lambda e: e.activation(out=junk[:], in_=xt[:], func=AF.Square, accum_out=ss[:]),
                             reads=[kx], writes=["junkA", ks])
                        rstd_from(ss[:], ss[:], D, [ks], [ks])
                        p.op("dve", lambda e: e.scalar_tensor_tensor(out=xn[:], in0=xt[:], scalar=ss[:, 0:1], in1=gattn[:],
                                                                     op0=ALU.mult, op1=ALU.mult),
                             reads=[kx, ks, "gattn"], writes=[kn])
                        xs.append((xn, kn))
                    def tr(sub):
                        xn, kn = xs[sub]
                        pT, kp = pT_r.next()
                        for c in range(8):
                            p.op("pe", lambda e: e.transpose(out=pT[:, c, :], in_=xn[:, c * 128:(c + 1) * 128], identity=ident[:]),
                                 reads=[kn, "ident"], writes=[kp])
                        p.op("dve", lambda e: e.tensor_copy(out=xnT[:, :, sub * 128:(sub + 1) * 128], in_=pT[:]),
                             reads=[kp], writes=[kT])
                    if defer:
                        return xnT, kT, tr
                    for sub in range(4):
                        tr(sub)
                    return xnT, kT

                def proj_fm(xnT, kT, col0, m, wkey="Win", W=None):
                    W = Win if W is None else W
                    pm, kpm = pm_r.next()
                    for k in range(8):
                        p.op("pe", lambda e, k=k, pm=pm, W=W: e.matmul(out=pm[0:m, :], lhsT=W[:, k, col0:col0 + m], rhs=xnT[:, k, :],
                                                                  start=(k == 0), stop=(k == 7)),
                             reads=[kT, wkey], writes=[kpm])
                    return pm, kpm

                def lowrank_norm(pms, nch, width, gc0):
                    for i, (pm, kpm) in enumerate(pms):
                        sq, ksq = sq_r.next()
                        p.op("act", lambda e, pm=pm, sq=sq: e.activation(out=sq[:], in_=pm[:], func=AF.Square), reads=[kpm], writes=[ksq])
                        p.op("pe", lambda e, sq=sq, i=i: e.matmul(out=pss[:], lhsT=ones[:], rhs=sq[:], start=(i == 0), stop=(i == nch - 1)),
                             reads=[ksq, "ones"], writes=["pssA"])
                    rstd_from(rbc[:], pss[:], width, ["pssA"], ["rbcA"])
                    cn, kcn = cn_r.next()
                    for i, (pm, kpm) in enumerate(pms):
                        p.op("dve", lambda e, pm=pm, i=i, cn=cn: e.scalar_tensor_tensor(out=cn[:, i, :], in0=pm[:], scalar=gcol[:, gc0 + i:gc0 + i + 1],
                                                                                    in1=rbc[:], op0=ALU.mult, op1=ALU.mult),
                             reads=[kpm, "gcol", "rbcA"], writes=[kcn])
                    return cn, kcn

                def store_fm(pm, kpm, rows, dst, eng="dve", scale=None):
                    ev, kev = ev_r.next()
                    if scale is None:
                        if eng == "act":
                            p.op("act", lambda e: e.copy(out=ev[0:rows, :], in_=pm[0:rows, :]), reads=[kpm], writes=[kev])
                        else:
                            p.op("dve", lambda e: e.tensor_copy(out=ev[0:rows, :], in_=pm[0:rows, :]), reads=[kpm], writes=[kev])
                    else:
                        p.op("act", lambda e: e.mul(out=ev[0:rows, :], in_=pm[0:rows, :], mul=scale), reads=[kpm], writes=[kev])
                    p.op("pool", lambda e: e.dma_start(out=dst, in_=ev[0:rows, :]), reads=[kev], writes=[], dma=kev + "s")

                def evac_to(pm, kpm, dst, kdst, eng="dve", scale=None):
                    if scale is not None:
                        p.op("act", lambda e: e.mul(out=dst, in_=pm[:], mul=scale), reads=[kpm], writes=[kdst])
                    elif eng == "act":
                        p.op("act", lambda e: e.copy(out=dst, in_=pm[:]), reads=[kpm], writes=[kdst])
                    else:
                        p.op("dve", lambda e: e.tensor_copy(out=dst, in_=pm[:]), reads=[kpm], writes=[kdst])

                KnT_v = KnT.rearrange("(c p) n -> p c n", p=128)
                KsT_v = KsT.rearrange("(c p) n -> p c n", p=128)
                QsT_v = QsT.rearrange("(c p) n -> p c n", p=128)
                VmD_v = VmD.rearrange("(s p) n -> p s n", p=128)
                VsD_v = VsD.rearrange("(s p) n -> p s n", p=128)
                QmT_v = QmT.rearrange("(h r) n -> r h n", r=96)

                srcs = [(xa, g_ * 512) for g_ in range(8)] + [(xo, g_ * 512) for g_ in range(4)]
                nxt = make_xnT(*srcs[0])
                for G in range(8):
                    c0 = G * 512
                    xnT, kT = nxt
                    nx_xnT, nx_kT, nx_tr = make_xnT(*srcs[G + 1], defer=True)
                    nxt = (nx_xnT, nx_kT)
                    ckv = [proj_fm(xnT, kT, 384 + m * 128, 128) for m in range(2)]
                    nx_tr(0)
                    cn, kcn = lowrank_norm(ckv, 2, 256, 3)
                    pa, kpa = proj_fm(xnT, kT, 0, 32, "Wkr", Wkr)
                    pb, kpb = proj_fm(xnT, kT, 32, 32, "Wkr", Wkr)
                    t1, kt1 = evf_r.next()
                    t2, kt2 = evf2_r.next()
                    p.op("dve", lambda e, pa=pa, t1=t1, c0=c0: e.tensor_tensor(out=t1[0:32, :], in0=pa[0:32, :], in1=Ck[:, c0:c0 + 512], op=ALU.mult),
                         reads=[kpa, "rkc"], writes=[kt1])
                    p.op("dve", lambda e, pb=pb, t2=t2, c0=c0: e.tensor_tensor(out=t2[0:32, :], in0=pb[0:32, :], in1=Sk[:, c0:c0 + 512], op=ALU.mult),
                         reads=[kpb, "rks"], writes=[kt2])
                    ev, kev = ev_r.next()
                    p.op("dve", lambda e, t1=t1, t2=t2, ev=ev: e.tensor_tensor(out=ev[0:32, :], in0=t1[0:32, :], in1=t2[0:32, :], op=ALU.add),
                         reads=[kt1, kt2], writes=[kev])
                    p.op("sp", lambda e, ev=ev, c0=c0: e.dma_start(out=KrT[:, c0:c0 + 512], in_=ev[0:32, :]), reads=[kev], writes=[], dma=kev + "s")
                    nx_tr(1)
                    st_, kst_ = stg_r.next()
                    for m in range(4):
                        pm, kpm = proj_fm(xnT, kT, 1184 + m * 128, 128)
                        evac_to(pm, kpm, st_[:, m, :], kst_, eng="act")
                    p.op("sp", lambda e: e.dma_start(out=KsT_v[:, :, c0:c0 + 512], in_=st_[:]), reads=[kst_], dma=kst_ + "s")
                    nx_tr(2)
                    st_, kst_ = stg_r.next()
                    for sub in range(4):
                        pm, kpm = pm_r.next()
                        for k in range(8):
                            p.op("pe", lambda e, k=k, pm=pm, sub=sub, xnT=xnT: e.matmul(out=pm[:], lhsT=xnT[:, k, sub * 128:(sub + 1) * 128],
                                                                                   rhs=Win[:, k, 1696:2208], start=(k == 0), stop=(k == 7)),
                                 reads=[kT, "Win"], writes=[kpm])
                        evac_to(pm, kpm, st_[:, sub, :], kst_, eng="dve")
                    p.op("sp", lambda e: e.dma_start(out=VsD_v[:, G * 4:(G + 1) * 4, :], in_=st_[:]), reads=[kst_], dma=kst_ + "s")

                    nx_tr(3)
                    st_, kst_ = stg_r.next()
                    for hp in range(4):
                        pm, kpm = pm_r.next()
                        for m in range(2):
                            p.op("pe", lambda e, m=m, pm=pm, hp=hp, cn=cn: e.matmul(out=pm[:], lhsT=Wkn[:, m, hp * 128:(hp + 1) * 128], rhs=cn[:, m, :],
                                                                               start=(m == 0), stop=(m == 1)),
                                 reads=[kcn, "Wkn"], writes=[kpm])
                        evac_to(pm, kpm, st_[:, hp, :], kst_, eng="act")
                    p.op("sp", lambda e: e.dma_start(out=KnT_v[:, :, c0:c0 + 512], in_=st_[:]), reads=[kst_], dma=kst_ + "s")
                    st_, kst_ = stg_r.next()
                    for sub in range(4):
                        pm, kpm = pm_r.next()
                        for m in range(2):
                            p.op("pe", lambda e, m=m, pm=pm, sub=sub, cn=cn: e.matmul(out=pm[:], lhsT=cn[:, m, sub * 128:(sub + 1) * 128], rhs=Wv[:, m, :],
                                                                                 start=(m == 0), stop=(m == 1)),
                                 reads=[kcn, "Wv"], writes=[kpm])
                        evac_to(pm, kpm, st_[:, sub, :], kst_, eng="dve")
                    p.op("sp", lambda e: e.dma_start(out=VmD_v[:, G * 4:(G + 1) * 4, :], in_=st_[:]), reads=[kst_], dma=kst_ + "s")
                for G in range(4):
                    c0 = G * 512
                    xnT, kT = nxt
                    if G + 1 < 4:
                        nxt = make_xnT(*srcs[8 + G + 1])
                    cq = [proj_fm(xnT, kT, m * 128, 128) for m in range(3)]
                    cn, kcn = lowrank_norm(cq, 3, 384, 0)
                    st8, kst8 = stg8_r.next()
                    for h in range(8):
                        pa, kpa = pm_r.next()
                        pb, kpb = pm_r.next()
                        for m in range(3):
                            p.op("pe", lambda e, m=m, pa=pa, h=h, cn=cn: e.matmul(out=pa[0:96, :], lhsT=Wuq[:, m, h * 96:(h + 1) * 96], rhs=cn[:, m, :],
                                                                             start=(m == 0), stop=(m == 2)),
                                 reads=[kcn, "Wuq"], writes=[kpa])
                        for m in range(3):
                            p.op("pe", lambda e, m=m, pb=pb, h=h, cn=cn: e.matmul(out=pb[0:96, :], lhsT=Wuqr[:, m, h * 96:(h + 1) * 96], rhs=cn[:, m, :],
                                                                             start=(m == 0), stop=(m == 2)),
                                 reads=[kcn, "Wuqr"], writes=[kpb])
                        t1, kt1 = evf_r.next()
                        t2, kt2 = evf2_r.next()
                        p.op("dve", lambda e, pa=pa, t1=t1, c0=c0: e.tensor_tensor(out=t1[0:96, :], in0=pa[0:96, :], in1=Cq[:, c0:c0 + 512], op=ALU.mult),
                             reads=[kpa, "rqc"], writes=[kt1])
                        p.op("dve", lambda e, pb=pb, t2=t2, c0=c0: e.tensor_tensor(out=t2[0:96, :], in0=pb[0:96, :], in1=Sq[:, c0:c0 + 512], op=ALU.mult),
                             reads=[kpb, "rqs"], writes=[kt2])
                        p.op("pool", lambda e, t1=t1, t2=t2: e.tensor_tensor(out=st8[0:96, h, :], in0=t1[0:96, :], in1=t2[0:96, :], op=ALU.add),
                             reads=[kt1, kt2], writes=[kst8])
                    p.op("sp", lambda e: e.dma_start(out=QmT_v[:, :, c0:c0 + 512], in_=st8[0:96, :, :]), reads=[kst8], dma=kst8 + "s")
                    st_, kst_ = stg_r.next()
                    for m in range(4):
                        pm, kpm = proj_fm(xnT, kT, 672 + m * 128, 128)
                        evac_to(pm, kpm, st_[:, m, :], kst_, scale=0.125)
                    p.op("sp", lambda e: e.dma_start(out=QsT_v[:, :, c0:c0 + 512], in_=st_[:]), reads=[kst_], dma=kst_ + "s")
                p.barrier()
                if stop_after == "A":
                    p.finish_wait("sp"); p.emit(top); return nc

            sbx = ExitStack()
            with sbx:
                sb, ps = mk_alloc(sbx)
                qrb = sb("qrb", [128, 512], F32)
                p.op("sp", lambda e: e.dma_start(out=qrb[:], in_=qrel.partition_broadcast(128)), writes=["qrb"], dma="qrb")
                kidx_i = sb("kidx_i", [128, 1], I32)
                krel = sb("krel", [128, 8], F32)
                p.op("pool", lambda e: e.iota(kidx_i[:], pattern=[[0, 1]], base=0, channel_multiplier=1), writes=["kidx_i"])
                p.op("dve", lambda e: e.tensor_copy(out=krel[:, 0:1], in_=kidx_i[:]), reads=["kidx_i"], writes=["krel"])
                for d in range(1, 8):
                    p.op("dve", lambda e, d=d: e.tensor_scalar(out=krel[:, d:d + 1], in0=krel[:, 0:1], scalar1=float(128 * d), scalar2=None, op0=ALU.add),
                         reads=["krel"], writes=["krel"])
                nmM = sb("nmM", [128, 8, 512], BF16)
                nmS = sb("nmS", [128, 8, 512], BF16)
                m01 = sb("m01", [128, 8, 512], BF16)
                for d in range(8):
                    p.op("dve", lambda e, d=d: e.tensor_scalar(out=nmM[:, d, :], in0=qrb[:], scalar1=krel[:, d:d + 1], scalar2=NEG, op0=ALU.is_lt, op1=ALU.mult),
                         reads=["qrb", "krel"], writes=["nmM"])
                    p.op("dve", lambda e, d=d: e.tensor_scalar(out=nmS[:, d, :], in0=qrb[:], scalar1=krel[:, d:d + 1], scalar2=NEG, op0=ALU.is_le, op1=ALU.mult),
                         reads=["qrb", "krel"], writes=["nmS"])
                    p.op("dve", lambda e, d=d: e.tensor_scalar(out=m01[:, d, :], in0=qrb[:], scalar1=krel[:, d:d + 1], scalar2=None, op0=ALU.is_gt),
                         reads=["qrb", "krel"], writes=["m01"])

                def blocks(t):
                    out = [(kb, 512, None) for kb in range(8 * t)]
                    out += [(8 * t + d, NDIAG[d], d) for d in range(8)]
                    return out

                sm = ExitStack()
                with sm:
                    def mla_gen():
                        sb, ps = mk_alloc(sm)
                        Vall = sb("Vall", [128, 32, 8, 65], BF16)
                        p.op("pool", lambda e: e.memset(Vall[:], 1.0), writes=["Vall%d" % i_ for i_ in range(8)])
                        vsrc = VmD.rearrange("(kb p) (h d) -> p kb h d", p=128, h=8)
                        for hh in range(8):
                            p.op("sp", lambda e: e.dma_start(out=Vall[:, :, hh, 0:64], in_=vsrc[:, :, hh, :]),
                                 reads=["Vall%d" % hh], writes=["Vall%d" % hh], dma="Vall%d" % hh)
                        KT_r = Ring(sb, "KTm", 2, [96, S], BF16)
                        QT_r = Ring(sb, "QTm", 2, [96, 2048], BF16)
                        pS_r = Ring(ps, "pSm", 2, [128, 512], F32)
                        pO_r = Ring(ps, "pOm", 1, [128, 512], F32)
                        pB_r = Ring(ps, "pBm", 1, [128, 512], F32)
                        P_r = Ring(sb, "Pm", 3, [128, 512], BF16)
                        Of_r = Ring(sb, "Ofm", 2, [65, 512], F32)
                        On_r = Ring(sb, "Onm", 2, [64, 512], BF16)
                        sel = sb("sel65", [65, 64], F32)
                        p.op("pool", lambda e: e.memset(sel[:], 0.0), writes=["sel65"])
                        p.op("pool", lambda e: e.memset(sel[64:65, :], 1.0), reads=["sel65"], writes=["sel65"])
                        for h in range(8):
                            KT, kKT = KT_r.next()
                            QT, kQT = QT_r.next()
                            p.op("sp", lambda e, KT=KT, h=h: e.dma_start(out=KT[0:64, :], in_=KnT[h * 64:(h + 1) * 64, :]), writes=[kKT], dma=kKT)
                            p.op("sp", lambda e, KT=KT: e.dma_start(out=KT[64:96, :], in_=KrT[:, :]), writes=[kKT], dma=kKT)
                            p.op("sp", lambda e, QT=QT, h=h: e.dma_start(out=QT[:], in_=QmT[h * 96:(h + 1) * 96, :]), writes=[kQT], dma=kQT)
                            for t in range(4):
                                bl = blocks(t)
                                pO, kpO = pO_r.next()
                                nb = len(bl)
                                stage = {}

                                def stA(i):
                                    kb, N, d = bl[i]
                                    pS, kpS = pS_r.next()
                                    p.op("pe", lambda e: e.matmul(out=pS[:, 0:N], lhsT=KT[:, kb * 128:(kb + 1) * 128], rhs=QT[:, t * 512:t * 512 + N],
                                                                  start=True, stop=(d is None)), reads=[kKT, kQT], writes=[kpS])
                                    if d is not None:
                                        p.op("pe", lambda e: e.matmul(out=pS[:, 0:N], lhsT=ident[:], rhs=nmM[:, d, 0:N], start=False, stop=True),
                                             reads=["ident", "nmM"], writes=[kpS])
                                    P, kP = P_r.next()
                                    p.op("act", lambda e: e.activation(out=P[:, 0:N], in_=pS[:, 0:N], func=AF.Exp), reads=[kpS], writes=[kP])
                                    stage[i] = (P, kP)

                                def stB(i):
                                    kb, N, d = bl[i]
                                    P, kP = stage.pop(i)
                                    p.op("pe", lambda e: e.matmul(out=pO[0:65, 0:N], lhsT=Vall[:, kb, h, :], rhs=P[:, 0:N], start=(i == 0), stop=(i == nb - 1)),
                                         reads=["Vall%d" % h, kP], writes=[kpO])

                                for it in range(nb + 2):
                                    if it < nb:
                                        stA(it)
                                    if it - 2 >= 0:
                                        stB(it - 2)
                                    yield
                                Of, kOf = Of_r.next()
                                p.op("dve", lambda e, Of=Of, pO=pO: e.tensor_copy(out=Of[:], in_=pO[0:65, :]), reads=[kpO], writes=[kOf])
                                p.op("dve", lambda e, Of=Of: e.reciprocal(out=Of[64:65, :], in_=Of[64:65, :]), reads=[kOf], writes=[kOf])
                                pB, kpB = pB_r.next()
                                p.op("pe", lambda e, Of=Of, pB=pB: e.matmul(out=pB[0:64, :], lhsT=sel[:], rhs=Of[:], start=True, stop=True),
                                     reads=[kOf, "sel65"], writes=[kpB])
                                On, kOn = On_r.next()
                                p.op("dve", lambda e, Of=Of, pB=pB, On=On: e.tensor_tensor(out=On[:], in0=Of[0:64, :], in1=pB[0:64, :], op=ALU.mult),
                                     reads=[kOf, kpB], writes=[kOn])
                                p.op("pool", lambda e, On=On, h=h, t=t: e.dma_start(out=OTd[h * 64:(h + 1) * 64, t * 512:(t + 1) * 512], in_=On[:]),
                                     reads=[kOn], writes=[], dma=kOn + "s")

                    def sb_gen():
                        sb, ps = mk_alloc(sm)
                        Vs = sb("Vsall", [128, 32, 512], BF16)
                        vsrc = VsD.rearrange("(kb p) n -> p kb n", p=128)
                        for q4 in range(4):
                            p.op("sp", lambda e, q4=q4: e.dma_start(out=Vs[:, q4 * 8:(q4 + 1) * 8, :], in_=vsrc[:, q4 * 8:(q4 + 1) * 8, :]),
                                 writes=["Vs%d" % q4], dma="Vs%d" % q4)
                        KT_r = Ring(sb, "KTs", 2, [64, S], BF16)
                        QT_r = Ring(sb, "QTs", 2, [64, 2048], BF16)
                        pZ_r = Ring(ps, "pZs", 2, [128, 512], F32)
                        pL_r = Ring(ps, "pLs", 1, [128, 512], F32)
                        pO_r = Ring(ps, "pOs", 1, [128, 512], F32)
                        E_r = Ring(sb, "Es", 3, [128, 512], F32)
                        SP_r = Ring(sb, "SPs", 4, [128, 512], BF16)
                        SM_r = Ring(sb, "SMs", 4, [128, 512], BF16)
                        A_r = Ring(sb, "As", 3, [128, 512], BF16)
                        CR_r = Ring(sb, "CRs", 3, [128, 512], BF16)
                        On_r = Ring(sb, "Ons", 2, [64, 512], BF16)
                        for h in range(8):
                            KT, kKT = KT_r.next()
                            QT, kQT = QT_r.next()
                            p.op("sp", lambda e, KT=KT, h=h: e.dma_start(out=KT[:], in_=KsT[h * 64:(h + 1) * 64, :]), writes=[kKT], dma=kKT)
                            p.op("sp", lambda e, QT=QT, h=h: e.dma_start(out=QT[:], in_=QsT[h * 64:(h + 1) * 64, :]), writes=[kQT], dma=kQT)
                            for t in range(4):
                                bl = blocks(t)[::-1]
                                nb = len(bl)
                                pO, kpO = pO_r.next()
                                p.op("pe", lambda e, pO=pO: e.matmul(out=pO[0:64, :], lhsT=zeros[:, 0:64], rhs=m01[:, 0, :],
                                                                      start=True, stop=False), reads=["zeros", "m01"], writes=[kpO])
                                stage = {}
                                carry = {"t": None, "k": None}

                                def stA(i):
                                    kb, N, d = bl[i]
                                    pZ, kpZ = pZ_r.next()
                                    p.op("pe", lambda e: e.matmul(out=pZ[:, 0:N], lhsT=KT[:, kb * 128:(kb + 1) * 128], rhs=QT[:, t * 512:t * 512 + N],
                                                                  start=True, stop=True), reads=[kKT, kQT], writes=[kpZ])
                                    E, kE = E_r.next()
                                    p.op("act", lambda e: e.activation(out=E[:, 0:N], in_=pZ[:, 0:N], func=AF.Exp), reads=[kpZ], writes=[kE])
                                    SPt, kSP = SP_r.next()
                                    p.op("act", lambda e: e.activation(out=SPt[:, 0:N], in_=E[:, 0:N], func=AF.Ln, bias=1.0), reads=[kE], writes=[kSP])
                                    if d is not None:
                                        SM, kSM = SM_r.next()
                                        p.op("dve", lambda e: e.tensor_tensor(out=SM[:, 0:N], in0=SPt[:, 0:N], in1=m01[:, d, 0:N], op=ALU.mult),
                                             reads=[kSP, "m01"], writes=[kSM])
                                    else:
                                        SM, kSM = SPt, kSP
                                    cprev, kcprev = carry["t"], carry["k"]
                                    stage[i] = (SM, kSM, cprev, kcprev, E, kE)
                                    if i < nb - 1:
                                        cn_, kcn_ = CR_r.next()
                                        if cprev is None:
                                            if N < 512:
                                                p.op("pool", lambda e: e.memset(cn_[:, N:512], 0.0), writes=[kcn_])
                                            p.op("pool", lambda e: e.tensor_copy(out=cn_[:, 0:N], in_=SM[:, 0:N]), reads=[kSM], writes=[kcn_])
                                        else:
                                            if N < 512:
                                                p.op("pool", lambda e: e.tensor_copy(out=cn_[:, N:512], in_=cprev[:, N:512]), reads=[kcprev], writes=[kcn_])
                                            p.op("dve", lambda e: e.tensor_tensor(out=cn_[:, 0:N], in0=cprev[:, 0:N], in1=SM[:, 0:N], op=ALU.add),
                                                 reads=[kcprev, kSM], writes=[kcn_])
                                        carry["t"], carry["k"] = cn_, kcn_

                                def stB(i):
                                    kb, N, d = bl[i]
                                    SM, kSM, cprev, kcprev, E, kE = stage[i]
                                    pL, kpL = pL_r.next()
                                    last = "tri"
                                    if cprev is not None:
                                        last = "carry"
                                    if d is not None:
                                        last = "mask"
                                    p.op("pe", lambda e: e.matmul(out=pL[:, 0:N], lhsT=ntri[:], rhs=SM[:, 0:N], start=True, stop=(last == "tri")),
                                         reads=["ntri", kSM], writes=[kpL])
                                    if cprev is not None:
                                        p.op("pe", lambda e: e.matmul(out=pL[:, 0:N], lhsT=nones[:], rhs=cprev[:, 0:N], start=False, stop=(last == "carry")),
                                             reads=["nones", kcprev], writes=[kpL])
                                    if d is not None:
                                        p.op("pe", lambda e: e.matmul(out=pL[:, 0:N], lhsT=ident[:], rhs=nmS[:, d, 0:N], start=False, stop=True),
                                             reads=["ident", "nmS"], writes=[kpL])
                                    A, kA = A_r.next()
                                    p.op("act", lambda e: e.activation(out=A[:, 0:N], in_=pL[:, 0:N], func=AF.Exp), reads=[kpL], writes=[kA])
                                    p.op("dve", lambda e: e.tensor_tensor(out=A[:, 0:N], in0=A[:, 0:N], in1=E[:, 0:N], op=ALU.mult), reads=[kA, kE], writes=[kA])
                                    stage[i] = (A, kA)

                                def stC(i):
                                    kb, N, d = bl[i]
                                    A, kA = stage.pop(i)
                                    p.op("pe", lambda e: e.matmul(out=pO[0:64, 0:N], lhsT=Vs[:, kb, h * 64:(h + 1) * 64], rhs=A[:, 0:N], start=False, stop=(i == nb - 1)),
                                         reads=["Vs%d" % (kb // 8), kA], writes=[kpO])

                                for it in range(nb + 2):
                                    if it < nb:
                                        stA(it)
                                    if 0 <= it - 1 < nb:
                                        stB(it - 1)
                                    if it - 2 >= 0:
                                        stC(it - 2)
                                    yield
                                On, kOn = On_r.next()
                                p.op("dve", lambda e, On=On, pO=pO: e.tensor_copy(out=On[:], in_=pO[0:64, :]), reads=[kpO], writes=[kOn])
                                p.op("pool", lambda e, On=On, h=h, t=t: e.dma_start(out=OTd[512 + h * 64:512 + (h + 1) * 64, t * 512:(t + 1) * 512], in_=On[:]),
                                     reads=[kOn], writes=[], dma=kOn + "s")

                    gens = [mla_gen(), sb_gen()]
                    while gens:
                        for g_ in list(gens):
                            try:
                                next(g_)
                            except StopIteration:
                                gens.remove(g_)
                    p.barrier()
                    if stop_after == "B":
                        p.finish_wait("sp"); p.emit(top); return nc
            sc = ExitStack()
            with sc:
                sb, ps = mk_alloc(sc)
                H = sb("H", [128, 16, D], F32)
                posm = sb("posm", [128, 16, NE], F32)
                maskb = sb("maskb", [128, 16, NE], BF16)
                gj = sb("gj", [128, 16, 4], F32)
                slI = sb("slI", [128, 16, 4], I32)
                Gt = sb("Gt", [128, 16, NE], F32)
                bgT = sb("bgT", [128, 512], F32)
                s1 = ExitStack()
                with s1:
                    sb1, ps1 = mk_alloc(s1)
                    gbc = sb1("gbc", [128, D], F32)
                    junk = sb1("junkC", [128, D], BF16)
                    Wo = sb1("Wo", [128, 8, D], BF16)
                    p.op("pool", lambda e: e.dma_start(out=Wo[:], in_=w_o.rearrange("(c p) n -> p c n", p=128)), writes=["Wo"], dma="Wo")
                    Wr = sb1("Wr", [128, 8, NE], F32)
                    p.op("sp", lambda e: e.dma_start(out=Wr[:], in_=w_router.rearrange("(c p) n -> p c n", p=128)), writes=["Wr"], dma="Wr")
                    brb = sb1("brb", [128, NE], F32)
                    p.op("sp", lambda e: e.dma_start(out=brb[:], in_=b_router.partition_broadcast(128)), writes=["brb"], dma="brb")
                    p.op("sp", lambda e: e.dma_start(out=gbc[:], in_=g_moe.partition_broadcast(128)), writes=["gbc"], dma="gbc")
                    bgl = sb1("bgl", [128, 4, 128], F32)
                    p.op("sp", lambda e: e.dma_start(out=bgl[:], in_=b_gu.rearrange("(a r) q -> r a q", r=128)), writes=["bgl"], dma="bgl")
                    pTf_r = Ring(ps1, "pTf", 1, [128, 4, 128], F32)
                    pbt, kpbt = pTf_r.next()
                    for a in range(4):
                        p.op("pe", lambda e, a=a: e.transpose(out=pbt[:, a, :], in_=bgl[:, a, :], identity=identf[:]), reads=["bgl", "identf"], writes=[kpbt])
                    p.op("dve", lambda e: e.tensor_copy(out=bgT[:].rearrange("p (a q) -> p a q", a=4), in_=pbt[:]), reads=[kpbt], writes=["bgT"])

                    OT_r = Ring(sb1, "OTl", 1, [128, 8, 512], BF16)
                    sq_r = Ring(sb1, "sqC", 2, [128, 512], BF16)
                    pss_r = Ring(ps1, "pssC", 1, [128, 512], F32)
                    rb_r = Ring(sb1, "rbC", 2, [128, 512], F32)
                    MX = sb1("MX", [128, 8, 512], BF16)
                    pA_r = Ring(ps1, "pAC", 2, [128, 512], F32)
                    xr_r = Ring(sb1, "xrC", 2, [128, D], F32)
                    ssc_r = Ring(sb1, "sscC", 2, [128, 1], F32)
                    u32_r = Ring(sb1, "u32C", 2, [128, D], F32)
                    uhT_r = Ring(sb1, "uhT", 2, [128, 8, 128], BF16)
                    tris = sb1("tris", [128, 128], BF16)
                    tmpf1 = sb1("tmpf1", [128, 128], F32)
                    p.op("pool", lambda e: e.memset(tmpf1[:], 1.0), writes=["tmpf1"])
                    p.op("pool", lambda e: e.affine_select(out=tmpf1[:], in_=tmpf1[:], pattern=[[1, 128]], compare_op=ALU.is_gt, fill=0.0,
                                                            base=0, channel_multiplier=-1), reads=["tmpf1"], writes=["tmpf1"])
                    p.op("dve", lambda e: e.tensor_copy(out=tris[:], in_=tmpf1[:]), reads=["tmpf1"], writes=["tris"])
                    gtmp_r = Ring(sb1, "gtmpC", 2, [128, NE], F32)
                    xnb_r = Ring(sb1, "xnbC", 2, [128, D], BF16)
                    ecap_i = sb1("ecap_i", [128, NE], I32)
                    ecap = sb1("ecap", [128, NE], F32)
                    p.op("pool", lambda e: e.iota(ecap_i[:], pattern=[[CAP, NE]], base=0, channel_multiplier=0), writes=["ecap_i"])
                    p.op("dve", lambda e: e.tensor_copy(out=ecap[:], in_=ecap_i[:]), reads=["ecap_i"], writes=["ecap"])
                    pq_r = Ring(sb1, "pq", 2, [128, NE], F32)
                    oh4_r = Ring(sb1, "oh4", 2, [128, 4, NE], F32)
                    pr4_r = Ring(sb1, "pr4", 2, [128, 4, NE], F32)
                    oh = sb1("oh", [128, NE], F32)
                    pr = sb1("pr", [128, NE], F32)
                    slf_r = Ring(sb1, "slf", 2, [128, 4], F32)
                    ulo_r = Ring(sb1, "uloC", 2, [128, D], BF16)
                    uloT_r = Ring(sb1, "uloT", 2, [128, 8, 128], BF16)
                    pTb_r = Ring(ps1, "pTb", 2, [128, 8, 128], BF16)
                    Wrh = sb1("Wrh", [128, 8, NE], BF16)
                    Wrl = sb1("Wrl", [128, 8, NE], BF16)
                    p.op("dve", lambda e: e.tensor_copy(out=Wrh[:], in_=Wr[:]), reads=["Wr"], writes=["Wrh"])
                    p.op("dve", lambda e: e.tensor_tensor(out=Wrl[:], in0=Wr[:], in1=Wrh[:], op=ALU.subtract), reads=["Wr", "Wrh"], writes=["Wrl"])
                    pLg_r = Ring(ps1, "pLg", 2, [128, 2, NE], F32)
                    lg_r = Ring(sb1, "lgC", 2, [128, NE], F32)
                    mx8_r = Ring(sb1, "mx8C", 2, [128, 8], F32)
                    msk_r = Ring(sb1, "mskC", 2, [128, NE], F32)
                    ex_r = Ring(sb1, "exC", 2, [128, NE], F32)
                    sm_r = Ring(sb1, "smC", 2, [128, 1], F32)
                    otv = OTd.rearrange("(c p) n -> p c n", p=128)
                    if stop_after == "C1a":
                        p.barrier()
                        p.op("sp", lambda e: e.dma_start(out=dbgH.rearrange("(t p) n -> p t n", p=128), in_=H[:]), reads=["H%d" % i_ for i_ in range(16)], dma="dbgH")
                        p.op("sp", lambda e: e.dma_start(out=dbgG.rearrange("(t p) n -> p t n", p=128), in_=Gt[:]), reads=["Gt%d" % i_ for i_ in range(16)], dma="dbgG")
                        p.finish_wait("sp"); p.emit(top); return nc
                    for G in range(4):
                        OT, kOT = OT_r.next()
                        p.op("sp", lambda e, OT=OT, G=G: e.dma_start(out=OT[:], in_=otv[:, :, G * 512:(G + 1) * 512]), writes=[kOT], dma=kOT)
                        rbs = []
                        for grp in range(2):
                            pss, kpss = pss_r.next()
                            for i in range(4):
                                c = grp * 4 + i
                                sq, ksq = sq_r.next()
                                p.op("act", lambda e, sq=sq, OT=OT, c=c: e.activation(out=sq[:], in_=OT[:, c, :], func=AF.Square), reads=[kOT], writes=[ksq])
                                p.op("pe", lambda e, sq=sq, pss=pss, i=i: e.matmul(out=pss[:], lhsT=ones[:], rhs=sq[:], start=(i == 0), stop=(i == 3)),
                                     reads=[ksq, "ones"], writes=[kpss])
                            rb, krb = rb_r.next()
                            rstd_from(rb[:], pss[:], 512, [kpss], [krb])
                            rbs.append((rb, krb))
                        for c in range(8):
                            rb, krb = rbs[c // 4]
                            p.op("dve", lambda e, c=c, rb=rb, OT=OT: e.scalar_tensor_tensor(out=MX[:, c, :], in0=OT[:, c, :], scalar=gcol[:, 5 + c:6 + c], in1=rb[:],
                                                                                        op0=ALU.mult, op1=ALU.mult),
                                 reads=[kOT, "gcol", krb], writes=["MX"])
                        def c1_tile(G, sub):
                            tile = G * 4 + sub
                            b_ = sub % 2
                            uhT, kuhT = uhT_r.tiles[b_], uhT_r.keys[b_]
                            uloT, kuloT = uloT_r.tiles[b_], uloT_r.keys[b_]
                            pq, kpq = pq_r.tiles[b_], pq_r.keys[b_]
                            slf, kslf = slf_r.tiles[b_], slf_r.keys[b_]
                            pLg2, kpLg = pLg_r.tiles[b_], pLg_r.keys[b_]
                            pLg = pLg2[:, 0, :]
                            pPos = pLg2[:, 1, :]
                            yield
                            xr, kxr = xr_r.next()
                            yield
                            p.op("sp", lambda e, xr=xr, tile=tile: e.dma_start(out=xr[:], in_=xo[tile * 128:(tile + 1) * 128, :]), writes=[kxr], dma=kxr)
                            yield
                            for half in range(2):
                                pA, kpA = pA_r.next()
                                for c in range(8):
                                    p.op("pe", lambda e, c=c, pA=pA, sub=sub, half=half: e.matmul(out=pA[:], lhsT=MX[:, c, sub * 128:(sub + 1) * 128],
                                                                                              rhs=Wo[:, c, half * 512:(half + 1) * 512], start=(c == 0), stop=(c == 7)),
                                         reads=["MX", "Wo"], writes=[kpA])
                                p.op("dve", lambda e, pA=pA, xr=xr, tile=tile, half=half: e.tensor_tensor(out=H[:, tile, half * 512:(half + 1) * 512], in0=pA[:],
                                                                                                      in1=xr[:, half * 512:(half + 1) * 512], op=ALU.add),
                                     reads=[kpA, kxr], writes=["H%d" % tile])
                            yield
                            yield
                            ssc, kss = ssc_r.next()
                            yield
                            p.op("act", lambda e, ssc=ssc, tile=tile: e.activation(out=junk[:], in_=H[:, tile, :], func=AF.Square, accum_out=ssc[:]),
                                 reads=["H%d" % tile], writes=["junkC", kss])
                            yield
                            rstd_from(ssc[:], ssc[:], D, [kss], [kss])
                            yield
                            u32, ku = u32_r.next()
                            yield
                            p.op("dve", lambda e, ssc=ssc, u32=u32, tile=tile: e.scalar_tensor_tensor(out=u32[:], in0=H[:, tile, :], scalar=ssc[:, 0:1], in1=gbc[:],
                                                                                                  op0=ALU.mult, op1=ALU.mult),
                                 reads=["H%d" % tile, kss, "gbc"], writes=[ku])
                            yield
                            xnb, kuhi = xnb_r.next()
                            yield
                            ulo, kulo = ulo_r.next()
                            yield
                            p.op("act", lambda e: e.copy(out=xnb[:], in_=u32[:]), reads=[ku], writes=[kuhi])
                            yield
                            p.op("pool", lambda e: e.dma_start(out=Ud[tile * 128:(tile + 1) * 128, :], in_=xnb[:]), reads=[kuhi], dma=kuhi + "s")
                            yield
                            p.op("dve", lambda e: e.tensor_tensor(out=ulo[:], in0=u32[:], in1=xnb[:], op=ALU.subtract), reads=[ku, kuhi], writes=[kulo])
                            yield
                            pT, kpT = pTb_r.next()
                            yield
                            for c in range(8):
                                p.op("pe", lambda e: e.transpose(out=pT[:, c, :], in_=xnb[:, c * 128:(c + 1) * 128], identity=ident[:]), reads=[kuhi, "ident"], writes=[kpT])
                            yield
                            p.op("dve", lambda e: e.tensor_copy(out=uhT[:], in_=pT[:]), reads=[kpT], writes=[kuhT])
                            yield
                            pT2, kpT2 = pTb_r.next()
                            yield
                            for c in range(8):
                                p.op("pe", lambda e: e.transpose(out=pT2[:, c, :], in_=ulo[:, c * 128:(c + 1) * 128], identity=ident[:]), reads=[kulo, "ident"], writes=[kpT2])
                            yield
                            p.op("act", lambda e: e.copy(out=uloT[:], in_=pT2[:]), reads=[kpT2], writes=[kuloT])
                            yield
                            n_ = 0
                            yield
                            for (A_, kA_, W_, kW_) in (("hi", kuhT, Wrh, "Wrh"), ("lo", kuloT, Wrh, "Wrh"), ("hi", kuhT, Wrl, "Wrl")):
                                for k in range(8):
                                    lh = uhT[:, k, :] if A_ == "hi" else uloT[:, k, :]
                                    p.op("pe", lambda e: e.matmul(out=pLg, lhsT=lh, rhs=W_[:, k, :], start=(n_ == 0), stop=(n_ == 23)),
                                         reads=[kA_, kW_], writes=[kpLg])
                                    n_ += 1
                            yield
                            lg, klg = lg_r.next()
                            yield
                            p.op("dve", lambda e, lg=lg: e.tensor_tensor(out=lg[:], in0=pLg, in1=brb[:], op=ALU.add), reads=[kpLg, "brb"], writes=[klg])
                            yield
                            mx8, kmx = mx8_r.next()
                            yield
                            p.op("dve", lambda e, lg=lg, mx8=mx8: e.max(out=mx8[:], in_=lg[:]), reads=[klg], writes=[kmx])
                            yield
                            msk, kmsk = msk_r.next()
                            yield
                            p.op("dve", lambda e, lg=lg, mx8=mx8, msk=msk: e.tensor_scalar(out=msk[:], in0=lg[:], scalar1=mx8[:, 3:4], scalar2=None, op0=ALU.is_ge),
                                 reads=[klg, kmx], writes=[kmsk])
                            yield
                            p.op("dve", lambda e, mx8=mx8: e.tensor_scalar(out=mx8[:, 7:8], in0=mx8[:, 0:1], scalar1=-1.0, scalar2=None, op0=ALU.mult),
                                 reads=[kmx, kmsk], writes=[kmx])
                            yield
                            ex, kex = ex_r.next()
                            yield
                            p.op("act", lambda e, lg=lg, mx8=mx8, ex=ex: e.activation(out=ex[:], in_=lg[:], func=AF.Exp, bias=mx8[:, 7:8]), reads=[klg, kmx], writes=[kex])
                            yield
                            sm_, ksm = sm_r.next()
                            yield
                            p.op("dve", lambda e, ex=ex, msk=msk: e.tensor_tensor(out=ex[:], in0=ex[:], in1=msk[:], op=ALU.mult), reads=[kex, kmsk], writes=[kex])
                            yield
                            p.op("dve", lambda e, ex=ex, sm_=sm_: e.reduce_sum(out=sm_[:], in_=ex[:], axis=mybir.AxisListType.X), reads=[kex], writes=[ksm])
                            yield
                            p.op("dve", lambda e, sm_=sm_: e.reciprocal(out=sm_[:], in_=sm_[:]), reads=[ksm], writes=[ksm])
                            yield
                            p.op("dve", lambda e, ex=ex, sm_=sm_, tile=tile: e.tensor_scalar(out=Gt[:, tile, :], in0=ex[:], scalar1=sm_[:, 0:1], scalar2=None, op0=ALU.mult),
                                 reads=[kex, ksm], writes=["Gt%d" % tile])
                            yield
                            p.op("dve", lambda e: e.tensor_copy(out=maskb[:, tile, :], in_=msk[:]), reads=[kmsk], writes=["maskb%d" % tile])
                            yield
                            gtmp, kgtmp = gtmp_r.next()
                            yield
                            p.op("pe", lambda e: e.matmul(out=pPos, lhsT=tris[:], rhs=maskb[:, tile, :], start=True, stop=(tile == 0)),
                                 reads=["tris", "maskb%d" % tile], writes=[kpLg])
                            yield
                            for j_ in range(tile):
                                p.op("pe", lambda e: e.matmul(out=pPos, lhsT=ones[:], rhs=maskb[:, j_, :], start=False, stop=(j_ == tile - 1)),
                                     reads=["ones", "maskb%d" % j_], writes=[kpLg])
                            yield
                            p.op("dve", lambda e: e.scalar_tensor_tensor(out=gtmp[:], in0=pPos, scalar=1.0, in1=msk[:], op0=ALU.add, op1=ALU.mult),
                                 reads=[kpLg, kmsk, kgtmp], writes=[kgtmp])
                            yield
                            p.op("dve", lambda e: e.tensor_scalar(out=posm[:, tile, :], in0=gtmp[:], scalar1=-1.0, scalar2=None, op0=ALU.add),
                                 reads=[kgtmp], writes=["posm%d" % tile])
                            yield
                            p.op("dve", lambda e: e.scalar_tensor_tensor(out=pq[:], in0=pPos, scalar=float(CAP - 1), in1=ecap[:], op0=ALU.min, op1=ALU.add),
                                 reads=[kpLg, "ecap"], writes=[kpq])
                            yield
                            yield
                            p.op("dve", lambda e: e.scalar_tensor_tensor(out=gtmp[:], in0=pPos, scalar=float(CAP) - 0.5, in1=Gt[:, tile, :], op0=ALU.is_lt, op1=ALU.mult),
                                 reads=[kpLg, "Gt%d" % tile, kgtmp], writes=[kgtmp])
                            yield
                            oh4, koh4 = oh4_r.tiles[b_], oh4_r.keys[b_]
                            pr4, kpr4 = pr4_r.tiles[b_], pr4_r.keys[b_]
                            lg_b = lg[:].unsqueeze(1).to_broadcast([128, 4, NE])
                            mx_b = mx8[:, 0:4].unsqueeze(2).to_broadcast([128, 4, NE])
                            p.op("dve", lambda e: e.tensor_tensor(out=oh4[:], in0=lg_b, in1=mx_b, op=ALU.is_equal), reads=[klg, kmx], writes=[koh4])
                            yield
                            p.op("dve", lambda e: e.tensor_tensor(out=pr4[:], in0=oh4[:], in1=pq[:].unsqueeze(1).to_broadcast([128, 4, NE]), op=ALU.mult),
                                 reads=[koh4, kpq], writes=[kpr4])
                            yield
                            p.op("dve", lambda e: e.reduce_sum(out=slf[:, 0:4], in_=pr4[:], axis=mybir.AxisListType.X), reads=[kpr4], writes=[kslf])
                            yield
                            p.op("dve", lambda e: e.tensor_tensor(out=pr4[:], in0=oh4[:], in1=gtmp[:].unsqueeze(1).to_broadcast([128, 4, NE]), op=ALU.mult),
                                 reads=[koh4, kgtmp, kpr4], writes=[kpr4])
                            yield
                            p.op("dve", lambda e: e.reduce_sum(out=gj[:, tile, :], in_=pr4[:], axis=mybir.AxisListType.X), reads=[kpr4], writes=["gj%d" % tile])
                            yield
                            p.op("dve", lambda e: e.tensor_copy(out=slI[:, tile, :], in_=slf[:]), reads=[kslf], writes=["slI%d" % tile])

                        for pair in range(2):
                            gens = [c1_tile(G, pair * 2), c1_tile(G, pair * 2 + 1)]
                            while gens:
                                for g_ in list(gens):
                                    try:
                                        next(g_)
                                    except StopIteration:
                                        gens.remove(g_)
                    p.barrier()
                    if stop_after == "C1":
                        if debug:
                            p.op("sp", lambda e: e.dma_start(out=dbgS.rearrange("(t p) n -> p t n", p=128), in_=slI[:]), reads=["slI%d" % i_ for i_ in range(16)], dma="dbgS")
                            p.op("sp", lambda e: e.dma_start(out=dbgJ.rearrange("(t p) n -> p t n", p=128), in_=gj[:]), reads=["gj%d" % i_ for i_ in range(16)], dma="dbgJ")
                            p.op("sp", lambda e: e.dma_start(out=dbgH.rearrange("(t p) n -> p t n", p=128), in_=H[:]), reads=["H%d" % i_ for i_ in range(16)], dma="dbgH")
                            p.op("sp", lambda e: e.dma_start(out=dbgG.rearrange("(t p) n -> p t n", p=128), in_=Gt[:]), reads=["Gt%d" % i_ for i_ in range(16)], dma="dbgG")
                        p.finish_wait("sp"); p.emit(top); return nc
                s2 = ExitStack()
                with s2:
                    sb2, ps2 = mk_alloc(s2)
                    W_r = Ring(sb2, "Wx", 6, [128, 8, 512], BF16)
                    bd_r = Ring(sb2, "bdn", 2, [1, D], BF16)
                    pGL_r = Ring(ps2, "pGL", 4, [128, 512], F32)
                    pA_r = Ring(ps2, "pA", 2, [128, 512], F32)
                    pTs = ps2("pTs", [128, 8, 128], BF16)
                    ptk = ps2("ptk", [128, 8], F32)
                    gl_r = Ring(sb2, "gl", 1, [128, CAP], F32)
                    sg_r = Ring(sb2, "sg", 1, [128, CAP], F32)
                    Sel = sb2("Sel", [128, 16, CAP], BF16)
                    Xe_r = Ring(sb2, "Xe", 2, [128, NS, D], BF16)
                    XeT = sb2("XeT", [128, 8, CAP], BF16)
                    aT = sb2("aTs", [128, 8, CAP], BF16)
                    Yst_r = Ring(sb2, "Yst", 2, [128, D], F32)
                    tks = sb2("tks", [128, 8], F32)
                    tkf = sb2("tkf", [128, 4], F32)
                    tkI_r = Ring(sb2, "tkI", 2, [128, 4], I32)
                    iota_i = sb2("iota_i", [128, CAP], I32)
                    iota_f = sb2("iota_f", [128, CAP], F32)
                    p.op("pool", lambda e: e.iota(iota_i[:], pattern=[[1, CAP]], base=0, channel_multiplier=0), writes=["iota_i"])
                    p.op("dve", lambda e: e.tensor_copy(out=iota_f[:], in_=iota_i[:]), reads=["iota_i"], writes=["iota_f"])
                    tid = sb2("tid", [128, 16], I32)
                    tidx = sb2("tidx", [128, 16], I32)
                    tidhl = sb2("tidhl", [128, 16, 2], BF16)
                    p.op("pool", lambda e: e.iota(tid[:], pattern=[[128, 16]], base=0, channel_multiplier=1), writes=["tid"])
                    p.op("dve", lambda e: e.tensor_scalar(out=tidx[:], in0=tid[:], scalar1=6, scalar2=None, op0=ALU.arith_shift_right), reads=["tid"], writes=["tidx"])
                    p.op("dve", lambda e: e.tensor_copy(out=tidhl[:, :, 0], in_=tidx[:]), reads=["tidx"], writes=["tidhl"])
                    p.op("dve", lambda e: e.tensor_scalar(out=tidx[:], in0=tid[:], scalar1=63, scalar2=None, op0=ALU.bitwise_and), reads=["tid", "tidx", "tidhl"], writes=["tidx"])
                    p.op("dve", lambda e: e.tensor_copy(out=tidhl[:, :, 1], in_=tidx[:]), reads=["tidx", "tidhl"], writes=["tidhl"])
                    bg3 = bgT[:].rearrange("p (e c) -> p e c", c=16)
                    p.op("dve", lambda e: e.tensor_scalar(out=bg3[:, :, 8:16], in0=bg3[:, :, 8:16], scalar1=1.0, scalar2=None, op0=ALU.add),
                         reads=["bgT"], writes=["bgT"])

                    def load_w(src, slot):
                        Wt, kW = W_r.tiles[slot], W_r.keys[slot]
                        p.op("pool", lambda e: e.dma_start(out=Wt[:], in_=src.rearrange("(c p) n -> p c n", p=128)), writes=[kW], dma=kW)
                        return Wt, kW

                    def load_bd(ex_):
                        bd, kbd = bd_r.next()
                        p.op("pool", lambda e: e.dma_start(out=bd[:], in_=b_dn[ex_:ex_ + 1, :]), writes=[kbd], dma=kbd)
                        return bd, kbd

                    xe_of = {}

                    def sel_build(ex_, tiles):
                        for tile in tiles:
                            p.op("dve", lambda e: e.tensor_scalar(out=Sel[:, tile, :], in0=iota_f[:], scalar1=posm[:, tile, ex_:ex_ + 1], scalar2=None, op0=ALU.is_equal),
                                 reads=["iota_f", "posm%d" % tile], writes=["Sel%d" % tile])

                    def dispatch(ex_, build=True):
                        if build:
                            sel_build(ex_, range(16))
                        for s_ in range(NS):
                            for tile in range(16):
                                p.op("pe", lambda e: e.matmul(out=ptk[:, 2 * s_:2 * s_ + 2], lhsT=Sel[:, tile, s_ * 128:(s_ + 1) * 128], rhs=tidhl[:, tile, :],
                                                              start=(tile == 0), stop=(tile == 15)), reads=["Sel%d" % tile, "tidhl"], writes=["ptk"])
                        p.op("dve", lambda e: e.tensor_copy(out=tks[:, 0:2 * NS], in_=ptk[:, 0:2 * NS]), reads=["ptk"], writes=["tks"])
                        tk3 = tks[:, 0:2 * NS].rearrange("p (s t) -> p s t", t=2)
                        p.op("dve", lambda e: e.scalar_tensor_tensor(out=tkf[:, 0:NS], in0=tk3[:, :, 0], scalar=64.0, in1=tk3[:, :, 1], op0=ALU.mult, op1=ALU.add),
                             reads=["tks"], writes=["tkf"])
                        tkI, ktkI = tkI_r.next()
                        p.op("dve", lambda e: e.tensor_copy(out=tkI[:, 0:NS], in_=tkf[:, 0:NS]), reads=["tkf"], writes=[ktkI])
                        Xe, kXe = Xe_r.next()
                        for s_ in range(NS):
                            p.op("pool", lambda e: e.indirect_dma_start(out=Xe[:, s_, :], out_offset=None, in_=Ud[:, :],
                                                                         in_offset=bass.IndirectOffsetOnAxis(ap=tkI[:, s_:s_ + 1], axis=0)),
                                 reads=[ktkI], writes=[kXe], dma=kXe)
                        xe_of[ex_] = (Xe, kXe)

                    def transposes_s(ex_, s_):
                        Xe, kXe = xe_of[ex_]
                        for k in range(8):
                            p.op("pe", lambda e: e.transpose(out=pTs[:, k, :], in_=Xe[:, s_, k * 128:(k + 1) * 128], identity=ident[:]),
                                 reads=[kXe, "ident"], writes=["pTs"])
                        if s_ % 2 == 0:
                            p.op("act", lambda e: e.copy(out=XeT[:, :, s_ * 128:(s_ + 1) * 128], in_=pTs[:]), reads=["pTs"], writes=["XeT"])
                        else:
                            p.op("dve", lambda e: e.tensor_copy(out=XeT[:, :, s_ * 128:(s_ + 1) * 128], in_=pTs[:]), reads=["pTs"], writes=["XeT"])
                        if s_ == NS - 1:
                            xe_of.pop(ex_)

                    def transposes(ex_):
                        for s_ in range(NS):
                            transposes_s(ex_, s_)

                    def gu_stage(ex_, st, Wg, kWg, Wl, kWl, sel_for=None):
                        for mc in range(4):
                            if sel_for is not None:
                                sel_build(sel_for, range(mc * 4, mc * 4 + 4))
                            c = st * 4 + mc
                            pG, kpG = pGL_r.next()
                            pLn, kpLn = pGL_r.next()
                            for k in range(8):
                                p.op("pe", lambda e: e.matmul(out=pG[:, 0:CAP], lhsT=Wg[:, k, mc * 128:(mc + 1) * 128], rhs=XeT[:, k, :],
                                                              start=(k == 0), stop=(k == 7)), reads=[kWg, "XeT"], writes=[kpG])
                            for k in range(8):
                                p.op("pe", lambda e: e.matmul(out=pLn[:, 0:CAP], lhsT=Wl[:, k, mc * 128:(mc + 1) * 128], rhs=XeT[:, k, :],
                                                              start=(k == 0), stop=(k == 7)), reads=[kWl, "XeT"], writes=[kpLn])
                            gl, kgl = gl_r.next()
                            sg, ksg = sg_r.next()
                            bgc = ex_ * 16 + c
                            blc = ex_ * 16 + 8 + c
                            kaT = "aT_%d" % st
                            p.op("dve", lambda e: e.tensor_scalar(out=gl[:], in0=pG[:, 0:CAP], scalar1=bgT[:, bgc:bgc + 1], scalar2=7.0, op0=ALU.add, op1=ALU.min),
                                 reads=[kpG, "bgT"], writes=[kgl])
                            p.op("act", lambda e: e.activation(out=sg[:], in_=gl[:], func=AF.Sigmoid, scale=1.702), reads=[kgl], writes=[ksg])
                            p.op("dve", lambda e: e.tensor_tensor(out=sg[:], in0=sg[:], in1=gl[:], op=ALU.mult), reads=[kgl, ksg], writes=[ksg])
                            p.op("dve", lambda e: e.tensor_scalar(out=gl[:], in0=pLn[:, 0:CAP], scalar1=bgT[:, blc:blc + 1], scalar2=-6.0, op0=ALU.add, op1=ALU.max),
                                 reads=[kpLn, "bgT", kgl], writes=[kgl])
                            p.op("dve", lambda e: e.scalar_tensor_tensor(out=aT[:, c, :], in0=gl[:], scalar=8.0, in1=sg[:], op0=ALU.min, op1=ALU.mult),
                                 reads=[ksg, kgl], writes=[kaT])

                    def dn_stage(ex_, d0, d1, bd, kbd, nxt=None):
                        for s_ in range(NS):
                            if nxt is not None:
                                transposes_s(nxt, s_)
                            Yst, kY = Yst_r.next()
                            for half in range(2):
                                Wd, kWd = (d0, d1)[half]
                                pA, kpA = pA_r.next()
                                for c in range(8):
                                    p.op("pe", lambda e: e.matmul(out=pA[:], lhsT=aT[:, c, s_ * 128:(s_ + 1) * 128], rhs=Wd[:, c, :], start=(c == 0), stop=False),
                                         reads=["aT_%d" % (c // 4), kWd], writes=[kpA])
                                p.op("pe", lambda e: e.matmul(out=pA[:], lhsT=ones[0:1, :], rhs=bd[0:1, half * 512:(half + 1) * 512], start=False, stop=True),
                                     reads=["ones", kbd], writes=[kpA])
                                p.op("act", lambda e: e.copy(out=Yst[:, half * 512:(half + 1) * 512], in_=pA[:]), reads=[kpA], writes=[kY])
                            r0 = ex_ * CAP + s_ * 128
                            p.op("sp", lambda e: e.dma_start(out=Yd[r0:r0 + 128, :], in_=Yst[:]), reads=[kY], dma=kY + "s")

                    def loads_gl0(ex_):
                        return load_w(w_gu[ex_, :, 0:512], 0), load_w(w_gu[ex_, :, 1024:1536], 1)

                    def loads_gl1(ex_):
                        return load_w(w_gu[ex_, :, 512:1024], 2), load_w(w_gu[ex_, :, 1536:2048], 3)

                    def loads_d(ex_):
                        return load_w(w_dn[ex_, :, 0:512], 4), load_w(w_dn[ex_, :, 512:1024], 5), load_bd(ex_)

                    g0, l0 = loads_gl0(0)
                    g1, l1 = loads_gl1(0)
                    d0, d1, (bd, kbd) = loads_d(0)
                    dispatch(0)
                    dispatch(1)
                    transposes(0)
                    for ex_ in range(NE):
                        gu_stage(ex_, 0, g0[0], g0[1], l0[0], l0[1], sel_for=(ex_ + 2 if ex_ + 2 < NE else None))
                        if ex_ + 1 < NE:
                            g0n, l0n = loads_gl0(ex_ + 1)
                        gu_stage(ex_, 1, g1[0], g1[1], l1[0], l1[1])
                        if ex_ + 2 < NE:
                            dispatch(ex_ + 2, build=False)
                        if ex_ + 1 < NE:
                            g1n, l1n = loads_gl1(ex_ + 1)
                        dn_stage(ex_, d0, d1, bd, kbd, nxt=(ex_ + 1 if ex_ + 1 < NE else None))
                        if ex_ + 1 < NE:
                            d0, d1, (bd, kbd) = loads_d(ex_ + 1)
                            g0, l0, g1, l1 = g0n, l0n, g1n, l1n
                    p.barrier()
                    Yg_r = Ring(sb2, "Yg", 4, [128, D], F32)
                    for tile in range(16):
                        hk = "H%d" % tile
                        for j_ in range(4):
                            Yg, kYg = Yg_r.next()
                            p.op("pool", lambda e: e.indirect_dma_start(out=Yg[:, :], out_offset=None, in_=Yd[:, :],
                                                                         in_offset=bass.IndirectOffsetOnAxis(ap=slI[:, tile, j_:j_ + 1], axis=0)),
                                 reads=["slI%d" % tile], writes=[kYg], dma=kYg)
                            p.op("dve", lambda e: e.scalar_tensor_tensor(out=H[:, tile, :], in0=Yg[:], scalar=gj[:, tile, j_:j_ + 1], in1=H[:, tile, :], op0=ALU.mult, op1=ALU.add),
                                 reads=[kYg, "gj%d" % tile, hk], writes=[hk])
                    p.barrier()
                    if stop_after == "C2":
                        if debug:
                            p.op("sp", lambda e: e.dma_start(out=dbgH.rearrange("(t p) n -> p t n", p=128), in_=H[:]), reads=["H%d" % i_ for i_ in range(16)], dma="dbgH")
                            p.op("sp", lambda e: e.dma_start(out=dbgG.rearrange("(t p) n -> p t n", p=128), in_=Gt[:]), reads=["Gt%d" % i_ for i_ in range(16)], dma="dbgG")
                        p.finish_wait("sp"); p.emit(top); return nc
                s3 = ExitStack()
                with s3:
                    sb3, ps3 = mk_alloc(s3)
                    gbc = sb3("gbc3", [128, D], F32)
                    junk = sb3("junkC3", [128, D], BF16)
                    Wpg = sb3("Wpg", [128, 8, D], BF16)
                    Wpp = sb3("Wpp", [128, 2, D], BF16)
                    p.op("pool", lambda e: e.dma_start(out=Wpg[:], in_=w_pg.rearrange("(c p) n -> p c n", p=128)), writes=["Wpg"], dma="Wpg")
                    p.op("pool", lambda e: e.dma_start(out=Wpp[:], in_=w_pp.rearrange("(c p) n -> p c n", p=128)), writes=["Wpp"], dma="Wpp")
                    gfin = sb3("gfin", [128, D], F32)
                    p.op("sp", lambda e: e.dma_start(out=gbc[:], in_=g_ple.partition_broadcast(128)), writes=["gbc"], dma="gbc3")
                    p.op("sp", lambda e: e.dma_start(out=gfin[:], in_=g_final.partition_broadcast(128)), writes=["gfin"], dma="gfin")
                    ss3_r = Ring(sb3, "ss3", 2, [128, 1], F32)
                    u3_r = Ring(sb3, "u3", 2, [128, D], BF16)
                    pT3_r = Ring(ps3, "pT3", 2, [128, 8, 128], BF16)
                    u3T_r = Ring(sb3, "u3T", 2, [128, 8, 128], BF16)
                    pp_r = Ring(sb3, "ppl", 2, [128, 256], F32)
                    ppb_r = Ring(sb3, "ppb", 2, [128, 256], BF16)
                    ppT_r = Ring(sb3, "ppT", 2, [128, 2, 128], BF16)
                    pg_r = Ring(ps3, "pg3", 2, [128, 512], F32)
                    pj_r = Ring(ps3, "pj3", 2, [128, 512], F32)
                    sg3_r = Ring(sb3, "sg3", 2, [128, 512], F32)
                    o_r = Ring(sb3, "o3", 2, [128, D], F32)
                    def c3_tile(tile):
                        hk = "H%d" % tile
                        yield
                        ss3, kss = ss3_r.next()
                        yield
                        p.op("act", lambda e, ss3=ss3, tile=tile: e.activation(out=junk[:], in_=H[:, tile, :], func=AF.Square, accum_out=ss3[:]), reads=[hk], writes=["junkC", kss])
                        yield
                        rstd_from(ss3[:], ss3[:], D, [kss], [kss])
                        yield
                        u3, ku3 = u3_r.next()
                        yield
                        p.op("dve", lambda e, ss3=ss3, u3=u3, tile=tile: e.scalar_tensor_tensor(out=u3[:], in0=H[:, tile, :], scalar=ss3[:, 0:1], in1=gbc[:], op0=ALU.mult, op1=ALU.mult),
                             reads=[hk, kss, "gbc"], writes=[ku3])
                        yield
                        pT, kpT = pT3_r.next()
                        yield
                        for c in range(8):
                            p.op("pe", lambda e, c=c, pT=pT, u3=u3: e.transpose(out=pT[:, c, :], in_=u3[:, c * 128:(c + 1) * 128], identity=ident[:]), reads=[ku3, "ident"], writes=[kpT])
                        yield
                        u3T, ku3T = u3T_r.next()
                        yield
                        p.op("act", lambda e, pT=pT, u3T=u3T: e.copy(out=u3T[:], in_=pT[:]), reads=[kpT], writes=[ku3T])
                        yield
                        pp, kpp = pp_r.next()
                        yield
                        p.op("sp", lambda e, pp=pp, tile=tile: e.dma_start(out=pp[:], in_=po[tile * 128:(tile + 1) * 128, :]), writes=[kpp], dma=kpp)
                        yield
                        ppb, kppb = ppb_r.next()
                        yield
                        p.op("pool", lambda e, pp=pp, ppb=ppb: e.tensor_copy(out=ppb[:], in_=pp[:]), reads=[kpp], writes=[kppb])
                        yield
                        pT2, kpT2 = pT3_r.next()
                        yield
                        for c in range(2):
                            p.op("pe", lambda e, c=c, pT2=pT2, ppb=ppb: e.transpose(out=pT2[:, c, :], in_=ppb[:, c * 128:(c + 1) * 128], identity=ident[:]), reads=[kppb, "ident"], writes=[kpT2])
                        yield
                        ppT, kppT = ppT_r.next()
                        yield
                        p.op("act", lambda e, pT2=pT2, ppT=ppT: e.copy(out=ppT[:], in_=pT2[:, 0:2, :]), reads=[kpT2], writes=[kppT])
                        yield
                        for half in range(2):
                            pg, kpg = pg_r.next()
                            pj, kpj = pj_r.next()
                            for c in range(8):
                                p.op("pe", lambda e, c=c, pg=pg, u3T=u3T, half=half: e.matmul(out=pg[:], lhsT=u3T[:, c, :], rhs=Wpg[:, c, half * 512:(half + 1) * 512], start=(c == 0), stop=(c == 7)),
                                     reads=[ku3T, "Wpg"], writes=[kpg])
                            for c in range(2):
                                p.op("pe", lambda e, c=c, pj=pj, ppT=ppT, half=half: e.matmul(out=pj[:], lhsT=ppT[:, c, :], rhs=Wpp[:, c, half * 512:(half + 1) * 512], start=(c == 0), stop=(c == 1)),
                                     reads=[kppT, "Wpp"], writes=[kpj])
                            sg, ksg = sg3_r.next()
                            p.op("act", lambda e, sg=sg, pg=pg: e.activation(out=sg[:], in_=pg[:], func=AF.Sigmoid), reads=[kpg], writes=[ksg])
                            p.op("dve", lambda e, sg=sg, pj=pj: e.tensor_tensor(out=sg[:], in0=sg[:], in1=pj[:], op=ALU.mult), reads=[ksg, kpj], writes=[ksg])
                            p.op("dve", lambda e, sg=sg, tile=tile, half=half: e.tensor_tensor(out=H[:, tile, half * 512:(half + 1) * 512], in0=H[:, tile, half * 512:(half + 1) * 512], in1=sg[:], op=ALU.add),
                                 reads=[ksg, hk], writes=[hk])
                        yield
                        ss4, kss4 = ss3_r.next()
                        yield
                        p.op("act", lambda e, ss4=ss4, tile=tile: e.activation(out=junk[:], in_=H[:, tile, :], func=AF.Square, accum_out=ss4[:]), reads=[hk], writes=["junkC", kss4])
                        yield
                        rstd_from(ss4[:], ss4[:], D, [kss4], [kss4])
                        yield
                        ot, kot = o_r.next()
                        yield
                        p.op("dve", lambda e, ss4=ss4, ot=ot, tile=tile: e.scalar_tensor_tensor(out=ot[:], in0=H[:, tile, :], scalar=ss4[:, 0:1], in1=gfin[:], op0=ALU.mult, op1=ALU.mult),
                             reads=[hk, kss4, "gfin"], writes=[kot])
                        yield
                        p.op("sp", lambda e, ot=ot, tile=tile: e.dma_start(out=yo[tile * 128:(tile + 1) * 128, :], in_=ot[:]), reads=[kot], writes=["yo"], dma=kot + "s")

                    for pair in range(8):
                        gens = [c3_tile(pair * 2), c3_tile(pair * 2 + 1)]
                        while gens:
                            for g_ in list(gens):
                                try:
                                    next(g_)
                                except StopIteration:
                                    gens.remove(g_)
        p.finish_wait("sp")
        p.emit(top)
    return nc


_CACHE = {}


def _perm(j):
    idx = []
    for t in range(4):
        for blk in ORDER[j]:
            b0 = (8 * t + blk) * 128
            idx.append(np.arange(b0, b0 + 128))
    return np.concatenate(idx)


def kernel(x, p, positions, w_in, g_attn, g_cq, w_uq, g_ckv, w_ukv, g_out_mla, g_out_sb, w_o,
           g_moe, w_router, b_router, w_gu, b_gu, w_dn, b_dn, g_ple, w_ple_gate, w_ple_proj, g_final):
    if "nc" not in _CACHE:
        _CACHE["nc"] = build_program()
    nc = _CACHE["nc"]
    in_maps, perms = make_in_maps(x, p, positions, w_in, g_attn, g_cq, w_uq, g_ckv, w_ukv, g_out_mla, g_out_sb, w_o,
                                  g_moe, w_router, b_router, w_gu, b_gu, w_dn, b_dn, g_ple, w_ple_gate, w_ple_proj, g_final)
    res = run_bass_kernel_spmd(nc, in_maps, core_ids=list(range(8)))
    out = np.empty((4, S, D), np.float32)
    for c in range(8):
        b, j = c // 2, c % 2
        out[b, perms[j]] = np.asarray(res.results[c]["yo"])
    return out


def make_in_maps(x, p, positions, w_in, g_attn, g_cq, w_uq, g_ckv, w_ukv, g_out_mla, g_out_sb, w_o,
                 g_moe, w_router, b_router, w_gu, b_gu, w_dn, b_dn, g_ple, w_ple_gate, w_ple_proj, g_final):
    f = lambda a: np.ascontiguousarray(np.asarray(a))
    x = f(x); p = f(p); positions = f(positions)
    invf = np.zeros((128, 1), np.float32)
    fr = (10000.0 ** (-np.arange(0, 32, 2, dtype=np.float32) / 32.0)).astype(np.float32)
    invf[0:16, 0] = fr
    invf[16:32, 0] = fr
    shared = {
        "invf": invf,
        "w_in": f(w_in[0]), "g_attn": f(g_attn[0:1]), "g_cq": f(g_cq[0:1]), "w_uq": f(w_uq[0]),
        "g_ckv": f(g_ckv[0:1]), "w_ukv": f(w_ukv[0]),
        "g_out": f(np.concatenate([np.asarray(g_out_mla[0]), np.asarray(g_out_sb[0])])[None, :]),
        "w_o": f(w_o[0]), "g_moe": f(g_moe[0:1]), "w_router": f(w_router[0]), "b_router": f(b_router[0:1]),
        "w_gu": f(w_gu[0]), "b_gu": f(np.asarray(b_gu[0]).reshape(NE * 16, 128)), "w_dn": f(w_dn[0]), "b_dn": f(b_dn[0]),
        "g_ple": f(g_ple[0:1]), "w_pg": f(w_ple_gate[0]), "w_pp": f(w_ple_proj[0]), "g_final": f(np.asarray(g_final)[None, :]),
    }
    in_maps = []
    perms = [_perm(0), _perm(1)]
    for c in range(8):
        b, j = c // 2, c % 2
        pm = perms[j]
        qr = np.concatenate([np.arange(blk * 128, blk * 128 + 128) for blk in ORDER[j]]).astype(np.float32)[None, :]
        m = dict(shared)
        m["xa"] = x[b]
        m["xo"] = f(x[b][pm])
        m["po"] = f(p[0, b][pm])
        m["posa"] = f(positions[b:b + 1].astype(np.int32))
        m["poso"] = f(positions[b:b + 1, pm].astype(np.int32))
        m["qrel"] = f(qr)
        in_maps.append(m)
    return in_maps, perms
```

```python
from contextlib import ExitStack
import numpy as np
import concourse.bass as bass
import concourse.mybir as mybir
from concourse.bass_utils import run_bass_kernel_spmd

F32 = mybir.dt.float32
BF16 = mybir.dt.bfloat16
I32 = mybir.dt.int32
AF = mybir.ActivationFunctionType
ALU = mybir.AluOpType

ENGS = ("pe", "act", "dve", "pool", "sp")
S = 4096
D = 1024
NE = 32
ORDER = ([6, 5, 3, 0], [7, 4, 2, 1])
NDIAG = [512, 512, 384, 384, 256, 256, 128, 128]
NEG = -30000.0
EPS = 1e-6


class Prog:
    def __init__(self, nc):
        self.nc = nc
        self.ops = {e: [] for e in ENGS}
        self.vcs = {}
        self.cur = {e: {} for e in ENGS}
        self.last_w = {}
        self.readers = {}
        self.excl = set()

    def op(self, eng, fn, reads=(), writes=(), dma=None):
        rec_ = _Rec()
        fn(rec_)
        assert len(rec_.calls) == 1
        fn = rec_.calls[0]
        clk = ("dma:" + dma) if dma else eng
        deps = []
        reads = list(reads)
        writes = list(writes)
        for k in reads:
            if k in self.excl and k not in writes:
                writes.append(k)
        for k in reads:
            lw = self.last_w.get(k)
            if lw:
                deps.append(lw)
        for k in writes:
            lw = self.last_w.get(k)
            if lw:
                deps.append(lw)
            for c, i in self.readers.get(k, {}).items():
                deps.append((c, i))
        cur = self.cur[eng]
        wmax = {}
        for (c, i) in deps:
            if c == "pe" and eng == "pe" and not dma:
                continue
            if cur.get(c, 0) >= i:
                continue
            wmax[c] = max(wmax.get(c, 0), i)
            for c2, i2 in self.vcs[c][i - 1].items():
                if cur.get(c2, 0) < i2:
                    cur[c2] = i2
            if cur.get(c, 0) < i:
                cur[c] = i
        vc = dict(cur)
        lst = self.vcs.setdefault(clk, [])
        lst.append(vc)
        idx = len(lst)
        vc[clk] = idx
        rec = {"fn": fn, "waits": wmax, "clk": clk, "idx": idx}
        self.ops[eng].append(rec)
        for k in reads:
            self.readers.setdefault(k, {})[clk] = idx
        for k in writes:
            self.last_w[k] = (clk, idx)
            self.readers[k] = {}
        return rec

    def finish_wait(self, eng):
        waits = {}
        for c, l in self.vcs.items():
            if len(l) and self.cur[eng].get(c, 0) < len(l):
                waits[c] = len(l)
                self.cur[eng][c] = len(l)
        self.ops[eng].append({"fn": None, "waits": waits, "clk": None, "idx": None})

    def barrier(self):
        for e in ENGS:
            self.finish_wait(e)
        full = {c: len(l) for c, l in self.vcs.items()}
        for e in ENGS:
            self.cur[e] = dict(full)

    def emit(self, stack):
        nc = self.nc
        waited = {}
        for e in ENGS:
            for r in self.ops[e]:
                for c, i in r["waits"].items():
                    waited.setdefault(c, set()).add(i)
        sems, semval = {}, {}
        for c, l in self.vcs.items():
            if c not in waited:
                continue
            sems[c] = stack.enter_context(nc.semaphore("s_" + c.replace(":", "_")))
            isd = c.startswith("dma:")
            v, m = 0, {}
            for i in range(1, len(l) + 1):
                if isd or i in waited[c]:
                    v += 16 if isd else 1
                    m[i] = v
            semval[c] = m
        block = stack.enter_context(nc.Block())
        engobj = {"pe": "tensor", "act": "scalar", "dve": "vector", "pool": "gpsimd", "sp": "sync"}

        def make(e):
            def body(eng):
                for r in self.ops[e]:
                    for c, i in r["waits"].items():
                        eng.wait_ge(sems[c], semval[c][i])
                    if r["fn"] is None:
                        continue
                    name, a, k = r["fn"]
                    ins = getattr(eng, name)(*a, **k)
                    c, i = r["clk"], r["idx"]
                    if c in sems and i in semval[c]:
                        ins.then_inc(sems[c], 16 if c.startswith("dma:") else 1)
            return body

        for e in ENGS:
            if self.ops[e]:
                getattr(block, engobj[e])(make(e))


class _Rec:
    def __init__(self):
        self.calls = []

    def __getattr__(self, name):
        def f(*a, **k):
            self.calls.append((name, a, k))
        return f


class Ring:
    def __init__(self, alloc, name, n, shape, dtype):
        self.tiles = [alloc("%s%d" % (name, i), shape, dtype) for i in range(n)]
        self.keys = ["%s%d" % (name, i) for i in range(n)]
        self.i = 0

    def next(self):
        t, k = self.tiles[self.i % len(self.tiles)], self.keys[self.i % len(self.tiles)]
        self.i += 1
        return t, k


class _Stop(Exception):
    pass


def build_program(stop_after=None, debug=False):
    nc = bass.Bass("TRN2", target_bir_lowering=False)

    def din(name, shape, dt=F32):
        return nc.dram_tensor(name, list(shape), dt, kind="ExternalInput").ap()

    def dscr(name, shape, dt=BF16):
        return nc.dram_tensor(name, list(shape), dt, kind="ExternalOutput" if debug else "Internal").ap()

    xa = din("xa", [S, D])
    xo = din("xo", [2048, D])
    po = din("po", [2048, 256])
    posa = din("posa", [1, S], I32)
    poso = din("poso", [1, 2048], I32)
    qrel = din("qrel", [1, 512])
    invf = din("invf", [128, 1])
    w_in = din("w_in", [D, 2208])
    g_attn = din("g_attn", [1, D])
    g_cq = din("g_cq", [1, 384])
    w_uq = din("w_uq", [384, 768])
    g_ckv = din("g_ckv", [1, 256])
    w_ukv = din("w_ukv", [256, 1024])
    g_out = din("g_out", [1, 1024])
    w_o = din("w_o", [D, D])
    g_moe = din("g_moe", [1, D])
    w_router = din("w_router", [D, NE])
    b_router = din("b_router", [1, NE])
    NEd = NE if stop_after in (None, "C2") else 1
    w_gu = din("w_gu", [NEd, D, 2048])
    b_gu = din("b_gu", [NE * 16, 128])
    w_dn = din("w_dn", [NEd, D, D])
    b_dn = din("b_dn", [NE, D])
    g_ple = din("g_ple", [1, D])
    w_pg = din("w_pg", [D, D])
    w_pp = din("w_pp", [256, D])
    g_final = din("g_final", [1, D])
    yo = nc.dram_tensor("yo", [2048, D], F32, kind="ExternalOutput").ap()

    KnT = dscr("KnT", [512, S])
    KrT = dscr("KrT", [32, S])
    VmD = dscr("VmD", [S, 512])
    KsT = dscr("KsT", [512, S])
    VsD = dscr("VsD", [S, 512])
    QmT = dscr("QmT", [8 * 96, 2048])
    QsT = dscr("QsT", [512, 2048])
    OTd = dscr("OTd", [1024, 2048])
    CAP = 512
    NS = CAP // 128
    Ud = nc.dram_tensor("Ud", [2048, D], BF16, kind="Internal").ap()
    Yd = nc.dram_tensor("Yd", [NE * CAP, D], F32, kind="Internal").ap()
    if debug:
        dbgH = nc.dram_tensor("dbgH", [2048, D], F32, kind="ExternalOutput").ap()
        dbgG = nc.dram_tensor("dbgG", [2048, NE], F32, kind="ExternalOutput").ap()
        dbgS = nc.dram_tensor("dbgS", [2048, 4], I32, kind="ExternalOutput").ap()
        dbgJ = nc.dram_tensor("dbgJ", [2048, 4], F32, kind="ExternalOutput").ap()

    top = ExitStack()
    with top:
        p = Prog(nc)
        if True:

            def mk_alloc(stack):
                def sb(n, s, d):
                    return stack.enter_context(nc.sbuf_tensor(n, list(s), d))

                def ps(n, s, d=F32):
                    return stack.enter_context(nc.psum_tensor(n, list(s), d))
                return sb, ps

            sb0, ps0 = mk_alloc(top)

            identf = sb0("identf", [128, 128], F32)
            ident = sb0("ident", [128, 128], BF16)
            ones = sb0("ones", [128, 128], BF16)
            ntri = sb0("ntri", [128, 128], BF16)
            nones = sb0("nones", [128, 128], BF16)
            zeros = sb0("zeros", [128, 128], BF16)
            p.op("pool", lambda e: e.memset(identf[:], 0.0), writes=["identf"])
            p.op("pool", lambda e: e.affine_select(out=identf[:], in_=identf[:], pattern=[[-1, 128]],
                                                    compare_op=ALU.not_equal, fill=1.0, base=0, channel_multiplier=1),
                 reads=["identf"], writes=["identf"])
            p.op("dve", lambda e: e.tensor_copy(out=ident[:], in_=identf[:]), reads=["identf"], writes=["ident"])
            p.op("pool", lambda e: e.memset(ones[:], 1.0), writes=["ones"])
            p.op("pool", lambda e: e.memset(nones[:], -1.0), writes=["nones"])
            p.op("pool", lambda e: e.memset(zeros[:], 0.0), writes=["zeros"])
            gcol = sb0("gcol", [128, 16], F32)
            p.op("sp", lambda e: e.dma_start(out=gcol[:, 0:3], in_=g_cq.rearrange("o (c p) -> p (o c)", p=128),
                                             allow_slow_non_contiguous=True), writes=["gcol"], dma="gcol")
            p.op("sp", lambda e: e.dma_start(out=gcol[:, 3:5], in_=g_ckv.rearrange("o (c p) -> p (o c)", p=128),
                                             allow_slow_non_contiguous=True), writes=["gcol"], dma="gcol")
            p.op("sp", lambda e: e.dma_start(out=gcol[:, 5:13], in_=g_out.rearrange("o (c p) -> p (o c)", p=128),
                                             allow_slow_non_contiguous=True), writes=["gcol"], dma="gcol")
            invc = sb0("invc", [128, 1], F32)
            p.op("sp", lambda e: e.dma_start(out=invc[:], in_=invf), writes=["invc"], dma="invc")

            def rstd_from(eng_out, src, n, rd, wr):
                p.op("act", lambda e: e.activation(out=eng_out, in_=src, func=AF.Ln, scale=1.0 / n, bias=EPS),
                     reads=rd, writes=wr)
                p.op("act", lambda e: e.activation(out=eng_out, in_=eng_out, func=AF.Exp, scale=-0.5),
                     reads=wr, writes=wr)

            sa = ExitStack()
            with sa:
                sb, ps = mk_alloc(sa)
                tmpf = sb("tmpf", [128, 128], F32)
                p.op("pool", lambda e: e.memset(tmpf[:], -1.0), writes=["tmpf"])
                p.op("pool", lambda e: e.affine_select(out=tmpf[:], in_=tmpf[:], pattern=[[-1, 128]],
                                                        compare_op=ALU.is_ge, fill=0.0, base=0, channel_multiplier=1),
                     reads=["tmpf"], writes=["tmpf"])
                p.op("dve", lambda e: e.tensor_copy(out=ntri[:], in_=tmpf[:]), reads=["tmpf"], writes=["ntri"])

                Win = sb("Win", [128, 8, 2208], BF16)
                for c in range(8):
                    p.op("pool", lambda e, c=c: e.dma_start(out=Win[:, c, :], in_=w_in[c * 128:(c + 1) * 128, :]),
                         writes=["Win"], dma="Win")
                Wuq = sb("Wuq", [128, 3, 768], BF16)
                Wuqr = sb("Wuqr", [128, 3, 768], BF16)
                p.op("pool", lambda e: e.dma_start(out=Wuq[:], in_=w_uq.rearrange("(c p) n -> p c n", p=128)),
                     writes=["Wuq"], dma="Wuq")
                Wkn = sb("Wkn", [128, 2, 512], BF16)
                Wv = sb("Wv", [128, 2, 512], BF16)
                ukv = w_ukv.rearrange("(c p) (h t d) -> p c h t d", p=128, h=8, t=2)
                for c in range(2):
                    p.op("pool", lambda e, c=c: e.dma_start(out=Wkn[:, c, :].rearrange("p (h d) -> p h d", h=8),
                                                          in_=ukv[:, c, :, 0, :]), writes=["Wkn"], dma="Wkn")
                    p.op("pool", lambda e, c=c: e.dma_start(out=Wv[:, c, :].rearrange("p (h d) -> p h d", h=8),
                                                          in_=ukv[:, c, :, 1, :]), writes=["Wv"], dma="Wv")
                p.op("pool", lambda e: e.memset(Wuqr[:], 0.0), writes=["Wuqr"])
                Wq4 = Wuq[:].rearrange("p c (h d) -> p c h d", h=8)
                Wr4 = Wuqr[:].rearrange("p c (h d) -> p c h d", h=8)
                for c in range(3):
                    p.op("dve", lambda e, c=c: e.tensor_scalar(out=Wr4[:, c, :, 64:80], in0=Wq4[:, c, :, 80:96], scalar1=-1.0,
                                                          scalar2=None, op0=ALU.mult), reads=["Wuq", "Wuqr"], writes=["Wuqr"])
                    p.op("dve", lambda e, c=c: e.tensor_copy(out=Wr4[:, c, :, 80:96], in_=Wq4[:, c, :, 64:80]),
                         reads=["Wuq", "Wuqr"], writes=["Wuqr"])
                Wkr = sb("Wkr", [128, 8, 64], BF16)
                p.op("dve", lambda e: e.tensor_copy(out=Wkr[:, :, 0:32], in_=Win[:, :, 640:672]), reads=["Win"], writes=["Wkr"])
                p.op("dve", lambda e: e.tensor_scalar(out=Wkr[:, :, 32:48], in0=Win[:, :, 656:672], scalar1=-1.0, scalar2=None,
                                                      op0=ALU.mult), reads=["Win", "Wkr"], writes=["Wkr"])
                p.op("dve", lambda e: e.tensor_copy(out=Wkr[:, :, 48:64], in_=Win[:, :, 640:656]), reads=["Win", "Wkr"], writes=["Wkr"])
                gattn = sb("gattn", [128, D], F32)
                p.op("sp", lambda e: e.dma_start(out=gattn[:], in_=g_attn.partition_broadcast(128)), writes=["gattn"], dma="gattn")

                def rope_table(Ct, St, pos_ap, n, rows, name, inv, kinv, sb):
                    posi = sb(name + "_pi", [128, n], I32)
                    ang = sb(name + "_ang", [128, n], F32)
                    kk = sb(name + "_k", [128, n], F32)
                    ki = sb(name + "_ki", [128, n], I32)
                    p.op("sp", lambda e: e.dma_start(out=posi[:], in_=pos_ap.partition_broadcast(128)), writes=[name + "pi"], dma=name + "pi")
                    p.op("dve", lambda e: e.tensor_copy(out=ang[:], in_=posi[:]), reads=[name + "pi"], writes=[name + "ang"])
                    p.op("dve", lambda e: e.tensor_scalar(out=ang[:], in0=ang[:], scalar1=inv[:, 0:1], scalar2=None, op0=ALU.mult),
                         reads=[name + "ang", kinv], writes=[name + "ang"])
                    for which, T in (("s", St), ("c", Ct)):
                        off = 0.0 if which == "s" else float(np.pi / 2)
                        p.op("dve", lambda e, off=off: e.tensor_scalar(out=kk[:], in0=ang[:], scalar1=off, scalar2=float(1.0 / (2 * np.pi)),
                                                                   op0=ALU.add, op1=ALU.mult), reads=[name + "ang"], writes=[name + "kk"])
                        p.op("dve", lambda e: e.tensor_copy(out=ki[:], in_=kk[:]), reads=[name + "kk"], writes=[name + "ki"])
                        p.op("dve", lambda e: e.tensor_copy(out=kk[:], in_=ki[:]), reads=[name + "ki"], writes=[name + "kk"])
                        p.op("dve", lambda e, T=T: e.scalar_tensor_tensor(out=T, in0=kk[:rows], scalar=-6.28125, in1=ang[:rows],
                                                                       op0=ALU.mult, op1=ALU.add),
                             reads=[name + "kk", name + "ang"], writes=[name + which])
                        p.op("dve", lambda e, T=T, off=off: e.scalar_tensor_tensor(out=T, in0=kk[:rows], scalar=float(-(2 * np.pi - 6.28125)), in1=T,
                                                                                op0=ALU.mult, op1=ALU.add),
                             reads=[name + "kk", name + which], writes=[name + which])
                        if off != 0.0:
                            p.op("dve", lambda e, T=T, off=off: e.tensor_scalar(out=T, in0=T, scalar1=off, scalar2=None, op0=ALU.add),
                                 reads=[name + which], writes=[name + which])
                        p.op("dve", lambda e: e.tensor_scalar(out=kk[:rows], in0=T, scalar1=float(np.pi), scalar2=float(-2 * np.pi),
                                                              op0=ALU.is_gt, op1=ALU.mult), reads=[name + which], writes=[name + "kk"])
                        p.op("dve", lambda e, T=T: e.tensor_tensor(out=T, in0=T, in1=kk[:rows], op=ALU.add),
                             reads=[name + which, name + "kk"], writes=[name + which])
                        p.op("dve", lambda e: e.tensor_scalar(out=kk[:rows], in0=T, scalar1=float(-np.pi), scalar2=float(2 * np.pi),
                                                              op0=ALU.is_lt, op1=ALU.mult), reads=[name + which], writes=[name + "kk"])
                        p.op("dve", lambda e, T=T: e.tensor_tensor(out=T, in0=T, in1=kk[:rows], op=ALU.add),
                             reads=[name + which, name + "kk"], writes=[name + which])
                        p.op("dve", lambda e, T=T: e.tensor_scalar(out=T, in0=T, scalar1=3.14159, scalar2=-3.14159, op0=ALU.min, op1=ALU.max),
                             reads=[name + which], writes=[name + which])
                        p.op("act", lambda e, T=T: e.activation(out=T, in_=T, func=AF.Sin), reads=[name + which], writes=[name + which])

                Ck = sb("Ck", [32, S], F32)
                Sk = sb("Sk", [32, S], F32)
                sa2 = ExitStack()
                with sa2:
                    rope_table(Ck[:], Sk[:], posa, S, 32, "rk", invc, "invc", mk_alloc(sa2)[0])
                    p.barrier()
                Cq = sb("Cq", [96, 2048], F32)
                Sq = sb("Sq", [96, 2048], F32)
                sa3 = ExitStack()
                with sa3:
                    sb3_ = mk_alloc(sa3)[0]
                    invq = sb3_("invq", [128, 1], F32)
                    p.op("pool", lambda e: e.memset(invq[:], 0.0), writes=["invq"])
                    p.op("sp", lambda e: e.dma_start(out=invq[64:96, :], in_=invf[0:32, :]), reads=["invq"], writes=["invq"], dma="invq")
                    rope_table(Cq[:], Sq[:], poso, 2048, 96, "rq", invq, "invq", sb3_)
                    p.barrier()
                    if stop_after == "R":
                        p.finish_wait("sp"); p.emit(top); return nc
                msc = float((64 + 32) ** -0.5)
                p.op("dve", lambda e: e.tensor_scalar(out=Cq[:], in0=Cq[:], scalar1=msc, scalar2=None, op0=ALU.mult),
                     reads=["rqc"], writes=["rqc"])
                p.op("dve", lambda e: e.tensor_scalar(out=Sq[:], in0=Sq[:], scalar1=msc, scalar2=None, op0=ALU.mult),
                     reads=["rqs"], writes=["rqs"])

                xt_r = Ring(sb, "xt", 4, [128, D], F32)
                junk = sb("junkA", [128, D], F32)
                ss_r = Ring(sb, "ssA", 4, [128, 1], F32)
                xn_r = Ring(sb, "xn", 4, [128, D], BF16)
                xnT_r = Ring(sb, "xnT", 2, [128, 8, 512], BF16)
                pT_r = Ring(ps, "pTA", 2, [128, 8, 128], BF16)
                pm_r = Ring(ps, "pmA", 4, [128, 512], F32)
                pss = ps("pssA", [128, 512], F32)
                sq_r = Ring(sb, "sqA", 2, [128, 512], BF16)
                rbc = sb("rbcA", [128, 512], F32)
                cn_r = Ring(sb, "cnA", 2, [128, 3, 512], BF16)
                ev_r = Ring(sb, "evA", 2, [128, 512], BF16)
                stg_r = Ring(sb, "stgA", 3, [128, 4, 512], BF16)
                stg8_r = Ring(sb, "stg8A", 2, [128, 8, 512], BF16)
                evf_r = Ring(sb, "evfA", 2, [128, 512], F32)
                evf2_r = Ring(sb, "evf2A", 2, [128, 512], F32)

                def make_xnT(src, tok0, defer=False):
                    xnT, kT = xnT_r.next()
                    xs = []
                    for sub in range(4):
                        xt, kx = xt_r.next()
                        ss, ks = ss_r.next()
                        xn, kn = xn_r.next()
                        r0 = tok0 + sub * 128
                        p.op("sp", lambda e: e.dma_start(out=xt[:], in_=src[r0:r0 + 128, :]), writes=[kx], dma=kx)
                        p.op("act", lambda e: e.activation(out=junk[:], in_=xt[:], func=AF.Square, accum_out=ss[:]),
                             reads=[kx], writes=["junkA", ks])
                        rstd_from(ss[:], ss[:], D, [ks], [ks])
                        p.op("dve", lambda e: e.scalar_tensor_tensor(out=xn[:], in0=xt[:], scalar=ss[:, 0:1], in1=gattn[:],
                                                                     op0=ALU.mult, op1=ALU.mult),
                             reads=[kx, ks, "gattn"], writes=[kn])
                        xs.append((xn, kn))
                    def tr(sub):
                        xn, kn = xs[sub]
                        pT, kp = pT_r.next()
                        for c in range(8):
                            p.op("pe", lambda e: e.transpose(out=pT[:, c, :], in_=xn[:, c * 128:(c + 1) * 128], identity=ident[:]),
                                 reads=[kn, "ident"], writes=[kp])
                        p.op("dve", lambda e: e.tensor_copy(out=xnT[:, :, sub * 128:(sub + 1) * 128], in_=pT[:]),
                             reads=[kp], writes=[kT])
                    if defer:
                        return xnT, kT, tr
                    for sub in range(4):
                        tr(sub)
                    return xnT, kT

                def proj_fm(xnT, kT, col0, m, wkey="Win", W=None):
                    W = Win if W is None else W
                    pm, kpm = pm_r.next()
                    for k in range(8):
                        p.op("pe", lambda e, k=k, pm=pm, W=W: e.matmul(out=pm[0:m, :], lhsT=W[:, k, col0:col0 + m], rhs=xnT[:, k, :],
                                                                  start=(k == 0), stop=(k == 7)),
                             reads=[kT, wkey], writes=[kpm])
                    return pm, kpm

                def lowrank_norm(pms, nch, width, gc0):
                    for i, (pm, kpm) in enumerate(pms):
                        sq, ksq = sq_r.next()
                        p.op("act", lambda e, pm=pm, sq=sq: e.activation(out=sq[:], in_=pm[:], func=AF.Square), reads=[kpm], writes=[ksq])
                        p.op("pe", lambda e, sq=sq, i=i: e.matmul(out=pss[:], lhsT=ones[:], rhs=sq[:], start=(i == 0), stop=(i == nch - 1)),
                             reads=[ksq, "ones"], writes=["pssA"])
                    rstd_from(rbc[:], pss[:], width, ["pssA"], ["rbcA"])
                    cn, kcn = cn_r.next()
                    for i, (pm, kpm) in enumerate(pms):
                        p.op("dve", lambda e, pm=pm, i=i, cn=cn: e.scalar_tensor_tensor(out=cn[:, i, :], in0=pm[:], scalar=gcol[:, gc0 + i:gc0 + i + 1],
                                                                                    in1=rbc[:], op0=ALU.mult, op1=ALU.mult),
                             reads=[kpm, "gcol", "rbcA"], writes=[kcn])
                    return cn, kcn

                def store_fm(pm, kpm, rows, dst, eng="dve", scale=None):
                    ev, kev = ev_r.next()
                    if scale is None:
                        if eng == "act":
                            p.op("act", lambda e: e.copy(out=ev[0:rows, :], in_=pm[0:rows, :]), reads=[kpm], writes=[kev])
                        else:
                            p.op("dve", lambda e: e.tensor_copy(out=ev[0:rows, :], in_=pm[0:rows, :]), reads=[kpm], writes=[kev])
                    else:
                        p.op("act", lambda e: e.mul(out=ev[0:rows, :], in_=pm[0:rows, :], mul=scale), reads=[kpm], writes=[kev])
                    p.op("pool", lambda e: e.dma_start(out=dst, in_=ev[0:rows, :]), reads=[kev], writes=[], dma=kev + "s")

                def evac_to(pm, kpm, dst, kdst, eng="dve", scale=None):
                    if scale is not None:
                        p.op("act", lambda e: e.mul(out=dst, in_=pm[:], mul=scale), reads=[kpm], writes=[kdst])
                    elif eng == "act":
                        p.op("act", lambda e: e.copy(out=dst, in_=pm[:]), reads=[kpm], writes=[kdst])
                    else:
                        p.op("dve", lambda e: e.tensor_copy(out=dst, in_=pm[:]), reads=[kpm], writes=[kdst])

                KnT_v = KnT.rearrange("(c p) n -> p c n", p=128)
                KsT_v = KsT.rearrange("(c p) n -> p c n", p=128)
                QsT_v = QsT.rearrange("(c p) n -> p c n", p=128)
                VmD_v = VmD.rearrange("(s p) n -> p s n", p=128)
                VsD_v = VsD.rearrange("(s p) n -> p s n", p=128)
                QmT_v = QmT.rearrange("(h r) n -> r h n", r=96)

                srcs = [(xa, g_ * 512) for g_ in range(8)] + [(xo, g_ * 512) for g_ in range(4)]
                nxt = make_xnT(*srcs[0])
                for G in range(8):
                    c0 = G * 512
                    xnT, kT = nxt
                    nx_xnT, nx_kT, nx_tr = make_xnT(*srcs[G + 1], defer=True)
                    nxt = (nx_xnT, nx_kT)
                    ckv = [proj_fm(xnT, kT, 384 + m * 128, 128) for m in range(2)]
                    nx_tr(0)
                    cn, kcn = lowrank_norm(ckv, 2, 256, 3)
                    pa, kpa = proj_fm(xnT, kT, 0, 32, "Wkr", Wkr)
                    pb, kpb = proj_fm(xnT, kT, 32, 32, "Wkr", Wkr)
                    t1, kt1 = evf_r.next()
                    t2, kt2 = evf2_r.next()
                    p.op("dve", lambda e, pa=pa, t1=t1, c0=c0: e.tensor_tensor(out=t1[0:32, :], in0=pa[0:32, :], in1=Ck[:, c0:c0 + 512], op=ALU.mult),
                         reads=[kpa, "rkc"], writes=[kt1])
                    p.op("dve", lambda e, pb=pb, t2=t2, c0=c0: e.tensor_tensor(out=t2[0:32, :], in0=pb[0:32, :], in1=Sk[:, c0:c0 + 512], op=ALU.mult),
                         reads=[kpb, "rks"], writes=[kt2])
                    ev, kev = ev_r.next()
                    p.op("dve", lambda e, t1=t1, t2=t2, ev=ev: e.tensor_tensor(out=ev[0:32, :], in0=t1[0:32, :], in1=t2[0:32, :], op=ALU.add),
                         reads=[kt1, kt2], writes=[kev])
                    p.op("sp", lambda e, ev=ev, c0=c0: e.dma_start(out=KrT[:, c0:c0 + 512], in_=ev[0:32, :]), reads=[kev], writes=[], dma=kev + "s")
                    nx_tr(1)
                    st_, kst_ = stg_r.next()
                    for m in range(4):
                        pm, kpm = proj_fm(xnT, kT, 1184 + m * 128, 128)
                        evac_to(pm, kpm, st_[:, m, :], kst_, eng="act")
                    p.op("sp", lambda e: e.dma_start(out=KsT_v[:, :, c0:c0 + 512], in_=st_[:]), reads=[kst_], dma=kst_ + "s")
                    nx_tr(2)
                    st_, kst_ = stg_r.next()
                    for sub in range(4):
                        pm, kpm = pm_r.next()
                        for k in range(8):
                            p.op("pe", lambda e, k=k, pm=pm, sub=sub, xnT=xnT: e.matmul(out=pm[:], lhsT=xnT[:, k, sub * 128:(sub + 1) * 128],
                                                                                   rhs=Win[:, k, 1696:2208], start=(k == 0), stop=(k == 7)),
                                 reads=[kT, "Win"], writes=[kpm])
                        evac_to(pm, kpm, st_[:, sub, :], kst_, eng="dve")
                    p.op("sp", lambda e: e.dma_start(out=VsD_v[:, G * 4:(G + 1) * 4, :], in_=st_[:]), reads=[kst_], dma=kst_ + "s")

                    nx_tr(3)
                    st_, kst_ = stg_r.next()
                    for hp in range(4):
                        pm, kpm = pm_r.next()
                        for m in range(2):
                            p.op("pe", lambda e, m=m, pm=pm, hp=hp, cn=cn: e.matmul(out=pm[:], lhsT=Wkn[:, m, hp * 128:(hp + 1) * 128], rhs=cn[:, m, :],
                                                                               start=(m == 0), stop=(m == 1)),
                                 reads=[kcn, "Wkn"], writes=[kpm])
                        evac_to(pm, kpm, st_[:, hp, :], kst_, eng="act")
                    p.op("sp", lambda e: e.dma_start(out=KnT_v[:, :, c0:c0 + 512], in_=st_[:]), reads=[kst_], dma=kst_ + "s")
                    st_, kst_ = stg_r.next()
                    for sub in range(4):
                        pm, kpm = pm_r.next()
                        for m in range(2):
                            p.op("pe", lambda e, m=m, pm=pm, sub=sub, cn=cn: e.matmul(out=pm[:], lhsT=cn[:, m, sub * 128:(sub + 1) * 128], rhs=Wv[:, m, :],
                                                                                 start=(m == 0), stop=(m == 1)),
                                 reads=[kcn, "Wv"], writes=[kpm])
                        evac_to(pm, kpm, st_[:, sub, :], kst_, eng="dve")
                    p.op("sp", lambda e: e.dma_start(out=VmD_v[:, G * 4:(G + 1) * 4, :], in_=st_[:]), reads=[kst_], dma=kst_ + "s")
                for G in range(4):
                    c0 = G * 512
                    xnT, kT = nxt
                    if G + 1 < 4:
                        nxt = make_xnT(*srcs[8 + G + 1])
                    cq = [proj_fm(xnT, kT, m * 128, 128) for m in range(3)]
                    cn, kcn = lowrank_norm(cq, 3, 384, 0)
                    st8, kst8 = stg8_r.next()
                    for h in range(8):
                        pa, kpa = pm_r.next()
                        pb, kpb = pm_r.next()
                        for m in range(3):
                            p.op("pe", lambda e, m=m, pa=pa, h=h, cn=cn: e.matmul(out=pa[0:96, :], lhsT=Wuq[:, m, h * 96:(h + 1) * 96], rhs=cn[:, m, :],
                                                                             start=(m == 0), stop=(m == 2)),
                                 reads=[kcn, "Wuq"], writes=[kpa])
                        for m in range(3):
                            p.op("pe", lambda e, m=m, pb=pb, h=h, cn=cn: e.matmul(out=pb[0:96, :], lhsT=Wuqr[:, m, h * 96:(h + 1) * 96], rhs=cn[:, m, :],
                                                                             start=(m == 0), stop=(m == 2)),
                                 reads=[kcn, "Wuqr"], writes=[kpb])
                        t1, kt1 = evf_r.next()
                        t2, kt2 = evf2_r.next()
                        p.op("dve", lambda e, pa=pa, t1=t1, c0=c0: e.tensor_tensor(out=t1[0:96, :], in0=pa[0:96, :], in1=Cq[:, c0:c0 + 512], op=ALU.mult),
                             reads=[kpa, "rqc"], writes=[kt1])
                        p.op("dve", lambda e, pb=pb, t2=t2, c0=c0: e.tensor_tensor(out=t2[0:96, :], in0=pb[0:96, :], in1=Sq[:, c0:c0 + 512], op=ALU.mult),
                             reads=[kpb, "rqs"], writes=[kt2])
                        p.op("pool", lambda e, t1=t1, t2=t2: e.tensor_tensor(out=st8[0:96, h, :], in0=t1[0:96, :], in1=t2[0:96, :], op=ALU.add),
                             reads=[kt1, kt2], writes=[kst8])
                    p.op("sp", lambda e: e.dma_start(out=QmT_v[:, :, c0:c0 + 512], in_=st8[0:96, :, :]), reads=[kst8], dma=kst8 + "s")
                    st_, kst_ = stg_r.next()
                    for m in range(4):
                        pm, kpm = proj_fm(xnT, kT, 672 + m * 128, 128)
                        evac_to(pm, kpm, st_[:, m, :], kst_, scale=0.125)
                    p.op("sp", lambda e: e.dma_start(out=QsT_v[:, :, c0:c0 + 512], in_=st_[:]), reads=[kst_], dma=kst_ + "s")
                p.barrier()
                if stop_after == "A":
                    p.finish_wait("sp"); p.emit(top); return nc

            sbx = ExitStack()
            with sbx:
                sb, ps = mk_alloc(sbx)
                qrb = sb("qrb", [128, 512], F32)
                p.op("sp", lambda e: e.dma_start(out=qrb[:], in_=qrel.partition_broadcast(128)), writes=["qrb"], dma="qrb")
                kidx_i = sb("kidx_i", [128, 1], I32)
                krel = sb("krel", [128, 8], F32)
                p.op("pool", lambda e: e.iota(kidx_i[:], pattern=[[0, 1]], base=0, channel_multiplier=1), writes=["kidx_i"])
                p.op("dve", lambda e: e.tensor_copy(out=krel[:, 0:1], in_=kidx_i[:]), reads=["kidx_i"], writes=["krel"])
                for d in range(1, 8):
                    p.op("dve", lambda e, d=d: e.tensor_scalar(out=krel[:, d:d + 1], in0=krel[:, 0:1], scalar1=float(128 * d), scalar2=None, op0=ALU.add),
                         reads=["krel"], writes=["krel"])
                nmM = sb("nmM", [128, 8, 512], BF16)
                nmS = sb("nmS", [128, 8, 512], BF16)
                m01 = sb("m01", [128, 8, 512], BF16)
                for d in range(8):
                    p.op("dve", lambda e, d=d: e.tensor_scalar(out=nmM[:, d, :], in0=qrb[:], scalar1=krel[:, d:d + 1], scalar2=NEG, op0=ALU.is_lt, op1=ALU.mult),
                         reads=["qrb", "krel"], writes=["nmM"])
                    p.op("dve", lambda e, d=d: e.tensor_scalar(out=nmS[:, d, :], in0=qrb[:], scalar1=krel[:, d:d + 1], scalar2=NEG, op0=ALU.is_le, op1=ALU.mult),
                         reads=["qrb", "krel"], writes=["nmS"])
                    p.op("dve", lambda e, d=d: e.tensor_scalar(out=m01[:, d, :], in0=qrb[:], scalar1=krel[:, d:d + 1], scalar2=None, op0=ALU.is_gt),
                         reads=["qrb", "krel"], writes=["m01"])

                def blocks(t):
                    out = [(kb, 512, None) for kb in range(8 * t)]
                    out += [(8 * t + d, NDIAG[d], d) for d in range(8)]
                    return out

                sm = ExitStack()
                with sm:
                    def mla_gen():
                        sb, ps = mk_alloc(sm)
                        Vall = sb("Vall", [128, 32, 8, 65], BF16)
                        p.op("pool", lambda e: e.memset(Vall[:, :, :, 64:65], 1.0), writes=["Vone"])
                        vsrc = VmD.rearrange("(kb p) (h d) -> p kb h d", p=128, h=8)
                        for hh in range(8):
                            p.op("sp", lambda e: e.dma_start(out=Vall[:, :, hh, 0:64], in_=vsrc[:, :, hh, :]),
                                 writes=["Vall%d" % hh], dma="Vall%d" % hh)
                        KT_r = Ring(sb, "KTm", 2, [96, S], BF16)
                        QT_r = Ring(sb, "QTm", 2, [96, 2048], BF16)
                        pS_r = Ring(ps, "pSm", 2, [128, 512], F32)
                        pO_r = Ring(ps, "pOm", 1, [128, 512], F32)
                        pB_r = Ring(ps, "pBm", 1, [128, 512], F32)
                        P_r = Ring(sb, "Pm", 3, [128, 512], BF16)
                        Of_r = Ring(sb, "Ofm", 2, [65, 512], F32)
                        On_r = Ring(sb, "Onm", 2, [64, 512], BF16)
                        sel = sb("sel65", [65, 64], F32)
                        p.op("pool", lambda e: e.memset(sel[:], 0.0), writes=["sel65"])
                        p.op("pool", lambda e: e.memset(sel[64:65, :], 1.0), reads=["sel65"], writes=["sel65"])
                        for h in range(8):
                            KT, kKT = KT_r.next()
                            QT, kQT = QT_r.next()
                            p.op("sp", lambda e, KT=KT, h=h: e.dma_start(out=KT[0:64, :], in_=KnT[h * 64:(h + 1) * 64, :]), writes=[kKT], dma=kKT)
                            p.op("sp", lambda e, KT=KT: e.dma_start(out=KT[64:96, :], in_=KrT[:, :]), writes=[kKT], dma=kKT)
                            p.op("sp", lambda e, QT=QT, h=h: e.dma_start(out=QT[:], in_=QmT[h * 96:(h + 1) * 96, :]), writes=[kQT], dma=kQT)
                            for t in range(4):
                                bl = blocks(t)
                                pO, kpO = pO_r.next()
                                nb = len(bl)
                                stage = {}

                                def stA(i):
                                    kb, N, d = bl[i]
                                    pS, kpS = pS_r.next()
                                    p.op("pe", lambda e: e.matmul(out=pS[:, 0:N], lhsT=KT[:, kb * 128:(kb + 1) * 128], rhs=QT[:, t * 512:t * 512 + N],
                                                                  start=True, stop=(d is None)), reads=[kKT, kQT], writes=[kpS])
                                    if d is not None:
                                        p.op("pe", lambda e: e.matmul(out=pS[:, 0:N], lhsT=ident[:], rhs=nmM[:, d, 0:N], start=False, stop=True),
                                             reads=["ident", "nmM"], writes=[kpS])
                                    P, kP = P_r.next()
                                    p.op("act", lambda e: e.activation(out=P[:, 0:N], in_=pS[:, 0:N], func=AF.Exp), reads=[kpS], writes=[kP])
                                    stage[i] = (P, kP)

                                def stB(i):
                                    kb, N, d = bl[i]
                                    P, kP = stage.pop(i)
                                    p.op("pe", lambda e: e.matmul(out=pO[0:65, 0:N], lhsT=Vall[:, kb, h, :], rhs=P[:, 0:N], start=(i == 0), stop=(i == nb - 1)),
                                         reads=["Vall%d" % h, "Vone", kP], writes=[kpO])

                                for it in range(nb + 2):
                                    if it < nb:
                                        stA(it)
                                    if it - 2 >= 0:
                                        stB(it - 2)
                                    yield
                                Of, kOf = Of_r.next()
                                p.op("dve", lambda e, Of=Of, pO=pO: e.tensor_copy(out=Of[:], in_=pO[0:65, :]), reads=[kpO], writes=[kOf])
                                p.op("dve", lambda e, Of=Of: e.reciprocal(out=Of[64:65, :], in_=Of[64:65, :]), reads=[kOf], writes=[kOf])
                                pB, kpB = pB_r.next()
                                p.op("pe", lambda e, Of=Of, pB=pB: e.matmul(out=pB[0:64, :], lhsT=sel[:], rhs=Of[:], start=True, stop=True),
                                     reads=[kOf, "sel65"], writes=[kpB])
                                On, kOn = On_r.next()
                                p.op("dve", lambda e, Of=Of, pB=pB, On=On: e.tensor_tensor(out=On[:], in0=Of[0:64, :], in1=pB[0:64, :], op=ALU.mult),
                                     reads=[kOf, kpB], writes=[kOn])
                                p.op("pool", lambda e, On=On, h=h, t=t: e.dma_start(out=OTd[h * 64:(h + 1) * 64, t * 512:(t + 1) * 512], in_=On[:]),
                                     reads=[kOn], writes=[], dma=kOn + "s")

                    def sb_gen():
                        sb, ps = mk_alloc(sm)
                        Vs = sb("Vsall", [128, 32, 512], BF16)
                        vsrc = VsD.rearrange("(kb p) n -> p kb n", p=128)
                        for q4 in range(4):
                            p.op("sp", lambda e, q4=q4: e.dma_start(out=Vs[:, q4 * 8:(q4 + 1) * 8, :], in_=vsrc[:, q4 * 8:(q4 + 1) * 8, :]),
                                 writes=["Vs%d" % q4], dma="Vs%d" % q4)
                        KT_r = Ring(sb, "KTs", 2, [64, S], BF16)
                        QT_r = Ring(sb, "QTs", 2, [64, 2048], BF16)
                        pZ_r = Ring(ps, "pZs", 2, [128, 512], F32)
                        pL_r = Ring(ps, "pLs", 1, [128, 512], F32)
                        pO_r = Ring(ps, "pOs", 1, [128, 512], F32)
                        E_r = Ring(sb, "Es", 3, [128, 512], F32)
                        SP_r = Ring(sb, "SPs", 4, [128, 512], BF16)
                        SM_r = Ring(sb, "SMs", 4, [128, 512], BF16)
                        A_r = Ring(sb, "As", 3, [128, 512], BF16)
                        CR_r = Ring(sb, "CRs", 3, [128, 512], BF16)
                        On_r = Ring(sb, "Ons", 2, [64, 512], BF16)
                        for h in range(8):
                            KT, kKT = KT_r.next()
                            QT, kQT = QT_r.next()
                            p.op("sp", lambda e, KT=KT, h=h: e.dma_start(out=KT[:], in_=KsT[h * 64:(h + 1) * 64, :]), writes=[kKT], dma=kKT)
                            p.op("sp", lambda e, QT=QT, h=h: e.dma_start(out=QT[:], in_=QsT[h * 64:(h + 1) * 64, :]), writes=[kQT], dma=kQT)
                            for t in range(4):
                                bl = blocks(t)[::-1]
                                nb = len(bl)
                                pO, kpO = pO_r.next()
                                p.op("pe", lambda e, pO=pO: e.matmul(out=pO[0:64, :], lhsT=zeros[:, 0:64], rhs=m01[:, 0, :],
                                                                      start=True, stop=False), reads=["zeros", "m01"], writes=[kpO])
                                stage = {}
                                carry = {"t": None, "k": None}

                                def stA(i):
                                    kb, N, d = bl[i]
                                    pZ, kpZ = pZ_r.next()
                                    p.op("pe", lambda e: e.matmul(out=pZ[:, 0:N], lhsT=KT[:, kb * 128:(kb + 1) * 128], rhs=QT[:, t * 512:t * 512 + N],
                                                                  start=True, stop=True), reads=[kKT, kQT], writes=[kpZ])
                                    E, kE = E_r.next()
                                    p.op("act", lambda e: e.activation(out=E[:, 0:N], in_=pZ[:, 0:N], func=AF.Exp), reads=[kpZ], writes=[kE])
                                    SPt, kSP = SP_r.next()
                                    p.op("act", lambda e: e.activation(out=SPt[:, 0:N], in_=E[:, 0:N], func=AF.Ln, bias=1.0), reads=[kE], writes=[kSP])
                                    if d is not None:
                                        SM, kSM = SM_r.next()
                                        p.op("dve", lambda e: e.tensor_tensor(out=SM[:, 0:N], in0=SPt[:, 0:N], in1=m01[:, d, 0:N], op=ALU.mult),
                                             reads=[kSP, "m01"], writes=[kSM])
                                    else:
                                        SM, kSM = SPt, kSP
                                    cprev, kcprev = carry["t"], carry["k"]
                                    stage[i] = (SM, kSM, cprev, kcprev, E, kE)
                                    if i < nb - 1:
                                        cn_, kcn_ = CR_r.next()
                                        if cprev is None:
                                            if N < 512:
                                                p.op("pool", lambda e: e.memset(cn_[:, N:512], 0.0), writes=[kcn_])
                                            p.op("pool", lambda e: e.tensor_copy(out=cn_[:, 0:N], in_=SM[:, 0:N]), reads=[kSM], writes=[kcn_])
                                        else:
                                            if N < 512:
                                                p.op("pool", lambda e: e.tensor_copy(out=cn_[:, N:512], in_=cprev[:, N:512]), reads=[kcprev], writes=[kcn_])
                                            p.op("dve", lambda e: e.tensor_tensor(out=cn_[:, 0:N], in0=cprev[:, 0:N], in1=SM[:, 0:N], op=ALU.add),
                                                 reads=[kcprev, kSM], writes=[kcn_])
                                        carry["t"], carry["k"] = cn_, kcn_

                                def stB(i):
                                    kb, N, d = bl[i]
                                    SM, kSM, cprev, kcprev, E, kE = stage[i]
                                    pL, kpL = pL_r.next()
                                    last = "tri"
                                    if cprev is not None:
                                        last = "carry"
                                    if d is not None:
                                        last = "mask"
                                    p.op("pe", lambda e: e.matmul(out=pL[:, 0:N], lhsT=ntri[:], rhs=SM[:, 0:N], start=True, stop=(last == "tri")),
                                         reads=["ntri", kSM], writes=[kpL])
                                    if cprev is not None:
                                        p.op("pe", lambda e: e.matmul(out=pL[:, 0:N], lhsT=nones[:], rhs=cprev[:, 0:N], start=False, stop=(last == "carry")),
                                             reads=["nones", kcprev], writes=[kpL])
                                    if d is not None:
                                        p.op("pe", lambda e: e.matmul(out=pL[:, 0:N], lhsT=ident[:], rhs=nmS[:, d, 0:N], start=False, stop=True),
                                             reads=["ident", "nmS"], writes=[kpL])
                                    A, kA = A_r.next()
                                    p.op("act", lambda e: e.activation(out=A[:, 0:N], in_=pL[:, 0:N], func=AF.Exp), reads=[kpL], writes=[kA])
                                    p.op("dve", lambda e: e.tensor_tensor(out=A[:, 0:N], in0=A[:, 0:N], in1=E[:, 0:N], op=ALU.mult), reads=[kA, kE], writes=[kA])
                                    stage[i] = (A, kA)

                                def stC(i):
                                    kb, N, d = bl[i]
                                    A, kA = stage.pop(i)
                                    p.op("pe", lambda e: e.matmul(out=pO[0:64, 0:N], lhsT=Vs[:, kb, h * 64:(h + 1) * 64], rhs=A[:, 0:N], start=False, stop=(i == nb - 1)),
                                         reads=["Vs%d" % (kb // 8), kA], writes=[kpO])

                                for it in range(nb + 2):
                                    if it < nb:
                                        stA(it)
                                    if 0 <= it - 1 < nb:
                                        stB(it - 1)
                                    if it - 2 >= 0:
                                        stC(it - 2)
                                    yield
                                On, kOn = On_r.next()
                                p.op("dve", lambda e, On=On, pO=pO: e.tensor_copy(out=On[:], in_=pO[0:64, :]), reads=[kpO], writes=[kOn])
                                p.op("pool", lambda e, On=On, h=h, t=t: e.dma_start(out=OTd[512 + h * 64:512 + (h + 1) * 64, t * 512:(t + 1) * 512], in_=On[:]),
                                     reads=[kOn], writes=[], dma=kOn + "s")

                    gens = [mla_gen(), sb_gen()]
                    while gens:
                        for g_ in list(gens):
                            try:
                                next(g_)
                            except StopIteration:
                                gens.remove(g_)
                    p.barrier()
                    if stop_after == "B":
                        p.finish_wait("sp"); p.emit(top); return nc
            sc = ExitStack()
            with sc:
                sb, ps = mk_alloc(sc)
                H = sb("H", [128, 16, D], F32)
                posm = sb("posm", [128, 16, NE], F32)
                maskb = sb("maskb", [128, 16, NE], BF16)
                gj = sb("gj", [128, 16, 4], F32)
                slI = sb("slI", [128, 16, 4], I32)
                Gt = sb("Gt", [128, 16, NE], F32)
                bgT = sb("bgT", [128, 512], F32)
                s1 = ExitStack()
                with s1:
                    sb1, ps1 = mk_alloc(s1)
                    gbc = sb1("gbc", [128, D], F32)
                    junk = sb1("junkC", [128, D], BF16)
                    Wo = sb1("Wo", [128, 8, D], BF16)
                    p.op("pool", lambda e: e.dma_start(out=Wo[:], in_=w_o.rearrange("(c p) n -> p c n", p=128)), writes=["Wo"], dma="Wo")
                    Wr = sb1("Wr", [128, 8, NE], F32)
                    p.op("sp", lambda e: e.dma_start(out=Wr[:], in_=w_router.rearrange("(c p) n -> p c n", p=128)), writes=["Wr"], dma="Wr")
                    brb = sb1("brb", [128, NE], F32)
                    p.op("sp", lambda e: e.dma_start(out=brb[:], in_=b_router.partition_broadcast(128)), writes=["brb"], dma="brb")
                    p.op("sp", lambda e: e.dma_start(out=gbc[:], in_=g_moe.partition_broadcast(128)), writes=["gbc"], dma="gbc")
                    bgl = sb1("bgl", [128, 4, 128], F32)
                    p.op("sp", lambda e: e.dma_start(out=bgl[:], in_=b_gu.rearrange("(a r) q -> r a q", r=128)), writes=["bgl"], dma="bgl")
                    pTf_r = Ring(ps1, "pTf", 1, [128, 4, 128], F32)
                    pbt, kpbt = pTf_r.next()
                    for a in range(4):
                        p.op("pe", lambda e, a=a: e.transpose(out=pbt[:, a, :], in_=bgl[:, a, :], identity=identf[:]), reads=["bgl", "identf"], writes=[kpbt])
                    p.op("dve", lambda e: e.tensor_copy(out=bgT[:].rearrange("p (a q) -> p a q", a=4), in_=pbt[:]), reads=[kpbt], writes=["bgT"])

                    OT_r = Ring(sb1, "OTl", 1, [128, 8, 512], BF16)
                    sq_r = Ring(sb1, "sqC", 2, [128, 512], BF16)
                    pss_r = Ring(ps1, "pssC", 1, [128, 512], F32)
                    rb_r = Ring(sb1, "rbC", 2, [128, 512], F32)
                    MX = sb1("MX", [128, 8, 512], BF16)
                    pA_r = Ring(ps1, "pAC", 2, [128, 512], F32)
                    xr_r = Ring(sb1, "xrC", 2, [128, D], F32)
                    ssc_r = Ring(sb1, "sscC", 2, [128, 1], F32)
                    u32_r = Ring(sb1, "u32C", 2, [128, D], F32)
                    uhT_r = Ring(sb1, "uhT", 2, [128, 8, 128], BF16)
                    tris = sb1("tris", [128, 128], BF16)
                    tmpf1 = sb1("tmpf1", [128, 128], F32)
                    p.op("pool", lambda e: e.memset(tmpf1[:], 1.0), writes=["tmpf1"])
                    p.op("pool", lambda e: e.affine_select(out=tmpf1[:], in_=tmpf1[:], pattern=[[1, 128]], compare_op=ALU.is_gt, fill=0.0,
                                                            base=0, channel_multiplier=-1), reads=["tmpf1"], writes=["tmpf1"])
                    p.op("dve", lambda e: e.tensor_copy(out=tris[:], in_=tmpf1[:]), reads=["tmpf1"], writes=["tris"])
                    gtmp_r = Ring(sb1, "gtmpC", 2, [128, NE], F32)
                    xnb_r = Ring(sb1, "xnbC", 2, [128, D], BF16)
                    ecap_i = sb1("ecap_i", [128, NE], I32)
                    ecap = sb1("ecap", [128, NE], F32)
                    p.op("pool", lambda e: e.iota(ecap_i[:], pattern=[[CAP, NE]], base=0, channel_multiplier=0), writes=["ecap_i"])
                    p.op("dve", lambda e: e.tensor_copy(out=ecap[:], in_=ecap_i[:]), reads=["ecap_i"], writes=["ecap"])
                    pq_r = Ring(sb1, "pq", 2, [128, NE], F32)
                    oh4_r = Ring(sb1, "oh4", 2, [128, 4, NE], F32)
                    pr4_r = Ring(sb1, "pr4", 2, [128, 4, NE], F32)
                    oh = sb1("oh", [128, NE], F32)
                    pr = sb1("pr", [128, NE], F32)
                    slf_r = Ring(sb1, "slf", 2, [128, 4], F32)
                    ulo_r = Ring(sb1, "uloC", 2, [128, D], BF16)
                    uloT_r = Ring(sb1, "uloT", 2, [128, 8, 128], BF16)
                    pTb_r = Ring(ps1, "pTb", 2, [128, 8, 128], BF16)
                    Wrh = sb1("Wrh", [128, 8, NE], BF16)
                    Wrl = sb1("Wrl", [128, 8, NE], BF16)
                    p.op("dve", lambda e: e.tensor_copy(out=Wrh[:], in_=Wr[:]), reads=["Wr"], writes=["Wrh"])
                    p.op("dve", lambda e: e.tensor_tensor(out=Wrl[:], in0=Wr[:], in1=Wrh[:], op=ALU.subtract), reads=["Wr", "Wrh"], writes=["Wrl"])
                    pLg_r = Ring(ps1, "pLg", 2, [128, 2, NE], F32)
                    lg_r = Ring(sb1, "lgC", 2, [128, NE], F32)
                    mx8_r = Ring(sb1, "mx8C", 2, [128, 8], F32)
                    msk_r = Ring(sb1, "mskC", 2, [128, NE], F32)
                    ex_r = Ring(sb1, "exC", 2, [128, NE], F32)
                    sm_r = Ring(sb1, "smC", 2, [128, 1], F32)
                    otv = OTd.rearrange("(c p) n -> p c n", p=128)
                    if stop_after == "C1a":
                        p.barrier()
                        p.op("sp", lambda e: e.dma_start(out=dbgH.rearrange("(t p) n -> p t n", p=128), in_=H[:]), reads=["H%d" % i_ for i_ in range(16)], dma="dbgH")
                        p.op("sp", lambda e: e.dma_start(out=dbgG.rearrange("(t p) n -> p t n", p=128), in_=Gt[:]), reads=["Gt%d" % i_ for i_ in range(16)], dma="dbgG")
                        p.finish_wait("sp"); p.emit(top); return nc
                    for G in range(4):
                        OT, kOT = OT_r.next()
                        p.op("sp", lambda e, OT=OT, G=G: e.dma_start(out=OT[:], in_=otv[:, :, G * 512:(G + 1) * 512]), writes=[kOT], dma=kOT)
                        rbs = []
                        for grp in range(2):
                            pss, kpss = pss_r.next()
                            for i in range(4):
                                c = grp * 4 + i
                                sq, ksq = sq_r.next()
                                p.op("act", lambda e, sq=sq, OT=OT, c=c: e.activation(out=sq[:], in_=OT[:, c, :], func=AF.Square), reads=[kOT], writes=[ksq])
                                p.op("pe", lambda e, sq=sq, pss=pss, i=i: e.matmul(out=pss[:], lhsT=ones[:], rhs=sq[:], start=(i == 0), stop=(i == 3)),
                                     reads=[ksq, "ones"], writes=[kpss])
                            rb, krb = rb_r.next()
                            rstd_from(rb[:], pss[:], 512, [kpss], [krb])
                            rbs.append((rb, krb))
                        for c in range(8):
                            rb, krb = rbs[c // 4]
                            p.op("dve", lambda e, c=c, rb=rb, OT=OT: e.scalar_tensor_tensor(out=MX[:, c, :], in0=OT[:, c, :], scalar=gcol[:, 5 + c:6 + c], in1=rb[:],
                                                                                        op0=ALU.mult, op1=ALU.mult),
                                 reads=[kOT, "gcol", krb], writes=["MX"])
                        def c1_tile(G, sub):
                            tile = G * 4 + sub
                            b_ = sub % 2
                            uhT, kuhT = uhT_r.tiles[b_], uhT_r.keys[b_]
                            uloT, kuloT = uloT_r.tiles[b_], uloT_r.keys[b_]
                            pq, kpq = pq_r.tiles[b_], pq_r.keys[b_]
                            slf, kslf = slf_r.tiles[b_], slf_r.keys[b_]
                            pLg2, kpLg = pLg_r.tiles[b_], pLg_r.keys[b_]
                            pLg = pLg2[:, 0, :]
                            pPos = pLg2[:, 1, :]
                            yield
                            xr, kxr = xr_r.next()
                            yield
                            p.op("sp", lambda e, xr=xr, tile=tile: e.dma_start(out=xr[:], in_=xo[tile * 128:(tile + 1) * 128, :]), writes=[kxr], dma=kxr)
                            yield
                            for half in range(2):
                                pA, kpA = pA_r.next()
                                for c in range(8):
                                    p.op("pe", lambda e, c=c, pA=pA, sub=sub, half=half: e.matmul(out=pA[:], lhsT=MX[:, c, sub * 128:(sub + 1) * 128],
                                                                                              rhs=Wo[:, c, half * 512:(half + 1) * 512], start=(c == 0), stop=(c == 7)),
                                         reads=["MX", "Wo"], writes=[kpA])
                                p.op("dve", lambda e, pA=pA, xr=xr, tile=tile, half=half: e.tensor_tensor(out=H[:, tile, half * 512:(half + 1) * 512], in0=pA[:],
                                                                                                      in1=xr[:, half * 512:(half + 1) * 512], op=ALU.add),
                                     reads=[kpA, kxr], writes=["H%d" % tile])
                            yield
                            yield
                            ssc, kss = ssc_r.next()
                            yield
                            p.op("act", lambda e, ssc=ssc, tile=tile: e.activation(out=junk[:], in_=H[:, tile, :], func=AF.Square, accum_out=ssc[:]),
                                 reads=["H%d" % tile], writes=["junkC", kss])
                            yield
                            rstd_from(ssc[:], ssc[:], D, [kss], [kss])
                            yield
                            u32, ku = u32_r.next()
                            yield
                            p.op("dve", lambda e, ssc=ssc, u32=u32, tile=tile: e.scalar_tensor_tensor(out=u32[:], in0=H[:, tile, :], scalar=ssc[:, 0:1], in1=gbc[:],
                                                                                                  op0=ALU.mult, op1=ALU.mult),
                                 reads=["H%d" % tile, kss, "gbc"], writes=[ku])
                            yield
                            xnb, kuhi = xnb_r.next()
                            yield
                            ulo, kulo = ulo_r.next()
                            yield
                            p.op("act", lambda e: e.copy(out=xnb[:], in_=u32[:]), reads=[ku], writes=[kuhi])
                            yield
                            p.op("pool", lambda e: e.dma_start(out=Ud[tile * 128:(tile + 1) * 128, :], in_=xnb[:]), reads=[kuhi], dma=kuhi + "s")
                            yield
                            p.op("dve", lambda e: e.tensor_tensor(out=ulo[:], in0=u32[:], in1=xnb[:], op=ALU.subtract), reads=[ku, kuhi], writes=[kulo])
                            yield
                            pT, kpT = pTb_r.next()
                            yield
                            for c in range(8):
                                p.op("pe", lambda e: e.transpose(out=pT[:, c, :], in_=xnb[:, c * 128:(c + 1) * 128], identity=ident[:]), reads=[kuhi, "ident"], writes=[kpT])
                            yield
                            p.op("dve", lambda e: e.tensor_copy(out=uhT[:], in_=pT[:]), reads=[kpT], writes=[kuhT])
                            yield
                            pT2, kpT2 = pTb_r.next()
                            yield
                            for c in range(8):
                                p.op("pe", lambda e: e.transpose(out=pT2[:, c, :], in_=ulo[:, c * 128:(c + 1) * 128], identity=ident[:]), reads=[kulo, "ident"], writes=[kpT2])
                            yield
                            p.op("act", lambda e: e.copy(out=uloT[:], in_=pT2[:]), reads=[kpT2], writes=[kuloT])
                            yield
                            n_ = 0
                            yield
                            for (A_, kA_, W_, kW_) in (("hi", kuhT, Wrh, "Wrh"), ("lo", kuloT, Wrh, "Wrh"), ("hi", kuhT, Wrl, "Wrl")):
                                for k in range(8):
                                    lh = uhT[:, k, :] if A_ == "hi" else uloT[:, k, :]
                                    p.op("pe", lambda e: e.matmul(out=pLg, lhsT=lh, rhs=W_[:, k, :], start=(n_ == 0), stop=(n_ == 23)),
                                         reads=[kA_, kW_], writes=[kpLg])
                                    n_ += 1
                            yield
                            lg, klg = lg_r.next()
                            yield
                            p.op("dve", lambda e, lg=lg: e.tensor_tensor(out=lg[:], in0=pLg, in1=brb[:], op=ALU.add), reads=[kpLg, "brb"], writes=[klg])
                            yield
                            mx8, kmx = mx8_r.next()
                            yield
                            p.op("dve", lambda e, lg=lg, mx8=mx8: e.max(out=mx8[:], in_=lg[:]), reads=[klg], writes=[kmx])
                            yield
                            msk, kmsk = msk_r.next()
                            yield
                            p.op("dve", lambda e, lg=lg, mx8=mx8, msk=msk: e.tensor_scalar(out=msk[:], in0=lg[:], scalar1=mx8[:, 3:4], scalar2=None, op0=ALU.is_ge),
                                 reads=[klg, kmx], writes=[kmsk])
                            yield
                            p.op("dve", lambda e, mx8=mx8: e.tensor_scalar(out=mx8[:, 7:8], in0=mx8[:, 0:1], scalar1=-1.0, scalar2=None, op0=ALU.mult),
                                 reads=[kmx, kmsk], writes=[kmx])
                            yield
                            ex, kex = ex_r.next()
                            yield
                            p.op("act", lambda e, lg=lg, mx8=mx8, ex=ex: e.activation(out=ex[:], in_=lg[:], func=AF.Exp, bias=mx8[:, 7:8]), reads=[klg, kmx], writes=[kex])
                            yield
                            sm_, ksm = sm_r.next()
                            yield
                            p.op("dve", lambda e, ex=ex, msk=msk: e.tensor_tensor(out=ex[:], in0=ex[:], in1=msk[:], op=ALU.mult), reads=[kex, kmsk], writes=[kex])
                            yield
                            p.op("dve", lambda e, ex=ex, sm_=sm_: e.reduce_sum(out=sm_[:], in_=ex[:], axis=mybir.AxisListType.X), reads=[kex], writes=[ksm])
                            yield
                            p.op("dve", lambda e, sm_=sm_: e.reciprocal(out=sm_[:], in_=sm_[:]), reads=[ksm], writes=[ksm])
                            yield
                            p.op("dve", lambda e, ex=ex, sm_=sm_, tile=tile: e.tensor_scalar(out=Gt[:, tile, :], in0=ex[:], scalar1=sm_[:, 0:1], scalar2=None, op0=ALU.mult),
                                 reads=[kex, ksm], writes=["Gt%d" % tile])
                            yield
                            p.op("dve", lambda e: e.tensor_copy(out=maskb[:, tile, :], in_=msk[:]), reads=[kmsk], writes=["maskb%d" % tile])
                            yield
                            gtmp, kgtmp = gtmp_r.next()
                            yield
                            p.op("pe", lambda e: e.matmul(out=pPos, lhsT=tris[:], rhs=maskb[:, tile, :], start=True, stop=(tile == 0)),
                                 reads=["tris", "maskb%d" % tile], writes=[kpLg])
                            yield
                            for j_ in range(tile):
                                p.op("pe", lambda e: e.matmul(out=pPos, lhsT=ones[:], rhs=maskb[:, j_, :], start=False, stop=(j_ == tile - 1)),
                                     reads=["ones", "maskb%d" % j_], writes=[kpLg])
                            yield
                            p.op("dve", lambda e: e.scalar_tensor_tensor(out=gtmp[:], in0=pPos, scalar=1.0, in1=msk[:], op0=ALU.add, op1=ALU.mult),
                                 reads=[kpLg, kmsk, kgtmp], writes=[kgtmp])
                            yield
                            p.op("dve", lambda e: e.tensor_scalar(out=posm[:, tile, :], in0=gtmp[:], scalar1=-1.0, scalar2=None, op0=ALU.add),
                                 reads=[kgtmp], writes=["posm%d" % tile])
                            yield
                            p.op("dve", lambda e: e.scalar_tensor_tensor(out=pq[:], in0=pPos, scalar=float(CAP - 1), in1=ecap[:], op0=ALU.min, op1=ALU.add),
                                 reads=[kpLg, "ecap"], writes=[kpq])
                            yield
                            yield
                            p.op("dve", lambda e: e.scalar_tensor_tensor(out=gtmp[:], in0=pPos, scalar=float(CAP) - 0.5, in1=Gt[:, tile, :], op0=ALU.is_lt, op1=ALU.mult),
                                 reads=[kpLg, "Gt%d" % tile, kgtmp], writes=[kgtmp])
                            yield
                            oh4, koh4 = oh4_r.tiles[b_], oh4_r.keys[b_]
                            pr4, kpr4 = pr4_r.tiles[b_], pr4_r.keys[b_]
                            lg_b = lg[:].unsqueeze(1).to_broadcast([128, 4, NE])
                            mx_b = mx8[:, 0:4].unsqueeze(2).to_broadcast([128, 4, NE])
                            p.op("dve", lambda e: e.tensor_tensor(out=oh4[:], in0=lg_b, in1=mx_b, op=ALU.is_equal), reads=[klg, kmx], writes=[koh4])
                            yield
                            p.op("dve", lambda e: e.tensor_tensor(out=pr4[:], in0=oh4[:], in1=pq[:].unsqueeze(1).to_broadcast([128, 4, NE]), op=ALU.mult),
                                 reads=[koh4, kpq], writes=[kpr4])
                            yield
                            p.op("dve", lambda e: e.reduce_sum(out=slf[:, 0:4], in_=pr4[:], axis=mybir.AxisListType.X), reads=[kpr4], writes=[kslf])
                            yield
                            p.op("dve", lambda e: e.tensor_tensor(out=pr4[:], in0=oh4[:], in1=gtmp[:].unsqueeze(1).to_broadcast([128, 4, NE]), op=ALU.mult),
                                 reads=[koh4, kgtmp, kpr4], writes=[kpr4])
                            yield
                            p.op("dve", lambda e: e.reduce_sum(out=gj[:, tile, :], in_=pr4[:], axis=mybir.AxisListType.X), reads=[kpr4], writes=["gj%d" % tile])
                            yield
                            p.op("dve", lambda e: e.tensor_copy(out=slI[:, tile, :], in_=slf[:]), reads=[kslf], writes=["slI%d" % tile])

                        for pair in range(2):
                            gens = [c1_tile(G, pair * 2), c1_tile(G, pair * 2 + 1)]
                            while gens:
                                for g_ in list(gens):
                                    try:
                                        next(g_)
                                    except StopIteration:
                                        gens.remove(g_)
                    p.barrier()
                    if stop_after == "C1":
                        if debug:
                            p.op("sp", lambda e: e.dma_start(out=dbgS.rearrange("(t p) n -> p t n", p=128), in_=slI[:]), reads=["slI%d" % i_ for i_ in range(16)], dma="dbgS")
                            p.op("sp", lambda e: e.dma_start(out=dbgJ.rearrange("(t p) n -> p t n", p=128), in_=gj[:]), reads=["gj%d" % i_ for i_ in range(16)], dma="dbgJ")
                            p.op("sp", lambda e: e.dma_start(out=dbgH.rearrange("(t p) n -> p t n", p=128), in_=H[:]), reads=["H%d" % i_ for i_ in range(16)], dma="dbgH")
                            p.op("sp", lambda e: e.dma_start(out=dbgG.rearrange("(t p) n -> p t n", p=128), in_=Gt[:]), reads=["Gt%d" % i_ for i_ in range(16)], dma="dbgG")
                        p.finish_wait("sp"); p.emit(top); return nc
                s2 = ExitStack()
                with s2:
                    sb2, ps2 = mk_alloc(s2)
                    W_r = Ring(sb2, "Wx", 6, [128, 8, 512], BF16)
                    bd_r = Ring(sb2, "bdn", 2, [1, D], BF16)
                    pGL_r = Ring(ps2, "pGL", 4, [128, 512], F32)
                    pA_r = Ring(ps2, "pA", 2, [128, 512], F32)
                    pTs = ps2("pTs", [128, 8, 128], BF16)
                    ptk = ps2("ptk", [128, 8], F32)
                    gl_r = Ring(sb2, "gl", 1, [128, CAP], F32)
                    sg_r = Ring(sb2, "sg", 1, [128, CAP], F32)
                    Sel = sb2("Sel", [128, 16, CAP], BF16)
                    Xe_r = Ring(sb2, "Xe", 2, [128, NS, D], BF16)
                    XeT = sb2("XeT", [128, 8, CAP], BF16)
                    aT = sb2("aTs", [128, 8, CAP], BF16)
                    Yst_r = Ring(sb2, "Yst", 2, [128, D], F32)
                    tks = sb2("tks", [128, 8], F32)
                    tkf = sb2("tkf", [128, 4], F32)
                    tkI_r = Ring(sb2, "tkI", 2, [128, 4], I32)
                    iota_i = sb2("iota_i", [128, CAP], I32)
                    iota_f = sb2("iota_f", [128, CAP], F32)
                    p.op("pool", lambda e: e.iota(iota_i[:], pattern=[[1, CAP]], base=0, channel_multiplier=0), writes=["iota_i"])
                    p.op("dve", lambda e: e.tensor_copy(out=iota_f[:], in_=iota_i[:]), reads=["iota_i"], writes=["iota_f"])
                    tid = sb2("tid", [128, 16], I32)
                    tidx = sb2("tidx", [128, 16], I32)
                    tidhl = sb2("tidhl", [128, 16, 2], BF16)
                    p.op("pool", lambda e: e.iota(tid[:], pattern=[[128, 16]], base=0, channel_multiplier=1), writes=["tid"])
                    p.op("dve", lambda e: e.tensor_scalar(out=tidx[:], in0=tid[:], scalar1=6, scalar2=None, op0=ALU.arith_shift_right), reads=["tid"], writes=["tidx"])
                    p.op("dve", lambda e: e.tensor_copy(out=tidhl[:, :, 0], in_=tidx[:]), reads=["tidx"], writes=["tidhl"])
                    p.op("dve", lambda e: e.tensor_scalar(out=tidx[:], in0=tid[:], scalar1=63, scalar2=None, op0=ALU.bitwise_and), reads=["tid", "tidx", "tidhl"], writes=["tidx"])
                    p.op("dve", lambda e: e.tensor_copy(out=tidhl[:, :, 1], in_=tidx[:]), reads=["tidx", "tidhl"], writes=["tidhl"])
                    bg3 = bgT[:].rearrange("p (e c) -> p e c", c=16)
                    p.op("dve", lambda e: e.tensor_scalar(out=bg3[:, :, 8:16], in0=bg3[:, :, 8:16], scalar1=1.0, scalar2=None, op0=ALU.add),
                         reads=["bgT"], writes=["bgT"])

                    def load_w(src, slot):
                        Wt, kW = W_r.tiles[slot], W_r.keys[slot]
                        p.op("pool", lambda e: e.dma_start(out=Wt[:], in_=src.rearrange("(c p) n -> p c n", p=128)), writes=[kW], dma=kW)
                        return Wt, kW

                    def load_bd(ex_):
                        bd, kbd = bd_r.next()
                        p.op("pool", lambda e: e.dma_start(out=bd[:], in_=b_dn[ex_:ex_ + 1, :]), writes=[kbd], dma=kbd)
                        return bd, kbd

                    xe_of = {}

                    def sel_build(ex_, tiles):
                        for tile in tiles:
                            p.op("dve", lambda e: e.tensor_scalar(out=Sel[:, tile, :], in0=iota_f[:], scalar1=posm[:, tile, ex_:ex_ + 1], scalar2=None, op0=ALU.is_equal),
                                 reads=["iota_f", "posm%d" % tile], writes=["Sel%d" % tile])

                    def dispatch(ex_, build=True):
                        if build:
                            sel_build(ex_, range(16))
                        for s_ in range(NS):
                            for tile in range(16):
                                p.op("pe", lambda e: e.matmul(out=ptk[:, 2 * s_:2 * s_ + 2], lhsT=Sel[:, tile, s_ * 128:(s_ + 1) * 128], rhs=tidhl[:, tile, :],
                                                              start=(tile == 0), stop=(tile == 15)), reads=["Sel%d" % tile, "tidhl"], writes=["ptk"])
                        p.op("dve", lambda e: e.tensor_copy(out=tks[:, 0:2 * NS], in_=ptk[:, 0:2 * NS]), reads=["ptk"], writes=["tks"])
                        tk3 = tks[:, 0:2 * NS].rearrange("p (s t) -> p s t", t=2)
                        p.op("dve", lambda e: e.scalar_tensor_tensor(out=tkf[:, 0:NS], in0=tk3[:, :, 0], scalar=64.0, in1=tk3[:, :, 1], op0=ALU.mult, op1=ALU.add),
                             reads=["tks"], writes=["tkf"])
                        tkI, ktkI = tkI_r.next()
                        p.op("dve", lambda e: e.tensor_copy(out=tkI[:, 0:NS], in_=tkf[:, 0:NS]), reads=["tkf"], writes=[ktkI])
                        Xe, kXe = Xe_r.next()
                        for s_ in range(NS):
                            p.op("pool", lambda e: e.indirect_dma_start(out=Xe[:, s_, :], out_offset=None, in_=Ud[:, :],
                                                                         in_offset=bass.IndirectOffsetOnAxis(ap=tkI[:, s_:s_ + 1], axis=0)),
                                 reads=[ktkI], writes=[kXe], dma=kXe)
                        xe_of[ex_] = (Xe, kXe)

                    def transposes_s(ex_, s_):
                        Xe, kXe = xe_of[ex_]
                        for k in range(8):
                            p.op("pe", lambda e: e.transpose(out=pTs[:, k, :], in_=Xe[:, s_, k * 128:(k + 1) * 128], identity=ident[:]),
                                 reads=[kXe, "ident"], writes=["pTs"])
                        if s_ % 2 == 0:
                            p.op("act", lambda e: e.copy(out=XeT[:, :, s_ * 128:(s_ + 1) * 128], in_=pTs[:]), reads=["pTs"], writes=["XeT"])
                        else:
                            p.op("dve", lambda e: e.tensor_copy(out=XeT[:, :, s_ * 128:(s_ + 1) * 128], in_=pTs[:]), reads=["pTs"], writes=["XeT"])
                        if s_ == NS - 1:
                            xe_of.pop(ex_)

                    def transposes(ex_):
                        for s_ in range(NS):
                            transposes_s(ex_, s_)

                    def gu_stage(ex_, st, Wg, kWg, Wl, kWl, sel_for=None):
                        for mc in range(4):
                            if sel_for is not None:
                                sel_build(sel_for, range(mc * 4, mc * 4 + 4))
                            c = st * 4 + mc
                            pG, kpG = pGL_r.next()
                            pLn, kpLn = pGL_r.next()
                            for k in range(8):
                                p.op("pe", lambda e: e.matmul(out=pG[:, 0:CAP], lhsT=Wg[:, k, mc * 128:(mc + 1) * 128], rhs=XeT[:, k, :],
                                                              start=(k == 0), stop=(k == 7)), reads=[kWg, "XeT"], writes=[kpG])
                            for k in range(8):
                                p.op("pe", lambda e: e.matmul(out=pLn[:, 0:CAP], lhsT=Wl[:, k, mc * 128:(mc + 1) * 128], rhs=XeT[:, k, :],
                                                              start=(k == 0), stop=(k == 7)), reads=[kWl, "XeT"], writes=[kpLn])
                            gl, kgl = gl_r.next()
                            sg, ksg = sg_r.next()
                            bgc = ex_ * 16 + c
                            blc = ex_ * 16 + 8 + c
                            kaT = "aT_%d" % st
                            p.op("dve", lambda e: e.tensor_scalar(out=gl[:], in0=pG[:, 0:CAP], scalar1=bgT[:, bgc:bgc + 1], scalar2=7.0, op0=ALU.add, op1=ALU.min),
                                 reads=[kpG, "bgT"], writes=[kgl])
                            p.op("act", lambda e: e.activation(out=sg[:], in_=gl[:], func=AF.Sigmoid, scale=1.702), reads=[kgl], writes=[ksg])
                            p.op("dve", lambda e: e.tensor_tensor(out=sg[:], in0=sg[:], in1=gl[:], op=ALU.mult), reads=[kgl, ksg], writes=[ksg])
                            p.op("dve", lambda e: e.tensor_scalar(out=gl[:], in0=pLn[:, 0:CAP], scalar1=bgT[:, blc:blc + 1], scalar2=-6.0, op0=ALU.add, op1=ALU.max),
                                 reads=[kpLn, "bgT", kgl], writes=[kgl])
                            p.op("dve", lambda e: e.scalar_tensor_tensor(out=aT[:, c, :], in0=gl[:], scalar=8.0, in1=sg[:], op0=ALU.min, op1=ALU.mult),
                                 reads=[ksg, kgl], writes=[kaT])

                    def dn_stage(ex_, d0, d1, bd, kbd, nxt=None):
                        for s_ in range(NS):
                            if nxt is not None:
                                transposes_s(nxt, s_)
                            Yst, kY = Yst_r.next()
                            for half in range(2):
                                Wd, kWd = (d0, d1)[half]
                                pA, kpA = pA_r.next()
                                for c in range(8):
                                    p.op("pe", lambda e: e.matmul(out=pA[:], lhsT=aT[:, c, s_ * 128:(s_ + 1) * 128], rhs=Wd[:, c, :], start=(c == 0), stop=False),
                                         reads=["aT_%d" % (c // 4), kWd], writes=[kpA])
                                p.op("pe", lambda e: e.matmul(out=pA[:], lhsT=ones[0:1, :], rhs=bd[0:1, half * 512:(half + 1) * 512], start=False, stop=True),
                                     reads=["ones", kbd], writes=[kpA])
                                p.op("act", lambda e: e.copy(out=Yst[:, half * 512:(half + 1) * 512], in_=pA[:]), reads=[kpA], writes=[kY])
                            r0 = ex_ * CAP + s_ * 128
                            p.op("sp", lambda e: e.dma_start(out=Yd[r0:r0 + 128, :], in_=Yst[:]), reads=[kY], dma=kY + "s")

                    def loads_gl0(ex_):
                        return load_w(w_gu[ex_, :, 0:512], 0), load_w(w_gu[ex_, :, 1024:1536], 1)

                    def loads_gl1(ex_):
                        return load_w(w_gu[ex_, :, 512:1024], 2), load_w(w_gu[ex_, :, 1536:2048], 3)

                    def loads_d(ex_):
                        return load_w(w_dn[ex_, :, 0:512], 4), load_w(w_dn[ex_, :, 512:1024], 5), load_bd(ex_)

                    g0, l0 = loads_gl0(0)
                    g1, l1 = loads_gl1(0)
                    d0, d1, (bd, kbd) = loads_d(0)
                    dispatch(0)
                    dispatch(1)
                    transposes(0)
                    for ex_ in range(NE):
                        gu_stage(ex_, 0, g0[0], g0[1], l0[0], l0[1], sel_for=(ex_ + 2 if ex_ + 2 < NE else None))
                        if ex_ + 1 < NE:
                            g0n, l0n = loads_gl0(ex_ + 1)
                        gu_stage(ex_, 1, g1[0], g1[1], l1[0], l1[1])
                        if ex_ + 2 < NE:
                            dispatch(ex_ + 2, build=False)
                        if ex_ + 1 < NE:
                            g1n, l1n = loads_gl1(ex_ + 1)
                        dn_stage(ex_, d0, d1, bd, kbd, nxt=(ex_ + 1 if ex_ + 1 < NE else None))
                        if ex_ + 1 < NE:
                            d0, d1, (bd, kbd) = loads_d(ex_ + 1)
                            g0, l0, g1, l1 = g0n, l0n, g1n, l1n
                    p.barrier()
                    Yg_r = Ring(sb2, "Yg", 4, [128, D], F32)
                    for tile in range(16):
                        hk = "H%d" % tile
                        for j_ in range(4):
                            Yg, kYg = Yg_r.next()
                            p.op("pool", lambda e: e.indirect_dma_start(out=Yg[:, :], out_offset=None, in_=Yd[:, :],
                                                                         in_offset=bass.IndirectOffsetOnAxis(ap=slI[:, tile, j_:j_ + 1], axis=0)),
                                 reads=["slI%d" % tile], writes=[kYg], dma=kYg)
                            p.op("dve", lambda e: e.scalar_tensor_tensor(out=H[:, tile, :], in0=Yg[:], scalar=gj[:, tile, j_:j_ + 1], in1=H[:, tile, :], op0=ALU.mult, op1=ALU.add),
                                 reads=[kYg, "gj%d" % tile, hk], writes=[hk])
                    p.barrier()
                    if stop_after == "C2":
                        if debug:
                            p.op("sp", lambda e: e.dma_start(out=dbgH.rearrange("(t p) n -> p t n", p=128), in_=H[:]), reads=["H%d" % i_ for i_ in range(16)], dma="dbgH")
                            p.op("sp", lambda e: e.dma_start(out=dbgG.rearrange("(t p) n -> p t n", p=128), in_=Gt[:]), reads=["Gt%d" % i_ for i_ in range(16)], dma="dbgG")
                        p.finish_wait("sp"); p.emit(top); return nc
                s3 = ExitStack()
                with s3:
                    sb3, ps3 = mk_alloc(s3)
                    gbc = sb3("gbc3", [128, D], F32)
                    junk = sb3("junkC3", [128, D], BF16)
                    Wpg = sb3("Wpg", [128, 8, D], BF16)
                    Wpp = sb3("Wpp", [128, 2, D], BF16)
                    p.op("pool", lambda e: e.dma_start(out=Wpg[:], in_=w_pg.rearrange("(c p) n -> p c n", p=128)), writes=["Wpg"], dma="Wpg")
                    p.op("pool", lambda e: e.dma_start(out=Wpp[:], in_=w_pp.rearrange("(c p) n -> p c n", p=128)), writes=["Wpp"], dma="Wpp")
                    gfin = sb3("gfin", [128, D], F32)
                    p.op("sp", lambda e: e.dma_start(out=gbc[:], in_=g_ple.partition_broadcast(128)), writes=["gbc"], dma="gbc3")
                    p.op("sp", lambda e: e.dma_start(out=gfin[:], in_=g_final.partition_broadcast(128)), writes=["gfin"], dma="gfin")
                    ss3_r = Ring(sb3, "ss3", 2, [128, 1], F32)
                    u3_r = Ring(sb3, "u3", 2, [128, D], BF16)
                    pT3_r = Ring(ps3, "pT3", 2, [128, 8, 128], BF16)
                    u3T_r = Ring(sb3, "u3T", 2, [128, 8, 128], BF16)
                    pp_r = Ring(sb3, "ppl", 2, [128, 256], F32)
                    ppb_r = Ring(sb3, "ppb", 2, [128, 256], BF16)
                    ppT_r = Ring(sb3, "ppT", 2, [128, 2, 128], BF16)
                    pg_r = Ring(ps3, "pg3", 2, [128, 512], F32)
                    pj_r = Ring(ps3, "pj3", 2, [128, 512], F32)
                    sg3_r = Ring(sb3, "sg3", 2, [128, 512], F32)
                    o_r = Ring(sb3, "o3", 2, [128, D], F32)
                    def c3_tile(tile):
                        hk = "H%d" % tile
                        yield
                        ss3, kss = ss3_r.next()
                        yield
                        p.op("act", lambda e, ss3=ss3, tile=tile: e.activation(out=junk[:], in_=H[:, tile, :], func=AF.Square, accum_out=ss3[:]), reads=[hk], writes=["junkC", kss])
                        yield
                        rstd_from(ss3[:], ss3[:], D, [kss], [kss])
                        yield
                        u3, ku3 = u3_r.next()
                        yield
                        p.op("dve", lambda e, ss3=ss3, u3=u3, tile=tile: e.scalar_tensor_tensor(out=u3[:], in0=H[:, tile, :], scalar=ss3[:, 0:1], in1=gbc[:], op0=ALU.mult, op1=ALU.mult),
                             reads=[hk, kss, "gbc"], writes=[ku3])
                        yield
                        pT, kpT = pT3_r.next()
                        yield
                        for c in range(8):
                            p.op("pe", lambda e, c=c, pT=pT, u3=u3: e.transpose(out=pT[:, c, :], in_=u3[:, c * 128:(c + 1) * 128], identity=ident[:]), reads=[ku3, "ident"], writes=[kpT])
                        yield
                        u3T, ku3T = u3T_r.next()
                        yield
                        p.op("act", lambda e, pT=pT, u3T=u3T: e.copy(out=u3T[:], in_=pT[:]), reads=[kpT], writes=[ku3T])
                        yield
                        pp, kpp = pp_r.next()
                        yield
                        p.op("sp", lambda e, pp=pp, tile=tile: e.dma_start(out=pp[:], in_=po[tile * 128:(tile + 1) * 128, :]), writes=[kpp], dma=kpp)
                        yield
                        ppb, kppb = ppb_r.next()
                        yield
                        p.op("pool", lambda e, pp=pp, ppb=ppb: e.tensor_copy(out=ppb[:], in_=pp[:]), reads=[kpp], writes=[kppb])
                        yield
                        pT2, kpT2 = pT3_r.next()
                        yield
                        for c in range(2):
                            p.op("pe", lambda e, c=c, pT2=pT2, ppb=ppb: e.transpose(out=pT2[:, c, :], in_=ppb[:, c * 128:(c + 1) * 128], identity=ident[:]), reads=[kppb, "ident"], writes=[kpT2])
                        yield
                        ppT, kppT = ppT_r.next()
                        yield
                        p.op("act", lambda e, pT2=pT2, ppT=ppT: e.copy(out=ppT[:], in_=pT2[:, 0:2, :]), reads=[kpT2], writes=[kppT])
                        yield
                        for half in range(2):
                            pg, kpg = pg_r.next()
                            pj, kpj = pj_r.next()
                            for c in range(8):
                                p.op("pe", lambda e, c=c, pg=pg, u3T=u3T, half=half: e.matmul(out=pg[:], lhsT=u3T[:, c, :], rhs=Wpg[:, c, half * 512:(half + 1) * 512], start=(c == 0), stop=(c == 7)),
                                     reads=[ku3T, "Wpg"], writes=[kpg])
                            for c in range(2):
                                p.op("pe", lambda e, c=c, pj=pj, ppT=ppT, half=half: e.matmul(out=pj[:], lhsT=ppT[:, c, :], rhs=Wpp[:, c, half * 512:(half + 1) * 512], start=(c == 0), stop=(c == 1)),
                                     reads=[kppT, "Wpp"], writes=[kpj])
                            sg, ksg = sg3_r.next()
                            p.op("act", lambda e, sg=sg, pg=pg: e.activation(out=sg[:], in_=pg[:], func=AF.Sigmoid), reads=[kpg], writes=[ksg])
                            p.op("dve", lambda e, sg=sg, pj=pj: e.tensor_tensor(out=sg[:], in0=sg[:], in1=pj[:], op=ALU.mult), reads=[ksg, kpj], writes=[ksg])
                            p.op("dve", lambda e, sg=sg, tile=tile, half=half: e.tensor_tensor(out=H[:, tile, half * 512:(half + 1) * 512], in0=H[:, tile, half * 512:(half + 1) * 512], in1=sg[:], op=ALU.add),
                                 reads=[ksg, hk], writes=[hk])
                        yield
                        ss4, kss4 = ss3_r.next()
                        yield
                        p.op("act", lambda e, ss4=ss4, tile=tile: e.activation(out=junk[:], in_=H[:, tile, :], func=AF.Square, accum_out=ss4[:]), reads=[hk], writes=["junkC", kss4])
                        yield
                        rstd_from(ss4[:], ss4[:], D, [kss4], [kss4])
                        yield
                        ot, kot = o_r.next()
                        yield
                        p.op("dve", lambda e, ss4=ss4, ot=ot, tile=tile: e.scalar_tensor_tensor(out=ot[:], in0=H[:, tile, :], scalar=ss4[:, 0:1], in1=gfin[:], op0=ALU.mult, op1=ALU.mult),
                             reads=[hk, kss4, "gfin"], writes=[kot])
                        yield
                        p.op("sp", lambda e, ot=ot, tile=tile: e.dma_start(out=yo[tile * 128:(tile + 1) * 128, :], in_=ot[:]), reads=[kot], writes=["yo"], dma=kot + "s")

                    for pair in range(8):
                        gens = [c3_tile(pair * 2), c3_tile(pair * 2 + 1)]
                        while gens:
                            for g_ in list(gens):
                                try:
                                    next(g_)
                                except StopIteration:
                                    gens.remove(g_)
        p.finish_wait("sp")
        p.emit(top)
    return nc


_CACHE = {}


def _perm(j):
    idx = []
    for t in range(4):
        for blk in ORDER[j]:
            b0 = (8 * t + blk) * 128
            idx.append(np.arange(b0, b0 + 128))
    return np.concatenate(idx)


def kernel(x, p, positions, w_in, g_attn, g_cq, w_uq, g_ckv, w_ukv, g_out_mla, g_out_sb, w_o,
           g_moe, w_router, b_router, w_gu, b_gu, w_dn, b_dn, g_ple, w_ple_gate, w_ple_proj, g_final):
    if "nc" not in _CACHE:
        _CACHE["nc"] = build_program()
    nc = _CACHE["nc"]
    in_maps, perms = make_in_maps(x, p, positions, w_in, g_attn, g_cq, w_uq, g_ckv, w_ukv, g_out_mla, g_out_sb, w_o,
                                  g_moe, w_router, b_router, w_gu, b_gu, w_dn, b_dn, g_ple, w_ple_gate, w_ple_proj, g_final)
    res = run_bass_kernel_spmd(nc, in_maps, core_ids=list(range(8)))
    out = np.empty((4, S, D), np.float32)
    for c in range(8):
        b, j = c // 2, c % 2
        out[b, perms[j]] = np.asarray(res.results[c]["yo"])
    return out


def make_in_maps(x, p, positions, w_in, g_attn, g_cq, w_uq, g_ckv, w_ukv, g_out_mla, g_out_sb, w_o,
                 g_moe, w_router, b_router, w_gu, b_gu, w_dn, b_dn, g_ple, w_ple_gate, w_ple_proj, g_final):
    f = lambda a: np.ascontiguousarray(np.asarray(a))
    x = f(x); p = f(p); positions = f(positions)
    invf = np.zeros((128, 1), np.float32)
    fr = (10000.0 ** (-np.arange(0, 32, 2, dtype=np.float32) / 32.0)).astype(np.float32)
    invf[0:16, 0] = fr
    invf[16:32, 0] = fr
    shared = {
        "invf": invf,
        "w_in": f(w_in[0]), "g_attn": f(g_attn[0:1]), "g_cq": f(g_cq[0:1]), "w_uq": f(w_uq[0]),
        "g_ckv": f(g_ckv[0:1]), "w_ukv": f(w_ukv[0]),
        "g_out": f(np.concatenate([np.asarray(g_out_mla[0]), np.asarray(g_out_sb[0])])[None, :]),
        "w_o": f(w_o[0]), "g_moe": f(g_moe[0:1]), "w_router": f(w_router[0]), "b_router": f(b_router[0:1]),
        "w_gu": f(w_gu[0]), "b_gu": f(np.asarray(b_gu[0]).reshape(NE * 16, 128)), "w_dn": f(w_dn[0]), "b_dn": f(b_dn[0]),
        "g_ple": f(g_ple[0:1]), "w_pg": f(w_ple_gate[0]), "w_pp": f(w_ple_proj[0]), "g_final": f(np.asarray(g_final)[None, :]),
    }
    in_maps = []
    perms = [_perm(0), _perm(1)]
    for c in range(8):
        b, j = c // 2, c % 2
        pm = perms[j]
        qr = np.concatenate([np.arange(blk * 128, blk * 128 + 128) for blk in ORDER[j]]).astype(np.float32)[None, :]
        m = dict(shared)
        m["xa"] = x[b]
        m["xo"] = f(x[b][pm])
        m["po"] = f(p[0, b][pm])
        m["posa"] = f(positions[b:b + 1].astype(np.int32))
        m["poso"] = f(positions[b:b + 1, pm].astype(np.int32))
        m["qrel"] = f(qr)
        in_maps.append(m)
    return in_maps, perms
```

```python
from contextlib import ExitStack
import numpy as np
import concourse.bass as bass
import concourse.mybir as mybir
from concourse.bass_utils import run_bass_kernel_spmd

F32 = mybir.dt.float32
BF16 = mybir.dt.bfloat16
I32 = mybir.dt.int32
AF = mybir.ActivationFunctionType
ALU = mybir.AluOpType

ENGS = ("pe", "act", "dve", "pool", "sp")
S = 4096
D = 1024
NE = 32
ORDER = ([6, 5, 3, 0], [7, 4, 2, 1])
NDIAG = [512, 512, 384, 384, 256, 256, 128, 128]
NEG = -30000.0
EPS = 1e-6


class Prog:
    def __init__(self, nc):
        self.nc = nc
        self.ops = {e: [] for e in ENGS}
        self.vcs = {}
        self.cur = {e: {} for e in ENGS}
        self.last_w = {}
        self.readers = {}
        self.excl = set()

    def op(self, eng, fn, reads=(), writes=(), dma=None):
        rec_ = _Rec()
        fn(rec_)
        assert len(rec_.calls) == 1
        fn = rec_.calls[0]
        clk = ("dma:" + dma) if dma else eng
        deps = []
        reads = list(reads)
        writes = list(writes)
        for k in reads:
            if k in self.excl and k not in writes:
                writes.append(k)
        for k in reads:
            lw = self.last_w.get(k)
            if lw:
                deps.append(lw)
        for k in writes:
            lw = self.last_w.get(k)
            if lw:
                deps.append(lw)
            for c, i in self.readers.get(k, {}).items():
                deps.append((c, i))
        cur = self.cur[eng]
        wmax = {}
        for (c, i) in deps:
            if c == "pe" and eng == "pe" and not dma:
                continue
            if cur.get(c, 0) >= i:
                continue
            wmax[c] = max(wmax.get(c, 0), i)
            for c2, i2 in self.vcs[c][i - 1].items():
                if cur.get(c2, 0) < i2:
                    cur[c2] = i2
            if cur.get(c, 0) < i:
                cur[c] = i
        vc = dict(cur)
        lst = self.vcs.setdefault(clk, [])
        lst.append(vc)
        idx = len(lst)
        vc[clk] = idx
        rec = {"fn": fn, "waits": wmax, "clk": clk, "idx": idx}
        self.ops[eng].append(rec)
        for k in reads:
            self.readers.setdefault(k, {})[clk] = idx
        for k in writes:
            self.last_w[k] = (clk, idx)
            self.readers[k] = {}
        return rec

    def finish_wait(self, eng):
        waits = {}
        for c, l in self.vcs.items():
            if len(l) and self.cur[eng].get(c, 0) < len(l):
                waits[c] = len(l)
                self.cur[eng][c] = len(l)
        self.ops[eng].append({"fn": None, "waits": waits, "clk": None, "idx": None})

    def barrier(self):
        for e in ENGS:
            self.finish_wait(e)
        full = {c: len(l) for c, l in self.vcs.items()}
        for e in ENGS:
            self.cur[e] = dict(full)

    def emit(self, stack):
        nc = self.nc
        waited = {}
        for e in ENGS:
            for r in self.ops[e]:
                for c, i in r["waits"].items():
                    waited.setdefault(c, set()).add(i)
        sems, semval = {}, {}
        for c, l in self.vcs.items():
            if c not in waited:
                continue
            sems[c] = stack.enter_context(nc.semaphore("s_" + c.replace(":", "_")))
            isd = c.startswith("dma:")
            v, m = 0, {}
            for i in range(1, len(l) + 1):
                if isd or i in waited[c]:
                    v += 16 if isd else 1
                    m[i] = v
            semval[c] = m
        block = stack.enter_context(nc.Block())
        engobj = {"pe": "tensor", "act": "scalar", "dve": "vector", "pool": "gpsimd", "sp": "sync"}

        def make(e):
            def body(eng):
                for r in self.ops[e]:
                    for c, i in r["waits"].items():
                        eng.wait_ge(sems[c], semval[c][i])
                    if r["fn"] is None:
                        continue
                    name, a, k = r["fn"]
                    ins = getattr(eng, name)(*a, **k)
                    c, i = r["clk"], r["idx"]
                    if c in sems and i in semval[c]:
                        ins.then_inc(sems[c], 16 if c.startswith("dma:") else 1)
            return body

        for e in ENGS:
            if self.ops[e]:
                getattr(block, engobj[e])(make(e))


class _Rec:
    def __init__(self):
        self.calls = []

    def __getattr__(self, name):
        def f(*a, **k):
            self.calls.append((name, a, k))
        return f


class Ring:
    def __init__(self, alloc, name, n, shape, dtype):
        self.tiles = [alloc("%s%d" % (name, i), shape, dtype) for i in range(n)]
        self.keys = ["%s%d" % (name, i) for i in range(n)]
        self.i = 0

    def next(self):
        t, k = self.tiles[self.i % len(self.tiles)], self.keys[self.i % len(self.tiles)]
        self.i += 1
        return t, k


class _Stop(Exception):
    pass


def build_program(stop_after=None, debug=False):
    nc = bass.Bass("TRN2", target_bir_lowering=False)

    def din(name, shape, dt=F32):
        return nc.dram_tensor(name, list(shape), dt, kind="ExternalInput").ap()

    def dscr(name, shape, dt=BF16):
        return nc.dram_tensor(name, list(shape), dt, kind="ExternalOutput" if debug else "Internal").ap()

    xa = din("xa", [S, D])
    xo = din("xo", [2048, D])
    po = din("po", [2048, 256])
    posa = din("posa", [1, S], I32)
    poso = din("poso", [1, 2048], I32)
    qrel = din("qrel", [1, 512])
    invf = din("invf", [128, 1])
    w_in = din("w_in", [D, 2208])
    g_attn = din("g_attn", [1, D])
    g_cq = din("g_cq", [1, 384])
    w_uq = din("w_uq", [384, 768])
    g_ckv = din("g_ckv", [1, 256])
    w_ukv = din("w_ukv", [256, 1024])
    g_out = din("g_out", [1, 1024])
    w_o = din("w_o", [D, D])
    g_moe = din("g_moe", [1, D])
    w_router = din("w_router", [D, NE])
    b_router = din("b_router", [1, NE])
    NEd = NE if stop_after in (None, "C2") else 1
    w_gu = din("w_gu", [NEd, D, 2048])
    b_gu = din("b_gu", [NE * 16, 128])
    w_dn = din("w_dn", [NEd, D, D])
    b_dn = din("b_dn", [NE, D])
    g_ple = din("g_ple", [1, D])
    w_pg = din("w_pg", [D, D])
    w_pp = din("w_pp", [256, D])
    g_final = din("g_final", [1, D])
    yo = nc.dram_tensor("yo", [2048, D], F32, kind="ExternalOutput").ap()

    KnT = dscr("KnT", [512, S])
    KrT = dscr("KrT", [32, S])
    VmD = dscr("VmD", [S, 512])
    KsT = dscr("KsT", [512, S])
    VsD = dscr("VsD", [S, 512])
    QmT = dscr("QmT", [8 * 96, 2048])
    QsT = dscr("QsT", [512, 2048])
    OTd = dscr("OTd", [1024, 2048])
    CAP = 512
    NS = CAP // 128
    Ud = nc.dram_tensor("Ud", [2048, D], BF16, kind="Internal").ap()
    Yd = nc.dram_tensor("Yd", [NE * CAP, D], F32, kind="Internal").ap()
    if debug:
        dbgH = nc.dram_tensor("dbgH", [2048, D], F32, kind="ExternalOutput").ap()
        dbgG = nc.dram_tensor("dbgG", [2048, NE], F32, kind="ExternalOutput").ap()
        dbgS = nc.dram_tensor("dbgS", [2048, 4], I32, kind="ExternalOutput").ap()
        dbgJ = nc.dram_tensor("dbgJ", [2048, 4], F32, kind="ExternalOutput").ap()

    top = ExitStack()
    with top:
        p = Prog(nc)
        if True:

            def mk_alloc(stack):
                def sb(n, s, d):
                    return stack.enter_context(nc.sbuf_tensor(n, list(s), d))

                def ps(n, s, d=F32):
                    return stack.enter_context(nc.psum_tensor(n, list(s), d))
                return sb, ps

            sb0, ps0 = mk_alloc(top)

            identf = sb0("identf", [128, 128], F32)
            ident = sb0("ident", [128, 128], BF16)
            ones = sb0("ones", [128, 128], BF16)
            ntri = sb0("ntri", [128, 128], BF16)
            nones = sb0("nones", [128, 128], BF16)
            zeros = sb0("zeros", [128, 128], BF16)
            p.op("pool", lambda e: e.memset(identf[:], 0.0), writes=["identf"])
            p.op("pool", lambda e: e.affine_select(out=identf[:], in_=identf[:], pattern=[[-1, 128]],
                                                    compare_op=ALU.not_equal, fill=1.0, base=0, channel_multiplier=1),
                 reads=["identf"], writes=["identf"])
            p.op("dve", lambda e: e.tensor_copy(out=ident[:], in_=identf[:]), reads=["identf"], writes=["ident"])
            p.op("pool", lambda e: e.memset(ones[:], 1.0), writes=["ones"])
            p.op("pool", lambda e: e.memset(nones[:], -1.0), writes=["nones"])
            p.op("pool", lambda e: e.memset(zeros[:], 0.0), writes=["zeros"])
            gcol = sb0("gcol", [128, 16], F32)
            p.op("sp", lambda e: e.dma_start(out=gcol[:, 0:3], in_=g_cq.rearrange("o (c p) -> p (o c)", p=128),
                                             allow_slow_non_contiguous=True), writes=["gcol"], dma="gcol")
            p.op("sp", lambda e: e.dma_start(out=gcol[:, 3:5], in_=g_ckv.rearrange("o (c p) -> p (o c)", p=128),
                                             allow_slow_non_contiguous=True), writes=["gcol"], dma="gcol")
            p.op("sp", lambda e: e.dma_start(out=gcol[:, 5:13], in_=g_out.rearrange("o (c p) -> p (o c)", p=128),
                                             allow_slow_non_contiguous=True), writes=["gcol"], dma="gcol")
            invc = sb0("invc", [128, 1], F32)
            p.op("sp", lambda e: e.dma_start(out=invc[:], in_=invf), writes=["invc"], dma="invc")

            def rstd_from(eng_out, src, n, rd, wr):
                p.op("act", lambda e: e.activation(out=eng_out, in_=src, func=AF.Ln, scale=1.0 / n, bias=EPS),
                     reads=rd, writes=wr)
                p.op("act", lambda e: e.activation(out=eng_out, in_=eng_out, func=AF.Exp, scale=-0.5),
                     reads=wr, writes=wr)

            sa = ExitStack()
            with sa:
                sb, ps = mk_alloc(sa)
                tmpf = sb("tmpf", [128, 128], F32)
                p.op("pool", lambda e: e.memset(tmpf[:], -1.0), writes=["tmpf"])
                p.op("pool", lambda e: e.affine_select(out=tmpf[:], in_=tmpf[:], pattern=[[-1, 128]],
                                                        compare_op=ALU.is_ge, fill=0.0, base=0, channel_multiplier=1),
                     reads=["tmpf"], writes=["tmpf"])
                p.op("dve", lambda e: e.tensor_copy(out=ntri[:], in_=tmpf[:]), reads=["tmpf"], writes=["ntri"])

                Win = sb("Win", [128, 8, 2208], BF16)
                for c in range(8):
                    p.op("pool", lambda e, c=c: e.dma_start(out=Win[:, c, :], in_=w_in[c * 128:(c + 1) * 128, :]),
                         writes=["Win"], dma="Win")
                Wuq = sb("Wuq", [128, 3, 768], BF16)
                Wuqr = sb("Wuqr", [128, 3, 768], BF16)
                p.op("pool", lambda e: e.dma_start(out=Wuq[:], in_=w_uq.rearrange("(c p) n -> p c n", p=128)),
                     writes=["Wuq"], dma="Wuq")
                Wkn = sb("Wkn", [128, 2, 512], BF16)
                Wv = sb("Wv", [128, 2, 512], BF16)
                ukv = w_ukv.rearrange("(c p) (h t d) -> p c h t d", p=128, h=8, t=2)
                for c in range(2):
                    p.op("pool", lambda e, c=c: e.dma_start(out=Wkn[:, c, :].rearrange("p (h d) -> p h d", h=8),
                                                          in_=ukv[:, c, :, 0, :]), writes=["Wkn"], dma="Wkn")
                    p.op("pool", lambda e, c=c: e.dma_start(out=Wv[:, c, :].rearrange("p (h d) -> p h d", h=8),
                                                          in_=ukv[:, c, :, 1, :]), writes=["Wv"], dma="Wv")
                p.op("pool", lambda e: e.memset(Wuqr[:], 0.0), writes=["Wuqr"])
                Wq4 = Wuq[:].rearrange("p c (h d) -> p c h d", h=8)
                Wr4 = Wuqr[:].rearrange("p c (h d) -> p c h d", h=8)
                for c in range(3):
                    p.op("dve", lambda e, c=c: e.tensor_scalar(out=Wr4[:, c, :, 64:80], in0=Wq4[:, c, :, 80:96], scalar1=-1.0,
                                                          scalar2=None, op0=ALU.mult), reads=["Wuq", "Wuqr"], writes=["Wuqr"])
                    p.op("dve", lambda e, c=c: e.tensor_copy(out=Wr4[:, c, :, 80:96], in_=Wq4[:, c, :, 64:80]),
                         reads=["Wuq", "Wuqr"], writes=["Wuqr"])
                Wkr = sb("Wkr", [128, 8, 64], BF16)
                p.op("dve", lambda e: e.tensor_copy(out=Wkr[:, :, 0:32], in_=Win[:, :, 640:672]), reads=["Win"], writes=["Wkr"])
                p.op("dve", lambda e: e.tensor_scalar(out=Wkr[:, :, 32:48], in0=Win[:, :, 656:672], scalar1=-1.0, scalar2=None,
                                                      op0=ALU.mult), reads=["Win", "Wkr"], writes=["Wkr"])
                p.op("dve", lambda e: e.tensor_copy(out=Wkr[:, :, 48:64], in_=Win[:, :, 640:656]), reads=["Win", "Wkr"], writes=["Wkr"])
                gattn = sb("gattn", [128, D], F32)
                p.op("sp", lambda e: e.dma_start(out=gattn[:], in_=g_attn.partition_broadcast(128)), writes=["gattn"], dma="gattn")

                def rope_table(Ct, St, pos_ap, n, rows, name, inv, kinv, sb, fold=1):
                    posi = sb(name + "_pi", [128, n], I32)
                    ang = sb(name + "_ang", [128, n], F32)
                    kk = sb(name + "_k", [128, n], F32)
                    ki = sb(name + "_ki", [128, n], I32)
                    if fold == 1:
                        p.op("sp", lambda e: e.dma_start(out=posi[:], in_=pos_ap.partition_broadcast(128)), writes=[name + "pi"], dma=name + "pi")
                    else:
                        pr_ = 128 // fold
                        for b_ in range(fold):
                            p.op("sp", lambda e: e.dma_start(out=posi[b_ * pr_:(b_ + 1) * pr_, :], in_=pos_ap[:, b_ * n:(b_ + 1) * n].partition_broadcast(pr_)),
                                 writes=[name + "pi"], dma=name + "pi%d" % b_)
                    p.op("dve", lambda e: e.tensor_copy(out=ang[:], in_=posi[:]), reads=[name + "pi"], writes=[name + "ang"])
                    p.op("dve", lambda e: e.tensor_scalar(out=ang[:], in0=ang[:], scalar1=inv[:, 0:1], scalar2=None, op0=ALU.mult),
                         reads=[name + "ang", kinv], writes=[name + "ang"])
                    for which, T in (("s", St), ("c", Ct)):
                        off = 0.0 if which == "s" else float(np.pi / 2)
                        p.op("dve", lambda e, off=off: e.tensor_scalar(out=kk[:], in0=ang[:], scalar1=off, scalar2=float(1.0 / (2 * np.pi)),
                                                                   op0=ALU.add, op1=ALU.mult), reads=[name + "ang"], writes=[name + "kk"])
                        p.op("dve", lambda e: e.tensor_copy(out=ki[:], in_=kk[:]), reads=[name + "kk"], writes=[name + "ki"])
                        p.op("dve", lambda e: e.tensor_copy(out=kk[:], in_=ki[:]), reads=[name + "ki"], writes=[name + "kk"])
                        p.op("dve", lambda e, T=T: e.scalar_tensor_tensor(out=T, in0=kk[:rows], scalar=-6.28125, in1=ang[:rows],
                                                                       op0=ALU.mult, op1=ALU.add),
                             reads=[name + "kk", name + "ang"], writes=[name + which])
                        p.op("dve", lambda e, T=T, off=off: e.scalar_tensor_tensor(out=T, in0=kk[:rows], scalar=float(-(2 * np.pi - 6.28125)), in1=T,
                                                                                op0=ALU.mult, op1=ALU.add),
                             reads=[name + "kk", name + which], writes=[name + which])
                        if off != 0.0:
                            p.op("dve", lambda e, T=T, off=off: e.tensor_scalar(out=T, in0=T, scalar1=off, scalar2=None, op0=ALU.add),
                                 reads=[name + which], writes=[name + which])
                        p.op("dve", lambda e: e.tensor_scalar(out=kk[:rows], in0=T, scalar1=float(np.pi), scalar2=float(-2 * np.pi),
                                                              op0=ALU.is_gt, op1=ALU.mult), reads=[name + which], writes=[name + "kk"])
                        p.op("dve", lambda e, T=T: e.tensor_tensor(out=T, in0=T, in1=kk[:rows], op=ALU.add),
                             reads=[name + which, name + "kk"], writes=[name + which])
                        p.op("dve", lambda e: e.tensor_scalar(out=kk[:rows], in0=T, scalar1=float(-np.pi), scalar2=float(2 * np.pi),
                                                              op0=ALU.is_lt, op1=ALU.mult), reads=[name + which], writes=[name + "kk"])
                        p.op("dve", lambda e, T=T: e.tensor_tensor(out=T, in0=T, in1=kk[:rows], op=ALU.add),
                             reads=[name + which, name + "kk"], writes=[name + which])
                        p.op("dve", lambda e, T=T: e.tensor_scalar(out=T, in0=T, scalar1=3.14159, scalar2=-3.14159, op0=ALU.min, op1=ALU.max),
                             reads=[name + which], writes=[name + which])
                        p.op("act", lambda e, T=T: e.activation(out=T, in_=T, func=AF.Sin), reads=[name + which], writes=[name + which])

                Ck = sb("Ck", [32, S], F32)
                Sk = sb("Sk", [32, S], F32)
                sa2 = ExitStack()
                with sa2:
                    sbt_ = mk_alloc(sa2)[0]
                    Ck4 = sbt_("Ck4", [128, 1024], F32)
                    Sk4 = sbt_("Sk4", [128, 1024], F32)
                    rope_table(Ck4[:], Sk4[:], posa, 1024, 128, "r4", invc, "invc", sbt_, fold=4)
                    for b_ in range(4):
                        p.op("sp", lambda e: e.dma_start(out=Ck[:, b_ * 1024:(b_ + 1) * 1024], in_=Ck4[b_ * 32:(b_ + 1) * 32, :]), reads=["r4c"], writes=["rkc"], dma="rkc%d" % b_)
                        p.op("sp", lambda e: e.dma_start(out=Sk[:, b_ * 1024:(b_ + 1) * 1024], in_=Sk4[b_ * 32:(b_ + 1) * 32, :]), reads=["r4s"], writes=["rks"], dma="rks%d" % b_)
                    p.barrier()
                Cq = sb("Cq", [96, 2048], F32)
                Sq = sb("Sq", [96, 2048], F32)
                sa3 = ExitStack()
                with sa3:
                    sb3_ = mk_alloc(sa3)[0]
                    invq = sb3_("invq", [128, 1], F32)
                    p.op("pool", lambda e: e.memset(invq[:], 0.0), writes=["invq"])
                    p.op("sp", lambda e: e.dma_start(out=invq[64:96, :], in_=invf[0:32, :]), reads=["invq"], writes=["invq"], dma="invq")
                    rope_table(Cq[:], Sq[:], poso, 2048, 96, "rq", invq, "invq", sb3_)
                    p.barrier()
                    if stop_after == "R":
                        p.finish_wait("sp"); p.emit(top); return nc
                msc = float((64 + 32) ** -0.5)
                p.op("dve", lambda e: e.tensor_scalar(out=Cq[:], in0=Cq[:], scalar1=msc, scalar2=None, op0=ALU.mult),
                     reads=["rqc"], writes=["rqc"])
                p.op("dve", lambda e: e.tensor_scalar(out=Sq[:], in0=Sq[:], scalar1=msc, scalar2=None, op0=ALU.mult),
                     reads=["rqs"], writes=["rqs"])

                xt_r = Ring(sb, "xt", 4, [128, D], F32)
                junk = sb("junkA", [128, D], F32)
                ss_r = Ring(sb, "ssA", 4, [128, 1], F32)
                xn_r = Ring(sb, "xn", 4, [128, D], BF16)
                xnT_r = Ring(sb, "xnT", 2, [128, 8, 512], BF16)
                pT_r = Ring(ps, "pTA", 2, [128, 8, 128], BF16)
                pm_r = Ring(ps, "pmA", 4, [128, 512], F32)
                pss = ps("pssA", [128, 512], F32)
                sq_r = Ring(sb, "sqA", 2, [128, 512], BF16)
                rbc = sb("rbcA", [128, 512], F32)
                cn_r = Ring(sb, "cnA", 2, [128, 3, 512], BF16)
                ev_r = Ring(sb, "evA", 2, [128, 512], BF16)
                stg_r = Ring(sb, "stgA", 3, [128, 4, 512], BF16)
                stg8_r = Ring(sb, "stg8A", 2, [128, 8, 512], BF16)
                evf_r = Ring(sb, "evfA", 2, [128, 512], F32)
                evf2_r = Ring(sb, "evf2A", 2, [128, 512], F32)

                def make_xnT(src, tok0, defer=False):
                    xnT, kT = xnT_r.next()
                    xs = []
                    for sub in range(4):
                        xt, kx = xt_r.next()
                        ss, ks = ss_r.next()
                        xn, kn = xn_r.next()
                        r0 = tok0 + sub * 128
                        p.op("sp", lambda e: e.dma_start(out=xt[:], in_=src[r0:r0 + 128, :]), writes=[kx], dma=kx)
                        p.op("act", lambda e: e.activation(out=junk[:], in_=xt[:], func=AF.Square, accum_out=ss[:]),
                             reads=[kx], writes=["junkA", ks])
                        rstd_from(ss[:], ss[:], D, [ks], [ks])
                        p.op("dve", lambda e: e.scalar_tensor_tensor(out=xn[:], in0=xt[:], scalar=ss[:, 0:1], in1=gattn[:],
                                                                     op0=ALU.mult, op1=ALU.mult),
                             reads=[kx, ks, "gattn"], writes=[kn])
                        xs.append((xn, kn))
                    def tr(sub):
                        xn, kn = xs[sub]
                        pT, kp = pT_r.next()
                        for c in range(8):
                            p.op("pe", lambda e: e.transpose(out=pT[:, c, :], in_=xn[:, c * 128:(c + 1) * 128], identity=ident[:]),
                                 reads=[kn, "ident"], writes=[kp])
                        p.op("dve", lambda e: e.tensor_copy(out=xnT[:, :, sub * 128:(sub + 1) * 128], in_=pT[:]),
                             reads=[kp], writes=[kT])
                    if defer:
                        return xnT, kT, tr
                    for sub in range(4):
                        tr(sub)
                    return xnT, kT

                def proj_fm(xnT, kT, col0, m, wkey="Win", W=None):
                    W = Win if W is None else W
                    pm, kpm = pm_r.next()
                    for k in range(8):
                        p.op("pe", lambda e, k=k, pm=pm, W=W: e.matmul(out=pm[0:m, :], lhsT=W[:, k, col0:col0 + m], rhs=xnT[:, k, :],
                                                                  start=(k == 0), stop=(k == 7)),
                             reads=[kT, wkey], writes=[kpm])
                    return pm, kpm

                def lowrank_norm(pms, nch, width, gc0):
                    for i, (pm, kpm) in enumerate(pms):
                        sq, ksq = sq_r.next()
                        p.op("act", lambda e, pm=pm, sq=sq: e.activation(out=sq[:], in_=pm[:], func=AF.Square), reads=[kpm], writes=[ksq])
                        p.op("pe", lambda e, sq=sq, i=i: e.matmul(out=pss[:], lhsT=ones[:], rhs=sq[:], start=(i == 0), stop=(i == nch - 1)),
                             reads=[ksq, "ones"], writes=["pssA"])
                    rstd_from(rbc[:], pss[:], width, ["pssA"], ["rbcA"])
                    cn, kcn = cn_r.next()
                    for i, (pm, kpm) in enumerate(pms):
                        p.op("dve", lambda e, pm=pm, i=i, cn=cn: e.scalar_tensor_tensor(out=cn[:, i, :], in0=pm[:], scalar=gcol[:, gc0 + i:gc0 + i + 1],
                                                                                    in1=rbc[:], op0=ALU.mult, op1=ALU.mult),
                             reads=[kpm, "gcol", "rbcA"], writes=[kcn])
                    return cn, kcn

                def store_fm(pm, kpm, rows, dst, eng="dve", scale=None):
                    ev, kev = ev_r.next()
                    if scale is None:
                        if eng == "act":
                            p.op("act", lambda e: e.copy(out=ev[0:rows, :], in_=pm[0:rows, :]), reads=[kpm], writes=[kev])
                        else:
                            p.op("dve", lambda e: e.tensor_copy(out=ev[0:rows, :], in_=pm[0:rows, :]), reads=[kpm], writes=[kev])
                    else:
                        p.op("act", lambda e: e.mul(out=ev[0:rows, :], in_=pm[0:rows, :], mul=scale), reads=[kpm], writes=[kev])
                    p.op("pool", lambda e: e.dma_start(out=dst, in_=ev[0:rows, :]), reads=[kev], writes=[], dma=kev + "s")

                def evac_to(pm, kpm, dst, kdst, eng="dve", scale=None):
                    if scale is not None:
                        p.op("act", lambda e: e.mul(out=dst, in_=pm[:], mul=scale), reads=[kpm], writes=[kdst])
                    elif eng == "act":
                        p.op("act", lambda e: e.copy(out=dst, in_=pm[:]), reads=[kpm], writes=[kdst])
                    else:
                        p.op("dve", lambda e: e.tensor_copy(out=dst, in_=pm[:]), reads=[kpm], writes=[kdst])

                KnT_v = KnT.rearrange("(c p) n -> p c n", p=128)
                KsT_v = KsT.rearrange("(c p) n -> p c n", p=128)
                QsT_v = QsT.rearrange("(c p) n -> p c n", p=128)
                VmD_v = VmD.rearrange("(s p) n -> p s n", p=128)
                VsD_v = VsD.rearrange("(s p) n -> p s n", p=128)
                QmT_v = QmT.rearrange("(h r) n -> r h n", r=96)

                srcs = [(xa, g_ * 512) for g_ in range(8)] + [(xo, g_ * 512) for g_ in range(4)]
                nxt = make_xnT(*srcs[0])
                for G in range(8):
                    c0 = G * 512
                    xnT, kT = nxt
                    nx_xnT, nx_kT, nx_tr = make_xnT(*srcs[G + 1], defer=True)
                    nxt = (nx_xnT, nx_kT)
                    ckv = [proj_fm(xnT, kT, 384 + m * 128, 128) for m in range(2)]
                    nx_tr(0)
                    cn, kcn = lowrank_norm(ckv, 2, 256, 3)
                    pa, kpa = proj_fm(xnT, kT, 0, 32, "Wkr", Wkr)
                    pb, kpb = proj_fm(xnT, kT, 32, 32, "Wkr", Wkr)
                    t1, kt1 = evf_r.next()
                    t2, kt2 = evf2_r.next()
                    p.op("dve", lambda e, pa=pa, t1=t1, c0=c0: e.tensor_tensor(out=t1[0:32, :], in0=pa[0:32, :], in1=Ck[:, c0:c0 + 512], op=ALU.mult),
                         reads=[kpa, "rkc"], writes=[kt1])
                    p.op("dve", lambda e, pb=pb, t2=t2, c0=c0: e.tensor_tensor(out=t2[0:32, :], in0=pb[0:32, :], in1=Sk[:, c0:c0 + 512], op=ALU.mult),
                         reads=[kpb, "rks"], writes=[kt2])
                    ev, kev = ev_r.next()
                    p.op("dve", lambda e, t1=t1, t2=t2, ev=ev: e.tensor_tensor(out=ev[0:32, :], in0=t1[0:32, :], in1=t2[0:32, :], op=ALU.add),
                         reads=[kt1, kt2], writes=[kev])
                    p.op("sp", lambda e, ev=ev, c0=c0: e.dma_start(out=KrT[:, c0:c0 + 512], in_=ev[0:32, :]), reads=[kev], writes=[], dma=kev + "s")
                    nx_tr(1)
                    st_, kst_ = stg_r.next()
                    for m in range(4):
                        pm, kpm = proj_fm(xnT, kT, 1184 + m * 128, 128)
                        evac_to(pm, kpm, st_[:, m, :], kst_, eng="act")
                    p.op("sp", lambda e: e.dma_start(out=KsT_v[:, :, c0:c0 + 512], in_=st_[:]), reads=[kst_], dma=kst_ + "s")
                    nx_tr(2)
                    st_, kst_ = stg_r.next()
                    for sub in range(4):
                        pm, kpm = pm_r.next()
                        for k in range(8):
                            p.op("pe", lambda e, k=k, pm=pm, sub=sub, xnT=xnT: e.matmul(out=pm[:], lhsT=xnT[:, k, sub * 128:(sub + 1) * 128],
                                                                                   rhs=Win[:, k, 1696:2208], start=(k == 0), stop=(k == 7)),
                                 reads=[kT, "Win"], writes=[kpm])
                        evac_to(pm, kpm, st_[:, sub, :], kst_, eng="dve")
                    p.op("sp", lambda e: e.dma_start(out=VsD_v[:, G * 4:(G + 1) * 4, :], in_=st_[:]), reads=[kst_], dma=kst_ + "s")

                    nx_tr(3)
                    st_, kst_ = stg_r.next()
                    for hp in range(4):
                        pm, kpm = pm_r.next()
                        for m in range(2):
                            p.op("pe", lambda e, m=m, pm=pm, hp=hp, cn=cn: e.matmul(out=pm[:], lhsT=Wkn[:, m, hp * 128:(hp + 1) * 128], rhs=cn[:, m, :],
                                                                               start=(m == 0), stop=(m == 1)),
                                 reads=[kcn, "Wkn"], writes=[kpm])
                        evac_to(pm, kpm, st_[:, hp, :], kst_, eng="act")
                    p.op("sp", lambda e: e.dma_start(out=KnT_v[:, :, c0:c0 + 512], in_=st_[:]), reads=[kst_], dma=kst_ + "s")
                    st_, kst_ = stg_r.next()
                    for sub in range(4):
                        pm, kpm = pm_r.next()
                        for m in range(2):
                            p.op("pe", lambda e, m=m, pm=pm, sub=sub, cn=cn: e.matmul(out=pm[:], lhsT=cn[:, m, sub * 128:(sub + 1) * 128], rhs=Wv[:, m, :],
                                                                                 start=(m == 0), stop=(m == 1)),
                                 reads=[kcn, "Wv"], writes=[kpm])
                        evac_to(pm, kpm, st_[:, sub, :], kst_, eng="dve")
                    p.op("sp", lambda e: e.dma_start(out=VmD_v[:, G * 4:(G + 1) * 4, :], in_=st_[:]), reads=[kst_], dma=kst_ + "s")
                for G in range(4):
                    c0 = G * 512
                    xnT, kT = nxt
                    if G + 1 < 4:
                        nxt = make_xnT(*srcs[8 + G + 1])
                    cq = [proj_fm(xnT, kT, m * 128, 128) for m in range(3)]
                    cn, kcn = lowrank_norm(cq, 3, 384, 0)
                    st8, kst8 = stg8_r.next()
                    for h in range(8):
                        pa, kpa = pm_r.next()
                        pb, kpb = pm_r.next()
                        for m in range(3):
                            p.op("pe", lambda e, m=m, pa=pa, h=h, cn=cn: e.matmul(out=pa[0:96, :], lhsT=Wuq[:, m, h * 96:(h + 1) * 96], rhs=cn[:, m, :],
                                                                             start=(m == 0), stop=(m == 2)),
                                 reads=[kcn, "Wuq"], writes=[kpa])
                        for m in range(3):
                            p.op("pe", lambda e, m=m, pb=pb, h=h, cn=cn: e.matmul(out=pb[0:96, :], lhsT=Wuqr[:, m, h * 96:(h + 1) * 96], rhs=cn[:, m, :],
                                                                             start=(m == 0), stop=(m == 2)),
                                 reads=[kcn, "Wuqr"], writes=[kpb])
                        t1, kt1 = evf_r.next()
                        t2, kt2 = evf2_r.next()
                        p.op("dve", lambda e, pa=pa, t1=t1, c0=c0: e.tensor_tensor(out=t1[0:96, :], in0=pa[0:96, :], in1=Cq[:, c0:c0 + 512], op=ALU.mult),
                             reads=[kpa, "rqc"], writes=[kt1])
                        p.op("dve", lambda e, pb=pb, t2=t2, c0=c0: e.tensor_tensor(out=t2[0:96, :], in0=pb[0:96, :], in1=Sq[:, c0:c0 + 512], op=ALU.mult),
                             reads=[kpb, "rqs"], writes=[kt2])
                        p.op("pool", lambda e, t1=t1, t2=t2: e.tensor_tensor(out=st8[0:96, h, :], in0=t1[0:96, :], in1=t2[0:96, :], op=ALU.add),
                             reads=[kt1, kt2], writes=[kst8])
                    p.op("sp", lambda e: e.dma_start(out=QmT_v[:, :, c0:c0 + 512], in_=st8[0:96, :, :]), reads=[kst8], dma=kst8 + "s")
                    st_, kst_ = stg_r.next()
                    for m in range(4):
                        pm, kpm = proj_fm(xnT, kT, 672 + m * 128, 128)
                        evac_to(pm, kpm, st_[:, m, :], kst_, scale=0.125)
                    p.op("sp", lambda e: e.dma_start(out=QsT_v[:, :, c0:c0 + 512], in_=st_[:]), reads=[kst_], dma=kst_ + "s")
                p.barrier()
                if stop_after == "A":
                    p.finish_wait("sp"); p.emit(top); return nc

            sbx = ExitStack()
            with sbx:
                sb, ps = mk_alloc(sbx)
                qrb = sb("qrb", [128, 512], F32)
                p.op("sp", lambda e: e.dma_start(out=qrb[:], in_=qrel.partition_broadcast(128)), writes=["qrb"], dma="qrb")
                kidx_i = sb("kidx_i", [128, 1], I32)
                krel = sb("krel", [128, 8], F32)
                p.op("pool", lambda e: e.iota(kidx_i[:], pattern=[[0, 1]], base=0, channel_multiplier=1), writes=["kidx_i"])
                p.op("dve", lambda e: e.tensor_copy(out=krel[:, 0:1], in_=kidx_i[:]), reads=["kidx_i"], writes=["krel"])
                for d in range(1, 8):
                    p.op("dve", lambda e, d=d: e.tensor_scalar(out=krel[:, d:d + 1], in0=krel[:, 0:1], scalar1=float(128 * d), scalar2=None, op0=ALU.add),
                         reads=["krel"], writes=["krel"])
                nmM = sb("nmM", [128, 8, 512], BF16)
                nmS = sb("nmS", [128, 8, 512], BF16)
                m01 = sb("m01", [128, 8, 512], BF16)
                for d in range(8):
                    p.op("dve", lambda e, d=d: e.tensor_scalar(out=nmM[:, d, :], in0=qrb[:], scalar1=krel[:, d:d + 1], scalar2=NEG, op0=ALU.is_lt, op1=ALU.mult),
                         reads=["qrb", "krel"], writes=["nmM"])
                    p.op("dve", lambda e, d=d: e.tensor_scalar(out=nmS[:, d, :], in0=qrb[:], scalar1=krel[:, d:d + 1], scalar2=NEG, op0=ALU.is_le, op1=ALU.mult),
                         reads=["qrb", "krel"], writes=["nmS"])
                    p.op("dve", lambda e, d=d: e.tensor_scalar(out=m01[:, d, :], in0=qrb[:], scalar1=krel[:, d:d + 1], scalar2=None, op0=ALU.is_gt),
                         reads=["qrb", "krel"], writes=["m01"])

                def blocks(t):
                    out = [(kb, 512, None) for kb in range(8 * t)]
                    out += [(8 * t + d, NDIAG[d], d) for d in range(8)]
                    return out

                sm = ExitStack()
                with sm:
                    def mla_gen():
                        sb, ps = mk_alloc(sm)
                        Vall = sb("Vall", [128, 32, 8, 65], BF16)
                        p.op("pool", lambda e: e.memset(Vall[:, :, :, 64:65], 1.0), writes=["Vone"])
                        vsrc = VmD.rearrange("(kb p) (h d) -> p kb h d", p=128, h=8)
                        for hh in range(8):
                            p.op("sp", lambda e: e.dma_start(out=Vall[:, :, hh, 0:64], in_=vsrc[:, :, hh, :]),
                                 writes=["Vall%d" % hh], dma="Vall%d" % hh)
                        KT_r = Ring(sb, "KTm", 2, [96, S], BF16)
                        QT_r = Ring(sb, "QTm", 2, [96, 2048], BF16)
                        pS_r = Ring(ps, "pSm", 2, [128, 512], F32)
                        pO_r = Ring(ps, "pOm", 1, [128, 512], F32)
                        pB_r = Ring(ps, "pBm", 1, [128, 512], F32)
                        P_r = Ring(sb, "Pm", 3, [128, 512], BF16)
                        Of_r = Ring(sb, "Ofm", 2, [65, 512], F32)
                        On_r = Ring(sb, "Onm", 2, [64, 512], BF16)
                        sel = sb("sel65", [65, 64], F32)
                        p.op("pool", lambda e: e.memset(sel[:], 0.0), writes=["sel65"])
                        p.op("pool", lambda e: e.memset(sel[64:65, :], 1.0), reads=["sel65"], writes=["sel65"])
                        for h in range(8):
                            KT, kKT = KT_r.next()
                            QT, kQT = QT_r.next()
                            p.op("sp", lambda e, KT=KT, h=h: e.dma_start(out=KT[0:64, :], in_=KnT[h * 64:(h + 1) * 64, :]), writes=[kKT], dma=kKT)
                            p.op("sp", lambda e, KT=KT: e.dma_start(out=KT[64:96, :], in_=KrT[:, :]), writes=[kKT], dma=kKT)
                            p.op("sp", lambda e, QT=QT, h=h: e.dma_start(out=QT[:], in_=QmT[h * 96:(h + 1) * 96, :]), writes=[kQT], dma=kQT)
                            for t in range(4):
                                bl = blocks(t)
                                pO, kpO = pO_r.next()
                                nb = len(bl)
                                stage = {}

                                def stA(i):
                                    kb, N, d = bl[i]
                                    pS, kpS = pS_r.next()
                                    p.op("pe", lambda e: e.matmul(out=pS[:, 0:N], lhsT=KT[:, kb * 128:(kb + 1) * 128], rhs=QT[:, t * 512:t * 512 + N],
                                                                  start=True, stop=(d is None)), reads=[kKT, kQT], writes=[kpS])
                                    if d is not None:
                                        p.op("pe", lambda e: e.matmul(out=pS[:, 0:N], lhsT=ident[:], rhs=nmM[:, d, 0:N], start=False, stop=True),
                                             reads=["ident", "nmM"], writes=[kpS])
                                    P, kP = P_r.next()
                                    p.op("act", lambda e: e.activation(out=P[:, 0:N], in_=pS[:, 0:N], func=AF.Exp), reads=[kpS], writes=[kP])
                                    stage[i] = (P, kP)

                                def stB(i):
                                    kb, N, d = bl[i]
                                    P, kP = stage.pop(i)
                                    p.op("pe", lambda e: e.matmul(out=pO[0:65, 0:N], lhsT=Vall[:, kb, h, :], rhs=P[:, 0:N], start=(i == 0), stop=(i == nb - 1)),
                                         reads=["Vall%d" % h, "Vone", kP], writes=[kpO])

                                for it in range(nb + 2):
                                    if it < nb:
                                        stA(it)
                                    if it - 2 >= 0:
                                        stB(it - 2)
                                    yield
                                Of, kOf = Of_r.next()
                                p.op("dve", lambda e, Of=Of, pO=pO: e.tensor_copy(out=Of[:], in_=pO[0:65, :]), reads=[kpO], writes=[kOf])
                                p.op("dve", lambda e, Of=Of: e.reciprocal(out=Of[64:65, :], in_=Of[64:65, :]), reads=[kOf], writes=[kOf])
                                pB, kpB = pB_r.next()
                                p.op("pe", lambda e, Of=Of, pB=pB: e.matmul(out=pB[0:64, :], lhsT=sel[:], rhs=Of[:], start=True, stop=True),
                                     reads=[kOf, "sel65"], writes=[kpB])
                                On, kOn = On_r.next()
                                p.op("dve", lambda e, Of=Of, pB=pB, On=On: e.tensor_tensor(out=On[:], in0=Of[0:64, :], in1=pB[0:64, :], op=ALU.mult),
                                     reads=[kOf, kpB], writes=[kOn])
                                p.op("pool", lambda e, On=On, h=h, t=t: e.dma_start(out=OTd[h * 64:(h + 1) * 64, t * 512:(t + 1) * 512], in_=On[:]),
                                     reads=[kOn], writes=[], dma=kOn + "s")

                    def sb_gen():
                        sb, ps = mk_alloc(sm)
                        Vs = sb("Vsall", [128, 32, 512], BF16)
                        vsrc = VsD.rearrange("(kb p) n -> p kb n", p=128)
                        for q4 in range(4):
                            p.op("sp", lambda e, q4=q4: e.dma_start(out=Vs[:, q4 * 8:(q4 + 1) * 8, :], in_=vsrc[:, q4 * 8:(q4 + 1) * 8, :]),
                                 writes=["Vs%d" % q4], dma="Vs%d" % q4)
                        KT_r = Ring(sb, "KTs", 2, [64, S], BF16)
                        QT_r = Ring(sb, "QTs", 2, [64, 2048], BF16)
                        pZ_r = Ring(ps, "pZs", 2, [128, 512], F32)
                        pL_r = Ring(ps, "pLs", 1, [128, 512], F32)
                        pO_r = Ring(ps, "pOs", 1, [128, 512], F32)
                        E_r = Ring(sb, "Es", 3, [128, 512], F32)
                        SP_r = Ring(sb, "SPs", 4, [128, 512], BF16)
                        SM_r = Ring(sb, "SMs", 4, [128, 512], BF16)
                        A_r = Ring(sb, "As", 3, [128, 512], BF16)
                        CR_r = Ring(sb, "CRs", 3, [128, 512], BF16)
                        On_r = Ring(sb, "Ons", 2, [64, 512], BF16)
                        for h in range(8):
                            KT, kKT = KT_r.next()
                            QT, kQT = QT_r.next()
                            p.op("sp", lambda e, KT=KT, h=h: e.dma_start(out=KT[:], in_=KsT[h * 64:(h + 1) * 64, :]), writes=[kKT], dma=kKT)
                            p.op("sp", lambda e, QT=QT, h=h: e.dma_start(out=QT[:], in_=QsT[h * 64:(h + 1) * 64, :]), writes=[kQT], dma=kQT)
                            for t in range(4):
                                bl = blocks(t)[::-1]
                                nb = len(bl)
                                pO, kpO = pO_r.next()
                                p.op("pe", lambda e, pO=pO: e.matmul(out=pO[0:64, :], lhsT=zeros[:, 0:64], rhs=m01[:, 0, :],
                                                                      start=True, stop=False), reads=["zeros", "m01"], writes=[kpO])
                                stage = {}
                                carry = {"t": None, "k": None}

                                def stA(i):
                                    kb, N, d = bl[i]
                                    pZ, kpZ = pZ_r.next()
                                    p.op("pe", lambda e: e.matmul(out=pZ[:, 0:N], lhsT=KT[:, kb * 128:(kb + 1) * 128], rhs=QT[:, t * 512:t * 512 + N],
                                                                  start=True, stop=True), reads=[kKT, kQT], writes=[kpZ])
                                    E, kE = E_r.next()
                                    p.op("act", lambda e: e.activation(out=E[:, 0:N], in_=pZ[:, 0:N], func=AF.Exp), reads=[kpZ], writes=[kE])
                                    SPt, kSP = SP_r.next()
                                    p.op("act", lambda e: e.activation(out=SPt[:, 0:N], in_=E[:, 0:N], func=AF.Ln, bias=1.0), reads=[kE], writes=[kSP])
                                    if d is not None:
                                        SM, kSM = SM_r.next()
                                        p.op("dve", lambda e: e.tensor_tensor(out=SM[:, 0:N], in0=SPt[:, 0:N], in1=m01[:, d, 0:N], op=ALU.mult),
                                             reads=[kSP, "m01"], writes=[kSM])
                                    else:
                                        SM, kSM = SPt, kSP
                                    cprev, kcprev = carry["t"], carry["k"]
                                    stage[i] = (SM, kSM, cprev, kcprev, E, kE)
                                    if i < nb - 1:
                                        cn_, kcn_ = CR_r.next()
                                        if cprev is None:
                                            if N < 512:
                                                p.op("pool", lambda e: e.memset(cn_[:, N:512], 0.0), writes=[kcn_])
                                            p.op("pool", lambda e: e.tensor_copy(out=cn_[:, 0:N], in_=SM[:, 0:N]), reads=[kSM], writes=[kcn_])
                                        else:
                                            if N < 512:
                                                p.op("pool", lambda e: e.tensor_copy(out=cn_[:, N:512], in_=cprev[:, N:512]), reads=[kcprev], writes=[kcn_])
                                            p.op("dve", lambda e: e.tensor_tensor(out=cn_[:, 0:N], in0=cprev[:, 0:N], in1=SM[:, 0:N], op=ALU.add),
                                                 reads=[kcprev, kSM], writes=[kcn_])
                                        carry["t"], carry["k"] = cn_, kcn_

                                def stB(i):
                                    kb, N, d = bl[i]
                                    SM, kSM, cprev, kcprev, E, kE = stage[i]
                                    pL, kpL = pL_r.next()
                                    last = "tri"
                                    if cprev is not None:
                                        last = "carry"
                                    if d is not None:
                                        last = "mask"
                                    p.op("pe", lambda e: e.matmul(out=pL[:, 0:N], lhsT=ntri[:], rhs=SM[:, 0:N], start=True, stop=(last == "tri")),
                                         reads=["ntri", kSM], writes=[kpL])
                                    if cprev is not None:
                                        p.op("pe", lambda e: e.matmul(out=pL[:, 0:N], lhsT=nones[:], rhs=cprev[:, 0:N], start=False, stop=(last == "carry")),
                                             reads=["nones", kcprev], writes=[kpL])
                                    if d is not None:
                                        p.op("pe", lambda e: e.matmul(out=pL[:, 0:N], lhsT=ident[:], rhs=nmS[:, d, 0:N], start=False, stop=True),
                                             reads=["ident", "nmS"], writes=[kpL])
                                    A, kA = A_r.next()
                                    p.op("act", lambda e: e.activation(out=A[:, 0:N], in_=pL[:, 0:N], func=AF.Exp), reads=[kpL], writes=[kA])
                                    p.op("dve", lambda e: e.tensor_tensor(out=A[:, 0:N], in0=A[:, 0:N], in1=E[:, 0:N], op=ALU.mult), reads=[kA, kE], writes=[kA])
                                    stage[i] = (A, kA)

                                def stC(i):
                                    kb, N, d = bl[i]
                                    A, kA = stage.pop(i)
                                    p.op("pe", lambda e: e.matmul(out=pO[0:64, 0:N], lhsT=Vs[:, kb, h * 64:(h + 1) * 64], rhs=A[:, 0:N], start=False, stop=(i == nb - 1)),
                                         reads=["Vs%d" % (kb // 8), kA], writes=[kpO])

                                for it in range(nb + 2):
                                    if it < nb:
                                        stA(it)
                                    if 0 <= it - 1 < nb:
                                        stB(it - 1)
                                    if it - 2 >= 0:
                                        stC(it - 2)
                                    yield
                                On, kOn = On_r.next()
                                p.op("dve", lambda e, On=On, pO=pO: e.tensor_copy(out=On[:], in_=pO[0:64, :]), reads=[kpO], writes=[kOn])
                                p.op("pool", lambda e, On=On, h=h, t=t: e.dma_start(out=OTd[512 + h * 64:512 + (h + 1) * 64, t * 512:(t + 1) * 512], in_=On[:]),
                                     reads=[kOn], writes=[], dma=kOn + "s")

                    gens = [mla_gen(), sb_gen()]
                    while gens:
                        for g_ in list(gens):
                            try:
                                next(g_)
                            except StopIteration:
                                gens.remove(g_)
                    p.barrier()
                    if stop_after == "B":
                        p.finish_wait("sp"); p.emit(top); return nc
            sc = ExitStack()
            with sc:
                sb, ps = mk_alloc(sc)
                H = sb("H", [128, 16, D], F32)
                posm = sb("posm", [128, 16, NE], F32)
                maskb = sb("maskb", [128, 16, NE], BF16)
                gj = sb("gj", [128, 16, 4], F32)
                slI = sb("slI", [128, 16, 4], I32)
                Gt = sb("Gt", [128, 16, NE], F32)
                bgT = sb("bgT", [128, 512], F32)
                s1 = ExitStack()
                with s1:
                    sb1, ps1 = mk_alloc(s1)
                    gbc = sb1("gbc", [128, D], F32)
                    junk = sb1("junkC", [128, D], BF16)
                    Wo = sb1("Wo", [128, 8, D], BF16)
                    p.op("pool", lambda e: e.dma_start(out=Wo[:], in_=w_o.rearrange("(c p) n -> p c n", p=128)), writes=["Wo"], dma="Wo")
                    Wr = sb1("Wr", [128, 8, NE], F32)
                    p.op("sp", lambda e: e.dma_start(out=Wr[:], in_=w_router.rearrange("(c p) n -> p c n", p=128)), writes=["Wr"], dma="Wr")
                    brb = sb1("brb", [128, NE], F32)
                    p.op("sp", lambda e: e.dma_start(out=brb[:], in_=b_router.partition_broadcast(128)), writes=["brb"], dma="brb")
                    p.op("sp", lambda e: e.dma_start(out=gbc[:], in_=g_moe.partition_broadcast(128)), writes=["gbc"], dma="gbc")
                    bgl = sb1("bgl", [128, 4, 128], F32)
                    p.op("sp", lambda e: e.dma_start(out=bgl[:], in_=b_gu.rearrange("(a r) q -> r a q", r=128)), writes=["bgl"], dma="bgl")
                    pTf_r = Ring(ps1, "pTf", 1, [128, 4, 128], F32)
                    pbt, kpbt = pTf_r.next()
                    for a in range(4):
                        p.op("pe", lambda e, a=a: e.transpose(out=pbt[:, a, :], in_=bgl[:, a, :], identity=identf[:]), reads=["bgl", "identf"], writes=[kpbt])
                    p.op("dve", lambda e: e.tensor_copy(out=bgT[:].rearrange("p (a q) -> p a q", a=4), in_=pbt[:]), reads=[kpbt], writes=["bgT"])

                    OT_r = Ring(sb1, "OTl", 1, [128, 8, 512], BF16)
                    sq_r = Ring(sb1, "sqC", 2, [128, 512], BF16)
                    pss_r = Ring(ps1, "pssC", 1, [128, 512], F32)
                    rb_r = Ring(sb1, "rbC", 2, [128, 512], F32)
                    MX = sb1("MX", [128, 8, 512], BF16)
                    pA_r = Ring(ps1, "pAC", 2, [128, 512], F32)
                    xr_r = Ring(sb1, "xrC", 2, [128, D], F32)
                    ssc_r = Ring(sb1, "sscC", 2, [128, 1], F32)
                    u32_r = Ring(sb1, "u32C", 2, [128, D], F32)
                    uhT_r = Ring(sb1, "uhT", 2, [128, 8, 128], BF16)
                    tris = sb1("tris", [128, 128], BF16)
                    tmpf1 = sb1("tmpf1", [128, 128], F32)
                    p.op("pool", lambda e: e.memset(tmpf1[:], 1.0), writes=["tmpf1"])
                    p.op("pool", lambda e: e.affine_select(out=tmpf1[:], in_=tmpf1[:], pattern=[[1, 128]], compare_op=ALU.is_gt, fill=0.0,
                                                            base=0, channel_multiplier=-1), reads=["tmpf1"], writes=["tmpf1"])
                    p.op("dve", lambda e: e.tensor_copy(out=tris[:], in_=tmpf1[:]), reads=["tmpf1"], writes=["tris"])
                    gtmp_r = Ring(sb1, "gtmpC", 2, [128, NE], F32)
                    xnb_r = Ring(sb1, "xnbC", 2, [128, D], BF16)
                    ecap_i = sb1("ecap_i", [128, NE], I32)
                    ecap = sb1("ecap", [128, NE], F32)
                    p.op("pool", lambda e: e.iota(ecap_i[:], pattern=[[CAP, NE]], base=0, channel_multiplier=0), writes=["ecap_i"])
                    p.op("dve", lambda e: e.tensor_copy(out=ecap[:], in_=ecap_i[:]), reads=["ecap_i"], writes=["ecap"])
                    pq_r = Ring(sb1, "pq", 2, [128, NE], F32)
                    oh4_r = Ring(sb1, "oh4", 2, [128, 4, NE], F32)
                    pr4_r = Ring(sb1, "pr4", 2, [128, 4, NE], F32)
                    oh = sb1("oh", [128, NE], F32)
                    pr = sb1("pr", [128, NE], F32)
                    slf_r = Ring(sb1, "slf", 2, [128, 4], F32)
                    ulo_r = Ring(sb1, "uloC", 2, [128, D], BF16)
                    uloT_r = Ring(sb1, "uloT", 2, [128, 8, 128], BF16)
                    pTb_r = Ring(ps1, "pTb", 2, [128, 8, 128], BF16)
                    Wrh = sb1("Wrh", [128, 8, NE], BF16)
                    Wrl = sb1("Wrl", [128, 8, NE], BF16)
                    p.op("dve", lambda e: e.tensor_copy(out=Wrh[:], in_=Wr[:]), reads=["Wr"], writes=["Wrh"])
                    p.op("dve", lambda e: e.tensor_tensor(out=Wrl[:], in0=Wr[:], in1=Wrh[:], op=ALU.subtract), reads=["Wr", "Wrh"], writes=["Wrl"])
                    pLg_r = Ring(ps1, "pLg", 2, [128, 2, NE], F32)
                    lg_r = Ring(sb1, "lgC", 2, [128, NE], F32)
                    mx8_r = Ring(sb1, "mx8C", 2, [128, 8], F32)
                    msk_r = Ring(sb1, "mskC", 2, [128, NE], F32)
                    ex_r = Ring(sb1, "exC", 2, [128, NE], F32)
                    sm_r = Ring(sb1, "smC", 2, [128, 1], F32)
                    otv = OTd.rearrange("(c p) n -> p c n", p=128)
                    if stop_after == "C1a":
                        p.barrier()
                        p.op("sp", lambda e: e.dma_start(out=dbgH.rearrange("(t p) n -> p t n", p=128), in_=H[:]), reads=["H%d" % i_ for i_ in range(16)], dma="dbgH")
                        p.op("sp", lambda e: e.dma_start(out=dbgG.rearrange("(t p) n -> p t n", p=128), in_=Gt[:]), reads=["Gt%d" % i_ for i_ in range(16)], dma="dbgG")
                        p.finish_wait("sp"); p.emit(top); return nc
                    for G in range(4):
                        OT, kOT = OT_r.next()
                        p.op("sp", lambda e, OT=OT, G=G: e.dma_start(out=OT[:], in_=otv[:, :, G * 512:(G + 1) * 512]), writes=[kOT], dma=kOT)
                        rbs = []
                        for grp in range(2):
                            pss, kpss = pss_r.next()
                            for i in range(4):
                                c = grp * 4 + i
                                sq, ksq = sq_r.next()
                                p.op("act", lambda e, sq=sq, OT=OT, c=c: e.activation(out=sq[:], in_=OT[:, c, :], func=AF.Square), reads=[kOT], writes=[ksq])
                                p.op("pe", lambda e, sq=sq, pss=pss, i=i: e.matmul(out=pss[:], lhsT=ones[:], rhs=sq[:], start=(i == 0), stop=(i == 3)),
                                     reads=[ksq, "ones"], writes=[kpss])
                            rb, krb = rb_r.next()
                            rstd_from(rb[:], pss[:], 512, [kpss], [krb])
                            rbs.append((rb, krb))
                        for c in range(8):
                            rb, krb = rbs[c // 4]
                            p.op("dve", lambda e, c=c, rb=rb, OT=OT: e.scalar_tensor_tensor(out=MX[:, c, :], in0=OT[:, c, :], scalar=gcol[:, 5 + c:6 + c], in1=rb[:],
                                                                                        op0=ALU.mult, op1=ALU.mult),
                                 reads=[kOT, "gcol", krb], writes=["MX"])
                        def c1_tile(G, sub):
                            tile = G * 4 + sub
                            b_ = sub % 2
                            uhT, kuhT = uhT_r.tiles[b_], uhT_r.keys[b_]
                            uloT, kuloT = uloT_r.tiles[b_], uloT_r.keys[b_]
                            pq, kpq = pq_r.tiles[b_], pq_r.keys[b_]
                            slf, kslf = slf_r.tiles[b_], slf_r.keys[b_]
                            pLg2, kpLg = pLg_r.tiles[b_], pLg_r.keys[b_]
                            pLg = pLg2[:, 0, :]
                            pPos = pLg2[:, 1, :]
                            yield
                            xr, kxr = xr_r.next()
                            yield
                            p.op("sp", lambda e, xr=xr, tile=tile: e.dma_start(out=xr[:], in_=xo[tile * 128:(tile + 1) * 128, :]), writes=[kxr], dma=kxr)
                            yield
                            for half in range(2):
                                pA, kpA = pA_r.next()
                                for c in range(8):
                                    p.op("pe", lambda e, c=c, pA=pA, sub=sub, half=half: e.matmul(out=pA[:], lhsT=MX[:, c, sub * 128:(sub + 1) * 128],
                                                                                              rhs=Wo[:, c, half * 512:(half + 1) * 512], start=(c == 0), stop=(c == 7)),
                                         reads=["MX", "Wo"], writes=[kpA])
                                p.op("dve", lambda e, pA=pA, xr=xr, tile=tile, half=half: e.tensor_tensor(out=H[:, tile, half * 512:(half + 1) * 512], in0=pA[:],
                                                                                                      in1=xr[:, half * 512:(half + 1) * 512], op=ALU.add),
                                     reads=[kpA, kxr], writes=["H%d" % tile])
                            yield
                            yield
                            ssc, kss = ssc_r.next()
                            yield
                            p.op("act", lambda e, ssc=ssc, tile=tile: e.activation(out=junk[:], in_=H[:, tile, :], func=AF.Square, accum_out=ssc[:]),
                                 reads=["H%d" % tile], writes=["junkC", kss])
                            yield
                            rstd_from(ssc[:], ssc[:], D, [kss], [kss])
                            yield
                            u32, ku = u32_r.next()
                            yield
                            p.op("dve", lambda e, ssc=ssc, u32=u32, tile=tile: e.scalar_tensor_tensor(out=u32[:], in0=H[:, tile, :], scalar=ssc[:, 0:1], in1=gbc[:],
                                                                                                  op0=ALU.mult, op1=ALU.mult),
                                 reads=["H%d" % tile, kss, "gbc"], writes=[ku])
                            yield
                            xnb, kuhi = xnb_r.next()
                            yield
                            ulo, kulo = ulo_r.next()
                            yield
                            p.op("act", lambda e: e.copy(out=xnb[:], in_=u32[:]), reads=[ku], writes=[kuhi])
                            yield
                            p.op("pool", lambda e: e.dma_start(out=Ud[tile * 128:(tile + 1) * 128, :], in_=xnb[:]), reads=[kuhi], dma=kuhi + "s")
                            yield
                            p.op("dve", lambda e: e.tensor_tensor(out=ulo[:], in0=u32[:], in1=xnb[:], op=ALU.subtract), reads=[ku, kuhi], writes=[kulo])
                            yield
                            pT, kpT = pTb_r.next()
                            yield
                            for c in range(8):
                                p.op("pe", lambda e: e.transpose(out=pT[:, c, :], in_=xnb[:, c * 128:(c + 1) * 128], identity=ident[:]), reads=[kuhi, "ident"], writes=[kpT])
                            yield
                            p.op("dve", lambda e: e.tensor_copy(out=uhT[:], in_=pT[:]), reads=[kpT], writes=[kuhT])
                            yield
                            pT2, kpT2 = pTb_r.next()
                            yield
                            for c in range(8):
                                p.op("pe", lambda e: e.transpose(out=pT2[:, c, :], in_=ulo[:, c * 128:(c + 1) * 128], identity=ident[:]), reads=[kulo, "ident"], writes=[kpT2])
                            yield
                            p.op("act", lambda e: e.copy(out=uloT[:], in_=pT2[:]), reads=[kpT2], writes=[kuloT])
                            yield
                            n_ = 0
                            yield
                            for (A_, kA_, W_, kW_) in (("hi", kuhT, Wrh, "Wrh"), ("lo", kuloT, Wrh, "Wrh"), ("hi", kuhT, Wrl, "Wrl")):
                                for k in range(8):
                                    lh = uhT[:, k, :] if A_ == "hi" else uloT[:, k, :]
                                    p.op("pe", lambda e: e.matmul(out=pLg, lhsT=lh, rhs=W_[:, k, :], start=(n_ == 0), stop=(n_ == 23)),
                                         reads=[kA_, kW_], writes=[kpLg])
                                    n_ += 1
                            yield
                            lg, klg = lg_r.next()
                            yield
                            p.op("dve", lambda e, lg=lg: e.tensor_tensor(out=lg[:], in0=pLg, in1=brb[:], op=ALU.add), reads=[kpLg, "brb"], writes=[klg])
                            yield
                            mx8, kmx = mx8_r.next()
                            yield
                            p.op("dve", lambda e, lg=lg, mx8=mx8: e.max(out=mx8[:], in_=lg[:]), reads=[klg], writes=[kmx])
                            yield
                            msk, kmsk = msk_r.next()
                            yield
                            p.op("dve", lambda e, lg=lg, mx8=mx8, msk=msk: e.tensor_scalar(out=msk[:], in0=lg[:], scalar1=mx8[:, 3:4], scalar2=None, op0=ALU.is_ge),
                                 reads=[klg, kmx], writes=[kmsk])
                            yield
                            p.op("dve", lambda e, mx8=mx8: e.tensor_scalar(out=mx8[:, 7:8], in0=mx8[:, 0:1], scalar1=-1.0, scalar2=None, op0=ALU.mult),
                                 reads=[kmx, kmsk], writes=[kmx])
                            yield
                            ex, kex = ex_r.next()
                            yield
                            p.op("act", lambda e, lg=lg, mx8=mx8, ex=ex: e.activation(out=ex[:], in_=lg[:], func=AF.Exp, bias=mx8[:, 7:8]), reads=[klg, kmx], writes=[kex])
                            yield
                            sm_, ksm = sm_r.next()
                            yield
                            p.op("dve", lambda e, ex=ex, msk=msk: e.tensor_tensor(out=ex[:], in0=ex[:], in1=msk[:], op=ALU.mult), reads=[kex, kmsk], writes=[kex])
                            yield
                            p.op("dve", lambda e, ex=ex, sm_=sm_: e.reduce_sum(out=sm_[:], in_=ex[:], axis=mybir.AxisListType.X), reads=[kex], writes=[ksm])
                            yield
                            p.op("dve", lambda e, sm_=sm_: e.reciprocal(out=sm_[:], in_=sm_[:]), reads=[ksm], writes=[ksm])
                            yield
                            p.op("dve", lambda e, ex=ex, sm_=sm_, tile=tile: e.tensor_scalar(out=Gt[:, tile, :], in0=ex[:], scalar1=sm_[:, 0:1], scalar2=None, op0=ALU.mult),
                                 reads=[kex, ksm], writes=["Gt%d" % tile])
                            yield
                            p.op("dve", lambda e: e.tensor_copy(out=maskb[:, tile, :], in_=msk[:]), reads=[kmsk], writes=["maskb%d" % tile])
                            yield
                            gtmp, kgtmp = gtmp_r.next()
                            yield
                            p.op("pe", lambda e: e.matmul(out=pPos, lhsT=tris[:], rhs=maskb[:, tile, :], start=True, stop=(tile == 0)),
                                 reads=["tris", "maskb%d" % tile], writes=[kpLg])
                            yield
                            for j_ in range(tile):
                                p.op("pe", lambda e: e.matmul(out=pPos, lhsT=ones[:], rhs=maskb[:, j_, :], start=False, stop=(j_ == tile - 1)),
                                     reads=["ones", "maskb%d" % j_], writes=[kpLg])
                            yield
                            p.op("dve", lambda e: e.scalar_tensor_tensor(out=gtmp[:], in0=pPos, scalar=1.0, in1=msk[:], op0=ALU.add, op1=ALU.mult),
                                 reads=[kpLg, kmsk, kgtmp], writes=[kgtmp])
                            yield
                            p.op("dve", lambda e: e.tensor_scalar(out=posm[:, tile, :], in0=gtmp[:], scalar1=-1.0, scalar2=None, op0=ALU.add),
                                 reads=[kgtmp], writes=["posm%d" % tile])
                            yield
                            p.op("dve", lambda e: e.scalar_tensor_tensor(out=pq[:], in0=pPos, scalar=float(CAP - 1), in1=ecap[:], op0=ALU.min, op1=ALU.add),
                                 reads=[kpLg, "ecap"], writes=[kpq])
                            yield
                            yield
                            p.op("dve", lambda e: e.scalar_tensor_tensor(out=gtmp[:], in0=pPos, scalar=float(CAP) - 0.5, in1=Gt[:, tile, :], op0=ALU.is_lt, op1=ALU.mult),
                                 reads=[kpLg, "Gt%d" % tile, kgtmp], writes=[kgtmp])
                            yield
                            oh4, koh4 = oh4_r.tiles[b_], oh4_r.keys[b_]
                            pr4, kpr4 = pr4_r.tiles[b_], pr4_r.keys[b_]
                            lg_b = lg[:].unsqueeze(1).to_broadcast([128, 4, NE])
                            mx_b = mx8[:, 0:4].unsqueeze(2).to_broadcast([128, 4, NE])
                            p.op("dve", lambda e: e.tensor_tensor(out=oh4[:], in0=lg_b, in1=mx_b, op=ALU.is_equal), reads=[klg, kmx], writes=[koh4])
                            yield
                            p.op("dve", lambda e: e.tensor_tensor(out=pr4[:], in0=oh4[:], in1=pq[:].unsqueeze(1).to_broadcast([128, 4, NE]), op=ALU.mult),
                                 reads=[koh4, kpq], writes=[kpr4])
                            yield
                            p.op("dve", lambda e: e.reduce_sum(out=slf[:, 0:4], in_=pr4[:], axis=mybir.AxisListType.X), reads=[kpr4], writes=[kslf])
                            yield
                            p.op("dve", lambda e: e.tensor_tensor(out=pr4[:], in0=oh4[:], in1=gtmp[:].unsqueeze(1).to_broadcast([128, 4, NE]), op=ALU.mult),
                                 reads=[koh4, kgtmp, kpr4], writes=[kpr4])
                            yield
                            p.op("dve", lambda e: e.reduce_sum(out=gj[:, tile, :], in_=pr4[:], axis=mybir.AxisListType.X), reads=[kpr4], writes=["gj%d" % tile])
                            yield
                            p.op("dve", lambda e: e.tensor_copy(out=slI[:, tile, :], in_=slf[:]), reads=[kslf], writes=["slI%d" % tile])

                        for pair in range(2):
                            gens = [c1_tile(G, pair * 2), c1_tile(G, pair * 2 + 1)]
                            while gens:
                                for g_ in list(gens):
                                    try:
                                        next(g_)
                                    except StopIteration:
                                        gens.remove(g_)
                    p.barrier()
                    if stop_after == "C1":
                        if debug:
                            p.op("sp", lambda e: e.dma_start(out=dbgS.rearrange("(t p) n -> p t n", p=128), in_=slI[:]), reads=["slI%d" % i_ for i_ in range(16)], dma="dbgS")
                            p.op("sp", lambda e: e.dma_start(out=dbgJ.rearrange("(t p) n -> p t n", p=128), in_=gj[:]), reads=["gj%d" % i_ for i_ in range(16)], dma="dbgJ")
                            p.op("sp", lambda e: e.dma_start(out=dbgH.rearrange("(t p) n -> p t n", p=128), in_=H[:]), reads=["H%d" % i_ for i_ in range(16)], dma="dbgH")
                            p.op("sp", lambda e: e.dma_start(out=dbgG.rearrange("(t p) n -> p t n", p=128), in_=Gt[:]), reads=["Gt%d" % i_ for i_ in range(16)], dma="dbgG")
                        p.finish_wait("sp"); p.emit(top); return nc
                s2 = ExitStack()
                with s2:
                    sb2, ps2 = mk_alloc(s2)
                    W_r = Ring(sb2, "Wx", 6, [128, 8, 512], BF16)
                    bd_r = Ring(sb2, "bdn", 2, [1, D], BF16)
                    pGL_r = Ring(ps2, "pGL", 4, [128, 512], F32)
                    pA_r = Ring(ps2, "pA", 2, [128, 512], F32)
                    pTs = ps2("pTs", [128, 8, 128], BF16)
                    ptk = ps2("ptk", [128, 8], F32)
                    gl_r = Ring(sb2, "gl", 1, [128, CAP], F32)
                    sg_r = Ring(sb2, "sg", 1, [128, CAP], F32)
                    Sel = sb2("Sel", [128, 16, CAP], BF16)
                    Xe_r = Ring(sb2, "Xe", 2, [128, NS, D], BF16)
                    XeT = sb2("XeT", [128, 8, CAP], BF16)
                    aT = sb2("aTs", [128, 8, CAP], BF16)
                    Yst_r = Ring(sb2, "Yst", 2, [128, D], F32)
                    tks = sb2("tks", [128, 8], F32)
                    tkf = sb2("tkf", [128, 4], F32)
                    tkI_r = Ring(sb2, "tkI", 2, [128, 4], I32)
                    iota_i = sb2("iota_i", [128, CAP], I32)
                    iota_f = sb2("iota_f", [128, CAP], F32)
                    p.op("pool", lambda e: e.iota(iota_i[:], pattern=[[1, CAP]], base=0, channel_multiplier=0), writes=["iota_i"])
                    p.op("dve", lambda e: e.tensor_copy(out=iota_f[:], in_=iota_i[:]), reads=["iota_i"], writes=["iota_f"])
                    tid = sb2("tid", [128, 16], I32)
                    tidx = sb2("tidx", [128, 16], I32)
                    tidhl = sb2("tidhl", [128, 16, 2], BF16)
                    p.op("pool", lambda e: e.iota(tid[:], pattern=[[128, 16]], base=0, channel_multiplier=1), writes=["tid"])
                    p.op("dve", lambda e: e.tensor_scalar(out=tidx[:], in0=tid[:], scalar1=6, scalar2=None, op0=ALU.arith_shift_right), reads=["tid"], writes=["tidx"])
                    p.op("dve", lambda e: e.tensor_copy(out=tidhl[:, :, 0], in_=tidx[:]), reads=["tidx"], writes=["tidhl"])
                    p.op("dve", lambda e: e.tensor_scalar(out=tidx[:], in0=tid[:], scalar1=63, scalar2=None, op0=ALU.bitwise_and), reads=["tid", "tidx", "tidhl"], writes=["tidx"])
                    p.op("dve", lambda e: e.tensor_copy(out=tidhl[:, :, 1], in_=tidx[:]), reads=["tidx", "tidhl"], writes=["tidhl"])
                    bg3 = bgT[:].rearrange("p (e c) -> p e c", c=16)
                    p.op("dve", lambda e: e.tensor_scalar(out=bg3[:, :, 8:16], in0=bg3[:, :, 8:16], scalar1=1.0, scalar2=None, op0=ALU.add),
                         reads=["bgT"], writes=["bgT"])

                    def load_w(src, slot):
                        Wt, kW = W_r.tiles[slot], W_r.keys[slot]
                        p.op("pool", lambda e: e.dma_start(out=Wt[:], in_=src.rearrange("(c p) n -> p c n", p=128)), writes=[kW], dma=kW)
                        return Wt, kW

                    def load_bd(ex_):
                        bd, kbd = bd_r.next()
                        p.op("pool", lambda e: e.dma_start(out=bd[:], in_=b_dn[ex_:ex_ + 1, :]), writes=[kbd], dma=kbd)
                        return bd, kbd

                    xe_of = {}

                    def sel_build(ex_, tiles):
                        for tile in tiles:
                            p.op("dve", lambda e: e.tensor_scalar(out=Sel[:, tile, :], in0=iota_f[:], scalar1=posm[:, tile, ex_:ex_ + 1], scalar2=None, op0=ALU.is_equal),
                                 reads=["iota_f", "posm%d" % tile], writes=["Sel%d" % tile])

                    def dispatch(ex_, build=True):
                        if build:
                            sel_build(ex_, range(16))
                        for s_ in range(NS):
                            for tile in range(16):
                                p.op("pe", lambda e: e.matmul(out=ptk[:, 2 * s_:2 * s_ + 2], lhsT=Sel[:, tile, s_ * 128:(s_ + 1) * 128], rhs=tidhl[:, tile, :],
                                                              start=(tile == 0), stop=(tile == 15)), reads=["Sel%d" % tile, "tidhl"], writes=["ptk"])
                        p.op("dve", lambda e: e.tensor_copy(out=tks[:, 0:2 * NS], in_=ptk[:, 0:2 * NS]), reads=["ptk"], writes=["tks"])
                        tk3 = tks[:, 0:2 * NS].rearrange("p (s t) -> p s t", t=2)
                        p.op("dve", lambda e: e.scalar_tensor_tensor(out=tkf[:, 0:NS], in0=tk3[:, :, 0], scalar=64.0, in1=tk3[:, :, 1], op0=ALU.mult, op1=ALU.add),
                             reads=["tks"], writes=["tkf"])
                        tkI, ktkI = tkI_r.next()
                        p.op("dve", lambda e: e.tensor_copy(out=tkI[:, 0:NS], in_=tkf[:, 0:NS]), reads=["tkf"], writes=[ktkI])
                        Xe, kXe = Xe_r.next()
                        for s_ in range(NS):
                            p.op("pool", lambda e: e.indirect_dma_start(out=Xe[:, s_, :], out_offset=None, in_=Ud[:, :],
                                                                         in_offset=bass.IndirectOffsetOnAxis(ap=tkI[:, s_:s_ + 1], axis=0)),
                                 reads=[ktkI], writes=[kXe], dma=kXe)
                        xe_of[ex_] = (Xe, kXe)

                    def transposes_s(ex_, s_):
                        Xe, kXe = xe_of[ex_]
                        for k in range(8):
                            p.op("pe", lambda e: e.transpose(out=pTs[:, k, :], in_=Xe[:, s_, k * 128:(k + 1) * 128], identity=ident[:]),
                                 reads=[kXe, "ident"], writes=["pTs"])
                        if s_ % 2 == 0:
                            p.op("act", lambda e: e.copy(out=XeT[:, :, s_ * 128:(s_ + 1) * 128], in_=pTs[:]), reads=["pTs"], writes=["XeT"])
                        else:
                            p.op("dve", lambda e: e.tensor_copy(out=XeT[:, :, s_ * 128:(s_ + 1) * 128], in_=pTs[:]), reads=["pTs"], writes=["XeT"])
                        if s_ == NS - 1:
                            xe_of.pop(ex_)

                    def transposes(ex_):
                        for s_ in range(NS):
                            transposes_s(ex_, s_)

                    def gu_stage(ex_, st, Wg, kWg, Wl, kWl, sel_for=None):
                        for mc in range(4):
                            if sel_for is not None:
                                sel_build(sel_for, range(mc * 4, mc * 4 + 4))
                            c = st * 4 + mc
                            pG, kpG = pGL_r.next()
                            pLn, kpLn = pGL_r.next()
                            for k in range(8):
                                p.op("pe", lambda e: e.matmul(out=pG[:, 0:CAP], lhsT=Wg[:, k, mc * 128:(mc + 1) * 128], rhs=XeT[:, k, :],
                                                              start=(k == 0), stop=(k == 7)), reads=[kWg, "XeT"], writes=[kpG])
                            for k in range(8):
                                p.op("pe", lambda e: e.matmul(out=pLn[:, 0:CAP], lhsT=Wl[:, k, mc * 128:(mc + 1) * 128], rhs=XeT[:, k, :],
                                                              start=(k == 0), stop=(k == 7)), reads=[kWl, "XeT"], writes=[kpLn])
                            gl, kgl = gl_r.next()
                            sg, ksg = sg_r.next()
                            bgc = ex_ * 16 + c
                            blc = ex_ * 16 + 8 + c
                            kaT = "aT_%d" % st
                            p.op("dve", lambda e: e.tensor_scalar(out=gl[:], in0=pG[:, 0:CAP], scalar1=bgT[:, bgc:bgc + 1], scalar2=7.0, op0=ALU.add, op1=ALU.min),
                                 reads=[kpG, "bgT"], writes=[kgl])
                            p.op("act", lambda e: e.activation(out=sg[:], in_=gl[:], func=AF.Sigmoid, scale=1.702), reads=[kgl], writes=[ksg])
                            p.op("dve", lambda e: e.tensor_tensor(out=sg[:], in0=sg[:], in1=gl[:], op=ALU.mult), reads=[kgl, ksg], writes=[ksg])
                            p.op("dve", lambda e: e.tensor_scalar(out=gl[:], in0=pLn[:, 0:CAP], scalar1=bgT[:, blc:blc + 1], scalar2=-6.0, op0=ALU.add, op1=ALU.max),
                                 reads=[kpLn, "bgT", kgl], writes=[kgl])
                            p.op("dve", lambda e: e.scalar_tensor_tensor(out=aT[:, c, :], in0=gl[:], scalar=8.0, in1=sg[:], op0=ALU.min, op1=ALU.mult),
                                 reads=[ksg, kgl], writes=[kaT])

                    def dn_stage(ex_, d0, d1, bd, kbd, nxt=None):
                        for s_ in range(NS):
                            if nxt is not None:
                                transposes_s(nxt, s_)
                            Yst, kY = Yst_r.next()
                            for half in range(2):
                                Wd, kWd = (d0, d1)[half]
                                pA, kpA = pA_r.next()
                                for c in range(8):
                                    p.op("pe", lambda e: e.matmul(out=pA[:], lhsT=aT[:, c, s_ * 128:(s_ + 1) * 128], rhs=Wd[:, c, :], start=(c == 0), stop=False),
                                         reads=["aT_%d" % (c // 4), kWd], writes=[kpA])
                                p.op("pe", lambda e: e.matmul(out=pA[:], lhsT=ones[0:1, :], rhs=bd[0:1, half * 512:(half + 1) * 512], start=False, stop=True),
                                     reads=["ones", kbd], writes=[kpA])
                                p.op("act", lambda e: e.copy(out=Yst[:, half * 512:(half + 1) * 512], in_=pA[:]), reads=[kpA], writes=[kY])
                            r0 = ex_ * CAP + s_ * 128
                            p.op("sp", lambda e: e.dma_start(out=Yd[r0:r0 + 128, :], in_=Yst[:]), reads=[kY], dma=kY + "s")

                    def loads_gl0(ex_):
                        return load_w(w_gu[ex_, :, 0:512], 0), load_w(w_gu[ex_, :, 1024:1536], 1)

                    def loads_gl1(ex_):
                        return load_w(w_gu[ex_, :, 512:1024], 2), load_w(w_gu[ex_, :, 1536:2048], 3)

                    def loads_d(ex_):
                        return load_w(w_dn[ex_, :, 0:512], 4), load_w(w_dn[ex_, :, 512:1024], 5), load_bd(ex_)

                    g0, l0 = loads_gl0(0)
                    g1, l1 = loads_gl1(0)
                    d0, d1, (bd, kbd) = loads_d(0)
                    dispatch(0)
                    dispatch(1)
                    transposes(0)
                    for ex_ in range(NE):
                        gu_stage(ex_, 0, g0[0], g0[1], l0[0], l0[1], sel_for=(ex_ + 2 if ex_ + 2 < NE else None))
                        if ex_ + 1 < NE:
                            g0n, l0n = loads_gl0(ex_ + 1)
                        gu_stage(ex_, 1, g1[0], g1[1], l1[0], l1[1])
                        if ex_ + 2 < NE:
                            dispatch(ex_ + 2, build=False)
                        if ex_ + 1 < NE:
                            g1n, l1n = loads_gl1(ex_ + 1)
                        dn_stage(ex_, d0, d1, bd, kbd, nxt=(ex_ + 1 if ex_ + 1 < NE else None))
                        if ex_ + 1 < NE:
                            d0, d1, (bd, kbd) = loads_d(ex_ + 1)
                            g0, l0, g1, l1 = g0n, l0n, g1n, l1n
                    p.barrier()
                    Yg_r = Ring(sb2, "Yg", 4, [128, D], F32)
                    for tile in range(16):
                        hk = "H%d" % tile
                        for j_ in range(4):
                            Yg, kYg = Yg_r.next()
                            p.op("pool", lambda e: e.indirect_dma_start(out=Yg[:, :], out_offset=None, in_=Yd[:, :],
                                                                         in_offset=bass.IndirectOffsetOnAxis(ap=slI[:, tile, j_:j_ + 1], axis=0)),
                                 reads=["slI%d" % tile], writes=[kYg], dma=kYg)
                            p.op("dve", lambda e: e.scalar_tensor_tensor(out=H[:, tile, :], in0=Yg[:], scalar=gj[:, tile, j_:j_ + 1], in1=H[:, tile, :], op0=ALU.mult, op1=ALU.add),
                                 reads=[kYg, "gj%d" % tile, hk], writes=[hk])
                    p.barrier()
                    if stop_after == "C2":
                        if debug:
                            p.op("sp", lambda e: e.dma_start(out=dbgH.rearrange("(t p) n -> p t n", p=128), in_=H[:]), reads=["H%d" % i_ for i_ in range(16)], dma="dbgH")
                            p.op("sp", lambda e: e.dma_start(out=dbgG.rearrange("(t p) n -> p t n", p=128), in_=Gt[:]), reads=["Gt%d" % i_ for i_ in range(16)], dma="dbgG")
                        p.finish_wait("sp"); p.emit(top); return nc
                s3 = ExitStack()
                with s3:
                    sb3, ps3 = mk_alloc(s3)
                    gbc = sb3("gbc3", [128, D], F32)
                    junk = sb3("junkC3", [128, D], BF16)
                    Wpg = sb3("Wpg", [128, 8, D], BF16)
                    Wpp = sb3("Wpp", [128, 2, D], BF16)
                    p.op("pool", lambda e: e.dma_start(out=Wpg[:], in_=w_pg.rearrange("(c p) n -> p c n", p=128)), writes=["Wpg"], dma="Wpg")
                    p.op("pool", lambda e: e.dma_start(out=Wpp[:], in_=w_pp.rearrange("(c p) n -> p c n", p=128)), writes=["Wpp"], dma="Wpp")
                    gfin = sb3("gfin", [128, D], F32)
                    p.op("sp", lambda e: e.dma_start(out=gbc[:], in_=g_ple.partition_broadcast(128)), writes=["gbc"], dma="gbc3")
                    p.op("sp", lambda e: e.dma_start(out=gfin[:], in_=g_final.partition_broadcast(128)), writes=["gfin"], dma="gfin")
                    ss3_r = Ring(sb3, "ss3", 2, [128, 1], F32)
                    u3_r = Ring(sb3, "u3", 2, [128, D], BF16)
                    pT3_r = Ring(ps3, "pT3", 2, [128, 8, 128], BF16)
                    u3T_r = Ring(sb3, "u3T", 2, [128, 8, 128], BF16)
                    pp_r = Ring(sb3, "ppl", 2, [128, 256], F32)
                    ppb_r = Ring(sb3, "ppb", 2, [128, 256], BF16)
                    ppT_r = Ring(sb3, "ppT", 2, [128, 2, 128], BF16)
                    pg_r = Ring(ps3, "pg3", 2, [128, 512], F32)
                    pj_r = Ring(ps3, "pj3", 2, [128, 512], F32)
                    sg3_r = Ring(sb3, "sg3", 2, [128, 512], F32)
                    o_r = Ring(sb3, "o3", 2, [128, D], F32)
                    def c3_tile(tile):
                        hk = "H%d" % tile
                        yield
                        ss3, kss = ss3_r.next()
                        yield
                        p.op("act", lambda e, ss3=ss3, tile=tile: e.activation(out=junk[:], in_=H[:, tile, :], func=AF.Square, accum_out=ss3[:]), reads=[hk], writes=["junkC", kss])
                        yield
                        rstd_from(ss3[:], ss3[:], D, [kss], [kss])
                        yield
                        u3, ku3 = u3_r.next()
                        yield
                        p.op("dve", lambda e, ss3=ss3, u3=u3, tile=tile: e.scalar_tensor_tensor(out=u3[:], in0=H[:, tile, :], scalar=ss3[:, 0:1], in1=gbc[:], op0=ALU.mult, op1=ALU.mult),
                             reads=[hk, kss, "gbc"], writes=[ku3])
                        yield
                        pT, kpT = pT3_r.next()
                        yield
                        for c in range(8):
                            p.op("pe", lambda e, c=c, pT=pT, u3=u3: e.transpose(out=pT[:, c, :], in_=u3[:, c * 128:(c + 1) * 128], identity=ident[:]), reads=[ku3, "ident"], writes=[kpT])
                        yield
                        u3T, ku3T = u3T_r.next()
                        yield
                        p.op("act", lambda e, pT=pT, u3T=u3T: e.copy(out=u3T[:], in_=pT[:]), reads=[kpT], writes=[ku3T])
                        yield
                        pp, kpp = pp_r.next()
                        yield
                        p.op("sp", lambda e, pp=pp, tile=tile: e.dma_start(out=pp[:], in_=po[tile * 128:(tile + 1) * 128, :]), writes=[kpp], dma=kpp)
                        yield
                        ppb, kppb = ppb_r.next()
                        yield
                        p.op("pool", lambda e, pp=pp, ppb=ppb: e.tensor_copy(out=ppb[:], in_=pp[:]), reads=[kpp], writes=[kppb])
                        yield
                        pT2, kpT2 = pT3_r.next()
                        yield
                        for c in range(2):
                            p.op("pe", lambda e, c=c, pT2=pT2, ppb=ppb: e.transpose(out=pT2[:, c, :], in_=ppb[:, c * 128:(c + 1) * 128], identity=ident[:]), reads=[kppb, "ident"], writes=[kpT2])
                        yield
                        ppT, kppT = ppT_r.next()
                        yield
                        p.op("act", lambda e, pT2=pT2, ppT=ppT: e.copy(out=ppT[:], in_=pT2[:, 0:2, :]), reads=[kpT2], writes=[kppT])
                        yield
                        for half in range(2):
                            pg, kpg = pg_r.next()
                            pj, kpj = pj_r.next()
                            for c in range(8):
                                p.op("pe", lambda e, c=c, pg=pg, u3T=u3T, half=half: e.matmul(out=pg[:], lhsT=u3T[:, c, :], rhs=Wpg[:, c, half * 512:(half + 1) * 512], start=(c == 0), stop=(c == 7)),
                                     reads=[ku3T, "Wpg"], writes=[kpg])
                            for c in range(2):
                                p.op("pe", lambda e, c=c, pj=pj, ppT=ppT, half=half: e.matmul(out=pj[:], lhsT=ppT[:, c, :], rhs=Wpp[:, c, half * 512:(half + 1) * 512], start=(c == 0), stop=(c == 1)),
                                     reads=[kppT, "Wpp"], writes=[kpj])
                            sg, ksg = sg3_r.next()
                            p.op("act", lambda e, sg=sg, pg=pg: e.activation(out=sg[:], in_=pg[:], func=AF.Sigmoid), reads=[kpg], writes=[ksg])
                            p.op("dve", lambda e, sg=sg, pj=pj: e.tensor_tensor(out=sg[:], in0=sg[:], in1=pj[:], op=ALU.mult), reads=[ksg, kpj], writes=[ksg])
                            p.op("dve", lambda e, sg=sg, tile=tile, half=half: e.tensor_tensor(out=H[:, tile, half * 512:(half + 1) * 512], in0=H[:, tile, half * 512:(half + 1) * 512], in1=sg[:], op=ALU.add),
                                 reads=[ksg, hk], writes=[hk])
                        yield
                        ss4, kss4 = ss3_r.next()
                        yield
                        p.op("act", lambda e, ss4=ss4, tile=tile: e.activation(out=junk[:], in_=H[:, tile, :], func=AF.Square, accum_out=ss4[:]), reads=[hk], writes=["junkC", kss4])
                        yield
                        rstd_from(ss4[:], ss4[:], D, [kss4], [kss4])
                        yield
                        ot, kot = o_r.next()
                        yield
                        p.op("dve", lambda e, ss4=ss4, ot=ot, tile=tile: e.scalar_tensor_tensor(out=ot[:], in0=H[:, tile, :], scalar=ss4[:, 0:1], in1=gfin[:], op0=ALU.mult, op1=ALU.mult),
                             reads=[hk, kss4, "gfin"], writes=[kot])
                        yield
                        p.op("sp", lambda e, ot=ot, tile=tile: e.dma_start(out=yo[tile * 128:(tile + 1) * 128, :], in_=ot[:]), reads=[kot], writes=["yo"], dma=kot + "s")

                    for pair in range(8):
                        gens = [c3_tile(pair * 2), c3_tile(pair * 2 + 1)]
                        while gens:
                            for g_ in list(gens):
                                try:
                                    next(g_)
                                except StopIteration:
                                    gens.remove(g_)
        p.finish_wait("sp")
        p.emit(top)
    return nc


_CACHE = {}


def _perm(j):
    idx = []
    for t in range(4):
        for blk in ORDER[j]:
            b0 = (8 * t + blk) * 128
            idx.append(np.arange(b0, b0 + 128))
    return np.concatenate(idx)


def kernel(x, p, positions, w_in, g_attn, g_cq, w_uq, g_ckv, w_ukv, g_out_mla, g_out_sb, w_o,
           g_moe, w_router, b_router, w_gu, b_gu, w_dn, b_dn, g_ple, w_ple_gate, w_ple_proj, g_final):
    if "nc" not in _CACHE:
        _CACHE["nc"] = build_program()
    nc = _CACHE["nc"]
    in_maps, perms = make_in_maps(x, p, positions, w_in, g_attn, g_cq, w_uq, g_ckv, w_ukv, g_out_mla, g_out_sb, w_o,
                                  g_moe, w_router, b_router, w_gu, b_gu, w_dn, b_dn, g_ple, w_ple_gate, w_ple_proj, g_final)
    res = run_bass_kernel_spmd(nc, in_maps, core_ids=list(range(8)))
    out = np.empty((4, S, D), np.float32)
    for c in range(8):
        b, j = c // 2, c % 2
        out[b, perms[j]] = np.asarray(res.results[c]["yo"])
    return out


def make_in_maps(x, p, positions, w_in, g_attn, g_cq, w_uq, g_ckv, w_ukv, g_out_mla, g_out_sb, w_o,
                 g_moe, w_router, b_router, w_gu, b_gu, w_dn, b_dn, g_ple, w_ple_gate, w_ple_proj, g_final):
    f = lambda a: np.ascontiguousarray(np.asarray(a))
    x = f(x); p = f(p); positions = f(positions)
    invf = np.zeros((128, 1), np.float32)
    fr = (10000.0 ** (-np.arange(0, 32, 2, dtype=np.float32) / 32.0)).astype(np.float32)
    for b_ in range(4):
        invf[b_ * 32:b_ * 32 + 16, 0] = fr
        invf[b_ * 32 + 16:b_ * 32 + 32, 0] = fr
    shared = {
        "invf": invf,
        "w_in": f(w_in[0]), "g_attn": f(g_attn[0:1]), "g_cq": f(g_cq[0:1]), "w_uq": f(w_uq[0]),
        "g_ckv": f(g_ckv[0:1]), "w_ukv": f(w_ukv[0]),
        "g_out": f(np.concatenate([np.asarray(g_out_mla[0]), np.asarray(g_out_sb[0])])[None, :]),
        "w_o": f(w_o[0]), "g_moe": f(g_moe[0:1]), "w_router": f(w_router[0]), "b_router": f(b_router[0:1]),
        "w_gu": f(w_gu[0]), "b_gu": f(np.asarray(b_gu[0]).reshape(NE * 16, 128)), "w_dn": f(w_dn[0]), "b_dn": f(b_dn[0]),
        "g_ple": f(g_ple[0:1]), "w_pg": f(w_ple_gate[0]), "w_pp": f(w_ple_proj[0]), "g_final": f(np.asarray(g_final)[None, :]),
    }
    in_maps = []
    perms = [_perm(0), _perm(1)]
    for c in range(8):
        b, j = c // 2, c % 2
        pm = perms[j]
        qr = np.concatenate([np.arange(blk * 128, blk * 128 + 128) for blk in ORDER[j]]).astype(np.float32)[None, :]
        m = dict(shared)
        m["xa"] = x[b]
        m["xo"] = f(x[b][pm])
        m["po"] = f(p[0, b][pm])
        m["posa"] = f(positions[b:b + 1].astype(np.int32))
        m["poso"] = f(positions[b:b + 1, pm].astype(np.int32))
        m["qrel"] = f(qr)
        in_maps.append(m)
    return in_maps, perms
```
